# Optimizing a Trainium2 kernel written in Bass

```python
import math
import jax, jax.numpy as jnp
from jax import lax
import numpy as np

D_MODEL = 1024
BATCH = 4
SEQ = 8192
DEPTH = 4

N_EVEN = (DEPTH + 1) // 2
N_ODD = DEPTH // 2
MIX_WIDTH = D_MODEL
A_WIDTH = MIX_WIDTH // 2
A_HEAD_DIM = 128
A_HEADS = A_WIDTH // A_HEAD_DIM
B_WIDTH = MIX_WIDTH - A_WIDTH
S5_GROUP = 16
S5_GROUPS = B_WIDTH // S5_GROUP
S5_STATE = 64
C_WIDTH = MIX_WIDTH // 2
C_HEAD_DIM = 64
C_HEADS = C_WIDTH // (2 * C_HEAD_DIM)
C_V_DIM = 2 * C_HEAD_DIM
D_WIDTH = MIX_WIDTH - C_WIDTH
D_HEAD_DIM = 128
D_HEADS = D_WIDTH // D_HEAD_DIM
CONV_WIDTH = 5
N_EXPERTS = 16
EXPERT_FF = 2 * D_MODEL
EC_CAPACITY_FACTOR = 2
REL_BUCKETS = 32
REL_MAX_DIST = 128
CHUNK = 64
Q_BLOCK = 128
EPS = 1e-6
EVEN_IN = 5 * A_WIDTH + B_WIDTH
ODD_IN = 3 * C_WIDTH + 3 * D_WIDTH + 4 * D_HEADS + D_WIDTH

kernel_name = 'hybrid_hgrn2_s5_diffattn_gdn_ecmoe_encoder'


def rmsnorm(x, g):
    xf = x.astype(jnp.float32)
    y = xf * lax.rsqrt(jnp.mean(xf * xf, axis=-1, keepdims=True) + EPS)
    return (y * g.astype(jnp.float32)).astype(x.dtype)


def l2norm(x):
    return x * lax.rsqrt(jnp.sum(x * x, axis=-1, keepdims=True) + EPS)


def _heads(t, n_heads):
    b, l, w = t.shape
    return jnp.transpose(t.reshape(b, l, n_heads, w // n_heads), (0, 2, 1, 3))


def _flip(t):
    return jnp.flip(t, axis=2)


def _to_chunks(t):
    b, h, l = t.shape[:3]
    t = t.reshape((b, h, l // CHUNK, CHUNK) + t.shape[3:])
    return jnp.moveaxis(t, 2, 0)


def _from_chunks(t):
    t = jnp.moveaxis(t, 0, 2)
    b, h, nc, c = t.shape[:4]
    return t.reshape((b, h, nc * c) + t.shape[4:])


def hgrn2_scan(q, k, v, log_f):
    tri = jnp.tril(jnp.ones((CHUNK, CHUNK), bool))[:, :, None]
    qc, kc, vc, lc = (_to_chunks(t) for t in (q, k, v, log_f))
    bc = jnp.cumsum(lc, axis=-2)

    def step(S, inp):
        q_, k_, v_, b_ = inp
        diff = b_[..., :, None, :] - b_[..., None, :, :]
        decay = jnp.where(tri, jnp.exp(jnp.where(tri, diff, 0.0)), 0.0)
        attn = jnp.einsum('bhtd,bhsd,bhtsd->bhts', q_, k_, decay)
        b_last = b_[..., -1, :]
        o = attn @ v_ + jnp.einsum('bhtd,bhdv->bhtv', q_ * jnp.exp(b_), S)
        S = jnp.exp(b_last)[..., None] * S + jnp.einsum(
            'bhsd,bhsv->bhdv', k_ * jnp.exp(b_last[..., None, :] - b_), v_)
        return S, o

    S0 = jnp.zeros(q.shape[:2] + (q.shape[-1], v.shape[-1]), jnp.float32)
    _, o = lax.scan(step, S0, (qc, kc, vc, bc))
    return _from_chunks(o)


def hgrn2_mixer(q_raw, f_fwd, f_bwd, i_raw, g_raw, lb, out_gain):
    f32 = jnp.float32
    b, l = q_raw.shape[:2]
    q = _heads(jax.nn.silu(q_raw.astype(f32)), A_HEADS)
    v = _heads(i_raw.astype(f32), A_HEADS)

    def gates(z, lbd):
        z = z.astype(f32)
        log_f = jnp.logaddexp(jnp.log(lbd), jnp.log1p(-lbd) + jax.nn.log_sigmoid(z))
        k = (1.0 - lbd) * jax.nn.sigmoid(-z)
        return _heads(log_f, A_HEADS), _heads(k, A_HEADS)

    lf_f, k_f = gates(f_fwd, lb[0])
    lf_b, k_b = gates(f_bwd, lb[1])
    o = hgrn2_scan(q, k_f, v, lf_f) + _flip(hgrn2_scan(_flip(q), _flip(k_b), _flip(v), _flip(lf_b)))
    o = rmsnorm(jnp.transpose(o, (0, 2, 1, 3)), out_gain).reshape(b, l, A_WIDTH)
    return o * jax.nn.silu(g_raw.astype(f32))


def s5_direction(u, lam_re, lam_im, log_step, b_re, b_im, c_re, c_im, reverse):
    step = jnp.exp(log_step)[:, None]
    mag = jnp.exp(lam_re * step)
    abar_re = mag * jnp.cos(lam_im * step)
    abar_im = mag * jnp.sin(lam_im * step)
    den = lam_re * lam_re + lam_im * lam_im
    fr = ((abar_re - 1.0) * lam_re + abar_im * lam_im) / den
    fi = (abar_im * lam_re - (abar_re - 1.0) * lam_im) / den
    bb_re = fr[..., None] * b_re - fi[..., None] * b_im
    bb_im = fr[..., None] * b_im + fi[..., None] * b_re
    bu_re = jnp.einsum('blgp,gnp->lbgn', u, bb_re)
    bu_im = jnp.einsum('blgp,gnp->lbgn', u, bb_im)
    l = u.shape[1]
    a_re = jnp.broadcast_to(abar_re, (l,) + abar_re.shape)
    a_im = jnp.broadcast_to(abar_im, (l,) + abar_im.shape)

    def combine(e1, e2):
        a1r, a1i, b1r, b1i = e1
        a2r, a2i, b2r, b2i = e2
        ar = a2r * a1r - a2i * a1i
        ai = a2r * a1i + a2i * a1r
        a2r_, a2i_ = a2r[:, None], a2i[:, None]
        br = a2r_ * b1r - a2i_ * b1i + b2r
        bi = a2r_ * b1i + a2i_ * b1r + b2i
        return ar, ai, br, bi

    _, _, xr, xi = lax.associative_scan(combine, (a_re, a_im, bu_re, bu_im), reverse=reverse, axis=0)
    return jnp.einsum('lbgn,gpn->blgp', xr, c_re) - jnp.einsum('lbgn,gpn->blgp', xi, c_im)


def s5_mixer(u_raw, lam_re, lam_im, log_step, b_re, b_im, c_re, c_im, d_skip, glu_w, glu_b):
    f32 = jnp.float32
    b, l = u_raw.shape[:2]
    u = u_raw.astype(f32).reshape(b, l, S5_GROUPS, S5_GROUP)
    y = d_skip.astype(f32).reshape(S5_GROUPS, S5_GROUP) * u
    for direction in range(2):
        y = y + s5_direction(u, lam_re[direction].astype(f32), lam_im[direction].astype(f32),
                             log_step[direction].astype(f32), b_re[direction].astype(f32),
                             b_im[direction].astype(f32), c_re[direction].astype(f32),
                             c_im[direction].astype(f32), reverse=(direction == 1))
    y = jax.nn.gelu(y.reshape(b, l, B_WIDTH))
    return y * jax.nn.sigmoid(y @ glu_w.astype(f32) + glu_b.astype(f32))


def t5_bucket(rel):
    half = REL_BUCKETS // 2
    max_exact = half // 2
    base = jnp.where(rel > 0, half, 0)
    n = jnp.abs(rel)
    nf = jnp.maximum(n, 1).astype(jnp.float32)
    large = max_exact + (jnp.log(nf / max_exact) / math.log(REL_MAX_DIST / max_exact)
                         * (half - max_exact)).astype(jnp.int32)
    large = jnp.minimum(large, half - 1)
    return base + jnp.where(n < max_exact, n, large)


def diff_attention(q_raw, k_raw, v_raw, q_gain, k_gain, lam, out_gain, rel_bias, layer_idx):
    f32 = jnp.float32
    b, l = q_raw.shape[:2]
    q = rmsnorm(q_raw.astype(f32).reshape(b, l, C_HEADS, 2, C_HEAD_DIM), q_gain) * (C_HEAD_DIM ** -0.5)
    k = rmsnorm(k_raw.astype(f32).reshape(b, l, C_HEADS, 2, C_HEAD_DIM), k_gain)
    v = v_raw.astype(f32).reshape(b, l, C_HEADS, C_V_DIM)
    lam_init = 0.8 - 0.6 * math.exp(-0.3 * layer_idx)
    lam_f = lam.astype(f32)
    lam_full = jnp.exp(jnp.sum(lam_f[0] * lam_f[1])) - jnp.exp(jnp.sum(lam_f[2] * lam_f[3])) + lam_init
    n_blocks = l // Q_BLOCK
    qb = jnp.moveaxis(q.reshape(b, n_blocks, Q_BLOCK, C_HEADS, 2, C_HEAD_DIM), 1, 0)
    k_pos = jnp.arange(l)
    table = rel_bias.astype(f32)

    def block(args):
        q_blk, blk = args
        q_pos = blk * Q_BLOCK + jnp.arange(Q_BLOCK)
        bias = jnp.transpose(table[t5_bucket(k_pos[None, :] - q_pos[:, None])], (2, 0, 1))
        s = jnp.einsum('bqhcd,bkhcd->bchqk', q_blk, k) + bias[None, None]
        p = jax.nn.softmax(s, axis=-1)
        w = p[:, 0] - lam_full * p[:, 1]
        return jnp.einsum('bhqk,bkhv->bqhv', w, v)

    o = lax.map(block, (qb, jnp.arange(n_blocks)))
    o = jnp.moveaxis(o, 0, 1).reshape(b, l, C_HEADS, C_V_DIM)
    o = rmsnorm(o, out_gain) * (1.0 - lam_init)
    return o.reshape(b, l, C_WIDTH)


def short_conv(x, w):
    ch = x.shape[-1]
    pad = CONV_WIDTH // 2
    return lax.conv_general_dilated(x, w[:, None, :].astype(x.dtype), window_strides=(1,),
                                    padding=[(pad, pad)], dimension_numbers=('NWC', 'WIO', 'NWC'),
                                    feature_group_count=ch)


def gdn_scan(q, k, v, g, beta):
    b, h, l, dk = q.shape
    dv = v.shape[-1]
    nc = l // CHUNK

    def chunks(t):
        return t.reshape((b, h, nc, CHUNK) + t.shape[3:])

    q, k, v, g, beta = (chunks(t) for t in (q, k, v, g, beta))
    gam = jnp.cumsum(g, axis=-1)
    incl = jnp.tril(jnp.ones((CHUNK, CHUNK), bool))
    strict = jnp.tril(jnp.ones((CHUNK, CHUNK), bool), -1)
    diff = gam[..., :, None] - gam[..., None, :]
    decay = jnp.where(incl, jnp.exp(jnp.where(incl, diff, 0.0)), 0.0)
    k_beta = k * beta[..., None]
    v_beta = v * beta[..., None]
    a = jnp.where(strict, jnp.einsum('bhntd,bhnsd->bhnts', k_beta, k) * decay, 0.0)
    m = a + jnp.eye(CHUNK, dtype=a.dtype)
    value = lax.linalg.triangular_solve(m, v_beta, left_side=True, lower=True, unit_diagonal=True)
    k_cum = lax.linalg.triangular_solve(m, k_beta * jnp.exp(gam)[..., None], left_side=True,
                                        lower=True, unit_diagonal=True)
    attn = jnp.einsum('bhntd,bhnsd->bhnts', q, k) * decay
    q_dec = q * jnp.exp(gam)[..., None]
    g_last = gam[..., -1]
    k_dec = k * jnp.exp(g_last[..., None] - gam)[..., None]
    xs = tuple(jnp.moveaxis(t, 2, 0) for t in (value, k_cum, attn, q_dec, k_dec, g_last))

    def step(S, inp):
        val, kc, at, qd, kd, gl = inp
        v_new = val - kc @ S
        o = qd @ S + at @ v_new
        S = jnp.exp(gl)[..., None, None] * S + jnp.einsum('bhsd,bhsv->bhdv', kd, v_new)
        return S, o

    S0 = jnp.zeros((b, h, dk, dv), jnp.float32)
    _, o = lax.scan(step, S0, xs)
    return jnp.moveaxis(o, 0, 2).reshape(b, h, l, dv)


def gated_deltanet(qkv_raw, a_raw, b_raw, g_raw, conv_w, a_log, dt_bias, out_gain):
    f32 = jnp.float32
    bsz, l = qkv_raw.shape[:2]
    qkv = jax.nn.silu(short_conv(qkv_raw, conv_w).astype(f32))
    q, k, v = jnp.split(qkv, 3, axis=-1)
    q = l2norm(_heads(q, D_HEADS)) * (D_HEAD_DIM ** -0.5)
    k = l2norm(_heads(k, D_HEADS))
    v = _heads(v, D_HEADS)
    a = a_raw.astype(f32).reshape(bsz, l, 2, D_HEADS)
    bb = b_raw.astype(f32).reshape(bsz, l, 2, D_HEADS)
    a_log = a_log.astype(f32)
    dt_bias = dt_bias.astype(f32)
    o = jnp.zeros((bsz, D_HEADS, l, D_HEAD_DIM), f32)
    for direction in range(2):
        g = -jnp.exp(a_log[direction]) * jax.nn.softplus(a[:, :, direction] + dt_bias[direction])
        beta = jax.nn.sigmoid(bb[:, :, direction])
        g = jnp.transpose(g, (0, 2, 1))
        beta = jnp.transpose(beta, (0, 2, 1))
        if direction == 0:
            o = o + gdn_scan(q, k, v, g, beta)
        else:
            o = o + _flip(gdn_scan(_flip(q), _flip(k), _flip(v), _flip(g), _flip(beta)))
    o = rmsnorm(jnp.transpose(o, (0, 2, 1, 3)), out_gain).reshape(bsz, l, D_WIDTH)
    return o * jax.nn.silu(g_raw.astype(f32))


def ec_moe(h, w_router, w_gate, w_up, w_down):
    b, l, d = h.shape
    cap = EC_CAPACITY_FACTOR * l // N_EXPERTS
    logits = jnp.einsum('bld,de->ble', h, w_router).astype(jnp.float32)
    aff = jax.nn.softmax(logits, axis=-1)
    gate, idx = lax.top_k(jnp.swapaxes(aff, 1, 2), cap)
    xs = jax.vmap(lambda hb, ib: hb[ib])(h, idx)
    hid = jax.nn.silu(jnp.einsum('becd,edf->becf', xs, w_gate)) * jnp.einsum('becd,edf->becf', xs, w_up)
    out = jnp.einsum('becf,efd->becd', hid, w_down) * gate[..., None].astype(h.dtype)
    return jax.vmap(lambda ob, ib: jnp.zeros((l, d), ob.dtype).at[ib.reshape(-1)].add(ob.reshape(-1, d)))(out, idx)


def setup_inputs(seed: int = 0) -> dict:
    key = jax.random.key(seed)
    ks = jax.random.split(key, 32)
    nrm = jax.random.normal
    f32 = jnp.float32
    x = nrm(ks[0], (BATCH, SEQ, D_MODEL), f32)
    mix_norm = 1.0 + 0.02 * nrm(ks[1], (DEPTH, D_MODEL), f32)
    ffn_norm = 1.0 + 0.02 * nrm(ks[2], (DEPTH, D_MODEL), f32)
    ev_w_in = nrm(ks[3], (N_EVEN, D_MODEL, EVEN_IN), f32) * D_MODEL ** -0.5
    ev_w_out = nrm(ks[4], (N_EVEN, MIX_WIDTH, D_MODEL), f32) * MIX_WIDTH ** -0.5
    a_lb_logits = 0.1 * nrm(ks[5], (N_EVEN, 2, A_WIDTH), f32)
    a_out_norm = 1.0 + 0.02 * nrm(ks[6], (N_EVEN, A_HEAD_DIM), f32)
    s5_shape = (N_EVEN, 2, S5_GROUPS, S5_STATE)
    s5_lambda_re = -0.5 + 0.01 * nrm(ks[7], s5_shape, f32)
    s5_lambda_im = math.pi * jnp.arange(S5_STATE, dtype=f32) + 0.01 * nrm(ks[8], s5_shape, f32)
    s5_log_step = jax.random.uniform(ks[9], (N_EVEN, 2, S5_GROUPS), f32, math.log(1e-3), math.log(1e-1))
    s5_b_re = nrm(ks[10], s5_shape + (S5_GROUP,), f32) * (2 * S5_GROUP) ** -0.5
    s5_b_im = nrm(ks[11], s5_shape + (S5_GROUP,), f32) * (2 * S5_GROUP) ** -0.5
    c_shape = (N_EVEN, 2, S5_GROUPS, S5_GROUP, S5_STATE)
    s5_c_re = nrm(ks[12], c_shape, f32) * S5_STATE ** -0.5
    s5_c_im = nrm(ks[13], c_shape, f32) * S5_STATE ** -0.5
    s5_d = 0.5 * nrm(ks[14], (N_EVEN, B_WIDTH), f32)
    s5_glu_w = nrm(ks[15], (N_EVEN, B_WIDTH, B_WIDTH), f32) * B_WIDTH ** -0.5
    s5_glu_b = 0.01 * nrm(ks[16], (N_EVEN, B_WIDTH), f32)
    od_w_in = nrm(ks[17], (N_ODD, D_MODEL, ODD_IN), f32) * D_MODEL ** -0.5
    od_w_out = nrm(ks[18], (N_ODD, MIX_WIDTH, D_MODEL), f32) * MIX_WIDTH ** -0.5
    c_q_norm = 1.0 + 0.02 * nrm(ks[19], (N_ODD, C_HEAD_DIM), f32)
    c_k_norm = 1.0 + 0.02 * nrm(ks[20], (N_ODD, C_HEAD_DIM), f32)
    c_lambda = 0.1 * nrm(ks[21], (N_ODD, 4, C_HEAD_DIM), f32)
    c_out_norm = 1.0 + 0.02 * nrm(ks[22], (N_ODD, C_V_DIM), f32)
    rel_bias = 0.5 * nrm(ks[23], (REL_BUCKETS, C_HEADS), f32)
    d_conv_w = nrm(ks[24], (N_ODD, CONV_WIDTH, 3 * D_WIDTH), f32) * CONV_WIDTH ** -0.5
    d_a_log = jnp.log(jax.random.uniform(ks[25], (N_ODD, 2, D_HEADS), f32, 1.0, 16.0))
    dt = jnp.exp(jax.random.uniform(ks[26], (N_ODD, 2, D_HEADS), f32, math.log(1e-3), math.log(1e-1)))
    d_dt_bias = dt + jnp.log(-jnp.expm1(-dt))
    d_out_norm = 1.0 + 0.02 * nrm(ks[27], (N_ODD, D_HEAD_DIM), f32)
    moe_router = nrm(ks[28], (DEPTH, D_MODEL, N_EXPERTS), f32) * D_MODEL ** -0.5
    moe_w_gate = nrm(ks[29], (DEPTH, N_EXPERTS, D_MODEL, EXPERT_FF), f32) * D_MODEL ** -0.5
    moe_w_up = nrm(ks[30], (DEPTH, N_EXPERTS, D_MODEL, EXPERT_FF), f32) * D_MODEL ** -0.5
    moe_w_down = nrm(ks[31], (DEPTH, N_EXPERTS, EXPERT_FF, D_MODEL), f32) * EXPERT_FF ** -0.5
    return {'x': x, 'mix_norm': mix_norm, 'ffn_norm': ffn_norm, 'ev_w_in': ev_w_in, 'ev_w_out': ev_w_out,
            'a_lb_logits': a_lb_logits, 'a_out_norm': a_out_norm, 's5_lambda_re': s5_lambda_re,
            's5_lambda_im': s5_lambda_im, 's5_log_step': s5_log_step, 's5_b_re': s5_b_re, 's5_b_im': s5_b_im,
            's5_c_re': s5_c_re, 's5_c_im': s5_c_im, 's5_d': s5_d, 's5_glu_w': s5_glu_w, 's5_glu_b': s5_glu_b,
            'od_w_in': od_w_in, 'od_w_out': od_w_out, 'c_q_norm': c_q_norm, 'c_k_norm': c_k_norm,
            'c_lambda': c_lambda, 'c_out_norm': c_out_norm, 'rel_bias': rel_bias, 'd_conv_w': d_conv_w,
            'd_a_log': d_a_log, 'd_dt_bias': d_dt_bias, 'd_out_norm': d_out_norm, 'moe_router': moe_router,
            'moe_w_gate': moe_w_gate, 'moe_w_up': moe_w_up, 'moe_w_down': moe_w_down}


def reference(x, mix_norm, ffn_norm, ev_w_in, ev_w_out, a_lb_logits, a_out_norm, s5_lambda_re,
              s5_lambda_im, s5_log_step, s5_b_re, s5_b_im, s5_c_re, s5_c_im, s5_d, s5_glu_w, s5_glu_b,
              od_w_in, od_w_out, c_q_norm, c_k_norm, c_lambda, c_out_norm, rel_bias, d_conv_w,
              d_a_log, d_dt_bias, d_out_norm, moe_router, moe_w_gate, moe_w_up, moe_w_down):
    p = jax.nn.softmax(a_lb_logits.astype(jnp.float32), axis=0)
    cum = jnp.cumsum(p, axis=0)
    lower_bounds = cum - cum[0:1]
    for layer in range(DEPTH):
        h = rmsnorm(x, mix_norm[layer])
        j = layer // 2
        if layer % 2 == 0:
            proj = h @ ev_w_in[j]
            q_a, f_fw, f_bw, i_a, g_a, u_b = jnp.split(
                proj, [A_WIDTH, 2 * A_WIDTH, 3 * A_WIDTH, 4 * A_WIDTH, 5 * A_WIDTH], axis=-1)
            o_a = hgrn2_mixer(q_a, f_fw, f_bw, i_a, g_a, lower_bounds[j], a_out_norm[j])
            o_b = s5_mixer(u_b, s5_lambda_re[j], s5_lambda_im[j], s5_log_step[j], s5_b_re[j], s5_b_im[j],
                           s5_c_re[j], s5_c_im[j], s5_d[j], s5_glu_w[j], s5_glu_b[j])
            mixed = jnp.concatenate([o_a, o_b], axis=-1).astype(x.dtype)
            x = x + mixed @ ev_w_out[j]
        else:
            o1 = 3 * C_WIDTH
            o2 = o1 + 3 * D_WIDTH
            o3 = o2 + 2 * D_HEADS
            o4 = o3 + 2 * D_HEADS
            q_c, k_c, v_c, qkv_d, a_d, b_d, g_d = jnp.split(
                h @ od_w_in[j], [C_WIDTH, 2 * C_WIDTH, o1, o2, o3, o4], axis=-1)
            o_c = diff_attention(q_c, k_c, v_c, c_q_norm[j], c_k_norm[j], c_lambda[j], c_out_norm[j],
                                 rel_bias, layer)
            o_d = gated_deltanet(qkv_d, a_d, b_d, g_d, d_conv_w[j], d_a_log[j], d_dt_bias[j], d_out_norm[j])
            mixed = jnp.concatenate([o_c, o_d], axis=-1).astype(x.dtype)
            x = x + mixed @ od_w_out[j]
        x = x + ec_moe(rmsnorm(x, ffn_norm[layer]), moe_router[layer], moe_w_gate[layer],
                       moe_w_up[layer], moe_w_down[layer])
    return x
```

```python
import math
from contextlib import ExitStack

import numpy as np
import concourse.bass as bass
import concourse.mybir as mybir
from concourse.bass_utils import run_bass_kernel_spmd

F32 = mybir.dt.float32
BF16 = mybir.dt.bfloat16
U32 = mybir.dt.uint32
I32 = mybir.dt.int32
AF = mybir.ActivationFunctionType
ALU = mybir.AluOpType
AX = mybir.AxisListType


class Buf:
    __slots__ = ("name", "w", "r", "pw")

    def __init__(self, name=""):
        self.name = name
        self.w = {}
        self.r = {}
        self.pw = {}

    def seal(self):
        for k, v in self.pw.items():
            if self.w.get(k, 0) < v:
                self.w[k] = v
        self.pw = {}


class Ctx:
    NDMA = 8

    def __init__(self, nc, same_engine_sync=True):
        self.nc = nc
        self.E = dict(pe=nc.tensor, dve=nc.vector, act=nc.scalar, pool=nc.gpsimd, sp=nc.sync)
        self.sem = {}
        self.cnt = {}
        for k in self.E:
            self.sem[k] = nc.alloc_semaphore("c_" + k)
            self.cnt[k] = 0
        self.dslot = {}
        for q in ("sp", "act", "pool"):
            for i in range(self.NDMA):
                key = "d_%s%d" % (q, i)
                self.sem[key] = nc.alloc_semaphore(key)
                self.cnt[key] = 0
            self.dslot[q] = 0
        self.seen = {k: {} for k in self.E}
        self.same = same_engine_sync
        self.ninst = 0

    def _wait(self, eng, tok):
        if tok is None:
            return
        key, val = tok
        if key == eng and (eng == "pe" or not self.same):
            return
        if self.seen[eng].get(key, 0) >= val:
            return
        self.E[eng].wait_ge(self.sem[key], val)
        self.seen[eng][key] = val

    def _deps(self, eng, reads, writes, pwrites=()):
        for b in reads:
            for k, v in b.w.items():
                self._wait(eng, (k, v))
            for k, v in b.pw.items():
                self._wait(eng, (k, v))
        for b in writes:
            for d in (b.w, b.pw, b.r):
                for k, v in d.items():
                    self._wait(eng, (k, v))
        for b in pwrites:
            for d in (b.w, b.r):
                for k, v in d.items():
                    self._wait(eng, (k, v))

    def _commit(self, tok, reads, writes, pwrites=()):
        k, v = tok
        for b in writes:
            b.w = {k: v}
            b.pw = {}
            b.r = {}
        for b in pwrites:
            if b.pw.get(k, 0) < v:
                b.pw[k] = v
        for b in reads:
            if b.r.get(k, 0) < v:
                b.r[k] = v

    def op(self, eng, fn, reads=(), writes=(), pwrites=()):
        self._deps(eng, reads, writes, pwrites)
        inst = fn(self.E[eng])
        self.cnt[eng] += 1
        tok = (eng, self.cnt[eng])
        inst.then_inc(self.sem[eng], 1)
        self._commit(tok, reads, writes, pwrites)
        self.ninst += 1
        return tok

    def dma(self, q, fn, reads=(), writes=(), pwrites=()):
        self._deps(q, reads, writes, pwrites)
        i = self.dslot[q]
        self.dslot[q] = (i + 1) % self.NDMA
        key = "d_%s%d" % (q, i)
        if self.cnt[key] > 0:
            self._wait(q, (key, self.cnt[key]))
        inst = fn(self.E[q])
        self.cnt[key] += 16
        tok = (key, self.cnt[key])
        inst.then_inc(self.sem[key], 16)
        self._commit(tok, reads, writes, pwrites)
        self.ninst += 1
        return tok

    def finish(self, bufs):
        for b in bufs:
            for d in (b.w, b.pw):
                for k, v in d.items():
                    self._wait("sp", (k, v))


def barrier(c):
    toks = [(k, v) for k, v in c.cnt.items() if v > 0]
    for eng in c.E:
        for tok in toks:
            c._wait(eng, tok)


_UNIQ = [0]


def uniq(name):
    _UNIQ[0] += 1
    return "%s_u%d" % (name, _UNIQ[0])


L = 8192
D = 1024
NE = 16
FF = 2048
CAP = 1024
NT = L // 128


class G:
    pass


def alloc_T(nc, name, shape, dtype, n=1, es=None):
    if es is None:
        return [(nc.alloc_sbuf_tensor("%s_%d" % (name, i), shape, dtype), Buf(name)) for i in range(n)]
    return [(es.enter_context(nc.sbuf_tensor(uniq("%s_%d" % (name, i)), shape, dtype)), Buf(name)) for i in range(n)]


def setup_consts(c, g):
    nc = c.nc
    g.ident32 = nc.alloc_sbuf_tensor("ident32", [128, 128], F32); g.b_ident32 = Buf()
    g.identb = nc.alloc_sbuf_tensor("identb", [128, 128], BF16); g.b_identb = Buf()
    g.ones32 = nc.alloc_sbuf_tensor("ones32", [1, 128], F32); g.b_ones32 = Buf()
    g.neghalf = nc.alloc_sbuf_tensor("neghalf", [128, 1], F32); g.b_neghalf = Buf()
    for t, b in ((g.ident32, g.b_ident32), (g.identb, g.b_identb)):
        c.op('pool', lambda e: e.memset(t[:], 0.0), writes=[b])
        c.op('pool', lambda e: e.affine_select(out=t[:], in_=t[:], pattern=[[-1, 128]], compare_op=ALU.not_equal,
                                               fill=1.0, base=0, channel_multiplier=1), reads=[b], writes=[b])
    g.J32 = nc.alloc_sbuf_tensor("J32", [128, 128], F32); g.b_J32 = Buf()
    g.Jb = nc.alloc_sbuf_tensor("Jb", [128, 128], BF16); g.b_Jb = Buf()
    for t, b in ((g.J32, g.b_J32), (g.Jb, g.b_Jb)):
        c.op('pool', lambda e: e.memset(t[:], 0.0), writes=[b])
        c.op('pool', lambda e: e.affine_select(out=t[:], in_=t[:], pattern=[[1, 128]], compare_op=ALU.not_equal,
                                               fill=1.0, base=-127, channel_multiplier=1), reads=[b], writes=[b])
    c.op('pool', lambda e: e.memset(g.ones32[:], 1.0), writes=[g.b_ones32])
    c.op('pool', lambda e: e.memset(g.neghalf[:], -0.5), writes=[g.b_neghalf])
    g.ps = []
    for i in range(7):
        g.ps.append((nc.alloc_psum_tensor("ps%d" % i, [128, 512], F32), Buf("ps%d" % i)))
    g.psb = (nc.alloc_psum_tensor("psb", [128, 1024], BF16), Buf("psb"))


def bcast_row(c, g, dst, bdst, src_ap, n, tmp, btmp, psi=6):
    c.dma('sp', lambda e: e.dma_start(out=tmp[0:1, 0:n], in_=src_ap.rearrange("(o n) -> o n", o=1)), writes=[btmp])
    ps, bps = g.ps[psi]
    for h in range(0, n, 512):
        w = min(512, n - h)
        c.op('pe', lambda e: e.matmul(ps[:, 0:w], lhsT=g.ones32[0:1, :], rhs=tmp[0:1, h:h + w], start=True, stop=True),
             reads=[g.b_ones32, btmp], writes=[bps])
        c.op('dve', lambda e: e.tensor_copy(out=dst[:, h:h + w], in_=ps[:, 0:w]), reads=[bps], writes=[bdst])


def rmsnorm_tile(c, g, xt, bxt, gB, bgB, h32, bh32, junk, bjunk, ss, bss):
    c.op('dve', lambda e: e.scalar_tensor_tensor(out=junk[:], in0=xt[:], scalar=1.0, in1=xt[:], op0=ALU.mult, op1=ALU.mult,
                                                 accum_out=ss[:, 0:1]), reads=[bxt], writes=[bjunk, bss])
    c.op('dve', lambda e: e.tensor_scalar(out=ss[:, 0:1], in0=ss[:, 0:1], scalar1=1.0 / D, scalar2=1e-6, op0=ALU.mult, op1=ALU.add),
         reads=[bss], writes=[bss])
    c.op('pool', lambda e: e.tensor_tensor(out=ss[:, 0:1], in0=ss[:, 0:1], in1=g.neghalf[:, 0:1], op=ALU.pow),
         reads=[bss, g.b_neghalf], writes=[bss])
    c.op('dve', lambda e: e.scalar_tensor_tensor(out=h32[:], in0=xt[:], scalar=ss[:, 0:1], in1=gB[:], op0=ALU.mult, op1=ALU.mult),
         reads=[bxt, bss, bgB], writes=[bh32])


def moe_phase(c, g, X, bX, HB, bHB, ffn_g, w_router, w_gate, w_up, w_down, sb, stage=3):
    nc = c.nc
    gB, bgB = sb['gB']
    tmpr, btmpr = sb['tmprow']
    bcast_row(c, g, gB, bgB, ffn_g, D, tmpr, btmpr)
    wr, bwr = sb['wr']
    c.dma('sp', lambda e: e.dma_start(out=wr[:], in_=w_router.rearrange("(k p) e -> p k e", p=128)), writes=[bwr])
    affT, baffT = sb['affT']
    for i in range(NT):
        xt, bxt = sb['xt'][i % 2]
        h32, bh32 = sb['h32'][i % 2]
        hb, bhb = sb['hb'][i % 2]
        junk, bjunk = sb['junk']
        ss, bss = sb['ss'][i % 2]
        c.dma('sp', lambda e: e.dma_start(out=xt[:], in_=X[i * 128:(i + 1) * 128, :]), reads=[bX], writes=[bxt])
        rmsnorm_tile(c, g, xt, bxt, gB, bgB, h32, bh32, junk, bjunk, ss, bss)
        c.op('act', lambda e: e.activation(out=hb[:], in_=h32[:], func=AF.Copy), reads=[bh32], writes=[bhb])
        c.dma('act', lambda e: e.dma_start(out=HB[i * 128:(i + 1) * 128, :], in_=hb[:]), reads=[bhb], pwrites=[bHB])
        hT, bhT = sb['hT32'][i % 2]
        for hh in range(2):
            ps, bps = g.ps[hh]
            for k in range(4):
                kk = hh * 4 + k
                c.op('pe', lambda e: e.transpose(out=ps[:, k * 128:(k + 1) * 128], in_=h32[:, kk * 128:(kk + 1) * 128],
                                                 identity=g.ident32[:]), reads=[bh32, g.b_ident32], writes=[bps])
            c.op('act', lambda e: e.activation(out=hT[:, hh * 512:(hh + 1) * 512], in_=ps[:, :], func=AF.Copy),
                 reads=[bps], writes=[bhT])
        pl, bpl = g.ps[2 + (i % 2)]
        for k in range(8):
            c.op('pe', lambda e: e.matmul(pl[:, 0:NE], lhsT=hT[:, k * 128:(k + 1) * 128], rhs=wr[:, k, :], start=(k == 0), stop=(k == 7)),
                 reads=[bhT, bwr], writes=[bpl])
        sm, bsm = sb['sm'][i % 2]
        ex, bex = sb['ex'][i % 2]
        c.op('dve', lambda e: e.tensor_reduce(out=sm[:, 0:1], in_=pl[:, 0:NE], axis=AX.X, op=ALU.max), reads=[bpl], writes=[bsm])
        c.op('dve', lambda e: e.tensor_scalar(out=sm[:, 0:1], in0=sm[:, 0:1], scalar1=-1.0, scalar2=None, op0=ALU.mult), reads=[bsm], writes=[bsm])
        c.op('act', lambda e: e.activation(out=ex[:], in_=pl[:, 0:NE], func=AF.Exp, bias=sm[:, 0:1], scale=1.0, accum_out=sm[:, 1:2]),
             reads=[bpl, bsm], writes=[bex, bsm])
        c.op('dve', lambda e: e.reciprocal(out=sm[:, 2:3], in_=sm[:, 1:2]), reads=[bsm], writes=[bsm])
        c.op('dve', lambda e: e.tensor_scalar(out=ex[:], in0=ex[:], scalar1=sm[:, 2:3], scalar2=None, op0=ALU.mult), reads=[bex, bsm], writes=[bex])
        pt, bpt = g.ps[4 + (i % 2)]
        c.op('pe', lambda e: e.transpose(out=pt[0:NE, 0:128], in_=ex[:, 0:NE], identity=g.ident32[:]), reads=[bex, g.b_ident32], writes=[bpt])
        c.op('act', lambda e: e.activation(out=affT[0:NE, i * 128:(i + 1) * 128], in_=pt[0:NE, 0:128], func=AF.Copy), reads=[bpt], writes=[baffT])
    bHB.seal()
    if stage < 2:
        return
    vals, bvals = sb['vals']
    idxu, bidxu = sb['idxu']
    for it in range(CAP // 8):
        sl = slice(it * 8, it * 8 + 8)
        c.op('dve', lambda e: e.max(out=vals[:, sl], in_=affT[:, :]), reads=[baffT], writes=[bvals])
        c.op('dve', lambda e: e.max_index(out=idxu[:, sl], in_max=vals[:, sl], in_values=affT[:, :]), reads=[baffT, bvals], writes=[bidxu])
        c.op('dve', lambda e: e.match_replace(out=affT[:, :], in_to_replace=vals[:, sl], in_values=affT[:, :], imm_value=-1.0),
             reads=[bvals, baffT], writes=[baffT])
    idxf, bidxf = sb['idxf']
    c.op('dve', lambda e: e.tensor_copy(out=idxf[:], in_=idxu[:]), reads=[bidxu], writes=[bidxf])
    idxT, bidxT = sb['idxT']
    gateT, bgateT = sb['gateT']
    for j in range(8):
        pt, bpt = g.ps[4 + (j % 2)]
        c.op('pe', lambda e: e.transpose(out=pt[:, 0:NE], in_=idxf[0:NE, j * 128:(j + 1) * 128], identity=g.ident32[0:NE, 0:NE]),
             reads=[bidxf, g.b_ident32], writes=[bpt])
        c.op('dve', lambda e: e.tensor_copy(out=idxT[:, j, :], in_=pt[:, 0:NE]), reads=[bpt], writes=[bidxT])
        pt2, bpt2 = g.ps[2 + (j % 2)]
        c.op('pe', lambda e: e.transpose(out=pt2[:, 0:NE], in_=vals[0:NE, j * 128:(j + 1) * 128], identity=g.ident32[0:NE, 0:NE]),
             reads=[bvals, g.b_ident32], writes=[bpt2])
        c.op('act', lambda e: e.activation(out=gateT[:, j, :], in_=pt2[:, 0:NE], func=AF.Copy), reads=[bpt2], writes=[bgateT])
    if stage < 3:
        return
    xsT, bxsT = sb['xsT']
    hidT, bhidT = sb['hidT']
    yacc, byacc = sb['yacc']
    ptb, bptb = g.psb
    qi = 0
    for ex_i in range(NE):
        for j in range(8):
            xs, bxs = sb['xs'][j % 2]
            c.dma('pool', lambda e: e.indirect_dma_start(out=xs[:, :], out_offset=None, in_=HB[:, :],
                                                        in_offset=bass.IndirectOffsetOnAxis(ap=idxT[:, j, ex_i:ex_i + 1], axis=0)),
                  reads=[bHB, bidxT], writes=[bxs])
            for k in range(8):
                c.op('pe', lambda e: e.transpose(out=ptb[:, k * 128:(k + 1) * 128], in_=xs[:, k * 128:(k + 1) * 128], identity=g.identb[:]),
                     reads=[bxs, g.b_identb], writes=[bptb])
            c.op('dve', lambda e: e.tensor_copy(out=xsT[:, :, j * 128:(j + 1) * 128], in_=ptb[:, :].rearrange("p (k s) -> p k s", k=8)),
                 reads=[bptb], writes=[bxsT])
        for q in range(4):
            wg, bwg = sb['wg'][qi % 2]
            wu, bwu = sb['wu'][qi % 2]
            wd, bwd = sb['wd'][qi % 2]
            qi += 1
            f0 = q * 512
            c.dma('pool', lambda e: e.dma_start(out=wg[:], in_=w_gate[ex_i, :, f0:f0 + 512].rearrange("(k p) f -> p k f", p=128)), writes=[bwg])
            c.dma('pool', lambda e: e.dma_start(out=wu[:], in_=w_up[ex_i, :, f0:f0 + 512].rearrange("(k p) f -> p k f", p=128)), writes=[bwu])
            c.dma('pool', lambda e: e.dma_start(out=wd[:], in_=w_down[ex_i, f0:f0 + 512, :].rearrange("(k p) d -> p k d", p=128)), writes=[bwd])
            n = 0
            for fc in range(4):
                for sh in range(2):
                    pg, bpg = g.ps[0 + (n % 2)]
                    pu, bpu = g.ps[2 + (n % 2)]
                    sg, bsg = sb['sg'][n % 2]
                    n += 1
                    for k in range(8):
                        c.op('pe', lambda e: e.matmul(pg[:, :], lhsT=wg[:, k, fc * 128:(fc + 1) * 128], rhs=xsT[:, k, sh * 512:(sh + 1) * 512],
                                                      start=(k == 0), stop=(k == 7)), reads=[bwg, bxsT], writes=[bpg])
                    for k in range(8):
                        c.op('pe', lambda e: e.matmul(pu[:, :], lhsT=wu[:, k, fc * 128:(fc + 1) * 128], rhs=xsT[:, k, sh * 512:(sh + 1) * 512],
                                                      start=(k == 0), stop=(k == 7)), reads=[bwu, bxsT], writes=[bpu])
                    c.op('act', lambda e: e.activation(out=sg[:], in_=pg[:, :], func=AF.Silu), reads=[bpg], writes=[bsg])
                    c.op('dve', lambda e: e.tensor_tensor(out=hidT[:, fc, sh * 512:(sh + 1) * 512], in0=sg[:], in1=pu[:, :], op=ALU.mult),
                         reads=[bsg, bpu], writes=[bhidT])
            m = 0
            for j in range(8):
                for dh in range(2):
                    py, bpy = g.ps[4 + (m % 2)]
                    m += 1
                    for fc in range(4):
                        c.op('pe', lambda e: e.matmul(py[:, :], lhsT=hidT[:, fc, j * 128:(j + 1) * 128], rhs=wd[:, fc, dh * 512:(dh + 1) * 512],
                                                      start=(fc == 0), stop=(fc == 3)), reads=[bhidT, bwd], writes=[bpy])
                    ysl = yacc[:, j, dh * 512:(dh + 1) * 512]
                    gsc = gateT[:, j, ex_i:ex_i + 1]
                    if q == 0:
                        c.op('dve', lambda e: e.tensor_scalar(out=ysl, in0=py[:, :], scalar1=gsc, scalar2=None, op0=ALU.mult),
                             reads=[bpy, bgateT], writes=[byacc])
                    else:
                        c.op('dve', lambda e: e.scalar_tensor_tensor(out=ysl, in0=py[:, :], scalar=gsc, in1=ysl, op0=ALU.mult, op1=ALU.add),
                             reads=[bpy, bgateT, byacc], writes=[byacc])
        for j in range(8):
            c.dma('pool', lambda e: e.indirect_dma_start(out=X[:, :], out_offset=bass.IndirectOffsetOnAxis(ap=idxT[:, j, ex_i:ex_i + 1], axis=0),
                                                        in_=yacc[:, j, :], in_offset=None, compute_op=ALU.add),
                  reads=[byacc, bidxT], pwrites=[bX])
        bX.seal()


def moe_alloc(nc, es=None):
    sb = {}
    sb['gB'] = alloc_T(nc, 'gB', [128, D], F32, 1, es=es)[0]
    sb['tmprow'] = alloc_T(nc, 'tmprow', [1, 1024], F32, 1, es=es)[0]
    sb['wr'] = alloc_T(nc, 'wr', [128, 8, NE], F32, 1, es=es)[0]
    big = es.enter_context(nc.sbuf_tensor(uniq('big'), [128, L], F32)) if es is not None else nc.alloc_sbuf_tensor('big', [128, L], F32); bbig = Buf('big')
    sb['affT'] = (big[0:NE, :], bbig)
    sb['xt'] = alloc_T(nc, 'xt', [128, D], F32, 2, es=es)
    sb['h32'] = alloc_T(nc, 'h32', [128, D], F32, 2, es=es)
    sb['hb'] = alloc_T(nc, 'hb', [128, D], BF16, 2, es=es)
    sb['junk'] = alloc_T(nc, 'junk', [128, D], F32, 1, es=es)[0]
    sb['ss'] = alloc_T(nc, 'ss', [128, 4], F32, 2, es=es)
    sb['hT32'] = alloc_T(nc, 'hT32', [128, D], F32, 2, es=es)
    sb['sm'] = alloc_T(nc, 'sm', [128, 4], F32, 2, es=es)
    sb['ex'] = alloc_T(nc, 'ex', [128, NE], F32, 2, es=es)
    sb['vals'] = alloc_T(nc, 'vals', [NE, CAP], F32, 1, es=es)[0]
    sb['idxu'] = alloc_T(nc, 'idxu', [NE, CAP], U32, 1, es=es)[0]
    sb['idxf'] = alloc_T(nc, 'idxf', [NE, CAP], F32, 1, es=es)[0]
    sb['idxT'] = alloc_T(nc, 'idxT', [128, 8, NE], U32, 1, es=es)[0]
    sb['gateT'] = alloc_T(nc, 'gateT', [128, 8, NE], F32, 1, es=es)[0]
    sb['xsT'] = alloc_T(nc, 'xsT', [128, 8, CAP], BF16, 1, es=es)[0]
    sb['hidT'] = alloc_T(nc, 'hidT', [128, 4, CAP], BF16, 1, es=es)[0]
    sb['yacc'] = (big[:, :].rearrange('p (j d) -> p j d', j=8), bbig)
    sb['xs'] = alloc_T(nc, 'xs', [128, D], BF16, 2, es=es)
    sb['wg'] = alloc_T(nc, 'wg', [128, 8, 512], BF16, 2, es=es)
    sb['wu'] = alloc_T(nc, 'wu', [128, 8, 512], BF16, 2, es=es)
    sb['wd'] = alloc_T(nc, 'wd', [128, 4, D], BF16, 2, es=es)
    sb['sg'] = alloc_T(nc, 'sg', [128, 512], BF16, 2, es=es)
    return sb


def moe_layer(c, g, X, bX, HB, bHB, ffn_g, w_router, w_gate, w_up, w_down):
    with ExitStack() as es:
        sb = moe_alloc(c.nc, es)
        moe_phase(c, g, X, bX, HB, bHB, ffn_g, w_router, w_gate, w_up, w_down, sb)
        barrier(c)


def proj_phase(c, g, X, bX, gain_ap, w_ap, nout, spec, PT, bPT, PV, bPV):
    nc = c.nc
    with ExitStack() as es:
        def T(name, shape, dt):
            return es.enter_context(nc.sbuf_tensor(uniq(name), shape, dt))
        wsb = T("pj_w", [128, 8, nout], BF16); bw = Buf()
        gB = T("pj_gB", [128, D], F32); bgB = Buf()
        tmpr = T("pj_tmpr", [1, D], F32); btmpr = Buf()
        xt = [T("pj_xt%d" % i, [128, 4, D], F32) for i in range(2)]; bxt = [Buf(), Buf()]
        junk = T("pj_junk", [128, D], F32); bjunk = Buf()
        ss = [T("pj_ss%d" % i, [128, 4], F32) for i in range(2)]; bss = [Buf(), Buf()]
        hb = [T("pj_hb%d" % i, [128, D], BF16) for i in range(2)]; bhb = [Buf(), Buf()]
        hT = [T("pj_hT%d" % i, [128, 8, 512], BF16) for i in range(2)]; bhT = [Buf(), Buf()]
        stF = [T("pj_stF%d" % i, [128, 512], F32) for i in range(3)]; bstF = [Buf() for _ in range(3)]
        stT = [T("pj_stT%d" % i, [128, 512], BF16) for i in range(3)]; bstT = [Buf() for _ in range(3)]
        bcast_row(c, g, gB, bgB, gain_ap, D, tmpr, btmpr)
        for c0 in range(0, nout, 512):
            wd = min(512, nout - c0)
            c.dma('pool', lambda e: e.dma_start(out=wsb[:, :, c0:c0 + wd], in_=w_ap[:, c0:c0 + wd].rearrange("(k p) f -> p k f", p=128)), writes=[bw])
        ptb, bptb = g.psb
        nF = 0; nT = 0; npz = 0
        for it in range(L // 512):
            t0 = it * 512
            x_, bx_ = xt[it % 2], bxt[it % 2]
            c.dma('sp', lambda e: e.dma_start(out=x_[:], in_=X[t0:t0 + 512, :].rearrange("(j p) d -> p j d", p=128)), reads=[bX], writes=[bx_])
            hT_, bhT_ = hT[it % 2], bhT[it % 2]
            for j in range(4):
                s_, bs_ = ss[j % 2], bss[j % 2]
                h_, bh_ = hb[j % 2], bhb[j % 2]
                c.op('dve', lambda e: e.scalar_tensor_tensor(out=junk[:], in0=x_[:, j, :], scalar=1.0, in1=x_[:, j, :], op0=ALU.mult, op1=ALU.mult,
                                                             accum_out=s_[:, 0:1]), reads=[bx_], writes=[bjunk, bs_])
                c.op('dve', lambda e: e.tensor_scalar(out=s_[:, 0:1], in0=s_[:, 0:1], scalar1=1.0 / D, scalar2=1e-6, op0=ALU.mult, op1=ALU.add),
                     reads=[bs_], writes=[bs_])
                c.op('pool', lambda e: e.tensor_tensor(out=s_[:, 0:1], in0=s_[:, 0:1], in1=g.neghalf[:, 0:1], op=ALU.pow),
                     reads=[bs_, g.b_neghalf], writes=[bs_])
                c.op('dve', lambda e: e.scalar_tensor_tensor(out=h_[:], in0=x_[:, j, :], scalar=s_[:, 0:1], in1=gB[:], op0=ALU.mult, op1=ALU.mult),
                     reads=[bx_, bs_, bgB], writes=[bh_])
                for k in range(8):
                    c.op('pe', lambda e: e.transpose(out=ptb[:, k * 128:(k + 1) * 128], in_=h_[:, k * 128:(k + 1) * 128], identity=g.identb[:]),
                         reads=[bh_, g.b_identb], writes=[bptb])
                c.op('act', lambda e: e.activation(out=hT_[:, :, j * 128:(j + 1) * 128], in_=ptb[:, :].rearrange("p (k s) -> p k s", k=8), func=AF.Copy),
                     reads=[bptb], writes=[bhT_])
            for (col0, ncols, mode, dst0) in spec:
                if mode == 'F':
                    for f0 in range(0, ncols, 128):
                        fw = min(128, ncols - f0)
                        ps, bps = g.ps[npz % 4]; npz += 1
                        for k in range(8):
                            c.op('pe', lambda e: e.matmul(ps[0:fw, :], lhsT=wsb[:, k, col0 + f0:col0 + f0 + fw], rhs=hT_[:, k, :], start=(k == 0), stop=(k == 7)),
                                 reads=[bw, bhT_], writes=[bps])
                        st, bst = stF[nF % 3], bstF[nF % 3]; nF += 1
                        eng = 'act' if nF % 2 else 'dve'
                        if eng == 'act':
                            c.op('act', lambda e: e.activation(out=st[0:fw, :], in_=ps[0:fw, :], func=AF.Copy), reads=[bps], writes=[bst])
                        else:
                            c.op('dve', lambda e: e.tensor_copy(out=st[0:fw, :], in_=ps[0:fw, :]), reads=[bps], writes=[bst])
                        c.dma('sp' if nF % 2 else 'act', lambda e: e.dma_start(out=PT[dst0 + f0:dst0 + f0 + fw, t0:t0 + 512], in_=st[0:fw, :]), reads=[bst], pwrites=[bPT])
                else:
                    for j in range(4):
                        for c0 in range(0, ncols, 512):
                            cw = min(512, ncols - c0)
                            ps, bps = g.ps[npz % 4]; npz += 1
                            for k in range(8):
                                c.op('pe', lambda e: e.matmul(ps[:, 0:cw], lhsT=hT_[:, k, j * 128:(j + 1) * 128], rhs=wsb[:, k, col0 + c0:col0 + c0 + cw], start=(k == 0), stop=(k == 7)),
                                     reads=[bw, bhT_], writes=[bps])
                            st, bst = stT[nT % 3], bstT[nT % 3]; nT += 1
                            eng = 'act' if nT % 2 else 'dve'
                            if eng == 'act':
                                c.op('act', lambda e: e.activation(out=st[:, 0:cw], in_=ps[:, 0:cw], func=AF.Copy), reads=[bps], writes=[bst])
                            else:
                                c.op('dve', lambda e: e.tensor_copy(out=st[:, 0:cw], in_=ps[:, 0:cw]), reads=[bps], writes=[bst])
                            c.dma('sp' if nT % 2 else 'act', lambda e: e.dma_start(out=PV[t0 + j * 128:t0 + (j + 1) * 128, dst0 + c0:dst0 + c0 + cw], in_=st[:, 0:cw]),
                                  reads=[bst], pwrites=[bPV])
        bPT.seal(); bPV.seal()
        barrier(c)


TBK = 2048
NTB = L // TBK


def hgrn2_phase(c, g, PT, bPT, PV, bPV, lb_logits, jl, a_out_norm, QK, bQK, OA, bOAs, MT, bMT, stage=3):
    nc = c.nc
    with ExitStack() as es0:
        def T0(name, shape, dt):
            return es0.enter_context(nc.sbuf_tensor(uniq(name), shape, dt))
        mcols = T0("hg_mcols", [128, 8, 128], F32); bmcols = Buf()
        with ExitStack() as es:
            def T(name, shape, dt):
                return es.enter_context(nc.sbuf_tensor(uniq(name), shape, dt))
            lbt = T("hg_lbt", [128, 2, 2, 4], F32); blbt = Buf()
            lbc = T("hg_lbc", [128, 8], F32); blbc = Buf()
            oml = T("hg_oml", [128, 8], F32); boml = Buf()
            noml = T("hg_noml", [128, 8], F32); bnoml = Buf()
            msk = T("hg_msk", [128, TBK], F32); bmsk = Buf()
            bmid = T("hg_bmid", [128, 128], F32); bbmid = Buf()
            blast = T("hg_blast", [128, 128], F32); bblast = Buf()
            zq = [T("hg_zq%d" % i, [128, TBK], F32) for i in range(2)]; bzq = [Buf(), Buf()]
            zf = [T("hg_zf%d" % i, [128, TBK], F32) for i in range(2)]; bzf = [Buf(), Buf()]
            q_ = T("hg_q", [128, TBK], F32); bq_ = Buf()
            sig = T("hg_sig", [128, TBK], F32); bsig = Buf()
            f_ = T("hg_f", [128, TBK], F32); bf_ = Buf()
            kk = T("hg_kk", [128, TBK], F32); bkk = Buf()
            b_ = T("hg_b", [128, TBK], F32); bb_ = Buf()
            e1 = T("hg_e1", [128, TBK], F32); be1 = Buf()
            eq = T("hg_eq", [128, TBK], F32); beq = Buf()
            ek = T("hg_ek", [128, TBK], F32); bek = Buf()
            qt = [T("hg_qt%d" % i, [128, TBK], BF16) for i in range(2)]; bqt = [Buf(), Buf()]
            kt = [T("hg_kt%d" % i, [128, TBK], BF16) for i in range(2)]; bkt = [Buf(), Buf()]
            if jl == 0:
                c.op('pool', lambda e: e.memset(lbc[:], 0.0), writes=[blbc])
            else:
                with nc.allow_non_contiguous_dma(reason="tiny"):
                    for j_ in range(2):
                        for r_ in range(2):
                            c.dma('sp', lambda e: e.dma_start(out=lbt[:, j_, r_, :], in_=lb_logits[j_, r_, :].rearrange("(h p) -> p h", p=128)), pwrites=[blbt])
                blbt.seal()
                c.op('dve', lambda e: e.tensor_tensor(out=lbc[:].rearrange("p (r h) -> p r h", r=2), in0=lbt[:, 1, :, :], in1=lbt[:, 0, :, :], op=ALU.subtract),
                     reads=[blbt], writes=[blbc])
                c.op('act', lambda e: e.activation(out=lbc[:], in_=lbc[:], func=AF.Sigmoid), reads=[blbc], writes=[blbc])
            c.op('dve', lambda e: e.tensor_scalar(out=oml[:], in0=lbc[:], scalar1=-1.0, scalar2=1.0, op0=ALU.mult, op1=ALU.add), reads=[blbc], writes=[boml])
            c.op('dve', lambda e: e.tensor_scalar(out=noml[:], in0=oml[:], scalar1=-1.0, scalar2=None, op0=ALU.mult), reads=[boml], writes=[bnoml])
            c.op('pool', lambda e: e.memset(msk[:], 1.0), writes=[bmsk])
            c.op('pool', lambda e: e.memset(msk[:].rearrange("p (c j) -> p c j", j=64)[:, :, 0:1], 0.0), writes=[bmsk])
            n = 0
            for h in range(4):
                for r in range(2):
                    hr = r * 4 + h
                    for tb in range(NTB):
                        nb = tb if r == 0 else NTB - 1 - tb
                        zq_, bzq_ = zq[n % 2], bzq[n % 2]
                        zf_, bzf_ = zf[n % 2], bzf[n % 2]
                        qt_, bqt_ = qt[n % 2], bqt[n % 2]
                        kt_, bkt_ = kt[n % 2], bkt[n % 2]
                        n += 1
                        c.dma('sp', lambda e: e.dma_start(out=zq_[:], in_=PT[h * 128:(h + 1) * 128, nb * TBK:(nb + 1) * TBK]), reads=[bPT], writes=[bzq_])
                        fr = 512 + r * 512 + h * 128
                        c.dma('act', lambda e: e.dma_start(out=zf_[:], in_=PT[fr:fr + 128, nb * TBK:(nb + 1) * TBK]), reads=[bPT], writes=[bzf_])
                        zqs = zq_[:, ::-1] if r else zq_[:, :]
                        zfs = zf_[:, ::-1] if r else zf_[:, :]
                        c.op('act', lambda e: e.activation(out=q_[:], in_=zqs, func=AF.Silu), reads=[bzq_], writes=[bq_])
                        c.op('act', lambda e: e.activation(out=sig[:], in_=zfs, func=AF.Sigmoid), reads=[bzf_], writes=[bsig])
                        c.op('dve', lambda e: e.tensor_scalar(out=f_[:], in0=sig[:], scalar1=oml[:, hr:hr + 1], scalar2=lbc[:, hr:hr + 1], op0=ALU.mult, op1=ALU.add),
                             reads=[bsig, boml, blbc], writes=[bf_])
                        c.op('act', lambda e: e.activation(out=f_[:], in_=f_[:], func=AF.Ln), reads=[bf_], writes=[bf_])
                        c.op('pool', lambda e: e.tensor_scalar(out=kk[:], in0=sig[:], scalar1=noml[:, hr:hr + 1], scalar2=oml[:, hr:hr + 1], op0=ALU.mult, op1=ALU.add),
                             reads=[bsig, bnoml, boml], writes=[bkk])
                        c.op('dve', lambda e: e.tensor_tensor_scan(out=b_[:], data0=msk[:], data1=f_[:], initial=0.0, op0=ALU.mult, op1=ALU.add),
                             reads=[bmsk, bf_], writes=[bb_])
                        b3 = b_[:].rearrange("p (c j) -> p c j", j=64)
                        c.op('dve', lambda e: e.tensor_tensor(out=e1[:].rearrange("p (c j) -> p c j", j=64), in0=b3, in1=b3[:, :, 31:32].broadcast_to([128, TBK // 64, 64]), op=ALU.subtract),
                             reads=[bb_], writes=[be1])
                        c.op('act', lambda e: e.activation(out=eq[:], in_=e1[:], func=AF.Exp), reads=[be1], writes=[beq])
                        c.op('act', lambda e: e.activation(out=ek[:], in_=e1[:], func=AF.Exp, scale=-1.0), reads=[be1], writes=[bek])
                        c.op('dve', lambda e: e.tensor_tensor(out=qt_[:], in0=q_[:], in1=eq[:], op=ALU.mult), reads=[bq_, beq], writes=[bqt_])
                        c.op('pool', lambda e: e.tensor_tensor(out=kt_[:], in0=kk[:], in1=ek[:], op=ALU.mult), reads=[bkk, bek], writes=[bkt_])
                        nch = TBK // 64
                        c.op('act', lambda e: e.activation(out=bmid[:, tb * nch:(tb + 1) * nch], in_=b3[:, :, 31], func=AF.Copy), reads=[bb_], writes=[bbmid])
                        c.op('act', lambda e: e.activation(out=blast[:, tb * nch:(tb + 1) * nch], in_=b3[:, :, 63], func=AF.Copy), reads=[bb_], writes=[bblast])
                        c.dma('sp', lambda e: e.dma_start(out=QK[h, r, 0, :, tb * TBK:(tb + 1) * TBK], in_=qt_[:]), reads=[bqt_], pwrites=[bQK])
                        c.dma('act', lambda e: e.dma_start(out=QK[h, r, 1, :, tb * TBK:(tb + 1) * TBK], in_=kt_[:]), reads=[bkt_], pwrites=[bQK])
                    c.op('dve', lambda e: e.tensor_tensor(out=blast[:], in0=blast[:], in1=bmid[:], op=ALU.subtract), reads=[bblast, bbmid], writes=[bblast])
                    c.op('dve', lambda e: e.tensor_tensor(out=blast[:, 0:127], in0=blast[:, 0:127], in1=bmid[:, 1:128], op=ALU.add), reads=[bblast, bbmid], writes=[bblast])
                    c.op('act', lambda e: e.activation(out=mcols[:, hr, :], in_=blast[:], func=AF.Exp), reads=[bblast], writes=[bmcols])
            bQK.seal()
            barrier(c)
        if stage < 2:
            return
        with ExitStack() as es:
            def T(name, shape, dt):
                return es.enter_context(nc.sbuf_tensor(uniq(name), shape, dt))
            qb = [T("hr_qb%d" % i, [128, TBK], BF16) for i in range(2)]; bqb = [Buf(), Buf()]
            kb = [T("hr_kb%d" % i, [128, TBK], BF16) for i in range(2)]; bkb = [Buf(), Buf()]
            vb = [T("hr_vb%d" % i, [128, TBK // 128, 128], BF16) for i in range(2)]; bvb = [Buf(), Buf()]
            mask = T("hr_mask", [128, 128], F32); bmask = Buf()
            attnT = [T("hr_attn%d" % i, [128, 128], BF16) for i in range(2)]; battn = [Buf(), Buf()]
            ktok = [T("hr_ktok%d" % i, [128, 128], BF16) for i in range(2)]; bktok = [Buf(), Buf()]
            M32 = T("hr_M32", [128, 128], F32); bM32 = Buf()
            Mb = T("hr_Mb", [128, 128], BF16); bMb = Buf()
            tmp32 = T("hr_tmp32", [128, 128], F32); btmp32 = Buf()
            osb = [T("hr_osb%d" % i, [128, 128], F32) for i in range(3)]; bosb = [Buf() for _ in range(3)]
            osf = [T("hr_osf%d" % i, [128, 128], F32) for i in range(3)]; bosf = [Buf() for _ in range(3)]
            vnat = T("hr_vnat", [128, TBK // 128, 128], BF16); bvnat = Buf()
            c.op('pool', lambda e: e.memset(mask[:], 1.0), writes=[bmask])
            c.op('pool', lambda e: e.affine_select(out=mask[:], in_=mask[:], pattern=[[1, 128]], compare_op=ALU.is_ge, fill=0.0, base=0, channel_multiplier=-1),
                 reads=[bmask], writes=[bmask])
            c.op('pool', lambda e: e.memset(mask[0:64, 64:128], 0.0), reads=[bmask], writes=[bmask])
            ptb, bptb = g.psb
            n = 0; nblk = 0; nch = 0
            for h in range(4):
                for r in range(2):
                    hr = r * 4 + h
                    c.op('pool', lambda e: e.memset(M32[:], 0.0), writes=[bM32])
                    c.op('pool', lambda e: e.memset(Mb[:], 0.0), writes=[bMb])
                    for tb in range(NTB):
                        qb_, bqb_ = qb[n % 2], bqb[n % 2]
                        kb_, bkb_ = kb[n % 2], bkb[n % 2]
                        vb_, bvb_ = vb[n % 2], bvb[n % 2]
                        n += 1
                        c.dma('sp', lambda e: e.dma_start(out=qb_[:], in_=QK[h, r, 0, :, tb * TBK:(tb + 1) * TBK]), reads=[bQK], writes=[bqb_])
                        c.dma('act', lambda e: e.dma_start(out=kb_[:], in_=QK[h, r, 1, :, tb * TBK:(tb + 1) * TBK]), reads=[bQK], writes=[bkb_])
                        if r == 0:
                            vsrc = PV[tb * TBK:(tb + 1) * TBK, h * 128:(h + 1) * 128].rearrange("(b p) d -> p b d", p=128)
                            c.dma('sp', lambda e: e.dma_start(out=vb_[:], in_=vsrc), reads=[bPV], writes=[bvb_])
                        else:
                            vsrc = PV[L - (tb + 1) * TBK:L - tb * TBK, h * 128:(h + 1) * 128].rearrange("(b p) d -> p b d", p=128)
                            c.dma('sp', lambda e: e.dma_start(out=vnat[:], in_=vsrc), reads=[bPV], writes=[bvnat])
                            nbk = TBK // 128
                            for b4 in range(0, nbk, 4):
                                pf, bpf = g.ps[4 + (b4 // 4) % 2]
                                c.op('pe', lambda e: e.matmul(pf[:, :], lhsT=g.Jb[:], rhs=vnat[:, b4:b4 + 4, :], start=True, stop=True), reads=[g.b_Jb, bvnat], writes=[bpf])
                                for bb in range(4):
                                    c.op('act', lambda e: e.activation(out=vb_[:, nbk - 1 - (b4 + bb), :], in_=pf[:, bb * 128:(bb + 1) * 128], func=AF.Copy), reads=[bpf], writes=[bvb_])
                        for b in range(TBK // 128):
                            blk = tb * (TBK // 128) + b
                            at_, bat_ = attnT[nblk % 2], battn[nblk % 2]
                            kt_, bkt_ = ktok[nblk % 2], bktok[nblk % 2]
                            pa, bpa = g.ps[nblk % 2]
                            po, bpo = g.ps[2 + nblk % 2]
                            os_, bos_ = osb[nblk % 3], bosb[nblk % 3]
                            nblk += 1
                            bs = slice(b * 128, (b + 1) * 128)
                            c.op('pe', lambda e: e.matmul(pa[:, 0:128], lhsT=kb_[:, bs], rhs=qb_[:, bs], start=True, stop=True), reads=[bkb_, bqb_], writes=[bpa])
                            c.op('dve', lambda e: e.tensor_tensor(out=at_[:], in0=pa[:, 0:128], in1=mask[:], op=ALU.mult), reads=[bpa, bmask], writes=[bat_])
                            c.op('pe', lambda e: e.transpose(out=ptb[:, 0:128], in_=kb_[:, bs], identity=g.identb[:]), reads=[bkb_, g.b_identb], writes=[bptb])
                            c.op('act', lambda e: e.activation(out=kt_[:], in_=ptb[:, 0:128], func=AF.Copy), reads=[bptb], writes=[bkt_])
                            for ci in range(2):
                                r0 = 64 * ci
                                cidx = 2 * blk + ci
                                pk, bpk = g.ps[4 + nch % 2]; nch += 1
                                c.op('pe', lambda e: e.matmul(po[r0:r0 + 64, 0:128], lhsT=at_[r0:r0 + 64, r0:r0 + 64], rhs=vb_[r0:r0 + 64, b, :], start=True, stop=False),
                                     reads=[bat_, bvb_], writes=[bpo])
                                c.op('pe', lambda e: e.matmul(po[r0:r0 + 64, 0:128], lhsT=qb_[:, b * 128 + r0:b * 128 + r0 + 64], rhs=Mb[:, :], start=False, stop=True),
                                     reads=[bqb_, bMb], writes=[bpo])
                                if cidx < 127:
                                    c.op('pe', lambda e: e.matmul(pk[:, 0:128], lhsT=kt_[r0:r0 + 64, :], rhs=vb_[r0:r0 + 64, b, :], start=True, stop=True),
                                         reads=[bkt_, bvb_], writes=[bpk])
                                    c.op('dve', lambda e: e.tensor_tensor(out=tmp32[:], in0=pk[:, 0:128], in1=M32[:], op=ALU.add), reads=[bpk, bM32], writes=[btmp32])
                                    c.op('dve', lambda e: e.tensor_scalar(out=M32[:], in0=tmp32[:], scalar1=mcols[:, hr, cidx:cidx + 1], scalar2=None, op0=ALU.mult),
                                         reads=[btmp32, bmcols], writes=[bM32])
                                    c.op('act', lambda e: e.activation(out=Mb[:], in_=M32[:], func=AF.Copy), reads=[bM32], writes=[bMb])
                            c.op('act', lambda e: e.activation(out=os_[:], in_=po[:, 0:128], func=AF.Copy), reads=[bpo], writes=[bos_])
                            if r == 0:
                                c.dma('sp', lambda e: e.dma_start(out=OA[blk * 128:(blk + 1) * 128, h * 128:(h + 1) * 128], in_=os_[:]), reads=[bos_], pwrites=[bOAs[h]])
                            else:
                                of_, bof_ = osf[nblk % 3], bosf[nblk % 3]
                                pf, bpf = g.ps[6]
                                c.op('pe', lambda e: e.matmul(pf[:, 0:128], lhsT=g.J32[:], rhs=os_[:], start=True, stop=True), reads=[g.b_J32, bos_], writes=[bpf])
                                c.op('act', lambda e: e.activation(out=of_[:], in_=pf[:, 0:128], func=AF.Copy), reads=[bpf], writes=[bof_])
                                c.dma('pool', lambda e: e.dma_start(out=OA[L - (blk + 1) * 128:L - blk * 128, h * 128:(h + 1) * 128], in_=of_[:], accum_op=ALU.add),
                                      reads=[bof_], pwrites=[bOAs[h]])
                    bOAs[h].seal()
            barrier(c)
        if stage < 3:
            return
        with ExitStack() as es:
            def T(name, shape, dt):
                return es.enter_context(nc.sbuf_tensor(uniq(name), shape, dt))
            gA = T("hf_gA", [128, 128], F32); bgA = Buf()
            tmpr = T("hf_tmpr", [1, 128], F32); btmpr = Buf()
            oa = [T("hf_oa%d" % i, [128, 512], F32) for i in range(2)]; boa = [Buf(), Buf()]
            ga = [T("hf_ga%d" % i, [128, 512], BF16) for i in range(2)]; bga = [Buf(), Buf()]
            sq = T("hf_sq", [128, 512], F32); bsq = Buf()
            ssq = [T("hf_ssq%d" % i, [128, 4], F32) for i in range(2)]; bssq = [Buf(), Buf()]
            sg = T("hf_sg", [128, 512], F32); bsg = Buf()
            t1 = T("hf_t1", [128, 512], F32); bt1 = Buf()
            ob = [T("hf_ob%d" % i, [128, 512], BF16) for i in range(2)]; bob = [Buf(), Buf()]
            mt = [T("hf_mt%d" % i, [128, 4, 512], BF16) for i in range(2)]; bmt = [Buf(), Buf()]
            bcast_row(c, g, gA, bgA, a_out_norm, 128, tmpr, btmpr)
            ptb, bptb = g.psb
            for i in range(NT):
                oa_, boa_ = oa[i % 2], boa[i % 2]
                ga_, bga_ = ga[i % 2], bga[i % 2]
                ss_, bss_ = ssq[i % 2], bssq[i % 2]
                ob_, bob_ = ob[i % 2], bob[i % 2]
                mt_, bmt_ = mt[(i // 4) % 2], bmt[(i // 4) % 2]
                c.dma('sp', lambda e: e.dma_start(out=oa_[:], in_=OA[i * 128:(i + 1) * 128, :]), reads=bOAs, writes=[boa_])
                c.dma('act', lambda e: e.dma_start(out=ga_[:], in_=PV[i * 128:(i + 1) * 128, 512:1024]), reads=[bPV], writes=[bga_])
                c.op('pool', lambda e: e.tensor_tensor(out=sq[:], in0=oa_[:], in1=oa_[:], op=ALU.mult), reads=[boa_], writes=[bsq])
                c.op('dve', lambda e: e.tensor_reduce(out=ss_[:], in_=sq[:].rearrange("p (h d) -> p h d", h=4), axis=AX.X, op=ALU.add), reads=[bsq], writes=[bss_])
                c.op('dve', lambda e: e.tensor_scalar(out=ss_[:], in0=ss_[:], scalar1=1.0 / 128, scalar2=1e-6, op0=ALU.mult, op1=ALU.add), reads=[bss_], writes=[bss_])
                c.op('pool', lambda e: e.tensor_tensor(out=ss_[:], in0=ss_[:], in1=g.neghalf[:, 0:1].broadcast_to([128, 4]), op=ALU.pow), reads=[bss_, g.b_neghalf], writes=[bss_])
                c.op('act', lambda e: e.activation(out=sg[:], in_=ga_[:], func=AF.Silu), reads=[bga_], writes=[bsg])
                c.op('dve', lambda e: e.tensor_tensor(out=t1[:].rearrange("p (h d) -> p h d", h=4), in0=oa_[:].rearrange("p (h d) -> p h d", h=4),
                                                      in1=ss_[:].unsqueeze(2).broadcast_to([128, 4, 128]), op=ALU.mult), reads=[boa_, bss_], writes=[bt1])
                c.op('pool', lambda e: e.tensor_tensor(out=t1[:].rearrange("p (h d) -> p h d", h=4), in0=t1[:].rearrange("p (h d) -> p h d", h=4),
                                                       in1=gA[:].unsqueeze(1).broadcast_to([128, 4, 128]), op=ALU.mult), reads=[bt1, bgA], writes=[bt1])
                c.op('dve', lambda e: e.tensor_tensor(out=ob_[:], in0=t1[:], in1=sg[:], op=ALU.mult), reads=[bt1, bsg], writes=[bob_])
                for k in range(4):
                    c.op('pe', lambda e: e.transpose(out=ptb[:, k * 128:(k + 1) * 128], in_=ob_[:, k * 128:(k + 1) * 128], identity=g.identb[:]),
                         reads=[bob_, g.b_identb], writes=[bptb])
                c.op('act', lambda e: e.activation(out=mt_[:, :, (i % 4) * 128:(i % 4 + 1) * 128], in_=ptb[:, 0:512].rearrange("p (k s) -> p k s", k=4), func=AF.Copy),
                     reads=[bptb], writes=[bmt_])
                if i % 4 == 3:
                    t0 = (i // 4) * 512
                    c.dma('sp', lambda e: e.dma_start(out=MT[0:512, t0:t0 + 512].rearrange("(k p) t -> p k t", p=128), in_=mt_[:]), reads=[bmt_], pwrites=[bMT])
            barrier(c)


TB5 = 1024
NTB5 = L // TB5
TWO_PI = 2.0 * math.pi


def s5_phase(c, g, PT, bPT, lam_re, lam_im, log_step, b_re, b_im, c_re, c_im, d_skip, glu_w, glu_b, YT, bYT, MT, bMT, stage=3):
    nc = c.nc
    U0 = 1536
    with ExitStack() as es0:
        def T0(name, shape, dt):
            return es0.enter_context(nc.sbuf_tensor(uniq(name), shape, dt))
        WB = [T0("s5_WB%d" % p, [128, 2, 4, 128], BF16) for p in range(2)]; bWB = Buf()
        WC = [T0("s5_WC%d" % p, [128, 2, 4, 128], BF16) for p in range(2)]; bWC = Buf()
        WBx = [T0("s5_WBx%d" % p, [128, 2, 4, 128], BF16) for p in range(2)]
        WCx = [T0("s5_WCx%d" % p, [128, 2, 4, 64], BF16) for p in range(2)]
        mag = T0("s5_mag", [128, 32], F32); bmag = Buf()
        pwc = T0("s5_pwc", [128, 11, 32], F32); bpw = Buf()
        pws = T0("s5_pws", [128, 11, 32], F32)
        with ExitStack() as es:
            def T(name, shape, dt):
                return es.enter_context(nc.sbuf_tensor(uniq(name), shape, dt))
            n_ = [0]

            def S(shape=[128, 32], dt=F32):
                n_[0] += 1
                return T("s5_t%d" % n_[0], shape, dt), Buf()
            lamre, blamre = S(); lamim, blamim = S()
            lsB, blsB = S([128, 64]); ls, bls = S()
            with nc.allow_non_contiguous_dma(reason="small params"):
                for r_ in range(2):
                    for g4 in range(0, 16, 4):
                        c.dma('sp', lambda e: e.dma_start(out=lamre[:, r_ * 16 + g4:r_ * 16 + g4 + 4], in_=lam_re[r_, 2 * g4:2 * g4 + 8, :].rearrange("(gp gl) n -> (gl n) gp", gl=2)), pwrites=[blamre])
                        c.dma('act', lambda e: e.dma_start(out=lamim[:, r_ * 16 + g4:r_ * 16 + g4 + 4], in_=lam_im[r_, 2 * g4:2 * g4 + 8, :].rearrange("(gp gl) n -> (gl n) gp", gl=2)), pwrites=[blamim])
                blamre.seal(); blamim.seal()
                c.dma('sp', lambda e: e.dma_start(out=lsB[:], in_=log_step.rearrange("r g -> (r g)").partition_broadcast(128)), writes=[blsB])
            lsv = lsB[:].rearrange("p (r gp gl) -> p r gp gl", r=2, gl=2)
            c.op('dve', lambda e: e.tensor_copy(out=ls[0:64, :].rearrange("p (r gp) -> p r gp", r=2), in_=lsv[0:64, :, :, 0]), reads=[blsB], writes=[bls])
            c.op('dve', lambda e: e.tensor_copy(out=ls[64:128, :].rearrange("p (r gp) -> p r gp", r=2), in_=lsv[64:128, :, :, 1]), reads=[blsB], writes=[bls])
            step, bstep = S()
            c.op('act', lambda e: e.activation(out=step[:], in_=ls[:], func=AF.Exp), reads=[bls], writes=[bstep])
            lrs, blrs = S(); ang, bang = S()
            c.op('dve', lambda e: e.tensor_tensor(out=lrs[:], in0=lamre[:], in1=step[:], op=ALU.mult), reads=[blamre, bstep], writes=[blrs])
            c.op('act', lambda e: e.activation(out=mag[:], in_=lrs[:], func=AF.Exp), reads=[blrs], writes=[bmag])
            c.op('dve', lambda e: e.tensor_tensor(out=ang[:], in0=lamim[:], in1=step[:], op=ALU.mult), reads=[blamim, bstep], writes=[bang])

            def sin_of(src, bsrc, offset, dst, bdst):
                q, bq = S(); qi, bqi = S(dt=I32); r, br = S(); m, bm = S()
                c.op('dve', lambda e: e.tensor_scalar(out=q[:], in0=src[:], scalar1=offset, scalar2=1.0 / TWO_PI, op0=ALU.add, op1=ALU.mult), reads=[bsrc], writes=[bq])
                c.op('dve', lambda e: e.tensor_copy(out=qi[:], in_=q[:]), reads=[bq], writes=[bqi])
                c.op('dve', lambda e: e.tensor_copy(out=q[:], in_=qi[:]), reads=[bqi], writes=[bq])
                c.op('dve', lambda e: e.scalar_tensor_tensor(out=r[:], in0=q[:], scalar=-TWO_PI, in1=src[:], op0=ALU.mult, op1=ALU.add), reads=[bq, bsrc], writes=[br])
                if offset != 0.0:
                    c.op('dve', lambda e: e.tensor_scalar(out=r[:], in0=r[:], scalar1=offset, scalar2=None, op0=ALU.add), reads=[br], writes=[br])
                c.op('dve', lambda e: e.tensor_scalar(out=m[:], in0=r[:], scalar1=math.pi, scalar2=-TWO_PI, op0=ALU.is_gt, op1=ALU.mult), reads=[br], writes=[bm])
                c.op('dve', lambda e: e.tensor_tensor(out=r[:], in0=r[:], in1=m[:], op=ALU.add), reads=[br, bm], writes=[br])
                c.op('dve', lambda e: e.tensor_scalar(out=m[:], in0=r[:], scalar1=-math.pi, scalar2=TWO_PI, op0=ALU.is_lt, op1=ALU.mult), reads=[br], writes=[bm])
                c.op('dve', lambda e: e.tensor_tensor(out=r[:], in0=r[:], in1=m[:], op=ALU.add), reads=[br, bm], writes=[br])
                c.op('dve', lambda e: e.tensor_scalar(out=r[:], in0=r[:], scalar1=math.pi, scalar2=-math.pi, op0=ALU.min, op1=ALU.max), reads=[br], writes=[br])
                c.op('act', lambda e: e.activation(out=dst, in_=r[:], func=AF.Sin), reads=[br], writes=[bdst])
            sin_of(ang, bang, 0.0, pws[:, 0, :], bpw)
            sin_of(ang, bang, math.pi / 2, pwc[:, 0, :], bpw)
            tq, btq = S(); tq2, btq2 = S()
            for k in range(10):
                c.op('dve', lambda e: e.tensor_tensor(out=tq[:], in0=pwc[:, k, :], in1=pwc[:, k, :], op=ALU.mult), reads=[bpw], writes=[btq])
                c.op('dve', lambda e: e.tensor_tensor(out=tq2[:], in0=pws[:, k, :], in1=pws[:, k, :], op=ALU.mult), reads=[bpw], writes=[btq2])
                c.op('dve', lambda e: e.tensor_tensor(out=pwc[:, k + 1, :], in0=tq[:], in1=tq2[:], op=ALU.subtract), reads=[btq, btq2, bpw], writes=[bpw])
                c.op('dve', lambda e: e.tensor_tensor(out=tq[:], in0=pws[:, k, :], in1=pwc[:, k, :], op=ALU.mult), reads=[bpw], writes=[btq])
                c.op('dve', lambda e: e.tensor_scalar(out=pws[:, k + 1, :], in0=tq[:], scalar1=2.0, scalar2=None, op0=ALU.mult), reads=[btq, bpw], writes=[bpw])
            are, bare = S(); aim, baim = S(); den, bden = S(); am1, bam1 = S(); fr, bfr = S(); fi, bfi = S(); tt, btt = S()
            c.op('dve', lambda e: e.tensor_tensor(out=are[:], in0=mag[:], in1=pwc[:, 0, :], op=ALU.mult), reads=[bmag, bpw], writes=[bare])
            c.op('dve', lambda e: e.tensor_tensor(out=aim[:], in0=mag[:], in1=pws[:, 0, :], op=ALU.mult), reads=[bmag, bpw], writes=[baim])
            c.op('dve', lambda e: e.tensor_tensor(out=den[:], in0=lamre[:], in1=lamre[:], op=ALU.mult), reads=[blamre], writes=[bden])
            c.op('dve', lambda e: e.tensor_tensor(out=tt[:], in0=lamim[:], in1=lamim[:], op=ALU.mult), reads=[blamim], writes=[btt])
            c.op('dve', lambda e: e.tensor_tensor(out=den[:], in0=den[:], in1=tt[:], op=ALU.add), reads=[bden, btt], writes=[bden])
            c.op('dve', lambda e: e.reciprocal(out=den[:], in_=den[:]), reads=[bden], writes=[bden])
            c.op('dve', lambda e: e.tensor_scalar(out=am1[:], in0=are[:], scalar1=-1.0, scalar2=None, op0=ALU.add), reads=[bare], writes=[bam1])
            c.op('dve', lambda e: e.tensor_tensor(out=fr[:], in0=am1[:], in1=lamre[:], op=ALU.mult), reads=[bam1, blamre], writes=[bfr])
            c.op('dve', lambda e: e.tensor_tensor(out=tt[:], in0=aim[:], in1=lamim[:], op=ALU.mult), reads=[baim, blamim], writes=[btt])
            c.op('dve', lambda e: e.tensor_tensor(out=fr[:], in0=fr[:], in1=tt[:], op=ALU.add), reads=[bfr, btt], writes=[bfr])
            c.op('dve', lambda e: e.tensor_tensor(out=fr[:], in0=fr[:], in1=den[:], op=ALU.mult), reads=[bfr, bden], writes=[bfr])
            c.op('dve', lambda e: e.tensor_tensor(out=fi[:], in0=aim[:], in1=lamre[:], op=ALU.mult), reads=[baim, blamre], writes=[bfi])
            c.op('dve', lambda e: e.tensor_tensor(out=tt[:], in0=am1[:], in1=lamim[:], op=ALU.mult), reads=[bam1, blamim], writes=[btt])
            c.op('dve', lambda e: e.tensor_tensor(out=fi[:], in0=fi[:], in1=tt[:], op=ALU.subtract), reads=[bfi, btt], writes=[bfi])
            c.op('dve', lambda e: e.tensor_tensor(out=fi[:], in0=fi[:], in1=den[:], op=ALU.mult), reads=[bfi, bden], writes=[bfi])
            mk, bmk = S([128, 2])
            c.op('pool', lambda e: e.memset(mk[:], 0.0), writes=[bmk])
            c.op('pool', lambda e: e.memset(mk[0:64, 0:1], 1.0), reads=[bmk], writes=[bmk])
            c.op('pool', lambda e: e.memset(mk[64:128, 1:2], 1.0), reads=[bmk], writes=[bmk])
            Bn = [S([128, 2, 16, 16]) for _ in range(2)]
            with nc.allow_non_contiguous_dma(reason="small params"):
                for r_ in range(2):
                    for g4 in range(0, 16, 4):
                        c.dma('sp', lambda e: e.dma_start(out=Bn[0][0][:, r_, g4:g4 + 4, :], in_=b_re[r_, 2 * g4:2 * g4 + 8].rearrange("(gp gl) n p -> (gl n) gp p", gl=2)), pwrites=[Bn[0][1]])
                        c.dma('act', lambda e: e.dma_start(out=Bn[1][0][:, r_, g4:g4 + 4, :], in_=b_im[r_, 2 * g4:2 * g4 + 8].rearrange("(gp gl) n p -> (gl n) gp p", gl=2)), pwrites=[Bn[1][1]])
                Bn[0][1].seal(); Bn[1][1].seal()
            frb = fr[:].rearrange("p (r gp) -> p r gp", r=2).unsqueeze(3).broadcast_to([128, 2, 16, 16])
            fib = fi[:].rearrange("p (r gp) -> p r gp", r=2).unsqueeze(3).broadcast_to([128, 2, 16, 16])
            bbr, bbbr = S([128, 2, 16, 16]); bbi, bbbi = S([128, 2, 16, 16]); t5, bt5 = S([128, 2, 16, 16])
            c.op('dve', lambda e: e.tensor_tensor(out=bbr[:], in0=Bn[0][0][:], in1=frb, op=ALU.mult), reads=[Bn[0][1], bfr], writes=[bbbr])
            c.op('dve', lambda e: e.tensor_tensor(out=t5[:], in0=Bn[1][0][:], in1=fib, op=ALU.mult), reads=[Bn[1][1], bfi], writes=[bt5])
            c.op('dve', lambda e: e.tensor_tensor(out=bbr[:], in0=bbr[:], in1=t5[:], op=ALU.subtract), reads=[bbbr, bt5], writes=[bbbr])
            c.op('dve', lambda e: e.tensor_tensor(out=bbi[:], in0=Bn[1][0][:], in1=frb, op=ALU.mult), reads=[Bn[1][1], bfr], writes=[bbbi])
            c.op('dve', lambda e: e.tensor_tensor(out=t5[:], in0=Bn[0][0][:], in1=fib, op=ALU.mult), reads=[Bn[0][1], bfi], writes=[bt5])
            c.op('dve', lambda e: e.tensor_tensor(out=bbi[:], in0=bbi[:], in1=t5[:], op=ALU.add), reads=[bbbi, bt5], writes=[bbbi])
            BBm, bBBm = S([128, 2, 16, 2, 16], BF16)
            ptb, bptb = g.psb
            for part, (src, bsrc) in enumerate(((bbr, bbbr), (bbi, bbbi))):
                for gl in range(2):
                    c.op('dve', lambda e: e.tensor_scalar(out=BBm[:, :, :, gl, :], in0=src[:], scalar1=mk[:, gl:gl + 1], scalar2=None, op0=ALU.mult), reads=[bsrc, bmk, bBBm], writes=[bBBm])
                for r in range(2):
                    for cb in range(4):
                        c.op('pe', lambda e: e.transpose(out=ptb[:, 0:128], in_=BBm[:, r, 4 * cb:4 * cb + 4, :, :].rearrange("p a b c -> p (a b c)"), identity=g.identb[:]),
                             reads=[bBBm, g.b_identb], writes=[bptb])
                        c.op('act', lambda e: e.activation(out=WB[part][:, r, cb, :], in_=ptb[:, 0:128], func=AF.Copy), reads=[bptb], writes=[bWB])
            Cn = [S([128, 2, 4, 64]) for _ in range(2)]
            c.dma('sp', lambda e: e.dma_start(out=Cn[0][0][:], in_=c_re.rearrange("r (cb g8) p n -> (g8 p) r cb n", g8=8)), writes=[Cn[0][1]])
            c.dma('act', lambda e: e.dma_start(out=Cn[1][0][:], in_=c_im.rearrange("r (cb g8) p n -> (g8 p) r cb n", g8=8)), writes=[Cn[1][1]])
            Cd, bCd = S([128, 2, 4, 2, 64], BF16)
            mkb = mk[:].unsqueeze(1).unsqueeze(3).broadcast_to([128, 4, 2, 16])
            for part in range(2):
                sc = 1.0 if part == 0 else -1.0
                for x in range(2):
                    c.op('dve', lambda e: e.tensor_scalar(out=Cd[:, :, :, x, :], in0=Cn[part][0][:], scalar1=sc, scalar2=None, op0=ALU.mult), reads=[Cn[part][1], bCd], writes=[bCd])
                for r in range(2):
                    for cb in range(4):
                        c.op('pe', lambda e: e.transpose(out=ptb[:, 0:128], in_=Cd[:, r, cb, :, :].rearrange("p a b -> p (a b)"), identity=g.identb[:]),
                             reads=[bCd, g.b_identb], writes=[bptb])
                        c.op('dve', lambda e: e.tensor_tensor(out=WC[part][:, r, cb, :].rearrange("p (k a b) -> p k a b", k=4, a=2), in0=ptb[:, 0:128].rearrange("p (k a b) -> p k a b", k=4, a=2),
                                                              in1=mkb, op=ALU.mult), reads=[bptb, bmk], writes=[bWC])
            for part in range(2):
                c.op('act', lambda e: e.activation(out=WBx[part][64:128], in_=WB[part][64:128], func=AF.Copy), reads=[bWB], writes=[bWB])
                c.op('pool', lambda e: e.memset(WBx[part][64:96], 0.0), reads=[bWB], writes=[bWB])
                c.op('act', lambda e: e.activation(out=WCx[part][:], in_=WC[part][:, :, :, 64:128], func=AF.Copy), reads=[bWC], writes=[bWC])
                c.op('pool', lambda e: e.memset(WCx[part][:, :, :, 0:32], 0.0), reads=[bWC], writes=[bWC])
            barrier(c)
        if stage < 2:
            return
        with ExitStack() as es:
            def T(name, shape, dt):
                return es.enter_context(nc.sbuf_tensor(uniq(name), shape, dt))
            uf = T("s5_uf", [128, TB5 * 2], F32); buf_ = Buf()
            ub = [T("s5_ub%d" % r, [128, L], BF16) for r in range(2)]; bub = [Buf(), Buf()]
            Xa = [[T("s5_X%d%d" % (p, r), [128, L], BF16) for r in range(2)] for p in range(2)]
            bXa = [[Buf(), Buf()], [Buf(), Buf()]]
            tcos = T("s5_cos", [128, TB5], F32); tsin = T("s5_sin", [128, TB5], F32); btab = Buf()
            BUs = [T("s5_BU%d" % p, [128, TB5], F32) for p in range(2)]; bBUs = [Buf(), Buf()]
            t = [T("s5_w%d" % i, [128, TB5], F32) for i in range(4)]; bt = [Buf() for _ in range(4)]
            ini = T("s5_ini", [128, 4], F32); bini = Buf()
            yst = [T("s5_yst%d" % i, [128, 512], F32) for i in range(2)]; byst = [Buf(), Buf()]
            npz = 0
            for cb in range(4):
                for r in range(2):
                    for hh in range(L // (2 * TB5)):
                        nb = hh if r == 0 else L // (2 * TB5) - 1 - hh
                        c.dma('sp', lambda e: e.dma_start(out=uf[:], in_=PT[U0 + cb * 128:U0 + (cb + 1) * 128, nb * 2 * TB5:(nb + 1) * 2 * TB5]), reads=[bPT], writes=[buf_])
                        src = uf[:, ::-1] if r else uf[:, :]
                        c.op('act', lambda e: e.activation(out=ub[r][:, hh * 2 * TB5:(hh + 1) * 2 * TB5], in_=src, func=AF.Copy), reads=[buf_], writes=[bub[r]])
                for k in range(4):
                    gp = cb * 4 + k
                    for r in range(2):
                        col = r * 16 + gp
                        c.op('pool', lambda e: e.memset(tcos[:, 0:1], 1.0), writes=[btab])
                        c.op('pool', lambda e: e.memset(tsin[:, 0:1], 0.0), reads=[btab], writes=[btab])
                        n = 1
                        kk = 0
                        while n < TB5:
                            cr = pwc[:, kk, col:col + 1]; ci = pws[:, kk, col:col + 1]
                            c.op('pool', lambda e: e.tensor_scalar(out=t[0][:, 0:n], in0=tsin[:, 0:n], scalar1=ci, scalar2=None, op0=ALU.mult), reads=[btab, bpw], writes=[bt[0]])
                            c.op('pool', lambda e: e.tensor_scalar(out=t[1][:, 0:n], in0=tsin[:, 0:n], scalar1=cr, scalar2=None, op0=ALU.mult), reads=[btab, bpw], writes=[bt[1]])
                            c.op('dve', lambda e: e.scalar_tensor_tensor(out=tsin[:, n:2 * n], in0=tcos[:, 0:n], scalar=ci, in1=t[1][:, 0:n], op0=ALU.mult, op1=ALU.add),
                                 reads=[btab, bpw, bt[1]], writes=[btab])
                            c.op('dve', lambda e: e.scalar_tensor_tensor(out=tcos[:, n:2 * n], in0=tcos[:, 0:n], scalar=cr, in1=t[0][:, 0:n], op0=ALU.mult, op1=ALU.subtract),
                                 reads=[btab, bpw, bt[0]], writes=[btab])
                            n *= 2; kk += 1
                        cTB = pwc[:, kk, col:col + 1]; sTB = pws[:, kk, col:col + 1]
                        rho = mag[:, col:col + 1]
                        c.op('pool', lambda e: e.memset(ini[:], 0.0), writes=[bini])
                        for tb in range(NTB5):
                            ts0 = tb * TB5
                            for part in range(2):
                                for hf in range(TB5 // 512):
                                    ps, bps = g.ps[npz % 4]; npz += 1
                                    if k < 3:
                                        lh = WB[part][32 * k:32 * k + 32, r, cb, :]; rh = ub[r][32 * k:32 * k + 32, ts0 + hf * 512:ts0 + (hf + 1) * 512]
                                    else:
                                        lh = WBx[part][64:128, r, cb, :]; rh = ub[r][64:128, ts0 + hf * 512:ts0 + (hf + 1) * 512]
                                    c.op('pe', lambda e: e.matmul(ps[:, :], lhsT=lh, rhs=rh, start=True, stop=True),
                                         reads=[bWB, bub[r]], writes=[bps])
                                    c.op('act', lambda e: e.activation(out=BUs[part][:, hf * 512:(hf + 1) * 512], in_=ps[:, :], func=AF.Copy), reads=[bps], writes=[bBUs[part]])
                            c.op('dve', lambda e: e.tensor_tensor(out=t[0][:], in0=BUs[0][:], in1=tcos[:], op=ALU.mult), reads=[bBUs[0], btab], writes=[bt[0]])
                            c.op('pool', lambda e: e.tensor_tensor(out=t[1][:], in0=BUs[1][:], in1=tsin[:], op=ALU.mult), reads=[bBUs[1], btab], writes=[bt[1]])
                            c.op('dve', lambda e: e.tensor_tensor(out=t[0][:], in0=t[0][:], in1=t[1][:], op=ALU.add), reads=[bt[0], bt[1]], writes=[bt[0]])
                            c.op('pool', lambda e: e.tensor_tensor(out=t[2][:], in0=BUs[1][:], in1=tcos[:], op=ALU.mult), reads=[bBUs[1], btab], writes=[bt[2]])
                            c.op('dve', lambda e: e.tensor_tensor(out=t[3][:], in0=BUs[0][:], in1=tsin[:], op=ALU.mult), reads=[bBUs[0], btab], writes=[bt[3]])
                            c.op('pool', lambda e: e.tensor_tensor(out=t[2][:], in0=t[2][:], in1=t[3][:], op=ALU.subtract), reads=[bt[2], bt[3]], writes=[bt[2]])
                            c.op('dve', lambda e: e.tensor_tensor_scan(out=t[1][:], data0=rho.broadcast_to([128, TB5]), data1=t[0][:], initial=ini[:, 0:1], op0=ALU.mult, op1=ALU.add),
                                 reads=[bmag, bt[0], bini], writes=[bt[1]])
                            c.op('dve', lambda e: e.tensor_tensor_scan(out=t[3][:], data0=rho.broadcast_to([128, TB5]), data1=t[2][:], initial=ini[:, 1:2], op0=ALU.mult, op1=ALU.add),
                                 reads=[bmag, bt[2], bini], writes=[bt[3]])
                            if tb < NTB5 - 1:
                                xr = t[1][:, TB5 - 1:TB5]; xi = t[3][:, TB5 - 1:TB5]
                                c.op('dve', lambda e: e.tensor_scalar(out=ini[:, 2:3], in0=xi, scalar1=sTB, scalar2=None, op0=ALU.mult), reads=[bt[3], bpw], writes=[bini])
                                c.op('dve', lambda e: e.scalar_tensor_tensor(out=ini[:, 0:1], in0=xr, scalar=cTB, in1=ini[:, 2:3], op0=ALU.mult, op1=ALU.subtract), reads=[bt[1], bpw, bini], writes=[bini])
                                c.op('dve', lambda e: e.tensor_scalar(out=ini[:, 3:4], in0=xi, scalar1=cTB, scalar2=None, op0=ALU.mult), reads=[bt[3], bpw], writes=[bini])
                                c.op('dve', lambda e: e.scalar_tensor_tensor(out=ini[:, 1:2], in0=xr, scalar=sTB, in1=ini[:, 3:4], op0=ALU.mult, op1=ALU.add), reads=[bt[1], bpw, bini], writes=[bini])
                            if r == 0:
                                oslc = slice(ts0, ts0 + TB5)
                                xo_re = Xa[0][r][:, oslc]; xo_im = Xa[1][r][:, oslc]
                            else:
                                lo = L - ts0 - TB5
                                xo_re = Xa[0][r][:, lo:lo + TB5][:, ::-1]; xo_im = Xa[1][r][:, lo:lo + TB5][:, ::-1]
                            c.op('dve', lambda e: e.tensor_tensor(out=t[0][:], in0=t[1][:], in1=tcos[:], op=ALU.mult), reads=[bt[1], btab], writes=[bt[0]])
                            c.op('pool', lambda e: e.tensor_tensor(out=t[2][:], in0=t[3][:], in1=tsin[:], op=ALU.mult), reads=[bt[3], btab], writes=[bt[2]])
                            c.op('pool', lambda e: e.tensor_tensor(out=xo_re, in0=t[0][:], in1=t[2][:], op=ALU.subtract), reads=[bt[0], bt[2]], pwrites=[bXa[0][r]])
                            c.op('dve', lambda e: e.tensor_tensor(out=t[0][:], in0=t[1][:], in1=tsin[:], op=ALU.mult), reads=[bt[1], btab], writes=[bt[0]])
                            c.op('pool', lambda e: e.tensor_tensor(out=t[2][:], in0=t[3][:], in1=tcos[:], op=ALU.mult), reads=[bt[3], btab], writes=[bt[2]])
                            c.op('dve', lambda e: e.tensor_tensor(out=xo_im, in0=t[0][:], in1=t[2][:], op=ALU.add), reads=[bt[0], bt[2]], pwrites=[bXa[1][r]])
                        bXa[0][r].seal(); bXa[1][r].seal()
                    for it in range(L // 512):
                        ps, bps = g.ps[4 + it % 2]
                        i = 0
                        for r in range(2):
                            for part in range(2):
                                if k < 3:
                                    po = ps[32 * k:32 * k + 32, :]; lh = WC[part][:, r, cb, 32 * k:32 * k + 32]
                                else:
                                    po = ps[64:128, :]; lh = WCx[part][:, r, cb, :]
                                c.op('pe', lambda e: e.matmul(po, lhsT=lh, rhs=Xa[part][r][:, it * 512:(it + 1) * 512], start=(i == 0), stop=(i == 3)),
                                     reads=[bWC, bXa[part][r]], writes=[bps])
                                i += 1
                        ys, bys = yst[it % 2], byst[it % 2]
                        e0 = 32 * k if k < 3 else 64
                        c.op('act', lambda e: e.activation(out=ys[e0:32 * k + 32, :], in_=ps[e0:32 * k + 32, :], func=AF.Copy), reads=[bps], writes=[bys])
                        c.dma('sp', lambda e: e.dma_start(out=YT[cb * 128 + 32 * k:cb * 128 + 32 * k + 32, it * 512:(it + 1) * 512], in_=ys[32 * k:32 * k + 32, :]), reads=[bys], pwrites=[bYT])
            bYT.seal()
            barrier(c)
        if stage < 3:
            return
        with ExitStack() as es:
            def T(name, shape, dt):
                return es.enter_context(nc.sbuf_tensor(uniq(name), shape, dt))
            gw = T("s5_gw", [128, 4, 512], BF16); bgw = Buf()
            dcol = T("s5_dcol", [128, 4], F32); bdcol = Buf()
            gbc = T("s5_gbc", [128, 4], F32); bgbc = Buf()
            yt = [T("s5_yt%d" % i, [128, 4, 512], F32) for i in range(2)]; byt = [Buf(), Buf()]
            ut = [T("s5_ut%d" % i, [128, 4, 512], F32) for i in range(2)]; but = [Buf(), Buf()]
            sq = T("s5_sq", [128, 4, 512], F32); bsq = Buf()
            gy = T("s5_gy", [128, 4, 512], F32); bgy = Buf()
            gyb = T("s5_gyb", [128, 4, 512], BF16); bgyb = Buf()
            sg = [T("s5_sg%d" % i, [128, 512], F32) for i in range(2)]; bsg = [Buf(), Buf()]
            ob = [T("s5_ob%d" % i, [128, 4, 512], BF16) for i in range(2)]; bob = [Buf(), Buf()]
            c.dma('pool', lambda e: e.dma_start(out=gw[:], in_=glu_w.rearrange("(k p) f -> p k f", p=128)), writes=[bgw])
            with nc.allow_non_contiguous_dma(reason="small params"):
                c.dma('sp', lambda e: e.dma_start(out=dcol[:], in_=d_skip.rearrange("(k p) -> p k", p=128)), writes=[bdcol])
                c.dma('sp', lambda e: e.dma_start(out=gbc[:], in_=glu_b.rearrange("(k p) -> p k", p=128)), writes=[bgbc])
            GC = 1.5957691216057308
            for it in range(L // 512):
                yt_, byt_ = yt[it % 2], byt[it % 2]
                ut_, but_ = ut[it % 2], but[it % 2]
                ob_, bob_ = ob[it % 2], bob[it % 2]
                tsl = slice(it * 512, (it + 1) * 512)
                c.dma('sp', lambda e: e.dma_start(out=yt_[:], in_=YT[:, tsl].rearrange("(k p) t -> p k t", p=128)), reads=[bYT], writes=[byt_])
                c.dma('act', lambda e: e.dma_start(out=ut_[:], in_=PT[U0:U0 + 512, tsl].rearrange("(k p) t -> p k t", p=128)), reads=[bPT], writes=[but_])
                for k in range(4):
                    c.op('dve', lambda e: e.scalar_tensor_tensor(out=yt_[:, k, :], in0=ut_[:, k, :], scalar=dcol[:, k:k + 1], in1=yt_[:, k, :], op0=ALU.mult, op1=ALU.add),
                         reads=[but_, bdcol, byt_], writes=[byt_])
                c.op('pool', lambda e: e.tensor_tensor(out=sq[:], in0=yt_[:], in1=yt_[:], op=ALU.mult), reads=[byt_], writes=[bsq])
                c.op('dve', lambda e: e.tensor_scalar(out=sq[:], in0=sq[:], scalar1=0.044715, scalar2=1.0, op0=ALU.mult, op1=ALU.add), reads=[bsq], writes=[bsq])
                c.op('pool', lambda e: e.tensor_tensor(out=sq[:], in0=sq[:], in1=yt_[:], op=ALU.mult), reads=[bsq, byt_], writes=[bsq])
                c.op('act', lambda e: e.activation(out=sq[:], in_=sq[:], func=AF.Sigmoid, scale=GC), reads=[bsq], writes=[bsq])
                c.op('dve', lambda e: e.tensor_tensor(out=gy[:], in0=sq[:], in1=yt_[:], op=ALU.mult), reads=[bsq, byt_], writes=[bgy])
                c.op('act', lambda e: e.activation(out=gyb[:], in_=gy[:], func=AF.Copy), reads=[bgy], writes=[bgyb])
                for co in range(4):
                    ps, bps = g.ps[co % 4]
                    for k in range(4):
                        c.op('pe', lambda e: e.matmul(ps[:, :], lhsT=gw[:, k, co * 128:(co + 1) * 128], rhs=gyb[:, k, :], start=(k == 0), stop=(k == 3)),
                             reads=[bgw, bgyb], writes=[bps])
                    sg_, bsg_ = sg[co % 2], bsg[co % 2]
                    c.op('act', lambda e: e.activation(out=sg_[:], in_=ps[:, :], func=AF.Sigmoid, bias=gbc[:, co:co + 1], scale=1.0), reads=[bps, bgbc], writes=[bsg_])
                    c.op('dve', lambda e: e.tensor_tensor(out=ob_[:, co, :], in0=gy[:, co, :], in1=sg_[:], op=ALU.mult), reads=[bgy, bsg_, bob_], writes=[bob_])
                c.dma('sp', lambda e: e.dma_start(out=MT[512:1024, tsl].rearrange("(k p) t -> p k t", p=128), in_=ob_[:]), reads=[bob_], pwrites=[bMT])
            barrier(c)


def outproj_phase(c, g, MT, bMT, w_out, X, bX):
    nc = c.nc
    bXn = Buf('Xn')
    with ExitStack() as es:
        def T(name, shape, dt):
            return es.enter_context(nc.sbuf_tensor(uniq(name), shape, dt))
        wsb = T("op_w", [128, 8, D], BF16); bw = Buf()
        mt = [T("op_mt%d" % i, [128, 8, 512], BF16) for i in range(2)]; bmt = [Buf(), Buf()]
        xt = [T("op_xt%d" % i, [128, 4, D], F32) for i in range(2)]; bxt = [Buf(), Buf()]
        xo = [T("op_xo%d" % i, [128, 4, D], F32) for i in range(2)]; bxo = [Buf(), Buf()]
        for c0 in range(0, D, 512):
            c.dma('pool', lambda e: e.dma_start(out=wsb[:, :, c0:c0 + 512], in_=w_out[:, c0:c0 + 512].rearrange("(k p) f -> p k f", p=128)), pwrites=[bw])
        bw.seal()
        n = 0
        for it in range(L // 512):
            t0 = it * 512
            mt_, bmt_ = mt[it % 2], bmt[it % 2]
            xt_, bxt_ = xt[it % 2], bxt[it % 2]
            xo_, bxo_ = xo[it % 2], bxo[it % 2]
            c.dma('sp', lambda e: e.dma_start(out=mt_[:], in_=MT[:, t0:t0 + 512].rearrange("(k p) t -> p k t", p=128)), reads=[bMT], writes=[bmt_])
            c.dma('act', lambda e: e.dma_start(out=xt_[:], in_=X[t0:t0 + 512, :].rearrange("(j p) d -> p j d", p=128)), reads=[bX], writes=[bxt_])
            for j in range(4):
                for dh in range(2):
                    ps, bps = g.ps[n % 4]; n += 1
                    for k in range(8):
                        c.op('pe', lambda e: e.matmul(ps[:, :], lhsT=mt_[:, k, j * 128:(j + 1) * 128], rhs=wsb[:, k, dh * 512:(dh + 1) * 512], start=(k == 0), stop=(k == 7)),
                             reads=[bmt_, bw], writes=[bps])
                    c.op('dve', lambda e: e.tensor_tensor(out=xo_[:, j, dh * 512:(dh + 1) * 512], in0=ps[:, :], in1=xt_[:, j, dh * 512:(dh + 1) * 512], op=ALU.add),
                         reads=[bps, bxt_, bxo_], writes=[bxo_])
            c.dma('sp', lambda e: e.dma_start(out=X[t0:t0 + 512, :].rearrange("(j p) d -> p j d", p=128), in_=xo_[:]), reads=[bxo_], pwrites=[bXn])
        bXn.seal()
        barrier(c)
    return bXn


def t5_onehot():
    half = 16; max_exact = 8
    rel = np.arange(-255, 256)
    n = np.abs(rel)
    nf = np.maximum(n, 1).astype(np.float32)
    large = max_exact + (np.log(nf / np.float32(max_exact)) / np.float32(math.log(128 / max_exact)) * np.float32(half - max_exact)).astype(np.int32)
    large = np.minimum(large, half - 1)
    b = np.where(rel > 0, half, 0) + np.where(n < max_exact, n, large)
    oh = np.zeros((32, 512), np.float32)
    oh[b, np.arange(511)] = 1.0
    return oh


def attn_phase(c, g, PV, bPV, q_gain, k_gain, c_lambda, out_gain, rel_bias, onehot, layer_idx, QKT, bQKT, FV, bFV, MT, bMT, stage=3):
    nc = c.nc
    lam_init = 0.8 - 0.6 * math.exp(-0.3 * layer_idx)
    ptb, bptb = g.psb
    with ExitStack() as es:
        def T(name, shape, dt):
            return es.enter_context(nc.sbuf_tensor(uniq(name), shape, dt))
        g64 = T("at_g64", [128, 2, 64], F32); bg64 = Buf()
        gQK = T("at_gQK", [128, 16, 64], F32); bgQK = Buf()
        xq = [T("at_xq%d" % i, [128, 1024], BF16) for i in range(2)]; bxq = [Buf(), Buf()]
        sq = T("at_sq", [128, 1024], F32); bsq = Buf()
        ss = [T("at_ss%d" % i, [128, 16], F32) for i in range(2)]; bss = [Buf(), Buf()]
        xn = T("at_xn", [128, 1024], F32); bxn = Buf()
        xb = [T("at_xb%d" % i, [128, 1024], BF16) for i in range(2)]; bxb = [Buf(), Buf()]
        st = [T("at_st%d" % i, [128, 8, 512], BF16) for i in range(2)]; bst = [Buf(), Buf()]
        c.dma('sp', lambda e: e.dma_start(out=g64[:, 0, :], in_=q_gain.partition_broadcast(128)), pwrites=[bg64])
        c.dma('sp', lambda e: e.dma_start(out=g64[:, 1, :], in_=k_gain.partition_broadcast(128)), pwrites=[bg64])
        bg64.seal()
        c.op('dve', lambda e: e.tensor_scalar(out=gQK[:, 0:8, :], in0=g64[:, 0:1, :].broadcast_to([128, 8, 64]), scalar1=0.125, scalar2=None, op0=ALU.mult), reads=[bg64], writes=[bgQK])
        c.op('dve', lambda e: e.tensor_copy(out=gQK[:, 8:16, :], in_=g64[:, 1:2, :].broadcast_to([128, 8, 64])), reads=[bg64, bgQK], writes=[bgQK])
        for i in range(NT):
            xq_, bxq_ = xq[i % 2], bxq[i % 2]
            ss_, bss_ = ss[i % 2], bss[i % 2]
            xb_, bxb_ = xb[i % 2], bxb[i % 2]
            st_, bst_ = st[(i // 4) % 2], bst[(i // 4) % 2]
            c.dma('sp', lambda e: e.dma_start(out=xq_[:], in_=PV[i * 128:(i + 1) * 128, 0:1024]), reads=[bPV], writes=[bxq_])
            c.op('pool', lambda e: e.tensor_tensor(out=sq[:], in0=xq_[:], in1=xq_[:], op=ALU.mult), reads=[bxq_], writes=[bsq])
            c.op('dve', lambda e: e.tensor_reduce(out=ss_[:], in_=sq[:].rearrange("p (a d) -> p a d", d=64), axis=AX.X, op=ALU.add), reads=[bsq], writes=[bss_])
            c.op('dve', lambda e: e.tensor_scalar(out=ss_[:], in0=ss_[:], scalar1=1.0 / 64, scalar2=1e-6, op0=ALU.mult, op1=ALU.add), reads=[bss_], writes=[bss_])
            c.op('pool', lambda e: e.tensor_tensor(out=ss_[:], in0=ss_[:], in1=g.neghalf[:, 0:1].broadcast_to([128, 16]), op=ALU.pow), reads=[bss_, g.b_neghalf], writes=[bss_])
            c.op('dve', lambda e: e.tensor_tensor(out=xn[:].rearrange("p (a d) -> p a d", d=64), in0=xq_[:].rearrange("p (a d) -> p a d", d=64),
                                                  in1=ss_[:].unsqueeze(2).broadcast_to([128, 16, 64]), op=ALU.mult), reads=[bxq_, bss_], writes=[bxn])
            c.op('pool', lambda e: e.tensor_tensor(out=xb_[:], in0=xn[:], in1=gQK[:].rearrange("p a d -> p (a d)"), op=ALU.mult), reads=[bxn, bgQK], writes=[bxb_])
            for a in range(8):
                c.op('pe', lambda e: e.transpose(out=ptb[:, a * 128:(a + 1) * 128], in_=xb_[:, a * 128:(a + 1) * 128], identity=g.identb[:]), reads=[bxb_, g.b_identb], writes=[bptb])
            c.op('act', lambda e: e.activation(out=st_[:, :, (i % 4) * 128:(i % 4 + 1) * 128], in_=ptb[:, :].rearrange("p (a s) -> p a s", a=8), func=AF.Copy), reads=[bptb], writes=[bst_])
            if i % 4 == 3:
                t0 = (i // 4) * 512
                for a in range(8):
                    c.dma('sp' if a % 2 else 'act', lambda e: e.dma_start(out=QKT[a, :, t0:t0 + 512], in_=st_[:, a, :]), reads=[bst_], pwrites=[bQKT])
        bQKT.seal()
        barrier(c)
    if stage < 2:
        return
    with ExitStack() as es:
        def T(name, shape, dt):
            return es.enter_context(nc.sbuf_tensor(uniq(name), shape, dt))
        KT = T("at_KT", [128, L], BF16); bKT = Buf()
        Va = T("at_Va", [128, 64, 130], BF16); bVa = Buf()
        QT = [T("at_QT%d" % i, [128, 512], BF16) for i in range(2)]; bQT = [Buf(), Buf()]
        Pt = [T("at_P%d" % i, [128, 512], BF16) for i in range(3)]; bPt = [Buf() for _ in range(3)]
        tmp = [T("at_tmp%d" % i, [128, 512], F32) for i in range(2)]; btmp = [Buf(), Buf()]
        biasT = T("at_bias", [128, 4, 3, 128], F32); bbias = Buf()
        hank = T("at_hank", [128, 128], F32); bhank = Buf()
        cfar = T("at_cfar", [128, 4, 2], F32); bcfar = Buf()
        tab = T("at_tab", [32, 4], F32); btab = Buf()
        oh = T("at_oh", [32, 512], F32); boh = Buf()
        fv = T("at_fv", [4, 512], F32); bfv = Buf()
        lamt = T("at_lamt", [128, 4, 64], F32); blamt = Buf()
        lam = T("at_lam", [128, 8], F32); blam = Buf()
        gO = T("at_gO", [128, 128], F32); bgO = Buf()
        rs = [T("at_rs%d" % i, [128, 4], F32) for i in range(2)]; brs = [Buf(), Buf()]
        t1 = T("at_t1", [128, 128], F32); bt1 = Buf()
        w_ = T("at_w", [128, 128], F32); bw_ = Buf()
        junk = T("at_junk", [128, 128], F32); bjunk = Buf()
        wb = [T("at_wb%d" % i, [128, 128], BF16) for i in range(2)]; bwb = [Buf(), Buf()]
        ost = [T("at_ost%d" % i, [128, 512], BF16) for i in range(2)]; bost = [Buf(), Buf()]
        c.dma('sp', lambda e: e.dma_start(out=lamt[:].rearrange("p a d -> p (a d)"), in_=c_lambda.rearrange("a d -> (a d)").partition_broadcast(128)), writes=[blamt])
        c.op('dve', lambda e: e.tensor_tensor(out=lamt[:, 0, :], in0=lamt[:, 0, :], in1=lamt[:, 1, :], op=ALU.mult), reads=[blamt], writes=[blamt])
        c.op('dve', lambda e: e.tensor_tensor(out=lamt[:, 2, :], in0=lamt[:, 2, :], in1=lamt[:, 3, :], op=ALU.mult), reads=[blamt], writes=[blamt])
        c.op('dve', lambda e: e.tensor_reduce(out=lam[:, 0:1], in_=lamt[:, 0, :], axis=AX.X, op=ALU.add), reads=[blamt], writes=[blam])
        c.op('dve', lambda e: e.tensor_reduce(out=lam[:, 1:2], in_=lamt[:, 2, :], axis=AX.X, op=ALU.add), reads=[blamt, blam], writes=[blam])
        c.op('act', lambda e: e.activation(out=lam[:, 2:4], in_=lam[:, 0:2], func=AF.Exp), reads=[blam], writes=[blam])
        c.op('dve', lambda e: e.tensor_tensor(out=lam[:, 4:5], in0=lam[:, 3:4], in1=lam[:, 2:3], op=ALU.subtract), reads=[blam], writes=[blam])
        c.op('dve', lambda e: e.tensor_scalar(out=lam[:, 4:5], in0=lam[:, 4:5], scalar1=-lam_init, scalar2=None, op0=ALU.add), reads=[blam], writes=[blam])
        c.dma('sp', lambda e: e.dma_start(out=gO[:], in_=out_gain.partition_broadcast(128)), writes=[bgO])
        c.op('dve', lambda e: e.tensor_scalar(out=gO[:], in0=gO[:], scalar1=1.0 - lam_init, scalar2=None, op0=ALU.mult), reads=[bgO], writes=[bgO])
        c.dma('sp', lambda e: e.dma_start(out=tab[:], in_=rel_bias), writes=[btab])
        c.dma('act', lambda e: e.dma_start(out=oh[:], in_=onehot), writes=[boh])
        ps6, bps6 = g.ps[6]
        c.op('pe', lambda e: e.matmul(ps6[0:4, :], lhsT=tab[:, :], rhs=oh[:, :], start=True, stop=True), reads=[btab, boh], writes=[bps6])
        c.op('dve', lambda e: e.tensor_copy(out=fv[:], in_=ps6[0:4, :]), reads=[bps6], writes=[bfv])
        c.dma('sp', lambda e: e.dma_start(out=FV, in_=fv[:]), reads=[bfv], writes=[bFV])
        for h in range(4):
            for o in (-1, 0, 1):
                off = h * 512 + 128 * o + 128
                src = bass.AP(FV.tensor, off, [[1, 128], [1, 128]])
                c.dma('sp', lambda e: e.dma_start(out=hank[:], in_=src), reads=[bFV], writes=[bhank])
                c.op('dve', lambda e: e.tensor_copy(out=biasT[:, h, o + 1, :], in_=hank[:, ::-1]), reads=[bhank, bbias], writes=[bbias])
            c.dma('sp', lambda e: e.dma_start(out=cfar[:, h, 0:1], in_=bass.AP(FV.tensor, h * 512 + 0, [[0, 128], [1, 1]])), reads=[bFV], pwrites=[bcfar])
            c.dma('sp', lambda e: e.dma_start(out=cfar[:, h, 1:2], in_=bass.AP(FV.tensor, h * 512 + 510, [[0, 128], [1, 1]])), reads=[bFV], pwrites=[bcfar])
        bcfar.seal()
        ones_col_done = False
        nS = 0; nP = 0; nq = 0; ntmp = 0; nout = 0
        for h in range(4):
            c.dma('sp', lambda e: e.dma_start(out=KT[:], in_=QKT[4 + h, :, :]), reads=[bQKT], writes=[bKT])
            for half in range(2):
                c.dma('act', lambda e: e.dma_start(out=Va[:, half * 32:(half + 1) * 32, 0:128], in_=PV[half * 4096:(half + 1) * 4096, 1024 + h * 128:1024 + (h + 1) * 128].rearrange("(b p) d -> p b d", p=128)),
                      reads=[bPV], writes=[bVa])
            c.op('pool', lambda e: e.memset(Va[:, :, 128:129], 1.0), reads=[bVa], writes=[bVa])
            for qt in range(16):
                QT_, bQT_ = QT[nq % 2], bQT[nq % 2]; nq += 1
                c.dma('sp', lambda e: e.dma_start(out=QT_[:], in_=QKT[h, :, qt * 512:(qt + 1) * 512]), reads=[bQKT], writes=[bQT_])
                for comp in range(2):
                    for kb in range(64):
                        S, bS = g.ps[nS % 3]; nS += 1
                        P_, bP_ = Pt[nP % 3], bPt[nP % 3]; nP += 1
                        c.op('pe', lambda e: e.matmul(S[:, :], lhsT=KT[64 * comp:64 * comp + 64, kb * 128:(kb + 1) * 128], rhs=QT_[64 * comp:64 * comp + 64, :], start=True, stop=True),
                             reads=[bKT, bQT_], writes=[bS])
                        near = (4 * qt - 1 <= kb <= 4 * qt + 4)
                        if not near:
                            col = cfar[:, h, 0:1] if kb < 4 * qt else cfar[:, h, 1:2]
                            c.op('act', lambda e: e.activation(out=P_[:], in_=S[:, :], func=AF.Exp, bias=col, scale=1.0), reads=[bS, bcfar], writes=[bP_])
                        else:
                            tm, btm = tmp[ntmp % 2], btmp[ntmp % 2]; ntmp += 1
                            for qs in range(4):
                                o = kb - (4 * qt + qs)
                                sl = slice(qs * 128, (qs + 1) * 128)
                                if abs(o) <= 1:
                                    c.op('dve', lambda e: e.tensor_tensor(out=tm[:, sl], in0=S[:, sl], in1=biasT[:, h, o + 1, :], op=ALU.add), reads=[bS, bbias, btm], writes=[btm])
                                else:
                                    col = cfar[:, h, 0:1] if o < 0 else cfar[:, h, 1:2]
                                    c.op('dve', lambda e: e.tensor_scalar(out=tm[:, sl], in0=S[:, sl], scalar1=col, scalar2=None, op0=ALU.add), reads=[bS, bcfar, btm], writes=[btm])
                            c.op('act', lambda e: e.activation(out=P_[:], in_=tm[:], func=AF.Exp), reads=[btm], writes=[bP_])
                        for qs in range(4):
                            a = comp * 4 + qs
                            acc, bacc = g.ps[3 + a // 3]
                            c0 = (a % 3) * 130
                            first = (kb == 0) and ((comp == 0 and a in (0, 3)) or (comp == 1 and a == 6))
                            c.op('pe', lambda e: e.matmul(acc[:, c0:c0 + 129], lhsT=P_[:, qs * 128:(qs + 1) * 128], rhs=Va[:, kb, 0:129], start=first, stop=(kb == 63), skip_group_check=True),
                                 reads=[bP_, bVa], writes=[bacc])
                os_, bos_ = ost[nout % 2], bost[nout % 2]; nout += 1
                for qs in range(4):
                    a0 = qs; a1 = 4 + qs
                    acc0, bacc0 = g.ps[3 + a0 // 3]; o0 = (a0 % 3) * 130
                    acc1, bacc1 = g.ps[3 + a1 // 3]; o1 = (a1 % 3) * 130
                    rs_, brs_ = rs[qs % 2], brs[qs % 2]
                    wb_, bwb_ = wb[qs % 2], bwb[qs % 2]
                    c.op('dve', lambda e: e.reciprocal(out=rs_[:, 0:1], in_=acc0[:, o0 + 128:o0 + 129]), reads=[bacc0], writes=[brs_])
                    c.op('dve', lambda e: e.reciprocal(out=rs_[:, 1:2], in_=acc1[:, o1 + 128:o1 + 129]), reads=[bacc1, brs_], writes=[brs_])
                    c.op('dve', lambda e: e.tensor_tensor(out=rs_[:, 1:2], in0=rs_[:, 1:2], in1=lam[:, 4:5], op=ALU.mult), reads=[brs_, blam], writes=[brs_])
                    c.op('dve', lambda e: e.tensor_scalar(out=t1[:], in0=acc1[:, o1:o1 + 128], scalar1=rs_[:, 1:2], scalar2=None, op0=ALU.mult), reads=[bacc1, brs_], writes=[bt1])
                    c.op('dve', lambda e: e.scalar_tensor_tensor(out=w_[:], in0=acc0[:, o0:o0 + 128], scalar=rs_[:, 0:1], in1=t1[:], op0=ALU.mult, op1=ALU.add), reads=[bacc0, brs_, bt1], writes=[bw_])
                    c.op('dve', lambda e: e.scalar_tensor_tensor(out=junk[:], in0=w_[:], scalar=1.0, in1=w_[:], op0=ALU.mult, op1=ALU.mult, accum_out=rs_[:, 2:3]), reads=[bw_, brs_], writes=[bjunk, brs_])
                    c.op('dve', lambda e: e.tensor_scalar(out=rs_[:, 2:3], in0=rs_[:, 2:3], scalar1=1.0 / 128, scalar2=1e-6, op0=ALU.mult, op1=ALU.add), reads=[brs_], writes=[brs_])
                    c.op('pool', lambda e: e.tensor_tensor(out=rs_[:, 2:3], in0=rs_[:, 2:3], in1=g.neghalf[:, 0:1], op=ALU.pow), reads=[brs_, g.b_neghalf], writes=[brs_])
                    c.op('dve', lambda e: e.scalar_tensor_tensor(out=wb_[:], in0=w_[:], scalar=rs_[:, 2:3], in1=gO[:], op0=ALU.mult, op1=ALU.mult), reads=[bw_, brs_, bgO], writes=[bwb_])
                    c.op('pe', lambda e: e.transpose(out=ptb[:, qs * 128:(qs + 1) * 128], in_=wb_[:], identity=g.identb[:]), reads=[bwb_, g.b_identb], writes=[bptb])
                c.op('act', lambda e: e.activation(out=os_[:], in_=ptb[:, 0:512], func=AF.Copy), reads=[bptb], writes=[bos_])
                c.dma('sp', lambda e: e.dma_start(out=MT[h * 128:(h + 1) * 128, qt * 512:(qt + 1) * 512], in_=os_[:]), reads=[bos_], pwrites=[bMT])
        barrier(c)


GTB = 512


def gated_norm_finalize(c, g, OA, bOAs, PV, bPV, gcol0, gain_ap, MT, bMT, row0, pfx):
    nc = c.nc
    with ExitStack() as es:
        def T(name, shape, dt):
            return es.enter_context(nc.sbuf_tensor(uniq(pfx + name), shape, dt))
        gA = T("gA", [128, 128], F32); bgA = Buf()
        oa = [T("oa%d" % i, [128, 512], F32) for i in range(2)]; boa = [Buf(), Buf()]
        ga = [T("ga%d" % i, [128, 512], BF16) for i in range(2)]; bga = [Buf(), Buf()]
        sq = T("sq", [128, 512], F32); bsq = Buf()
        ssq = [T("ssq%d" % i, [128, 4], F32) for i in range(2)]; bssq = [Buf(), Buf()]
        sg = T("sg", [128, 512], F32); bsg = Buf()
        t1 = T("t1", [128, 512], F32); bt1 = Buf()
        ob = [T("ob%d" % i, [128, 512], BF16) for i in range(2)]; bob = [Buf(), Buf()]
        mt = [T("mt%d" % i, [128, 4, 512], BF16) for i in range(2)]; bmt = [Buf(), Buf()]
        c.dma('sp', lambda e: e.dma_start(out=gA[:], in_=gain_ap.partition_broadcast(128)), writes=[bgA])
        ptb, bptb = g.psb
        for i in range(NT):
            oa_, boa_ = oa[i % 2], boa[i % 2]
            ga_, bga_ = ga[i % 2], bga[i % 2]
            ss_, bss_ = ssq[i % 2], bssq[i % 2]
            ob_, bob_ = ob[i % 2], bob[i % 2]
            mt_, bmt_ = mt[(i // 4) % 2], bmt[(i // 4) % 2]
            c.dma('sp', lambda e: e.dma_start(out=oa_[:], in_=OA[i * 128:(i + 1) * 128, :]), reads=bOAs, writes=[boa_])
            c.dma('act', lambda e: e.dma_start(out=ga_[:], in_=PV[i * 128:(i + 1) * 128, gcol0:gcol0 + 512]), reads=[bPV], writes=[bga_])
            c.op('pool', lambda e: e.tensor_tensor(out=sq[:], in0=oa_[:], in1=oa_[:], op=ALU.mult), reads=[boa_], writes=[bsq])
            c.op('dve', lambda e: e.tensor_reduce(out=ss_[:], in_=sq[:].rearrange("p (h d) -> p h d", h=4), axis=AX.X, op=ALU.add), reads=[bsq], writes=[bss_])
            c.op('dve', lambda e: e.tensor_scalar(out=ss_[:], in0=ss_[:], scalar1=1.0 / 128, scalar2=1e-6, op0=ALU.mult, op1=ALU.add), reads=[bss_], writes=[bss_])
            c.op('pool', lambda e: e.tensor_tensor(out=ss_[:], in0=ss_[:], in1=g.neghalf[:, 0:1].broadcast_to([128, 4]), op=ALU.pow), reads=[bss_, g.b_neghalf], writes=[bss_])
            c.op('act', lambda e: e.activation(out=sg[:], in_=ga_[:], func=AF.Silu), reads=[bga_], writes=[bsg])
            c.op('dve', lambda e: e.tensor_tensor(out=t1[:].rearrange("p (h d) -> p h d", h=4), in0=oa_[:].rearrange("p (h d) -> p h d", h=4),
                                                  in1=ss_[:].unsqueeze(2).broadcast_to([128, 4, 128]), op=ALU.mult), reads=[boa_, bss_], writes=[bt1])
            c.op('pool', lambda e: e.tensor_tensor(out=t1[:].rearrange("p (h d) -> p h d", h=4), in0=t1[:].rearrange("p (h d) -> p h d", h=4),
                                                   in1=gA[:].unsqueeze(1).broadcast_to([128, 4, 128]), op=ALU.mult), reads=[bt1, bgA], writes=[bt1])
            c.op('dve', lambda e: e.tensor_tensor(out=ob_[:], in0=t1[:], in1=sg[:], op=ALU.mult), reads=[bt1, bsg], writes=[bob_])
            for k in range(4):
                c.op('pe', lambda e: e.transpose(out=ptb[:, k * 128:(k + 1) * 128], in_=ob_[:, k * 128:(k + 1) * 128], identity=g.identb[:]),
                     reads=[bob_, g.b_identb], writes=[bptb])
            c.op('act', lambda e: e.activation(out=mt_[:, :, (i % 4) * 128:(i % 4 + 1) * 128], in_=ptb[:, 0:512].rearrange("p (k s) -> p k s", k=4), func=AF.Copy),
                 reads=[bptb], writes=[bmt_])
            if i % 4 == 3:
                t0 = (i // 4) * 512
                c.dma('sp', lambda e: e.dma_start(out=MT[row0:row0 + 512, t0:t0 + 512].rearrange("(k p) t -> p k t", p=128), in_=mt_[:]), reads=[bmt_], pwrites=[bMT])
        barrier(c)


def gdn_phase(c, g, PT, bPT, PV, bPV, conv_w, a_log, dt_bias, out_gain, GQ, bGQ, GR, bGR, OD, bODs, MT, bMT, stage=4):
    nc = c.nc
    ptb, bptb = g.psb
    NB = TBK
    with ExitStack() as es:
        def T(name, shape, dt):
            return es.enter_context(nc.sbuf_tensor(uniq(name), shape, dt))
        cw = T("gd_cw", [128, 12, 5], F32); bcw = Buf()
        onesb = T("gd_onesb", [128, 128], BF16); bonesb = Buf()
        xin = [T("gd_xin%d" % i, [128, NB + 4], F32) for i in range(2)]; bxin = [Buf(), Buf()]
        y = T("gd_y", [128, NB], F32); by = Buf()
        s = T("gd_s", [128, NB], F32); bs = Buf()
        sqb = T("gd_sqb", [128, NB], BF16); bsqb = Buf()
        rst = T("gd_rst", [128, NB], F32); brst = Buf()
        ob = [T("gd_ob%d" % i, [128, NB], BF16) for i in range(2)]; bob = [Buf(), Buf()]
        with nc.allow_non_contiguous_dma(reason="small params"):
            for j in range(5):
                c.dma('sp', lambda e: e.dma_start(out=cw[:, :, j], in_=conv_w[j, :].rearrange("(k p) -> p k", p=128)), pwrites=[bcw])
        bcw.seal()
        c.op('pool', lambda e: e.memset(onesb[:], 1.0), writes=[bonesb])
        n = 0
        for cbk in range(12):
            for tb in range(L // NB):
                x_, bx_ = xin[n % 2], bxin[n % 2]
                o_, bo_ = ob[n % 2], bob[n % 2]
                n += 1
                t0 = tb * NB
                lo = max(t0 - 2, 0); hi = min(t0 + NB + 2, L)
                if tb == 0:
                    c.op('pool', lambda e: e.memset(x_[:, 0:2], 0.0), writes=[bx_])
                if tb == L // NB - 1:
                    c.op('pool', lambda e: e.memset(x_[:, NB + 2:NB + 4], 0.0), writes=[bx_])
                c.dma('sp', lambda e: e.dma_start(out=x_[:, lo - (t0 - 2):hi - (t0 - 2)], in_=PT[cbk * 128:(cbk + 1) * 128, lo:hi]), reads=[bPT, bx_], writes=[bx_])
                c.op('dve', lambda e: e.tensor_scalar(out=y[:], in0=x_[:, 0:NB], scalar1=cw[:, cbk, 0:1], scalar2=None, op0=ALU.mult), reads=[bx_, bcw], writes=[by])
                for j in range(1, 5):
                    c.op('dve', lambda e: e.scalar_tensor_tensor(out=y[:], in0=x_[:, j:j + NB], scalar=cw[:, cbk, j:j + 1], in1=y[:], op0=ALU.mult, op1=ALU.add),
                         reads=[bx_, bcw, by], writes=[by])
                c.op('act', lambda e: e.activation(out=s[:], in_=y[:], func=AF.Silu), reads=[by], writes=[bs])
                if cbk < 8:
                    c.op('pool', lambda e: e.tensor_tensor(out=sqb[:], in0=s[:], in1=s[:], op=ALU.mult), reads=[bs], writes=[bsqb])
                    for hf in range(NB // 512):
                        ps, bps = g.ps[hf % 4]
                        c.op('pe', lambda e: e.matmul(ps[:, :], lhsT=onesb[:], rhs=sqb[:, hf * 512:(hf + 1) * 512], start=True, stop=True), reads=[bonesb, bsqb], writes=[bps])
                        c.op('dve', lambda e: e.tensor_scalar(out=rst[:, hf * 512:(hf + 1) * 512], in0=ps[:, :], scalar1=1e-6, scalar2=None, op0=ALU.add), reads=[bps, brst], writes=[brst])
                    c.op('pool', lambda e: e.tensor_tensor(out=rst[:], in0=rst[:], in1=g.neghalf[:, 0:1].broadcast_to([128, NB]), op=ALU.pow), reads=[brst, g.b_neghalf], writes=[brst])
                    sc = (128.0 ** -0.5) if cbk < 4 else 1.0
                    c.op('dve', lambda e: e.scalar_tensor_tensor(out=o_[:], in0=s[:], scalar=sc, in1=rst[:], op0=ALU.mult, op1=ALU.mult), reads=[bs, brst], writes=[bo_])
                else:
                    c.op('act', lambda e: e.activation(out=o_[:], in_=s[:], func=AF.Copy), reads=[bs], writes=[bo_])
                c.dma('act', lambda e: e.dma_start(out=GQ[cbk, :, t0:t0 + NB], in_=o_[:]), reads=[bo_], pwrites=[bGQ])
        bGQ.seal()
        barrier(c)
    if stage < 2:
        return
    with ExitStack() as es0:
        def T0(name, shape, dt):
            return es0.enter_context(nc.sbuf_tensor(uniq(name), shape, dt))
        NQ = 5
        cols = [T0("gd_cols%d" % d, [128, 64, 4 * NQ], F32) for d in range(2)]; bcols = [Buf(), Buf()]
        sel = T0("gd_sel", [4, 4, 128], F32); bsel = Buf()
        with ExitStack() as es:
            def T(name, shape, dt):
                return es.enter_context(nc.sbuf_tensor(uniq(name), shape, dt))
            GP = 2048
            ar = T("gd_ar", [4, GP], F32); bar_ = Buf()
            br = T("gd_br", [4, GP], F32); bbr = Buf()
            w1 = T("gd_w1", [4, GP], F32); bw1 = Buf()
            w2 = T("gd_w2", [4, GP], F32); bw2 = Buf()
            gam = T("gd_gam", [4, GP], F32); bet = T("gd_bet", [4, GP], F32); egam = T("gd_egam", [4, GP], F32); brw = Buf()
            q3 = T("gd_q3", [4, GP], F32); bq3 = Buf()
            q4 = T("gd_q4", [4, GP], F32); bq4 = Buf()
            q5 = T("gd_q5", [4, GP], F32); bq5 = Buf()
            msk = T("gd_msk", [4, GP], F32); bmsk = Buf()
            pc = T("gd_pc", [4, 4], F32); bpc = Buf()
            c.op('pool', lambda e: e.memset(msk[:], 1.0), writes=[bmsk])
            c.op('pool', lambda e: e.memset(msk[:].rearrange("p (c j) -> p c j", j=64)[:, :, 0:1], 0.0), reads=[bmsk], writes=[bmsk])
            c.op('pool', lambda e: e.memset(sel[:], 0.0), writes=[bsel])
            c.op('pool', lambda e: e.affine_select(out=sel[:], in_=sel[:], pattern=[[-1, 4], [0, 128]], compare_op=ALU.not_equal, fill=1.0, base=0, channel_multiplier=1),
                 reads=[bsel], writes=[bsel])
            for d in range(2):
                with nc.allow_non_contiguous_dma(reason="small params"):
                    c.dma('sp', lambda e: e.dma_start(out=pc[:, 0:1], in_=dt_bias[d, :].rearrange("(h o) -> h o", o=1)), reads=[bpc], writes=[bpc])
                    c.dma('sp', lambda e: e.dma_start(out=pc[:, 1:2], in_=a_log[d, :].rearrange("(h o) -> h o", o=1)), reads=[bpc], writes=[bpc])
                c.op('act', lambda e: e.activation(out=pc[:, 2:3], in_=pc[:, 1:2], func=AF.Exp), reads=[bpc], writes=[bpc])
                c.op('dve', lambda e: e.tensor_scalar(out=pc[:, 2:3], in0=pc[:, 2:3], scalar1=-1.0, scalar2=None, op0=ALU.mult), reads=[bpc], writes=[bpc])
                for tp in range(L // GP):
                    nbp = tp if d == 0 else L // GP - 1 - tp
                    c.dma('sp', lambda e: e.dma_start(out=ar[:], in_=PT[1536 + 4 * d:1540 + 4 * d, nbp * GP:(nbp + 1) * GP]), reads=[bPT, bar_], writes=[bar_])
                    c.dma('act', lambda e: e.dma_start(out=br[:], in_=PT[1544 + 4 * d:1548 + 4 * d, nbp * GP:(nbp + 1) * GP]), reads=[bPT, bbr], writes=[bbr])
                    asrc = ar[:, ::-1] if d else ar[:, :]
                    bsrc = br[:, ::-1] if d else br[:, :]
                    c.op('dve', lambda e: e.tensor_scalar(out=w1[:], in0=asrc, scalar1=pc[:, 0:1], scalar2=None, op0=ALU.add), reads=[bar_, bpc], writes=[bw1])
                    c.op('dve', lambda e: e.tensor_scalar(out=w2[:], in0=w1[:], scalar1=-1.0, scalar2=None, op0=ALU.mult), reads=[bw1], writes=[bw2])
                    c.op('dve', lambda e: e.tensor_tensor(out=w2[:], in0=w2[:], in1=w1[:], op=ALU.min), reads=[bw1, bw2], writes=[bw2])
                    c.op('act', lambda e: e.activation(out=w2[:], in_=w2[:], func=AF.Exp), reads=[bw2], writes=[bw2])
                    c.op('act', lambda e: e.activation(out=w2[:], in_=w2[:], func=AF.Ln, bias=1.0, scale=1.0), reads=[bw2], writes=[bw2])
                    c.op('dve', lambda e: e.scalar_tensor_tensor(out=w1[:], in0=w1[:], scalar=0.0, in1=w2[:], op0=ALU.max, op1=ALU.add), reads=[bw1, bw2], writes=[bw1])
                    c.op('dve', lambda e: e.tensor_scalar(out=w1[:], in0=w1[:], scalar1=pc[:, 2:3], scalar2=None, op0=ALU.mult), reads=[bw1, bpc], writes=[bw1])
                    c.op('dve', lambda e: e.tensor_tensor_scan(out=gam[:], data0=msk[:], data1=w1[:], initial=0.0, op0=ALU.mult, op1=ALU.add), reads=[bmsk, bw1, brw], writes=[brw])
                    c.op('act', lambda e: e.activation(out=bet[:], in_=bsrc, func=AF.Sigmoid), reads=[bbr, brw], writes=[brw])
                    c.op('act', lambda e: e.activation(out=egam[:], in_=gam[:], func=AF.Exp), reads=[brw], writes=[brw])
                    c.op('dve', lambda e: e.tensor_tensor(out=q3[:], in0=bet[:], in1=egam[:], op=ALU.mult), reads=[brw, bq3], writes=[bq3])
                    g3 = gam[:].rearrange("p (c j) -> p c j", j=64)
                    c.op('dve', lambda e: e.tensor_tensor(out=q4[:].rearrange("p (c j) -> p c j", j=64), in0=g3[:, :, 63:64].broadcast_to([4, GP // 64, 64]), in1=g3, op=ALU.subtract),
                         reads=[brw, bq4], writes=[bq4])
                    c.op('act', lambda e: e.activation(out=q4[:], in_=q4[:], func=AF.Exp), reads=[bq4], writes=[bq4])
                    c.op('dve', lambda e: e.tensor_scalar(out=q5[:], in0=gam[:], scalar1=-1.0, scalar2=None, op0=ALU.mult), reads=[brw, bq5], writes=[bq5])
                    quants = [(gam, brw), (bet, brw), (q3, bq3), (q4, bq4), (q5, bq5)]
                    for bl in range(GP // 128):
                        blk = tp * (GP // 128) + bl
                        pc_, bpc_ = g.ps[blk % 2]
                        for qi, (qt_, bq_) in enumerate(quants):
                            c.op('pe', lambda e: e.transpose(out=pc_[:, qi * 4:(qi + 1) * 4], in_=qt_[0:4, bl * 128:(bl + 1) * 128], identity=g.ident32[0:4, 0:4]),
                                 reads=[bq_, g.b_ident32], writes=[bpc_])
                        c.op('act', lambda e: e.activation(out=cols[d][:, blk, :], in_=pc_[:, 0:4 * NQ], func=AF.Copy), reads=[bpc_, bcols[d]], writes=[bcols[d]])
                    for qi, rt in enumerate((gam, bet, egam)):
                        c.dma('sp', lambda e: e.dma_start(out=GR[d, qi, :, tp * GP:(tp + 1) * GP], in_=rt[:]), reads=[brw], pwrites=[bGR])
            bGR.seal()
            barrier(c)
        if stage < 3:
            return
        with ExitStack() as es:
            def T(name, shape, dt):
                return es.enter_context(nc.sbuf_tensor(uniq(name), shape, dt))
            nat = [T("gm_nat%d" % i, [128, GTB], BF16) for i in range(3)]; bnat = [Buf() for _ in range(3)]
            arr = [[T("gm_arr%d_%d" % (i, j), [128, GTB], BF16) for j in range(2)] for i in range(3)]; barr = [[Buf(), Buf()] for _ in range(3)]
            nm_le = T("gm_nmle", [128, 128], F32)
            nm_geT = T("gm_nmgeT", [128, 128], F32)
            m_stT = T("gm_mstT", [128, 128], F32)
            bmk = Buf()
            rts = [[T("gm_rt%d_%d" % (q, j), [4, GTB], F32) for j in range(2)] for q in range(3)]; brts = [[Buf(), Buf()] for _ in range(3)]
            S32 = T("gm_S32", [128, 128], F32); bS32 = Buf()
            Sb = T("gm_Sb", [128, 128], BF16); bSb = Buf()

            def TT(name, shape, dt):
                return (T("gm_" + name, shape, dt), Buf())
            tmpD, btmpD = TT("tmpD", [128, 128], F32)
            Dst, bDst = TT("Dst", [128, 128], F32)
            DTi, bDTi = TT("DTi", [128, 128], F32)
            DTs, bDTs = TT("DTs", [128, 128], F32)
            A_, bA_ = TT("A", [128, 128], BF16)
            AT_, bAT_ = TT("AT", [128, 128], BF16)
            atT, batT = TT("attnT", [128, 128], BF16)
            Pm = [TT("P%d" % i, [128, 128], BF16) for i in range(6)]
            Qm = [TT("Q%d" % i, [128, 128], BF16) for i in range(5)]
            W32, bW32 = TT("W32", [128, 256], F32)
            Wb, bWb = TT("Wb", [128, 256], BF16)
            kdec, bkdec = TT("kdec", [128, 128], BF16)
            kcT, bkcT = TT("kcT", [128, 128], BF16)
            qdec, bqdec = TT("qdec", [128, 128], BF16)
            vnew, bvnew = TT("vnew", [128, 128], BF16)
            elc, belc = TT("elc", [128, 2], F32)
            osb = [TT("osb%d" % i, [128, 128], F32) for i in range(2)]
            osf = [TT("osf%d" % i, [128, 128], F32) for i in range(2)]
            c.op('pool', lambda e: e.memset(nm_le[:], 0.0), writes=[bmk])
            c.op('pool', lambda e: e.affine_select(out=nm_le[:], in_=nm_le[:], pattern=[[-1, 128]], compare_op=ALU.is_gt, fill=-30000.0, base=0, channel_multiplier=1), reads=[bmk], writes=[bmk])
            c.op('pool', lambda e: e.memset(nm_le[64:128, 0:64], -30000.0), reads=[bmk], writes=[bmk])
            c.op('pool', lambda e: e.memset(nm_geT[:], 0.0), reads=[bmk], writes=[bmk])
            c.op('pool', lambda e: e.affine_select(out=nm_geT[:], in_=nm_geT[:], pattern=[[1, 128]], compare_op=ALU.is_ge, fill=-30000.0, base=0, channel_multiplier=-1), reads=[bmk], writes=[bmk])
            c.op('pool', lambda e: e.memset(nm_geT[0:64, 64:128], -30000.0), reads=[bmk], writes=[bmk])
            c.op('pool', lambda e: e.memset(m_stT[:], 1.0), reads=[bmk], writes=[bmk])
            c.op('pool', lambda e: e.affine_select(out=m_stT[:], in_=m_stT[:], pattern=[[1, 128]], compare_op=ALU.is_gt, fill=0.0, base=0, channel_multiplier=-1), reads=[bmk], writes=[bmk])
            nblk = 0
            for h in range(4):
                for d in range(2):
                    c.op('pool', lambda e: e.memset(S32[:], 0.0), reads=[bS32], writes=[bS32])
                    c.op('pool', lambda e: e.memset(Sb[:], 0.0), reads=[bSb], writes=[bSb])
                    for tb in range(L // GTB):
                        cur = []
                        for ai in range(3):
                            a_, ba_ = arr[ai][tb % 2], barr[ai][tb % 2]
                            if d == 0:
                                c.dma('sp' if ai % 2 else 'act', lambda e: e.dma_start(out=a_[:], in_=GQ[ai * 4 + h, :, tb * GTB:(tb + 1) * GTB]), reads=[bGQ], writes=[ba_])
                            else:
                                c.dma('sp' if ai % 2 else 'act', lambda e: e.dma_start(out=nat[ai][:], in_=GQ[ai * 4 + h, :, L - (tb + 1) * GTB:L - tb * GTB]), reads=[bGQ], writes=[bnat[ai]])
                                c.op('pool', lambda e: e.tensor_copy(out=a_[:], in_=nat[ai][:, ::-1]), reads=[bnat[ai]], writes=[ba_])
                            cur.append((a_, ba_))
                        (qA, bqA), (kA, bkA), (vA, bvA) = cur
                        rcur = []
                        for qi in range(3):
                            r_, br_ = rts[qi][tb % 2], brts[qi][tb % 2]
                            c.dma('sp', lambda e: e.dma_start(out=r_[:], in_=GR[d, qi, :, tb * GTB:(tb + 1) * GTB]), reads=[bGR], writes=[br_])
                            rcur.append((r_, br_))
                        for b in range(GTB // 128):
                            blk = tb * (GTB // 128) + b
                            bs_ = slice(b * 128, (b + 1) * 128)
                            gs_ = slice(blk * 128, (blk + 1) * 128)
                            cl = cols[d][:, blk, :]
                            gcol = cl[:, 0 + h:0 + h + 1]; bcol = cl[:, 4 + h:4 + h + 1]; begcol = cl[:, 8 + h:8 + h + 1]
                            ekdcol = cl[:, 12 + h:12 + h + 1]; ngcol = cl[:, 16 + h:16 + h + 1]
                            bc, bbc = g.ps[0]
                            for qi, (rt, brt) in enumerate(rcur):
                                c.op('pe', lambda e: e.matmul(bc[:, qi * 128:(qi + 1) * 128], lhsT=sel[:, h, :], rhs=rt[0:4, bs_], start=True, stop=True, skip_group_check=True),
                                     reads=[bsel, brt], writes=[bbc])
                            Gp, bGp = g.ps[1]
                            QKp, bQKp = g.ps[2]
                            c.op('pe', lambda e: e.matmul(Gp[:, 0:128], lhsT=kA[:, bs_], rhs=kA[:, bs_], start=True, stop=True), reads=[bkA], writes=[bGp])
                            c.op('pe', lambda e: e.matmul(QKp[:, 0:128], lhsT=kA[:, bs_], rhs=qA[:, bs_], start=True, stop=True), reads=[bkA, bqA], writes=[bQKp])
                            c.op('pe', lambda e: e.transpose(out=ptb[:, 0:128], in_=vA[:, bs_], identity=g.identb[:]), reads=[bvA, g.b_identb], writes=[bptb])
                            c.op('pe', lambda e: e.transpose(out=ptb[:, 128:256], in_=kA[:, bs_], identity=g.identb[:]), reads=[bkA, g.b_identb], writes=[bptb])
                            c.op('dve', lambda e: e.scalar_tensor_tensor(out=tmpD[:], in0=bc[:, 0:128], scalar=-1.0, in1=nm_le[:], op0=ALU.mult, op1=ALU.add), reads=[bbc, bmk], writes=[btmpD])
                            c.op('act', lambda e: e.activation(out=Dst[:], in_=tmpD[:], func=AF.Exp, bias=gcol, scale=1.0), reads=[btmpD, bcols[d]], writes=[bDst])
                            c.op('dve', lambda e: e.tensor_tensor(out=tmpD[:], in0=bc[:, 0:128], in1=nm_geT[:], op=ALU.add), reads=[bbc, bmk, btmpD], writes=[btmpD])
                            c.op('act', lambda e: e.activation(out=DTi[:], in_=tmpD[:], func=AF.Exp, bias=ngcol, scale=1.0), reads=[btmpD, bcols[d]], writes=[bDTi])
                            c.op('pool', lambda e: e.tensor_tensor(out=DTs[:], in0=DTi[:], in1=m_stT[:], op=ALU.mult), reads=[bDTi, bmk], writes=[bDTs])
                            c.op('dve', lambda e: e.tensor_tensor(out=DTs[:], in0=bc[:, 128:256], in1=DTs[:], op=ALU.mult), reads=[bbc, bDTs], writes=[bDTs])
                            c.op('dve', lambda e: e.scalar_tensor_tensor(out=A_[:], in0=Gp[:, 0:128], scalar=bcol, in1=Dst[:], op0=ALU.mult, op1=ALU.mult), reads=[bGp, bcols[d], bDst], writes=[bA_])
                            c.op('dve', lambda e: e.tensor_tensor(out=AT_[:], in0=Gp[:, 0:128], in1=DTs[:], op=ALU.mult), reads=[bGp, bDTs], writes=[bAT_])
                            c.op('dve', lambda e: e.tensor_tensor(out=atT[:], in0=QKp[:, 0:128], in1=DTi[:], op=ALU.mult), reads=[bQKp, bDTi], writes=[batT])
                            c.op('dve', lambda e: e.tensor_scalar(out=W32[:, 0:128], in0=ptb[:, 0:128], scalar1=bcol, scalar2=None, op0=ALU.mult), reads=[bptb, bcols[d], bW32], writes=[bW32])
                            c.op('dve', lambda e: e.tensor_scalar(out=W32[:, 128:256], in0=ptb[:, 128:256], scalar1=begcol, scalar2=None, op0=ALU.mult), reads=[bptb, bcols[d], bW32], writes=[bW32])
                            c.op('act', lambda e: e.activation(out=kdec[:], in_=ptb[:, 128:256], func=AF.Copy, scale=ekdcol), reads=[bptb, bcols[d]], writes=[bkdec])
                            c.op('act', lambda e: e.activation(out=Wb[:], in_=W32[:], func=AF.Copy), reads=[bW32], writes=[bWb])
                            c.op('dve', lambda e: e.tensor_tensor(out=qdec[:], in0=bc[:, 256:384], in1=qA[:, bs_], op=ALU.mult), reads=[bbc, bqA], writes=[bqdec])
                            c.op('act', lambda e: e.activation(out=elc[:], in_=bc[:, 256:384].rearrange("p (c j) -> p c j", j=64)[:, :, 63], func=AF.Copy), reads=[bbc], writes=[belc])
                            Pc, bPc = AT_, bAT_
                            Qc, bQc = A_, bA_
                            for lev in range(6):
                                pa, bpa = g.ps[4]
                                c.op('pe', lambda e: e.matmul(pa[:, 0:256], lhsT=Pc[:], rhs=Wb[:], start=True, stop=True), reads=[bPc, bWb], writes=[bpa])
                                c.op('dve', lambda e: e.tensor_tensor(out=W32[:], in0=W32[:], in1=pa[:, 0:256], op=(ALU.subtract if lev == 0 else ALU.add)), reads=[bW32, bpa], writes=[bW32])
                                c.op('act', lambda e: e.activation(out=Wb[:], in_=W32[:], func=AF.Copy), reads=[bW32], writes=[bWb])
                                if lev < 5:
                                    psq, bpsq = g.ps[3]
                                    Pn, bPn = Pm[lev + 1]
                                    c.op('pe', lambda e: e.matmul(psq[:, 0:128], lhsT=Qc[:], rhs=Pc[:], start=True, stop=True), reads=[bQc, bPc], writes=[bpsq])
                                    if lev < 4:
                                        Qn, bQn = Qm[lev + 1]
                                        c.op('pe', lambda e: e.matmul(psq[:, 128:256], lhsT=Pc[:], rhs=Qc[:], start=True, stop=True, skip_group_check=True), reads=[bQc, bPc], writes=[bpsq])
                                        c.op('dve', lambda e: e.tensor_copy(out=Qn[:], in_=psq[:, 128:256]), reads=[bpsq], writes=[bQn])
                                    c.op('act', lambda e: e.activation(out=Pn[:], in_=psq[:, 0:128], func=AF.Copy), reads=[bpsq], writes=[bPn])
                                    Pc, bPc = Pn, bPn
                                    if lev < 4:
                                        Qc, bQc = Qn, bQn
                            c.op('pe', lambda e: e.transpose(out=ptb[:, 256:384], in_=Wb[:, 128:256], identity=g.identb[:]), reads=[bWb, g.b_identb], writes=[bptb])
                            c.op('act', lambda e: e.activation(out=kcT[:], in_=ptb[:, 256:384], func=AF.Copy), reads=[bptb], writes=[bkcT])
                            po, bpo = g.ps[6]
                            for ci in range(2):
                                r0 = 64 * ci
                                p1, bp1 = g.ps[5]
                                pk, bpk = g.ps[1]
                                c.op('pe', lambda e: e.matmul(p1[r0:r0 + 64, 0:128], lhsT=kcT[:, r0:r0 + 64], rhs=Sb[:, :], start=True, stop=True), reads=[bkcT, bSb], writes=[bp1])
                                c.op('dve', lambda e: e.tensor_tensor(out=vnew[r0:r0 + 64, :], in0=W32[r0:r0 + 64, 0:128], in1=p1[r0:r0 + 64, 0:128], op=ALU.subtract), reads=[bW32, bp1, bvnew], writes=[bvnew])
                                c.op('pe', lambda e: e.matmul(po[r0:r0 + 64, 0:128], lhsT=qdec[:, r0:r0 + 64], rhs=Sb[:, :], start=True, stop=False), reads=[bqdec, bSb], writes=[bpo])
                                c.op('pe', lambda e: e.matmul(po[r0:r0 + 64, 0:128], lhsT=atT[r0:r0 + 64, r0:r0 + 64], rhs=vnew[r0:r0 + 64, :], start=False, stop=True), reads=[batT, bvnew], writes=[bpo])
                                c.op('pe', lambda e: e.matmul(pk[:, 0:128], lhsT=kdec[r0:r0 + 64, :], rhs=vnew[r0:r0 + 64, :], start=True, stop=True), reads=[bkdec, bvnew], writes=[bpk])
                                c.op('dve', lambda e: e.scalar_tensor_tensor(out=S32[:], in0=S32[:], scalar=elc[:, ci:ci + 1], in1=pk[:, 0:128], op0=ALU.mult, op1=ALU.add), reads=[bS32, belc, bpk], writes=[bS32])
                                c.op('act', lambda e: e.activation(out=Sb[:], in_=S32[:], func=AF.Copy), reads=[bS32], writes=[bSb])
                            os_, bos_ = osb[nblk % 2]
                            of_, bof_ = osf[nblk % 2]
                            nblk += 1
                            c.op('act', lambda e: e.activation(out=os_[:], in_=po[:, 0:128], func=AF.Copy), reads=[bpo], writes=[bos_])
                            if d == 0:
                                c.dma('sp', lambda e: e.dma_start(out=OD[blk * 128:(blk + 1) * 128, h * 128:(h + 1) * 128], in_=os_[:]), reads=[bos_], pwrites=[bODs[h]])
                            else:
                                pf, bpf = g.ps[5]
                                c.op('pe', lambda e: e.matmul(pf[:, 128:256], lhsT=g.J32[:], rhs=os_[:], start=True, stop=True, skip_group_check=True), reads=[g.b_J32, bos_], writes=[bpf])
                                c.op('act', lambda e: e.activation(out=of_[:], in_=pf[:, 128:256], func=AF.Copy), reads=[bpf], writes=[bof_])
                                c.dma('pool', lambda e: e.dma_start(out=OD[L - (blk + 1) * 128:L - blk * 128, h * 128:(h + 1) * 128], in_=of_[:], accum_op=ALU.add),
                                      reads=[bof_], pwrites=[bODs[h]])
                    bODs[h].seal()
            barrier(c)
    if stage < 4:
        return
    gated_norm_finalize(c, g, OD, bODs, PV, bPV, 1536, out_gain, MT, bMT, 512, "gf_")


N_ACTIVE = 4
DEPTH = 4

PARAM_NAMES = ['mix_norm', 'ffn_norm', 'ev_w_in', 'ev_w_out', 'a_lb_logits', 'a_out_norm', 's5_lambda_re', 's5_lambda_im',
               's5_log_step', 's5_b_re', 's5_b_im', 's5_c_re', 's5_c_im', 's5_d', 's5_glu_w', 's5_glu_b', 'od_w_in', 'od_w_out',
               'c_q_norm', 'c_k_norm', 'c_lambda', 'c_out_norm', 'rel_bias', 'd_conv_w', 'd_a_log', 'd_dt_bias', 'd_out_norm',
               'moe_router', 'moe_w_gate', 'moe_w_up', 'moe_w_down']

PARAM_SHAPES = {
    'mix_norm': (4, 1024), 'ffn_norm': (4, 1024), 'ev_w_in': (2, 1024, 3072), 'ev_w_out': (2, 1024, 1024), 'a_lb_logits': (2, 2, 512),
    'a_out_norm': (2, 128), 's5_lambda_re': (2, 2, 32, 64), 's5_lambda_im': (2, 2, 32, 64), 's5_log_step': (2, 2, 32),
    's5_b_re': (2, 2, 32, 64, 16), 's5_b_im': (2, 2, 32, 64, 16), 's5_c_re': (2, 2, 32, 16, 64), 's5_c_im': (2, 2, 32, 16, 64),
    's5_d': (2, 512), 's5_glu_w': (2, 512, 512), 's5_glu_b': (2, 512), 'od_w_in': (2, 1024, 3600), 'od_w_out': (2, 1024, 1024),
    'c_q_norm': (2, 64), 'c_k_norm': (2, 64), 'c_lambda': (2, 4, 64), 'c_out_norm': (2, 128), 'rel_bias': (32, 4),
    'd_conv_w': (2, 5, 1536), 'd_a_log': (2, 2, 4), 'd_dt_bias': (2, 2, 4), 'd_out_norm': (2, 128), 'moe_router': (4, 1024, 16),
    'moe_w_gate': (4, 16, 1024, 2048), 'moe_w_up': (4, 16, 1024, 2048), 'moe_w_down': (4, 16, 2048, 1024)}


def build_program(layers=range(DEPTH), do_mixer=True, do_moe=True):
    nc = bass.Bass('TRN2', target_bir_lowering=False)
    xin = nc.dram_tensor("x", [L, D], F32, kind="ExternalInput").ap()
    P = {n: nc.dram_tensor(n, list(PARAM_SHAPES[n]), F32, kind="ExternalInput").ap() for n in PARAM_NAMES}
    onehot = nc.dram_tensor("t5_onehot", [32, 512], F32, kind="ExternalInput").ap()
    X = nc.dram_tensor("y", [L, D], F32, kind="ExternalOutput").ap()
    PT = nc.dram_tensor("PT", [2048, L], F32).ap()
    PV = nc.dram_tensor("PV", [L, 2048], BF16).ap()
    QK = nc.dram_tensor("QK", [16, 128, L], BF16).ap()
    QK5 = QK.rearrange("(h r w) p t -> h r w p t", h=4, r=2)
    OA = nc.dram_tensor("OA", [L, 512], F32).ap()
    YT = nc.dram_tensor("YT", [512, L], F32).ap()
    MT = nc.dram_tensor("MT", [1024, L], BF16).ap()
    HB = nc.dram_tensor("HB", [L, D], BF16).ap()
    GQ = nc.dram_tensor("GQ", [12, 128, L], BF16).ap()
    GR = nc.dram_tensor("GR", [2, 3, 4, L], F32).ap()
    FV = nc.dram_tensor("FV", [4, 512], F32).ap()
    c = Ctx(nc); g = G()
    setup_consts(c, g)
    bX = Buf('X'); bPT = Buf(); bPV = Buf(); bQK = Buf(); bOAs = [Buf() for _ in range(4)]; bYT = Buf(); bMT = Buf()
    bHB = Buf(); bGQ = Buf(); bGR = Buf(); bFV = Buf()
    for r in range(0, L, 512):
        c.dma('sp', lambda e: e.dma_start(out=X[r:r + 512, :], in_=xin[r:r + 512, :]), pwrites=[bX])
    bX.seal()
    for layer in layers:
        j = layer // 2
        if do_mixer:
            if layer % 2 == 0:
                spec = [(0, 512, 'F', 0), (512, 512, 'F', 512), (1024, 512, 'F', 1024), (2560, 512, 'F', 1536), (1536, 512, 'T', 0), (2048, 512, 'T', 512)]
                proj_phase(c, g, X, bX, P['mix_norm'][layer], P['ev_w_in'][j], 3072, spec, PT, bPT, PV, bPV)
                hgrn2_phase(c, g, PT, bPT, PV, bPV, P['a_lb_logits'], j, P['a_out_norm'][j], QK5, bQK, OA, bOAs, MT, bMT)
                s5_phase(c, g, PT, bPT, P['s5_lambda_re'][j], P['s5_lambda_im'][j], P['s5_log_step'][j], P['s5_b_re'][j], P['s5_b_im'][j],
                         P['s5_c_re'][j], P['s5_c_im'][j], P['s5_d'][j], P['s5_glu_w'][j], P['s5_glu_b'][j], YT, bYT, MT, bMT)
                bMT.seal()
                bX = outproj_phase(c, g, MT, bMT, P['ev_w_out'][j], X, bX)
            else:
                spec = [(0, 512, 'T', 0), (512, 512, 'T', 512), (1024, 512, 'T', 1024), (1536, 1536, 'F', 0), (3072, 16, 'F', 1536), (3088, 512, 'T', 1536)]
                proj_phase(c, g, X, bX, P['mix_norm'][layer], P['od_w_in'][j], 3600, spec, PT, bPT, PV, bPV)
                attn_phase(c, g, PV, bPV, P['c_q_norm'][j], P['c_k_norm'][j], P['c_lambda'][j], P['c_out_norm'][j], P['rel_bias'], onehot, layer,
                           QK, bQK, FV, bFV, MT, bMT)
                gdn_phase(c, g, PT, bPT, PV, bPV, P['d_conv_w'][j], P['d_a_log'][j], P['d_dt_bias'][j], P['d_out_norm'][j], GQ, bGQ, GR, bGR, OA, bOAs, MT, bMT)
                bMT.seal()
                bX = outproj_phase(c, g, MT, bMT, P['od_w_out'][j], X, bX)
        if do_moe:
            moe_layer(c, g, X, bX, HB, bHB, P['ffn_norm'][layer], P['moe_router'][layer], P['moe_w_gate'][layer], P['moe_w_up'][layer], P['moe_w_down'][layer])
    barrier(c)
    c.finish([bX])
    return nc, c


def kernel(**inputs):
    x = np.ascontiguousarray(np.asarray(inputs['x'], dtype=np.float32))
    B = x.shape[0]
    assert B == N_ACTIVE and x.shape[1] == L and x.shape[2] == D
    nc, c = build_program()
    params = {n: np.ascontiguousarray(np.asarray(inputs[n], dtype=np.float32)) for n in PARAM_NAMES}
    oh = t5_onehot()
    in_maps = []
    for b in range(N_ACTIVE):
        m = {"x": x[b], "t5_onehot": oh}
        m.update(params)
        in_maps.append(m)
    res = run_bass_kernel_spmd(nc, in_maps, core_ids=list(range(N_ACTIVE)))
    out = np.stack([np.asarray(res.results[b]["y"], dtype=np.float32) for b in range(N_ACTIVE)], axis=0)
    return out
```

```python
import math
from contextlib import ExitStack

import numpy as np
import concourse.bass as bass
import concourse.mybir as mybir
from concourse.bass_utils import run_bass_kernel_spmd

F32 = mybir.dt.float32
BF16 = mybir.dt.bfloat16
U32 = mybir.dt.uint32
I32 = mybir.dt.int32
AF = mybir.ActivationFunctionType
ALU = mybir.AluOpType
AX = mybir.AxisListType


class Buf:
    __slots__ = ("name", "w", "r", "pw")

    def __init__(self, name=""):
        self.name = name
        self.w = {}
        self.r = {}
        self.pw = {}

    def seal(self):
        for k, v in self.pw.items():
            if self.w.get(k, 0) < v:
                self.w[k] = v
        self.pw = {}


class Ctx:
    NDMA = 8

    def __init__(self, nc, same_engine_sync=True):
        self.nc = nc
        self.E = dict(pe=nc.tensor, dve=nc.vector, act=nc.scalar, pool=nc.gpsimd, sp=nc.sync)
        self.sem = {}
        self.cnt = {}
        for k in self.E:
            self.sem[k] = nc.alloc_semaphore("c_" + k)
            self.cnt[k] = 0
        self.dslot = {}
        for q in ("sp", "act", "pool"):
            for i in range(self.NDMA):
                key = "d_%s%d" % (q, i)
                self.sem[key] = nc.alloc_semaphore(key)
                self.cnt[key] = 0
            self.dslot[q] = 0
        self.seen = {k: {} for k in self.E}
        self.same = same_engine_sync
        self.ninst = 0

    def _wait(self, eng, tok):
        if tok is None:
            return
        key, val = tok
        if key == eng and (eng == "pe" or not self.same):
            return
        if self.seen[eng].get(key, 0) >= val:
            return
        self.E[eng].wait_ge(self.sem[key], val)
        self.seen[eng][key] = val

    def _deps(self, eng, reads, writes, pwrites=()):
        for b in reads:
            for k, v in b.w.items():
                self._wait(eng, (k, v))
            for k, v in b.pw.items():
                self._wait(eng, (k, v))
        for b in writes:
            for d in (b.w, b.pw, b.r):
                for k, v in d.items():
                    self._wait(eng, (k, v))
        for b in pwrites:
            for d in (b.w, b.r):
                for k, v in d.items():
                    self._wait(eng, (k, v))

    def _commit(self, tok, reads, writes, pwrites=()):
        k, v = tok
        for b in writes:
            b.w = {k: v}
            b.pw = {}
            b.r = {}
        for b in pwrites:
            if b.pw.get(k, 0) < v:
                b.pw[k] = v
        for b in reads:
            if b.r.get(k, 0) < v:
                b.r[k] = v

    def op(self, eng, fn, reads=(), writes=(), pwrites=()):
        self._deps(eng, reads, writes, pwrites)
        inst = fn(self.E[eng])
        self.cnt[eng] += 1
        tok = (eng, self.cnt[eng])
        inst.then_inc(self.sem[eng], 1)
        self._commit(tok, reads, writes, pwrites)
        self.ninst += 1
        return tok

    def dma(self, q, fn, reads=(), writes=(), pwrites=()):
        self._deps(q, reads, writes, pwrites)
        i = self.dslot[q]
        self.dslot[q] = (i + 1) % self.NDMA
        key = "d_%s%d" % (q, i)
        if self.cnt[key] > 0:
            self._wait(q, (key, self.cnt[key]))
        inst = fn(self.E[q])
        self.cnt[key] += 16
        tok = (key, self.cnt[key])
        inst.then_inc(self.sem[key], 16)
        self._commit(tok, reads, writes, pwrites)
        self.ninst += 1
        return tok

    def finish(self, bufs):
        for b in bufs:
            for d in (b.w, b.pw):
                for k, v in d.items():
                    self._wait("sp", (k, v))


def barrier(c):
    toks = [(k, v) for k, v in c.cnt.items() if v > 0]
    for eng in c.E:
        for tok in toks:
            c._wait(eng, tok)


_UNIQ = [0]


def uniq(name):
    _UNIQ[0] += 1
    return "%s_u%d" % (name, _UNIQ[0])


L = 8192
D = 1024
NE = 16
FF = 2048
CAP = 1024
NT = L // 128


class G:
    pass


def alloc_T(nc, name, shape, dtype, n=1, es=None):
    if es is None:
        return [(nc.alloc_sbuf_tensor("%s_%d" % (name, i), shape, dtype), Buf(name)) for i in range(n)]
    return [(es.enter_context(nc.sbuf_tensor(uniq("%s_%d" % (name, i)), shape, dtype)), Buf(name)) for i in range(n)]


def setup_consts(c, g):
    nc = c.nc
    g.ident32 = nc.alloc_sbuf_tensor("ident32", [128, 128], F32); g.b_ident32 = Buf()
    g.identb = nc.alloc_sbuf_tensor("identb", [128, 128], BF16); g.b_identb = Buf()
    g.ones32 = nc.alloc_sbuf_tensor("ones32", [1, 128], F32); g.b_ones32 = Buf()
    g.neghalf = nc.alloc_sbuf_tensor("neghalf", [128, 1], F32); g.b_neghalf = Buf()
    for t, b in ((g.ident32, g.b_ident32), (g.identb, g.b_identb)):
        c.op('pool', lambda e: e.memset(t[:], 0.0), writes=[b])
        c.op('pool', lambda e: e.affine_select(out=t[:], in_=t[:], pattern=[[-1, 128]], compare_op=ALU.not_equal,
                                               fill=1.0, base=0, channel_multiplier=1), reads=[b], writes=[b])
    g.J32 = nc.alloc_sbuf_tensor("J32", [128, 128], F32); g.b_J32 = Buf()
    g.Jb = nc.alloc_sbuf_tensor("Jb", [128, 128], BF16); g.b_Jb = Buf()
    for t, b in ((g.J32, g.b_J32), (g.Jb, g.b_Jb)):
        c.op('pool', lambda e: e.memset(t[:], 0.0), writes=[b])
        c.op('pool', lambda e: e.affine_select(out=t[:], in_=t[:], pattern=[[1, 128]], compare_op=ALU.not_equal,
                                               fill=1.0, base=-127, channel_multiplier=1), reads=[b], writes=[b])
    c.op('pool', lambda e: e.memset(g.ones32[:], 1.0), writes=[g.b_ones32])
    c.op('pool', lambda e: e.memset(g.neghalf[:], -0.5), writes=[g.b_neghalf])
    g.ps = []
    for i in range(7):
        g.ps.append((nc.alloc_psum_tensor("ps%d" % i, [128, 512], F32), Buf("ps%d" % i)))
    g.psb = (nc.alloc_psum_tensor("psb", [128, 1024], BF16), Buf("psb"))


def bcast_row(c, g, dst, bdst, src_ap, n, tmp, btmp, psi=6):
    c.dma('sp', lambda e: e.dma_start(out=tmp[0:1, 0:n], in_=src_ap.rearrange("(o n) -> o n", o=1)), writes=[btmp])
    ps, bps = g.ps[psi]
    for h in range(0, n, 512):
        w = min(512, n - h)
        c.op('pe', lambda e: e.matmul(ps[:, 0:w], lhsT=g.ones32[0:1, :], rhs=tmp[0:1, h:h + w], start=True, stop=True),
             reads=[g.b_ones32, btmp], writes=[bps])
        c.op('dve', lambda e: e.tensor_copy(out=dst[:, h:h + w], in_=ps[:, 0:w]), reads=[bps], writes=[bdst])


def rmsnorm_tile(c, g, xt, bxt, gB, bgB, h32, bh32, junk, bjunk, ss, bss):
    c.op('dve', lambda e: e.scalar_tensor_tensor(out=junk[:], in0=xt[:], scalar=1.0, in1=xt[:], op0=ALU.mult, op1=ALU.mult,
                                                 accum_out=ss[:, 0:1]), reads=[bxt], writes=[bjunk, bss])
    c.op('dve', lambda e: e.tensor_scalar(out=ss[:, 0:1], in0=ss[:, 0:1], scalar1=1.0 / D, scalar2=1e-6, op0=ALU.mult, op1=ALU.add),
         reads=[bss], writes=[bss])
    c.op('pool', lambda e: e.tensor_tensor(out=ss[:, 0:1], in0=ss[:, 0:1], in1=g.neghalf[:, 0:1], op=ALU.pow),
         reads=[bss, g.b_neghalf], writes=[bss])
    c.op('dve', lambda e: e.scalar_tensor_tensor(out=h32[:], in0=xt[:], scalar=ss[:, 0:1], in1=gB[:], op0=ALU.mult, op1=ALU.mult),
         reads=[bxt, bss, bgB], writes=[bh32])


def moe_phase(c, g, X, bX, HB, bHB, ffn_g, w_router, w_gate, w_up, w_down, sb, stage=3):
    nc = c.nc
    gB, bgB = sb['gB']
    tmpr, btmpr = sb['tmprow']
    bcast_row(c, g, gB, bgB, ffn_g, D, tmpr, btmpr)
    wr, bwr = sb['wr']
    c.dma('sp', lambda e: e.dma_start(out=wr[:], in_=w_router.rearrange("(k p) e -> p k e", p=128)), writes=[bwr])
    affT, baffT = sb['affT']
    for i in range(NT):
        xt, bxt = sb['xt'][i % 2]
        h32, bh32 = sb['h32'][i % 2]
        hb, bhb = sb['hb'][i % 2]
        junk, bjunk = sb['junk']
        ss, bss = sb['ss'][i % 2]
        c.dma('sp', lambda e: e.dma_start(out=xt[:], in_=X[i * 128:(i + 1) * 128, :]), reads=[bX], writes=[bxt])
        rmsnorm_tile(c, g, xt, bxt, gB, bgB, h32, bh32, junk, bjunk, ss, bss)
        c.op('act', lambda e: e.activation(out=hb[:], in_=h32[:], func=AF.Copy), reads=[bh32], writes=[bhb])
        c.dma('act', lambda e: e.dma_start(out=HB[i * 128:(i + 1) * 128, :], in_=hb[:]), reads=[bhb], pwrites=[bHB])
        hT, bhT = sb['hT32'][i % 2]
        for hh in range(2):
            ps, bps = g.ps[hh]
            for k in range(4):
                kk = hh * 4 + k
                c.op('pe', lambda e: e.transpose(out=ps[:, k * 128:(k + 1) * 128], in_=h32[:, kk * 128:(kk + 1) * 128],
                                                 identity=g.ident32[:]), reads=[bh32, g.b_ident32], writes=[bps])
            c.op('act', lambda e: e.activation(out=hT[:, hh * 512:(hh + 1) * 512], in_=ps[:, :], func=AF.Copy),
                 reads=[bps], writes=[bhT])
        pl, bpl = g.ps[2 + (i % 2)]
        for k in range(8):
            c.op('pe', lambda e: e.matmul(pl[:, 0:NE], lhsT=hT[:, k * 128:(k + 1) * 128], rhs=wr[:, k, :], start=(k == 0), stop=(k == 7)),
                 reads=[bhT, bwr], writes=[bpl])
        sm, bsm = sb['sm'][i % 2]
        ex, bex = sb['ex'][i % 2]
        c.op('dve', lambda e: e.tensor_reduce(out=sm[:, 0:1], in_=pl[:, 0:NE], axis=AX.X, op=ALU.max), reads=[bpl], writes=[bsm])
        c.op('dve', lambda e: e.tensor_scalar(out=sm[:, 0:1], in0=sm[:, 0:1], scalar1=-1.0, scalar2=None, op0=ALU.mult), reads=[bsm], writes=[bsm])
        c.op('act', lambda e: e.activation(out=ex[:], in_=pl[:, 0:NE], func=AF.Exp, bias=sm[:, 0:1], scale=1.0, accum_out=sm[:, 1:2]),
             reads=[bpl, bsm], writes=[bex, bsm])
        c.op('dve', lambda e: e.reciprocal(out=sm[:, 2:3], in_=sm[:, 1:2]), reads=[bsm], writes=[bsm])
        c.op('dve', lambda e: e.tensor_scalar(out=ex[:], in0=ex[:], scalar1=sm[:, 2:3], scalar2=None, op0=ALU.mult), reads=[bex, bsm], writes=[bex])
        pt, bpt = g.ps[4 + (i % 2)]
        c.op('pe', lambda e: e.transpose(out=pt[0:NE, 0:128], in_=ex[:, 0:NE], identity=g.ident32[:]), reads=[bex, g.b_ident32], writes=[bpt])
        c.op('act', lambda e: e.activation(out=affT[0:NE, i * 128:(i + 1) * 128], in_=pt[0:NE, 0:128], func=AF.Copy), reads=[bpt], writes=[baffT])
    bHB.seal()
    if stage < 2:
        return
    vals, bvals = sb['vals']
    idxu, bidxu = sb['idxu']
    for it in range(CAP // 8):
        sl = slice(it * 8, it * 8 + 8)
        c.op('dve', lambda e: e.max(out=vals[:, sl], in_=affT[:, :]), reads=[baffT], writes=[bvals])
        c.op('dve', lambda e: e.max_index(out=idxu[:, sl], in_max=vals[:, sl], in_values=affT[:, :]), reads=[baffT, bvals], writes=[bidxu])
        c.op('dve', lambda e: e.match_replace(out=affT[:, :], in_to_replace=vals[:, sl], in_values=affT[:, :], imm_value=-1.0),
             reads=[bvals, baffT], writes=[baffT])
    idxf, bidxf = sb['idxf']
    c.op('dve', lambda e: e.tensor_copy(out=idxf[:], in_=idxu[:]), reads=[bidxu], writes=[bidxf])
    idxT, bidxT = sb['idxT']
    gateT, bgateT = sb['gateT']
    for j in range(8):
        pt, bpt = g.ps[4 + (j % 2)]
        c.op('pe', lambda e: e.transpose(out=pt[:, 0:NE], in_=idxf[0:NE, j * 128:(j + 1) * 128], identity=g.ident32[0:NE, 0:NE]),
             reads=[bidxf, g.b_ident32], writes=[bpt])
        c.op('dve', lambda e: e.tensor_copy(out=idxT[:, j, :], in_=pt[:, 0:NE]), reads=[bpt], writes=[bidxT])
        pt2, bpt2 = g.ps[2 + (j % 2)]
        c.op('pe', lambda e: e.transpose(out=pt2[:, 0:NE], in_=vals[0:NE, j * 128:(j + 1) * 128], identity=g.ident32[0:NE, 0:NE]),
             reads=[bvals, g.b_ident32], writes=[bpt2])
        c.op('act', lambda e: e.activation(out=gateT[:, j, :], in_=pt2[:, 0:NE], func=AF.Copy), reads=[bpt2], writes=[bgateT])
    if stage < 3:
        return
    xsT, bxsT = sb['xsT']
    hidT, bhidT = sb['hidT']
    yacc, byacc = sb['yacc']
    ptb, bptb = g.psb
    qi = 0
    for ex_i in range(NE):
        for j in range(8):
            xs, bxs = sb['xs'][j % 2]
            c.dma('pool', lambda e: e.indirect_dma_start(out=xs[:, :], out_offset=None, in_=HB[:, :],
                                                        in_offset=bass.IndirectOffsetOnAxis(ap=idxT[:, j, ex_i:ex_i + 1], axis=0)),
                  reads=[bHB, bidxT], writes=[bxs])
            for k in range(8):
                c.op('pe', lambda e: e.transpose(out=ptb[:, k * 128:(k + 1) * 128], in_=xs[:, k * 128:(k + 1) * 128], identity=g.identb[:]),
                     reads=[bxs, g.b_identb], writes=[bptb])
            c.op('dve', lambda e: e.tensor_copy(out=xsT[:, :, j * 128:(j + 1) * 128], in_=ptb[:, :].rearrange("p (k s) -> p k s", k=8)),
                 reads=[bptb], writes=[bxsT])
        for q in range(4):
            wg, bwg = sb['wg'][qi % 2]
            wu, bwu = sb['wu'][qi % 2]
            wd, bwd = sb['wd'][qi % 2]
            qi += 1
            f0 = q * 512
            c.dma('pool', lambda e: e.dma_start(out=wg[:], in_=w_gate[ex_i, :, f0:f0 + 512].rearrange("(k p) f -> p k f", p=128)), writes=[bwg])
            c.dma('pool', lambda e: e.dma_start(out=wu[:], in_=w_up[ex_i, :, f0:f0 + 512].rearrange("(k p) f -> p k f", p=128)), writes=[bwu])
            c.dma('pool', lambda e: e.dma_start(out=wd[:], in_=w_down[ex_i, f0:f0 + 512, :].rearrange("(k p) d -> p k d", p=128)), writes=[bwd])
            n = 0
            for fc in range(4):
                for sh in range(2):
                    pg, bpg = g.ps[0 + (n % 2)]
                    pu, bpu = g.ps[2 + (n % 2)]
                    sg, bsg = sb['sg'][n % 2]
                    n += 1
                    for k in range(8):
                        c.op('pe', lambda e: e.matmul(pg[:, :], lhsT=wg[:, k, fc * 128:(fc + 1) * 128], rhs=xsT[:, k, sh * 512:(sh + 1) * 512],
                                                      start=(k == 0), stop=(k == 7)), reads=[bwg, bxsT], writes=[bpg])
                    for k in range(8):
                        c.op('pe', lambda e: e.matmul(pu[:, :], lhsT=wu[:, k, fc * 128:(fc + 1) * 128], rhs=xsT[:, k, sh * 512:(sh + 1) * 512],
                                                      start=(k == 0), stop=(k == 7)), reads=[bwu, bxsT], writes=[bpu])
                    c.op('act', lambda e: e.activation(out=sg[:], in_=pg[:, :], func=AF.Silu), reads=[bpg], writes=[bsg])
                    c.op('dve', lambda e: e.tensor_tensor(out=hidT[:, fc, sh * 512:(sh + 1) * 512], in0=sg[:], in1=pu[:, :], op=ALU.mult),
                         reads=[bsg, bpu], writes=[bhidT])
            m = 0
            for j in range(8):
                for dh in range(2):
                    py, bpy = g.ps[4 + (m % 2)]
                    m += 1
                    for fc in range(4):
                        c.op('pe', lambda e: e.matmul(py[:, :], lhsT=hidT[:, fc, j * 128:(j + 1) * 128], rhs=wd[:, fc, dh * 512:(dh + 1) * 512],
                                                      start=(fc == 0), stop=(fc == 3)), reads=[bhidT, bwd], writes=[bpy])
                    ysl = yacc[:, j, dh * 512:(dh + 1) * 512]
                    gsc = gateT[:, j, ex_i:ex_i + 1]
                    if q == 0:
                        c.op('dve', lambda e: e.tensor_scalar(out=ysl, in0=py[:, :], scalar1=gsc, scalar2=None, op0=ALU.mult),
                             reads=[bpy, bgateT], writes=[byacc])
                    else:
                        c.op('dve', lambda e: e.scalar_tensor_tensor(out=ysl, in0=py[:, :], scalar=gsc, in1=ysl, op0=ALU.mult, op1=ALU.add),
                             reads=[bpy, bgateT, byacc], writes=[byacc])
        for j in range(8):
            c.dma('pool', lambda e: e.indirect_dma_start(out=X[:, :], out_offset=bass.IndirectOffsetOnAxis(ap=idxT[:, j, ex_i:ex_i + 1], axis=0),
                                                        in_=yacc[:, j, :], in_offset=None, compute_op=ALU.add),
                  reads=[byacc, bidxT], pwrites=[bX])
        bX.seal()


def moe_alloc(nc, es=None):
    sb = {}
    sb['gB'] = alloc_T(nc, 'gB', [128, D], F32, 1, es=es)[0]
    sb['tmprow'] = alloc_T(nc, 'tmprow', [1, 1024], F32, 1, es=es)[0]
    sb['wr'] = alloc_T(nc, 'wr', [128, 8, NE], F32, 1, es=es)[0]
    big = es.enter_context(nc.sbuf_tensor(uniq('big'), [128, L], F32)) if es is not None else nc.alloc_sbuf_tensor('big', [128, L], F32); bbig = Buf('big')
    sb['affT'] = (big[0:NE, :], bbig)
    sb['xt'] = alloc_T(nc, 'xt', [128, D], F32, 2, es=es)
    sb['h32'] = alloc_T(nc, 'h32', [128, D], F32, 2, es=es)
    sb['hb'] = alloc_T(nc, 'hb', [128, D], BF16, 2, es=es)
    sb['junk'] = alloc_T(nc, 'junk', [128, D], F32, 1, es=es)[0]
    sb['ss'] = alloc_T(nc, 'ss', [128, 4], F32, 2, es=es)
    sb['hT32'] = alloc_T(nc, 'hT32', [128, D], F32, 2, es=es)
    sb['sm'] = alloc_T(nc, 'sm', [128, 4], F32, 2, es=es)
    sb['ex'] = alloc_T(nc, 'ex', [128, NE], F32, 2, es=es)
    sb['vals'] = alloc_T(nc, 'vals', [NE, CAP], F32, 1, es=es)[0]
    sb['idxu'] = alloc_T(nc, 'idxu', [NE, CAP], U32, 1, es=es)[0]
    sb['idxf'] = alloc_T(nc, 'idxf', [NE, CAP], F32, 1, es=es)[0]
    sb['idxT'] = alloc_T(nc, 'idxT', [128, 8, NE], U32, 1, es=es)[0]
    sb['gateT'] = alloc_T(nc, 'gateT', [128, 8, NE], F32, 1, es=es)[0]
    sb['xsT'] = alloc_T(nc, 'xsT', [128, 8, CAP], BF16, 1, es=es)[0]
    sb['hidT'] = alloc_T(nc, 'hidT', [128, 4, CAP], BF16, 1, es=es)[0]
    sb['yacc'] = (big[:, :].rearrange('p (j d) -> p j d', j=8), bbig)
    sb['xs'] = alloc_T(nc, 'xs', [128, D], BF16, 2, es=es)
    sb['wg'] = alloc_T(nc, 'wg', [128, 8, 512], BF16, 2, es=es)
    sb['wu'] = alloc_T(nc, 'wu', [128, 8, 512], BF16, 2, es=es)
    sb['wd'] = alloc_T(nc, 'wd', [128, 4, D], BF16, 2, es=es)
    sb['sg'] = alloc_T(nc, 'sg', [128, 512], BF16, 2, es=es)
    return sb


def moe_layer(c, g, X, bX, HB, bHB, ffn_g, w_router, w_gate, w_up, w_down):
    with ExitStack() as es:
        sb = moe_alloc(c.nc, es)
        moe_phase(c, g, X, bX, HB, bHB, ffn_g, w_router, w_gate, w_up, w_down, sb)
        barrier(c)


def proj_phase(c, g, X, bX, gain_ap, w_ap, nout, spec, PT, bPT, PV, bPV):
    nc = c.nc
    with ExitStack() as es:
        def T(name, shape, dt):
            return es.enter_context(nc.sbuf_tensor(uniq(name), shape, dt))
        wsb = T("pj_w", [128, 8, nout], BF16); bw = Buf()
        gB = T("pj_gB", [128, D], F32); bgB = Buf()
        tmpr = T("pj_tmpr", [1, D], F32); btmpr = Buf()
        xt = [T("pj_xt%d" % i, [128, 4, D], F32) for i in range(2)]; bxt = [Buf(), Buf()]
        junk = T("pj_junk", [128, D], F32); bjunk = Buf()
        ss = [T("pj_ss%d" % i, [128, 4], F32) for i in range(2)]; bss = [Buf(), Buf()]
        hb = [T("pj_hb%d" % i, [128, D], BF16) for i in range(2)]; bhb = [Buf(), Buf()]
        hT = [T("pj_hT%d" % i, [128, 8, 512], BF16) for i in range(2)]; bhT = [Buf(), Buf()]
        stF = [T("pj_stF%d" % i, [128, 512], F32) for i in range(3)]; bstF = [Buf() for _ in range(3)]
        stT = [T("pj_stT%d" % i, [128, 512], BF16) for i in range(3)]; bstT = [Buf() for _ in range(3)]
        bcast_row(c, g, gB, bgB, gain_ap, D, tmpr, btmpr)
        for c0 in range(0, nout, 512):
            wd = min(512, nout - c0)
            c.dma('pool', lambda e: e.dma_start(out=wsb[:, :, c0:c0 + wd], in_=w_ap[:, c0:c0 + wd].rearrange("(k p) f -> p k f", p=128)), writes=[bw])
        ptb, bptb = g.psb
        nF = 0; nT = 0; npz = 0
        for it in range(L // 512):
            t0 = it * 512
            x_, bx_ = xt[it % 2], bxt[it % 2]
            c.dma('sp', lambda e: e.dma_start(out=x_[:], in_=X[t0:t0 + 512, :].rearrange("(j p) d -> p j d", p=128)), reads=[bX], writes=[bx_])
            hT_, bhT_ = hT[it % 2], bhT[it % 2]
            for j in range(4):
                s_, bs_ = ss[j % 2], bss[j % 2]
                h_, bh_ = hb[j % 2], bhb[j % 2]
                c.op('dve', lambda e: e.scalar_tensor_tensor(out=junk[:], in0=x_[:, j, :], scalar=1.0, in1=x_[:, j, :], op0=ALU.mult, op1=ALU.mult,
                                                             accum_out=s_[:, 0:1]), reads=[bx_], writes=[bjunk, bs_])
                c.op('dve', lambda e: e.tensor_scalar(out=s_[:, 0:1], in0=s_[:, 0:1], scalar1=1.0 / D, scalar2=1e-6, op0=ALU.mult, op1=ALU.add),
                     reads=[bs_], writes=[bs_])
                c.op('pool', lambda e: e.tensor_tensor(out=s_[:, 0:1], in0=s_[:, 0:1], in1=g.neghalf[:, 0:1], op=ALU.pow),
                     reads=[bs_, g.b_neghalf], writes=[bs_])
                c.op('dve', lambda e: e.scalar_tensor_tensor(out=h_[:], in0=x_[:, j, :], scalar=s_[:, 0:1], in1=gB[:], op0=ALU.mult, op1=ALU.mult),
                     reads=[bx_, bs_, bgB], writes=[bh_])
                for k in range(8):
                    c.op('pe', lambda e: e.transpose(out=ptb[:, k * 128:(k + 1) * 128], in_=h_[:, k * 128:(k + 1) * 128], identity=g.identb[:]),
                         reads=[bh_, g.b_identb], writes=[bptb])
                c.op('act', lambda e: e.activation(out=hT_[:, :, j * 128:(j + 1) * 128], in_=ptb[:, :].rearrange("p (k s) -> p k s", k=8), func=AF.Copy),
                     reads=[bptb], writes=[bhT_])
            for (col0, ncols, mode, dst0) in spec:
                if mode == 'F':
                    for f0 in range(0, ncols, 128):
                        fw = min(128, ncols - f0)
                        ps, bps = g.ps[npz % 4]; npz += 1
                        for k in range(8):
                            c.op('pe', lambda e: e.matmul(ps[0:fw, :], lhsT=wsb[:, k, col0 + f0:col0 + f0 + fw], rhs=hT_[:, k, :], start=(k == 0), stop=(k == 7)),
                                 reads=[bw, bhT_], writes=[bps])
                        st, bst = stF[nF % 3], bstF[nF % 3]; nF += 1
                        eng = 'act' if nF % 2 else 'dve'
                        if eng == 'act':
                            c.op('act', lambda e: e.activation(out=st[0:fw, :], in_=ps[0:fw, :], func=AF.Copy), reads=[bps], writes=[bst])
                        else:
                            c.op('dve', lambda e: e.tensor_copy(out=st[0:fw, :], in_=ps[0:fw, :]), reads=[bps], writes=[bst])
                        c.dma('sp' if nF % 2 else 'act', lambda e: e.dma_start(out=PT[dst0 + f0:dst0 + f0 + fw, t0:t0 + 512], in_=st[0:fw, :]), reads=[bst], pwrites=[bPT])
                else:
                    for j in range(4):
                        for c0 in range(0, ncols, 512):
                            cw = min(512, ncols - c0)
                            ps, bps = g.ps[npz % 4]; npz += 1
                            for k in range(8):
                                c.op('pe', lambda e: e.matmul(ps[:, 0:cw], lhsT=hT_[:, k, j * 128:(j + 1) * 128], rhs=wsb[:, k, col0 + c0:col0 + c0 + cw], start=(k == 0), stop=(k == 7)),
                                     reads=[bw, bhT_], writes=[bps])
                            st, bst = stT[nT % 3], bstT[nT % 3]; nT += 1
                            eng = 'act' if nT % 2 else 'dve'
                            if eng == 'act':
                                c.op('act', lambda e: e.activation(out=st[:, 0:cw], in_=ps[:, 0:cw], func=AF.Copy), reads=[bps], writes=[bst])
                            else:
                                c.op('dve', lambda e: e.tensor_copy(out=st[:, 0:cw], in_=ps[:, 0:cw]), reads=[bps], writes=[bst])
                            c.dma('sp' if nT % 2 else 'act', lambda e: e.dma_start(out=PV[t0 + j * 128:t0 + (j + 1) * 128, dst0 + c0:dst0 + c0 + cw], in_=st[:, 0:cw]),
                                  reads=[bst], pwrites=[bPV])
        bPT.seal(); bPV.seal()
        barrier(c)


TBK = 2048
NTB = L // TBK


def hgrn2_phase(c, g, PT, bPT, PV, bPV, lb_logits, jl, a_out_norm, QK, bQK, OA, bOAs, MT, bMT, stage=3):
    nc = c.nc
    with ExitStack() as es0:
        def T0(name, shape, dt):
            return es0.enter_context(nc.sbuf_tensor(uniq(name), shape, dt))
        mcols = T0("hg_mcols", [128, 8, 128], F32); bmcols = Buf()
        with ExitStack() as es:
            def T(name, shape, dt):
                return es.enter_context(nc.sbuf_tensor(uniq(name), shape, dt))
            lbt = T("hg_lbt", [128, 2, 2, 4], F32); blbt = Buf()
            lbc = T("hg_lbc", [128, 8], F32); blbc = Buf()
            oml = T("hg_oml", [128, 8], F32); boml = Buf()
            noml = T("hg_noml", [128, 8], F32); bnoml = Buf()
            msk = T("hg_msk", [128, TBK], F32); bmsk = Buf()
            bmid = T("hg_bmid", [128, 128], F32); bbmid = Buf()
            blast = T("hg_blast", [128, 128], F32); bblast = Buf()
            zq = [T("hg_zq%d" % i, [128, TBK], F32) for i in range(2)]; bzq = [Buf(), Buf()]
            zf = [T("hg_zf%d" % i, [128, TBK], F32) for i in range(2)]; bzf = [Buf(), Buf()]
            q_ = T("hg_q", [128, TBK], F32); bq_ = Buf()
            sig = T("hg_sig", [128, TBK], F32); bsig = Buf()
            f_ = T("hg_f", [128, TBK], F32); bf_ = Buf()
            kk = T("hg_kk", [128, TBK], F32); bkk = Buf()
            b_ = T("hg_b", [128, TBK], F32); bb_ = Buf()
            e1 = T("hg_e1", [128, TBK], F32); be1 = Buf()
            eq = T("hg_eq", [128, TBK], F32); beq = Buf()
            ek = T("hg_ek", [128, TBK], F32); bek = Buf()
            qt = [T("hg_qt%d" % i, [128, TBK], BF16) for i in range(2)]; bqt = [Buf(), Buf()]
            kt = [T("hg_kt%d" % i, [128, TBK], BF16) for i in range(2)]; bkt = [Buf(), Buf()]
            if jl == 0:
                c.op('pool', lambda e: e.memset(lbc[:], 0.0), writes=[blbc])
            else:
                with nc.allow_non_contiguous_dma(reason="tiny"):
                    for j_ in range(2):
                        for r_ in range(2):
                            c.dma('sp', lambda e: e.dma_start(out=lbt[:, j_, r_, :], in_=lb_logits[j_, r_, :].rearrange("(h p) -> p h", p=128)), pwrites=[blbt])
                blbt.seal()
                c.op('dve', lambda e: e.tensor_tensor(out=lbc[:].rearrange("p (r h) -> p r h", r=2), in0=lbt[:, 1, :, :], in1=lbt[:, 0, :, :], op=ALU.subtract),
                     reads=[blbt], writes=[blbc])
                c.op('act', lambda e: e.activation(out=lbc[:], in_=lbc[:], func=AF.Sigmoid), reads=[blbc], writes=[blbc])
            c.op('dve', lambda e: e.tensor_scalar(out=oml[:], in0=lbc[:], scalar1=-1.0, scalar2=1.0, op0=ALU.mult, op1=ALU.add), reads=[blbc], writes=[boml])
            c.op('dve', lambda e: e.tensor_scalar(out=noml[:], in0=oml[:], scalar1=-1.0, scalar2=None, op0=ALU.mult), reads=[boml], writes=[bnoml])
            c.op('pool', lambda e: e.memset(msk[:], 1.0), writes=[bmsk])
            c.op('pool', lambda e: e.memset(msk[:].rearrange("p (c j) -> p c j", j=64)[:, :, 0:1], 0.0), writes=[bmsk])
            n = 0
            for h in range(4):
                for r in range(2):
                    hr = r * 4 + h
                    for tb in range(NTB):
                        nb = tb if r == 0 else NTB - 1 - tb
                        zq_, bzq_ = zq[n % 2], bzq[n % 2]
                        zf_, bzf_ = zf[n % 2], bzf[n % 2]
                        qt_, bqt_ = qt[n % 2], bqt[n % 2]
                        kt_, bkt_ = kt[n % 2], bkt[n % 2]
                        n += 1
                        c.dma('sp', lambda e: e.dma_start(out=zq_[:], in_=PT[h * 128:(h + 1) * 128, nb * TBK:(nb + 1) * TBK]), reads=[bPT], writes=[bzq_])
                        fr = 512 + r * 512 + h * 128
                        c.dma('act', lambda e: e.dma_start(out=zf_[:], in_=PT[fr:fr + 128, nb * TBK:(nb + 1) * TBK]), reads=[bPT], writes=[bzf_])
                        zqs = zq_[:, ::-1] if r else zq_[:, :]
                        zfs = zf_[:, ::-1] if r else zf_[:, :]
                        c.op('act', lambda e: e.activation(out=q_[:], in_=zqs, func=AF.Silu), reads=[bzq_], writes=[bq_])
                        c.op('act', lambda e: e.activation(out=sig[:], in_=zfs, func=AF.Sigmoid), reads=[bzf_], writes=[bsig])
                        c.op('dve', lambda e: e.tensor_scalar(out=f_[:], in0=sig[:], scalar1=oml[:, hr:hr + 1], scalar2=lbc[:, hr:hr + 1], op0=ALU.mult, op1=ALU.add),
                             reads=[bsig, boml, blbc], writes=[bf_])
                        c.op('act', lambda e: e.activation(out=f_[:], in_=f_[:], func=AF.Ln), reads=[bf_], writes=[bf_])
                        c.op('pool', lambda e: e.tensor_scalar(out=kk[:], in0=sig[:], scalar1=noml[:, hr:hr + 1], scalar2=oml[:, hr:hr + 1], op0=ALU.mult, op1=ALU.add),
                             reads=[bsig, bnoml, boml], writes=[bkk])
                        c.op('dve', lambda e: e.tensor_tensor_scan(out=b_[:], data0=msk[:], data1=f_[:], initial=0.0, op0=ALU.mult, op1=ALU.add),
                             reads=[bmsk, bf_], writes=[bb_])
                        b3 = b_[:].rearrange("p (c j) -> p c j", j=64)
                        c.op('dve', lambda e: e.tensor_tensor(out=e1[:].rearrange("p (c j) -> p c j", j=64), in0=b3, in1=b3[:, :, 31:32].broadcast_to([128, TBK // 64, 64]), op=ALU.subtract),
                             reads=[bb_], writes=[be1])
                        c.op('act', lambda e: e.activation(out=eq[:], in_=e1[:], func=AF.Exp), reads=[be1], writes=[beq])
                        c.op('act', lambda e: e.activation(out=ek[:], in_=e1[:], func=AF.Exp, scale=-1.0), reads=[be1], writes=[bek])
                        c.op('dve', lambda e: e.tensor_tensor(out=qt_[:], in0=q_[:], in1=eq[:], op=ALU.mult), reads=[bq_, beq], writes=[bqt_])
                        c.op('pool', lambda e: e.tensor_tensor(out=kt_[:], in0=kk[:], in1=ek[:], op=ALU.mult), reads=[bkk, bek], writes=[bkt_])
                        nch = TBK // 64
                        c.op('act', lambda e: e.activation(out=bmid[:, tb * nch:(tb + 1) * nch], in_=b3[:, :, 31], func=AF.Copy), reads=[bb_], writes=[bbmid])
                        c.op('act', lambda e: e.activation(out=blast[:, tb * nch:(tb + 1) * nch], in_=b3[:, :, 63], func=AF.Copy), reads=[bb_], writes=[bblast])
                        c.dma('sp', lambda e: e.dma_start(out=QK[h, r, 0, :, tb * TBK:(tb + 1) * TBK], in_=qt_[:]), reads=[bqt_], pwrites=[bQK])
                        c.dma('act', lambda e: e.dma_start(out=QK[h, r, 1, :, tb * TBK:(tb + 1) * TBK], in_=kt_[:]), reads=[bkt_], pwrites=[bQK])
                    c.op('dve', lambda e: e.tensor_tensor(out=blast[:], in0=blast[:], in1=bmid[:], op=ALU.subtract), reads=[bblast, bbmid], writes=[bblast])
                    c.op('dve', lambda e: e.tensor_tensor(out=blast[:, 0:127], in0=blast[:, 0:127], in1=bmid[:, 1:128], op=ALU.add), reads=[bblast, bbmid], writes=[bblast])
                    c.op('act', lambda e: e.activation(out=mcols[:, hr, :], in_=blast[:], func=AF.Exp), reads=[bblast], writes=[bmcols])
            bQK.seal()
            barrier(c)
        if stage < 2:
            return
        with ExitStack() as es:
            def T(name, shape, dt):
                return es.enter_context(nc.sbuf_tensor(uniq(name), shape, dt))
            qb = [T("hr_qb%d" % i, [128, TBK], BF16) for i in range(2)]; bqb = [Buf(), Buf()]
            kb = [T("hr_kb%d" % i, [128, TBK], BF16) for i in range(2)]; bkb = [Buf(), Buf()]
            vb = [T("hr_vb%d" % i, [128, TBK // 128, 128], BF16) for i in range(2)]; bvb = [Buf(), Buf()]
            mask = T("hr_mask", [128, 128], F32); bmask = Buf()
            attnT = [T("hr_attn%d" % i, [128, 128], BF16) for i in range(2)]; battn = [Buf(), Buf()]
            ktok = [T("hr_ktok%d" % i, [128, 128], BF16) for i in range(2)]; bktok = [Buf(), Buf()]
            M32 = T("hr_M32", [128, 128], F32); bM32 = Buf()
            Mb = T("hr_Mb", [128, 128], BF16); bMb = Buf()
            tmp32 = T("hr_tmp32", [128, 128], F32); btmp32 = Buf()
            osb = [T("hr_osb%d" % i, [128, 128], F32) for i in range(3)]; bosb = [Buf() for _ in range(3)]
            osf = [T("hr_osf%d" % i, [128, 128], F32) for i in range(3)]; bosf = [Buf() for _ in range(3)]
            vnat = T("hr_vnat", [128, TBK // 128, 128], BF16); bvnat = Buf()
            c.op('pool', lambda e: e.memset(mask[:], 1.0), writes=[bmask])
            c.op('pool', lambda e: e.affine_select(out=mask[:], in_=mask[:], pattern=[[1, 128]], compare_op=ALU.is_ge, fill=0.0, base=0, channel_multiplier=-1),
                 reads=[bmask], writes=[bmask])
            c.op('pool', lambda e: e.memset(mask[0:64, 64:128], 0.0), reads=[bmask], writes=[bmask])
            ptb, bptb = g.psb
            n = 0; nblk = 0; nch = 0
            for h in range(4):
                for r in range(2):
                    hr = r * 4 + h
                    c.op('pool', lambda e: e.memset(M32[:], 0.0), writes=[bM32])
                    c.op('pool', lambda e: e.memset(Mb[:], 0.0), writes=[bMb])
                    for tb in range(NTB):
                        qb_, bqb_ = qb[n % 2], bqb[n % 2]
                        kb_, bkb_ = kb[n % 2], bkb[n % 2]
                        vb_, bvb_ = vb[n % 2], bvb[n % 2]
                        n += 1
                        c.dma('sp', lambda e: e.dma_start(out=qb_[:], in_=QK[h, r, 0, :, tb * TBK:(tb + 1) * TBK]), reads=[bQK], writes=[bqb_])
                        c.dma('act', lambda e: e.dma_start(out=kb_[:], in_=QK[h, r, 1, :, tb * TBK:(tb + 1) * TBK]), reads=[bQK], writes=[bkb_])
                        if r == 0:
                            vsrc = PV[tb * TBK:(tb + 1) * TBK, h * 128:(h + 1) * 128].rearrange("(b p) d -> p b d", p=128)
                            c.dma('sp', lambda e: e.dma_start(out=vb_[:], in_=vsrc), reads=[bPV], writes=[bvb_])
                        else:
                            vsrc = PV[L - (tb + 1) * TBK:L - tb * TBK, h * 128:(h + 1) * 128].rearrange("(b p) d -> p b d", p=128)
                            c.dma('sp', lambda e: e.dma_start(out=vnat[:], in_=vsrc), reads=[bPV], writes=[bvnat])
                            nbk = TBK // 128
                            for b4 in range(0, nbk, 4):
                                pf, bpf = g.ps[4 + (b4 // 4) % 2]
                                c.op('pe', lambda e: e.matmul(pf[:, :], lhsT=g.Jb[:], rhs=vnat[:, b4:b4 + 4, :], start=True, stop=True), reads=[g.b_Jb, bvnat], writes=[bpf])
                                for bb in range(4):
                                    c.op('act', lambda e: e.activation(out=vb_[:, nbk - 1 - (b4 + bb), :], in_=pf[:, bb * 128:(bb + 1) * 128], func=AF.Copy), reads=[bpf], writes=[bvb_])
                        for b in range(TBK // 128):
                            blk = tb * (TBK // 128) + b
                            at_, bat_ = attnT[nblk % 2], battn[nblk % 2]
                            kt_, bkt_ = ktok[nblk % 2], bktok[nblk % 2]
                            pa, bpa = g.ps[nblk % 2]
                            po, bpo = g.ps[2 + nblk % 2]
                            os_, bos_ = osb[nblk % 3], bosb[nblk % 3]
                            nblk += 1
                            bs = slice(b * 128, (b + 1) * 128)
                            c.op('pe', lambda e: e.matmul(pa[:, 0:128], lhsT=kb_[:, bs], rhs=qb_[:, bs], start=True, stop=True), reads=[bkb_, bqb_], writes=[bpa])
                            c.op('dve', lambda e: e.tensor_tensor(out=at_[:], in0=pa[:, 0:128], in1=mask[:], op=ALU.mult), reads=[bpa, bmask], writes=[bat_])
                            c.op('pe', lambda e: e.transpose(out=ptb[:, 0:128], in_=kb_[:, bs], identity=g.identb[:]), reads=[bkb_, g.b_identb], writes=[bptb])
                            c.op('act', lambda e: e.activation(out=kt_[:], in_=ptb[:, 0:128], func=AF.Copy), reads=[bptb], writes=[bkt_])
                            for ci in range(2):
                                r0 = 64 * ci
                                cidx = 2 * blk + ci
                                pk, bpk = g.ps[4 + nch % 2]; nch += 1
                                c.op('pe', lambda e: e.matmul(po[r0:r0 + 64, 0:128], lhsT=at_[r0:r0 + 64, r0:r0 + 64], rhs=vb_[r0:r0 + 64, b, :], start=True, stop=False),
                                     reads=[bat_, bvb_], writes=[bpo])
                                c.op('pe', lambda e: e.matmul(po[r0:r0 + 64, 0:128], lhsT=qb_[:, b * 128 + r0:b * 128 + r0 + 64], rhs=Mb[:, :], start=False, stop=True),
                                     reads=[bqb_, bMb], writes=[bpo])
                                if cidx < 127:
                                    c.op('pe', lambda e: e.matmul(pk[:, 0:128], lhsT=kt_[r0:r0 + 64, :], rhs=vb_[r0:r0 + 64, b, :], start=True, stop=True),
                                         reads=[bkt_, bvb_], writes=[bpk])
                                    c.op('dve', lambda e: e.tensor_tensor(out=tmp32[:], in0=pk[:, 0:128], in1=M32[:], op=ALU.add), reads=[bpk, bM32], writes=[btmp32])
                                    c.op('dve', lambda e: e.tensor_scalar(out=M32[:], in0=tmp32[:], scalar1=mcols[:, hr, cidx:cidx + 1], scalar2=None, op0=ALU.mult),
                                         reads=[btmp32, bmcols], writes=[bM32])
                                    c.op('act', lambda e: e.activation(out=Mb[:], in_=M32[:], func=AF.Copy), reads=[bM32], writes=[bMb])
                            c.op('act', lambda e: e.activation(out=os_[:], in_=po[:, 0:128], func=AF.Copy), reads=[bpo], writes=[bos_])
                            if r == 0:
                                c.dma('sp', lambda e: e.dma_start(out=OA[blk * 128:(blk + 1) * 128, h * 128:(h + 1) * 128], in_=os_[:]), reads=[bos_], pwrites=[bOAs[h]])
                            else:
                                of_, bof_ = osf[nblk % 3], bosf[nblk % 3]
                                pf, bpf = g.ps[6]
                                c.op('pe', lambda e: e.matmul(pf[:, 0:128], lhsT=g.J32[:], rhs=os_[:], start=True, stop=True), reads=[g.b_J32, bos_], writes=[bpf])
                                c.op('act', lambda e: e.activation(out=of_[:], in_=pf[:, 0:128], func=AF.Copy), reads=[bpf], writes=[bof_])
                                c.dma('pool', lambda e: e.dma_start(out=OA[L - (blk + 1) * 128:L - blk * 128, h * 128:(h + 1) * 128], in_=of_[:], accum_op=ALU.add),
                                      reads=[bof_], pwrites=[bOAs[h]])
                    bOAs[h].seal()
            barrier(c)
        if stage < 3:
            return
        with ExitStack() as es:
            def T(name, shape, dt):
                return es.enter_context(nc.sbuf_tensor(uniq(name), shape, dt))
            gA = T("hf_gA", [128, 128], F32); bgA = Buf()
            tmpr = T("hf_tmpr", [1, 128], F32); btmpr = Buf()
            oa = [T("hf_oa%d" % i, [128, 512], F32) for i in range(2)]; boa = [Buf(), Buf()]
            ga = [T("hf_ga%d" % i, [128, 512], BF16) for i in range(2)]; bga = [Buf(), Buf()]
            sq = T("hf_sq", [128, 512], F32); bsq = Buf()
            ssq = [T("hf_ssq%d" % i, [128, 4], F32) for i in range(2)]; bssq = [Buf(), Buf()]
            sg = T("hf_sg", [128, 512], F32); bsg = Buf()
            t1 = T("hf_t1", [128, 512], F32); bt1 = Buf()
            ob = [T("hf_ob%d" % i, [128, 512], BF16) for i in range(2)]; bob = [Buf(), Buf()]
            mt = [T("hf_mt%d" % i, [128, 4, 512], BF16) for i in range(2)]; bmt = [Buf(), Buf()]
            bcast_row(c, g, gA, bgA, a_out_norm, 128, tmpr, btmpr)
            ptb, bptb = g.psb
            for i in range(NT):
                oa_, boa_ = oa[i % 2], boa[i % 2]
                ga_, bga_ = ga[i % 2], bga[i % 2]
                ss_, bss_ = ssq[i % 2], bssq[i % 2]
                ob_, bob_ = ob[i % 2], bob[i % 2]
                mt_, bmt_ = mt[(i // 4) % 2], bmt[(i // 4) % 2]
                c.dma('sp', lambda e: e.dma_start(out=oa_[:], in_=OA[i * 128:(i + 1) * 128, :]), reads=bOAs, writes=[boa_])
                c.dma('act', lambda e: e.dma_start(out=ga_[:], in_=PV[i * 128:(i + 1) * 128, 512:1024]), reads=[bPV], writes=[bga_])
                c.op('pool', lambda e: e.tensor_tensor(out=sq[:], in0=oa_[:], in1=oa_[:], op=ALU.mult), reads=[boa_], writes=[bsq])
                c.op('dve', lambda e: e.tensor_reduce(out=ss_[:], in_=sq[:].rearrange("p (h d) -> p h d", h=4), axis=AX.X, op=ALU.add), reads=[bsq], writes=[bss_])
                c.op('dve', lambda e: e.tensor_scalar(out=ss_[:], in0=ss_[:], scalar1=1.0 / 128, scalar2=1e-6, op0=ALU.mult, op1=ALU.add), reads=[bss_], writes=[bss_])
                c.op('pool', lambda e: e.tensor_tensor(out=ss_[:], in0=ss_[:], in1=g.neghalf[:, 0:1].broadcast_to([128, 4]), op=ALU.pow), reads=[bss_, g.b_neghalf], writes=[bss_])
                c.op('act', lambda e: e.activation(out=sg[:], in_=ga_[:], func=AF.Silu), reads=[bga_], writes=[bsg])
                c.op('dve', lambda e: e.tensor_tensor(out=t1[:].rearrange("p (h d) -> p h d", h=4), in0=oa_[:].rearrange("p (h d) -> p h d", h=4),
                                                      in1=ss_[:].unsqueeze(2).broadcast_to([128, 4, 128]), op=ALU.mult), reads=[boa_, bss_], writes=[bt1])
                c.op('pool', lambda e: e.tensor_tensor(out=t1[:].rearrange("p (h d) -> p h d", h=4), in0=t1[:].rearrange("p (h d) -> p h d", h=4),
                                                       in1=gA[:].unsqueeze(1).broadcast_to([128, 4, 128]), op=ALU.mult), reads=[bt1, bgA], writes=[bt1])
                c.op('dve', lambda e: e.tensor_tensor(out=ob_[:], in0=t1[:], in1=sg[:], op=ALU.mult), reads=[bt1, bsg], writes=[bob_])
                for k in range(4):
                    c.op('pe', lambda e: e.transpose(out=ptb[:, k * 128:(k + 1) * 128], in_=ob_[:, k * 128:(k + 1) * 128], identity=g.identb[:]),
                         reads=[bob_, g.b_identb], writes=[bptb])
                c.op('act', lambda e: e.activation(out=mt_[:, :, (i % 4) * 128:(i % 4 + 1) * 128], in_=ptb[:, 0:512].rearrange("p (k s) -> p k s", k=4), func=AF.Copy),
                     reads=[bptb], writes=[bmt_])
                if i % 4 == 3:
                    t0 = (i // 4) * 512
                    c.dma('sp', lambda e: e.dma_start(out=MT[0:512, t0:t0 + 512].rearrange("(k p) t -> p k t", p=128), in_=mt_[:]), reads=[bmt_], pwrites=[bMT])
            barrier(c)


TB5 = 1024
NTB5 = L // TB5
TWO_PI = 2.0 * math.pi


def s5_phase(c, g, PT, bPT, lam_re, lam_im, log_step, b_re, b_im, c_re, c_im, d_skip, glu_w, glu_b, YT, bYT, MT, bMT, stage=3):
    nc = c.nc
    U0 = 1536
    with ExitStack() as es0:
        def T0(name, shape, dt):
            return es0.enter_context(nc.sbuf_tensor(uniq(name), shape, dt))
        WB = [T0("s5_WB%d" % p, [128, 2, 4, 128], BF16) for p in range(2)]; bWB = Buf()
        WC = [T0("s5_WC%d" % p, [128, 2, 4, 128], BF16) for p in range(2)]; bWC = Buf()
        WBx = [T0("s5_WBx%d" % p, [128, 2, 4, 128], BF16) for p in range(2)]
        WCx = [T0("s5_WCx%d" % p, [128, 2, 4, 64], BF16) for p in range(2)]
        mag = T0("s5_mag", [128, 32], F32); bmag = Buf()
        pwc = T0("s5_pwc", [128, 11, 32], F32); bpw = Buf()
        pws = T0("s5_pws", [128, 11, 32], F32)
        with ExitStack() as es:
            def T(name, shape, dt):
                return es.enter_context(nc.sbuf_tensor(uniq(name), shape, dt))
            n_ = [0]

            def S(shape=[128, 32], dt=F32):
                n_[0] += 1
                return T("s5_t%d" % n_[0], shape, dt), Buf()
            lamre, blamre = S(); lamim, blamim = S()
            lsB, blsB = S([128, 64]); ls, bls = S()
            with nc.allow_non_contiguous_dma(reason="small params"):
                for r_ in range(2):
                    for g4 in range(0, 16, 4):
                        c.dma('sp', lambda e: e.dma_start(out=lamre[:, r_ * 16 + g4:r_ * 16 + g4 + 4], in_=lam_re[r_, 2 * g4:2 * g4 + 8, :].rearrange("(gp gl) n -> (gl n) gp", gl=2)), pwrites=[blamre])
                        c.dma('act', lambda e: e.dma_start(out=lamim[:, r_ * 16 + g4:r_ * 16 + g4 + 4], in_=lam_im[r_, 2 * g4:2 * g4 + 8, :].rearrange("(gp gl) n -> (gl n) gp", gl=2)), pwrites=[blamim])
                blamre.seal(); blamim.seal()
                c.dma('sp', lambda e: e.dma_start(out=lsB[:], in_=log_step.rearrange("r g -> (r g)").partition_broadcast(128)), writes=[blsB])
            lsv = lsB[:].rearrange("p (r gp gl) -> p r gp gl", r=2, gl=2)
            c.op('dve', lambda e: e.tensor_copy(out=ls[0:64, :].rearrange("p (r gp) -> p r gp", r=2), in_=lsv[0:64, :, :, 0]), reads=[blsB], writes=[bls])
            c.op('dve', lambda e: e.tensor_copy(out=ls[64:128, :].rearrange("p (r gp) -> p r gp", r=2), in_=lsv[64:128, :, :, 1]), reads=[blsB], writes=[bls])
            step, bstep = S()
            c.op('act', lambda e: e.activation(out=step[:], in_=ls[:], func=AF.Exp), reads=[bls], writes=[bstep])
            lrs, blrs = S(); ang, bang = S()
            c.op('dve', lambda e: e.tensor_tensor(out=lrs[:], in0=lamre[:], in1=step[:], op=ALU.mult), reads=[blamre, bstep], writes=[blrs])
            c.op('act', lambda e: e.activation(out=mag[:], in_=lrs[:], func=AF.Exp), reads=[blrs], writes=[bmag])
            c.op('dve', lambda e: e.tensor_tensor(out=ang[:], in0=lamim[:], in1=step[:], op=ALU.mult), reads=[blamim, bstep], writes=[bang])

            def sin_of(src, bsrc, offset, dst, bdst):
                q, bq = S(); qi, bqi = S(dt=I32); r, br = S(); m, bm = S()
                c.op('dve', lambda e: e.tensor_scalar(out=q[:], in0=src[:], scalar1=offset, scalar2=1.0 / TWO_PI, op0=ALU.add, op1=ALU.mult), reads=[bsrc], writes=[bq])
                c.op('dve', lambda e: e.tensor_copy(out=qi[:], in_=q[:]), reads=[bq], writes=[bqi])
                c.op('dve', lambda e: e.tensor_copy(out=q[:], in_=qi[:]), reads=[bqi], writes=[bq])
                c.op('dve', lambda e: e.scalar_tensor_tensor(out=r[:], in0=q[:], scalar=-TWO_PI, in1=src[:], op0=ALU.mult, op1=ALU.add), reads=[bq, bsrc], writes=[br])
                if offset != 0.0:
                    c.op('dve', lambda e: e.tensor_scalar(out=r[:], in0=r[:], scalar1=offset, scalar2=None, op0=ALU.add), reads=[br], writes=[br])
                c.op('dve', lambda e: e.tensor_scalar(out=m[:], in0=r[:], scalar1=math.pi, scalar2=-TWO_PI, op0=ALU.is_gt, op1=ALU.mult), reads=[br], writes=[bm])
                c.op('dve', lambda e: e.tensor_tensor(out=r[:], in0=r[:], in1=m[:], op=ALU.add), reads=[br, bm], writes=[br])
                c.op('dve', lambda e: e.tensor_scalar(out=m[:], in0=r[:], scalar1=-math.pi, scalar2=TWO_PI, op0=ALU.is_lt, op1=ALU.mult), reads=[br], writes=[bm])
                c.op('dve', lambda e: e.tensor_tensor(out=r[:], in0=r[:], in1=m[:], op=ALU.add), reads=[br, bm], writes=[br])
                c.op('dve', lambda e: e.tensor_scalar(out=r[:], in0=r[:], scalar1=math.pi, scalar2=-math.pi, op0=ALU.min, op1=ALU.max), reads=[br], writes=[br])
                c.op('act', lambda e: e.activation(out=dst, in_=r[:], func=AF.Sin), reads=[br], writes=[bdst])
            sin_of(ang, bang, 0.0, pws[:, 0, :], bpw)
            sin_of(ang, bang, math.pi / 2, pwc[:, 0, :], bpw)
            tq, btq = S(); tq2, btq2 = S()
            for k in range(10):
                c.op('dve', lambda e: e.tensor_tensor(out=tq[:], in0=pwc[:, k, :], in1=pwc[:, k, :], op=ALU.mult), reads=[bpw], writes=[btq])
                c.op('dve', lambda e: e.tensor_tensor(out=tq2[:], in0=pws[:, k, :], in1=pws[:, k, :], op=ALU.mult), reads=[bpw], writes=[btq2])
                c.op('dve', lambda e: e.tensor_tensor(out=pwc[:, k + 1, :], in0=tq[:], in1=tq2[:], op=ALU.subtract), reads=[btq, btq2, bpw], writes=[bpw])
                c.op('dve', lambda e: e.tensor_tensor(out=tq[:], in0=pws[:, k, :], in1=pwc[:, k, :], op=ALU.mult), reads=[bpw], writes=[btq])
                c.op('dve', lambda e: e.tensor_scalar(out=pws[:, k + 1, :], in0=tq[:], scalar1=2.0, scalar2=None, op0=ALU.mult), reads=[btq, bpw], writes=[bpw])
            are, bare = S(); aim, baim = S(); den, bden = S(); am1, bam1 = S(); fr, bfr = S(); fi, bfi = S(); tt, btt = S()
            c.op('dve', lambda e: e.tensor_tensor(out=are[:], in0=mag[:], in1=pwc[:, 0, :], op=ALU.mult), reads=[bmag, bpw], writes=[bare])
            c.op('dve', lambda e: e.tensor_tensor(out=aim[:], in0=mag[:], in1=pws[:, 0, :], op=ALU.mult), reads=[bmag, bpw], writes=[baim])
            c.op('dve', lambda e: e.tensor_tensor(out=den[:], in0=lamre[:], in1=lamre[:], op=ALU.mult), reads=[blamre], writes=[bden])
            c.op('dve', lambda e: e.tensor_tensor(out=tt[:], in0=lamim[:], in1=lamim[:], op=ALU.mult), reads=[blamim], writes=[btt])
            c.op('dve', lambda e: e.tensor_tensor(out=den[:], in0=den[:], in1=tt[:], op=ALU.add), reads=[bden, btt], writes=[bden])
            c.op('dve', lambda e: e.reciprocal(out=den[:], in_=den[:]), reads=[bden], writes=[bden])
            c.op('dve', lambda e: e.tensor_scalar(out=am1[:], in0=are[:], scalar1=-1.0, scalar2=None, op0=ALU.add), reads=[bare], writes=[bam1])
            c.op('dve', lambda e: e.tensor_tensor(out=fr[:], in0=am1[:], in1=lamre[:], op=ALU.mult), reads=[bam1, blamre], writes=[bfr])
            c.op('dve', lambda e: e.tensor_tensor(out=tt[:], in0=aim[:], in1=lamim[:], op=ALU.mult), reads=[baim, blamim], writes=[btt])
            c.op('dve', lambda e: e.tensor_tensor(out=fr[:], in0=fr[:], in1=tt[:], op=ALU.add), reads=[bfr, btt], writes=[bfr])
            c.op('dve', lambda e: e.tensor_tensor(out=fr[:], in0=fr[:], in1=den[:], op=ALU.mult), reads=[bfr, bden], writes=[bfr])
            c.op('dve', lambda e: e.tensor_tensor(out=fi[:], in0=aim[:], in1=lamre[:], op=ALU.mult), reads=[baim, blamre], writes=[bfi])
            c.op('dve', lambda e: e.tensor_tensor(out=tt[:], in0=am1[:], in1=lamim[:], op=ALU.mult), reads=[bam1, blamim], writes=[btt])
            c.op('dve', lambda e: e.tensor_tensor(out=fi[:], in0=fi[:], in1=tt[:], op=ALU.subtract), reads=[bfi, btt], writes=[bfi])
            c.op('dve', lambda e: e.tensor_tensor(out=fi[:], in0=fi[:], in1=den[:], op=ALU.mult), reads=[bfi, bden], writes=[bfi])
            mk, bmk = S([128, 2])
            c.op('pool', lambda e: e.memset(mk[:], 0.0), writes=[bmk])
            c.op('pool', lambda e: e.memset(mk[0:64, 0:1], 1.0), reads=[bmk], writes=[bmk])
            c.op('pool', lambda e: e.memset(mk[64:128, 1:2], 1.0), reads=[bmk], writes=[bmk])
            Bn = [S([128, 2, 16, 16]) for _ in range(2)]
            with nc.allow_non_contiguous_dma(reason="small params"):
                for r_ in range(2):
                    for g4 in range(0, 16, 4):
                        c.dma('sp', lambda e: e.dma_start(out=Bn[0][0][:, r_, g4:g4 + 4, :], in_=b_re[r_, 2 * g4:2 * g4 + 8].rearrange("(gp gl) n p -> (gl n) gp p", gl=2)), pwrites=[Bn[0][1]])
                        c.dma('act', lambda e: e.dma_start(out=Bn[1][0][:, r_, g4:g4 + 4, :], in_=b_im[r_, 2 * g4:2 * g4 + 8].rearrange("(gp gl) n p -> (gl n) gp p", gl=2)), pwrites=[Bn[1][1]])
                Bn[0][1].seal(); Bn[1][1].seal()
            frb = fr[:].rearrange("p (r gp) -> p r gp", r=2).unsqueeze(3).broadcast_to([128, 2, 16, 16])
            fib = fi[:].rearrange("p (r gp) -> p r gp", r=2).unsqueeze(3).broadcast_to([128, 2, 16, 16])
            bbr, bbbr = S([128, 2, 16, 16]); bbi, bbbi = S([128, 2, 16, 16]); t5, bt5 = S([128, 2, 16, 16])
            c.op('dve', lambda e: e.tensor_tensor(out=bbr[:], in0=Bn[0][0][:], in1=frb, op=ALU.mult), reads=[Bn[0][1], bfr], writes=[bbbr])
            c.op('dve', lambda e: e.tensor_tensor(out=t5[:], in0=Bn[1][0][:], in1=fib, op=ALU.mult), reads=[Bn[1][1], bfi], writes=[bt5])
            c.op('dve', lambda e: e.tensor_tensor(out=bbr[:], in0=bbr[:], in1=t5[:], op=ALU.subtract), reads=[bbbr, bt5], writes=[bbbr])
            c.op('dve', lambda e: e.tensor_tensor(out=bbi[:], in0=Bn[1][0][:], in1=frb, op=ALU.mult), reads=[Bn[1][1], bfr], writes=[bbbi])
            c.op('dve', lambda e: e.tensor_tensor(out=t5[:], in0=Bn[0][0][:], in1=fib, op=ALU.mult), reads=[Bn[0][1], bfi], writes=[bt5])
            c.op('dve', lambda e: e.tensor_tensor(out=bbi[:], in0=bbi[:], in1=t5[:], op=ALU.add), reads=[bbbi, bt5], writes=[bbbi])
            BBm, bBBm = S([128, 2, 16, 2, 16], BF16)
            ptb, bptb = g.psb
            for part, (src, bsrc) in enumerate(((bbr, bbbr), (bbi, bbbi))):
                for gl in range(2):
                    c.op('dve', lambda e: e.tensor_scalar(out=BBm[:, :, :, gl, :], in0=src[:], scalar1=mk[:, gl:gl + 1], scalar2=None, op0=ALU.mult), reads=[bsrc, bmk, bBBm], writes=[bBBm])
                for r in range(2):
                    for cb in range(4):
                        c.op('pe', lambda e: e.transpose(out=ptb[:, 0:128], in_=BBm[:, r, 4 * cb:4 * cb + 4, :, :].rearrange("p a b c -> p (a b c)"), identity=g.identb[:]),
                             reads=[bBBm, g.b_identb], writes=[bptb])
                        c.op('act', lambda e: e.activation(out=WB[part][:, r, cb, :], in_=ptb[:, 0:128], func=AF.Copy), reads=[bptb], writes=[bWB])
            Cn = [S([128, 2, 4, 64]) for _ in range(2)]
            c.dma('sp', lambda e: e.dma_start(out=Cn[0][0][:], in_=c_re.rearrange("r (cb g8) p n -> (g8 p) r cb n", g8=8)), writes=[Cn[0][1]])
            c.dma('act', lambda e: e.dma_start(out=Cn[1][0][:], in_=c_im.rearrange("r (cb g8) p n -> (g8 p) r cb n", g8=8)), writes=[Cn[1][1]])
            Cd, bCd = S([128, 2, 4, 2, 64], BF16)
            mkb = mk[:].unsqueeze(1).unsqueeze(3).broadcast_to([128, 4, 2, 16])
            for part in range(2):
                sc = 1.0 if part == 0 else -1.0
                for x in range(2):
                    c.op('dve', lambda e: e.tensor_scalar(out=Cd[:, :, :, x, :], in0=Cn[part][0][:], scalar1=sc, scalar2=None, op0=ALU.mult), reads=[Cn[part][1], bCd], writes=[bCd])
                for r in range(2):
                    for cb in range(4):
                        c.op('pe', lambda e: e.transpose(out=ptb[:, 0:128], in_=Cd[:, r, cb, :, :].rearrange("p a b -> p (a b)"), identity=g.identb[:]),
                             reads=[bCd, g.b_identb], writes=[bptb])
                        c.op('dve', lambda e: e.tensor_tensor(out=WC[part][:, r, cb, :].rearrange("p (k a b) -> p k a b", k=4, a=2), in0=ptb[:, 0:128].rearrange("p (k a b) -> p k a b", k=4, a=2),
                                                              in1=mkb, op=ALU.mult), reads=[bptb, bmk], writes=[bWC])
            for part in range(2):
                c.op('act', lambda e: e.activation(out=WBx[part][64:128], in_=WB[part][64:128], func=AF.Copy), reads=[bWB], writes=[bWB])
                c.op('pool', lambda e: e.memset(WBx[part][64:96], 0.0), reads=[bWB], writes=[bWB])
                c.op('act', lambda e: e.activation(out=WCx[part][:], in_=WC[part][:, :, :, 64:128], func=AF.Copy), reads=[bWC], writes=[bWC])
                c.op('pool', lambda e: e.memset(WCx[part][:, :, :, 0:32], 0.0), reads=[bWC], writes=[bWC])
            barrier(c)
        if stage < 2:
            return
        with ExitStack() as es:
            def T(name, shape, dt):
                return es.enter_context(nc.sbuf_tensor(uniq(name), shape, dt))
            uf = T("s5_uf", [128, TB5 * 2], F32); buf_ = Buf()
            ub = [T("s5_ub%d" % r, [128, L], BF16) for r in range(2)]; bub = [Buf(), Buf()]
            Xa = [[T("s5_X%d%d" % (p, r), [128, L], BF16) for r in range(2)] for p in range(2)]
            bXa = [[Buf(), Buf()], [Buf(), Buf()]]
            tcos = T("s5_cos", [128, TB5], F32); tsin = T("s5_sin", [128, TB5], F32); btab = Buf()
            BUs = [T("s5_BU%d" % p, [128, TB5], F32) for p in range(2)]; bBUs = [Buf(), Buf()]
            t = [T("s5_w%d" % i, [128, TB5], F32) for i in range(4)]; bt = [Buf() for _ in range(4)]
            ini = T("s5_ini", [128, 4], F32); bini = Buf()
            yst = [T("s5_yst%d" % i, [128, 512], F32) for i in range(2)]; byst = [Buf(), Buf()]
            npz = 0
            for cb in range(4):
                for r in range(2):
                    for hh in range(L // (2 * TB5)):
                        nb = hh if r == 0 else L // (2 * TB5) - 1 - hh
                        c.dma('sp', lambda e: e.dma_start(out=uf[:], in_=PT[U0 + cb * 128:U0 + (cb + 1) * 128, nb * 2 * TB5:(nb + 1) * 2 * TB5]), reads=[bPT], writes=[buf_])
                        src = uf[:, ::-1] if r else uf[:, :]
                        c.op('act', lambda e: e.activation(out=ub[r][:, hh * 2 * TB5:(hh + 1) * 2 * TB5], in_=src, func=AF.Copy), reads=[buf_], writes=[bub[r]])
                for k in range(4):
                    gp = cb * 4 + k
                    for r in range(2):
                        col = r * 16 + gp
                        c.op('pool', lambda e: e.memset(tcos[:, 0:1], 1.0), writes=[btab])
                        c.op('pool', lambda e: e.memset(tsin[:, 0:1], 0.0), reads=[btab], writes=[btab])
                        n = 1
                        kk = 0
                        while n < TB5:
                            cr = pwc[:, kk, col:col + 1]; ci = pws[:, kk, col:col + 1]
                            c.op('pool', lambda e: e.tensor_scalar(out=t[0][:, 0:n], in0=tsin[:, 0:n], scalar1=ci, scalar2=None, op0=ALU.mult), reads=[btab, bpw], writes=[bt[0]])
                            c.op('pool', lambda e: e.tensor_scalar(out=t[1][:, 0:n], in0=tsin[:, 0:n], scalar1=cr, scalar2=None, op0=ALU.mult), reads=[btab, bpw], writes=[bt[1]])
                            c.op('dve', lambda e: e.scalar_tensor_tensor(out=tsin[:, n:2 * n], in0=tcos[:, 0:n], scalar=ci, in1=t[1][:, 0:n], op0=ALU.mult, op1=ALU.add),
                                 reads=[btab, bpw, bt[1]], writes=[btab])
                            c.op('dve', lambda e: e.scalar_tensor_tensor(out=tcos[:, n:2 * n], in0=tcos[:, 0:n], scalar=cr, in1=t[0][:, 0:n], op0=ALU.mult, op1=ALU.subtract),
                                 reads=[btab, bpw, bt[0]], writes=[btab])
                            n *= 2; kk += 1
                        cTB = pwc[:, kk, col:col + 1]; sTB = pws[:, kk, col:col + 1]
                        rho = mag[:, col:col + 1]
                        c.op('pool', lambda e: e.memset(ini[:], 0.0), writes=[bini])
                        for tb in range(NTB5):
                            ts0 = tb * TB5
                            for part in range(2):
                                for hf in range(TB5 // 512):
                                    ps, bps = g.ps[npz % 4]; npz += 1
                                    if k < 3:
                                        lh = WB[part][32 * k:32 * k + 32, r, cb, :]; rh = ub[r][32 * k:32 * k + 32, ts0 + hf * 512:ts0 + (hf + 1) * 512]
                                    else:
                                        lh = WBx[part][64:128, r, cb, :]; rh = ub[r][64:128, ts0 + hf * 512:ts0 + (hf + 1) * 512]
                                    c.op('pe', lambda e: e.matmul(ps[:, :], lhsT=lh, rhs=rh, start=True, stop=True),
                                         reads=[bWB, bub[r]], writes=[bps])
                                    c.op('act', lambda e: e.activation(out=BUs[part][:, hf * 512:(hf + 1) * 512], in_=ps[:, :], func=AF.Copy), reads=[bps], writes=[bBUs[part]])
                            c.op('dve', lambda e: e.tensor_tensor(out=t[0][:], in0=BUs[0][:], in1=tcos[:], op=ALU.mult), reads=[bBUs[0], btab], writes=[bt[0]])
                            c.op('pool', lambda e: e.tensor_tensor(out=t[1][:], in0=BUs[1][:], in1=tsin[:], op=ALU.mult), reads=[bBUs[1], btab], writes=[bt[1]])
                            c.op('dve', lambda e: e.tensor_tensor(out=t[0][:], in0=t[0][:], in1=t[1][:], op=ALU.add), reads=[bt[0], bt[1]], writes=[bt[0]])
                            c.op('pool', lambda e: e.tensor_tensor(out=t[2][:], in0=BUs[1][:], in1=tcos[:], op=ALU.mult), reads=[bBUs[1], btab], writes=[bt[2]])
                            c.op('dve', lambda e: e.tensor_tensor(out=t[3][:], in0=BUs[0][:], in1=tsin[:], op=ALU.mult), reads=[bBUs[0], btab], writes=[bt[3]])
                            c.op('pool', lambda e: e.tensor_tensor(out=t[2][:], in0=t[2][:], in1=t[3][:], op=ALU.subtract), reads=[bt[2], bt[3]], writes=[bt[2]])
                            c.op('dve', lambda e: e.tensor_tensor_scan(out=t[1][:], data0=rho.broadcast_to([128, TB5]), data1=t[0][:], initial=ini[:, 0:1], op0=ALU.mult, op1=ALU.add),
                                 reads=[bmag, bt[0], bini], writes=[bt[1]])
                            c.op('dve', lambda e: e.tensor_tensor_scan(out=t[3][:], data0=rho.broadcast_to([128, TB5]), data1=t[2][:], initial=ini[:, 1:2], op0=ALU.mult, op1=ALU.add),
                                 reads=[bmag, bt[2], bini], writes=[bt[3]])
                            if tb < NTB5 - 1:
                                xr = t[1][:, TB5 - 1:TB5]; xi = t[3][:, TB5 - 1:TB5]
                                c.op('dve', lambda e: e.tensor_scalar(out=ini[:, 2:3], in0=xi, scalar1=sTB, scalar2=None, op0=ALU.mult), reads=[bt[3], bpw], writes=[bini])
                                c.op('dve', lambda e: e.scalar_tensor_tensor(out=ini[:, 0:1], in0=xr, scalar=cTB, in1=ini[:, 2:3], op0=ALU.mult, op1=ALU.subtract), reads=[bt[1], bpw, bini], writes=[bini])
                                c.op('dve', lambda e: e.tensor_scalar(out=ini[:, 3:4], in0=xi, scalar1=cTB, scalar2=None, op0=ALU.mult), reads=[bt[3], bpw], writes=[bini])
                                c.op('dve', lambda e: e.scalar_tensor_tensor(out=ini[:, 1:2], in0=xr, scalar=sTB, in1=ini[:, 3:4], op0=ALU.mult, op1=ALU.add), reads=[bt[1], bpw, bini], writes=[bini])
                            if r == 0:
                                oslc = slice(ts0, ts0 + TB5)
                                xo_re = Xa[0][r][:, oslc]; xo_im = Xa[1][r][:, oslc]
                            else:
                                lo = L - ts0 - TB5
                                xo_re = Xa[0][r][:, lo:lo + TB5][:, ::-1]; xo_im = Xa[1][r][:, lo:lo + TB5][:, ::-1]
                            c.op('dve', lambda e: e.tensor_tensor(out=t[0][:], in0=t[1][:], in1=tcos[:], op=ALU.mult), reads=[bt[1], btab], writes=[bt[0]])
                            c.op('pool', lambda e: e.tensor_tensor(out=t[2][:], in0=t[3][:], in1=tsin[:], op=ALU.mult), reads=[bt[3], btab], writes=[bt[2]])
                            c.op('pool', lambda e: e.tensor_tensor(out=xo_re, in0=t[0][:], in1=t[2][:], op=ALU.subtract), reads=[bt[0], bt[2]], pwrites=[bXa[0][r]])
                            c.op('dve', lambda e: e.tensor_tensor(out=t[0][:], in0=t[1][:], in1=tsin[:], op=ALU.mult), reads=[bt[1], btab], writes=[bt[0]])
                            c.op('pool', lambda e: e.tensor_tensor(out=t[2][:], in0=t[3][:], in1=tcos[:], op=ALU.mult), reads=[bt[3], btab], writes=[bt[2]])
                            c.op('dve', lambda e: e.tensor_tensor(out=xo_im, in0=t[0][:], in1=t[2][:], op=ALU.add), reads=[bt[0], bt[2]], pwrites=[bXa[1][r]])
                        bXa[0][r].seal(); bXa[1][r].seal()
                    for it in range(L // 512):
                        ps, bps = g.ps[4 + it % 2]
                        i = 0
                        for r in range(2):
                            for part in range(2):
                                if k < 3:
                                    po = ps[32 * k:32 * k + 32, :]; lh = WC[part][:, r, cb, 32 * k:32 * k + 32]
                                else:
                                    po = ps[64:128, :]; lh = WCx[part][:, r, cb, :]
                                c.op('pe', lambda e: e.matmul(po, lhsT=lh, rhs=Xa[part][r][:, it * 512:(it + 1) * 512], start=(i == 0), stop=(i == 3)),
                                     reads=[bWC, bXa[part][r]], writes=[bps])
                                i += 1
                        ys, bys = yst[it % 2], byst[it % 2]
                        e0 = 32 * k if k < 3 else 64
                        c.op('act', lambda e: e.activation(out=ys[e0:32 * k + 32, :], in_=ps[e0:32 * k + 32, :], func=AF.Copy), reads=[bps], writes=[bys])
                        c.dma('sp', lambda e: e.dma_start(out=YT[cb * 128 + 32 * k:cb * 128 + 32 * k + 32, it * 512:(it + 1) * 512], in_=ys[32 * k:32 * k + 32, :]), reads=[bys], pwrites=[bYT])
            bYT.seal()
            barrier(c)
        if stage < 3:
            return
        with ExitStack() as es:
            def T(name, shape, dt):
                return es.enter_context(nc.sbuf_tensor(uniq(name), shape, dt))
            gw = T("s5_gw", [128, 4, 512], BF16); bgw = Buf()
            dcol = T("s5_dcol", [128, 4], F32); bdcol = Buf()
            gbc = T("s5_gbc", [128, 4], F32); bgbc = Buf()
            yt = [T("s5_yt%d" % i, [128, 4, 512], F32) for i in range(2)]; byt = [Buf(), Buf()]
            ut = [T("s5_ut%d" % i, [128, 4, 512], F32) for i in range(2)]; but = [Buf(), Buf()]
            sq = T("s5_sq", [128, 4, 512], F32); bsq = Buf()
            gy = T("s5_gy", [128, 4, 512], F32); bgy = Buf()
            gyb = T("s5_gyb", [128, 4, 512], BF16); bgyb = Buf()
            sg = [T("s5_sg%d" % i, [128, 512], F32) for i in range(2)]; bsg = [Buf(), Buf()]
            ob = [T("s5_ob%d" % i, [128, 4, 512], BF16) for i in range(2)]; bob = [Buf(), Buf()]
            c.dma('pool', lambda e: e.dma_start(out=gw[:], in_=glu_w.rearrange("(k p) f -> p k f", p=128)), writes=[bgw])
            with nc.allow_non_contiguous_dma(reason="small params"):
                c.dma('sp', lambda e: e.dma_start(out=dcol[:], in_=d_skip.rearrange("(k p) -> p k", p=128)), writes=[bdcol])
                c.dma('sp', lambda e: e.dma_start(out=gbc[:], in_=glu_b.rearrange("(k p) -> p k", p=128)), writes=[bgbc])
            GC = 1.5957691216057308
            for it in range(L // 512):
                yt_, byt_ = yt[it % 2], byt[it % 2]
                ut_, but_ = ut[it % 2], but[it % 2]
                ob_, bob_ = ob[it % 2], bob[it % 2]
                tsl = slice(it * 512, (it + 1) * 512)
                c.dma('sp', lambda e: e.dma_start(out=yt_[:], in_=YT[:, tsl].rearrange("(k p) t -> p k t", p=128)), reads=[bYT], writes=[byt_])
                c.dma('act', lambda e: e.dma_start(out=ut_[:], in_=PT[U0:U0 + 512, tsl].rearrange("(k p) t -> p k t", p=128)), reads=[bPT], writes=[but_])
                for k in range(4):
                    c.op('dve', lambda e: e.scalar_tensor_tensor(out=yt_[:, k, :], in0=ut_[:, k, :], scalar=dcol[:, k:k + 1], in1=yt_[:, k, :], op0=ALU.mult, op1=ALU.add),
                         reads=[but_, bdcol, byt_], writes=[byt_])
                c.op('pool', lambda e: e.tensor_tensor(out=sq[:], in0=yt_[:], in1=yt_[:], op=ALU.mult), reads=[byt_], writes=[bsq])
                c.op('dve', lambda e: e.tensor_scalar(out=sq[:], in0=sq[:], scalar1=0.044715, scalar2=1.0, op0=ALU.mult, op1=ALU.add), reads=[bsq], writes=[bsq])
                c.op('pool', lambda e: e.tensor_tensor(out=sq[:], in0=sq[:], in1=yt_[:], op=ALU.mult), reads=[bsq, byt_], writes=[bsq])
                c.op('act', lambda e: e.activation(out=sq[:], in_=sq[:], func=AF.Sigmoid, scale=GC), reads=[bsq], writes=[bsq])
                c.op('dve', lambda e: e.tensor_tensor(out=gy[:], in0=sq[:], in1=yt_[:], op=ALU.mult), reads=[bsq, byt_], writes=[bgy])
                c.op('act', lambda e: e.activation(out=gyb[:], in_=gy[:], func=AF.Copy), reads=[bgy], writes=[bgyb])
                for co in range(4):
                    ps, bps = g.ps[co % 4]
                    for k in range(4):
                        c.op('pe', lambda e: e.matmul(ps[:, :], lhsT=gw[:, k, co * 128:(co + 1) * 128], rhs=gyb[:, k, :], start=(k == 0), stop=(k == 3)),
                             reads=[bgw, bgyb], writes=[bps])
                    sg_, bsg_ = sg[co % 2], bsg[co % 2]
                    c.op('act', lambda e: e.activation(out=sg_[:], in_=ps[:, :], func=AF.Sigmoid, bias=gbc[:, co:co + 1], scale=1.0), reads=[bps, bgbc], writes=[bsg_])
                    c.op('dve', lambda e: e.tensor_tensor(out=ob_[:, co, :], in0=gy[:, co, :], in1=sg_[:], op=ALU.mult), reads=[bgy, bsg_, bob_], writes=[bob_])
                c.dma('sp', lambda e: e.dma_start(out=MT[512:1024, tsl].rearrange("(k p) t -> p k t", p=128), in_=ob_[:]), reads=[bob_], pwrites=[bMT])
            barrier(c)


def outproj_phase(c, g, MT, bMT, w_out, X, bX):
    nc = c.nc
    bXn = Buf('Xn')
    with ExitStack() as es:
        def T(name, shape, dt):
            return es.enter_context(nc.sbuf_tensor(uniq(name), shape, dt))
        wsb = T("op_w", [128, 8, D], BF16); bw = Buf()
        mt = [T("op_mt%d" % i, [128, 8, 512], BF16) for i in range(2)]; bmt = [Buf(), Buf()]
        xt = [T("op_xt%d" % i, [128, 4, D], F32) for i in range(2)]; bxt = [Buf(), Buf()]
        xo = [T("op_xo%d" % i, [128, 4, D], F32) for i in range(2)]; bxo = [Buf(), Buf()]
        for c0 in range(0, D, 512):
            c.dma('pool', lambda e: e.dma_start(out=wsb[:, :, c0:c0 + 512], in_=w_out[:, c0:c0 + 512].rearrange("(k p) f -> p k f", p=128)), pwrites=[bw])
        bw.seal()
        n = 0
        for it in range(L // 512):
            t0 = it * 512
            mt_, bmt_ = mt[it % 2], bmt[it % 2]
            xt_, bxt_ = xt[it % 2], bxt[it % 2]
            xo_, bxo_ = xo[it % 2], bxo[it % 2]
            c.dma('sp', lambda e: e.dma_start(out=mt_[:], in_=MT[:, t0:t0 + 512].rearrange("(k p) t -> p k t", p=128)), reads=[bMT], writes=[bmt_])
            c.dma('act', lambda e: e.dma_start(out=xt_[:], in_=X[t0:t0 + 512, :].rearrange("(j p) d -> p j d", p=128)), reads=[bX], writes=[bxt_])
            for j in range(4):
                for dh in range(2):
                    ps, bps = g.ps[n % 4]; n += 1
                    for k in range(8):
                        c.op('pe', lambda e: e.matmul(ps[:, :], lhsT=mt_[:, k, j * 128:(j + 1) * 128], rhs=wsb[:, k, dh * 512:(dh + 1) * 512], start=(k == 0), stop=(k == 7)),
                             reads=[bmt_, bw], writes=[bps])
                    c.op('dve', lambda e: e.tensor_tensor(out=xo_[:, j, dh * 512:(dh + 1) * 512], in0=ps[:, :], in1=xt_[:, j, dh * 512:(dh + 1) * 512], op=ALU.add),
                         reads=[bps, bxt_, bxo_], writes=[bxo_])
            c.dma('sp', lambda e: e.dma_start(out=X[t0:t0 + 512, :].rearrange("(j p) d -> p j d", p=128), in_=xo_[:]), reads=[bxo_], pwrites=[bXn])
        bXn.seal()
        barrier(c)
    return bXn


def t5_onehot():
    half = 16; max_exact = 8
    rel = np.arange(-255, 256)
    n = np.abs(rel)
    nf = np.maximum(n, 1).astype(np.float32)
    large = max_exact + (np.log(nf / np.float32(max_exact)) / np.float32(math.log(128 / max_exact)) * np.float32(half - max_exact)).astype(np.int32)
    large = np.minimum(large, half - 1)
    b = np.where(rel > 0, half, 0) + np.where(n < max_exact, n, large)
    oh = np.zeros((32, 512), np.float32)
    oh[b, np.arange(511)] = 1.0
    return oh


def attn_phase(c, g, PV, bPV, q_gain, k_gain, c_lambda, out_gain, rel_bias, onehot, layer_idx, QKT, bQKT, FV, bFV, MT, bMT, stage=3):
    nc = c.nc
    lam_init = 0.8 - 0.6 * math.exp(-0.3 * layer_idx)
    ptb, bptb = g.psb
    with ExitStack() as es:
        def T(name, shape, dt):
            return es.enter_context(nc.sbuf_tensor(uniq(name), shape, dt))
        g64 = T("at_g64", [128, 2, 64], F32); bg64 = Buf()
        gQK = T("at_gQK", [128, 16, 64], F32); bgQK = Buf()
        xq = [T("at_xq%d" % i, [128, 1024], BF16) for i in range(2)]; bxq = [Buf(), Buf()]
        sq = T("at_sq", [128, 1024], F32); bsq = Buf()
        ss = [T("at_ss%d" % i, [128, 16], F32) for i in range(2)]; bss = [Buf(), Buf()]
        xn = T("at_xn", [128, 1024], F32); bxn = Buf()
        xb = [T("at_xb%d" % i, [128, 1024], BF16) for i in range(2)]; bxb = [Buf(), Buf()]
        st = [T("at_st%d" % i, [128, 8, 512], BF16) for i in range(2)]; bst = [Buf(), Buf()]
        c.dma('sp', lambda e: e.dma_start(out=g64[:, 0, :], in_=q_gain.partition_broadcast(128)), pwrites=[bg64])
        c.dma('sp', lambda e: e.dma_start(out=g64[:, 1, :], in_=k_gain.partition_broadcast(128)), pwrites=[bg64])
        bg64.seal()
        c.op('dve', lambda e: e.tensor_scalar(out=gQK[:, 0:8, :], in0=g64[:, 0:1, :].broadcast_to([128, 8, 64]), scalar1=0.125, scalar2=None, op0=ALU.mult), reads=[bg64], writes=[bgQK])
        c.op('dve', lambda e: e.tensor_copy(out=gQK[:, 8:16, :], in_=g64[:, 1:2, :].broadcast_to([128, 8, 64])), reads=[bg64, bgQK], writes=[bgQK])
        for i in range(NT):
            xq_, bxq_ = xq[i % 2], bxq[i % 2]
            ss_, bss_ = ss[i % 2], bss[i % 2]
            xb_, bxb_ = xb[i % 2], bxb[i % 2]
            st_, bst_ = st[(i // 4) % 2], bst[(i // 4) % 2]
            c.dma('sp', lambda e: e.dma_start(out=xq_[:], in_=PV[i * 128:(i + 1) * 128, 0:1024]), reads=[bPV], writes=[bxq_])
            c.op('pool', lambda e: e.tensor_tensor(out=sq[:], in0=xq_[:], in1=xq_[:], op=ALU.mult), reads=[bxq_], writes=[bsq])
            c.op('dve', lambda e: e.tensor_reduce(out=ss_[:], in_=sq[:].rearrange("p (a d) -> p a d", d=64), axis=AX.X, op=ALU.add), reads=[bsq], writes=[bss_])
            c.op('dve', lambda e: e.tensor_scalar(out=ss_[:], in0=ss_[:], scalar1=1.0 / 64, scalar2=1e-6, op0=ALU.mult, op1=ALU.add), reads=[bss_], writes=[bss_])
            c.op('pool', lambda e: e.tensor_tensor(out=ss_[:], in0=ss_[:], in1=g.neghalf[:, 0:1].broadcast_to([128, 16]), op=ALU.pow), reads=[bss_, g.b_neghalf], writes=[bss_])
            c.op('dve', lambda e: e.tensor_tensor(out=xn[:].rearrange("p (a d) -> p a d", d=64), in0=xq_[:].rearrange("p (a d) -> p a d", d=64),
                                                  in1=ss_[:].unsqueeze(2).broadcast_to([128, 16, 64]), op=ALU.mult), reads=[bxq_, bss_], writes=[bxn])
            c.op('pool', lambda e: e.tensor_tensor(out=xb_[:], in0=xn[:], in1=gQK[:].rearrange("p a d -> p (a d)"), op=ALU.mult), reads=[bxn, bgQK], writes=[bxb_])
            for a in range(8):
                c.op('pe', lambda e: e.transpose(out=ptb[:, a * 128:(a + 1) * 128], in_=xb_[:, a * 128:(a + 1) * 128], identity=g.identb[:]), reads=[bxb_, g.b_identb], writes=[bptb])
            c.op('act', lambda e: e.activation(out=st_[:, :, (i % 4) * 128:(i % 4 + 1) * 128], in_=ptb[:, :].rearrange("p (a s) -> p a s", a=8), func=AF.Copy), reads=[bptb], writes=[bst_])
            if i % 4 == 3:
                t0 = (i // 4) * 512
                for a in range(8):
                    c.dma('sp' if a % 2 else 'act', lambda e: e.dma_start(out=QKT[a, :, t0:t0 + 512], in_=st_[:, a, :]), reads=[bst_], pwrites=[bQKT])
        bQKT.seal()
        barrier(c)
    if stage < 2:
        return
    with ExitStack() as es:
        def T(name, shape, dt):
            return es.enter_context(nc.sbuf_tensor(uniq(name), shape, dt))
        KT = T("at_KT", [128, L], BF16); bKT = Buf()
        Va = T("at_Va", [128, 64, 130], BF16); bVa = Buf()
        QT = [T("at_QT%d" % i, [128, 512], BF16) for i in range(2)]; bQT = [Buf(), Buf()]
        Pt = [T("at_P%d" % i, [128, 512], BF16) for i in range(3)]; bPt = [Buf() for _ in range(3)]
        tmp = [T("at_tmp%d" % i, [128, 512], F32) for i in range(2)]; btmp = [Buf(), Buf()]
        biasT = T("at_bias", [128, 4, 3, 128], F32); bbias = Buf()
        hank = T("at_hank", [128, 128], F32); bhank = Buf()
        cfar = T("at_cfar", [128, 4, 2], F32); bcfar = Buf()
        tab = T("at_tab", [32, 4], F32); btab = Buf()
        oh = T("at_oh", [32, 512], F32); boh = Buf()
        fv = T("at_fv", [4, 512], F32); bfv = Buf()
        lamt = T("at_lamt", [128, 4, 64], F32); blamt = Buf()
        lam = T("at_lam", [128, 8], F32); blam = Buf()
        gO = T("at_gO", [128, 128], F32); bgO = Buf()
        rs = [T("at_rs%d" % i, [128, 4], F32) for i in range(2)]; brs = [Buf(), Buf()]
        t1 = T("at_t1", [128, 128], F32); bt1 = Buf()
        w_ = T("at_w", [128, 128], F32); bw_ = Buf()
        junk = T("at_junk", [128, 128], F32); bjunk = Buf()
        wb = [T("at_wb%d" % i, [128, 128], BF16) for i in range(2)]; bwb = [Buf(), Buf()]
        ost = [T("at_ost%d" % i, [128, 512], BF16) for i in range(2)]; bost = [Buf(), Buf()]
        c.dma('sp', lambda e: e.dma_start(out=lamt[:].rearrange("p a d -> p (a d)"), in_=c_lambda.rearrange("a d -> (a d)").partition_broadcast(128)), writes=[blamt])
        c.op('dve', lambda e: e.tensor_tensor(out=lamt[:, 0, :], in0=lamt[:, 0, :], in1=lamt[:, 1, :], op=ALU.mult), reads=[blamt], writes=[blamt])
        c.op('dve', lambda e: e.tensor_tensor(out=lamt[:, 2, :], in0=lamt[:, 2, :], in1=lamt[:, 3, :], op=ALU.mult), reads=[blamt], writes=[blamt])
        c.op('dve', lambda e: e.tensor_reduce(out=lam[:, 0:1], in_=lamt[:, 0, :], axis=AX.X, op=ALU.add), reads=[blamt], writes=[blam])
        c.op('dve', lambda e: e.tensor_reduce(out=lam[:, 1:2], in_=lamt[:, 2, :], axis=AX.X, op=ALU.add), reads=[blamt, blam], writes=[blam])
        c.op('act', lambda e: e.activation(out=lam[:, 2:4], in_=lam[:, 0:2], func=AF.Exp), reads=[blam], writes=[blam])
        c.op('dve', lambda e: e.tensor_tensor(out=lam[:, 4:5], in0=lam[:, 3:4], in1=lam[:, 2:3], op=ALU.subtract), reads=[blam], writes=[blam])
        c.op('dve', lambda e: e.tensor_scalar(out=lam[:, 4:5], in0=lam[:, 4:5], scalar1=-lam_init, scalar2=None, op0=ALU.add), reads=[blam], writes=[blam])
        c.dma('sp', lambda e: e.dma_start(out=gO[:], in_=out_gain.partition_broadcast(128)), writes=[bgO])
        c.op('dve', lambda e: e.tensor_scalar(out=gO[:], in0=gO[:], scalar1=1.0 - lam_init, scalar2=None, op0=ALU.mult), reads=[bgO], writes=[bgO])
        c.dma('sp', lambda e: e.dma_start(out=tab[:], in_=rel_bias), writes=[btab])
        c.dma('act', lambda e: e.dma_start(out=oh[:], in_=onehot), writes=[boh])
        ps6, bps6 = g.ps[6]
        c.op('pe', lambda e: e.matmul(ps6[0:4, :], lhsT=tab[:, :], rhs=oh[:, :], start=True, stop=True), reads=[btab, boh], writes=[bps6])
        c.op('dve', lambda e: e.tensor_copy(out=fv[:], in_=ps6[0:4, :]), reads=[bps6], writes=[bfv])
        c.dma('sp', lambda e: e.dma_start(out=FV, in_=fv[:]), reads=[bfv], writes=[bFV])
        for h in range(4):
            for o in (-1, 0, 1):
                off = h * 512 + 128 * o + 128
                src = bass.AP(FV.tensor, off, [[1, 128], [1, 128]])
                c.dma('sp', lambda e: e.dma_start(out=hank[:], in_=src), reads=[bFV], writes=[bhank])
                c.op('dve', lambda e: e.tensor_copy(out=biasT[:, h, o + 1, :], in_=hank[:, ::-1]), reads=[bhank, bbias], writes=[bbias])
            c.dma('sp', lambda e: e.dma_start(out=cfar[:, h, 0:1], in_=bass.AP(FV.tensor, h * 512 + 0, [[0, 128], [1, 1]])), reads=[bFV], pwrites=[bcfar])
            c.dma('sp', lambda e: e.dma_start(out=cfar[:, h, 1:2], in_=bass.AP(FV.tensor, h * 512 + 510, [[0, 128], [1, 1]])), reads=[bFV], pwrites=[bcfar])
        bcfar.seal()
        ones_col_done = False
        nS = 0; nP = 0; nq = 0; ntmp = 0; nout = 0
        for h in range(4):
            c.dma('sp', lambda e: e.dma_start(out=KT[:], in_=QKT[4 + h, :, :]), reads=[bQKT], writes=[bKT])
            for half in range(2):
                c.dma('act', lambda e: e.dma_start(out=Va[:, half * 32:(half + 1) * 32, 0:128], in_=PV[half * 4096:(half + 1) * 4096, 1024 + h * 128:1024 + (h + 1) * 128].rearrange("(b p) d -> p b d", p=128)),
                      reads=[bPV], writes=[bVa])
            c.op('pool', lambda e: e.memset(Va[:, :, 128:129], 1.0), reads=[bVa], writes=[bVa])
            for qt in range(16):
                QT_, bQT_ = QT[nq % 2], bQT[nq % 2]; nq += 1
                c.dma('sp', lambda e: e.dma_start(out=QT_[:], in_=QKT[h, :, qt * 512:(qt + 1) * 512]), reads=[bQKT], writes=[bQT_])
                steps = [(comp, kb) for comp in range(2) for kb in range(64)]
                Sbank = {}

                def emit_S(i):
                    comp, kb = steps[i]
                    S, bS = g.ps[i % 3]
                    c.op('pe', lambda e: e.matmul(S[:, :], lhsT=KT[64 * comp:64 * comp + 64, kb * 128:(kb + 1) * 128], rhs=QT_[64 * comp:64 * comp + 64, :], start=True, stop=True),
                         reads=[bKT, bQT_], writes=[bS])
                emit_S(0); emit_S(1)
                for i, (comp, kb) in enumerate(steps):
                    S, bS = g.ps[i % 3]
                    P_, bP_ = Pt[nP % 3], bPt[nP % 3]; nP += 1
                    near = (4 * qt - 1 <= kb <= 4 * qt + 4)
                    if not near:
                        col = cfar[:, h, 0:1] if kb < 4 * qt else cfar[:, h, 1:2]
                        c.op('act', lambda e: e.activation(out=P_[:], in_=S[:, :], func=AF.Exp, bias=col, scale=1.0), reads=[bS, bcfar], writes=[bP_])
                    else:
                        tm, btm = tmp[ntmp % 2], btmp[ntmp % 2]; ntmp += 1
                        for qs in range(4):
                            o = kb - (4 * qt + qs)
                            sl = slice(qs * 128, (qs + 1) * 128)
                            if abs(o) <= 1:
                                c.op('dve', lambda e: e.tensor_tensor(out=tm[:, sl], in0=S[:, sl], in1=biasT[:, h, o + 1, :], op=ALU.add), reads=[bS, bbias, btm], writes=[btm])
                            else:
                                col = cfar[:, h, 0:1] if o < 0 else cfar[:, h, 1:2]
                                c.op('dve', lambda e: e.tensor_scalar(out=tm[:, sl], in0=S[:, sl], scalar1=col, scalar2=None, op0=ALU.add), reads=[bS, bcfar, btm], writes=[btm])
                        c.op('act', lambda e: e.activation(out=P_[:], in_=tm[:], func=AF.Exp), reads=[btm], writes=[bP_])
                    if i + 2 < len(steps):
                        emit_S(i + 2)
                    for qs in range(4):
                        a = comp * 4 + qs
                        acc, bacc = g.ps[3 + a // 3]
                        c0 = (a % 3) * 130
                        first = (kb == 0) and ((comp == 0 and a in (0, 3)) or (comp == 1 and a == 6))
                        c.op('pe', lambda e: e.matmul(acc[:, c0:c0 + 129], lhsT=P_[:, qs * 128:(qs + 1) * 128], rhs=Va[:, kb, 0:129], start=first, stop=(kb == 63), skip_group_check=True),
                             reads=[bP_, bVa], writes=[bacc])
                os_, bos_ = ost[nout % 2], bost[nout % 2]; nout += 1
                for qs in range(4):
                    a0 = qs; a1 = 4 + qs
                    acc0, bacc0 = g.ps[3 + a0 // 3]; o0 = (a0 % 3) * 130
                    acc1, bacc1 = g.ps[3 + a1 // 3]; o1 = (a1 % 3) * 130
                    rs_, brs_ = rs[qs % 2], brs[qs % 2]
                    wb_, bwb_ = wb[qs % 2], bwb[qs % 2]
                    c.op('dve', lambda e: e.reciprocal(out=rs_[:, 0:1], in_=acc0[:, o0 + 128:o0 + 129]), reads=[bacc0], writes=[brs_])
                    c.op('dve', lambda e: e.reciprocal(out=rs_[:, 1:2], in_=acc1[:, o1 + 128:o1 + 129]), reads=[bacc1, brs_], writes=[brs_])
                    c.op('dve', lambda e: e.tensor_tensor(out=rs_[:, 1:2], in0=rs_[:, 1:2], in1=lam[:, 4:5], op=ALU.mult), reads=[brs_, blam], writes=[brs_])
                    c.op('dve', lambda e: e.tensor_scalar(out=t1[:], in0=acc1[:, o1:o1 + 128], scalar1=rs_[:, 1:2], scalar2=None, op0=ALU.mult), reads=[bacc1, brs_], writes=[bt1])
                    c.op('dve', lambda e: e.scalar_tensor_tensor(out=w_[:], in0=acc0[:, o0:o0 + 128], scalar=rs_[:, 0:1], in1=t1[:], op0=ALU.mult, op1=ALU.add), reads=[bacc0, brs_, bt1], writes=[bw_])
                    c.op('dve', lambda e: e.scalar_tensor_tensor(out=junk[:], in0=w_[:], scalar=1.0, in1=w_[:], op0=ALU.mult, op1=ALU.mult, accum_out=rs_[:, 2:3]), reads=[bw_, brs_], writes=[bjunk, brs_])
                    c.op('dve', lambda e: e.tensor_scalar(out=rs_[:, 2:3], in0=rs_[:, 2:3], scalar1=1.0 / 128, scalar2=1e-6, op0=ALU.mult, op1=ALU.add), reads=[brs_], writes=[brs_])
                    c.op('pool', lambda e: e.tensor_tensor(out=rs_[:, 2:3], in0=rs_[:, 2:3], in1=g.neghalf[:, 0:1], op=ALU.pow), reads=[brs_, g.b_neghalf], writes=[brs_])
                    c.op('dve', lambda e: e.scalar_tensor_tensor(out=wb_[:], in0=w_[:], scalar=rs_[:, 2:3], in1=gO[:], op0=ALU.mult, op1=ALU.mult), reads=[bw_, brs_, bgO], writes=[bwb_])
                    c.op('pe', lambda e: e.transpose(out=ptb[:, qs * 128:(qs + 1) * 128], in_=wb_[:], identity=g.identb[:]), reads=[bwb_, g.b_identb], writes=[bptb])
                c.op('act', lambda e: e.activation(out=os_[:], in_=ptb[:, 0:512], func=AF.Copy), reads=[bptb], writes=[bos_])
                c.dma('sp', lambda e: e.dma_start(out=MT[h * 128:(h + 1) * 128, qt * 512:(qt + 1) * 512], in_=os_[:]), reads=[bos_], pwrites=[bMT])
        barrier(c)


GTB = 512


def gated_norm_finalize(c, g, OA, bOAs, PV, bPV, gcol0, gain_ap, MT, bMT, row0, pfx, OA2=None, bOA2s=()):
    nc = c.nc
    with ExitStack() as es:
        def T(name, shape, dt):
            return es.enter_context(nc.sbuf_tensor(uniq(pfx + name), shape, dt))
        gA = T("gA", [128, 128], F32); bgA = Buf()
        oa = [T("oa%d" % i, [128, 512], F32) for i in range(2)]; boa = [Buf(), Buf()]
        oa2 = [T("oa2%d" % i, [128, 512], F32) for i in range(2)]; boa2 = [Buf(), Buf()]
        ga = [T("ga%d" % i, [128, 512], BF16) for i in range(2)]; bga = [Buf(), Buf()]
        sq = T("sq", [128, 512], F32); bsq = Buf()
        ssq = [T("ssq%d" % i, [128, 4], F32) for i in range(2)]; bssq = [Buf(), Buf()]
        sg = T("sg", [128, 512], F32); bsg = Buf()
        t1 = T("t1", [128, 512], F32); bt1 = Buf()
        ob = [T("ob%d" % i, [128, 512], BF16) for i in range(2)]; bob = [Buf(), Buf()]
        mt = [T("mt%d" % i, [128, 4, 512], BF16) for i in range(2)]; bmt = [Buf(), Buf()]
        c.dma('sp', lambda e: e.dma_start(out=gA[:], in_=gain_ap.partition_broadcast(128)), writes=[bgA])
        ptb, bptb = g.psb
        for i in range(NT):
            oa_, boa_ = oa[i % 2], boa[i % 2]
            ga_, bga_ = ga[i % 2], bga[i % 2]
            ss_, bss_ = ssq[i % 2], bssq[i % 2]
            ob_, bob_ = ob[i % 2], bob[i % 2]
            mt_, bmt_ = mt[(i // 4) % 2], bmt[(i // 4) % 2]
            c.dma('sp', lambda e: e.dma_start(out=oa_[:], in_=OA[i * 128:(i + 1) * 128, :]), reads=bOAs, writes=[boa_])
            c.dma('act', lambda e: e.dma_start(out=ga_[:], in_=PV[i * 128:(i + 1) * 128, gcol0:gcol0 + 512]), reads=[bPV], writes=[bga_])
            if OA2 is not None:
                o2_, bo2_ = oa2[i % 2], boa2[i % 2]
                c.dma('act', lambda e: e.dma_start(out=o2_[:], in_=OA2[i * 128:(i + 1) * 128, :]), reads=list(bOA2s), writes=[bo2_])
                c.op('dve', lambda e: e.tensor_tensor(out=oa_[:], in0=oa_[:], in1=o2_[:], op=ALU.add), reads=[boa_, bo2_], writes=[boa_])
            c.op('pool', lambda e: e.tensor_tensor(out=sq[:], in0=oa_[:], in1=oa_[:], op=ALU.mult), reads=[boa_], writes=[bsq])
            c.op('dve', lambda e: e.tensor_reduce(out=ss_[:], in_=sq[:].rearrange("p (h d) -> p h d", h=4), axis=AX.X, op=ALU.add), reads=[bsq], writes=[bss_])
            c.op('dve', lambda e: e.tensor_scalar(out=ss_[:], in0=ss_[:], scalar1=1.0 / 128, scalar2=1e-6, op0=ALU.mult, op1=ALU.add), reads=[bss_], writes=[bss_])
            c.op('pool', lambda e: e.tensor_tensor(out=ss_[:], in0=ss_[:], in1=g.neghalf[:, 0:1].broadcast_to([128, 4]), op=ALU.pow), reads=[bss_, g.b_neghalf], writes=[bss_])
            c.op('act', lambda e: e.activation(out=sg[:], in_=ga_[:], func=AF.Silu), reads=[bga_], writes=[bsg])
            c.op('dve', lambda e: e.tensor_tensor(out=t1[:].rearrange("p (h d) -> p h d", h=4), in0=oa_[:].rearrange("p (h d) -> p h d", h=4),
                                                  in1=ss_[:].unsqueeze(2).broadcast_to([128, 4, 128]), op=ALU.mult), reads=[boa_, bss_], writes=[bt1])
            c.op('pool', lambda e: e.tensor_tensor(out=t1[:].rearrange("p (h d) -> p h d", h=4), in0=t1[:].rearrange("p (h d) -> p h d", h=4),
                                                   in1=gA[:].unsqueeze(1).broadcast_to([128, 4, 128]), op=ALU.mult), reads=[bt1, bgA], writes=[bt1])
            c.op('dve', lambda e: e.tensor_tensor(out=ob_[:], in0=t1[:], in1=sg[:], op=ALU.mult), reads=[bt1, bsg], writes=[bob_])
            for k in range(4):
                c.op('pe', lambda e: e.transpose(out=ptb[:, k * 128:(k + 1) * 128], in_=ob_[:, k * 128:(k + 1) * 128], identity=g.identb[:]),
                     reads=[bob_, g.b_identb], writes=[bptb])
            c.op('act', lambda e: e.activation(out=mt_[:, :, (i % 4) * 128:(i % 4 + 1) * 128], in_=ptb[:, 0:512].rearrange("p (k s) -> p k s", k=4), func=AF.Copy),
                 reads=[bptb], writes=[bmt_])
            if i % 4 == 3:
                t0 = (i // 4) * 512
                c.dma('sp', lambda e: e.dma_start(out=MT[row0:row0 + 512, t0:t0 + 512].rearrange("(k p) t -> p k t", p=128), in_=mt_[:]), reads=[bmt_], pwrites=[bMT])
        barrier(c)


def gdn_phase(c, g, PT, bPT, PV, bPV, conv_w, a_log, dt_bias, out_gain, GQ, bGQ, GR, bGR, OD, bODs, OD2, bOD2s, MT, bMT, stage=4):
    nc = c.nc
    ptb, bptb = g.psb
    NB = TBK
    with ExitStack() as es:
        def T(name, shape, dt):
            return es.enter_context(nc.sbuf_tensor(uniq(name), shape, dt))
        cw = T("gd_cw", [128, 12, 5], F32); bcw = Buf()
        onesb = T("gd_onesb", [128, 128], BF16); bonesb = Buf()
        xin = [T("gd_xin%d" % i, [128, NB + 4], F32) for i in range(2)]; bxin = [Buf(), Buf()]
        y = T("gd_y", [128, NB], F32); by = Buf()
        s = T("gd_s", [128, NB], F32); bs = Buf()
        sqb = T("gd_sqb", [128, NB], BF16); bsqb = Buf()
        rst = T("gd_rst", [128, NB], F32); brst = Buf()
        ob = [T("gd_ob%d" % i, [128, NB], BF16) for i in range(2)]; bob = [Buf(), Buf()]
        with nc.allow_non_contiguous_dma(reason="small params"):
            for j in range(5):
                c.dma('sp', lambda e: e.dma_start(out=cw[:, :, j], in_=conv_w[j, :].rearrange("(k p) -> p k", p=128)), pwrites=[bcw])
        bcw.seal()
        c.op('pool', lambda e: e.memset(onesb[:], 1.0), writes=[bonesb])
        n = 0
        for cbk in range(12):
            for tb in range(L // NB):
                x_, bx_ = xin[n % 2], bxin[n % 2]
                o_, bo_ = ob[n % 2], bob[n % 2]
                n += 1
                t0 = tb * NB
                lo = max(t0 - 2, 0); hi = min(t0 + NB + 2, L)
                if tb == 0:
                    c.op('pool', lambda e: e.memset(x_[:, 0:2], 0.0), writes=[bx_])
                if tb == L // NB - 1:
                    c.op('pool', lambda e: e.memset(x_[:, NB + 2:NB + 4], 0.0), writes=[bx_])
                c.dma('sp', lambda e: e.dma_start(out=x_[:, lo - (t0 - 2):hi - (t0 - 2)], in_=PT[cbk * 128:(cbk + 1) * 128, lo:hi]), reads=[bPT, bx_], writes=[bx_])
                c.op('dve', lambda e: e.tensor_scalar(out=y[:], in0=x_[:, 0:NB], scalar1=cw[:, cbk, 0:1], scalar2=None, op0=ALU.mult), reads=[bx_, bcw], writes=[by])
                for j in range(1, 5):
                    c.op('dve', lambda e: e.scalar_tensor_tensor(out=y[:], in0=x_[:, j:j + NB], scalar=cw[:, cbk, j:j + 1], in1=y[:], op0=ALU.mult, op1=ALU.add),
                         reads=[bx_, bcw, by], writes=[by])
                c.op('act', lambda e: e.activation(out=s[:], in_=y[:], func=AF.Silu), reads=[by], writes=[bs])
                if cbk < 8:
                    c.op('pool', lambda e: e.tensor_tensor(out=sqb[:], in0=s[:], in1=s[:], op=ALU.mult), reads=[bs], writes=[bsqb])
                    for hf in range(NB // 512):
                        ps, bps = g.ps[hf % 4]
                        c.op('pe', lambda e: e.matmul(ps[:, :], lhsT=onesb[:], rhs=sqb[:, hf * 512:(hf + 1) * 512], start=True, stop=True), reads=[bonesb, bsqb], writes=[bps])
                        c.op('dve', lambda e: e.tensor_scalar(out=rst[:, hf * 512:(hf + 1) * 512], in0=ps[:, :], scalar1=1e-6, scalar2=None, op0=ALU.add), reads=[bps, brst], writes=[brst])
                    c.op('pool', lambda e: e.tensor_tensor(out=rst[:], in0=rst[:], in1=g.neghalf[:, 0:1].broadcast_to([128, NB]), op=ALU.pow), reads=[brst, g.b_neghalf], writes=[brst])
                    sc = (128.0 ** -0.5) if cbk < 4 else 1.0
                    c.op('dve', lambda e: e.scalar_tensor_tensor(out=o_[:], in0=s[:], scalar=sc, in1=rst[:], op0=ALU.mult, op1=ALU.mult), reads=[bs, brst], writes=[bo_])
                else:
                    c.op('act', lambda e: e.activation(out=o_[:], in_=s[:], func=AF.Copy), reads=[bs], writes=[bo_])
                c.dma('act', lambda e: e.dma_start(out=GQ[cbk, :, t0:t0 + NB], in_=o_[:]), reads=[bo_], pwrites=[bGQ])
        bGQ.seal()
        barrier(c)
    if stage < 2:
        return
    with ExitStack() as es0:
        def T0(name, shape, dt):
            return es0.enter_context(nc.sbuf_tensor(uniq(name), shape, dt))
        NQ = 5
        cols = [T0("gd_cols%d" % d, [128, 64, 4 * NQ], F32) for d in range(2)]; bcols = [Buf(), Buf()]
        sel = T0("gd_sel", [4, 4, 128], F32); bsel = Buf()
        with ExitStack() as es:
            def T(name, shape, dt):
                return es.enter_context(nc.sbuf_tensor(uniq(name), shape, dt))
            GP = 2048
            ar = T("gd_ar", [4, GP], F32); bar_ = Buf()
            br = T("gd_br", [4, GP], F32); bbr = Buf()
            w1 = T("gd_w1", [4, GP], F32); bw1 = Buf()
            w2 = T("gd_w2", [4, GP], F32); bw2 = Buf()
            gam = T("gd_gam", [4, GP], F32); bet = T("gd_bet", [4, GP], F32); egam = T("gd_egam", [4, GP], F32); brw = Buf()
            q3 = T("gd_q3", [4, GP], F32); bq3 = Buf()
            q4 = T("gd_q4", [4, GP], F32); bq4 = Buf()
            q5 = T("gd_q5", [4, GP], F32); bq5 = Buf()
            msk = T("gd_msk", [4, GP], F32); bmsk = Buf()
            pc = T("gd_pc", [4, 4], F32); bpc = Buf()
            c.op('pool', lambda e: e.memset(msk[:], 1.0), writes=[bmsk])
            c.op('pool', lambda e: e.memset(msk[:].rearrange("p (c j) -> p c j", j=64)[:, :, 0:1], 0.0), reads=[bmsk], writes=[bmsk])
            c.op('pool', lambda e: e.memset(sel[:], 0.0), writes=[bsel])
            c.op('pool', lambda e: e.affine_select(out=sel[:], in_=sel[:], pattern=[[-1, 4], [0, 128]], compare_op=ALU.not_equal, fill=1.0, base=0, channel_multiplier=1),
                 reads=[bsel], writes=[bsel])
            for d in range(2):
                with nc.allow_non_contiguous_dma(reason="small params"):
                    c.dma('sp', lambda e: e.dma_start(out=pc[:, 0:1], in_=dt_bias[d, :].rearrange("(h o) -> h o", o=1)), reads=[bpc], writes=[bpc])
                    c.dma('sp', lambda e: e.dma_start(out=pc[:, 1:2], in_=a_log[d, :].rearrange("(h o) -> h o", o=1)), reads=[bpc], writes=[bpc])
                c.op('act', lambda e: e.activation(out=pc[:, 2:3], in_=pc[:, 1:2], func=AF.Exp), reads=[bpc], writes=[bpc])
                c.op('dve', lambda e: e.tensor_scalar(out=pc[:, 2:3], in0=pc[:, 2:3], scalar1=-1.0, scalar2=None, op0=ALU.mult), reads=[bpc], writes=[bpc])
                for tp in range(L // GP):
                    nbp = tp if d == 0 else L // GP - 1 - tp
                    c.dma('sp', lambda e: e.dma_start(out=ar[:], in_=PT[1536 + 4 * d:1540 + 4 * d, nbp * GP:(nbp + 1) * GP]), reads=[bPT, bar_], writes=[bar_])
                    c.dma('act', lambda e: e.dma_start(out=br[:], in_=PT[1544 + 4 * d:1548 + 4 * d, nbp * GP:(nbp + 1) * GP]), reads=[bPT, bbr], writes=[bbr])
                    asrc = ar[:, ::-1] if d else ar[:, :]
                    bsrc = br[:, ::-1] if d else br[:, :]
                    c.op('dve', lambda e: e.tensor_scalar(out=w1[:], in0=asrc, scalar1=pc[:, 0:1], scalar2=None, op0=ALU.add), reads=[bar_, bpc], writes=[bw1])
                    c.op('dve', lambda e: e.tensor_scalar(out=w2[:], in0=w1[:], scalar1=-1.0, scalar2=None, op0=ALU.mult), reads=[bw1], writes=[bw2])
                    c.op('dve', lambda e: e.tensor_tensor(out=w2[:], in0=w2[:], in1=w1[:], op=ALU.min), reads=[bw1, bw2], writes=[bw2])
                    c.op('act', lambda e: e.activation(out=w2[:], in_=w2[:], func=AF.Exp), reads=[bw2], writes=[bw2])
                    c.op('act', lambda e: e.activation(out=w2[:], in_=w2[:], func=AF.Ln, bias=1.0, scale=1.0), reads=[bw2], writes=[bw2])
                    c.op('dve', lambda e: e.scalar_tensor_tensor(out=w1[:], in0=w1[:], scalar=0.0, in1=w2[:], op0=ALU.max, op1=ALU.add), reads=[bw1, bw2], writes=[bw1])
                    c.op('dve', lambda e: e.tensor_scalar(out=w1[:], in0=w1[:], scalar1=pc[:, 2:3], scalar2=None, op0=ALU.mult), reads=[bw1, bpc], writes=[bw1])
                    c.op('dve', lambda e: e.tensor_tensor_scan(out=gam[:], data0=msk[:], data1=w1[:], initial=0.0, op0=ALU.mult, op1=ALU.add), reads=[bmsk, bw1, brw], writes=[brw])
                    c.op('act', lambda e: e.activation(out=bet[:], in_=bsrc, func=AF.Sigmoid), reads=[bbr, brw], writes=[brw])
                    c.op('act', lambda e: e.activation(out=egam[:], in_=gam[:], func=AF.Exp), reads=[brw], writes=[brw])
                    c.op('dve', lambda e: e.tensor_tensor(out=q3[:], in0=bet[:], in1=egam[:], op=ALU.mult), reads=[brw, bq3], writes=[bq3])
                    g3 = gam[:].rearrange("p (c j) -> p c j", j=64)
                    c.op('dve', lambda e: e.tensor_tensor(out=q4[:].rearrange("p (c j) -> p c j", j=64), in0=g3[:, :, 63:64].broadcast_to([4, GP // 64, 64]), in1=g3, op=ALU.subtract),
                         reads=[brw, bq4], writes=[bq4])
                    c.op('act', lambda e: e.activation(out=q4[:], in_=q4[:], func=AF.Exp), reads=[bq4], writes=[bq4])
                    c.op('dve', lambda e: e.tensor_scalar(out=q5[:], in0=gam[:], scalar1=-1.0, scalar2=None, op0=ALU.mult), reads=[brw, bq5], writes=[bq5])
                    quants = [(gam, brw), (bet, brw), (q3, bq3), (q4, bq4), (q5, bq5)]
                    for bl in range(GP // 128):
                        blk = tp * (GP // 128) + bl
                        pc_, bpc_ = g.ps[blk % 2]
                        for qi, (qt_, bq_) in enumerate(quants):
                            c.op('pe', lambda e: e.transpose(out=pc_[:, qi * 4:(qi + 1) * 4], in_=qt_[0:4, bl * 128:(bl + 1) * 128], identity=g.ident32[0:4, 0:4]),
                                 reads=[bq_, g.b_ident32], writes=[bpc_])
                        c.op('act', lambda e: e.activation(out=cols[d][:, blk, :], in_=pc_[:, 0:4 * NQ], func=AF.Copy), reads=[bpc_, bcols[d]], writes=[bcols[d]])
                    for qi, rt in enumerate((gam, bet, egam)):
                        c.dma('sp', lambda e: e.dma_start(out=GR[d, qi, :, tp * GP:(tp + 1) * GP], in_=rt[:]), reads=[brw], pwrites=[bGR])
            bGR.seal()
            barrier(c)
        if stage < 3:
            return
        with ExitStack() as es:
            def T(name, shape, dt):
                return es.enter_context(nc.sbuf_tensor(uniq(name), shape, dt))
            nm_le = T("gm_nmle", [128, 128], F32)
            nm_geT = T("gm_nmgeT", [128, 128], F32)
            m_stT = T("gm_mstT", [128, 128], F32)
            bmk = Buf()
            c.op('pool', lambda e: e.memset(nm_le[:], 0.0), writes=[bmk])
            c.op('pool', lambda e: e.affine_select(out=nm_le[:], in_=nm_le[:], pattern=[[-1, 128]], compare_op=ALU.is_gt, fill=-30000.0, base=0, channel_multiplier=1), reads=[bmk], writes=[bmk])
            c.op('pool', lambda e: e.memset(nm_le[64:128, 0:64], -30000.0), reads=[bmk], writes=[bmk])
            c.op('pool', lambda e: e.memset(nm_geT[:], 0.0), reads=[bmk], writes=[bmk])
            c.op('pool', lambda e: e.affine_select(out=nm_geT[:], in_=nm_geT[:], pattern=[[1, 128]], compare_op=ALU.is_ge, fill=-30000.0, base=0, channel_multiplier=-1), reads=[bmk], writes=[bmk])
            c.op('pool', lambda e: e.memset(nm_geT[0:64, 64:128], -30000.0), reads=[bmk], writes=[bmk])
            c.op('pool', lambda e: e.memset(m_stT[:], 1.0), reads=[bmk], writes=[bmk])
            c.op('pool', lambda e: e.affine_select(out=m_stT[:], in_=m_stT[:], pattern=[[1, 128]], compare_op=ALU.is_gt, fill=0.0, base=0, channel_multiplier=-1), reads=[bmk], writes=[bmk])

            class CH:
                pass
            chs = []
            for d in range(2):
                ch = CH(); chs.append(ch)
                ch.d = d

                def TT(name, shape, dt, d=d):
                    return (T("gm%d_%s" % (d, name), shape, dt), Buf())
                ch.nat = [TT("nat%d" % i, [128, GTB], BF16) for i in range(3)]
                ch.arr = [[TT("arr%d_%d" % (i, j), [128, GTB], BF16) for j in range(2)] for i in range(3)]
                ch.rts = [[TT("rt%d_%d" % (q, j), [4, GTB], F32) for j in range(2)] for q in range(3)]
                ch.S32 = TT("S32", [128, 128], F32); ch.Sb = TT("Sb", [128, 128], BF16)
                ch.tmpD = TT("tmpD", [128, 128], F32); ch.Dst = TT("Dst", [128, 128], F32); ch.DTi = TT("DTi", [128, 128], F32); ch.DTs = TT("DTs", [128, 128], F32)
                ch.A_ = TT("A", [128, 128], BF16); ch.AT_ = TT("AT", [128, 128], BF16); ch.atT = TT("attnT", [128, 128], BF16)
                ch.Pm = [TT("P%d" % i, [128, 128], BF16) for i in range(6)]
                ch.Qm = [TT("Q%d" % i, [128, 128], BF16) for i in range(5)]
                ch.W32 = TT("W32", [128, 256], F32); ch.Wb = TT("Wb", [128, 256], BF16)
                ch.kdec = TT("kdec", [128, 128], BF16); ch.kcT = TT("kcT", [128, 128], BF16); ch.qdec = TT("qdec", [128, 128], BF16); ch.vnew = TT("vnew", [128, 128], BF16)
                ch.elc = TT("elc", [128, 2], F32)
                ch.osb = [TT("osb%d" % i, [128, 128], F32) for i in range(2)]
                ch.osf = [TT("osf%d" % i, [128, 128], F32) for i in range(2)]
                ch.bA = g.ps[3 * d + 0]; ch.bB = g.ps[3 * d + 1]; ch.bC = g.ps[3 * d + 2]
                ch.pb0 = 512 * d
                ch.nblk = 0

            def block_gen(ch, h, blk, b, cur, rcur):
                d = ch.d
                (qA, bqA), (kA, bkA), (vA, bvA) = cur
                bs_ = slice(b * 128, (b + 1) * 128)
                cl = cols[d][:, blk, :]
                gcol = cl[:, 0 + h:0 + h + 1]; bcol = cl[:, 4 + h:4 + h + 1]; begcol = cl[:, 8 + h:8 + h + 1]
                ekdcol = cl[:, 12 + h:12 + h + 1]; ngcol = cl[:, 16 + h:16 + h + 1]
                pA, bpA = ch.bA; pB, bpB = ch.bB; pC, bpC = ch.bC
                pb0 = ch.pb0
                tmpD, btmpD = ch.tmpD; Dst, bDst = ch.Dst; DTi, bDTi = ch.DTi; DTs, bDTs = ch.DTs
                A_, bA_ = ch.A_; AT_, bAT_ = ch.AT_; atT, batT = ch.atT
                W32, bW32 = ch.W32; Wb, bWb = ch.Wb; kdec, bkdec = ch.kdec; kcT, bkcT = ch.kcT; qdec, bqdec = ch.qdec; vnew, bvnew = ch.vnew
                elc, belc = ch.elc; S32, bS32 = ch.S32; Sb, bSb = ch.Sb
                for qi, (rt, brt) in enumerate(rcur):
                    c.op('pe', lambda e: e.matmul(pA[:, qi * 128:(qi + 1) * 128], lhsT=sel[:, h, :], rhs=rt[0:4, bs_], start=True, stop=True, skip_group_check=True),
                         reads=[bsel, brt], writes=[bpA])
                    yield
                c.op('pe', lambda e: e.matmul(pB[:, 0:128], lhsT=kA[:, bs_], rhs=kA[:, bs_], start=True, stop=True, skip_group_check=True), reads=[bkA], writes=[bpB]); yield
                c.op('pe', lambda e: e.matmul(pB[:, 128:256], lhsT=kA[:, bs_], rhs=qA[:, bs_], start=True, stop=True, skip_group_check=True), reads=[bkA, bqA], writes=[bpB]); yield
                c.op('pe', lambda e: e.transpose(out=ptb[:, pb0:pb0 + 128], in_=vA[:, bs_], identity=g.identb[:]), reads=[bvA, g.b_identb], writes=[bptb]); yield
                c.op('pe', lambda e: e.transpose(out=ptb[:, pb0 + 128:pb0 + 256], in_=kA[:, bs_], identity=g.identb[:]), reads=[bkA, g.b_identb], writes=[bptb]); yield
                c.op('dve', lambda e: e.scalar_tensor_tensor(out=tmpD[:], in0=pA[:, 0:128], scalar=-1.0, in1=nm_le[:], op0=ALU.mult, op1=ALU.add), reads=[bpA, bmk], writes=[btmpD]); yield
                c.op('act', lambda e: e.activation(out=Dst[:], in_=tmpD[:], func=AF.Exp, bias=gcol, scale=1.0), reads=[btmpD, bcols[d]], writes=[bDst]); yield
                c.op('dve', lambda e: e.tensor_tensor(out=tmpD[:], in0=pA[:, 0:128], in1=nm_geT[:], op=ALU.add), reads=[bpA, bmk, btmpD], writes=[btmpD]); yield
                c.op('act', lambda e: e.activation(out=DTi[:], in_=tmpD[:], func=AF.Exp, bias=ngcol, scale=1.0), reads=[btmpD, bcols[d]], writes=[bDTi]); yield
                c.op('pool', lambda e: e.tensor_tensor(out=DTs[:], in0=DTi[:], in1=m_stT[:], op=ALU.mult), reads=[bDTi, bmk], writes=[bDTs]); yield
                c.op('dve', lambda e: e.tensor_tensor(out=DTs[:], in0=pA[:, 128:256], in1=DTs[:], op=ALU.mult), reads=[bpA, bDTs], writes=[bDTs]); yield
                c.op('dve', lambda e: e.scalar_tensor_tensor(out=A_[:], in0=pB[:, 0:128], scalar=bcol, in1=Dst[:], op0=ALU.mult, op1=ALU.mult), reads=[bpB, bcols[d], bDst], writes=[bA_]); yield
                c.op('dve', lambda e: e.tensor_tensor(out=AT_[:], in0=pB[:, 0:128], in1=DTs[:], op=ALU.mult), reads=[bpB, bDTs], writes=[bAT_]); yield
                c.op('dve', lambda e: e.tensor_tensor(out=atT[:], in0=pB[:, 128:256], in1=DTi[:], op=ALU.mult), reads=[bpB, bDTi], writes=[batT]); yield
                c.op('dve', lambda e: e.tensor_scalar(out=W32[:, 0:128], in0=ptb[:, pb0:pb0 + 128], scalar1=bcol, scalar2=None, op0=ALU.mult), reads=[bptb, bcols[d], bW32], writes=[bW32]); yield
                c.op('dve', lambda e: e.tensor_scalar(out=W32[:, 128:256], in0=ptb[:, pb0 + 128:pb0 + 256], scalar1=begcol, scalar2=None, op0=ALU.mult), reads=[bptb, bcols[d], bW32], writes=[bW32]); yield
                c.op('act', lambda e: e.activation(out=kdec[:], in_=ptb[:, pb0 + 128:pb0 + 256], func=AF.Copy, scale=ekdcol), reads=[bptb, bcols[d]], writes=[bkdec]); yield
                c.op('act', lambda e: e.activation(out=Wb[:], in_=W32[:], func=AF.Copy), reads=[bW32], writes=[bWb]); yield
                c.op('dve', lambda e: e.tensor_tensor(out=qdec[:], in0=pA[:, 256:384], in1=qA[:, bs_], op=ALU.mult), reads=[bpA, bqA], writes=[bqdec]); yield
                c.op('act', lambda e: e.activation(out=elc[:], in_=pA[:, 256:384].rearrange("p (c j) -> p c j", j=64)[:, :, 63], func=AF.Copy), reads=[bpA], writes=[belc]); yield
                Pc, bPc = AT_, bAT_
                Qc, bQc = A_, bA_
                for lev in range(6):
                    c.op('pe', lambda e: e.matmul(pC[:, 0:256], lhsT=Pc[:], rhs=Wb[:], start=True, stop=True, skip_group_check=True), reads=[bPc, bWb], writes=[bpC]); yield
                    c.op('dve', lambda e: e.tensor_tensor(out=W32[:], in0=W32[:], in1=pC[:, 0:256], op=(ALU.subtract if lev == 0 else ALU.add)), reads=[bW32, bpC], writes=[bW32]); yield
                    c.op('act', lambda e: e.activation(out=Wb[:], in_=W32[:], func=AF.Copy), reads=[bW32], writes=[bWb]); yield
                    if lev < 5:
                        Pn, bPn = ch.Pm[lev + 1]
                        c.op('pe', lambda e: e.matmul(pB[:, 256:384], lhsT=Qc[:], rhs=Pc[:], start=True, stop=True, skip_group_check=True), reads=[bQc, bPc], writes=[bpB]); yield
                        if lev < 4:
                            Qn, bQn = ch.Qm[lev + 1]
                            c.op('pe', lambda e: e.matmul(pB[:, 384:512], lhsT=Pc[:], rhs=Qc[:], start=True, stop=True, skip_group_check=True), reads=[bQc, bPc], writes=[bpB]); yield
                            c.op('dve', lambda e: e.tensor_copy(out=Qn[:], in_=pB[:, 384:512]), reads=[bpB], writes=[bQn]); yield
                        c.op('act', lambda e: e.activation(out=Pn[:], in_=pB[:, 256:384], func=AF.Copy), reads=[bpB], writes=[bPn]); yield
                        Pc, bPc = Pn, bPn
                        if lev < 4:
                            Qc, bQc = Qn, bQn
                c.op('pe', lambda e: e.transpose(out=ptb[:, pb0 + 256:pb0 + 384], in_=Wb[:, 128:256], identity=g.identb[:]), reads=[bWb, g.b_identb], writes=[bptb]); yield
                c.op('act', lambda e: e.activation(out=kcT[:], in_=ptb[:, pb0 + 256:pb0 + 384], func=AF.Copy), reads=[bptb], writes=[bkcT]); yield
                for ci in range(2):
                    r0 = 64 * ci
                    c.op('pe', lambda e: e.matmul(pC[r0:r0 + 64, 256:384], lhsT=kcT[:, r0:r0 + 64], rhs=Sb[:, :], start=True, stop=True, skip_group_check=True), reads=[bkcT, bSb], writes=[bpC]); yield
                    c.op('dve', lambda e: e.tensor_tensor(out=vnew[r0:r0 + 64, :], in0=W32[r0:r0 + 64, 0:128], in1=pC[r0:r0 + 64, 256:384], op=ALU.subtract), reads=[bW32, bpC, bvnew], writes=[bvnew]); yield
                    c.op('pe', lambda e: e.matmul(pA[r0:r0 + 64, 384:512], lhsT=qdec[:, r0:r0 + 64], rhs=Sb[:, :], start=True, stop=False, skip_group_check=True), reads=[bqdec, bSb], writes=[bpA])
                    c.op('pe', lambda e: e.matmul(pA[r0:r0 + 64, 384:512], lhsT=atT[r0:r0 + 64, r0:r0 + 64], rhs=vnew[r0:r0 + 64, :], start=False, stop=True, skip_group_check=True), reads=[batT, bvnew], writes=[bpA]); yield
                    c.op('pe', lambda e: e.matmul(pC[:, 384:512], lhsT=kdec[r0:r0 + 64, :], rhs=vnew[r0:r0 + 64, :], start=True, stop=True, skip_group_check=True), reads=[bkdec, bvnew], writes=[bpC]); yield
                    c.op('dve', lambda e: e.scalar_tensor_tensor(out=S32[:], in0=S32[:], scalar=elc[:, ci:ci + 1], in1=pC[:, 384:512], op0=ALU.mult, op1=ALU.add), reads=[bS32, belc, bpC], writes=[bS32]); yield
                    c.op('act', lambda e: e.activation(out=Sb[:], in_=S32[:], func=AF.Copy), reads=[bS32], writes=[bSb]); yield
                os_, bos_ = ch.osb[ch.nblk % 2]
                of_, bof_ = ch.osf[ch.nblk % 2]
                ch.nblk += 1
                c.op('act', lambda e: e.activation(out=os_[:], in_=pA[:, 384:512], func=AF.Copy), reads=[bpA], writes=[bos_]); yield
                if d == 0:
                    c.dma('sp', lambda e: e.dma_start(out=OD[blk * 128:(blk + 1) * 128, h * 128:(h + 1) * 128], in_=os_[:]), reads=[bos_], pwrites=[bODs[h]]); yield
                else:
                    pf, bpf = g.ps[6]
                    c.op('pe', lambda e: e.matmul(pf[:, 0:128], lhsT=g.J32[:], rhs=os_[:], start=True, stop=True), reads=[g.b_J32, bos_], writes=[bpf]); yield
                    c.op('act', lambda e: e.activation(out=of_[:], in_=pf[:, 0:128], func=AF.Copy), reads=[bpf], writes=[bof_]); yield
                    c.dma('act', lambda e: e.dma_start(out=OD2[L - (blk + 1) * 128:L - blk * 128, h * 128:(h + 1) * 128], in_=of_[:]), reads=[bof_], pwrites=[bOD2s[h]]); yield

            for h in range(4):
                for ch in chs:
                    S32, bS32 = ch.S32; Sb, bSb = ch.Sb
                    c.op('pool', lambda e: e.memset(S32[:], 0.0), reads=[bS32], writes=[bS32])
                    c.op('pool', lambda e: e.memset(Sb[:], 0.0), reads=[bSb], writes=[bSb])
                for tb in range(L // GTB):
                    curs = []; rcurs = []
                    for ch in chs:
                        d = ch.d
                        cur = []
                        for ai in range(3):
                            a_, ba_ = ch.arr[ai][tb % 2]
                            if d == 0:
                                c.dma('sp' if ai % 2 else 'act', lambda e: e.dma_start(out=a_[:], in_=GQ[ai * 4 + h, :, tb * GTB:(tb + 1) * GTB]), reads=[bGQ], writes=[ba_])
                            else:
                                n_, bn_ = ch.nat[ai]
                                c.dma('sp' if ai % 2 else 'act', lambda e: e.dma_start(out=n_[:], in_=GQ[ai * 4 + h, :, L - (tb + 1) * GTB:L - tb * GTB]), reads=[bGQ], writes=[bn_])
                                c.op('pool', lambda e: e.tensor_copy(out=a_[:], in_=n_[:, ::-1]), reads=[bn_], writes=[ba_])
                            cur.append((a_, ba_))
                        rcur = []
                        for qi in range(3):
                            r_, br_ = ch.rts[qi][tb % 2]
                            c.dma('sp', lambda e: e.dma_start(out=r_[:], in_=GR[d, qi, :, tb * GTB:(tb + 1) * GTB]), reads=[bGR], writes=[br_])
                            rcur.append((r_, br_))
                        curs.append(cur); rcurs.append(rcur)
                    for b in range(GTB // 128):
                        blk = tb * (GTB // 128) + b
                        gens = [block_gen(ch, h, blk, b, curs[i], rcurs[i]) for i, ch in enumerate(chs)]
                        alive = list(gens)
                        while alive:
                            for gnr in list(alive):
                                try:
                                    next(gnr)
                                except StopIteration:
                                    alive.remove(gnr)
                bODs[h].seal(); bOD2s[h].seal()
            barrier(c)
    if stage < 4:
        return
    gated_norm_finalize(c, g, OD, bODs, PV, bPV, 1536, out_gain, MT, bMT, 512, "gf_", OA2=OD2, bOA2s=bOD2s)


N_ACTIVE = 4
DEPTH = 4

PARAM_NAMES = ['mix_norm', 'ffn_norm', 'ev_w_in', 'ev_w_out', 'a_lb_logits', 'a_out_norm', 's5_lambda_re', 's5_lambda_im',
               's5_log_step', 's5_b_re', 's5_b_im', 's5_c_re', 's5_c_im', 's5_d', 's5_glu_w', 's5_glu_b', 'od_w_in', 'od_w_out',
               'c_q_norm', 'c_k_norm', 'c_lambda', 'c_out_norm', 'rel_bias', 'd_conv_w', 'd_a_log', 'd_dt_bias', 'd_out_norm',
               'moe_router', 'moe_w_gate', 'moe_w_up', 'moe_w_down']

PARAM_SHAPES = {
    'mix_norm': (4, 1024), 'ffn_norm': (4, 1024), 'ev_w_in': (2, 1024, 3072), 'ev_w_out': (2, 1024, 1024), 'a_lb_logits': (2, 2, 512),
    'a_out_norm': (2, 128), 's5_lambda_re': (2, 2, 32, 64), 's5_lambda_im': (2, 2, 32, 64), 's5_log_step': (2, 2, 32),
    's5_b_re': (2, 2, 32, 64, 16), 's5_b_im': (2, 2, 32, 64, 16), 's5_c_re': (2, 2, 32, 16, 64), 's5_c_im': (2, 2, 32, 16, 64),
    's5_d': (2, 512), 's5_glu_w': (2, 512, 512), 's5_glu_b': (2, 512), 'od_w_in': (2, 1024, 3600), 'od_w_out': (2, 1024, 1024),
    'c_q_norm': (2, 64), 'c_k_norm': (2, 64), 'c_lambda': (2, 4, 64), 'c_out_norm': (2, 128), 'rel_bias': (32, 4),
    'd_conv_w': (2, 5, 1536), 'd_a_log': (2, 2, 4), 'd_dt_bias': (2, 2, 4), 'd_out_norm': (2, 128), 'moe_router': (4, 1024, 16),
    'moe_w_gate': (4, 16, 1024, 2048), 'moe_w_up': (4, 16, 1024, 2048), 'moe_w_down': (4, 16, 2048, 1024)}


def build_program(layers=range(DEPTH), do_mixer=True, do_moe=True):
    nc = bass.Bass('TRN2', target_bir_lowering=False)
    xin = nc.dram_tensor("x", [L, D], F32, kind="ExternalInput").ap()
    P = {n: nc.dram_tensor(n, list(PARAM_SHAPES[n]), F32, kind="ExternalInput").ap() for n in PARAM_NAMES}
    onehot = nc.dram_tensor("t5_onehot", [32, 512], F32, kind="ExternalInput").ap()
    X = nc.dram_tensor("y", [L, D], F32, kind="ExternalOutput").ap()
    PT = nc.dram_tensor("PT", [2048, L], F32).ap()
    PV = nc.dram_tensor("PV", [L, 2048], BF16).ap()
    QK = nc.dram_tensor("QK", [16, 128, L], BF16).ap()
    QK5 = QK.rearrange("(h r w) p t -> h r w p t", h=4, r=2)
    OA = nc.dram_tensor("OA", [L, 512], F32).ap()
    YT = nc.dram_tensor("YT", [512, L], F32).ap()
    MT = nc.dram_tensor("MT", [1024, L], BF16).ap()
    HB = nc.dram_tensor("HB", [L, D], BF16).ap()
    GQ = nc.dram_tensor("GQ", [12, 128, L], BF16).ap()
    GR = nc.dram_tensor("GR", [2, 3, 4, L], F32).ap()
    OD2 = nc.dram_tensor("OD2", [L, 512], F32).ap()
    FV = nc.dram_tensor("FV", [4, 512], F32).ap()
    c = Ctx(nc); g = G()
    setup_consts(c, g)
    bX = Buf('X'); bPT = Buf(); bPV = Buf(); bQK = Buf(); bOAs = [Buf() for _ in range(4)]; bYT = Buf(); bMT = Buf()
    bHB = Buf(); bGQ = Buf(); bGR = Buf(); bFV = Buf(); bOD2s = [Buf() for _ in range(4)]
    for r in range(0, L, 512):
        c.dma('sp', lambda e: e.dma_start(out=X[r:r + 512, :], in_=xin[r:r + 512, :]), pwrites=[bX])
    bX.seal()
    for layer in layers:
        j = layer // 2
        if do_mixer:
            if layer % 2 == 0:
                spec = [(0, 512, 'F', 0), (512, 512, 'F', 512), (1024, 512, 'F', 1024), (2560, 512, 'F', 1536), (1536, 512, 'T', 0), (2048, 512, 'T', 512)]
                proj_phase(c, g, X, bX, P['mix_norm'][layer], P['ev_w_in'][j], 3072, spec, PT, bPT, PV, bPV)
                hgrn2_phase(c, g, PT, bPT, PV, bPV, P['a_lb_logits'], j, P['a_out_norm'][j], QK5, bQK, OA, bOAs, MT, bMT)
                s5_phase(c, g, PT, bPT, P['s5_lambda_re'][j], P['s5_lambda_im'][j], P['s5_log_step'][j], P['s5_b_re'][j], P['s5_b_im'][j],
                         P['s5_c_re'][j], P['s5_c_im'][j], P['s5_d'][j], P['s5_glu_w'][j], P['s5_glu_b'][j], YT, bYT, MT, bMT)
                bMT.seal()
                bX = outproj_phase(c, g, MT, bMT, P['ev_w_out'][j], X, bX)
            else:
                spec = [(0, 512, 'T', 0), (512, 512, 'T', 512), (1024, 512, 'T', 1024), (1536, 1536, 'F', 0), (3072, 16, 'F', 1536), (3088, 512, 'T', 1536)]
                proj_phase(c, g, X, bX, P['mix_norm'][layer], P['od_w_in'][j], 3600, spec, PT, bPT, PV, bPV)
                attn_phase(c, g, PV, bPV, P['c_q_norm'][j], P['c_k_norm'][j], P['c_lambda'][j], P['c_out_norm'][j], P['rel_bias'], onehot, layer,
                           QK, bQK, FV, bFV, MT, bMT)
                gdn_phase(c, g, PT, bPT, PV, bPV, P['d_conv_w'][j], P['d_a_log'][j], P['d_dt_bias'][j], P['d_out_norm'][j], GQ, bGQ, GR, bGR, OA, bOAs, OD2, bOD2s, MT, bMT)
                bMT.seal()
                bX = outproj_phase(c, g, MT, bMT, P['od_w_out'][j], X, bX)
        if do_moe:
            moe_layer(c, g, X, bX, HB, bHB, P['ffn_norm'][layer], P['moe_router'][layer], P['moe_w_gate'][layer], P['moe_w_up'][layer], P['moe_w_down'][layer])
    barrier(c)
    c.finish([bX])
    return nc, c


def kernel(**inputs):
    x = np.ascontiguousarray(np.asarray(inputs['x'], dtype=np.float32))
    B = x.shape[0]
    assert B == N_ACTIVE and x.shape[1] == L and x.shape[2] == D
    nc, c = build_program()
    params = {n: np.ascontiguousarray(np.asarray(inputs[n], dtype=np.float32)) for n in PARAM_NAMES}
    oh = t5_onehot()
    in_maps = []
    for b in range(N_ACTIVE):
        m = {"x": x[b], "t5_onehot": oh}
        m.update(params)
        in_maps.append(m)
    res = run_bass_kernel_spmd(nc, in_maps, core_ids=list(range(N_ACTIVE)))
    out = np.stack([np.asarray(res.results[b]["y"], dtype=np.float32) for b in range(N_ACTIVE)], axis=0)
    return out
```

```python
import math
from contextlib import ExitStack

import numpy as np
import concourse.bass as bass
import concourse.mybir as mybir
from concourse.bass_utils import run_bass_kernel_spmd

F32 = mybir.dt.float32
BF16 = mybir.dt.bfloat16
U32 = mybir.dt.uint32
I32 = mybir.dt.int32
AF = mybir.ActivationFunctionType
ALU = mybir.AluOpType
AX = mybir.AxisListType


class Buf:
    __slots__ = ("name", "w", "r", "pw", "psum")

    def __init__(self, name="", psum=False):
        self.name = name
        self.psum = psum
        self.w = {}
        self.r = {}
        self.pw = {}

    def seal(self):
        for k, v in self.pw.items():
            if self.w.get(k, 0) < v:
                self.w[k] = v
        self.pw = {}


class Ctx:
    NDMA = 8

    def __init__(self, nc, same_engine_sync=True):
        self.nc = nc
        self.E = dict(pe=nc.tensor, dve=nc.vector, act=nc.scalar, pool=nc.gpsimd, sp=nc.sync)
        self.sem = {}
        self.cnt = {}
        for k in self.E:
            self.sem[k] = nc.alloc_semaphore("c_" + k)
            self.cnt[k] = 0
        self.dslot = {}
        for q in ("sp", "act", "pool"):
            for i in range(self.NDMA):
                key = "d_%s%d" % (q, i)
                self.sem[key] = nc.alloc_semaphore(key)
                self.cnt[key] = 0
            self.dslot[q] = 0
        self.seen = {k: {} for k in self.E}
        self.same = same_engine_sync
        self.ninst = 0

    def _wait(self, eng, tok):
        if tok is None:
            return
        key, val = tok
        if key == eng and (eng == "pe" or not self.same):
            return
        if self.seen[eng].get(key, 0) >= val:
            return
        self.E[eng].wait_ge(self.sem[key], val)
        self.seen[eng][key] = val

    def _deps(self, eng, reads, writes, pwrites=()):
        for b in reads:
            for k, v in b.w.items():
                self._wait(eng, (k, v))
            for k, v in b.pw.items():
                self._wait(eng, (k, v))
            if b.psum:
                for k, v in b.r.items():
                    if k != eng:
                        self._wait(eng, (k, v))
        for b in writes:
            for d in (b.w, b.pw, b.r):
                for k, v in d.items():
                    self._wait(eng, (k, v))
        for b in pwrites:
            for d in (b.w, b.r):
                for k, v in d.items():
                    self._wait(eng, (k, v))

    def _commit(self, tok, reads, writes, pwrites=()):
        k, v = tok
        for b in writes:
            b.w = {k: v}
            b.pw = {}
            b.r = {}
        for b in pwrites:
            if b.pw.get(k, 0) < v:
                b.pw[k] = v
        for b in reads:
            if b.r.get(k, 0) < v:
                b.r[k] = v

    def op(self, eng, fn, reads=(), writes=(), pwrites=()):
        self._deps(eng, reads, writes, pwrites)
        inst = fn(self.E[eng])
        self.cnt[eng] += 1
        tok = (eng, self.cnt[eng])
        inst.then_inc(self.sem[eng], 1)
        self._commit(tok, reads, writes, pwrites)
        self.ninst += 1
        return tok

    def dma(self, q, fn, reads=(), writes=(), pwrites=()):
        self._deps(q, reads, writes, pwrites)
        i = self.dslot[q]
        self.dslot[q] = (i + 1) % self.NDMA
        key = "d_%s%d" % (q, i)
        if self.cnt[key] > 0:
            self._wait(q, (key, self.cnt[key]))
        inst = fn(self.E[q])
        self.cnt[key] += 16
        tok = (key, self.cnt[key])
        inst.then_inc(self.sem[key], 16)
        self._commit(tok, reads, writes, pwrites)
        self.ninst += 1
        return tok

    def finish(self, bufs):
        for b in bufs:
            for d in (b.w, b.pw):
                for k, v in d.items():
                    self._wait("sp", (k, v))


def barrier(c):
    toks = [(k, v) for k, v in c.cnt.items() if v > 0]
    for eng in c.E:
        for tok in toks:
            c._wait(eng, tok)


_UNIQ = [0]


def uniq(name):
    _UNIQ[0] += 1
    return "%s_u%d" % (name, _UNIQ[0])


L = 8192
D = 1024
NE = 16
FF = 2048
CAP = 1024
NT = L // 128


class G:
    pass


def alloc_T(nc, name, shape, dtype, n=1, es=None):
    if es is None:
        return [(nc.alloc_sbuf_tensor("%s_%d" % (name, i), shape, dtype), Buf(name)) for i in range(n)]
    return [(es.enter_context(nc.sbuf_tensor(uniq("%s_%d" % (name, i)), shape, dtype)), Buf(name)) for i in range(n)]


def setup_consts(c, g):
    nc = c.nc
    g.ident32 = nc.alloc_sbuf_tensor("ident32", [128, 128], F32); g.b_ident32 = Buf()
    g.identb = nc.alloc_sbuf_tensor("identb", [128, 128], BF16); g.b_identb = Buf()
    g.ones32 = nc.alloc_sbuf_tensor("ones32", [1, 128], F32); g.b_ones32 = Buf()
    g.neghalf = nc.alloc_sbuf_tensor("neghalf", [128, 1], F32); g.b_neghalf = Buf()
    for t, b in ((g.ident32, g.b_ident32), (g.identb, g.b_identb)):
        c.op('pool', lambda e: e.memset(t[:], 0.0), writes=[b])
        c.op('pool', lambda e: e.affine_select(out=t[:], in_=t[:], pattern=[[-1, 128]], compare_op=ALU.not_equal,
                                               fill=1.0, base=0, channel_multiplier=1), reads=[b], writes=[b])
    g.J32 = nc.alloc_sbuf_tensor("J32", [128, 128], F32); g.b_J32 = Buf()
    g.Jb = nc.alloc_sbuf_tensor("Jb", [128, 128], BF16); g.b_Jb = Buf()
    for t, b in ((g.J32, g.b_J32), (g.Jb, g.b_Jb)):
        c.op('pool', lambda e: e.memset(t[:], 0.0), writes=[b])
        c.op('pool', lambda e: e.affine_select(out=t[:], in_=t[:], pattern=[[1, 128]], compare_op=ALU.not_equal,
                                               fill=1.0, base=-127, channel_multiplier=1), reads=[b], writes=[b])
    c.op('pool', lambda e: e.memset(g.ones32[:], 1.0), writes=[g.b_ones32])
    c.op('pool', lambda e: e.memset(g.neghalf[:], -0.5), writes=[g.b_neghalf])
    g.ps = []
    for i in range(7):
        g.ps.append((nc.alloc_psum_tensor("ps%d" % i, [128, 512], F32), Buf("ps%d" % i, psum=True)))
    g.psb = (nc.alloc_psum_tensor("psb", [128, 1024], BF16), Buf("psb", psum=True))


def bcast_row(c, g, dst, bdst, src_ap, n, tmp, btmp, psi=6):
    c.dma('sp', lambda e: e.dma_start(out=tmp[0:1, 0:n], in_=src_ap.rearrange("(o n) -> o n", o=1)), writes=[btmp])
    ps, bps = g.ps[psi]
    for h in range(0, n, 512):
        w = min(512, n - h)
        c.op('pe', lambda e: e.matmul(ps[:, 0:w], lhsT=g.ones32[0:1, :], rhs=tmp[0:1, h:h + w], start=True, stop=True),
             reads=[g.b_ones32, btmp], writes=[bps])
        c.op('dve', lambda e: e.tensor_copy(out=dst[:, h:h + w], in_=ps[:, 0:w]), reads=[bps], writes=[bdst])


def rmsnorm_tile(c, g, xt, bxt, gB, bgB, h32, bh32, junk, bjunk, ss, bss):
    c.op('dve', lambda e: e.scalar_tensor_tensor(out=junk[:], in0=xt[:], scalar=1.0, in1=xt[:], op0=ALU.mult, op1=ALU.mult,
                                                 accum_out=ss[:, 0:1]), reads=[bxt], writes=[bjunk, bss])
    c.op('dve', lambda e: e.tensor_scalar(out=ss[:, 0:1], in0=ss[:, 0:1], scalar1=1.0 / D, scalar2=1e-6, op0=ALU.mult, op1=ALU.add),
         reads=[bss], writes=[bss])
    c.op('act', lambda e: e.activation(out=ss[:, 0:1], in_=ss[:, 0:1], func=AF.Ln), reads=[bss], writes=[bss])
    c.op('act', lambda e: e.activation(out=ss[:, 0:1], in_=ss[:, 0:1], func=AF.Exp, scale=-0.5), reads=[bss], writes=[bss])
    c.op('dve', lambda e: e.scalar_tensor_tensor(out=h32[:], in0=xt[:], scalar=ss[:, 0:1], in1=gB[:], op0=ALU.mult, op1=ALU.mult),
         reads=[bxt, bss, bgB], writes=[bh32])


def moe_phase(c, g, X, bX, HB, bHB, ffn_g, w_router, w_gate, w_up, w_down, sb, stage=3):
    nc = c.nc
    gB, bgB = sb['gB']
    tmpr, btmpr = sb['tmprow']
    bcast_row(c, g, gB, bgB, ffn_g, D, tmpr, btmpr)
    wr, bwr = sb['wr']
    c.dma('sp', lambda e: e.dma_start(out=wr[:], in_=w_router.rearrange("(k p) e -> p k e", p=128)), writes=[bwr])
    affT, baffT = sb['affT']
    for i in range(NT):
        xt, bxt = sb['xt'][i % 2]
        h32, bh32 = sb['h32'][i % 2]
        hb, bhb = sb['hb'][i % 2]
        junk, bjunk = sb['junk']
        ss, bss = sb['ss'][i % 2]
        c.dma('sp', lambda e: e.dma_start(out=xt[:], in_=X[i * 128:(i + 1) * 128, :]), reads=[bX], writes=[bxt])
        rmsnorm_tile(c, g, xt, bxt, gB, bgB, h32, bh32, junk, bjunk, ss, bss)
        c.op('act', lambda e: e.activation(out=hb[:], in_=h32[:], func=AF.Copy), reads=[bh32], writes=[bhb])
        c.dma('act', lambda e: e.dma_start(out=HB[i * 128:(i + 1) * 128, :], in_=hb[:]), reads=[bhb], pwrites=[bHB])
        hT, bhT = sb['hT32'][i % 2]
        for hh in range(2):
            ps, bps = g.ps[hh]
            for k in range(4):
                kk = hh * 4 + k
                c.op('pe', lambda e: e.transpose(out=ps[:, k * 128:(k + 1) * 128], in_=h32[:, kk * 128:(kk + 1) * 128],
                                                 identity=g.ident32[:]), reads=[bh32, g.b_ident32], writes=[bps])
            c.op('act', lambda e: e.activation(out=hT[:, hh * 512:(hh + 1) * 512], in_=ps[:, :], func=AF.Copy),
                 reads=[bps], writes=[bhT])
        pl, bpl = g.ps[2 + (i % 2)]
        for k in range(8):
            c.op('pe', lambda e: e.matmul(pl[:, 0:NE], lhsT=hT[:, k * 128:(k + 1) * 128], rhs=wr[:, k, :], start=(k == 0), stop=(k == 7)),
                 reads=[bhT, bwr], writes=[bpl])
        sm, bsm = sb['sm'][i % 2]
        ex, bex = sb['ex'][i % 2]
        c.op('dve', lambda e: e.tensor_reduce(out=sm[:, 0:1], in_=pl[:, 0:NE], axis=AX.X, op=ALU.max), reads=[bpl], writes=[bsm])
        c.op('dve', lambda e: e.tensor_scalar(out=sm[:, 0:1], in0=sm[:, 0:1], scalar1=-1.0, scalar2=None, op0=ALU.mult), reads=[bsm], writes=[bsm])
        c.op('act', lambda e: e.activation(out=ex[:], in_=pl[:, 0:NE], func=AF.Exp, bias=sm[:, 0:1], scale=1.0, accum_out=sm[:, 1:2]),
             reads=[bpl, bsm], writes=[bex, bsm])
        c.op('dve', lambda e: e.reciprocal(out=sm[:, 2:3], in_=sm[:, 1:2]), reads=[bsm], writes=[bsm])
        c.op('dve', lambda e: e.tensor_scalar(out=ex[:], in0=ex[:], scalar1=sm[:, 2:3], scalar2=None, op0=ALU.mult), reads=[bex, bsm], writes=[bex])
        pt, bpt = g.ps[4 + (i % 2)]
        c.op('pe', lambda e: e.transpose(out=pt[0:NE, 0:128], in_=ex[:, 0:NE], identity=g.ident32[:]), reads=[bex, g.b_ident32], writes=[bpt])
        c.op('act', lambda e: e.activation(out=affT[0:NE, i * 128:(i + 1) * 128], in_=pt[0:NE, 0:128], func=AF.Copy), reads=[bpt], writes=[baffT])
    bHB.seal()
    if stage < 2:
        return
    vals, bvals = sb['vals']
    idxu, bidxu = sb['idxu']
    for it in range(CAP // 8):
        sl = slice(it * 8, it * 8 + 8)
        c.op('dve', lambda e: e.max(out=vals[:, sl], in_=affT[:, :]), reads=[baffT], writes=[bvals])
        c.op('dve', lambda e: e.max_index(out=idxu[:, sl], in_max=vals[:, sl], in_values=affT[:, :]), reads=[baffT, bvals], writes=[bidxu])
        c.op('dve', lambda e: e.match_replace(out=affT[:, :], in_to_replace=vals[:, sl], in_values=affT[:, :], imm_value=-1.0),
             reads=[bvals, baffT], writes=[baffT])
    idxf, bidxf = sb['idxf']
    c.op('dve', lambda e: e.tensor_copy(out=idxf[:], in_=idxu[:]), reads=[bidxu], writes=[bidxf])
    idxT, bidxT = sb['idxT']
    gateT, bgateT = sb['gateT']
    for j in range(8):
        pt, bpt = g.ps[4 + (j % 2)]
        c.op('pe', lambda e: e.transpose(out=pt[:, 0:NE], in_=idxf[0:NE, j * 128:(j + 1) * 128], identity=g.ident32[0:NE, 0:NE]),
             reads=[bidxf, g.b_ident32], writes=[bpt])
        c.op('dve', lambda e: e.tensor_copy(out=idxT[:, j, :], in_=pt[:, 0:NE]), reads=[bpt], writes=[bidxT])
        pt2, bpt2 = g.ps[2 + (j % 2)]
        c.op('pe', lambda e: e.transpose(out=pt2[:, 0:NE], in_=vals[0:NE, j * 128:(j + 1) * 128], identity=g.ident32[0:NE, 0:NE]),
             reads=[bvals, g.b_ident32], writes=[bpt2])
        c.op('act', lambda e: e.activation(out=gateT[:, j, :], in_=pt2[:, 0:NE], func=AF.Copy), reads=[bpt2], writes=[bgateT])
    if stage < 3:
        return
    xsT, bxsT = sb['xsT']
    hidT, bhidT = sb['hidT']
    yacc, byacc = sb['yacc']
    ptb, bptb = g.psb
    qi = 0
    for ex_i in range(NE):
        for j in range(8):
            xs, bxs = sb['xs'][j % 2]
            c.dma('pool', lambda e: e.indirect_dma_start(out=xs[:, :], out_offset=None, in_=HB[:, :],
                                                        in_offset=bass.IndirectOffsetOnAxis(ap=idxT[:, j, ex_i:ex_i + 1], axis=0)),
                  reads=[bHB, bidxT], writes=[bxs])
            for k in range(8):
                c.op('pe', lambda e: e.transpose(out=ptb[:, k * 128:(k + 1) * 128], in_=xs[:, k * 128:(k + 1) * 128], identity=g.identb[:]),
                     reads=[bxs, g.b_identb], writes=[bptb])
            c.op('dve', lambda e: e.tensor_copy(out=xsT[:, :, j * 128:(j + 1) * 128], in_=ptb[:, :].rearrange("p (k s) -> p k s", k=8)),
                 reads=[bptb], writes=[bxsT])
        for q in range(4):
            wg, bwg = sb['wg'][qi % 2]
            wu, bwu = sb['wu'][qi % 2]
            wd, bwd = sb['wd'][qi % 2]
            qi += 1
            f0 = q * 512
            c.dma('pool', lambda e: e.dma_start(out=wg[:], in_=w_gate[ex_i, :, f0:f0 + 512].rearrange("(k p) f -> p k f", p=128)), writes=[bwg])
            c.dma('pool', lambda e: e.dma_start(out=wu[:], in_=w_up[ex_i, :, f0:f0 + 512].rearrange("(k p) f -> p k f", p=128)), writes=[bwu])
            c.dma('pool', lambda e: e.dma_start(out=wd[:], in_=w_down[ex_i, f0:f0 + 512, :].rearrange("(k p) d -> p k d", p=128)), writes=[bwd])
            n = 0
            for fc in range(4):
                for sh in range(2):
                    pg, bpg = g.ps[0 + (n % 2)]
                    pu, bpu = g.ps[2 + (n % 2)]
                    sg, bsg = sb['sg'][n % 2]
                    n += 1
                    for k in range(8):
                        c.op('pe', lambda e: e.matmul(pg[:, :], lhsT=wg[:, k, fc * 128:(fc + 1) * 128], rhs=xsT[:, k, sh * 512:(sh + 1) * 512],
                                                      start=(k == 0), stop=(k == 7)), reads=[bwg, bxsT], writes=[bpg])
                    for k in range(8):
                        c.op('pe', lambda e: e.matmul(pu[:, :], lhsT=wu[:, k, fc * 128:(fc + 1) * 128], rhs=xsT[:, k, sh * 512:(sh + 1) * 512],
                                                      start=(k == 0), stop=(k == 7)), reads=[bwu, bxsT], writes=[bpu])
                    c.op('act', lambda e: e.activation(out=sg[:], in_=pg[:, :], func=AF.Silu), reads=[bpg], writes=[bsg])
                    c.op('dve', lambda e: e.tensor_tensor(out=hidT[:, fc, sh * 512:(sh + 1) * 512], in0=sg[:], in1=pu[:, :], op=ALU.mult),
                         reads=[bsg, bpu], writes=[bhidT])
            m = 0
            for j in range(8):
                for dh in range(2):
                    py, bpy = g.ps[4 + (m % 2)]
                    m += 1
                    for fc in range(4):
                        c.op('pe', lambda e: e.matmul(py[:, :], lhsT=hidT[:, fc, j * 128:(j + 1) * 128], rhs=wd[:, fc, dh * 512:(dh + 1) * 512],
                                                      start=(fc == 0), stop=(fc == 3)), reads=[bhidT, bwd], writes=[bpy])
                    ysl = yacc[:, j, dh * 512:(dh + 1) * 512]
                    gsc = gateT[:, j, ex_i:ex_i + 1]
                    if q == 0:
                        c.op('dve', lambda e: e.tensor_scalar(out=ysl, in0=py[:, :], scalar1=gsc, scalar2=None, op0=ALU.mult),
                             reads=[bpy, bgateT], writes=[byacc])
                    else:
                        c.op('dve', lambda e: e.scalar_tensor_tensor(out=ysl, in0=py[:, :], scalar=gsc, in1=ysl, op0=ALU.mult, op1=ALU.add),
                             reads=[bpy, bgateT, byacc], writes=[byacc])
        for j in range(8):
            c.dma('pool', lambda e: e.indirect_dma_start(out=X[:, :], out_offset=bass.IndirectOffsetOnAxis(ap=idxT[:, j, ex_i:ex_i + 1], axis=0),
                                                        in_=yacc[:, j, :], in_offset=None, compute_op=ALU.add),
                  reads=[byacc, bidxT], pwrites=[bX])
        bX.seal()


def moe_alloc(nc, es=None):
    sb = {}
    sb['gB'] = alloc_T(nc, 'gB', [128, D], F32, 1, es=es)[0]
    sb['tmprow'] = alloc_T(nc, 'tmprow', [1, 1024], F32, 1, es=es)[0]
    sb['wr'] = alloc_T(nc, 'wr', [128, 8, NE], F32, 1, es=es)[0]
    big = es.enter_context(nc.sbuf_tensor(uniq('big'), [128, L], F32)) if es is not None else nc.alloc_sbuf_tensor('big', [128, L], F32); bbig = Buf('big')
    sb['affT'] = (big[0:NE, :], bbig)
    sb['xt'] = alloc_T(nc, 'xt', [128, D], F32, 2, es=es)
    sb['h32'] = alloc_T(nc, 'h32', [128, D], F32, 2, es=es)
    sb['hb'] = alloc_T(nc, 'hb', [128, D], BF16, 2, es=es)
    sb['junk'] = alloc_T(nc, 'junk', [128, D], F32, 1, es=es)[0]
    sb['ss'] = alloc_T(nc, 'ss', [128, 4], F32, 2, es=es)
    sb['hT32'] = alloc_T(nc, 'hT32', [128, D], F32, 2, es=es)
    sb['sm'] = alloc_T(nc, 'sm', [128, 4], F32, 2, es=es)
    sb['ex'] = alloc_T(nc, 'ex', [128, NE], F32, 2, es=es)
    sb['vals'] = alloc_T(nc, 'vals', [NE, CAP], F32, 1, es=es)[0]
    sb['idxu'] = alloc_T(nc, 'idxu', [NE, CAP], U32, 1, es=es)[0]
    sb['idxf'] = alloc_T(nc, 'idxf', [NE, CAP], F32, 1, es=es)[0]
    sb['idxT'] = alloc_T(nc, 'idxT', [128, 8, NE], U32, 1, es=es)[0]
    sb['gateT'] = alloc_T(nc, 'gateT', [128, 8, NE], F32, 1, es=es)[0]
    sb['xsT'] = alloc_T(nc, 'xsT', [128, 8, CAP], BF16, 1, es=es)[0]
    sb['hidT'] = alloc_T(nc, 'hidT', [128, 4, CAP], BF16, 1, es=es)[0]
    sb['yacc'] = (big[:, :].rearrange('p (j d) -> p j d', j=8), bbig)
    sb['xs'] = alloc_T(nc, 'xs', [128, D], BF16, 2, es=es)
    sb['wg'] = alloc_T(nc, 'wg', [128, 8, 512], BF16, 2, es=es)
    sb['wu'] = alloc_T(nc, 'wu', [128, 8, 512], BF16, 2, es=es)
    sb['wd'] = alloc_T(nc, 'wd', [128, 4, D], BF16, 2, es=es)
    sb['sg'] = alloc_T(nc, 'sg', [128, 512], BF16, 2, es=es)
    return sb


def moe_layer(c, g, X, bX, HB, bHB, ffn_g, w_router, w_gate, w_up, w_down):
    with ExitStack() as es:
        sb = moe_alloc(c.nc, es)
        moe_phase(c, g, X, bX, HB, bHB, ffn_g, w_router, w_gate, w_up, w_down, sb)
        barrier(c)


def proj_phase(c, g, X, bX, gain_ap, w_ap, nout, spec, PT, bPT, PV, bPV):
    nc = c.nc
    with ExitStack() as es:
        def T(name, shape, dt):
            return es.enter_context(nc.sbuf_tensor(uniq(name), shape, dt))
        wsb = T("pj_w", [128, 8, nout], BF16); bw = Buf()
        gB = T("pj_gB", [128, D], F32); bgB = Buf()
        tmpr = T("pj_tmpr", [1, D], F32); btmpr = Buf()
        xt = [T("pj_xt%d" % i, [128, 4, D], F32) for i in range(2)]; bxt = [Buf(), Buf()]
        junk = T("pj_junk", [128, D], F32); bjunk = Buf()
        ss = [T("pj_ss%d" % i, [128, 4], F32) for i in range(2)]; bss = [Buf(), Buf()]
        hb = [T("pj_hb%d" % i, [128, D], BF16) for i in range(2)]; bhb = [Buf(), Buf()]
        hT = [T("pj_hT%d" % i, [128, 8, 512], BF16) for i in range(2)]; bhT = [Buf(), Buf()]
        stF = [T("pj_stF%d" % i, [128, 512], F32) for i in range(3)]; bstF = [Buf() for _ in range(3)]
        stT = [T("pj_stT%d" % i, [128, 512], BF16) for i in range(3)]; bstT = [Buf() for _ in range(3)]
        bcast_row(c, g, gB, bgB, gain_ap, D, tmpr, btmpr)
        for c0 in range(0, nout, 512):
            wd = min(512, nout - c0)
            c.dma('pool', lambda e: e.dma_start(out=wsb[:, :, c0:c0 + wd], in_=w_ap[:, c0:c0 + wd].rearrange("(k p) f -> p k f", p=128)), writes=[bw])
        ptb, bptb = g.psb
        nF = 0; nT = 0; npz = 0
        for it in range(L // 512):
            t0 = it * 512
            x_, bx_ = xt[it % 2], bxt[it % 2]
            c.dma('sp', lambda e: e.dma_start(out=x_[:], in_=X[t0:t0 + 512, :].rearrange("(j p) d -> p j d", p=128)), reads=[bX], writes=[bx_])
            hT_, bhT_ = hT[it % 2], bhT[it % 2]
            for j in range(4):
                s_, bs_ = ss[j % 2], bss[j % 2]
                h_, bh_ = hb[j % 2], bhb[j % 2]
                c.op('dve', lambda e: e.scalar_tensor_tensor(out=junk[:], in0=x_[:, j, :], scalar=1.0, in1=x_[:, j, :], op0=ALU.mult, op1=ALU.mult,
                                                             accum_out=s_[:, 0:1]), reads=[bx_], writes=[bjunk, bs_])
                c.op('dve', lambda e: e.tensor_scalar(out=s_[:, 0:1], in0=s_[:, 0:1], scalar1=1.0 / D, scalar2=1e-6, op0=ALU.mult, op1=ALU.add),
                     reads=[bs_], writes=[bs_])
                c.op('act', lambda e: e.activation(out=s_[:, 0:1], in_=s_[:, 0:1], func=AF.Ln), reads=[bs_], writes=[bs_])
                c.op('act', lambda e: e.activation(out=s_[:, 0:1], in_=s_[:, 0:1], func=AF.Exp, scale=-0.5), reads=[bs_], writes=[bs_])
                c.op('dve', lambda e: e.scalar_tensor_tensor(out=h_[:], in0=x_[:, j, :], scalar=s_[:, 0:1], in1=gB[:], op0=ALU.mult, op1=ALU.mult),
                     reads=[bx_, bs_, bgB], writes=[bh_])
                for k in range(8):
                    c.op('pe', lambda e: e.transpose(out=ptb[:, k * 128:(k + 1) * 128], in_=h_[:, k * 128:(k + 1) * 128], identity=g.identb[:]),
                         reads=[bh_, g.b_identb], writes=[bptb])
                c.op('act', lambda e: e.activation(out=hT_[:, :, j * 128:(j + 1) * 128], in_=ptb[:, :].rearrange("p (k s) -> p k s", k=8), func=AF.Copy),
                     reads=[bptb], writes=[bhT_])
            for (col0, ncols, mode, dst0) in spec:
                if mode == 'F':
                    for f0 in range(0, ncols, 128):
                        fw = min(128, ncols - f0)
                        ps, bps = g.ps[npz % 4]; npz += 1
                        for k in range(8):
                            c.op('pe', lambda e: e.matmul(ps[0:fw, :], lhsT=wsb[:, k, col0 + f0:col0 + f0 + fw], rhs=hT_[:, k, :], start=(k == 0), stop=(k == 7)),
                                 reads=[bw, bhT_], writes=[bps])
                        st, bst = stF[nF % 3], bstF[nF % 3]; nF += 1
                        eng = 'act' if nF % 2 else 'dve'
                        if eng == 'act':
                            c.op('act', lambda e: e.activation(out=st[0:fw, :], in_=ps[0:fw, :], func=AF.Copy), reads=[bps], writes=[bst])
                        else:
                            c.op('dve', lambda e: e.tensor_copy(out=st[0:fw, :], in_=ps[0:fw, :]), reads=[bps], writes=[bst])
                        c.dma('sp' if nF % 2 else 'act', lambda e: e.dma_start(out=PT[dst0 + f0:dst0 + f0 + fw, t0:t0 + 512], in_=st[0:fw, :]), reads=[bst], pwrites=[bPT])
                else:
                    for j in range(4):
                        for c0 in range(0, ncols, 512):
                            cw = min(512, ncols - c0)
                            ps, bps = g.ps[npz % 4]; npz += 1
                            for k in range(8):
                                c.op('pe', lambda e: e.matmul(ps[:, 0:cw], lhsT=hT_[:, k, j * 128:(j + 1) * 128], rhs=wsb[:, k, col0 + c0:col0 + c0 + cw], start=(k == 0), stop=(k == 7)),
                                     reads=[bw, bhT_], writes=[bps])
                            st, bst = stT[nT % 3], bstT[nT % 3]; nT += 1
                            eng = 'act' if nT % 2 else 'dve'
                            if eng == 'act':
                                c.op('act', lambda e: e.activation(out=st[:, 0:cw], in_=ps[:, 0:cw], func=AF.Copy), reads=[bps], writes=[bst])
                            else:
                                c.op('dve', lambda e: e.tensor_copy(out=st[:, 0:cw], in_=ps[:, 0:cw]), reads=[bps], writes=[bst])
                            c.dma('sp' if nT % 2 else 'act', lambda e: e.dma_start(out=PV[t0 + j * 128:t0 + (j + 1) * 128, dst0 + c0:dst0 + c0 + cw], in_=st[:, 0:cw]),
                                  reads=[bst], pwrites=[bPV])
        bPT.seal(); bPV.seal()
        barrier(c)


TBK = 2048
NTB = L // TBK


def hgrn2_phase(c, g, PT, bPT, PV, bPV, lb_logits, jl, a_out_norm, QK, bQK, OA, bOAs, MT, bMT, stage=3):
    nc = c.nc
    with ExitStack() as es0:
        def T0(name, shape, dt):
            return es0.enter_context(nc.sbuf_tensor(uniq(name), shape, dt))
        mcols = T0("hg_mcols", [128, 8, 128], F32); bmcols = Buf()
        with ExitStack() as es:
            def T(name, shape, dt):
                return es.enter_context(nc.sbuf_tensor(uniq(name), shape, dt))
            lbt = T("hg_lbt", [128, 2, 2, 4], F32); blbt = Buf()
            lbc = T("hg_lbc", [128, 8], F32); blbc = Buf()
            oml = T("hg_oml", [128, 8], F32); boml = Buf()
            noml = T("hg_noml", [128, 8], F32); bnoml = Buf()
            msk = T("hg_msk", [128, TBK], F32); bmsk = Buf()
            bmid = T("hg_bmid", [128, 128], F32); bbmid = Buf()
            blast = T("hg_blast", [128, 128], F32); bblast = Buf()
            zq = [T("hg_zq%d" % i, [128, TBK], F32) for i in range(2)]; bzq = [Buf(), Buf()]
            zf = [T("hg_zf%d" % i, [128, TBK], F32) for i in range(2)]; bzf = [Buf(), Buf()]
            q_ = T("hg_q", [128, TBK], F32); bq_ = Buf()
            sig = T("hg_sig", [128, TBK], F32); bsig = Buf()
            f_ = T("hg_f", [128, TBK], F32); bf_ = Buf()
            kk = T("hg_kk", [128, TBK], F32); bkk = Buf()
            b_ = T("hg_b", [128, TBK], F32); bb_ = Buf()
            e1 = T("hg_e1", [128, TBK], F32); be1 = Buf()
            eq = T("hg_eq", [128, TBK], F32); beq = Buf()
            ek = T("hg_ek", [128, TBK], F32); bek = Buf()
            qt = [T("hg_qt%d" % i, [128, TBK], BF16) for i in range(2)]; bqt = [Buf(), Buf()]
            kt = [T("hg_kt%d" % i, [128, TBK], BF16) for i in range(2)]; bkt = [Buf(), Buf()]
            if jl == 0:
                c.op('pool', lambda e: e.memset(lbc[:], 0.0), writes=[blbc])
            else:
                with nc.allow_non_contiguous_dma(reason="tiny"):
                    for j_ in range(2):
                        for r_ in range(2):
                            c.dma('sp', lambda e: e.dma_start(out=lbt[:, j_, r_, :], in_=lb_logits[j_, r_, :].rearrange("(h p) -> p h", p=128)), pwrites=[blbt])
                blbt.seal()
                c.op('dve', lambda e: e.tensor_tensor(out=lbc[:].rearrange("p (r h) -> p r h", r=2), in0=lbt[:, 1, :, :], in1=lbt[:, 0, :, :], op=ALU.subtract),
                     reads=[blbt], writes=[blbc])
                c.op('act', lambda e: e.activation(out=lbc[:], in_=lbc[:], func=AF.Sigmoid), reads=[blbc], writes=[blbc])
            c.op('dve', lambda e: e.tensor_scalar(out=oml[:], in0=lbc[:], scalar1=-1.0, scalar2=1.0, op0=ALU.mult, op1=ALU.add), reads=[blbc], writes=[boml])
            c.op('dve', lambda e: e.tensor_scalar(out=noml[:], in0=oml[:], scalar1=-1.0, scalar2=None, op0=ALU.mult), reads=[boml], writes=[bnoml])
            c.op('pool', lambda e: e.memset(msk[:], 1.0), writes=[bmsk])
            c.op('pool', lambda e: e.memset(msk[:].rearrange("p (c j) -> p c j", j=64)[:, :, 0:1], 0.0), writes=[bmsk])
            n = 0
            for h in range(4):
                for r in range(2):
                    hr = r * 4 + h
                    for tb in range(NTB):
                        nb = tb if r == 0 else NTB - 1 - tb
                        zq_, bzq_ = zq[n % 2], bzq[n % 2]
                        zf_, bzf_ = zf[n % 2], bzf[n % 2]
                        qt_, bqt_ = qt[n % 2], bqt[n % 2]
                        kt_, bkt_ = kt[n % 2], bkt[n % 2]
                        n += 1
                        c.dma('sp', lambda e: e.dma_start(out=zq_[:], in_=PT[h * 128:(h + 1) * 128, nb * TBK:(nb + 1) * TBK]), reads=[bPT], writes=[bzq_])
                        fr = 512 + r * 512 + h * 128
                        c.dma('act', lambda e: e.dma_start(out=zf_[:], in_=PT[fr:fr + 128, nb * TBK:(nb + 1) * TBK]), reads=[bPT], writes=[bzf_])
                        zqs = zq_[:, ::-1] if r else zq_[:, :]
                        zfs = zf_[:, ::-1] if r else zf_[:, :]
                        c.op('act', lambda e: e.activation(out=q_[:], in_=zqs, func=AF.Silu), reads=[bzq_], writes=[bq_])
                        c.op('act', lambda e: e.activation(out=sig[:], in_=zfs, func=AF.Sigmoid), reads=[bzf_], writes=[bsig])
                        c.op('dve', lambda e: e.tensor_scalar(out=f_[:], in0=sig[:], scalar1=oml[:, hr:hr + 1], scalar2=lbc[:, hr:hr + 1], op0=ALU.mult, op1=ALU.add),
                             reads=[bsig, boml, blbc], writes=[bf_])
                        c.op('act', lambda e: e.activation(out=f_[:], in_=f_[:], func=AF.Ln), reads=[bf_], writes=[bf_])
                        c.op('dve', lambda e: e.tensor_scalar(out=kk[:], in0=sig[:], scalar1=noml[:, hr:hr + 1], scalar2=oml[:, hr:hr + 1], op0=ALU.mult, op1=ALU.add),
                             reads=[bsig, bnoml, boml], writes=[bkk])
                        c.op('dve', lambda e: e.tensor_tensor_scan(out=b_[:], data0=msk[:], data1=f_[:], initial=0.0, op0=ALU.mult, op1=ALU.add),
                             reads=[bmsk, bf_], writes=[bb_])
                        b3 = b_[:].rearrange("p (c j) -> p c j", j=64)
                        c.op('dve', lambda e: e.tensor_tensor(out=e1[:].rearrange("p (c j) -> p c j", j=64), in0=b3, in1=b3[:, :, 31:32].broadcast_to([128, TBK // 64, 64]), op=ALU.subtract),
                             reads=[bb_], writes=[be1])
                        c.op('act', lambda e: e.activation(out=eq[:], in_=e1[:], func=AF.Exp), reads=[be1], writes=[beq])
                        c.op('act', lambda e: e.activation(out=ek[:], in_=e1[:], func=AF.Exp, scale=-1.0), reads=[be1], writes=[bek])
                        c.op('dve', lambda e: e.tensor_tensor(out=qt_[:], in0=q_[:], in1=eq[:], op=ALU.mult), reads=[bq_, beq], writes=[bqt_])
                        c.op('dve', lambda e: e.tensor_tensor(out=kt_[:], in0=kk[:], in1=ek[:], op=ALU.mult), reads=[bkk, bek], writes=[bkt_])
                        nch = TBK // 64
                        c.op('act', lambda e: e.activation(out=bmid[:, tb * nch:(tb + 1) * nch], in_=b3[:, :, 31], func=AF.Copy), reads=[bb_], writes=[bbmid])
                        c.op('act', lambda e: e.activation(out=blast[:, tb * nch:(tb + 1) * nch], in_=b3[:, :, 63], func=AF.Copy), reads=[bb_], writes=[bblast])
                        c.dma('sp', lambda e: e.dma_start(out=QK[h, r, 0, :, tb * TBK:(tb + 1) * TBK], in_=qt_[:]), reads=[bqt_], pwrites=[bQK])
                        c.dma('act', lambda e: e.dma_start(out=QK[h, r, 1, :, tb * TBK:(tb + 1) * TBK], in_=kt_[:]), reads=[bkt_], pwrites=[bQK])
                    c.op('dve', lambda e: e.tensor_tensor(out=blast[:], in0=blast[:], in1=bmid[:], op=ALU.subtract), reads=[bblast, bbmid], writes=[bblast])
                    c.op('dve', lambda e: e.tensor_tensor(out=blast[:, 0:127], in0=blast[:, 0:127], in1=bmid[:, 1:128], op=ALU.add), reads=[bblast, bbmid], writes=[bblast])
                    c.op('act', lambda e: e.activation(out=mcols[:, hr, :], in_=blast[:], func=AF.Exp), reads=[bblast], writes=[bmcols])
            bQK.seal()
            barrier(c)
        if stage < 2:
            return
        with ExitStack() as es:
            def T(name, shape, dt):
                return es.enter_context(nc.sbuf_tensor(uniq(name), shape, dt))
            qb = [T("hr_qb%d" % i, [128, TBK], BF16) for i in range(2)]; bqb = [Buf(), Buf()]
            kb = [T("hr_kb%d" % i, [128, TBK], BF16) for i in range(2)]; bkb = [Buf(), Buf()]
            vb = [T("hr_vb%d" % i, [128, TBK // 128, 128], BF16) for i in range(2)]; bvb = [Buf(), Buf()]
            mask = T("hr_mask", [128, 128], F32); bmask = Buf()
            attnT = [T("hr_attn%d" % i, [128, 128], BF16) for i in range(2)]; battn = [Buf(), Buf()]
            ktok = [T("hr_ktok%d" % i, [128, 128], BF16) for i in range(2)]; bktok = [Buf(), Buf()]
            M32 = T("hr_M32", [128, 128], F32); bM32 = Buf()
            Mb = T("hr_Mb", [128, 128], BF16); bMb = Buf()
            tmp32 = T("hr_tmp32", [128, 128], F32); btmp32 = Buf()
            osb = [T("hr_osb%d" % i, [128, 128], F32) for i in range(3)]; bosb = [Buf() for _ in range(3)]
            osf = [T("hr_osf%d" % i, [128, 128], F32) for i in range(3)]; bosf = [Buf() for _ in range(3)]
            vnat = T("hr_vnat", [128, TBK // 128, 128], BF16); bvnat = Buf()
            c.op('pool', lambda e: e.memset(mask[:], 1.0), writes=[bmask])
            c.op('pool', lambda e: e.affine_select(out=mask[:], in_=mask[:], pattern=[[1, 128]], compare_op=ALU.is_ge, fill=0.0, base=0, channel_multiplier=-1),
                 reads=[bmask], writes=[bmask])
            c.op('pool', lambda e: e.memset(mask[0:64, 64:128], 0.0), reads=[bmask], writes=[bmask])
            ptb, bptb = g.psb
            n = 0; nblk = 0; nch = 0
            for h in range(4):
                for r in range(2):
                    hr = r * 4 + h
                    c.op('pool', lambda e: e.memset(M32[:], 0.0), writes=[bM32])
                    c.op('pool', lambda e: e.memset(Mb[:], 0.0), writes=[bMb])
                    for tb in range(NTB):
                        qb_, bqb_ = qb[n % 2], bqb[n % 2]
                        kb_, bkb_ = kb[n % 2], bkb[n % 2]
                        vb_, bvb_ = vb[n % 2], bvb[n % 2]
                        n += 1
                        c.dma('sp', lambda e: e.dma_start(out=qb_[:], in_=QK[h, r, 0, :, tb * TBK:(tb + 1) * TBK]), reads=[bQK], writes=[bqb_])
                        c.dma('act', lambda e: e.dma_start(out=kb_[:], in_=QK[h, r, 1, :, tb * TBK:(tb + 1) * TBK]), reads=[bQK], writes=[bkb_])
                        if r == 0:
                            vsrc = PV[tb * TBK:(tb + 1) * TBK, h * 128:(h + 1) * 128].rearrange("(b p) d -> p b d", p=128)
                            c.dma('sp', lambda e: e.dma_start(out=vb_[:], in_=vsrc), reads=[bPV], writes=[bvb_])
                        else:
                            vsrc = PV[L - (tb + 1) * TBK:L - tb * TBK, h * 128:(h + 1) * 128].rearrange("(b p) d -> p b d", p=128)
                            c.dma('sp', lambda e: e.dma_start(out=vnat[:], in_=vsrc), reads=[bPV], writes=[bvnat])
                            nbk = TBK // 128
                            for b4 in range(0, nbk, 4):
                                pf, bpf = g.ps[4 + (b4 // 4) % 2]
                                c.op('pe', lambda e: e.matmul(pf[:, :], lhsT=g.Jb[:], rhs=vnat[:, b4:b4 + 4, :], start=True, stop=True), reads=[g.b_Jb, bvnat], writes=[bpf])
                                for bb in range(4):
                                    c.op('act', lambda e: e.activation(out=vb_[:, nbk - 1 - (b4 + bb), :], in_=pf[:, bb * 128:(bb + 1) * 128], func=AF.Copy), reads=[bpf], writes=[bvb_])
                        for b in range(TBK // 128):
                            blk = tb * (TBK // 128) + b
                            at_, bat_ = attnT[nblk % 2], battn[nblk % 2]
                            kt_, bkt_ = ktok[nblk % 2], bktok[nblk % 2]
                            pa, bpa = g.ps[nblk % 2]
                            po, bpo = g.ps[2 + nblk % 2]
                            os_, bos_ = osb[nblk % 3], bosb[nblk % 3]
                            nblk += 1
                            bs = slice(b * 128, (b + 1) * 128)
                            c.op('pe', lambda e: e.matmul(pa[:, 0:128], lhsT=kb_[:, bs], rhs=qb_[:, bs], start=True, stop=True), reads=[bkb_, bqb_], writes=[bpa])
                            c.op('dve', lambda e: e.tensor_tensor(out=at_[:], in0=pa[:, 0:128], in1=mask[:], op=ALU.mult), reads=[bpa, bmask], writes=[bat_])
                            c.op('pe', lambda e: e.transpose(out=ptb[:, 0:128], in_=kb_[:, bs], identity=g.identb[:]), reads=[bkb_, g.b_identb], writes=[bptb])
                            c.op('act', lambda e: e.activation(out=kt_[:], in_=ptb[:, 0:128], func=AF.Copy), reads=[bptb], writes=[bkt_])
                            for ci in range(2):
                                r0 = 64 * ci
                                cidx = 2 * blk + ci
                                pk, bpk = g.ps[4 + nch % 2]; nch += 1
                                c.op('pe', lambda e: e.matmul(po[r0:r0 + 64, 0:128], lhsT=at_[r0:r0 + 64, r0:r0 + 64], rhs=vb_[r0:r0 + 64, b, :], start=True, stop=False),
                                     reads=[bat_, bvb_], writes=[bpo])
                                c.op('pe', lambda e: e.matmul(po[r0:r0 + 64, 0:128], lhsT=qb_[:, b * 128 + r0:b * 128 + r0 + 64], rhs=Mb[:, :], start=False, stop=True),
                                     reads=[bqb_, bMb], writes=[bpo])
                                if cidx < 127:
                                    c.op('pe', lambda e: e.matmul(pk[:, 0:128], lhsT=kt_[r0:r0 + 64, :], rhs=vb_[r0:r0 + 64, b, :], start=True, stop=True),
                                         reads=[bkt_, bvb_], writes=[bpk])
                                    c.op('dve', lambda e: e.tensor_tensor(out=tmp32[:], in0=pk[:, 0:128], in1=M32[:], op=ALU.add), reads=[bpk, bM32], writes=[btmp32])
                                    c.op('dve', lambda e: e.tensor_scalar(out=M32[:], in0=tmp32[:], scalar1=mcols[:, hr, cidx:cidx + 1], scalar2=None, op0=ALU.mult),
                                         reads=[btmp32, bmcols], writes=[bM32])
                                    c.op('act', lambda e: e.activation(out=Mb[:], in_=M32[:], func=AF.Copy), reads=[bM32], writes=[bMb])
                            c.op('act', lambda e: e.activation(out=os_[:], in_=po[:, 0:128], func=AF.Copy), reads=[bpo], writes=[bos_])
                            if r == 0:
                                c.dma('sp', lambda e: e.dma_start(out=OA[blk * 128:(blk + 1) * 128, h * 128:(h + 1) * 128], in_=os_[:]), reads=[bos_], pwrites=[bOAs[h]])
                            else:
                                of_, bof_ = osf[nblk % 3], bosf[nblk % 3]
                                pf, bpf = g.ps[6]
                                c.op('pe', lambda e: e.matmul(pf[:, 0:128], lhsT=g.J32[:], rhs=os_[:], start=True, stop=True), reads=[g.b_J32, bos_], writes=[bpf])
                                c.op('act', lambda e: e.activation(out=of_[:], in_=pf[:, 0:128], func=AF.Copy), reads=[bpf], writes=[bof_])
                                c.dma('pool', lambda e: e.dma_start(out=OA[L - (blk + 1) * 128:L - blk * 128, h * 128:(h + 1) * 128], in_=of_[:], accum_op=ALU.add),
                                      reads=[bof_], pwrites=[bOAs[h]])
                    bOAs[h].seal()
            barrier(c)
        if stage < 3:
            return
        with ExitStack() as es:
            def T(name, shape, dt):
                return es.enter_context(nc.sbuf_tensor(uniq(name), shape, dt))
            gA = T("hf_gA", [128, 128], F32); bgA = Buf()
            tmpr = T("hf_tmpr", [1, 128], F32); btmpr = Buf()
            oa = [T("hf_oa%d" % i, [128, 512], F32) for i in range(2)]; boa = [Buf(), Buf()]
            ga = [T("hf_ga%d" % i, [128, 512], BF16) for i in range(2)]; bga = [Buf(), Buf()]
            sq = T("hf_sq", [128, 512], F32); bsq = Buf()
            ssq = [T("hf_ssq%d" % i, [128, 4], F32) for i in range(2)]; bssq = [Buf(), Buf()]
            sg = T("hf_sg", [128, 512], F32); bsg = Buf()
            t1 = T("hf_t1", [128, 512], F32); bt1 = Buf()
            ob = [T("hf_ob%d" % i, [128, 512], BF16) for i in range(2)]; bob = [Buf(), Buf()]
            mt = [T("hf_mt%d" % i, [128, 4, 512], BF16) for i in range(2)]; bmt = [Buf(), Buf()]
            bcast_row(c, g, gA, bgA, a_out_norm, 128, tmpr, btmpr)
            ptb, bptb = g.psb
            for i in range(NT):
                oa_, boa_ = oa[i % 2], boa[i % 2]
                ga_, bga_ = ga[i % 2], bga[i % 2]
                ss_, bss_ = ssq[i % 2], bssq[i % 2]
                ob_, bob_ = ob[i % 2], bob[i % 2]
                mt_, bmt_ = mt[(i // 4) % 2], bmt[(i // 4) % 2]
                c.dma('sp', lambda e: e.dma_start(out=oa_[:], in_=OA[i * 128:(i + 1) * 128, :]), reads=bOAs, writes=[boa_])
                c.dma('act', lambda e: e.dma_start(out=ga_[:], in_=PV[i * 128:(i + 1) * 128, 512:1024]), reads=[bPV], writes=[bga_])
                c.op('act', lambda e: e.activation(out=sq[:], in_=oa_[:], func=AF.Square), reads=[boa_], writes=[bsq])
                c.op('dve', lambda e: e.tensor_reduce(out=ss_[:], in_=sq[:].rearrange("p (h d) -> p h d", h=4), axis=AX.X, op=ALU.add), reads=[bsq], writes=[bss_])
                c.op('dve', lambda e: e.tensor_scalar(out=ss_[:], in0=ss_[:], scalar1=1.0 / 128, scalar2=1e-6, op0=ALU.mult, op1=ALU.add), reads=[bss_], writes=[bss_])
                c.op('pool', lambda e: e.tensor_tensor(out=ss_[:], in0=ss_[:], in1=g.neghalf[:, 0:1].broadcast_to([128, 4]), op=ALU.pow), reads=[bss_, g.b_neghalf], writes=[bss_])
                c.op('act', lambda e: e.activation(out=sg[:], in_=ga_[:], func=AF.Silu), reads=[bga_], writes=[bsg])
                c.op('dve', lambda e: e.tensor_tensor(out=t1[:].rearrange("p (h d) -> p h d", h=4), in0=oa_[:].rearrange("p (h d) -> p h d", h=4),
                                                      in1=ss_[:].unsqueeze(2).broadcast_to([128, 4, 128]), op=ALU.mult), reads=[boa_, bss_], writes=[bt1])
                c.op('dve', lambda e: e.tensor_tensor(out=t1[:].rearrange("p (h d) -> p h d", h=4), in0=t1[:].rearrange("p (h d) -> p h d", h=4),
                                                       in1=gA[:].unsqueeze(1).broadcast_to([128, 4, 128]), op=ALU.mult), reads=[bt1, bgA], writes=[bt1])
                c.op('dve', lambda e: e.tensor_tensor(out=ob_[:], in0=t1[:], in1=sg[:], op=ALU.mult), reads=[bt1, bsg], writes=[bob_])
                for k in range(4):
                    c.op('pe', lambda e: e.transpose(out=ptb[:, k * 128:(k + 1) * 128], in_=ob_[:, k * 128:(k + 1) * 128], identity=g.identb[:]),
                         reads=[bob_, g.b_identb], writes=[bptb])
                c.op('act', lambda e: e.activation(out=mt_[:, :, (i % 4) * 128:(i % 4 + 1) * 128], in_=ptb[:, 0:512].rearrange("p (k s) -> p k s", k=4), func=AF.Copy),
                     reads=[bptb], writes=[bmt_])
                if i % 4 == 3:
                    t0 = (i // 4) * 512
                    c.dma('sp', lambda e: e.dma_start(out=MT[0:512, t0:t0 + 512].rearrange("(k p) t -> p k t", p=128), in_=mt_[:]), reads=[bmt_], pwrites=[bMT])
            barrier(c)


TB5 = 1024
NTB5 = L // TB5
TWO_PI = 2.0 * math.pi


def s5_phase(c, g, PT, bPT, lam_re, lam_im, log_step, b_re, b_im, c_re, c_im, d_skip, glu_w, glu_b, YT, bYT, MT, bMT, stage=3):
    nc = c.nc
    U0 = 1536
    with ExitStack() as es0:
        def T0(name, shape, dt):
            return es0.enter_context(nc.sbuf_tensor(uniq(name), shape, dt))
        WB = [T0("s5_WB%d" % p, [128, 2, 4, 128], BF16) for p in range(2)]; bWB = Buf()
        WC = [T0("s5_WC%d" % p, [128, 2, 4, 128], BF16) for p in range(2)]; bWC = Buf()
        WBx = [T0("s5_WBx%d" % p, [128, 2, 4, 128], BF16) for p in range(2)]
        WCx = [T0("s5_WCx%d" % p, [128, 2, 4, 64], BF16) for p in range(2)]
        mag = T0("s5_mag", [128, 32], F32); bmag = Buf()
        pwc = T0("s5_pwc", [128, 11, 32], F32); bpw = Buf()
        pws = T0("s5_pws", [128, 11, 32], F32)
        with ExitStack() as es:
            def T(name, shape, dt):
                return es.enter_context(nc.sbuf_tensor(uniq(name), shape, dt))
            n_ = [0]

            def S(shape=[128, 32], dt=F32):
                n_[0] += 1
                return T("s5_t%d" % n_[0], shape, dt), Buf()
            lamre, blamre = S(); lamim, blamim = S()
            lsB, blsB = S([128, 64]); ls, bls = S()
            with nc.allow_non_contiguous_dma(reason="small params"):
                for r_ in range(2):
                    for g4 in range(0, 16, 4):
                        c.dma('sp', lambda e: e.dma_start(out=lamre[:, r_ * 16 + g4:r_ * 16 + g4 + 4], in_=lam_re[r_, 2 * g4:2 * g4 + 8, :].rearrange("(gp gl) n -> (gl n) gp", gl=2)), pwrites=[blamre])
                        c.dma('act', lambda e: e.dma_start(out=lamim[:, r_ * 16 + g4:r_ * 16 + g4 + 4], in_=lam_im[r_, 2 * g4:2 * g4 + 8, :].rearrange("(gp gl) n -> (gl n) gp", gl=2)), pwrites=[blamim])
                blamre.seal(); blamim.seal()
                c.dma('sp', lambda e: e.dma_start(out=lsB[:], in_=log_step.rearrange("r g -> (r g)").partition_broadcast(128)), writes=[blsB])
            lsv = lsB[:].rearrange("p (r gp gl) -> p r gp gl", r=2, gl=2)
            c.op('dve', lambda e: e.tensor_copy(out=ls[0:64, :].rearrange("p (r gp) -> p r gp", r=2), in_=lsv[0:64, :, :, 0]), reads=[blsB], writes=[bls])
            c.op('dve', lambda e: e.tensor_copy(out=ls[64:128, :].rearrange("p (r gp) -> p r gp", r=2), in_=lsv[64:128, :, :, 1]), reads=[blsB], writes=[bls])
            step, bstep = S()
            c.op('act', lambda e: e.activation(out=step[:], in_=ls[:], func=AF.Exp), reads=[bls], writes=[bstep])
            lrs, blrs = S(); ang, bang = S()
            c.op('dve', lambda e: e.tensor_tensor(out=lrs[:], in0=lamre[:], in1=step[:], op=ALU.mult), reads=[blamre, bstep], writes=[blrs])
            c.op('act', lambda e: e.activation(out=mag[:], in_=lrs[:], func=AF.Exp), reads=[blrs], writes=[bmag])
            c.op('dve', lambda e: e.tensor_tensor(out=ang[:], in0=lamim[:], in1=step[:], op=ALU.mult), reads=[blamim, bstep], writes=[bang])

            def sin_of(src, bsrc, offset, dst, bdst):
                q, bq = S(); qi, bqi = S(dt=I32); r, br = S(); m, bm = S()
                c.op('dve', lambda e: e.tensor_scalar(out=q[:], in0=src[:], scalar1=offset, scalar2=1.0 / TWO_PI, op0=ALU.add, op1=ALU.mult), reads=[bsrc], writes=[bq])
                c.op('dve', lambda e: e.tensor_copy(out=qi[:], in_=q[:]), reads=[bq], writes=[bqi])
                c.op('dve', lambda e: e.tensor_copy(out=q[:], in_=qi[:]), reads=[bqi], writes=[bq])
                c.op('dve', lambda e: e.scalar_tensor_tensor(out=r[:], in0=q[:], scalar=-TWO_PI, in1=src[:], op0=ALU.mult, op1=ALU.add), reads=[bq, bsrc], writes=[br])
                if offset != 0.0:
                    c.op('dve', lambda e: e.tensor_scalar(out=r[:], in0=r[:], scalar1=offset, scalar2=None, op0=ALU.add), reads=[br], writes=[br])
                c.op('dve', lambda e: e.tensor_scalar(out=m[:], in0=r[:], scalar1=math.pi, scalar2=-TWO_PI, op0=ALU.is_gt, op1=ALU.mult), reads=[br], writes=[bm])
                c.op('dve', lambda e: e.tensor_tensor(out=r[:], in0=r[:], in1=m[:], op=ALU.add), reads=[br, bm], writes=[br])
                c.op('dve', lambda e: e.tensor_scalar(out=m[:], in0=r[:], scalar1=-math.pi, scalar2=TWO_PI, op0=ALU.is_lt, op1=ALU.mult), reads=[br], writes=[bm])
                c.op('dve', lambda e: e.tensor_tensor(out=r[:], in0=r[:], in1=m[:], op=ALU.add), reads=[br, bm], writes=[br])
                c.op('dve', lambda e: e.tensor_scalar(out=r[:], in0=r[:], scalar1=math.pi, scalar2=-math.pi, op0=ALU.min, op1=ALU.max), reads=[br], writes=[br])
                c.op('act', lambda e: e.activation(out=dst, in_=r[:], func=AF.Sin), reads=[br], writes=[bdst])
            sin_of(ang, bang, 0.0, pws[:, 0, :], bpw)
            sin_of(ang, bang, math.pi / 2, pwc[:, 0, :], bpw)
            tq, btq = S(); tq2, btq2 = S()
            for k in range(10):
                c.op('dve', lambda e: e.tensor_tensor(out=tq[:], in0=pwc[:, k, :], in1=pwc[:, k, :], op=ALU.mult), reads=[bpw], writes=[btq])
                c.op('dve', lambda e: e.tensor_tensor(out=tq2[:], in0=pws[:, k, :], in1=pws[:, k, :], op=ALU.mult), reads=[bpw], writes=[btq2])
                c.op('dve', lambda e: e.tensor_tensor(out=pwc[:, k + 1, :], in0=tq[:], in1=tq2[:], op=ALU.subtract), reads=[btq, btq2, bpw], writes=[bpw])
                c.op('dve', lambda e: e.tensor_tensor(out=tq[:], in0=pws[:, k, :], in1=pwc[:, k, :], op=ALU.mult), reads=[bpw], writes=[btq])
                c.op('dve', lambda e: e.tensor_scalar(out=pws[:, k + 1, :], in0=tq[:], scalar1=2.0, scalar2=None, op0=ALU.mult), reads=[btq, bpw], writes=[bpw])
            are, bare = S(); aim, baim = S(); den, bden = S(); am1, bam1 = S(); fr, bfr = S(); fi, bfi = S(); tt, btt = S()
            c.op('dve', lambda e: e.tensor_tensor(out=are[:], in0=mag[:], in1=pwc[:, 0, :], op=ALU.mult), reads=[bmag, bpw], writes=[bare])
            c.op('dve', lambda e: e.tensor_tensor(out=aim[:], in0=mag[:], in1=pws[:, 0, :], op=ALU.mult), reads=[bmag, bpw], writes=[baim])
            c.op('dve', lambda e: e.tensor_tensor(out=den[:], in0=lamre[:], in1=lamre[:], op=ALU.mult), reads=[blamre], writes=[bden])
            c.op('dve', lambda e: e.tensor_tensor(out=tt[:], in0=lamim[:], in1=lamim[:], op=ALU.mult), reads=[blamim], writes=[btt])
            c.op('dve', lambda e: e.tensor_tensor(out=den[:], in0=den[:], in1=tt[:], op=ALU.add), reads=[bden, btt], writes=[bden])
            c.op('dve', lambda e: e.reciprocal(out=den[:], in_=den[:]), reads=[bden], writes=[bden])
            c.op('dve', lambda e: e.tensor_scalar(out=am1[:], in0=are[:], scalar1=-1.0, scalar2=None, op0=ALU.add), reads=[bare], writes=[bam1])
            c.op('dve', lambda e: e.tensor_tensor(out=fr[:], in0=am1[:], in1=lamre[:], op=ALU.mult), reads=[bam1, blamre], writes=[bfr])
            c.op('dve', lambda e: e.tensor_tensor(out=tt[:], in0=aim[:], in1=lamim[:], op=ALU.mult), reads=[baim, blamim], writes=[btt])
            c.op('dve', lambda e: e.tensor_tensor(out=fr[:], in0=fr[:], in1=tt[:], op=ALU.add), reads=[bfr, btt], writes=[bfr])
            c.op('dve', lambda e: e.tensor_tensor(out=fr[:], in0=fr[:], in1=den[:], op=ALU.mult), reads=[bfr, bden], writes=[bfr])
            c.op('dve', lambda e: e.tensor_tensor(out=fi[:], in0=aim[:], in1=lamre[:], op=ALU.mult), reads=[baim, blamre], writes=[bfi])
            c.op('dve', lambda e: e.tensor_tensor(out=tt[:], in0=am1[:], in1=lamim[:], op=ALU.mult), reads=[bam1, blamim], writes=[btt])
            c.op('dve', lambda e: e.tensor_tensor(out=fi[:], in0=fi[:], in1=tt[:], op=ALU.subtract), reads=[bfi, btt], writes=[bfi])
            c.op('dve', lambda e: e.tensor_tensor(out=fi[:], in0=fi[:], in1=den[:], op=ALU.mult), reads=[bfi, bden], writes=[bfi])
            mk, bmk = S([128, 2])
            c.op('pool', lambda e: e.memset(mk[:], 0.0), writes=[bmk])
            c.op('pool', lambda e: e.memset(mk[0:64, 0:1], 1.0), reads=[bmk], writes=[bmk])
            c.op('pool', lambda e: e.memset(mk[64:128, 1:2], 1.0), reads=[bmk], writes=[bmk])
            Bn = [S([128, 2, 16, 16]) for _ in range(2)]
            with nc.allow_non_contiguous_dma(reason="small params"):
                for r_ in range(2):
                    for g4 in range(0, 16, 4):
                        c.dma('sp', lambda e: e.dma_start(out=Bn[0][0][:, r_, g4:g4 + 4, :], in_=b_re[r_, 2 * g4:2 * g4 + 8].rearrange("(gp gl) n p -> (gl n) gp p", gl=2)), pwrites=[Bn[0][1]])
                        c.dma('act', lambda e: e.dma_start(out=Bn[1][0][:, r_, g4:g4 + 4, :], in_=b_im[r_, 2 * g4:2 * g4 + 8].rearrange("(gp gl) n p -> (gl n) gp p", gl=2)), pwrites=[Bn[1][1]])
                Bn[0][1].seal(); Bn[1][1].seal()
            frb = fr[:].rearrange("p (r gp) -> p r gp", r=2).unsqueeze(3).broadcast_to([128, 2, 16, 16])
            fib = fi[:].rearrange("p (r gp) -> p r gp", r=2).unsqueeze(3).broadcast_to([128, 2, 16, 16])
            bbr, bbbr = S([128, 2, 16, 16]); bbi, bbbi = S([128, 2, 16, 16]); t5, bt5 = S([128, 2, 16, 16])
            c.op('dve', lambda e: e.tensor_tensor(out=bbr[:], in0=Bn[0][0][:], in1=frb, op=ALU.mult), reads=[Bn[0][1], bfr], writes=[bbbr])
            c.op('dve', lambda e: e.tensor_tensor(out=t5[:], in0=Bn[1][0][:], in1=fib, op=ALU.mult), reads=[Bn[1][1], bfi], writes=[bt5])
            c.op('dve', lambda e: e.tensor_tensor(out=bbr[:], in0=bbr[:], in1=t5[:], op=ALU.subtract), reads=[bbbr, bt5], writes=[bbbr])
            c.op('dve', lambda e: e.tensor_tensor(out=bbi[:], in0=Bn[1][0][:], in1=frb, op=ALU.mult), reads=[Bn[1][1], bfr], writes=[bbbi])
            c.op('dve', lambda e: e.tensor_tensor(out=t5[:], in0=Bn[0][0][:], in1=fib, op=ALU.mult), reads=[Bn[0][1], bfi], writes=[bt5])
            c.op('dve', lambda e: e.tensor_tensor(out=bbi[:], in0=bbi[:], in1=t5[:], op=ALU.add), reads=[bbbi, bt5], writes=[bbbi])
            BBm, bBBm = S([128, 2, 16, 2, 16], BF16)
            ptb, bptb = g.psb
            for part, (src, bsrc) in enumerate(((bbr, bbbr), (bbi, bbbi))):
                for gl in range(2):
                    c.op('dve', lambda e: e.tensor_scalar(out=BBm[:, :, :, gl, :], in0=src[:], scalar1=mk[:, gl:gl + 1], scalar2=None, op0=ALU.mult), reads=[bsrc, bmk, bBBm], writes=[bBBm])
                for r in range(2):
                    for cb in range(4):
                        c.op('pe', lambda e: e.transpose(out=ptb[:, 0:128], in_=BBm[:, r, 4 * cb:4 * cb + 4, :, :].rearrange("p a b c -> p (a b c)"), identity=g.identb[:]),
                             reads=[bBBm, g.b_identb], writes=[bptb])
                        c.op('act', lambda e: e.activation(out=WB[part][:, r, cb, :], in_=ptb[:, 0:128], func=AF.Copy), reads=[bptb], writes=[bWB])
            Cn = [S([128, 2, 4, 64]) for _ in range(2)]
            c.dma('sp', lambda e: e.dma_start(out=Cn[0][0][:], in_=c_re.rearrange("r (cb g8) p n -> (g8 p) r cb n", g8=8)), writes=[Cn[0][1]])
            c.dma('act', lambda e: e.dma_start(out=Cn[1][0][:], in_=c_im.rearrange("r (cb g8) p n -> (g8 p) r cb n", g8=8)), writes=[Cn[1][1]])
            Cd, bCd = S([128, 2, 4, 2, 64], BF16)
            mkb = mk[:].unsqueeze(1).unsqueeze(3).broadcast_to([128, 4, 2, 16])
            for part in range(2):
                sc = 1.0 if part == 0 else -1.0
                for x in range(2):
                    c.op('dve', lambda e: e.tensor_scalar(out=Cd[:, :, :, x, :], in0=Cn[part][0][:], scalar1=sc, scalar2=None, op0=ALU.mult), reads=[Cn[part][1], bCd], writes=[bCd])
                for r in range(2):
                    for cb in range(4):
                        c.op('pe', lambda e: e.transpose(out=ptb[:, 0:128], in_=Cd[:, r, cb, :, :].rearrange("p a b -> p (a b)"), identity=g.identb[:]),
                             reads=[bCd, g.b_identb], writes=[bptb])
                        c.op('dve', lambda e: e.tensor_tensor(out=WC[part][:, r, cb, :].rearrange("p (k a b) -> p k a b", k=4, a=2), in0=ptb[:, 0:128].rearrange("p (k a b) -> p k a b", k=4, a=2),
                                                              in1=mkb, op=ALU.mult), reads=[bptb, bmk], writes=[bWC])
            for part in range(2):
                c.op('act', lambda e: e.activation(out=WBx[part][64:128], in_=WB[part][64:128], func=AF.Copy), reads=[bWB], writes=[bWB])
                c.op('pool', lambda e: e.memset(WBx[part][64:96], 0.0), reads=[bWB], writes=[bWB])
                c.op('act', lambda e: e.activation(out=WCx[part][:], in_=WC[part][:, :, :, 64:128], func=AF.Copy), reads=[bWC], writes=[bWC])
                c.op('pool', lambda e: e.memset(WCx[part][:, :, :, 0:32], 0.0), reads=[bWC], writes=[bWC])
            barrier(c)
        if stage < 2:
            return
        with ExitStack() as es:
            def T(name, shape, dt):
                return es.enter_context(nc.sbuf_tensor(uniq(name), shape, dt))
            uf = T("s5_uf", [128, TB5 * 2], F32); buf_ = Buf()
            ub = [T("s5_ub%d" % r, [128, L], BF16) for r in range(2)]; bub = [Buf(), Buf()]
            Xa = [[T("s5_X%d%d" % (p, r), [128, L], BF16) for r in range(2)] for p in range(2)]
            bXa = [[Buf(), Buf()], [Buf(), Buf()]]
            tcos = T("s5_cos", [128, TB5], F32); tsin = T("s5_sin", [128, TB5], F32); btab = Buf()
            BUs = [T("s5_BU%d" % p, [128, TB5], F32) for p in range(2)]; bBUs = [Buf(), Buf()]
            t = [T("s5_w%d" % i, [128, TB5], F32) for i in range(4)]; bt = [Buf() for _ in range(4)]
            ini = T("s5_ini", [128, 4], F32); bini = Buf()
            yst = [T("s5_yst%d" % i, [128, 512], F32) for i in range(2)]; byst = [Buf(), Buf()]
            npz = 0
            for cb in range(4):
                for r in range(2):
                    for hh in range(L // (2 * TB5)):
                        nb = hh if r == 0 else L // (2 * TB5) - 1 - hh
                        c.dma('sp', lambda e: e.dma_start(out=uf[:], in_=PT[U0 + cb * 128:U0 + (cb + 1) * 128, nb * 2 * TB5:(nb + 1) * 2 * TB5]), reads=[bPT], writes=[buf_])
                        src = uf[:, ::-1] if r else uf[:, :]
                        c.op('act', lambda e: e.activation(out=ub[r][:, hh * 2 * TB5:(hh + 1) * 2 * TB5], in_=src, func=AF.Copy), reads=[buf_], writes=[bub[r]])
                for k in range(4):
                    gp = cb * 4 + k
                    for r in range(2):
                        col = r * 16 + gp
                        c.op('pool', lambda e: e.memset(tcos[:, 0:1], 1.0), writes=[btab])
                        c.op('pool', lambda e: e.memset(tsin[:, 0:1], 0.0), reads=[btab], writes=[btab])
                        n = 1
                        kk = 0
                        while n < TB5:
                            cr = pwc[:, kk, col:col + 1]; ci = pws[:, kk, col:col + 1]
                            c.op('dve', lambda e: e.tensor_scalar(out=t[0][:, 0:n], in0=tsin[:, 0:n], scalar1=ci, scalar2=None, op0=ALU.mult), reads=[btab, bpw], writes=[bt[0]])
                            c.op('dve', lambda e: e.tensor_scalar(out=t[1][:, 0:n], in0=tsin[:, 0:n], scalar1=cr, scalar2=None, op0=ALU.mult), reads=[btab, bpw], writes=[bt[1]])
                            c.op('dve', lambda e: e.scalar_tensor_tensor(out=tsin[:, n:2 * n], in0=tcos[:, 0:n], scalar=ci, in1=t[1][:, 0:n], op0=ALU.mult, op1=ALU.add),
                                 reads=[btab, bpw, bt[1]], writes=[btab])
                            c.op('dve', lambda e: e.scalar_tensor_tensor(out=tcos[:, n:2 * n], in0=tcos[:, 0:n], scalar=cr, in1=t[0][:, 0:n], op0=ALU.mult, op1=ALU.subtract),
                                 reads=[btab, bpw, bt[0]], writes=[btab])
                            n *= 2; kk += 1
                        cTB = pwc[:, kk, col:col + 1]; sTB = pws[:, kk, col:col + 1]
                        rho = mag[:, col:col + 1]
                        c.op('pool', lambda e: e.memset(ini[:], 0.0), writes=[bini])
                        for tb in range(NTB5):
                            ts0 = tb * TB5
                            for part in range(2):
                                for hf in range(TB5 // 512):
                                    ps, bps = g.ps[npz % 4]; npz += 1
                                    if k < 3:
                                        lh = WB[part][32 * k:32 * k + 32, r, cb, :]; rh = ub[r][32 * k:32 * k + 32, ts0 + hf * 512:ts0 + (hf + 1) * 512]
                                    else:
                                        lh = WBx[part][64:128, r, cb, :]; rh = ub[r][64:128, ts0 + hf * 512:ts0 + (hf + 1) * 512]
                                    c.op('pe', lambda e: e.matmul(ps[:, :], lhsT=lh, rhs=rh, start=True, stop=True),
                                         reads=[bWB, bub[r]], writes=[bps])
                                    c.op('act', lambda e: e.activation(out=BUs[part][:, hf * 512:(hf + 1) * 512], in_=ps[:, :], func=AF.Copy), reads=[bps], writes=[bBUs[part]])
                            c.op('dve', lambda e: e.tensor_tensor(out=t[0][:], in0=BUs[0][:], in1=tcos[:], op=ALU.mult), reads=[bBUs[0], btab], writes=[bt[0]])
                            c.op('dve', lambda e: e.tensor_tensor(out=t[1][:], in0=BUs[1][:], in1=tsin[:], op=ALU.mult), reads=[bBUs[1], btab], writes=[bt[1]])
                            c.op('dve', lambda e: e.tensor_tensor(out=t[0][:], in0=t[0][:], in1=t[1][:], op=ALU.add), reads=[bt[0], bt[1]], writes=[bt[0]])
                            c.op('pool', lambda e: e.tensor_tensor(out=t[2][:], in0=BUs[1][:], in1=tcos[:], op=ALU.mult), reads=[bBUs[1], btab], writes=[bt[2]])
                            c.op('dve', lambda e: e.tensor_tensor(out=t[3][:], in0=BUs[0][:], in1=tsin[:], op=ALU.mult), reads=[bBUs[0], btab], writes=[bt[3]])
                            c.op('dve', lambda e: e.tensor_tensor(out=t[2][:], in0=t[2][:], in1=t[3][:], op=ALU.subtract), reads=[bt[2], bt[3]], writes=[bt[2]])
                            c.op('dve', lambda e: e.tensor_tensor_scan(out=t[1][:], data0=rho.broadcast_to([128, TB5]), data1=t[0][:], initial=ini[:, 0:1], op0=ALU.mult, op1=ALU.add),
                                 reads=[bmag, bt[0], bini], writes=[bt[1]])
                            c.op('dve', lambda e: e.tensor_tensor_scan(out=t[3][:], data0=rho.broadcast_to([128, TB5]), data1=t[2][:], initial=ini[:, 1:2], op0=ALU.mult, op1=ALU.add),
                                 reads=[bmag, bt[2], bini], writes=[bt[3]])
                            if tb < NTB5 - 1:
                                xr = t[1][:, TB5 - 1:TB5]; xi = t[3][:, TB5 - 1:TB5]
                                c.op('dve', lambda e: e.tensor_scalar(out=ini[:, 2:3], in0=xi, scalar1=sTB, scalar2=None, op0=ALU.mult), reads=[bt[3], bpw], writes=[bini])
                                c.op('dve', lambda e: e.scalar_tensor_tensor(out=ini[:, 0:1], in0=xr, scalar=cTB, in1=ini[:, 2:3], op0=ALU.mult, op1=ALU.subtract), reads=[bt[1], bpw, bini], writes=[bini])
                                c.op('dve', lambda e: e.tensor_scalar(out=ini[:, 3:4], in0=xi, scalar1=cTB, scalar2=None, op0=ALU.mult), reads=[bt[3], bpw], writes=[bini])
                                c.op('dve', lambda e: e.scalar_tensor_tensor(out=ini[:, 1:2], in0=xr, scalar=sTB, in1=ini[:, 3:4], op0=ALU.mult, op1=ALU.add), reads=[bt[1], bpw, bini], writes=[bini])
                            if r == 0:
                                oslc = slice(ts0, ts0 + TB5)
                                xo_re = Xa[0][r][:, oslc]; xo_im = Xa[1][r][:, oslc]
                            else:
                                lo = L - ts0 - TB5
                                xo_re = Xa[0][r][:, lo:lo + TB5][:, ::-1]; xo_im = Xa[1][r][:, lo:lo + TB5][:, ::-1]
                            c.op('dve', lambda e: e.tensor_tensor(out=t[0][:], in0=t[1][:], in1=tcos[:], op=ALU.mult), reads=[bt[1], btab], writes=[bt[0]])
                            c.op('pool', lambda e: e.tensor_tensor(out=t[2][:], in0=t[3][:], in1=tsin[:], op=ALU.mult), reads=[bt[3], btab], writes=[bt[2]])
                            c.op('dve', lambda e: e.tensor_tensor(out=xo_re, in0=t[0][:], in1=t[2][:], op=ALU.subtract), reads=[bt[0], bt[2]], pwrites=[bXa[0][r]])
                            c.op('dve', lambda e: e.tensor_tensor(out=t[0][:], in0=t[1][:], in1=tsin[:], op=ALU.mult), reads=[bt[1], btab], writes=[bt[0]])
                            c.op('dve', lambda e: e.tensor_tensor(out=t[2][:], in0=t[3][:], in1=tcos[:], op=ALU.mult), reads=[bt[3], btab], writes=[bt[2]])
                            c.op('dve', lambda e: e.tensor_tensor(out=xo_im, in0=t[0][:], in1=t[2][:], op=ALU.add), reads=[bt[0], bt[2]], pwrites=[bXa[1][r]])
                        bXa[0][r].seal(); bXa[1][r].seal()
                    for it in range(L // 512):
                        ps, bps = g.ps[4 + it % 2]
                        i = 0
                        for r in range(2):
                            for part in range(2):
                                if k < 3:
                                    po = ps[32 * k:32 * k + 32, :]; lh = WC[part][:, r, cb, 32 * k:32 * k + 32]
                                else:
                                    po = ps[64:128, :]; lh = WCx[part][:, r, cb, :]
                                c.op('pe', lambda e: e.matmul(po, lhsT=lh, rhs=Xa[part][r][:, it * 512:(it + 1) * 512], start=(i == 0), stop=(i == 3)),
                                     reads=[bWC, bXa[part][r]], writes=[bps])
                                i += 1
                        ys, bys = yst[it % 2], byst[it % 2]
                        e0 = 32 * k if k < 3 else 64
                        c.op('act', lambda e: e.activation(out=ys[e0:32 * k + 32, :], in_=ps[e0:32 * k + 32, :], func=AF.Copy), reads=[bps], writes=[bys])
                        c.dma('sp', lambda e: e.dma_start(out=YT[cb * 128 + 32 * k:cb * 128 + 32 * k + 32, it * 512:(it + 1) * 512], in_=ys[32 * k:32 * k + 32, :]), reads=[bys], pwrites=[bYT])
            bYT.seal()
            barrier(c)
        if stage < 3:
            return
        with ExitStack() as es:
            def T(name, shape, dt):
                return es.enter_context(nc.sbuf_tensor(uniq(name), shape, dt))
            gw = T("s5_gw", [128, 4, 512], BF16); bgw = Buf()
            dcol = T("s5_dcol", [128, 4], F32); bdcol = Buf()
            gbc = T("s5_gbc", [128, 4], F32); bgbc = Buf()
            yt = [T("s5_yt%d" % i, [128, 4, 512], F32) for i in range(2)]; byt = [Buf(), Buf()]
            ut = [T("s5_ut%d" % i, [128, 4, 512], F32) for i in range(2)]; but = [Buf(), Buf()]
            sq = T("s5_sq", [128, 4, 512], F32); bsq = Buf()
            gy = T("s5_gy", [128, 4, 512], F32); bgy = Buf()
            gyb = T("s5_gyb", [128, 4, 512], BF16); bgyb = Buf()
            sg = [T("s5_sg%d" % i, [128, 512], F32) for i in range(2)]; bsg = [Buf(), Buf()]
            ob = [T("s5_ob%d" % i, [128, 4, 512], BF16) for i in range(2)]; bob = [Buf(), Buf()]
            c.dma('pool', lambda e: e.dma_start(out=gw[:], in_=glu_w.rearrange("(k p) f -> p k f", p=128)), writes=[bgw])
            with nc.allow_non_contiguous_dma(reason="small params"):
                c.dma('sp', lambda e: e.dma_start(out=dcol[:], in_=d_skip.rearrange("(k p) -> p k", p=128)), writes=[bdcol])
                c.dma('sp', lambda e: e.dma_start(out=gbc[:], in_=glu_b.rearrange("(k p) -> p k", p=128)), writes=[bgbc])
            GC = 1.5957691216057308
            for it in range(L // 512):
                yt_, byt_ = yt[it % 2], byt[it % 2]
                ut_, but_ = ut[it % 2], but[it % 2]
                ob_, bob_ = ob[it % 2], bob[it % 2]
                tsl = slice(it * 512, (it + 1) * 512)
                c.dma('sp', lambda e: e.dma_start(out=yt_[:], in_=YT[:, tsl].rearrange("(k p) t -> p k t", p=128)), reads=[bYT], writes=[byt_])
                c.dma('act', lambda e: e.dma_start(out=ut_[:], in_=PT[U0:U0 + 512, tsl].rearrange("(k p) t -> p k t", p=128)), reads=[bPT], writes=[but_])
                for k in range(4):
                    c.op('dve', lambda e: e.scalar_tensor_tensor(out=yt_[:, k, :], in0=ut_[:, k, :], scalar=dcol[:, k:k + 1], in1=yt_[:, k, :], op0=ALU.mult, op1=ALU.add),
                         reads=[but_, bdcol, byt_], writes=[byt_])
                c.op('act', lambda e: e.activation(out=sq[:], in_=yt_[:], func=AF.Square), reads=[byt_], writes=[bsq])
                c.op('dve', lambda e: e.tensor_scalar(out=sq[:], in0=sq[:], scalar1=0.044715, scalar2=1.0, op0=ALU.mult, op1=ALU.add), reads=[bsq], writes=[bsq])
                c.op('dve', lambda e: e.tensor_tensor(out=sq[:], in0=sq[:], in1=yt_[:], op=ALU.mult), reads=[bsq, byt_], writes=[bsq])
                c.op('act', lambda e: e.activation(out=sq[:], in_=sq[:], func=AF.Sigmoid, scale=GC), reads=[bsq], writes=[bsq])
                c.op('dve', lambda e: e.tensor_tensor(out=gy[:], in0=sq[:], in1=yt_[:], op=ALU.mult), reads=[bsq, byt_], writes=[bgy])
                c.op('act', lambda e: e.activation(out=gyb[:], in_=gy[:], func=AF.Copy), reads=[bgy], writes=[bgyb])
                for co in range(4):
                    ps, bps = g.ps[co % 4]
                    for k in range(4):
                        c.op('pe', lambda e: e.matmul(ps[:, :], lhsT=gw[:, k, co * 128:(co + 1) * 128], rhs=gyb[:, k, :], start=(k == 0), stop=(k == 3)),
                             reads=[bgw, bgyb], writes=[bps])
                    sg_, bsg_ = sg[co % 2], bsg[co % 2]
                    c.op('act', lambda e: e.activation(out=sg_[:], in_=ps[:, :], func=AF.Sigmoid, bias=gbc[:, co:co + 1], scale=1.0), reads=[bps, bgbc], writes=[bsg_])
                    c.op('dve', lambda e: e.tensor_tensor(out=ob_[:, co, :], in0=gy[:, co, :], in1=sg_[:], op=ALU.mult), reads=[bgy, bsg_, bob_], writes=[bob_])
                c.dma('sp', lambda e: e.dma_start(out=MT[512:1024, tsl].rearrange("(k p) t -> p k t", p=128), in_=ob_[:]), reads=[bob_], pwrites=[bMT])
            barrier(c)


def outproj_phase(c, g, MT, bMT, w_out, X, bX):
    nc = c.nc
    bXn = Buf('Xn')
    with ExitStack() as es:
        def T(name, shape, dt):
            return es.enter_context(nc.sbuf_tensor(uniq(name), shape, dt))
        wsb = T("op_w", [128, 8, D], BF16); bw = Buf()
        mt = [T("op_mt%d" % i, [128, 8, 512], BF16) for i in range(2)]; bmt = [Buf(), Buf()]
        xt = [T("op_xt%d" % i, [128, 4, D], F32) for i in range(2)]; bxt = [Buf(), Buf()]
        xo = [T("op_xo%d" % i, [128, 4, D], F32) for i in range(2)]; bxo = [Buf(), Buf()]
        for c0 in range(0, D, 512):
            c.dma('pool', lambda e: e.dma_start(out=wsb[:, :, c0:c0 + 512], in_=w_out[:, c0:c0 + 512].rearrange("(k p) f -> p k f", p=128)), pwrites=[bw])
        bw.seal()
        n = 0
        for it in range(L // 512):
            t0 = it * 512
            mt_, bmt_ = mt[it % 2], bmt[it % 2]
            xt_, bxt_ = xt[it % 2], bxt[it % 2]
            xo_, bxo_ = xo[it % 2], bxo[it % 2]
            c.dma('sp', lambda e: e.dma_start(out=mt_[:], in_=MT[:, t0:t0 + 512].rearrange("(k p) t -> p k t", p=128)), reads=[bMT], writes=[bmt_])
            c.dma('act', lambda e: e.dma_start(out=xt_[:], in_=X[t0:t0 + 512, :].rearrange("(j p) d -> p j d", p=128)), reads=[bX], writes=[bxt_])
            for j in range(4):
                for dh in range(2):
                    ps, bps = g.ps[n % 4]; n += 1
                    for k in range(8):
                        c.op('pe', lambda e: e.matmul(ps[:, :], lhsT=mt_[:, k, j * 128:(j + 1) * 128], rhs=wsb[:, k, dh * 512:(dh + 1) * 512], start=(k == 0), stop=(k == 7)),
                             reads=[bmt_, bw], writes=[bps])
                    c.op('dve', lambda e: e.tensor_tensor(out=xo_[:, j, dh * 512:(dh + 1) * 512], in0=ps[:, :], in1=xt_[:, j, dh * 512:(dh + 1) * 512], op=ALU.add),
                         reads=[bps, bxt_, bxo_], writes=[bxo_])
            c.dma('sp', lambda e: e.dma_start(out=X[t0:t0 + 512, :].rearrange("(j p) d -> p j d", p=128), in_=xo_[:]), reads=[bxo_], pwrites=[bXn])
        bXn.seal()
        barrier(c)
    return bXn


def t5_onehot():
    half = 16; max_exact = 8
    rel = np.arange(-255, 256)
    n = np.abs(rel)
    nf = np.maximum(n, 1).astype(np.float32)
    large = max_exact + (np.log(nf / np.float32(max_exact)) / np.float32(math.log(128 / max_exact)) * np.float32(half - max_exact)).astype(np.int32)
    large = np.minimum(large, half - 1)
    b = np.where(rel > 0, half, 0) + np.where(n < max_exact, n, large)
    oh = np.zeros((32, 512), np.float32)
    oh[b, np.arange(511)] = 1.0
    return oh


def attn_phase(c, g, PV, bPV, q_gain, k_gain, c_lambda, out_gain, rel_bias, onehot, layer_idx, QKT, bQKT, FV, bFV, MT, bMT, stage=3):
    nc = c.nc
    lam_init = 0.8 - 0.6 * math.exp(-0.3 * layer_idx)
    ptb, bptb = g.psb
    with ExitStack() as es:
        def T(name, shape, dt):
            return es.enter_context(nc.sbuf_tensor(uniq(name), shape, dt))
        g64 = T("at_g64", [128, 2, 64], F32); bg64 = Buf()
        gQK = T("at_gQK", [128, 16, 64], F32); bgQK = Buf()
        xq = [T("at_xq%d" % i, [128, 1024], BF16) for i in range(2)]; bxq = [Buf(), Buf()]
        sq = T("at_sq", [128, 1024], F32); bsq = Buf()
        ss = [T("at_ss%d" % i, [128, 16], F32) for i in range(2)]; bss = [Buf(), Buf()]
        xn = T("at_xn", [128, 1024], F32); bxn = Buf()
        xb = [T("at_xb%d" % i, [128, 1024], BF16) for i in range(2)]; bxb = [Buf(), Buf()]
        st = [T("at_st%d" % i, [128, 8, 512], BF16) for i in range(2)]; bst = [Buf(), Buf()]
        c.dma('sp', lambda e: e.dma_start(out=g64[:, 0, :], in_=q_gain.partition_broadcast(128)), pwrites=[bg64])
        c.dma('sp', lambda e: e.dma_start(out=g64[:, 1, :], in_=k_gain.partition_broadcast(128)), pwrites=[bg64])
        bg64.seal()
        c.op('dve', lambda e: e.tensor_scalar(out=gQK[:, 0:8, :], in0=g64[:, 0:1, :].broadcast_to([128, 8, 64]), scalar1=0.125, scalar2=None, op0=ALU.mult), reads=[bg64], writes=[bgQK])
        c.op('dve', lambda e: e.tensor_copy(out=gQK[:, 8:16, :], in_=g64[:, 1:2, :].broadcast_to([128, 8, 64])), reads=[bg64, bgQK], writes=[bgQK])
        for i in range(NT):
            xq_, bxq_ = xq[i % 2], bxq[i % 2]
            ss_, bss_ = ss[i % 2], bss[i % 2]
            xb_, bxb_ = xb[i % 2], bxb[i % 2]
            st_, bst_ = st[(i // 4) % 2], bst[(i // 4) % 2]
            c.dma('sp', lambda e: e.dma_start(out=xq_[:], in_=PV[i * 128:(i + 1) * 128, 0:1024]), reads=[bPV], writes=[bxq_])
            c.op('act', lambda e: e.activation(out=sq[:], in_=xq_[:], func=AF.Square), reads=[bxq_], writes=[bsq])
            c.op('dve', lambda e: e.tensor_reduce(out=ss_[:], in_=sq[:].rearrange("p (a d) -> p a d", d=64), axis=AX.X, op=ALU.add), reads=[bsq], writes=[bss_])
            c.op('dve', lambda e: e.tensor_scalar(out=ss_[:], in0=ss_[:], scalar1=1.0 / 64, scalar2=1e-6, op0=ALU.mult, op1=ALU.add), reads=[bss_], writes=[bss_])
            c.op('pool', lambda e: e.tensor_tensor(out=ss_[:], in0=ss_[:], in1=g.neghalf[:, 0:1].broadcast_to([128, 16]), op=ALU.pow), reads=[bss_, g.b_neghalf], writes=[bss_])
            c.op('dve', lambda e: e.tensor_tensor(out=xn[:].rearrange("p (a d) -> p a d", d=64), in0=xq_[:].rearrange("p (a d) -> p a d", d=64),
                                                  in1=ss_[:].unsqueeze(2).broadcast_to([128, 16, 64]), op=ALU.mult), reads=[bxq_, bss_], writes=[bxn])
            c.op('dve', lambda e: e.tensor_tensor(out=xb_[:], in0=xn[:], in1=gQK[:].rearrange("p a d -> p (a d)"), op=ALU.mult), reads=[bxn, bgQK], writes=[bxb_])
            for a in range(8):
                c.op('pe', lambda e: e.transpose(out=ptb[:, a * 128:(a + 1) * 128], in_=xb_[:, a * 128:(a + 1) * 128], identity=g.identb[:]), reads=[bxb_, g.b_identb], writes=[bptb])
            c.op('act', lambda e: e.activation(out=st_[:, :, (i % 4) * 128:(i % 4 + 1) * 128], in_=ptb[:, :].rearrange("p (a s) -> p a s", a=8), func=AF.Copy), reads=[bptb], writes=[bst_])
            if i % 4 == 3:
                t0 = (i // 4) * 512
                for a in range(8):
                    c.dma('sp' if a % 2 else 'act', lambda e: e.dma_start(out=QKT[a, :, t0:t0 + 512], in_=st_[:, a, :]), reads=[bst_], pwrites=[bQKT])
        bQKT.seal()
        barrier(c)
    if stage < 2:
        return
    with ExitStack() as es:
        def T(name, shape, dt):
            return es.enter_context(nc.sbuf_tensor(uniq(name), shape, dt))
        KT = T("at_KT", [128, L], BF16); bKT = Buf()
        Va = T("at_Va", [128, 64, 130], BF16); bVa = Buf()
        QT = [T("at_QT%d" % i, [128, 512], BF16) for i in range(2)]; bQT = [Buf(), Buf()]
        Pt = [T("at_P%d" % i, [128, 512], BF16) for i in range(3)]; bPt = [Buf() for _ in range(3)]
        tmp = [T("at_tmp%d" % i, [128, 512], F32) for i in range(2)]; btmp = [Buf(), Buf()]
        biasT = T("at_bias", [128, 4, 3, 128], F32); bbias = Buf()
        hank = T("at_hank", [128, 128], F32); bhank = Buf()
        cfar = T("at_cfar", [128, 4, 2], F32); bcfar = Buf()
        tab = T("at_tab", [32, 4], F32); btab = Buf()
        oh = T("at_oh", [32, 512], F32); boh = Buf()
        fv = T("at_fv", [4, 512], F32); bfv = Buf()
        lamt = T("at_lamt", [128, 4, 64], F32); blamt = Buf()
        lam = T("at_lam", [128, 8], F32); blam = Buf()
        gO = T("at_gO", [128, 128], F32); bgO = Buf()
        rs = [T("at_rs%d" % i, [128, 4], F32) for i in range(2)]; brs = [Buf(), Buf()]
        t1 = T("at_t1", [128, 128], F32); bt1 = Buf()
        w_ = T("at_w", [128, 128], F32); bw_ = Buf()
        junk = T("at_junk", [128, 128], F32); bjunk = Buf()
        wb = [T("at_wb%d" % i, [128, 128], BF16) for i in range(2)]; bwb = [Buf(), Buf()]
        ost = [T("at_ost%d" % i, [128, 512], BF16) for i in range(2)]; bost = [Buf(), Buf()]
        c.dma('sp', lambda e: e.dma_start(out=lamt[:].rearrange("p a d -> p (a d)"), in_=c_lambda.rearrange("a d -> (a d)").partition_broadcast(128)), writes=[blamt])
        c.op('dve', lambda e: e.tensor_tensor(out=lamt[:, 0, :], in0=lamt[:, 0, :], in1=lamt[:, 1, :], op=ALU.mult), reads=[blamt], writes=[blamt])
        c.op('dve', lambda e: e.tensor_tensor(out=lamt[:, 2, :], in0=lamt[:, 2, :], in1=lamt[:, 3, :], op=ALU.mult), reads=[blamt], writes=[blamt])
        c.op('dve', lambda e: e.tensor_reduce(out=lam[:, 0:1], in_=lamt[:, 0, :], axis=AX.X, op=ALU.add), reads=[blamt], writes=[blam])
        c.op('dve', lambda e: e.tensor_reduce(out=lam[:, 1:2], in_=lamt[:, 2, :], axis=AX.X, op=ALU.add), reads=[blamt, blam], writes=[blam])
        c.op('act', lambda e: e.activation(out=lam[:, 2:4], in_=lam[:, 0:2], func=AF.Exp), reads=[blam], writes=[blam])
        c.op('dve', lambda e: e.tensor_tensor(out=lam[:, 4:5], in0=lam[:, 3:4], in1=lam[:, 2:3], op=ALU.subtract), reads=[blam], writes=[blam])
        c.op('dve', lambda e: e.tensor_scalar(out=lam[:, 4:5], in0=lam[:, 4:5], scalar1=-lam_init, scalar2=None, op0=ALU.add), reads=[blam], writes=[blam])
        c.dma('sp', lambda e: e.dma_start(out=gO[:], in_=out_gain.partition_broadcast(128)), writes=[bgO])
        c.op('dve', lambda e: e.tensor_scalar(out=gO[:], in0=gO[:], scalar1=1.0 - lam_init, scalar2=None, op0=ALU.mult), reads=[bgO], writes=[bgO])
        c.dma('sp', lambda e: e.dma_start(out=tab[:], in_=rel_bias), writes=[btab])
        c.dma('act', lambda e: e.dma_start(out=oh[:], in_=onehot), writes=[boh])
        ps6, bps6 = g.ps[6]
        c.op('pe', lambda e: e.matmul(ps6[0:4, :], lhsT=tab[:, :], rhs=oh[:, :], start=True, stop=True), reads=[btab, boh], writes=[bps6])
        c.op('dve', lambda e: e.tensor_copy(out=fv[:], in_=ps6[0:4, :]), reads=[bps6], writes=[bfv])
        c.dma('sp', lambda e: e.dma_start(out=FV, in_=fv[:]), reads=[bfv], writes=[bFV])
        for h in range(4):
            for o in (-1, 0, 1):
                off = h * 512 + 128 * o + 128
                src = bass.AP(FV.tensor, off, [[1, 128], [1, 128]])
                c.dma('sp', lambda e: e.dma_start(out=hank[:], in_=src), reads=[bFV], writes=[bhank])
                c.op('dve', lambda e: e.tensor_copy(out=biasT[:, h, o + 1, :], in_=hank[:, ::-1]), reads=[bhank, bbias], writes=[bbias])
            c.dma('sp', lambda e: e.dma_start(out=cfar[:, h, 0:1], in_=bass.AP(FV.tensor, h * 512 + 0, [[0, 128], [1, 1]])), reads=[bFV], pwrites=[bcfar])
            c.dma('sp', lambda e: e.dma_start(out=cfar[:, h, 1:2], in_=bass.AP(FV.tensor, h * 512 + 510, [[0, 128], [1, 1]])), reads=[bFV], pwrites=[bcfar])
        bcfar.seal()
        ones_col_done = False
        nS = 0; nP = 0; nq = 0; ntmp = 0; nout = 0
        for h in range(4):
            c.dma('sp', lambda e: e.dma_start(out=KT[:], in_=QKT[4 + h, :, :]), reads=[bQKT], writes=[bKT])
            for half in range(2):
                c.dma('act', lambda e: e.dma_start(out=Va[:, half * 32:(half + 1) * 32, 0:128], in_=PV[half * 4096:(half + 1) * 4096, 1024 + h * 128:1024 + (h + 1) * 128].rearrange("(b p) d -> p b d", p=128)),
                      reads=[bPV], writes=[bVa])
            c.op('pool', lambda e: e.memset(Va[:, :, 128:129], 1.0), reads=[bVa], writes=[bVa])
            for qt in range(16):
                QT_, bQT_ = QT[nq % 2], bQT[nq % 2]; nq += 1
                c.dma('sp', lambda e: e.dma_start(out=QT_[:], in_=QKT[h, :, qt * 512:(qt + 1) * 512]), reads=[bQKT], writes=[bQT_])
                steps = [(comp, kb) for comp in range(2) for kb in range(64)]
                Sbank = {}

                def emit_S(i):
                    comp, kb = steps[i]
                    S, bS = g.ps[i % 3]
                    c.op('pe', lambda e: e.matmul(S[:, :], lhsT=KT[64 * comp:64 * comp + 64, kb * 128:(kb + 1) * 128], rhs=QT_[64 * comp:64 * comp + 64, :], start=True, stop=True),
                         reads=[bKT, bQT_], writes=[bS])
                emit_S(0); emit_S(1)
                for i, (comp, kb) in enumerate(steps):
                    S, bS = g.ps[i % 3]
                    P_, bP_ = Pt[nP % 3], bPt[nP % 3]; nP += 1
                    near = (4 * qt - 1 <= kb <= 4 * qt + 4)
                    if not near:
                        col = cfar[:, h, 0:1] if kb < 4 * qt else cfar[:, h, 1:2]
                        c.op('act', lambda e: e.activation(out=P_[:], in_=S[:, :], func=AF.Exp, bias=col, scale=1.0), reads=[bS, bcfar], writes=[bP_])
                    else:
                        tm, btm = tmp[ntmp % 2], btmp[ntmp % 2]; ntmp += 1
                        for qs in range(4):
                            o = kb - (4 * qt + qs)
                            sl = slice(qs * 128, (qs + 1) * 128)
                            if abs(o) <= 1:
                                c.op('dve', lambda e: e.tensor_tensor(out=tm[:, sl], in0=S[:, sl], in1=biasT[:, h, o + 1, :], op=ALU.add), reads=[bS, bbias, btm], writes=[btm])
                            else:
                                col = cfar[:, h, 0:1] if o < 0 else cfar[:, h, 1:2]
                                c.op('dve', lambda e: e.tensor_scalar(out=tm[:, sl], in0=S[:, sl], scalar1=col, scalar2=None, op0=ALU.add), reads=[bS, bcfar, btm], writes=[btm])
                        c.op('act', lambda e: e.activation(out=P_[:], in_=tm[:], func=AF.Exp), reads=[btm], writes=[bP_])
                    if i + 2 < len(steps):
                        emit_S(i + 2)
                    for qs in range(4):
                        a = comp * 4 + qs
                        acc, bacc = g.ps[3 + a // 3]
                        c0 = (a % 3) * 130
                        first = (kb == 0) and ((comp == 0 and a in (0, 3)) or (comp == 1 and a == 6))
                        c.op('pe', lambda e: e.matmul(acc[:, c0:c0 + 129], lhsT=P_[:, qs * 128:(qs + 1) * 128], rhs=Va[:, kb, 0:129], start=first, stop=(kb == 63), skip_group_check=True),
                             reads=[bP_, bVa], writes=[bacc])
                os_, bos_ = ost[nout % 2], bost[nout % 2]; nout += 1
                for qs in range(4):
                    a0 = qs; a1 = 4 + qs
                    acc0, bacc0 = g.ps[3 + a0 // 3]; o0 = (a0 % 3) * 130
                    acc1, bacc1 = g.ps[3 + a1 // 3]; o1 = (a1 % 3) * 130
                    rs_, brs_ = rs[qs % 2], brs[qs % 2]
                    wb_, bwb_ = wb[qs % 2], bwb[qs % 2]
                    c.op('dve', lambda e: e.reciprocal(out=rs_[:, 0:1], in_=acc0[:, o0 + 128:o0 + 129]), reads=[bacc0], writes=[brs_])
                    c.op('dve', lambda e: e.reciprocal(out=rs_[:, 1:2], in_=acc1[:, o1 + 128:o1 + 129]), reads=[bacc1, brs_], writes=[brs_])
                    c.op('dve', lambda e: e.tensor_tensor(out=rs_[:, 1:2], in0=rs_[:, 1:2], in1=lam[:, 4:5], op=ALU.mult), reads=[brs_, blam], writes=[brs_])
                    c.op('dve', lambda e: e.tensor_scalar(out=t1[:], in0=acc1[:, o1:o1 + 128], scalar1=rs_[:, 1:2], scalar2=None, op0=ALU.mult), reads=[bacc1, brs_], writes=[bt1])
                    c.op('dve', lambda e: e.scalar_tensor_tensor(out=w_[:], in0=acc0[:, o0:o0 + 128], scalar=rs_[:, 0:1], in1=t1[:], op0=ALU.mult, op1=ALU.add), reads=[bacc0, brs_, bt1], writes=[bw_])
                    c.op('dve', lambda e: e.scalar_tensor_tensor(out=junk[:], in0=w_[:], scalar=1.0, in1=w_[:], op0=ALU.mult, op1=ALU.mult, accum_out=rs_[:, 2:3]), reads=[bw_, brs_], writes=[bjunk, brs_])
                    c.op('dve', lambda e: e.tensor_scalar(out=rs_[:, 2:3], in0=rs_[:, 2:3], scalar1=1.0 / 128, scalar2=1e-6, op0=ALU.mult, op1=ALU.add), reads=[brs_], writes=[brs_])
                    c.op('pool', lambda e: e.tensor_tensor(out=rs_[:, 2:3], in0=rs_[:, 2:3], in1=g.neghalf[:, 0:1], op=ALU.pow), reads=[brs_, g.b_neghalf], writes=[brs_])
                    c.op('dve', lambda e: e.scalar_tensor_tensor(out=wb_[:], in0=w_[:], scalar=rs_[:, 2:3], in1=gO[:], op0=ALU.mult, op1=ALU.mult), reads=[bw_, brs_, bgO], writes=[bwb_])
                    c.op('pe', lambda e: e.transpose(out=ptb[:, qs * 128:(qs + 1) * 128], in_=wb_[:], identity=g.identb[:]), reads=[bwb_, g.b_identb], writes=[bptb])
                c.op('act', lambda e: e.activation(out=os_[:], in_=ptb[:, 0:512], func=AF.Copy), reads=[bptb], writes=[bos_])
                c.dma('sp', lambda e: e.dma_start(out=MT[h * 128:(h + 1) * 128, qt * 512:(qt + 1) * 512], in_=os_[:]), reads=[bos_], pwrites=[bMT])
        barrier(c)


GTB = 512


def gated_norm_finalize(c, g, OA, bOAs, PV, bPV, gcol0, gain_ap, MT, bMT, row0, pfx, OA2=None, bOA2s=()):
    nc = c.nc
    with ExitStack() as es:
        def T(name, shape, dt):
            return es.enter_context(nc.sbuf_tensor(uniq(pfx + name), shape, dt))
        gA = T("gA", [128, 128], F32); bgA = Buf()
        oa = [T("oa%d" % i, [128, 512], F32) for i in range(2)]; boa = [Buf(), Buf()]
        oa2 = [T("oa2%d" % i, [128, 512], F32) for i in range(2)]; boa2 = [Buf(), Buf()]
        ga = [T("ga%d" % i, [128, 512], BF16) for i in range(2)]; bga = [Buf(), Buf()]
        sq = T("sq", [128, 512], F32); bsq = Buf()
        ssq = [T("ssq%d" % i, [128, 4], F32) for i in range(2)]; bssq = [Buf(), Buf()]
        sg = T("sg", [128, 512], F32); bsg = Buf()
        t1 = T("t1", [128, 512], F32); bt1 = Buf()
        ob = [T("ob%d" % i, [128, 512], BF16) for i in range(2)]; bob = [Buf(), Buf()]
        mt = [T("mt%d" % i, [128, 4, 512], BF16) for i in range(2)]; bmt = [Buf(), Buf()]
        c.dma('sp', lambda e: e.dma_start(out=gA[:], in_=gain_ap.partition_broadcast(128)), writes=[bgA])
        ptb, bptb = g.psb
        for i in range(NT):
            oa_, boa_ = oa[i % 2], boa[i % 2]
            ga_, bga_ = ga[i % 2], bga[i % 2]
            ss_, bss_ = ssq[i % 2], bssq[i % 2]
            ob_, bob_ = ob[i % 2], bob[i % 2]
            mt_, bmt_ = mt[(i // 4) % 2], bmt[(i // 4) % 2]
            c.dma('sp', lambda e: e.dma_start(out=oa_[:], in_=OA[i * 128:(i + 1) * 128, :]), reads=bOAs, writes=[boa_])
            c.dma('act', lambda e: e.dma_start(out=ga_[:], in_=PV[i * 128:(i + 1) * 128, gcol0:gcol0 + 512]), reads=[bPV], writes=[bga_])
            if OA2 is not None:
                o2_, bo2_ = oa2[i % 2], boa2[i % 2]
                c.dma('act', lambda e: e.dma_start(out=o2_[:], in_=OA2[i * 128:(i + 1) * 128, :]), reads=list(bOA2s), writes=[bo2_])
                c.op('dve', lambda e: e.tensor_tensor(out=oa_[:], in0=oa_[:], in1=o2_[:], op=ALU.add), reads=[boa_, bo2_], writes=[boa_])
            c.op('act', lambda e: e.activation(out=sq[:], in_=oa_[:], func=AF.Square), reads=[boa_], writes=[bsq])
            c.op('dve', lambda e: e.tensor_reduce(out=ss_[:], in_=sq[:].rearrange("p (h d) -> p h d", h=4), axis=AX.X, op=ALU.add), reads=[bsq], writes=[bss_])
            c.op('dve', lambda e: e.tensor_scalar(out=ss_[:], in0=ss_[:], scalar1=1.0 / 128, scalar2=1e-6, op0=ALU.mult, op1=ALU.add), reads=[bss_], writes=[bss_])
            c.op('pool', lambda e: e.tensor_tensor(out=ss_[:], in0=ss_[:], in1=g.neghalf[:, 0:1].broadcast_to([128, 4]), op=ALU.pow), reads=[bss_, g.b_neghalf], writes=[bss_])
            c.op('act', lambda e: e.activation(out=sg[:], in_=ga_[:], func=AF.Silu), reads=[bga_], writes=[bsg])
            c.op('dve', lambda e: e.tensor_tensor(out=t1[:].rearrange("p (h d) -> p h d", h=4), in0=oa_[:].rearrange("p (h d) -> p h d", h=4),
                                                  in1=ss_[:].unsqueeze(2).broadcast_to([128, 4, 128]), op=ALU.mult), reads=[boa_, bss_], writes=[bt1])
            c.op('dve', lambda e: e.tensor_tensor(out=t1[:].rearrange("p (h d) -> p h d", h=4), in0=t1[:].rearrange("p (h d) -> p h d", h=4),
                                                   in1=gA[:].unsqueeze(1).broadcast_to([128, 4, 128]), op=ALU.mult), reads=[bt1, bgA], writes=[bt1])
            c.op('dve', lambda e: e.tensor_tensor(out=ob_[:], in0=t1[:], in1=sg[:], op=ALU.mult), reads=[bt1, bsg], writes=[bob_])
            for k in range(4):
                c.op('pe', lambda e: e.transpose(out=ptb[:, k * 128:(k + 1) * 128], in_=ob_[:, k * 128:(k + 1) * 128], identity=g.identb[:]),
                     reads=[bob_, g.b_identb], writes=[bptb])
            c.op('act', lambda e: e.activation(out=mt_[:, :, (i % 4) * 128:(i % 4 + 1) * 128], in_=ptb[:, 0:512].rearrange("p (k s) -> p k s", k=4), func=AF.Copy),
                 reads=[bptb], writes=[bmt_])
            if i % 4 == 3:
                t0 = (i // 4) * 512
                c.dma('sp', lambda e: e.dma_start(out=MT[row0:row0 + 512, t0:t0 + 512].rearrange("(k p) t -> p k t", p=128), in_=mt_[:]), reads=[bmt_], pwrites=[bMT])
        barrier(c)


def gdn_phase(c, g, PT, bPT, PV, bPV, conv_w, a_log, dt_bias, out_gain, GQ, bGQ, GR, bGR, OD, bODs, OD2, bOD2s, MT, bMT, stage=4):
    nc = c.nc
    ptb, bptb = g.psb
    NB = TBK
    with ExitStack() as es:
        def T(name, shape, dt):
            return es.enter_context(nc.sbuf_tensor(uniq(name), shape, dt))
        cw = T("gd_cw", [128, 12, 5], F32); bcw = Buf()
        onesb = T("gd_onesb", [128, 128], BF16); bonesb = Buf()
        xin = [T("gd_xin%d" % i, [128, NB + 4], F32) for i in range(2)]; bxin = [Buf(), Buf()]
        y = T("gd_y", [128, NB], F32); by = Buf()
        s = T("gd_s", [128, NB], F32); bs = Buf()
        sqb = T("gd_sqb", [128, NB], BF16); bsqb = Buf()
        rst = T("gd_rst", [128, NB], F32); brst = Buf()
        ob = [T("gd_ob%d" % i, [128, NB], BF16) for i in range(2)]; bob = [Buf(), Buf()]
        with nc.allow_non_contiguous_dma(reason="small params"):
            for j in range(5):
                c.dma('sp', lambda e: e.dma_start(out=cw[:, :, j], in_=conv_w[j, :].rearrange("(k p) -> p k", p=128)), pwrites=[bcw])
        bcw.seal()
        c.op('pool', lambda e: e.memset(onesb[:], 1.0), writes=[bonesb])
        n = 0
        for cbk in range(12):
            for tb in range(L // NB):
                x_, bx_ = xin[n % 2], bxin[n % 2]
                o_, bo_ = ob[n % 2], bob[n % 2]
                n += 1
                t0 = tb * NB
                lo = max(t0 - 2, 0); hi = min(t0 + NB + 2, L)
                if tb == 0:
                    c.op('pool', lambda e: e.memset(x_[:, 0:2], 0.0), writes=[bx_])
                if tb == L // NB - 1:
                    c.op('pool', lambda e: e.memset(x_[:, NB + 2:NB + 4], 0.0), writes=[bx_])
                c.dma('sp', lambda e: e.dma_start(out=x_[:, lo - (t0 - 2):hi - (t0 - 2)], in_=PT[cbk * 128:(cbk + 1) * 128, lo:hi]), reads=[bPT, bx_], writes=[bx_])
                c.op('dve', lambda e: e.tensor_scalar(out=y[:], in0=x_[:, 0:NB], scalar1=cw[:, cbk, 0:1], scalar2=None, op0=ALU.mult), reads=[bx_, bcw], writes=[by])
                for j in range(1, 5):
                    c.op('dve', lambda e: e.scalar_tensor_tensor(out=y[:], in0=x_[:, j:j + NB], scalar=cw[:, cbk, j:j + 1], in1=y[:], op0=ALU.mult, op1=ALU.add),
                         reads=[bx_, bcw, by], writes=[by])
                c.op('act', lambda e: e.activation(out=s[:], in_=y[:], func=AF.Silu), reads=[by], writes=[bs])
                if cbk < 8:
                    c.op('act', lambda e: e.activation(out=sqb[:], in_=s[:], func=AF.Square), reads=[bs], writes=[bsqb])
                    for hf in range(NB // 512):
                        ps, bps = g.ps[hf % 4]
                        c.op('pe', lambda e: e.matmul(ps[:, :], lhsT=onesb[:], rhs=sqb[:, hf * 512:(hf + 1) * 512], start=True, stop=True), reads=[bonesb, bsqb], writes=[bps])
                        c.op('dve', lambda e: e.tensor_scalar(out=rst[:, hf * 512:(hf + 1) * 512], in0=ps[:, :], scalar1=1e-6, scalar2=None, op0=ALU.add), reads=[bps, brst], writes=[brst])
                    c.op('act', lambda e: e.activation(out=rst[:], in_=rst[:], func=AF.Ln), reads=[brst], writes=[brst])
                    c.op('act', lambda e: e.activation(out=rst[:], in_=rst[:], func=AF.Exp, scale=-0.5), reads=[brst], writes=[brst])
                    sc = (128.0 ** -0.5) if cbk < 4 else 1.0
                    c.op('dve', lambda e: e.scalar_tensor_tensor(out=o_[:], in0=s[:], scalar=sc, in1=rst[:], op0=ALU.mult, op1=ALU.mult), reads=[bs, brst], writes=[bo_])
                else:
                    c.op('act', lambda e: e.activation(out=o_[:], in_=s[:], func=AF.Copy), reads=[bs], writes=[bo_])
                c.dma('act', lambda e: e.dma_start(out=GQ[cbk, :, t0:t0 + NB], in_=o_[:]), reads=[bo_], pwrites=[bGQ])
        bGQ.seal()
        barrier(c)
    if stage < 2:
        return
    with ExitStack() as es0:
        def T0(name, shape, dt):
            return es0.enter_context(nc.sbuf_tensor(uniq(name), shape, dt))
        NQ = 5
        cols = [T0("gd_cols%d" % d, [128, 64, 4 * NQ], F32) for d in range(2)]; bcols = [Buf(), Buf()]
        sel = T0("gd_sel", [4, 4, 128], F32); bsel = Buf()
        with ExitStack() as es:
            def T(name, shape, dt):
                return es.enter_context(nc.sbuf_tensor(uniq(name), shape, dt))
            GP = 2048
            ar = T("gd_ar", [4, GP], F32); bar_ = Buf()
            br = T("gd_br", [4, GP], F32); bbr = Buf()
            w1 = T("gd_w1", [4, GP], F32); bw1 = Buf()
            w2 = T("gd_w2", [4, GP], F32); bw2 = Buf()
            gam = T("gd_gam", [4, GP], F32); bet = T("gd_bet", [4, GP], F32); egam = T("gd_egam", [4, GP], F32); brw = Buf()
            q3 = T("gd_q3", [4, GP], F32); bq3 = Buf()
            q4 = T("gd_q4", [4, GP], F32); bq4 = Buf()
            q5 = T("gd_q5", [4, GP], F32); bq5 = Buf()
            msk = T("gd_msk", [4, GP], F32); bmsk = Buf()
            pc = T("gd_pc", [4, 4], F32); bpc = Buf()
            c.op('pool', lambda e: e.memset(msk[:], 1.0), writes=[bmsk])
            c.op('pool', lambda e: e.memset(msk[:].rearrange("p (c j) -> p c j", j=64)[:, :, 0:1], 0.0), reads=[bmsk], writes=[bmsk])
            c.op('pool', lambda e: e.memset(sel[:], 0.0), writes=[bsel])
            c.op('pool', lambda e: e.affine_select(out=sel[:], in_=sel[:], pattern=[[-1, 4], [0, 128]], compare_op=ALU.not_equal, fill=1.0, base=0, channel_multiplier=1),
                 reads=[bsel], writes=[bsel])
            for d in range(2):
                with nc.allow_non_contiguous_dma(reason="small params"):
                    c.dma('sp', lambda e: e.dma_start(out=pc[:, 0:1], in_=dt_bias[d, :].rearrange("(h o) -> h o", o=1)), reads=[bpc], writes=[bpc])
                    c.dma('sp', lambda e: e.dma_start(out=pc[:, 1:2], in_=a_log[d, :].rearrange("(h o) -> h o", o=1)), reads=[bpc], writes=[bpc])
                c.op('act', lambda e: e.activation(out=pc[:, 2:3], in_=pc[:, 1:2], func=AF.Exp), reads=[bpc], writes=[bpc])
                c.op('dve', lambda e: e.tensor_scalar(out=pc[:, 2:3], in0=pc[:, 2:3], scalar1=-1.0, scalar2=None, op0=ALU.mult), reads=[bpc], writes=[bpc])
                for tp in range(L // GP):
                    nbp = tp if d == 0 else L // GP - 1 - tp
                    c.dma('sp', lambda e: e.dma_start(out=ar[:], in_=PT[1536 + 4 * d:1540 + 4 * d, nbp * GP:(nbp + 1) * GP]), reads=[bPT, bar_], writes=[bar_])
                    c.dma('act', lambda e: e.dma_start(out=br[:], in_=PT[1544 + 4 * d:1548 + 4 * d, nbp * GP:(nbp + 1) * GP]), reads=[bPT, bbr], writes=[bbr])
                    asrc = ar[:, ::-1] if d else ar[:, :]
                    bsrc = br[:, ::-1] if d else br[:, :]
                    c.op('dve', lambda e: e.tensor_scalar(out=w1[:], in0=asrc, scalar1=pc[:, 0:1], scalar2=None, op0=ALU.add), reads=[bar_, bpc], writes=[bw1])
                    c.op('dve', lambda e: e.tensor_scalar(out=w2[:], in0=w1[:], scalar1=-1.0, scalar2=None, op0=ALU.mult), reads=[bw1], writes=[bw2])
                    c.op('dve', lambda e: e.tensor_tensor(out=w2[:], in0=w2[:], in1=w1[:], op=ALU.min), reads=[bw1, bw2], writes=[bw2])
                    c.op('act', lambda e: e.activation(out=w2[:], in_=w2[:], func=AF.Exp), reads=[bw2], writes=[bw2])
                    c.op('act', lambda e: e.activation(out=w2[:], in_=w2[:], func=AF.Ln, bias=1.0, scale=1.0), reads=[bw2], writes=[bw2])
                    c.op('dve', lambda e: e.scalar_tensor_tensor(out=w1[:], in0=w1[:], scalar=0.0, in1=w2[:], op0=ALU.max, op1=ALU.add), reads=[bw1, bw2], writes=[bw1])
                    c.op('dve', lambda e: e.tensor_scalar(out=w1[:], in0=w1[:], scalar1=pc[:, 2:3], scalar2=None, op0=ALU.mult), reads=[bw1, bpc], writes=[bw1])
                    c.op('dve', lambda e: e.tensor_tensor_scan(out=gam[:], data0=msk[:], data1=w1[:], initial=0.0, op0=ALU.mult, op1=ALU.add), reads=[bmsk, bw1, brw], writes=[brw])
                    c.op('act', lambda e: e.activation(out=bet[:], in_=bsrc, func=AF.Sigmoid), reads=[bbr, brw], writes=[brw])
                    c.op('act', lambda e: e.activation(out=egam[:], in_=gam[:], func=AF.Exp), reads=[brw], writes=[brw])
                    c.op('dve', lambda e: e.tensor_tensor(out=q3[:], in0=bet[:], in1=egam[:], op=ALU.mult), reads=[brw, bq3], writes=[bq3])
                    g3 = gam[:].rearrange("p (c j) -> p c j", j=64)
                    c.op('dve', lambda e: e.tensor_tensor(out=q4[:].rearrange("p (c j) -> p c j", j=64), in0=g3[:, :, 63:64].broadcast_to([4, GP // 64, 64]), in1=g3, op=ALU.subtract),
                         reads=[brw, bq4], writes=[bq4])
                    c.op('act', lambda e: e.activation(out=q4[:], in_=q4[:], func=AF.Exp), reads=[bq4], writes=[bq4])
                    c.op('dve', lambda e: e.tensor_scalar(out=q5[:], in0=gam[:], scalar1=-1.0, scalar2=None, op0=ALU.mult), reads=[brw, bq5], writes=[bq5])
                    quants = [(gam, brw), (bet, brw), (q3, bq3), (q4, bq4), (q5, bq5)]
                    for bl in range(GP // 128):
                        blk = tp * (GP // 128) + bl
                        pc_, bpc_ = g.ps[blk % 2]
                        for qi, (qt_, bq_) in enumerate(quants):
                            c.op('pe', lambda e: e.transpose(out=pc_[:, qi * 4:(qi + 1) * 4], in_=qt_[0:4, bl * 128:(bl + 1) * 128], identity=g.ident32[0:4, 0:4]),
                                 reads=[bq_, g.b_ident32], writes=[bpc_])
                        c.op('act', lambda e: e.activation(out=cols[d][:, blk, :], in_=pc_[:, 0:4 * NQ], func=AF.Copy), reads=[bpc_, bcols[d]], writes=[bcols[d]])
                    for qi, rt in enumerate((gam, bet, egam)):
                        c.dma('sp', lambda e: e.dma_start(out=GR[d, qi, :, tp * GP:(tp + 1) * GP], in_=rt[:]), reads=[brw], pwrites=[bGR])
            bGR.seal()
            barrier(c)
        if stage < 3:
            return
        with ExitStack() as es:
            def T(name, shape, dt):
                return es.enter_context(nc.sbuf_tensor(uniq(name), shape, dt))
            nm_le = T("gm_nmle", [128, 128], F32)
            nm_geT = T("gm_nmgeT", [128, 128], F32)
            m_stT = T("gm_mstT", [128, 128], F32)
            bmk = Buf()
            c.op('pool', lambda e: e.memset(nm_le[:], 0.0), writes=[bmk])
            c.op('pool', lambda e: e.affine_select(out=nm_le[:], in_=nm_le[:], pattern=[[-1, 128]], compare_op=ALU.is_gt, fill=-30000.0, base=0, channel_multiplier=1), reads=[bmk], writes=[bmk])
            c.op('pool', lambda e: e.memset(nm_le[64:128, 0:64], -30000.0), reads=[bmk], writes=[bmk])
            c.op('pool', lambda e: e.memset(nm_geT[:], 0.0), reads=[bmk], writes=[bmk])
            c.op('pool', lambda e: e.affine_select(out=nm_geT[:], in_=nm_geT[:], pattern=[[1, 128]], compare_op=ALU.is_ge, fill=-30000.0, base=0, channel_multiplier=-1), reads=[bmk], writes=[bmk])
            c.op('pool', lambda e: e.memset(nm_geT[0:64, 64:128], -30000.0), reads=[bmk], writes=[bmk])
            c.op('pool', lambda e: e.memset(m_stT[:], 1.0), reads=[bmk], writes=[bmk])
            c.op('pool', lambda e: e.affine_select(out=m_stT[:], in_=m_stT[:], pattern=[[1, 128]], compare_op=ALU.is_gt, fill=0.0, base=0, channel_multiplier=-1), reads=[bmk], writes=[bmk])

            class CH:
                pass
            chs = []
            for d in range(2):
                ch = CH(); chs.append(ch)
                ch.d = d

                def TT(name, shape, dt, d=d):
                    return (T("gm%d_%s" % (d, name), shape, dt), Buf())
                ch.nat = [TT("nat%d" % i, [128, GTB], BF16) for i in range(3)]
                ch.arr = [[TT("arr%d_%d" % (i, j), [128, GTB], BF16) for j in range(2)] for i in range(3)]
                ch.rts = [[TT("rt%d_%d" % (q, j), [4, GTB], F32) for j in range(2)] for q in range(3)]
                ch.S32 = TT("S32", [128, 128], F32); ch.Sb = TT("Sb", [128, 128], BF16)
                ch.tmpD = TT("tmpD", [128, 128], F32); ch.Dst = TT("Dst", [128, 128], F32); ch.DTi = TT("DTi", [128, 128], F32); ch.DTs = TT("DTs", [128, 128], F32)
                ch.A_ = TT("A", [128, 128], BF16); ch.AT_ = TT("AT", [128, 128], BF16); ch.atT = TT("attnT", [128, 128], BF16)
                ch.Pm = [TT("P%d" % i, [128, 128], BF16) for i in range(6)]
                ch.Qm = [TT("Q%d" % i, [128, 128], BF16) for i in range(5)]
                ch.W32 = TT("W32", [128, 256], F32); ch.Wb = TT("Wb", [128, 256], BF16)
                ch.kdec = TT("kdec", [128, 128], BF16); ch.kcT = TT("kcT", [128, 128], BF16); ch.qdec = TT("qdec", [128, 128], BF16); ch.vnew = TT("vnew", [128, 128], BF16)
                ch.elc = TT("elc", [128, 2], F32)
                ch.osb = [TT("osb%d" % i, [128, 128], F32) for i in range(2)]
                ch.osf = [TT("osf%d" % i, [128, 128], F32) for i in range(2)]
                ch.bA = g.ps[3 * d + 0]; ch.bB = g.ps[3 * d + 1]; ch.bC = g.ps[3 * d + 2]
                ch.pb0 = 512 * d
                ch.nblk = 0

            def block_gen(ch, h, blk, b, cur, rcur):
                d = ch.d
                (qA, bqA), (kA, bkA), (vA, bvA) = cur
                bs_ = slice(b * 128, (b + 1) * 128)
                cl = cols[d][:, blk, :]
                gcol = cl[:, 0 + h:0 + h + 1]; bcol = cl[:, 4 + h:4 + h + 1]; begcol = cl[:, 8 + h:8 + h + 1]
                ekdcol = cl[:, 12 + h:12 + h + 1]; ngcol = cl[:, 16 + h:16 + h + 1]
                pA, bpA = ch.bA; pB, bpB = ch.bB; pC, bpC = ch.bC
                pb0 = ch.pb0
                tmpD, btmpD = ch.tmpD; Dst, bDst = ch.Dst; DTi, bDTi = ch.DTi; DTs, bDTs = ch.DTs
                A_, bA_ = ch.A_; AT_, bAT_ = ch.AT_; atT, batT = ch.atT
                W32, bW32 = ch.W32; Wb, bWb = ch.Wb; kdec, bkdec = ch.kdec; kcT, bkcT = ch.kcT; qdec, bqdec = ch.qdec; vnew, bvnew = ch.vnew
                elc, belc = ch.elc; S32, bS32 = ch.S32; Sb, bSb = ch.Sb
                for qi, (rt, brt) in enumerate(rcur):
                    c.op('pe', lambda e: e.matmul(pA[:, qi * 128:(qi + 1) * 128], lhsT=sel[:, h, :], rhs=rt[0:4, bs_], start=True, stop=True, skip_group_check=True),
                         reads=[bsel, brt], writes=[bpA])
                    yield
                c.op('pe', lambda e: e.matmul(pB[:, 0:128], lhsT=kA[:, bs_], rhs=kA[:, bs_], start=True, stop=True, skip_group_check=True), reads=[bkA], writes=[bpB]); yield
                c.op('pe', lambda e: e.matmul(pB[:, 128:256], lhsT=kA[:, bs_], rhs=qA[:, bs_], start=True, stop=True, skip_group_check=True), reads=[bkA, bqA], writes=[bpB]); yield
                c.op('pe', lambda e: e.transpose(out=ptb[:, pb0:pb0 + 128], in_=vA[:, bs_], identity=g.identb[:]), reads=[bvA, g.b_identb], writes=[bptb]); yield
                c.op('pe', lambda e: e.transpose(out=ptb[:, pb0 + 128:pb0 + 256], in_=kA[:, bs_], identity=g.identb[:]), reads=[bkA, g.b_identb], writes=[bptb]); yield
                c.op('dve', lambda e: e.scalar_tensor_tensor(out=tmpD[:], in0=pA[:, 0:128], scalar=-1.0, in1=nm_le[:], op0=ALU.mult, op1=ALU.add), reads=[bpA, bmk], writes=[btmpD]); yield
                c.op('act', lambda e: e.activation(out=Dst[:], in_=tmpD[:], func=AF.Exp, bias=gcol, scale=1.0), reads=[btmpD, bcols[d]], writes=[bDst]); yield
                c.op('dve', lambda e: e.tensor_tensor(out=tmpD[:], in0=pA[:, 0:128], in1=nm_geT[:], op=ALU.add), reads=[bpA, bmk, btmpD], writes=[btmpD]); yield
                c.op('act', lambda e: e.activation(out=DTi[:], in_=tmpD[:], func=AF.Exp, bias=ngcol, scale=1.0), reads=[btmpD, bcols[d]], writes=[bDTi]); yield
                c.op('dve', lambda e: e.tensor_tensor(out=DTs[:], in0=DTi[:], in1=m_stT[:], op=ALU.mult), reads=[bDTi, bmk], writes=[bDTs]); yield
                c.op('dve', lambda e: e.tensor_tensor(out=DTs[:], in0=pA[:, 128:256], in1=DTs[:], op=ALU.mult), reads=[bpA, bDTs], writes=[bDTs]); yield
                c.op('dve', lambda e: e.scalar_tensor_tensor(out=A_[:], in0=pB[:, 0:128], scalar=bcol, in1=Dst[:], op0=ALU.mult, op1=ALU.mult), reads=[bpB, bcols[d], bDst], writes=[bA_]); yield
                c.op('dve', lambda e: e.tensor_tensor(out=AT_[:], in0=pB[:, 0:128], in1=DTs[:], op=ALU.mult), reads=[bpB, bDTs], writes=[bAT_]); yield
                c.op('dve', lambda e: e.tensor_tensor(out=atT[:], in0=pB[:, 128:256], in1=DTi[:], op=ALU.mult), reads=[bpB, bDTi], writes=[batT]); yield
                c.op('dve', lambda e: e.tensor_scalar(out=Wb[:, 0:128], in0=ptb[:, pb0:pb0 + 128], scalar1=bcol, scalar2=None, op0=ALU.mult), reads=[bptb, bcols[d], bWb], writes=[bWb]); yield
                c.op('dve', lambda e: e.tensor_scalar(out=Wb[:, 128:256], in0=ptb[:, pb0 + 128:pb0 + 256], scalar1=begcol, scalar2=None, op0=ALU.mult), reads=[bptb, bcols[d], bWb], writes=[bWb]); yield
                c.op('act', lambda e: e.activation(out=kdec[:], in_=ptb[:, pb0 + 128:pb0 + 256], func=AF.Copy, scale=ekdcol), reads=[bptb, bcols[d]], writes=[bkdec]); yield
                c.op('dve', lambda e: e.tensor_tensor(out=qdec[:], in0=pA[:, 256:384], in1=qA[:, bs_], op=ALU.mult), reads=[bpA, bqA], writes=[bqdec]); yield
                c.op('act', lambda e: e.activation(out=elc[:], in_=pA[:, 256:384].rearrange("p (c j) -> p c j", j=64)[:, :, 63], func=AF.Copy), reads=[bpA], writes=[belc]); yield
                Pc, bPc = AT_, bAT_
                Qc, bQc = A_, bA_
                for lev in range(6):
                    c.op('pe', lambda e: e.matmul(pC[:, 0:256], lhsT=Pc[:], rhs=Wb[:], start=True, stop=True, skip_group_check=True), reads=[bPc, bWb], writes=[bpC]); yield
                    c.op('dve', lambda e: e.tensor_tensor(out=Wb[:], in0=Wb[:], in1=pC[:, 0:256], op=(ALU.subtract if lev == 0 else ALU.add)), reads=[bWb, bpC], writes=[bWb]); yield
                    if lev < 5:
                        Pn, bPn = ch.Pm[lev + 1]
                        c.op('pe', lambda e: e.matmul(pB[:, 256:384], lhsT=Qc[:], rhs=Pc[:], start=True, stop=True, skip_group_check=True), reads=[bQc, bPc], writes=[bpB]); yield
                        if lev < 4:
                            Qn, bQn = ch.Qm[lev + 1]
                            c.op('pe', lambda e: e.matmul(pB[:, 384:512], lhsT=Pc[:], rhs=Qc[:], start=True, stop=True, skip_group_check=True), reads=[bQc, bPc], writes=[bpB]); yield
                            c.op('dve', lambda e: e.tensor_copy(out=Qn[:], in_=pB[:, 384:512]), reads=[bpB], writes=[bQn]); yield
                        c.op('act', lambda e: e.activation(out=Pn[:], in_=pB[:, 256:384], func=AF.Copy), reads=[bpB], writes=[bPn]); yield
                        Pc, bPc = Pn, bPn
                        if lev < 4:
                            Qc, bQc = Qn, bQn
                c.op('pe', lambda e: e.transpose(out=ptb[:, pb0 + 256:pb0 + 384], in_=Wb[:, 128:256], identity=g.identb[:]), reads=[bWb, g.b_identb], writes=[bptb]); yield
                c.op('act', lambda e: e.activation(out=kcT[:], in_=ptb[:, pb0 + 256:pb0 + 384], func=AF.Copy), reads=[bptb], writes=[bkcT]); yield
                for ci in range(2):
                    r0 = 64 * ci
                    c.op('pe', lambda e: e.matmul(pC[r0:r0 + 64, 256:384], lhsT=kcT[:, r0:r0 + 64], rhs=Sb[:, :], start=True, stop=True, skip_group_check=True), reads=[bkcT, bSb], writes=[bpC]); yield
                    c.op('dve', lambda e: e.tensor_tensor(out=vnew[r0:r0 + 64, :], in0=Wb[r0:r0 + 64, 0:128], in1=pC[r0:r0 + 64, 256:384], op=ALU.subtract), reads=[bWb, bpC, bvnew], writes=[bvnew]); yield
                    c.op('pe', lambda e: e.matmul(pA[r0:r0 + 64, 384:512], lhsT=qdec[:, r0:r0 + 64], rhs=Sb[:, :], start=True, stop=False, skip_group_check=True), reads=[bqdec, bSb], writes=[bpA])
                    c.op('pe', lambda e: e.matmul(pA[r0:r0 + 64, 384:512], lhsT=atT[r0:r0 + 64, r0:r0 + 64], rhs=vnew[r0:r0 + 64, :], start=False, stop=True, skip_group_check=True), reads=[batT, bvnew], writes=[bpA]); yield
                    c.op('pe', lambda e: e.matmul(pC[:, 384:512], lhsT=kdec[r0:r0 + 64, :], rhs=vnew[r0:r0 + 64, :], start=True, stop=True, skip_group_check=True), reads=[bkdec, bvnew], writes=[bpC]); yield
                    c.op('dve', lambda e: e.scalar_tensor_tensor(out=Sb[:], in0=S32[:], scalar=elc[:, ci:ci + 1], in1=pC[:, 384:512], op0=ALU.mult, op1=ALU.add), reads=[bS32, belc, bpC], writes=[bSb]); yield
                    c.op('dve', lambda e: e.scalar_tensor_tensor(out=S32[:], in0=S32[:], scalar=elc[:, ci:ci + 1], in1=pC[:, 384:512], op0=ALU.mult, op1=ALU.add), reads=[bS32, belc, bpC], writes=[bS32]); yield
                os_, bos_ = ch.osb[ch.nblk % 2]
                of_, bof_ = ch.osf[ch.nblk % 2]
                ch.nblk += 1
                c.op('act', lambda e: e.activation(out=os_[:], in_=pA[:, 384:512], func=AF.Copy), reads=[bpA], writes=[bos_]); yield
                if d == 0:
                    c.dma('sp', lambda e: e.dma_start(out=OD[blk * 128:(blk + 1) * 128, h * 128:(h + 1) * 128], in_=os_[:]), reads=[bos_], pwrites=[bODs[h]]); yield
                else:
                    pf, bpf = g.ps[6]
                    c.op('pe', lambda e: e.matmul(pf[:, 0:128], lhsT=g.J32[:], rhs=os_[:], start=True, stop=True), reads=[g.b_J32, bos_], writes=[bpf]); yield
                    c.op('act', lambda e: e.activation(out=of_[:], in_=pf[:, 0:128], func=AF.Copy), reads=[bpf], writes=[bof_]); yield
                    c.dma('act', lambda e: e.dma_start(out=OD2[L - (blk + 1) * 128:L - blk * 128, h * 128:(h + 1) * 128], in_=of_[:]), reads=[bof_], pwrites=[bOD2s[h]]); yield

            for h in range(4):
                for ch in chs:
                    S32, bS32 = ch.S32; Sb, bSb = ch.Sb
                    c.op('pool', lambda e: e.memset(S32[:], 0.0), reads=[bS32], writes=[bS32])
                    c.op('pool', lambda e: e.memset(Sb[:], 0.0), reads=[bSb], writes=[bSb])
                for tb in range(L // GTB):
                    curs = []; rcurs = []
                    for ch in chs:
                        d = ch.d
                        cur = []
                        for ai in range(3):
                            a_, ba_ = ch.arr[ai][tb % 2]
                            if d == 0:
                                c.dma('sp' if ai % 2 else 'act', lambda e: e.dma_start(out=a_[:], in_=GQ[ai * 4 + h, :, tb * GTB:(tb + 1) * GTB]), reads=[bGQ], writes=[ba_])
                            else:
                                n_, bn_ = ch.nat[ai]
                                c.dma('sp' if ai % 2 else 'act', lambda e: e.dma_start(out=n_[:], in_=GQ[ai * 4 + h, :, L - (tb + 1) * GTB:L - tb * GTB]), reads=[bGQ], writes=[bn_])
                                c.op('pool', lambda e: e.tensor_copy(out=a_[:], in_=n_[:, ::-1]), reads=[bn_], writes=[ba_])
                            cur.append((a_, ba_))
                        rcur = []
                        for qi in range(3):
                            r_, br_ = ch.rts[qi][tb % 2]
                            c.dma('sp', lambda e: e.dma_start(out=r_[:], in_=GR[d, qi, :, tb * GTB:(tb + 1) * GTB]), reads=[bGR], writes=[br_])
                            rcur.append((r_, br_))
                        curs.append(cur); rcurs.append(rcur)
                    for b in range(GTB // 128):
                        blk = tb * (GTB // 128) + b
                        gens = [block_gen(ch, h, blk, b, curs[i], rcurs[i]) for i, ch in enumerate(chs)]
                        alive = list(gens)
                        while alive:
                            for gnr in list(alive):
                                try:
                                    next(gnr)
                                except StopIteration:
                                    alive.remove(gnr)
                bODs[h].seal(); bOD2s[h].seal()
            barrier(c)
    if stage < 4:
        return
    gated_norm_finalize(c, g, OD, bODs, PV, bPV, 1536, out_gain, MT, bMT, 512, "gf_", OA2=OD2, bOA2s=bOD2s)


N_ACTIVE = 4
DEPTH = 4

PARAM_NAMES = ['mix_norm', 'ffn_norm', 'ev_w_in', 'ev_w_out', 'a_lb_logits', 'a_out_norm', 's5_lambda_re', 's5_lambda_im',
               's5_log_step', 's5_b_re', 's5_b_im', 's5_c_re', 's5_c_im', 's5_d', 's5_glu_w', 's5_glu_b', 'od_w_in', 'od_w_out',
               'c_q_norm', 'c_k_norm', 'c_lambda', 'c_out_norm', 'rel_bias', 'd_conv_w', 'd_a_log', 'd_dt_bias', 'd_out_norm',
               'moe_router', 'moe_w_gate', 'moe_w_up', 'moe_w_down']

PARAM_SHAPES = {
    'mix_norm': (4, 1024), 'ffn_norm': (4, 1024), 'ev_w_in': (2, 1024, 3072), 'ev_w_out': (2, 1024, 1024), 'a_lb_logits': (2, 2, 512),
    'a_out_norm': (2, 128), 's5_lambda_re': (2, 2, 32, 64), 's5_lambda_im': (2, 2, 32, 64), 's5_log_step': (2, 2, 32),
    's5_b_re': (2, 2, 32, 64, 16), 's5_b_im': (2, 2, 32, 64, 16), 's5_c_re': (2, 2, 32, 16, 64), 's5_c_im': (2, 2, 32, 16, 64),
    's5_d': (2, 512), 's5_glu_w': (2, 512, 512), 's5_glu_b': (2, 512), 'od_w_in': (2, 1024, 3600), 'od_w_out': (2, 1024, 1024),
    'c_q_norm': (2, 64), 'c_k_norm': (2, 64), 'c_lambda': (2, 4, 64), 'c_out_norm': (2, 128), 'rel_bias': (32, 4),
    'd_conv_w': (2, 5, 1536), 'd_a_log': (2, 2, 4), 'd_dt_bias': (2, 2, 4), 'd_out_norm': (2, 128), 'moe_router': (4, 1024, 16),
    'moe_w_gate': (4, 16, 1024, 2048), 'moe_w_up': (4, 16, 1024, 2048), 'moe_w_down': (4, 16, 2048, 1024)}


def build_program(layers=range(DEPTH), do_mixer=True, do_moe=True):
    nc = bass.Bass('TRN2', target_bir_lowering=False)
    xin = nc.dram_tensor("x", [L, D], F32, kind="ExternalInput").ap()
    P = {n: nc.dram_tensor(n, list(PARAM_SHAPES[n]), F32, kind="ExternalInput").ap() for n in PARAM_NAMES}
    onehot = nc.dram_tensor("t5_onehot", [32, 512], F32, kind="ExternalInput").ap()
    X = nc.dram_tensor("y", [L, D], F32, kind="ExternalOutput").ap()
    PT = nc.dram_tensor("PT", [2048, L], F32).ap()
    PV = nc.dram_tensor("PV", [L, 2048], BF16).ap()
    QK = nc.dram_tensor("QK", [16, 128, L], BF16).ap()
    QK5 = QK.rearrange("(h r w) p t -> h r w p t", h=4, r=2)
    OA = nc.dram_tensor("OA", [L, 512], F32).ap()
    YT = nc.dram_tensor("YT", [512, L], F32).ap()
    MT = nc.dram_tensor("MT", [1024, L], BF16).ap()
    HB = nc.dram_tensor("HB", [L, D], BF16).ap()
    GQ = nc.dram_tensor("GQ", [12, 128, L], BF16).ap()
    GR = nc.dram_tensor("GR", [2, 3, 4, L], F32).ap()
    OD2 = nc.dram_tensor("OD2", [L, 512], F32).ap()
    FV = nc.dram_tensor("FV", [4, 512], F32).ap()
    c = Ctx(nc); g = G()
    setup_consts(c, g)
    bX = Buf('X'); bPT = Buf(); bPV = Buf(); bQK = Buf(); bOAs = [Buf() for _ in range(4)]; bYT = Buf(); bMT = Buf()
    bHB = Buf(); bGQ = Buf(); bGR = Buf(); bFV = Buf(); bOD2s = [Buf() for _ in range(4)]
    for r in range(0, L, 512):
        c.dma('sp', lambda e: e.dma_start(out=X[r:r + 512, :], in_=xin[r:r + 512, :]), pwrites=[bX])
    bX.seal()
    for layer in layers:
        j = layer // 2
        if do_mixer:
            if layer % 2 == 0:
                spec = [(0, 512, 'F', 0), (512, 512, 'F', 512), (1024, 512, 'F', 1024), (2560, 512, 'F', 1536), (1536, 512, 'T', 0), (2048, 512, 'T', 512)]
                proj_phase(c, g, X, bX, P['mix_norm'][layer], P['ev_w_in'][j], 3072, spec, PT, bPT, PV, bPV)
                hgrn2_phase(c, g, PT, bPT, PV, bPV, P['a_lb_logits'], j, P['a_out_norm'][j], QK5, bQK, OA, bOAs, MT, bMT)
                s5_phase(c, g, PT, bPT, P['s5_lambda_re'][j], P['s5_lambda_im'][j], P['s5_log_step'][j], P['s5_b_re'][j], P['s5_b_im'][j],
                         P['s5_c_re'][j], P['s5_c_im'][j], P['s5_d'][j], P['s5_glu_w'][j], P['s5_glu_b'][j], YT, bYT, MT, bMT)
                bMT.seal()
                bX = outproj_phase(c, g, MT, bMT, P['ev_w_out'][j], X, bX)
            else:
                spec = [(0, 512, 'T', 0), (512, 512, 'T', 512), (1024, 512, 'T', 1024), (1536, 1536, 'F', 0), (3072, 16, 'F', 1536), (3088, 512, 'T', 1536)]
                proj_phase(c, g, X, bX, P['mix_norm'][layer], P['od_w_in'][j], 3600, spec, PT, bPT, PV, bPV)
                attn_phase(c, g, PV, bPV, P['c_q_norm'][j], P['c_k_norm'][j], P['c_lambda'][j], P['c_out_norm'][j], P['rel_bias'], onehot, layer,
                           QK, bQK, FV, bFV, MT, bMT)
                gdn_phase(c, g, PT, bPT, PV, bPV, P['d_conv_w'][j], P['d_a_log'][j], P['d_dt_bias'][j], P['d_out_norm'][j], GQ, bGQ, GR, bGR, OA, bOAs, OD2, bOD2s, MT, bMT)
                bMT.seal()
                bX = outproj_phase(c, g, MT, bMT, P['od_w_out'][j], X, bX)
        if do_moe:
            moe_layer(c, g, X, bX, HB, bHB, P['ffn_norm'][layer], P['moe_router'][layer], P['moe_w_gate'][layer], P['moe_w_up'][layer], P['moe_w_down'][layer])
    barrier(c)
    c.finish([bX])
    return nc, c


def kernel(**inputs):
    x = np.ascontiguousarray(np.asarray(inputs['x'], dtype=np.float32))
    B = x.shape[0]
    assert B == N_ACTIVE and x.shape[1] == L and x.shape[2] == D
    nc, c = build_program()
    params = {n: np.ascontiguousarray(np.asarray(inputs[n], dtype=np.float32)) for n in PARAM_NAMES}
    oh = t5_onehot()
    in_maps = []
    for b in range(N_ACTIVE):
        m = {"x": x[b], "t5_onehot": oh}
        m.update(params)
        in_maps.append(m)
    res = run_bass_kernel_spmd(nc, in_maps, core_ids=list(range(N_ACTIVE)))
    out = np.stack([np.asarray(res.results[b]["y"], dtype=np.float32) for b in range(N_ACTIVE)], axis=0)
    return out
```

```python
import math
from contextlib import ExitStack

import numpy as np
import concourse.bass as bass
import concourse.mybir as mybir
from concourse.bass_utils import run_bass_kernel_spmd

F32 = mybir.dt.float32
BF16 = mybir.dt.bfloat16
U32 = mybir.dt.uint32
I32 = mybir.dt.int32
AF = mybir.ActivationFunctionType
ALU = mybir.AluOpType
AX = mybir.AxisListType


class Buf:
    __slots__ = ("name", "w", "r", "pw", "psum")

    def __init__(self, name="", psum=False):
        self.name = name
        self.psum = psum
        self.w = {}
        self.r = {}
        self.pw = {}

    def seal(self):
        for k, v in self.pw.items():
            if self.w.get(k, 0) < v:
                self.w[k] = v
        self.pw = {}


class Ctx:
    NDMA = 8

    def __init__(self, nc, same_engine_sync=True):
        self.nc = nc
        self.E = dict(pe=nc.tensor, dve=nc.vector, act=nc.scalar, pool=nc.gpsimd, sp=nc.sync)
        self.sem = {}
        self.cnt = {}
        for k in self.E:
            self.sem[k] = nc.alloc_semaphore("c_" + k)
            self.cnt[k] = 0
        self.dslot = {}
        for q in ("sp", "act", "pool"):
            for i in range(self.NDMA):
                key = "d_%s%d" % (q, i)
                self.sem[key] = nc.alloc_semaphore(key)
                self.cnt[key] = 0
            self.dslot[q] = 0
        self.seen = {k: {} for k in self.E}
        self.same = same_engine_sync
        self.ninst = 0

    def _wait(self, eng, tok):
        if tok is None:
            return
        key, val = tok
        if key == eng and (eng == "pe" or not self.same):
            return
        if self.seen[eng].get(key, 0) >= val:
            return
        self.E[eng].wait_ge(self.sem[key], val)
        self.seen[eng][key] = val

    def _deps(self, eng, reads, writes, pwrites=()):
        for b in reads:
            for k, v in b.w.items():
                self._wait(eng, (k, v))
            for k, v in b.pw.items():
                self._wait(eng, (k, v))
            if b.psum:
                for k, v in b.r.items():
                    if k != eng:
                        self._wait(eng, (k, v))
        for b in writes:
            for d in (b.w, b.pw, b.r):
                for k, v in d.items():
                    self._wait(eng, (k, v))
        for b in pwrites:
            for d in (b.w, b.r):
                for k, v in d.items():
                    self._wait(eng, (k, v))

    def _commit(self, tok, reads, writes, pwrites=()):
        k, v = tok
        for b in writes:
            b.w = {k: v}
            b.pw = {}
            b.r = {}
        for b in pwrites:
            if b.pw.get(k, 0) < v:
                b.pw[k] = v
        for b in reads:
            if b.r.get(k, 0) < v:
                b.r[k] = v

    def op(self, eng, fn, reads=(), writes=(), pwrites=()):
        self._deps(eng, reads, writes, pwrites)
        inst = fn(self.E[eng])
        self.cnt[eng] += 1
        tok = (eng, self.cnt[eng])
        inst.then_inc(self.sem[eng], 1)
        self._commit(tok, reads, writes, pwrites)
        self.ninst += 1
        return tok

    def dma(self, q, fn, reads=(), writes=(), pwrites=()):
        self._deps(q, reads, writes, pwrites)
        i = self.dslot[q]
        self.dslot[q] = (i + 1) % self.NDMA
        key = "d_%s%d" % (q, i)
        if self.cnt[key] > 0:
            self._wait(q, (key, self.cnt[key]))
        inst = fn(self.E[q])
        self.cnt[key] += 16
        tok = (key, self.cnt[key])
        inst.then_inc(self.sem[key], 16)
        self._commit(tok, reads, writes, pwrites)
        self.ninst += 1
        return tok

    def finish(self, bufs):
        for b in bufs:
            for d in (b.w, b.pw):
                for k, v in d.items():
                    self._wait("sp", (k, v))


def barrier(c):
    toks = [(k, v) for k, v in c.cnt.items() if v > 0]
    for eng in c.E:
        for tok in toks:
            c._wait(eng, tok)


_UNIQ = [0]


def uniq(name):
    _UNIQ[0] += 1
    return "%s_u%d" % (name, _UNIQ[0])


L = 8192
D = 1024
NE = 16
FF = 2048
CAP = 1024
NT = L // 128


class G:
    pass


def alloc_T(nc, name, shape, dtype, n=1, es=None):
    if es is None:
        return [(nc.alloc_sbuf_tensor("%s_%d" % (name, i), shape, dtype), Buf(name)) for i in range(n)]
    return [(es.enter_context(nc.sbuf_tensor(uniq("%s_%d" % (name, i)), shape, dtype)), Buf(name)) for i in range(n)]


def setup_consts(c, g):
    nc = c.nc
    g.ident32 = nc.alloc_sbuf_tensor("ident32", [128, 128], F32); g.b_ident32 = Buf()
    g.identb = nc.alloc_sbuf_tensor("identb", [128, 128], BF16); g.b_identb = Buf()
    g.ones32 = nc.alloc_sbuf_tensor("ones32", [1, 128], F32); g.b_ones32 = Buf()
    g.neghalf = nc.alloc_sbuf_tensor("neghalf", [128, 1], F32); g.b_neghalf = Buf()
    for t, b in ((g.ident32, g.b_ident32), (g.identb, g.b_identb)):
        c.op('pool', lambda e: e.memset(t[:], 0.0), writes=[b])
        c.op('pool', lambda e: e.affine_select(out=t[:], in_=t[:], pattern=[[-1, 128]], compare_op=ALU.not_equal,
                                               fill=1.0, base=0, channel_multiplier=1), reads=[b], writes=[b])
    g.J32 = nc.alloc_sbuf_tensor("J32", [128, 128], F32); g.b_J32 = Buf()
    g.Jb = nc.alloc_sbuf_tensor("Jb", [128, 128], BF16); g.b_Jb = Buf()
    for t, b in ((g.J32, g.b_J32), (g.Jb, g.b_Jb)):
        c.op('pool', lambda e: e.memset(t[:], 0.0), writes=[b])
        c.op('pool', lambda e: e.affine_select(out=t[:], in_=t[:], pattern=[[1, 128]], compare_op=ALU.not_equal,
                                               fill=1.0, base=-127, channel_multiplier=1), reads=[b], writes=[b])
    c.op('pool', lambda e: e.memset(g.ones32[:], 1.0), writes=[g.b_ones32])
    c.op('pool', lambda e: e.memset(g.neghalf[:], -0.5), writes=[g.b_neghalf])
    g.ps = []
    for i in range(7):
        g.ps.append((nc.alloc_psum_tensor("ps%d" % i, [128, 512], F32), Buf("ps%d" % i, psum=True)))
    g.psb = (nc.alloc_psum_tensor("psb", [128, 1024], BF16), Buf("psb", psum=True))


def bcast_row(c, g, dst, bdst, src_ap, n, tmp, btmp, psi=6):
    c.dma('sp', lambda e: e.dma_start(out=tmp[0:1, 0:n], in_=src_ap.rearrange("(o n) -> o n", o=1)), writes=[btmp])
    ps, bps = g.ps[psi]
    for h in range(0, n, 512):
        w = min(512, n - h)
        c.op('pe', lambda e: e.matmul(ps[:, 0:w], lhsT=g.ones32[0:1, :], rhs=tmp[0:1, h:h + w], start=True, stop=True),
             reads=[g.b_ones32, btmp], writes=[bps])
        c.op('dve', lambda e: e.tensor_copy(out=dst[:, h:h + w], in_=ps[:, 0:w]), reads=[bps], writes=[bdst])


def rmsnorm_tile(c, g, xt, bxt, gB, bgB, h32, bh32, junk, bjunk, ss, bss):
    c.op('dve', lambda e: e.scalar_tensor_tensor(out=junk[:], in0=xt[:], scalar=1.0, in1=xt[:], op0=ALU.mult, op1=ALU.mult,
                                                 accum_out=ss[:, 0:1]), reads=[bxt], writes=[bjunk, bss])
    c.op('dve', lambda e: e.tensor_scalar(out=ss[:, 0:1], in0=ss[:, 0:1], scalar1=1.0 / D, scalar2=1e-6, op0=ALU.mult, op1=ALU.add),
         reads=[bss], writes=[bss])
    c.op('act', lambda e: e.activation(out=ss[:, 0:1], in_=ss[:, 0:1], func=AF.Ln), reads=[bss], writes=[bss])
    c.op('act', lambda e: e.activation(out=ss[:, 0:1], in_=ss[:, 0:1], func=AF.Exp, scale=-0.5), reads=[bss], writes=[bss])
    c.op('dve', lambda e: e.scalar_tensor_tensor(out=h32[:], in0=xt[:], scalar=ss[:, 0:1], in1=gB[:], op0=ALU.mult, op1=ALU.mult),
         reads=[bxt, bss, bgB], writes=[bh32])


def moe_phase(c, g, X, bX, HB, bHB, ffn_g, w_router, w_gate, w_up, w_down, sb, stage=3):
    nc = c.nc
    gB, bgB = sb['gB']
    tmpr, btmpr = sb['tmprow']
    bcast_row(c, g, gB, bgB, ffn_g, D, tmpr, btmpr)
    wr, bwr = sb['wr']
    c.dma('sp', lambda e: e.dma_start(out=wr[:], in_=w_router.rearrange("(k p) e -> p k e", p=128)), writes=[bwr])
    affT, baffT = sb['affT']
    for i in range(NT):
        xt, bxt = sb['xt'][i % 2]
        h32, bh32 = sb['h32'][i % 2]
        hb, bhb = sb['hb'][i % 2]
        junk, bjunk = sb['junk']
        ss, bss = sb['ss'][i % 2]
        c.dma('sp', lambda e: e.dma_start(out=xt[:], in_=X[i * 128:(i + 1) * 128, :]), reads=[bX], writes=[bxt])
        rmsnorm_tile(c, g, xt, bxt, gB, bgB, h32, bh32, junk, bjunk, ss, bss)
        c.op('act', lambda e: e.activation(out=hb[:], in_=h32[:], func=AF.Copy), reads=[bh32], writes=[bhb])
        c.dma('act', lambda e: e.dma_start(out=HB[i * 128:(i + 1) * 128, :], in_=hb[:]), reads=[bhb], pwrites=[bHB])
        hT, bhT = sb['hT32'][i % 2]
        for hh in range(2):
            ps, bps = g.ps[hh]
            for k in range(4):
                kk = hh * 4 + k
                c.op('pe', lambda e: e.transpose(out=ps[:, k * 128:(k + 1) * 128], in_=h32[:, kk * 128:(kk + 1) * 128],
                                                 identity=g.ident32[:]), reads=[bh32, g.b_ident32], writes=[bps])
            c.op('act', lambda e: e.activation(out=hT[:, hh * 512:(hh + 1) * 512], in_=ps[:, :], func=AF.Copy),
                 reads=[bps], writes=[bhT])
        pl, bpl = g.ps[2 + (i % 2)]
        for k in range(8):
            c.op('pe', lambda e: e.matmul(pl[:, 0:NE], lhsT=hT[:, k * 128:(k + 1) * 128], rhs=wr[:, k, :], start=(k == 0), stop=(k == 7)),
                 reads=[bhT, bwr], writes=[bpl])
        sm, bsm = sb['sm'][i % 2]
        ex, bex = sb['ex'][i % 2]
        c.op('dve', lambda e: e.tensor_reduce(out=sm[:, 0:1], in_=pl[:, 0:NE], axis=AX.X, op=ALU.max), reads=[bpl], writes=[bsm])
        c.op('dve', lambda e: e.tensor_scalar(out=sm[:, 0:1], in0=sm[:, 0:1], scalar1=-1.0, scalar2=None, op0=ALU.mult), reads=[bsm], writes=[bsm])
        c.op('act', lambda e: e.activation(out=ex[:], in_=pl[:, 0:NE], func=AF.Exp, bias=sm[:, 0:1], scale=1.0, accum_out=sm[:, 1:2]),
             reads=[bpl, bsm], writes=[bex, bsm])
        c.op('dve', lambda e: e.reciprocal(out=sm[:, 2:3], in_=sm[:, 1:2]), reads=[bsm], writes=[bsm])
        c.op('dve', lambda e: e.tensor_scalar(out=ex[:], in0=ex[:], scalar1=sm[:, 2:3], scalar2=None, op0=ALU.mult), reads=[bex, bsm], writes=[bex])
        pt, bpt = g.ps[4 + (i % 2)]
        c.op('pe', lambda e: e.transpose(out=pt[0:NE, 0:128], in_=ex[:, 0:NE], identity=g.ident32[:]), reads=[bex, g.b_ident32], writes=[bpt])
        c.op('act', lambda e: e.activation(out=affT[0:NE, i * 128:(i + 1) * 128], in_=pt[0:NE, 0:128], func=AF.Copy), reads=[bpt], writes=[baffT])
    bHB.seal()
    if stage < 2:
        return
    vals, bvals = sb['vals']
    idxu, bidxu = sb['idxu']
    for it in range(CAP // 8):
        sl = slice(it * 8, it * 8 + 8)
        c.op('dve', lambda e: e.max(out=vals[:, sl], in_=affT[:, :]), reads=[baffT], writes=[bvals])
        c.op('dve', lambda e: e.max_index(out=idxu[:, sl], in_max=vals[:, sl], in_values=affT[:, :]), reads=[baffT, bvals], writes=[bidxu])
        c.op('dve', lambda e: e.match_replace(out=affT[:, :], in_to_replace=vals[:, sl], in_values=affT[:, :], imm_value=-1.0),
             reads=[bvals, baffT], writes=[baffT])
    idxf, bidxf = sb['idxf']
    c.op('dve', lambda e: e.tensor_copy(out=idxf[:], in_=idxu[:]), reads=[bidxu], writes=[bidxf])
    idxT, bidxT = sb['idxT']
    gateT, bgateT = sb['gateT']
    for j in range(8):
        pt, bpt = g.ps[4 + (j % 2)]
        c.op('pe', lambda e: e.transpose(out=pt[:, 0:NE], in_=idxf[0:NE, j * 128:(j + 1) * 128], identity=g.ident32[0:NE, 0:NE]),
             reads=[bidxf, g.b_ident32], writes=[bpt])
        c.op('dve', lambda e: e.tensor_copy(out=idxT[:, j, :], in_=pt[:, 0:NE]), reads=[bpt], writes=[bidxT])
        pt2, bpt2 = g.ps[2 + (j % 2)]
        c.op('pe', lambda e: e.transpose(out=pt2[:, 0:NE], in_=vals[0:NE, j * 128:(j + 1) * 128], identity=g.ident32[0:NE, 0:NE]),
             reads=[bvals, g.b_ident32], writes=[bpt2])
        c.op('act', lambda e: e.activation(out=gateT[:, j, :], in_=pt2[:, 0:NE], func=AF.Copy), reads=[bpt2], writes=[bgateT])
    if stage < 3:
        return
    xsT, bxsT = sb['xsT']
    hidT, bhidT = sb['hidT']
    yacc, byacc = sb['yacc']
    ptb, bptb = g.psb
    qi = 0
    for ex_i in range(NE):
        for j in range(8):
            xs, bxs = sb['xs'][j % 2]
            c.dma('pool', lambda e: e.indirect_dma_start(out=xs[:, :], out_offset=None, in_=HB[:, :],
                                                        in_offset=bass.IndirectOffsetOnAxis(ap=idxT[:, j, ex_i:ex_i + 1], axis=0)),
                  reads=[bHB, bidxT], writes=[bxs])
            for k in range(8):
                c.op('pe', lambda e: e.transpose(out=ptb[:, k * 128:(k + 1) * 128], in_=xs[:, k * 128:(k + 1) * 128], identity=g.identb[:]),
                     reads=[bxs, g.b_identb], writes=[bptb])
            c.op('dve', lambda e: e.tensor_copy(out=xsT[:, :, j * 128:(j + 1) * 128], in_=ptb[:, :].rearrange("p (k s) -> p k s", k=8)),
                 reads=[bptb], writes=[bxsT])
        for q in range(4):
            wg, bwg = sb['wg'][qi % 2]
            wu, bwu = sb['wu'][qi % 2]
            wd, bwd = sb['wd'][qi % 2]
            qi += 1
            f0 = q * 512
            c.dma('pool', lambda e: e.dma_start(out=wg[:], in_=w_gate[ex_i, :, f0:f0 + 512].rearrange("(k p) f -> p k f", p=128)), writes=[bwg])
            c.dma('pool', lambda e: e.dma_start(out=wu[:], in_=w_up[ex_i, :, f0:f0 + 512].rearrange("(k p) f -> p k f", p=128)), writes=[bwu])
            c.dma('pool', lambda e: e.dma_start(out=wd[:], in_=w_down[ex_i, f0:f0 + 512, :].rearrange("(k p) d -> p k d", p=128)), writes=[bwd])
            n = 0
            for fc in range(4):
                for sh in range(2):
                    pg, bpg = g.ps[0 + (n % 2)]
                    pu, bpu = g.ps[2 + (n % 2)]
                    sg, bsg = sb['sg'][n % 2]
                    n += 1
                    for k in range(8):
                        c.op('pe', lambda e: e.matmul(pg[:, :], lhsT=wg[:, k, fc * 128:(fc + 1) * 128], rhs=xsT[:, k, sh * 512:(sh + 1) * 512],
                                                      start=(k == 0), stop=(k == 7)), reads=[bwg, bxsT], writes=[bpg])
                    for k in range(8):
                        c.op('pe', lambda e: e.matmul(pu[:, :], lhsT=wu[:, k, fc * 128:(fc + 1) * 128], rhs=xsT[:, k, sh * 512:(sh + 1) * 512],
                                                      start=(k == 0), stop=(k == 7)), reads=[bwu, bxsT], writes=[bpu])
                    c.op('act', lambda e: e.activation(out=sg[:], in_=pg[:, :], func=AF.Silu), reads=[bpg], writes=[bsg])
                    c.op('dve', lambda e: e.tensor_tensor(out=hidT[:, fc, sh * 512:(sh + 1) * 512], in0=sg[:], in1=pu[:, :], op=ALU.mult),
                         reads=[bsg, bpu], writes=[bhidT])
            m = 0
            for j in range(8):
                for dh in range(2):
                    py, bpy = g.ps[4 + (m % 2)]
                    m += 1
                    for fc in range(4):
                        c.op('pe', lambda e: e.matmul(py[:, :], lhsT=hidT[:, fc, j * 128:(j + 1) * 128], rhs=wd[:, fc, dh * 512:(dh + 1) * 512],
                                                      start=(fc == 0), stop=(fc == 3)), reads=[bhidT, bwd], writes=[bpy])
                    ysl = yacc[:, j, dh * 512:(dh + 1) * 512]
                    gsc = gateT[:, j, ex_i:ex_i + 1]
                    if q == 0:
                        c.op('dve', lambda e: e.tensor_scalar(out=ysl, in0=py[:, :], scalar1=gsc, scalar2=None, op0=ALU.mult),
                             reads=[bpy, bgateT], writes=[byacc])
                    else:
                        c.op('dve', lambda e: e.scalar_tensor_tensor(out=ysl, in0=py[:, :], scalar=gsc, in1=ysl, op0=ALU.mult, op1=ALU.add),
                             reads=[bpy, bgateT, byacc], writes=[byacc])
        for j in range(8):
            c.dma('pool', lambda e: e.indirect_dma_start(out=X[:, :], out_offset=bass.IndirectOffsetOnAxis(ap=idxT[:, j, ex_i:ex_i + 1], axis=0),
                                                        in_=yacc[:, j, :], in_offset=None, compute_op=ALU.add),
                  reads=[byacc, bidxT], pwrites=[bX])
        bX.seal()


def moe_alloc(nc, es=None):
    sb = {}
    sb['gB'] = alloc_T(nc, 'gB', [128, D], F32, 1, es=es)[0]
    sb['tmprow'] = alloc_T(nc, 'tmprow', [1, 1024], F32, 1, es=es)[0]
    sb['wr'] = alloc_T(nc, 'wr', [128, 8, NE], F32, 1, es=es)[0]
    big = es.enter_context(nc.sbuf_tensor(uniq('big'), [128, L], F32)) if es is not None else nc.alloc_sbuf_tensor('big', [128, L], F32); bbig = Buf('big')
    sb['affT'] = (big[0:NE, :], bbig)
    sb['xt'] = alloc_T(nc, 'xt', [128, D], F32, 2, es=es)
    sb['h32'] = alloc_T(nc, 'h32', [128, D], F32, 2, es=es)
    sb['hb'] = alloc_T(nc, 'hb', [128, D], BF16, 2, es=es)
    sb['junk'] = alloc_T(nc, 'junk', [128, D], F32, 1, es=es)[0]
    sb['ss'] = alloc_T(nc, 'ss', [128, 4], F32, 2, es=es)
    sb['hT32'] = alloc_T(nc, 'hT32', [128, D], F32, 2, es=es)
    sb['sm'] = alloc_T(nc, 'sm', [128, 4], F32, 2, es=es)
    sb['ex'] = alloc_T(nc, 'ex', [128, NE], F32, 2, es=es)
    sb['vals'] = alloc_T(nc, 'vals', [NE, CAP], F32, 1, es=es)[0]
    sb['idxu'] = alloc_T(nc, 'idxu', [NE, CAP], U32, 1, es=es)[0]
    sb['idxf'] = alloc_T(nc, 'idxf', [NE, CAP], F32, 1, es=es)[0]
    sb['idxT'] = alloc_T(nc, 'idxT', [128, 8, NE], U32, 1, es=es)[0]
    sb['gateT'] = alloc_T(nc, 'gateT', [128, 8, NE], F32, 1, es=es)[0]
    sb['xsT'] = alloc_T(nc, 'xsT', [128, 8, CAP], BF16, 1, es=es)[0]
    sb['hidT'] = alloc_T(nc, 'hidT', [128, 4, CAP], BF16, 1, es=es)[0]
    sb['yacc'] = (big[:, :].rearrange('p (j d) -> p j d', j=8), bbig)
    sb['xs'] = alloc_T(nc, 'xs', [128, D], BF16, 2, es=es)
    sb['wg'] = alloc_T(nc, 'wg', [128, 8, 512], BF16, 2, es=es)
    sb['wu'] = alloc_T(nc, 'wu', [128, 8, 512], BF16, 2, es=es)
    sb['wd'] = alloc_T(nc, 'wd', [128, 4, D], BF16, 2, es=es)
    sb['sg'] = alloc_T(nc, 'sg', [128, 512], BF16, 2, es=es)
    return sb


def moe_layer(c, g, X, bX, HB, bHB, ffn_g, w_router, w_gate, w_up, w_down):
    with ExitStack() as es:
        sb = moe_alloc(c.nc, es)
        moe_phase(c, g, X, bX, HB, bHB, ffn_g, w_router, w_gate, w_up, w_down, sb)
        barrier(c)


def proj_phase(c, g, X, bX, gain_ap, w_ap, nout, spec, PT, bPT, PV, bPV):
    nc = c.nc
    with ExitStack() as es:
        def T(name, shape, dt):
            return es.enter_context(nc.sbuf_tensor(uniq(name), shape, dt))
        wsb = T("pj_w", [128, 8, nout], BF16); bw = Buf()
        gB = T("pj_gB", [128, D], F32); bgB = Buf()
        tmpr = T("pj_tmpr", [1, D], F32); btmpr = Buf()
        xt = [T("pj_xt%d" % i, [128, 4, D], F32) for i in range(2)]; bxt = [Buf(), Buf()]
        junk = T("pj_junk", [128, D], F32); bjunk = Buf()
        ss = [T("pj_ss%d" % i, [128, 4], F32) for i in range(2)]; bss = [Buf(), Buf()]
        hb = [T("pj_hb%d" % i, [128, D], BF16) for i in range(2)]; bhb = [Buf(), Buf()]
        hT = [T("pj_hT%d" % i, [128, 8, 512], BF16) for i in range(2)]; bhT = [Buf(), Buf()]
        stF = [T("pj_stF%d" % i, [128, 512], F32) for i in range(3)]; bstF = [Buf() for _ in range(3)]
        stT = [T("pj_stT%d" % i, [128, 512], BF16) for i in range(3)]; bstT = [Buf() for _ in range(3)]
        bcast_row(c, g, gB, bgB, gain_ap, D, tmpr, btmpr)
        for c0 in range(0, nout, 512):
            wd = min(512, nout - c0)
            c.dma('pool', lambda e: e.dma_start(out=wsb[:, :, c0:c0 + wd], in_=w_ap[:, c0:c0 + wd].rearrange("(k p) f -> p k f", p=128)), writes=[bw])
        ptb, bptb = g.psb
        nF = 0; nT = 0; npz = 0
        for it in range(L // 512):
            t0 = it * 512
            x_, bx_ = xt[it % 2], bxt[it % 2]
            c.dma('sp', lambda e: e.dma_start(out=x_[:], in_=X[t0:t0 + 512, :].rearrange("(j p) d -> p j d", p=128)), reads=[bX], writes=[bx_])
            hT_, bhT_ = hT[it % 2], bhT[it % 2]
            for j in range(4):
                s_, bs_ = ss[j % 2], bss[j % 2]
                h_, bh_ = hb[j % 2], bhb[j % 2]
                c.op('dve', lambda e: e.scalar_tensor_tensor(out=junk[:], in0=x_[:, j, :], scalar=1.0, in1=x_[:, j, :], op0=ALU.mult, op1=ALU.mult,
                                                             accum_out=s_[:, 0:1]), reads=[bx_], writes=[bjunk, bs_])
                c.op('dve', lambda e: e.tensor_scalar(out=s_[:, 0:1], in0=s_[:, 0:1], scalar1=1.0 / D, scalar2=1e-6, op0=ALU.mult, op1=ALU.add),
                     reads=[bs_], writes=[bs_])
                c.op('act', lambda e: e.activation(out=s_[:, 0:1], in_=s_[:, 0:1], func=AF.Ln), reads=[bs_], writes=[bs_])
                c.op('act', lambda e: e.activation(out=s_[:, 0:1], in_=s_[:, 0:1], func=AF.Exp, scale=-0.5), reads=[bs_], writes=[bs_])
                c.op('dve', lambda e: e.scalar_tensor_tensor(out=h_[:], in0=x_[:, j, :], scalar=s_[:, 0:1], in1=gB[:], op0=ALU.mult, op1=ALU.mult),
                     reads=[bx_, bs_, bgB], writes=[bh_])
                for k in range(8):
                    c.op('pe', lambda e: e.transpose(out=ptb[:, k * 128:(k + 1) * 128], in_=h_[:, k * 128:(k + 1) * 128], identity=g.identb[:]),
                         reads=[bh_, g.b_identb], writes=[bptb])
                c.op('act', lambda e: e.activation(out=hT_[:, :, j * 128:(j + 1) * 128], in_=ptb[:, :].rearrange("p (k s) -> p k s", k=8), func=AF.Copy),
                     reads=[bptb], writes=[bhT_])
            for (col0, ncols, mode, dst0) in spec:
                if mode == 'F':
                    for f0 in range(0, ncols, 128):
                        fw = min(128, ncols - f0)
                        ps, bps = g.ps[npz % 4]; npz += 1
                        for k in range(8):
                            c.op('pe', lambda e: e.matmul(ps[0:fw, :], lhsT=wsb[:, k, col0 + f0:col0 + f0 + fw], rhs=hT_[:, k, :], start=(k == 0), stop=(k == 7)),
                                 reads=[bw, bhT_], writes=[bps])
                        st, bst = stF[nF % 3], bstF[nF % 3]; nF += 1
                        eng = 'act' if nF % 2 else 'dve'
                        if eng == 'act':
                            c.op('act', lambda e: e.activation(out=st[0:fw, :], in_=ps[0:fw, :], func=AF.Copy), reads=[bps], writes=[bst])
                        else:
                            c.op('dve', lambda e: e.tensor_copy(out=st[0:fw, :], in_=ps[0:fw, :]), reads=[bps], writes=[bst])
                        c.dma('sp' if nF % 2 else 'act', lambda e: e.dma_start(out=PT[dst0 + f0:dst0 + f0 + fw, t0:t0 + 512], in_=st[0:fw, :]), reads=[bst], pwrites=[bPT])
                else:
                    for j in range(4):
                        for c0 in range(0, ncols, 512):
                            cw = min(512, ncols - c0)
                            ps, bps = g.ps[npz % 4]; npz += 1
                            for k in range(8):
                                c.op('pe', lambda e: e.matmul(ps[:, 0:cw], lhsT=hT_[:, k, j * 128:(j + 1) * 128], rhs=wsb[:, k, col0 + c0:col0 + c0 + cw], start=(k == 0), stop=(k == 7)),
                                     reads=[bw, bhT_], writes=[bps])
                            st, bst = stT[nT % 3], bstT[nT % 3]; nT += 1
                            eng = 'act' if nT % 2 else 'dve'
                            if eng == 'act':
                                c.op('act', lambda e: e.activation(out=st[:, 0:cw], in_=ps[:, 0:cw], func=AF.Copy), reads=[bps], writes=[bst])
                            else:
                                c.op('dve', lambda e: e.tensor_copy(out=st[:, 0:cw], in_=ps[:, 0:cw]), reads=[bps], writes=[bst])
                            c.dma('sp' if nT % 2 else 'act', lambda e: e.dma_start(out=PV[t0 + j * 128:t0 + (j + 1) * 128, dst0 + c0:dst0 + c0 + cw], in_=st[:, 0:cw]),
                                  reads=[bst], pwrites=[bPV])
        bPT.seal(); bPV.seal()
        barrier(c)


TBK = 2048
NTB = L // TBK


def hgrn2_phase(c, g, PT, bPT, PV, bPV, lb_logits, jl, a_out_norm, QK, bQK, OA, bOAs, MT, bMT, stage=3):
    nc = c.nc
    with ExitStack() as es0:
        def T0(name, shape, dt):
            return es0.enter_context(nc.sbuf_tensor(uniq(name), shape, dt))
        mcols = T0("hg_mcols", [128, 8, 128], F32); bmcols = Buf()
        with ExitStack() as es:
            def T(name, shape, dt):
                return es.enter_context(nc.sbuf_tensor(uniq(name), shape, dt))
            lbt = T("hg_lbt", [128, 2, 2, 4], F32); blbt = Buf()
            lbc = T("hg_lbc", [128, 8], F32); blbc = Buf()
            oml = T("hg_oml", [128, 8], F32); boml = Buf()
            noml = T("hg_noml", [128, 8], F32); bnoml = Buf()
            msk = T("hg_msk", [128, TBK], F32); bmsk = Buf()
            bmid = T("hg_bmid", [128, 128], F32); bbmid = Buf()
            blast = T("hg_blast", [128, 128], F32); bblast = Buf()
            zq = [T("hg_zq%d" % i, [128, TBK], F32) for i in range(2)]; bzq = [Buf(), Buf()]
            zf = [T("hg_zf%d" % i, [128, TBK], F32) for i in range(2)]; bzf = [Buf(), Buf()]
            q_ = T("hg_q", [128, TBK], F32); bq_ = Buf()
            sig = T("hg_sig", [128, TBK], F32); bsig = Buf()
            f_ = T("hg_f", [128, TBK], F32); bf_ = Buf()
            kk = T("hg_kk", [128, TBK], F32); bkk = Buf()
            b_ = T("hg_b", [128, TBK], F32); bb_ = Buf()
            e1 = T("hg_e1", [128, TBK], F32); be1 = Buf()
            eq = T("hg_eq", [128, TBK], F32); beq = Buf()
            ek = T("hg_ek", [128, TBK], F32); bek = Buf()
            qt = [T("hg_qt%d" % i, [128, TBK], BF16) for i in range(2)]; bqt = [Buf(), Buf()]
            kt = [T("hg_kt%d" % i, [128, TBK], BF16) for i in range(2)]; bkt = [Buf(), Buf()]
            if jl == 0:
                c.op('pool', lambda e: e.memset(lbc[:], 0.0), writes=[blbc])
            else:
                with nc.allow_non_contiguous_dma(reason="tiny"):
                    for j_ in range(2):
                        for r_ in range(2):
                            c.dma('sp', lambda e: e.dma_start(out=lbt[:, j_, r_, :], in_=lb_logits[j_, r_, :].rearrange("(h p) -> p h", p=128)), pwrites=[blbt])
                blbt.seal()
                c.op('dve', lambda e: e.tensor_tensor(out=lbc[:].rearrange("p (r h) -> p r h", r=2), in0=lbt[:, 1, :, :], in1=lbt[:, 0, :, :], op=ALU.subtract),
                     reads=[blbt], writes=[blbc])
                c.op('act', lambda e: e.activation(out=lbc[:], in_=lbc[:], func=AF.Sigmoid), reads=[blbc], writes=[blbc])
            c.op('dve', lambda e: e.tensor_scalar(out=oml[:], in0=lbc[:], scalar1=-1.0, scalar2=1.0, op0=ALU.mult, op1=ALU.add), reads=[blbc], writes=[boml])
            c.op('dve', lambda e: e.tensor_scalar(out=noml[:], in0=oml[:], scalar1=-1.0, scalar2=None, op0=ALU.mult), reads=[boml], writes=[bnoml])
            c.op('pool', lambda e: e.memset(msk[:], 1.0), writes=[bmsk])
            c.op('pool', lambda e: e.memset(msk[:].rearrange("p (c j) -> p c j", j=64)[:, :, 0:1], 0.0), writes=[bmsk])
            n = 0
            for h in range(4):
                for r in range(2):
                    hr = r * 4 + h
                    for tb in range(NTB):
                        nb = tb if r == 0 else NTB - 1 - tb
                        zq_, bzq_ = zq[n % 2], bzq[n % 2]
                        zf_, bzf_ = zf[n % 2], bzf[n % 2]
                        qt_, bqt_ = qt[n % 2], bqt[n % 2]
                        kt_, bkt_ = kt[n % 2], bkt[n % 2]
                        n += 1
                        c.dma('sp', lambda e: e.dma_start(out=zq_[:], in_=PT[h * 128:(h + 1) * 128, nb * TBK:(nb + 1) * TBK]), reads=[bPT], writes=[bzq_])
                        fr = 512 + r * 512 + h * 128
                        c.dma('act', lambda e: e.dma_start(out=zf_[:], in_=PT[fr:fr + 128, nb * TBK:(nb + 1) * TBK]), reads=[bPT], writes=[bzf_])
                        zqs = zq_[:, ::-1] if r else zq_[:, :]
                        zfs = zf_[:, ::-1] if r else zf_[:, :]
                        c.op('act', lambda e: e.activation(out=q_[:], in_=zqs, func=AF.Silu), reads=[bzq_], writes=[bq_])
                        c.op('act', lambda e: e.activation(out=sig[:], in_=zfs, func=AF.Sigmoid), reads=[bzf_], writes=[bsig])
                        c.op('dve', lambda e: e.tensor_scalar(out=f_[:], in0=sig[:], scalar1=oml[:, hr:hr + 1], scalar2=lbc[:, hr:hr + 1], op0=ALU.mult, op1=ALU.add),
                             reads=[bsig, boml, blbc], writes=[bf_])
                        c.op('act', lambda e: e.activation(out=f_[:], in_=f_[:], func=AF.Ln), reads=[bf_], writes=[bf_])
                        c.op('dve', lambda e: e.tensor_scalar(out=kk[:], in0=sig[:], scalar1=noml[:, hr:hr + 1], scalar2=oml[:, hr:hr + 1], op0=ALU.mult, op1=ALU.add),
                             reads=[bsig, bnoml, boml], writes=[bkk])
                        c.op('dve', lambda e: e.tensor_tensor_scan(out=b_[:], data0=msk[:], data1=f_[:], initial=0.0, op0=ALU.mult, op1=ALU.add),
                             reads=[bmsk, bf_], writes=[bb_])
                        b3 = b_[:].rearrange("p (c j) -> p c j", j=64)
                        c.op('dve', lambda e: e.tensor_tensor(out=e1[:].rearrange("p (c j) -> p c j", j=64), in0=b3, in1=b3[:, :, 31:32].broadcast_to([128, TBK // 64, 64]), op=ALU.subtract),
                             reads=[bb_], writes=[be1])
                        c.op('act', lambda e: e.activation(out=eq[:], in_=e1[:], func=AF.Exp), reads=[be1], writes=[beq])
                        c.op('act', lambda e: e.activation(out=ek[:], in_=e1[:], func=AF.Exp, scale=-1.0), reads=[be1], writes=[bek])
                        c.op('dve', lambda e: e.tensor_tensor(out=qt_[:], in0=q_[:], in1=eq[:], op=ALU.mult), reads=[bq_, beq], writes=[bqt_])
                        c.op('dve', lambda e: e.tensor_tensor(out=kt_[:], in0=kk[:], in1=ek[:], op=ALU.mult), reads=[bkk, bek], writes=[bkt_])
                        nch = TBK // 64
                        c.op('act', lambda e: e.activation(out=bmid[:, tb * nch:(tb + 1) * nch], in_=b3[:, :, 31], func=AF.Copy), reads=[bb_], writes=[bbmid])
                        c.op('act', lambda e: e.activation(out=blast[:, tb * nch:(tb + 1) * nch], in_=b3[:, :, 63], func=AF.Copy), reads=[bb_], writes=[bblast])
                        c.dma('sp', lambda e: e.dma_start(out=QK[h, r, 0, :, tb * TBK:(tb + 1) * TBK], in_=qt_[:]), reads=[bqt_], pwrites=[bQK])
                        c.dma('act', lambda e: e.dma_start(out=QK[h, r, 1, :, tb * TBK:(tb + 1) * TBK], in_=kt_[:]), reads=[bkt_], pwrites=[bQK])
                    c.op('dve', lambda e: e.tensor_tensor(out=blast[:], in0=blast[:], in1=bmid[:], op=ALU.subtract), reads=[bblast, bbmid], writes=[bblast])
                    c.op('dve', lambda e: e.tensor_tensor(out=blast[:, 0:127], in0=blast[:, 0:127], in1=bmid[:, 1:128], op=ALU.add), reads=[bblast, bbmid], writes=[bblast])
                    c.op('act', lambda e: e.activation(out=mcols[:, hr, :], in_=blast[:], func=AF.Exp), reads=[bblast], writes=[bmcols])
            bQK.seal()
            barrier(c)
        if stage < 2:
            return
        with ExitStack() as es:
            def T(name, shape, dt):
                return es.enter_context(nc.sbuf_tensor(uniq(name), shape, dt))
            qb = [T("hr_qb%d" % i, [128, TBK], BF16) for i in range(2)]; bqb = [Buf(), Buf()]
            kb = [T("hr_kb%d" % i, [128, TBK], BF16) for i in range(2)]; bkb = [Buf(), Buf()]
            vb = [T("hr_vb%d" % i, [128, TBK // 128, 128], BF16) for i in range(2)]; bvb = [Buf(), Buf()]
            mask = T("hr_mask", [128, 128], F32); bmask = Buf()
            attnT = [T("hr_attn%d" % i, [128, 128], BF16) for i in range(2)]; battn = [Buf(), Buf()]
            ktok = [T("hr_ktok%d" % i, [128, 128], BF16) for i in range(2)]; bktok = [Buf(), Buf()]
            M32 = T("hr_M32", [128, 128], F32); bM32 = Buf()
            Mb = T("hr_Mb", [128, 128], BF16); bMb = Buf()
            tmp32 = T("hr_tmp32", [128, 128], F32); btmp32 = Buf()
            osb = [T("hr_osb%d" % i, [128, 128], F32) for i in range(3)]; bosb = [Buf() for _ in range(3)]
            osf = [T("hr_osf%d" % i, [128, 128], F32) for i in range(3)]; bosf = [Buf() for _ in range(3)]
            vnat = T("hr_vnat", [128, TBK // 128, 128], BF16); bvnat = Buf()
            c.op('pool', lambda e: e.memset(mask[:], 1.0), writes=[bmask])
            c.op('pool', lambda e: e.affine_select(out=mask[:], in_=mask[:], pattern=[[1, 128]], compare_op=ALU.is_ge, fill=0.0, base=0, channel_multiplier=-1),
                 reads=[bmask], writes=[bmask])
            c.op('pool', lambda e: e.memset(mask[0:64, 64:128], 0.0), reads=[bmask], writes=[bmask])
            ptb, bptb = g.psb
            n = 0; nblk = 0; nch = 0
            for h in range(4):
                for r in range(2):
                    hr = r * 4 + h
                    c.op('pool', lambda e: e.memset(M32[:], 0.0), writes=[bM32])
                    c.op('pool', lambda e: e.memset(Mb[:], 0.0), writes=[bMb])
                    for tb in range(NTB):
                        qb_, bqb_ = qb[n % 2], bqb[n % 2]
                        kb_, bkb_ = kb[n % 2], bkb[n % 2]
                        vb_, bvb_ = vb[n % 2], bvb[n % 2]
                        n += 1
                        c.dma('sp', lambda e: e.dma_start(out=qb_[:], in_=QK[h, r, 0, :, tb * TBK:(tb + 1) * TBK]), reads=[bQK], writes=[bqb_])
                        c.dma('act', lambda e: e.dma_start(out=kb_[:], in_=QK[h, r, 1, :, tb * TBK:(tb + 1) * TBK]), reads=[bQK], writes=[bkb_])
                        if r == 0:
                            vsrc = PV[tb * TBK:(tb + 1) * TBK, h * 128:(h + 1) * 128].rearrange("(b p) d -> p b d", p=128)
                            c.dma('sp', lambda e: e.dma_start(out=vb_[:], in_=vsrc), reads=[bPV], writes=[bvb_])
                        else:
                            vsrc = PV[L - (tb + 1) * TBK:L - tb * TBK, h * 128:(h + 1) * 128].rearrange("(b p) d -> p b d", p=128)
                            c.dma('sp', lambda e: e.dma_start(out=vnat[:], in_=vsrc), reads=[bPV], writes=[bvnat])
                            nbk = TBK // 128
                            for b4 in range(0, nbk, 4):
                                pf, bpf = g.ps[4 + (b4 // 4) % 2]
                                c.op('pe', lambda e: e.matmul(pf[:, :], lhsT=g.Jb[:], rhs=vnat[:, b4:b4 + 4, :], start=True, stop=True), reads=[g.b_Jb, bvnat], writes=[bpf])
                                for bb in range(4):
                                    c.op('act', lambda e: e.activation(out=vb_[:, nbk - 1 - (b4 + bb), :], in_=pf[:, bb * 128:(bb + 1) * 128], func=AF.Copy), reads=[bpf], writes=[bvb_])
                        for b in range(TBK // 128):
                            blk = tb * (TBK // 128) + b
                            at_, bat_ = attnT[nblk % 2], battn[nblk % 2]
                            kt_, bkt_ = ktok[nblk % 2], bktok[nblk % 2]
                            pa, bpa = g.ps[nblk % 2]
                            po, bpo = g.ps[2 + nblk % 2]
                            os_, bos_ = osb[nblk % 3], bosb[nblk % 3]
                            nblk += 1
                            bs = slice(b * 128, (b + 1) * 128)
                            c.op('pe', lambda e: e.matmul(pa[:, 0:128], lhsT=kb_[:, bs], rhs=qb_[:, bs], start=True, stop=True), reads=[bkb_, bqb_], writes=[bpa])
                            c.op('dve', lambda e: e.tensor_tensor(out=at_[:], in0=pa[:, 0:128], in1=mask[:], op=ALU.mult), reads=[bpa, bmask], writes=[bat_])
                            c.op('pe', lambda e: e.transpose(out=ptb[:, 0:128], in_=kb_[:, bs], identity=g.identb[:]), reads=[bkb_, g.b_identb], writes=[bptb])
                            c.op('act', lambda e: e.activation(out=kt_[:], in_=ptb[:, 0:128], func=AF.Copy), reads=[bptb], writes=[bkt_])
                            for ci in range(2):
                                r0 = 64 * ci
                                cidx = 2 * blk + ci
                                pk, bpk = g.ps[4 + nch % 2]; nch += 1
                                c.op('pe', lambda e: e.matmul(po[r0:r0 + 64, 0:128], lhsT=at_[r0:r0 + 64, r0:r0 + 64], rhs=vb_[r0:r0 + 64, b, :], start=True, stop=False),
                                     reads=[bat_, bvb_], writes=[bpo])
                                c.op('pe', lambda e: e.matmul(po[r0:r0 + 64, 0:128], lhsT=qb_[:, b * 128 + r0:b * 128 + r0 + 64], rhs=Mb[:, :], start=False, stop=True),
                                     reads=[bqb_, bMb], writes=[bpo])
                                if cidx < 127:
                                    c.op('pe', lambda e: e.matmul(pk[:, 0:128], lhsT=kt_[r0:r0 + 64, :], rhs=vb_[r0:r0 + 64, b, :], start=True, stop=True),
                                         reads=[bkt_, bvb_], writes=[bpk])
                                    c.op('dve', lambda e: e.tensor_tensor(out=tmp32[:], in0=pk[:, 0:128], in1=M32[:], op=ALU.add), reads=[bpk, bM32], writes=[btmp32])
                                    c.op('dve', lambda e: e.tensor_scalar(out=M32[:], in0=tmp32[:], scalar1=mcols[:, hr, cidx:cidx + 1], scalar2=None, op0=ALU.mult),
                                         reads=[btmp32, bmcols], writes=[bM32])
                                    c.op('act', lambda e: e.activation(out=Mb[:], in_=M32[:], func=AF.Copy), reads=[bM32], writes=[bMb])
                            c.op('act', lambda e: e.activation(out=os_[:], in_=po[:, 0:128], func=AF.Copy), reads=[bpo], writes=[bos_])
                            if r == 0:
                                c.dma('sp', lambda e: e.dma_start(out=OA[blk * 128:(blk + 1) * 128, h * 128:(h + 1) * 128], in_=os_[:]), reads=[bos_], pwrites=[bOAs[h]])
                            else:
                                of_, bof_ = osf[nblk % 3], bosf[nblk % 3]
                                pf, bpf = g.ps[6]
                                c.op('pe', lambda e: e.matmul(pf[:, 0:128], lhsT=g.J32[:], rhs=os_[:], start=True, stop=True), reads=[g.b_J32, bos_], writes=[bpf])
                                c.op('act', lambda e: e.activation(out=of_[:], in_=pf[:, 0:128], func=AF.Copy), reads=[bpf], writes=[bof_])
                                c.dma('pool', lambda e: e.dma_start(out=OA[L - (blk + 1) * 128:L - blk * 128, h * 128:(h + 1) * 128], in_=of_[:], accum_op=ALU.add),
                                      reads=[bof_], pwrites=[bOAs[h]])
                    bOAs[h].seal()
            barrier(c)
        if stage < 3:
            return
        with ExitStack() as es:
            def T(name, shape, dt):
                return es.enter_context(nc.sbuf_tensor(uniq(name), shape, dt))
            gA = T("hf_gA", [128, 128], F32); bgA = Buf()
            tmpr = T("hf_tmpr", [1, 128], F32); btmpr = Buf()
            oa = [T("hf_oa%d" % i, [128, 512], F32) for i in range(2)]; boa = [Buf(), Buf()]
            ga = [T("hf_ga%d" % i, [128, 512], BF16) for i in range(2)]; bga = [Buf(), Buf()]
            sq = T("hf_sq", [128, 512], F32); bsq = Buf()
            ssq = [T("hf_ssq%d" % i, [128, 4], F32) for i in range(2)]; bssq = [Buf(), Buf()]
            sg = T("hf_sg", [128, 512], F32); bsg = Buf()
            t1 = T("hf_t1", [128, 512], F32); bt1 = Buf()
            ob = [T("hf_ob%d" % i, [128, 512], BF16) for i in range(2)]; bob = [Buf(), Buf()]
            mt = [T("hf_mt%d" % i, [128, 4, 512], BF16) for i in range(2)]; bmt = [Buf(), Buf()]
            bcast_row(c, g, gA, bgA, a_out_norm, 128, tmpr, btmpr)
            ptb, bptb = g.psb
            for i in range(NT):
                oa_, boa_ = oa[i % 2], boa[i % 2]
                ga_, bga_ = ga[i % 2], bga[i % 2]
                ss_, bss_ = ssq[i % 2], bssq[i % 2]
                ob_, bob_ = ob[i % 2], bob[i % 2]
                mt_, bmt_ = mt[(i // 4) % 2], bmt[(i // 4) % 2]
                c.dma('sp', lambda e: e.dma_start(out=oa_[:], in_=OA[i * 128:(i + 1) * 128, :]), reads=bOAs, writes=[boa_])
                c.dma('act', lambda e: e.dma_start(out=ga_[:], in_=PV[i * 128:(i + 1) * 128, 512:1024]), reads=[bPV], writes=[bga_])
                c.op('act', lambda e: e.activation(out=sq[:], in_=oa_[:], func=AF.Square), reads=[boa_], writes=[bsq])
                c.op('dve', lambda e: e.tensor_reduce(out=ss_[:], in_=sq[:].rearrange("p (h d) -> p h d", h=4), axis=AX.X, op=ALU.add), reads=[bsq], writes=[bss_])
                c.op('dve', lambda e: e.tensor_scalar(out=ss_[:], in0=ss_[:], scalar1=1.0 / 128, scalar2=1e-6, op0=ALU.mult, op1=ALU.add), reads=[bss_], writes=[bss_])
                c.op('pool', lambda e: e.tensor_tensor(out=ss_[:], in0=ss_[:], in1=g.neghalf[:, 0:1].broadcast_to([128, 4]), op=ALU.pow), reads=[bss_, g.b_neghalf], writes=[bss_])
                c.op('act', lambda e: e.activation(out=sg[:], in_=ga_[:], func=AF.Silu), reads=[bga_], writes=[bsg])
                c.op('dve', lambda e: e.tensor_tensor(out=t1[:].rearrange("p (h d) -> p h d", h=4), in0=oa_[:].rearrange("p (h d) -> p h d", h=4),
                                                      in1=ss_[:].unsqueeze(2).broadcast_to([128, 4, 128]), op=ALU.mult), reads=[boa_, bss_], writes=[bt1])
                c.op('dve', lambda e: e.tensor_tensor(out=t1[:].rearrange("p (h d) -> p h d", h=4), in0=t1[:].rearrange("p (h d) -> p h d", h=4),
                                                       in1=gA[:].unsqueeze(1).broadcast_to([128, 4, 128]), op=ALU.mult), reads=[bt1, bgA], writes=[bt1])
                c.op('dve', lambda e: e.tensor_tensor(out=ob_[:], in0=t1[:], in1=sg[:], op=ALU.mult), reads=[bt1, bsg], writes=[bob_])
                for k in range(4):
                    c.op('pe', lambda e: e.transpose(out=ptb[:, k * 128:(k + 1) * 128], in_=ob_[:, k * 128:(k + 1) * 128], identity=g.identb[:]),
                         reads=[bob_, g.b_identb], writes=[bptb])
                c.op('act', lambda e: e.activation(out=mt_[:, :, (i % 4) * 128:(i % 4 + 1) * 128], in_=ptb[:, 0:512].rearrange("p (k s) -> p k s", k=4), func=AF.Copy),
                     reads=[bptb], writes=[bmt_])
                if i % 4 == 3:
                    t0 = (i // 4) * 512
                    c.dma('sp', lambda e: e.dma_start(out=MT[0:512, t0:t0 + 512].rearrange("(k p) t -> p k t", p=128), in_=mt_[:]), reads=[bmt_], pwrites=[bMT])
            barrier(c)


TB5 = 1024
NTB5 = L // TB5
TWO_PI = 2.0 * math.pi


def s5_phase(c, g, PT, bPT, lam_re, lam_im, log_step, b_re, b_im, c_re, c_im, d_skip, glu_w, glu_b, YT, bYT, MT, bMT, stage=3):
    nc = c.nc
    U0 = 1536
    with ExitStack() as es0:
        def T0(name, shape, dt):
            return es0.enter_context(nc.sbuf_tensor(uniq(name), shape, dt))
        WB = [T0("s5_WB%d" % p, [128, 2, 4, 128], BF16) for p in range(2)]; bWB = Buf()
        WC = [T0("s5_WC%d" % p, [128, 2, 4, 128], BF16) for p in range(2)]; bWC = Buf()
        WBx = [T0("s5_WBx%d" % p, [128, 2, 4, 128], BF16) for p in range(2)]
        WCx = [T0("s5_WCx%d" % p, [128, 2, 4, 64], BF16) for p in range(2)]
        mag = T0("s5_mag", [128, 32], F32); bmag = Buf()
        pwc = T0("s5_pwc", [128, 11, 32], F32); bpw = Buf()
        pws = T0("s5_pws", [128, 11, 32], F32)
        with ExitStack() as es:
            def T(name, shape, dt):
                return es.enter_context(nc.sbuf_tensor(uniq(name), shape, dt))
            n_ = [0]

            def S(shape=[128, 32], dt=F32):
                n_[0] += 1
                return T("s5_t%d" % n_[0], shape, dt), Buf()
            lamre, blamre = S(); lamim, blamim = S()
            lsB, blsB = S([128, 64]); ls, bls = S()
            with nc.allow_non_contiguous_dma(reason="small params"):
                for r_ in range(2):
                    for g4 in range(0, 16, 4):
                        c.dma('sp', lambda e: e.dma_start(out=lamre[:, r_ * 16 + g4:r_ * 16 + g4 + 4], in_=lam_re[r_, 2 * g4:2 * g4 + 8, :].rearrange("(gp gl) n -> (gl n) gp", gl=2)), pwrites=[blamre])
                        c.dma('act', lambda e: e.dma_start(out=lamim[:, r_ * 16 + g4:r_ * 16 + g4 + 4], in_=lam_im[r_, 2 * g4:2 * g4 + 8, :].rearrange("(gp gl) n -> (gl n) gp", gl=2)), pwrites=[blamim])
                blamre.seal(); blamim.seal()
                c.dma('sp', lambda e: e.dma_start(out=lsB[:], in_=log_step.rearrange("r g -> (r g)").partition_broadcast(128)), writes=[blsB])
            lsv = lsB[:].rearrange("p (r gp gl) -> p r gp gl", r=2, gl=2)
            c.op('dve', lambda e: e.tensor_copy(out=ls[0:64, :].rearrange("p (r gp) -> p r gp", r=2), in_=lsv[0:64, :, :, 0]), reads=[blsB], writes=[bls])
            c.op('dve', lambda e: e.tensor_copy(out=ls[64:128, :].rearrange("p (r gp) -> p r gp", r=2), in_=lsv[64:128, :, :, 1]), reads=[blsB], writes=[bls])
            step, bstep = S()
            c.op('act', lambda e: e.activation(out=step[:], in_=ls[:], func=AF.Exp), reads=[bls], writes=[bstep])
            lrs, blrs = S(); ang, bang = S()
            c.op('dve', lambda e: e.tensor_tensor(out=lrs[:], in0=lamre[:], in1=step[:], op=ALU.mult), reads=[blamre, bstep], writes=[blrs])
            c.op('act', lambda e: e.activation(out=mag[:], in_=lrs[:], func=AF.Exp), reads=[blrs], writes=[bmag])
            c.op('dve', lambda e: e.tensor_tensor(out=ang[:], in0=lamim[:], in1=step[:], op=ALU.mult), reads=[blamim, bstep], writes=[bang])

            def sin_of(src, bsrc, offset, dst, bdst):
                q, bq = S(); qi, bqi = S(dt=I32); r, br = S(); m, bm = S()
                c.op('dve', lambda e: e.tensor_scalar(out=q[:], in0=src[:], scalar1=offset, scalar2=1.0 / TWO_PI, op0=ALU.add, op1=ALU.mult), reads=[bsrc], writes=[bq])
                c.op('dve', lambda e: e.tensor_copy(out=qi[:], in_=q[:]), reads=[bq], writes=[bqi])
                c.op('dve', lambda e: e.tensor_copy(out=q[:], in_=qi[:]), reads=[bqi], writes=[bq])
                c.op('dve', lambda e: e.scalar_tensor_tensor(out=r[:], in0=q[:], scalar=-TWO_PI, in1=src[:], op0=ALU.mult, op1=ALU.add), reads=[bq, bsrc], writes=[br])
                if offset != 0.0:
                    c.op('dve', lambda e: e.tensor_scalar(out=r[:], in0=r[:], scalar1=offset, scalar2=None, op0=ALU.add), reads=[br], writes=[br])
                c.op('dve', lambda e: e.tensor_scalar(out=m[:], in0=r[:], scalar1=math.pi, scalar2=-TWO_PI, op0=ALU.is_gt, op1=ALU.mult), reads=[br], writes=[bm])
                c.op('dve', lambda e: e.tensor_tensor(out=r[:], in0=r[:], in1=m[:], op=ALU.add), reads=[br, bm], writes=[br])
                c.op('dve', lambda e: e.tensor_scalar(out=m[:], in0=r[:], scalar1=-math.pi, scalar2=TWO_PI, op0=ALU.is_lt, op1=ALU.mult), reads=[br], writes=[bm])
                c.op('dve', lambda e: e.tensor_tensor(out=r[:], in0=r[:], in1=m[:], op=ALU.add), reads=[br, bm], writes=[br])
                c.op('dve', lambda e: e.tensor_scalar(out=r[:], in0=r[:], scalar1=math.pi, scalar2=-math.pi, op0=ALU.min, op1=ALU.max), reads=[br], writes=[br])
                c.op('act', lambda e: e.activation(out=dst, in_=r[:], func=AF.Sin), reads=[br], writes=[bdst])
            sin_of(ang, bang, 0.0, pws[:, 0, :], bpw)
            sin_of(ang, bang, math.pi / 2, pwc[:, 0, :], bpw)
            tq, btq = S(); tq2, btq2 = S()
            for k in range(10):
                c.op('dve', lambda e: e.tensor_tensor(out=tq[:], in0=pwc[:, k, :], in1=pwc[:, k, :], op=ALU.mult), reads=[bpw], writes=[btq])
                c.op('dve', lambda e: e.tensor_tensor(out=tq2[:], in0=pws[:, k, :], in1=pws[:, k, :], op=ALU.mult), reads=[bpw], writes=[btq2])
                c.op('dve', lambda e: e.tensor_tensor(out=pwc[:, k + 1, :], in0=tq[:], in1=tq2[:], op=ALU.subtract), reads=[btq, btq2, bpw], writes=[bpw])
                c.op('dve', lambda e: e.tensor_tensor(out=tq[:], in0=pws[:, k, :], in1=pwc[:, k, :], op=ALU.mult), reads=[bpw], writes=[btq])
                c.op('dve', lambda e: e.tensor_scalar(out=pws[:, k + 1, :], in0=tq[:], scalar1=2.0, scalar2=None, op0=ALU.mult), reads=[btq, bpw], writes=[bpw])
            are, bare = S(); aim, baim = S(); den, bden = S(); am1, bam1 = S(); fr, bfr = S(); fi, bfi = S(); tt, btt = S()
            c.op('dve', lambda e: e.tensor_tensor(out=are[:], in0=mag[:], in1=pwc[:, 0, :], op=ALU.mult), reads=[bmag, bpw], writes=[bare])
            c.op('dve', lambda e: e.tensor_tensor(out=aim[:], in0=mag[:], in1=pws[:, 0, :], op=ALU.mult), reads=[bmag, bpw], writes=[baim])
            c.op('dve', lambda e: e.tensor_tensor(out=den[:], in0=lamre[:], in1=lamre[:], op=ALU.mult), reads=[blamre], writes=[bden])
            c.op('dve', lambda e: e.tensor_tensor(out=tt[:], in0=lamim[:], in1=lamim[:], op=ALU.mult), reads=[blamim], writes=[btt])
            c.op('dve', lambda e: e.tensor_tensor(out=den[:], in0=den[:], in1=tt[:], op=ALU.add), reads=[bden, btt], writes=[bden])
            c.op('dve', lambda e: e.reciprocal(out=den[:], in_=den[:]), reads=[bden], writes=[bden])
            c.op('dve', lambda e: e.tensor_scalar(out=am1[:], in0=are[:], scalar1=-1.0, scalar2=None, op0=ALU.add), reads=[bare], writes=[bam1])
            c.op('dve', lambda e: e.tensor_tensor(out=fr[:], in0=am1[:], in1=lamre[:], op=ALU.mult), reads=[bam1, blamre], writes=[bfr])
            c.op('dve', lambda e: e.tensor_tensor(out=tt[:], in0=aim[:], in1=lamim[:], op=ALU.mult), reads=[baim, blamim], writes=[btt])
            c.op('dve', lambda e: e.tensor_tensor(out=fr[:], in0=fr[:], in1=tt[:], op=ALU.add), reads=[bfr, btt], writes=[bfr])
            c.op('dve', lambda e: e.tensor_tensor(out=fr[:], in0=fr[:], in1=den[:], op=ALU.mult), reads=[bfr, bden], writes=[bfr])
            c.op('dve', lambda e: e.tensor_tensor(out=fi[:], in0=aim[:], in1=lamre[:], op=ALU.mult), reads=[baim, blamre], writes=[bfi])
            c.op('dve', lambda e: e.tensor_tensor(out=tt[:], in0=am1[:], in1=lamim[:], op=ALU.mult), reads=[bam1, blamim], writes=[btt])
            c.op('dve', lambda e: e.tensor_tensor(out=fi[:], in0=fi[:], in1=tt[:], op=ALU.subtract), reads=[bfi, btt], writes=[bfi])
            c.op('dve', lambda e: e.tensor_tensor(out=fi[:], in0=fi[:], in1=den[:], op=ALU.mult), reads=[bfi, bden], writes=[bfi])
            mk, bmk = S([128, 2])
            c.op('pool', lambda e: e.memset(mk[:], 0.0), writes=[bmk])
            c.op('pool', lambda e: e.memset(mk[0:64, 0:1], 1.0), reads=[bmk], writes=[bmk])
            c.op('pool', lambda e: e.memset(mk[64:128, 1:2], 1.0), reads=[bmk], writes=[bmk])
            Bn = [S([128, 2, 16, 16]) for _ in range(2)]
            with nc.allow_non_contiguous_dma(reason="small params"):
                for r_ in range(2):
                    for g4 in range(0, 16, 4):
                        c.dma('sp', lambda e: e.dma_start(out=Bn[0][0][:, r_, g4:g4 + 4, :], in_=b_re[r_, 2 * g4:2 * g4 + 8].rearrange("(gp gl) n p -> (gl n) gp p", gl=2)), pwrites=[Bn[0][1]])
                        c.dma('act', lambda e: e.dma_start(out=Bn[1][0][:, r_, g4:g4 + 4, :], in_=b_im[r_, 2 * g4:2 * g4 + 8].rearrange("(gp gl) n p -> (gl n) gp p", gl=2)), pwrites=[Bn[1][1]])
                Bn[0][1].seal(); Bn[1][1].seal()
            frb = fr[:].rearrange("p (r gp) -> p r gp", r=2).unsqueeze(3).broadcast_to([128, 2, 16, 16])
            fib = fi[:].rearrange("p (r gp) -> p r gp", r=2).unsqueeze(3).broadcast_to([128, 2, 16, 16])
            bbr, bbbr = S([128, 2, 16, 16]); bbi, bbbi = S([128, 2, 16, 16]); t5, bt5 = S([128, 2, 16, 16])
            c.op('dve', lambda e: e.tensor_tensor(out=bbr[:], in0=Bn[0][0][:], in1=frb, op=ALU.mult), reads=[Bn[0][1], bfr], writes=[bbbr])
            c.op('dve', lambda e: e.tensor_tensor(out=t5[:], in0=Bn[1][0][:], in1=fib, op=ALU.mult), reads=[Bn[1][1], bfi], writes=[bt5])
            c.op('dve', lambda e: e.tensor_tensor(out=bbr[:], in0=bbr[:], in1=t5[:], op=ALU.subtract), reads=[bbbr, bt5], writes=[bbbr])
            c.op('dve', lambda e: e.tensor_tensor(out=bbi[:], in0=Bn[1][0][:], in1=frb, op=ALU.mult), reads=[Bn[1][1], bfr], writes=[bbbi])
            c.op('dve', lambda e: e.tensor_tensor(out=t5[:], in0=Bn[0][0][:], in1=fib, op=ALU.mult), reads=[Bn[0][1], bfi], writes=[bt5])
            c.op('dve', lambda e: e.tensor_tensor(out=bbi[:], in0=bbi[:], in1=t5[:], op=ALU.add), reads=[bbbi, bt5], writes=[bbbi])
            BBm, bBBm = S([128, 2, 16, 2, 16], BF16)
            ptb, bptb = g.psb
            for part, (src, bsrc) in enumerate(((bbr, bbbr), (bbi, bbbi))):
                for gl in range(2):
                    c.op('dve', lambda e: e.tensor_scalar(out=BBm[:, :, :, gl, :], in0=src[:], scalar1=mk[:, gl:gl + 1], scalar2=None, op0=ALU.mult), reads=[bsrc, bmk, bBBm], writes=[bBBm])
                for r in range(2):
                    for cb in range(4):
                        c.op('pe', lambda e: e.transpose(out=ptb[:, 0:128], in_=BBm[:, r, 4 * cb:4 * cb + 4, :, :].rearrange("p a b c -> p (a b c)"), identity=g.identb[:]),
                             reads=[bBBm, g.b_identb], writes=[bptb])
                        c.op('act', lambda e: e.activation(out=WB[part][:, r, cb, :], in_=ptb[:, 0:128], func=AF.Copy), reads=[bptb], writes=[bWB])
            Cn = [S([128, 2, 4, 64]) for _ in range(2)]
            c.dma('sp', lambda e: e.dma_start(out=Cn[0][0][:], in_=c_re.rearrange("r (cb g8) p n -> (g8 p) r cb n", g8=8)), writes=[Cn[0][1]])
            c.dma('act', lambda e: e.dma_start(out=Cn[1][0][:], in_=c_im.rearrange("r (cb g8) p n -> (g8 p) r cb n", g8=8)), writes=[Cn[1][1]])
            Cd, bCd = S([128, 2, 4, 2, 64], BF16)
            mkb = mk[:].unsqueeze(1).unsqueeze(3).broadcast_to([128, 4, 2, 16])
            for part in range(2):
                sc = 1.0 if part == 0 else -1.0
                for x in range(2):
                    c.op('dve', lambda e: e.tensor_scalar(out=Cd[:, :, :, x, :], in0=Cn[part][0][:], scalar1=sc, scalar2=None, op0=ALU.mult), reads=[Cn[part][1], bCd], writes=[bCd])
                for r in range(2):
                    for cb in range(4):
                        c.op('pe', lambda e: e.transpose(out=ptb[:, 0:128], in_=Cd[:, r, cb, :, :].rearrange("p a b -> p (a b)"), identity=g.identb[:]),
                             reads=[bCd, g.b_identb], writes=[bptb])
                        c.op('dve', lambda e: e.tensor_tensor(out=WC[part][:, r, cb, :].rearrange("p (k a b) -> p k a b", k=4, a=2), in0=ptb[:, 0:128].rearrange("p (k a b) -> p k a b", k=4, a=2),
                                                              in1=mkb, op=ALU.mult), reads=[bptb, bmk], writes=[bWC])
            for part in range(2):
                c.op('act', lambda e: e.activation(out=WBx[part][64:128], in_=WB[part][64:128], func=AF.Copy), reads=[bWB], writes=[bWB])
                c.op('pool', lambda e: e.memset(WBx[part][64:96], 0.0), reads=[bWB], writes=[bWB])
                c.op('act', lambda e: e.activation(out=WCx[part][:], in_=WC[part][:, :, :, 64:128], func=AF.Copy), reads=[bWC], writes=[bWC])
                c.op('pool', lambda e: e.memset(WCx[part][:, :, :, 0:32], 0.0), reads=[bWC], writes=[bWC])
            barrier(c)
        if stage < 2:
            return
        with ExitStack() as es:
            def T(name, shape, dt):
                return es.enter_context(nc.sbuf_tensor(uniq(name), shape, dt))
            uf = T("s5_uf", [128, TB5 * 2], F32); buf_ = Buf()
            ub = [T("s5_ub%d" % r, [128, L], BF16) for r in range(2)]; bub = [Buf(), Buf()]
            Xa = [[T("s5_X%d%d" % (p, r), [128, L], BF16) for r in range(2)] for p in range(2)]
            bXa = [[Buf(), Buf()], [Buf(), Buf()]]
            tcos = T("s5_cos", [128, TB5], F32); tsin = T("s5_sin", [128, TB5], F32); btab = Buf()
            BUs = [T("s5_BU%d" % p, [128, TB5], F32) for p in range(2)]; bBUs = [Buf(), Buf()]
            t = [T("s5_w%d" % i, [128, TB5], F32) for i in range(4)]; bt = [Buf() for _ in range(4)]
            ini = T("s5_ini", [128, 4], F32); bini = Buf()
            yst = [T("s5_yst%d" % i, [128, 512], F32) for i in range(2)]; byst = [Buf(), Buf()]
            npz = 0
            for cb in range(4):
                for r in range(2):
                    for hh in range(L // (2 * TB5)):
                        nb = hh if r == 0 else L // (2 * TB5) - 1 - hh
                        c.dma('sp', lambda e: e.dma_start(out=uf[:], in_=PT[U0 + cb * 128:U0 + (cb + 1) * 128, nb * 2 * TB5:(nb + 1) * 2 * TB5]), reads=[bPT], writes=[buf_])
                        src = uf[:, ::-1] if r else uf[:, :]
                        c.op('act', lambda e: e.activation(out=ub[r][:, hh * 2 * TB5:(hh + 1) * 2 * TB5], in_=src, func=AF.Copy), reads=[buf_], writes=[bub[r]])
                for k in range(4):
                    gp = cb * 4 + k
                    for r in range(2):
                        col = r * 16 + gp
                        c.op('pool', lambda e: e.memset(tcos[:, 0:1], 1.0), writes=[btab])
                        c.op('pool', lambda e: e.memset(tsin[:, 0:1], 0.0), reads=[btab], writes=[btab])
                        n = 1
                        kk = 0
                        while n < TB5:
                            cr = pwc[:, kk, col:col + 1]; ci = pws[:, kk, col:col + 1]
                            c.op('dve', lambda e: e.tensor_scalar(out=t[0][:, 0:n], in0=tsin[:, 0:n], scalar1=ci, scalar2=None, op0=ALU.mult), reads=[btab, bpw], writes=[bt[0]])
                            c.op('dve', lambda e: e.tensor_scalar(out=t[1][:, 0:n], in0=tsin[:, 0:n], scalar1=cr, scalar2=None, op0=ALU.mult), reads=[btab, bpw], writes=[bt[1]])
                            c.op('dve', lambda e: e.scalar_tensor_tensor(out=tsin[:, n:2 * n], in0=tcos[:, 0:n], scalar=ci, in1=t[1][:, 0:n], op0=ALU.mult, op1=ALU.add),
                                 reads=[btab, bpw, bt[1]], writes=[btab])
                            c.op('dve', lambda e: e.scalar_tensor_tensor(out=tcos[:, n:2 * n], in0=tcos[:, 0:n], scalar=cr, in1=t[0][:, 0:n], op0=ALU.mult, op1=ALU.subtract),
                                 reads=[btab, bpw, bt[0]], writes=[btab])
                            n *= 2; kk += 1
                        cTB = pwc[:, kk, col:col + 1]; sTB = pws[:, kk, col:col + 1]
                        rho = mag[:, col:col + 1]
                        c.op('pool', lambda e: e.memset(ini[:], 0.0), writes=[bini])
                        for tb in range(NTB5):
                            ts0 = tb * TB5
                            for part in range(2):
                                for hf in range(TB5 // 512):
                                    ps, bps = g.ps[npz % 4]; npz += 1
                                    if k < 3:
                                        lh = WB[part][32 * k:32 * k + 32, r, cb, :]; rh = ub[r][32 * k:32 * k + 32, ts0 + hf * 512:ts0 + (hf + 1) * 512]
                                    else:
                                        lh = WBx[part][64:128, r, cb, :]; rh = ub[r][64:128, ts0 + hf * 512:ts0 + (hf + 1) * 512]
                                    c.op('pe', lambda e: e.matmul(ps[:, :], lhsT=lh, rhs=rh, start=True, stop=True),
                                         reads=[bWB, bub[r]], writes=[bps])
                                    c.op('act', lambda e: e.activation(out=BUs[part][:, hf * 512:(hf + 1) * 512], in_=ps[:, :], func=AF.Copy), reads=[bps], writes=[bBUs[part]])
                            c.op('dve', lambda e: e.tensor_tensor(out=t[0][:], in0=BUs[0][:], in1=tcos[:], op=ALU.mult), reads=[bBUs[0], btab], writes=[bt[0]])
                            c.op('dve', lambda e: e.tensor_tensor(out=t[1][:], in0=BUs[1][:], in1=tsin[:], op=ALU.mult), reads=[bBUs[1], btab], writes=[bt[1]])
                            c.op('dve', lambda e: e.tensor_tensor(out=t[0][:], in0=t[0][:], in1=t[1][:], op=ALU.add), reads=[bt[0], bt[1]], writes=[bt[0]])
                            c.op('pool', lambda e: e.tensor_tensor(out=t[2][:], in0=BUs[1][:], in1=tcos[:], op=ALU.mult), reads=[bBUs[1], btab], writes=[bt[2]])
                            c.op('dve', lambda e: e.tensor_tensor(out=t[3][:], in0=BUs[0][:], in1=tsin[:], op=ALU.mult), reads=[bBUs[0], btab], writes=[bt[3]])
                            c.op('dve', lambda e: e.tensor_tensor(out=t[2][:], in0=t[2][:], in1=t[3][:], op=ALU.subtract), reads=[bt[2], bt[3]], writes=[bt[2]])
                            c.op('dve', lambda e: e.tensor_tensor_scan(out=t[1][:], data0=rho.broadcast_to([128, TB5]), data1=t[0][:], initial=ini[:, 0:1], op0=ALU.mult, op1=ALU.add),
                                 reads=[bmag, bt[0], bini], writes=[bt[1]])
                            c.op('dve', lambda e: e.tensor_tensor_scan(out=t[3][:], data0=rho.broadcast_to([128, TB5]), data1=t[2][:], initial=ini[:, 1:2], op0=ALU.mult, op1=ALU.add),
                                 reads=[bmag, bt[2], bini], writes=[bt[3]])
                            if tb < NTB5 - 1:
                                xr = t[1][:, TB5 - 1:TB5]; xi = t[3][:, TB5 - 1:TB5]
                                c.op('dve', lambda e: e.tensor_scalar(out=ini[:, 2:3], in0=xi, scalar1=sTB, scalar2=None, op0=ALU.mult), reads=[bt[3], bpw], writes=[bini])
                                c.op('dve', lambda e: e.scalar_tensor_tensor(out=ini[:, 0:1], in0=xr, scalar=cTB, in1=ini[:, 2:3], op0=ALU.mult, op1=ALU.subtract), reads=[bt[1], bpw, bini], writes=[bini])
                                c.op('dve', lambda e: e.tensor_scalar(out=ini[:, 3:4], in0=xi, scalar1=cTB, scalar2=None, op0=ALU.mult), reads=[bt[3], bpw], writes=[bini])
                                c.op('dve', lambda e: e.scalar_tensor_tensor(out=ini[:, 1:2], in0=xr, scalar=sTB, in1=ini[:, 3:4], op0=ALU.mult, op1=ALU.add), reads=[bt[1], bpw, bini], writes=[bini])
                            if r == 0:
                                oslc = slice(ts0, ts0 + TB5)
                                xo_re = Xa[0][r][:, oslc]; xo_im = Xa[1][r][:, oslc]
                            else:
                                lo = L - ts0 - TB5
                                xo_re = Xa[0][r][:, lo:lo + TB5][:, ::-1]; xo_im = Xa[1][r][:, lo:lo + TB5][:, ::-1]
                            c.op('dve', lambda e: e.tensor_tensor(out=t[0][:], in0=t[1][:], in1=tcos[:], op=ALU.mult), reads=[bt[1], btab], writes=[bt[0]])
                            c.op('pool', lambda e: e.tensor_tensor(out=t[2][:], in0=t[3][:], in1=tsin[:], op=ALU.mult), reads=[bt[3], btab], writes=[bt[2]])
                            c.op('dve', lambda e: e.tensor_tensor(out=xo_re, in0=t[0][:], in1=t[2][:], op=ALU.subtract), reads=[bt[0], bt[2]], pwrites=[bXa[0][r]])
                            c.op('dve', lambda e: e.tensor_tensor(out=t[0][:], in0=t[1][:], in1=tsin[:], op=ALU.mult), reads=[bt[1], btab], writes=[bt[0]])
                            c.op('dve', lambda e: e.tensor_tensor(out=t[2][:], in0=t[3][:], in1=tcos[:], op=ALU.mult), reads=[bt[3], btab], writes=[bt[2]])
                            c.op('dve', lambda e: e.tensor_tensor(out=xo_im, in0=t[0][:], in1=t[2][:], op=ALU.add), reads=[bt[0], bt[2]], pwrites=[bXa[1][r]])
                        bXa[0][r].seal(); bXa[1][r].seal()
                    for it in range(L // 512):
                        ps, bps = g.ps[4 + it % 2]
                        i = 0
                        for r in range(2):
                            for part in range(2):
                                if k < 3:
                                    po = ps[32 * k:32 * k + 32, :]; lh = WC[part][:, r, cb, 32 * k:32 * k + 32]
                                else:
                                    po = ps[64:128, :]; lh = WCx[part][:, r, cb, :]
                                c.op('pe', lambda e: e.matmul(po, lhsT=lh, rhs=Xa[part][r][:, it * 512:(it + 1) * 512], start=(i == 0), stop=(i == 3)),
                                     reads=[bWC, bXa[part][r]], writes=[bps])
                                i += 1
                        ys, bys = yst[it % 2], byst[it % 2]
                        e0 = 32 * k if k < 3 else 64
                        c.op('act', lambda e: e.activation(out=ys[e0:32 * k + 32, :], in_=ps[e0:32 * k + 32, :], func=AF.Copy), reads=[bps], writes=[bys])
                        c.dma('sp', lambda e: e.dma_start(out=YT[cb * 128 + 32 * k:cb * 128 + 32 * k + 32, it * 512:(it + 1) * 512], in_=ys[32 * k:32 * k + 32, :]), reads=[bys], pwrites=[bYT])
            bYT.seal()
            barrier(c)
        if stage < 3:
            return
        with ExitStack() as es:
            def T(name, shape, dt):
                return es.enter_context(nc.sbuf_tensor(uniq(name), shape, dt))
            gw = T("s5_gw", [128, 4, 512], BF16); bgw = Buf()
            dcol = T("s5_dcol", [128, 4], F32); bdcol = Buf()
            gbc = T("s5_gbc", [128, 4], F32); bgbc = Buf()
            yt = [T("s5_yt%d" % i, [128, 4, 512], F32) for i in range(2)]; byt = [Buf(), Buf()]
            ut = [T("s5_ut%d" % i, [128, 4, 512], F32) for i in range(2)]; but = [Buf(), Buf()]
            sq = T("s5_sq", [128, 4, 512], F32); bsq = Buf()
            gy = T("s5_gy", [128, 4, 512], F32); bgy = Buf()
            gyb = T("s5_gyb", [128, 4, 512], BF16); bgyb = Buf()
            sg = [T("s5_sg%d" % i, [128, 512], F32) for i in range(2)]; bsg = [Buf(), Buf()]
            ob = [T("s5_ob%d" % i, [128, 4, 512], BF16) for i in range(2)]; bob = [Buf(), Buf()]
            c.dma('pool', lambda e: e.dma_start(out=gw[:], in_=glu_w.rearrange("(k p) f -> p k f", p=128)), writes=[bgw])
            with nc.allow_non_contiguous_dma(reason="small params"):
                c.dma('sp', lambda e: e.dma_start(out=dcol[:], in_=d_skip.rearrange("(k p) -> p k", p=128)), writes=[bdcol])
                c.dma('sp', lambda e: e.dma_start(out=gbc[:], in_=glu_b.rearrange("(k p) -> p k", p=128)), writes=[bgbc])
            GC = 1.5957691216057308
            for it in range(L // 512):
                yt_, byt_ = yt[it % 2], byt[it % 2]
                ut_, but_ = ut[it % 2], but[it % 2]
                ob_, bob_ = ob[it % 2], bob[it % 2]
                tsl = slice(it * 512, (it + 1) * 512)
                c.dma('sp', lambda e: e.dma_start(out=yt_[:], in_=YT[:, tsl].rearrange("(k p) t -> p k t", p=128)), reads=[bYT], writes=[byt_])
                c.dma('act', lambda e: e.dma_start(out=ut_[:], in_=PT[U0:U0 + 512, tsl].rearrange("(k p) t -> p k t", p=128)), reads=[bPT], writes=[but_])
                for k in range(4):
                    c.op('dve', lambda e: e.scalar_tensor_tensor(out=yt_[:, k, :], in0=ut_[:, k, :], scalar=dcol[:, k:k + 1], in1=yt_[:, k, :], op0=ALU.mult, op1=ALU.add),
                         reads=[but_, bdcol, byt_], writes=[byt_])
                c.op('act', lambda e: e.activation(out=sq[:], in_=yt_[:], func=AF.Square), reads=[byt_], writes=[bsq])
                c.op('dve', lambda e: e.tensor_scalar(out=sq[:], in0=sq[:], scalar1=0.044715, scalar2=1.0, op0=ALU.mult, op1=ALU.add), reads=[bsq], writes=[bsq])
                c.op('dve', lambda e: e.tensor_tensor(out=sq[:], in0=sq[:], in1=yt_[:], op=ALU.mult), reads=[bsq, byt_], writes=[bsq])
                c.op('act', lambda e: e.activation(out=sq[:], in_=sq[:], func=AF.Sigmoid, scale=GC), reads=[bsq], writes=[bsq])
                c.op('dve', lambda e: e.tensor_tensor(out=gy[:], in0=sq[:], in1=yt_[:], op=ALU.mult), reads=[bsq, byt_], writes=[bgy])
                c.op('act', lambda e: e.activation(out=gyb[:], in_=gy[:], func=AF.Copy), reads=[bgy], writes=[bgyb])
                for co in range(4):
                    ps, bps = g.ps[co % 4]
                    for k in range(4):
                        c.op('pe', lambda e: e.matmul(ps[:, :], lhsT=gw[:, k, co * 128:(co + 1) * 128], rhs=gyb[:, k, :], start=(k == 0), stop=(k == 3)),
                             reads=[bgw, bgyb], writes=[bps])
                    sg_, bsg_ = sg[co % 2], bsg[co % 2]
                    c.op('act', lambda e: e.activation(out=sg_[:], in_=ps[:, :], func=AF.Sigmoid, bias=gbc[:, co:co + 1], scale=1.0), reads=[bps, bgbc], writes=[bsg_])
                    c.op('dve', lambda e: e.tensor_tensor(out=ob_[:, co, :], in0=gy[:, co, :], in1=sg_[:], op=ALU.mult), reads=[bgy, bsg_, bob_], writes=[bob_])
                c.dma('sp', lambda e: e.dma_start(out=MT[512:1024, tsl].rearrange("(k p) t -> p k t", p=128), in_=ob_[:]), reads=[bob_], pwrites=[bMT])
            barrier(c)


def outproj_phase(c, g, MT, bMT, w_out, X, bX):
    nc = c.nc
    bXn = Buf('Xn')
    with ExitStack() as es:
        def T(name, shape, dt):
            return es.enter_context(nc.sbuf_tensor(uniq(name), shape, dt))
        wsb = T("op_w", [128, 8, D], BF16); bw = Buf()
        mt = [T("op_mt%d" % i, [128, 8, 512], BF16) for i in range(2)]; bmt = [Buf(), Buf()]
        xt = [T("op_xt%d" % i, [128, 4, D], F32) for i in range(2)]; bxt = [Buf(), Buf()]
        xo = [T("op_xo%d" % i, [128, 4, D], F32) for i in range(2)]; bxo = [Buf(), Buf()]
        for c0 in range(0, D, 512):
            c.dma('pool', lambda e: e.dma_start(out=wsb[:, :, c0:c0 + 512], in_=w_out[:, c0:c0 + 512].rearrange("(k p) f -> p k f", p=128)), pwrites=[bw])
        bw.seal()
        n = 0
        for it in range(L // 512):
            t0 = it * 512
            mt_, bmt_ = mt[it % 2], bmt[it % 2]
            xt_, bxt_ = xt[it % 2], bxt[it % 2]
            xo_, bxo_ = xo[it % 2], bxo[it % 2]
            c.dma('sp', lambda e: e.dma_start(out=mt_[:], in_=MT[:, t0:t0 + 512].rearrange("(k p) t -> p k t", p=128)), reads=[bMT], writes=[bmt_])
            c.dma('act', lambda e: e.dma_start(out=xt_[:], in_=X[t0:t0 + 512, :].rearrange("(j p) d -> p j d", p=128)), reads=[bX], writes=[bxt_])
            for j in range(4):
                for dh in range(2):
                    ps, bps = g.ps[n % 4]; n += 1
                    for k in range(8):
                        c.op('pe', lambda e: e.matmul(ps[:, :], lhsT=mt_[:, k, j * 128:(j + 1) * 128], rhs=wsb[:, k, dh * 512:(dh + 1) * 512], start=(k == 0), stop=(k == 7)),
                             reads=[bmt_, bw], writes=[bps])
                    c.op('dve', lambda e: e.tensor_tensor(out=xo_[:, j, dh * 512:(dh + 1) * 512], in0=ps[:, :], in1=xt_[:, j, dh * 512:(dh + 1) * 512], op=ALU.add),
                         reads=[bps, bxt_, bxo_], writes=[bxo_])
            c.dma('sp', lambda e: e.dma_start(out=X[t0:t0 + 512, :].rearrange("(j p) d -> p j d", p=128), in_=xo_[:]), reads=[bxo_], pwrites=[bXn])
        bXn.seal()
        barrier(c)
    return bXn


def t5_onehot():
    half = 16; max_exact = 8
    rel = np.arange(-255, 256)
    n = np.abs(rel)
    nf = np.maximum(n, 1).astype(np.float32)
    large = max_exact + (np.log(nf / np.float32(max_exact)) / np.float32(math.log(128 / max_exact)) * np.float32(half - max_exact)).astype(np.int32)
    large = np.minimum(large, half - 1)
    b = np.where(rel > 0, half, 0) + np.where(n < max_exact, n, large)
    oh = np.zeros((32, 512), np.float32)
    oh[b, np.arange(511)] = 1.0
    return oh


def attn_phase(c, g, PV, bPV, q_gain, k_gain, c_lambda, out_gain, rel_bias, onehot, layer_idx, QKT, bQKT, FV, bFV, MT, bMT, stage=3):
    nc = c.nc
    lam_init = 0.8 - 0.6 * math.exp(-0.3 * layer_idx)
    ptb, bptb = g.psb
    with ExitStack() as es:
        def T(name, shape, dt):
            return es.enter_context(nc.sbuf_tensor(uniq(name), shape, dt))
        g64 = T("at_g64", [128, 2, 64], F32); bg64 = Buf()
        gQK = T("at_gQK", [128, 16, 64], F32); bgQK = Buf()
        xq = [T("at_xq%d" % i, [128, 1024], BF16) for i in range(2)]; bxq = [Buf(), Buf()]
        sq = T("at_sq", [128, 1024], F32); bsq = Buf()
        ss = [T("at_ss%d" % i, [128, 16], F32) for i in range(2)]; bss = [Buf(), Buf()]
        xn = T("at_xn", [128, 1024], F32); bxn = Buf()
        xb = [T("at_xb%d" % i, [128, 1024], BF16) for i in range(2)]; bxb = [Buf(), Buf()]
        st = [T("at_st%d" % i, [128, 8, 512], BF16) for i in range(2)]; bst = [Buf(), Buf()]
        c.dma('sp', lambda e: e.dma_start(out=g64[:, 0, :], in_=q_gain.partition_broadcast(128)), pwrites=[bg64])
        c.dma('sp', lambda e: e.dma_start(out=g64[:, 1, :], in_=k_gain.partition_broadcast(128)), pwrites=[bg64])
        bg64.seal()
        c.op('dve', lambda e: e.tensor_scalar(out=gQK[:, 0:8, :], in0=g64[:, 0:1, :].broadcast_to([128, 8, 64]), scalar1=0.125, scalar2=None, op0=ALU.mult), reads=[bg64], writes=[bgQK])
        c.op('dve', lambda e: e.tensor_copy(out=gQK[:, 8:16, :], in_=g64[:, 1:2, :].broadcast_to([128, 8, 64])), reads=[bg64, bgQK], writes=[bgQK])
        for i in range(NT):
            xq_, bxq_ = xq[i % 2], bxq[i % 2]
            ss_, bss_ = ss[i % 2], bss[i % 2]
            xb_, bxb_ = xb[i % 2], bxb[i % 2]
            st_, bst_ = st[(i // 4) % 2], bst[(i // 4) % 2]
            c.dma('sp', lambda e: e.dma_start(out=xq_[:], in_=PV[i * 128:(i + 1) * 128, 0:1024]), reads=[bPV], writes=[bxq_])
            c.op('act', lambda e: e.activation(out=sq[:], in_=xq_[:], func=AF.Square), reads=[bxq_], writes=[bsq])
            c.op('dve', lambda e: e.tensor_reduce(out=ss_[:], in_=sq[:].rearrange("p (a d) -> p a d", d=64), axis=AX.X, op=ALU.add), reads=[bsq], writes=[bss_])
            c.op('dve', lambda e: e.tensor_scalar(out=ss_[:], in0=ss_[:], scalar1=1.0 / 64, scalar2=1e-6, op0=ALU.mult, op1=ALU.add), reads=[bss_], writes=[bss_])
            c.op('pool', lambda e: e.tensor_tensor(out=ss_[:], in0=ss_[:], in1=g.neghalf[:, 0:1].broadcast_to([128, 16]), op=ALU.pow), reads=[bss_, g.b_neghalf], writes=[bss_])
            c.op('dve', lambda e: e.tensor_tensor(out=xn[:].rearrange("p (a d) -> p a d", d=64), in0=xq_[:].rearrange("p (a d) -> p a d", d=64),
                                                  in1=ss_[:].unsqueeze(2).broadcast_to([128, 16, 64]), op=ALU.mult), reads=[bxq_, bss_], writes=[bxn])
            c.op('dve', lambda e: e.tensor_tensor(out=xb_[:], in0=xn[:], in1=gQK[:].rearrange("p a d -> p (a d)"), op=ALU.mult), reads=[bxn, bgQK], writes=[bxb_])
            for a in range(8):
                c.op('pe', lambda e: e.transpose(out=ptb[:, a * 128:(a + 1) * 128], in_=xb_[:, a * 128:(a + 1) * 128], identity=g.identb[:]), reads=[bxb_, g.b_identb], writes=[bptb])
            c.op('act', lambda e: e.activation(out=st_[:, :, (i % 4) * 128:(i % 4 + 1) * 128], in_=ptb[:, :].rearrange("p (a s) -> p a s", a=8), func=AF.Copy), reads=[bptb], writes=[bst_])
            if i % 4 == 3:
                t0 = (i // 4) * 512
                for a in range(8):
                    c.dma('sp' if a % 2 else 'act', lambda e: e.dma_start(out=QKT[a, :, t0:t0 + 512], in_=st_[:, a, :]), reads=[bst_], pwrites=[bQKT])
        bQKT.seal()
        barrier(c)
    if stage < 2:
        return
    with ExitStack() as es:
        def T(name, shape, dt):
            return es.enter_context(nc.sbuf_tensor(uniq(name), shape, dt))
        KT = T("at_KT", [128, L], BF16); bKT = Buf()
        Va = T("at_Va", [128, 64, 130], BF16); bVa = Buf()
        QT = [T("at_QT%d" % i, [128, 512], BF16) for i in range(2)]; bQT = [Buf(), Buf()]
        Pt = [T("at_P%d" % i, [128, 512], BF16) for i in range(4)]; bPt = [Buf() for _ in range(4)]
        tmp = [T("at_tmp%d" % i, [128, 512], F32) for i in range(2)]; btmp = [Buf(), Buf()]
        biasT = T("at_bias", [128, 4, 3, 128], F32); bbias = Buf()
        hank = T("at_hank", [128, 128], F32); bhank = Buf()
        cfar = T("at_cfar", [128, 4, 2], F32); bcfar = Buf()
        tab = T("at_tab", [32, 4], F32); btab = Buf()
        oh = T("at_oh", [32, 512], F32); boh = Buf()
        fv = T("at_fv", [4, 512], F32); bfv = Buf()
        lamt = T("at_lamt", [128, 4, 64], F32); blamt = Buf()
        lam = T("at_lam", [128, 8], F32); blam = Buf()
        gO = T("at_gO", [128, 128], F32); bgO = Buf()
        rs = [T("at_rs%d" % i, [128, 4], F32) for i in range(2)]; brs = [Buf(), Buf()]
        t1 = T("at_t1", [128, 128], F32); bt1 = Buf()
        w_ = T("at_w", [128, 128], F32); bw_ = Buf()
        junk = T("at_junk", [128, 128], F32); bjunk = Buf()
        wb = [T("at_wb%d" % i, [128, 128], BF16) for i in range(2)]; bwb = [Buf(), Buf()]
        ost = [T("at_ost%d" % i, [128, 512], BF16) for i in range(2)]; bost = [Buf(), Buf()]
        c.dma('sp', lambda e: e.dma_start(out=lamt[:].rearrange("p a d -> p (a d)"), in_=c_lambda.rearrange("a d -> (a d)").partition_broadcast(128)), writes=[blamt])
        c.op('dve', lambda e: e.tensor_tensor(out=lamt[:, 0, :], in0=lamt[:, 0, :], in1=lamt[:, 1, :], op=ALU.mult), reads=[blamt], writes=[blamt])
        c.op('dve', lambda e: e.tensor_tensor(out=lamt[:, 2, :], in0=lamt[:, 2, :], in1=lamt[:, 3, :], op=ALU.mult), reads=[blamt], writes=[blamt])
        c.op('dve', lambda e: e.tensor_reduce(out=lam[:, 0:1], in_=lamt[:, 0, :], axis=AX.X, op=ALU.add), reads=[blamt], writes=[blam])
        c.op('dve', lambda e: e.tensor_reduce(out=lam[:, 1:2], in_=lamt[:, 2, :], axis=AX.X, op=ALU.add), reads=[blamt, blam], writes=[blam])
        c.op('act', lambda e: e.activation(out=lam[:, 2:4], in_=lam[:, 0:2], func=AF.Exp), reads=[blam], writes=[blam])
        c.op('dve', lambda e: e.tensor_tensor(out=lam[:, 4:5], in0=lam[:, 3:4], in1=lam[:, 2:3], op=ALU.subtract), reads=[blam], writes=[blam])
        c.op('dve', lambda e: e.tensor_scalar(out=lam[:, 4:5], in0=lam[:, 4:5], scalar1=-lam_init, scalar2=None, op0=ALU.add), reads=[blam], writes=[blam])
        c.dma('sp', lambda e: e.dma_start(out=gO[:], in_=out_gain.partition_broadcast(128)), writes=[bgO])
        c.op('dve', lambda e: e.tensor_scalar(out=gO[:], in0=gO[:], scalar1=1.0 - lam_init, scalar2=None, op0=ALU.mult), reads=[bgO], writes=[bgO])
        c.dma('sp', lambda e: e.dma_start(out=tab[:], in_=rel_bias), writes=[btab])
        c.dma('act', lambda e: e.dma_start(out=oh[:], in_=onehot), writes=[boh])
        ps6, bps6 = g.ps[6]
        c.op('pe', lambda e: e.matmul(ps6[0:4, :], lhsT=tab[:, :], rhs=oh[:, :], start=True, stop=True), reads=[btab, boh], writes=[bps6])
        c.op('dve', lambda e: e.tensor_copy(out=fv[:], in_=ps6[0:4, :]), reads=[bps6], writes=[bfv])
        c.dma('sp', lambda e: e.dma_start(out=FV, in_=fv[:]), reads=[bfv], writes=[bFV])
        for h in range(4):
            for o in (-1, 0, 1):
                off = h * 512 + 128 * o + 128
                src = bass.AP(FV.tensor, off, [[1, 128], [1, 128]])
                c.dma('sp', lambda e: e.dma_start(out=hank[:], in_=src), reads=[bFV], writes=[bhank])
                c.op('dve', lambda e: e.tensor_copy(out=biasT[:, h, o + 1, :], in_=hank[:, ::-1]), reads=[bhank, bbias], writes=[bbias])
            c.dma('sp', lambda e: e.dma_start(out=cfar[:, h, 0:1], in_=bass.AP(FV.tensor, h * 512 + 0, [[0, 128], [1, 1]])), reads=[bFV], pwrites=[bcfar])
            c.dma('sp', lambda e: e.dma_start(out=cfar[:, h, 1:2], in_=bass.AP(FV.tensor, h * 512 + 510, [[0, 128], [1, 1]])), reads=[bFV], pwrites=[bcfar])
        bcfar.seal()
        ones_col_done = False
        nS = 0; nP = 0; nq = 0; ntmp = 0; nout = 0
        for h in range(4):
            c.dma('sp', lambda e: e.dma_start(out=KT[:], in_=QKT[4 + h, :, :]), reads=[bQKT], writes=[bKT])
            for half in range(2):
                c.dma('act', lambda e: e.dma_start(out=Va[:, half * 32:(half + 1) * 32, 0:128], in_=PV[half * 4096:(half + 1) * 4096, 1024 + h * 128:1024 + (h + 1) * 128].rearrange("(b p) d -> p b d", p=128)),
                      reads=[bPV], writes=[bVa])
            c.op('pool', lambda e: e.memset(Va[:, :, 128:129], 1.0), reads=[bVa], writes=[bVa])
            for qt in range(16):
                QT_, bQT_ = QT[nq % 2], bQT[nq % 2]; nq += 1
                c.dma('sp', lambda e: e.dma_start(out=QT_[:], in_=QKT[h, :, qt * 512:(qt + 1) * 512]), reads=[bQKT], writes=[bQT_])
                steps = [(comp, kb) for kb in range(64) for comp in range(2)]
                Sbank = {}

                SB = [0, 1, 2, 6]

                def emit_S(i):
                    comp, kb = steps[i]
                    S, bS = g.ps[SB[i % 4]]
                    c.op('pe', lambda e: e.matmul(S[:, :], lhsT=KT[64 * comp:64 * comp + 64, kb * 128:(kb + 1) * 128], rhs=QT_[64 * comp:64 * comp + 64, :], start=True, stop=True),
                         reads=[bKT, bQT_], writes=[bS])

                def emit_exp(i):
                    comp, kb = steps[i]
                    S, bS = g.ps[SB[i % 4]]
                    P_, bP_ = Pt[i % 4], bPt[i % 4]
                    near = (4 * qt - 1 <= kb <= 4 * qt + 4)
                    if not near:
                        col = cfar[:, h, 0:1] if kb < 4 * qt else cfar[:, h, 1:2]
                        c.op('act', lambda e: e.activation(out=P_[:], in_=S[:, :], func=AF.Exp, bias=col, scale=1.0), reads=[bS, bcfar], writes=[bP_])
                    else:
                        tm, btm = tmp[i % 2], btmp[i % 2]
                        for qs in range(4):
                            o = kb - (4 * qt + qs)
                            sl = slice(qs * 128, (qs + 1) * 128)
                            if abs(o) <= 1:
                                c.op('dve', lambda e: e.tensor_tensor(out=tm[:, sl], in0=S[:, sl], in1=biasT[:, h, o + 1, :], op=ALU.add), reads=[bS, bbias, btm], writes=[btm])
                            else:
                                col = cfar[:, h, 0:1] if o < 0 else cfar[:, h, 1:2]
                                c.op('dve', lambda e: e.tensor_scalar(out=tm[:, sl], in0=S[:, sl], scalar1=col, scalar2=None, op0=ALU.add), reads=[bS, bcfar, btm], writes=[btm])
                        c.op('act', lambda e: e.activation(out=P_[:], in_=tm[:], func=AF.Exp), reads=[btm], writes=[bP_])

                def emit_PV(i):
                    comp, kb = steps[i]
                    P_, bP_ = Pt[i % 4], bPt[i % 4]
                    for qs in range(4):
                        a = comp * 4 + qs
                        acc, bacc = g.ps[3 + a // 3]
                        c0 = (a % 3) * 130
                        first = (kb == 0) and ((comp == 0 and a in (0, 3)) or (comp == 1 and a == 6))
                        c.op('pe', lambda e: e.matmul(acc[:, c0:c0 + 129], lhsT=P_[:, qs * 128:(qs + 1) * 128], rhs=Va[:, kb, 0:129], start=first, stop=(kb == 63), skip_group_check=True),
                             reads=[bP_, bVa], writes=[bacc])
                npair = len(steps) // 2
                emit_S(0); emit_S(1); emit_S(2); emit_S(3)
                for j in range(npair):
                    emit_exp(2 * j); emit_exp(2 * j + 1)
                    if j + 2 < npair:
                        emit_S(2 * j + 4); emit_S(2 * j + 5)
                    emit_PV(2 * j); emit_PV(2 * j + 1)
                os_, bos_ = ost[nout % 2], bost[nout % 2]; nout += 1
                for qs in range(4):
                    a0 = qs; a1 = 4 + qs
                    acc0, bacc0 = g.ps[3 + a0 // 3]; o0 = (a0 % 3) * 130
                    acc1, bacc1 = g.ps[3 + a1 // 3]; o1 = (a1 % 3) * 130
                    rs_, brs_ = rs[qs % 2], brs[qs % 2]
                    wb_, bwb_ = wb[qs % 2], bwb[qs % 2]
                    c.op('dve', lambda e: e.reciprocal(out=rs_[:, 0:1], in_=acc0[:, o0 + 128:o0 + 129]), reads=[bacc0], writes=[brs_])
                    c.op('dve', lambda e: e.reciprocal(out=rs_[:, 1:2], in_=acc1[:, o1 + 128:o1 + 129]), reads=[bacc1, brs_], writes=[brs_])
                    c.op('dve', lambda e: e.tensor_tensor(out=rs_[:, 1:2], in0=rs_[:, 1:2], in1=lam[:, 4:5], op=ALU.mult), reads=[brs_, blam], writes=[brs_])
                    c.op('dve', lambda e: e.tensor_scalar(out=t1[:], in0=acc1[:, o1:o1 + 128], scalar1=rs_[:, 1:2], scalar2=None, op0=ALU.mult), reads=[bacc1, brs_], writes=[bt1])
                    c.op('dve', lambda e: e.scalar_tensor_tensor(out=w_[:], in0=acc0[:, o0:o0 + 128], scalar=rs_[:, 0:1], in1=t1[:], op0=ALU.mult, op1=ALU.add), reads=[bacc0, brs_, bt1], writes=[bw_])
                    c.op('dve', lambda e: e.scalar_tensor_tensor(out=junk[:], in0=w_[:], scalar=1.0, in1=w_[:], op0=ALU.mult, op1=ALU.mult, accum_out=rs_[:, 2:3]), reads=[bw_, brs_], writes=[bjunk, brs_])
                    c.op('dve', lambda e: e.tensor_scalar(out=rs_[:, 2:3], in0=rs_[:, 2:3], scalar1=1.0 / 128, scalar2=1e-6, op0=ALU.mult, op1=ALU.add), reads=[brs_], writes=[brs_])
                    c.op('pool', lambda e: e.tensor_tensor(out=rs_[:, 2:3], in0=rs_[:, 2:3], in1=g.neghalf[:, 0:1], op=ALU.pow), reads=[brs_, g.b_neghalf], writes=[brs_])
                    c.op('dve', lambda e: e.scalar_tensor_tensor(out=wb_[:], in0=w_[:], scalar=rs_[:, 2:3], in1=gO[:], op0=ALU.mult, op1=ALU.mult), reads=[bw_, brs_, bgO], writes=[bwb_])
                    c.op('pe', lambda e: e.transpose(out=ptb[:, qs * 128:(qs + 1) * 128], in_=wb_[:], identity=g.identb[:]), reads=[bwb_, g.b_identb], writes=[bptb])
                c.op('act', lambda e: e.activation(out=os_[:], in_=ptb[:, 0:512], func=AF.Copy), reads=[bptb], writes=[bos_])
                c.dma('sp', lambda e: e.dma_start(out=MT[h * 128:(h + 1) * 128, qt * 512:(qt + 1) * 512], in_=os_[:]), reads=[bos_], pwrites=[bMT])
        barrier(c)


GTB = 512


def gated_norm_finalize(c, g, OA, bOAs, PV, bPV, gcol0, gain_ap, MT, bMT, row0, pfx, OA2=None, bOA2s=()):
    nc = c.nc
    with ExitStack() as es:
        def T(name, shape, dt):
            return es.enter_context(nc.sbuf_tensor(uniq(pfx + name), shape, dt))
        gA = T("gA", [128, 128], F32); bgA = Buf()
        oa = [T("oa%d" % i, [128, 512], F32) for i in range(2)]; boa = [Buf(), Buf()]
        oa2 = [T("oa2%d" % i, [128, 512], F32) for i in range(2)]; boa2 = [Buf(), Buf()]
        ga = [T("ga%d" % i, [128, 512], BF16) for i in range(2)]; bga = [Buf(), Buf()]
        sq = T("sq", [128, 512], F32); bsq = Buf()
        ssq = [T("ssq%d" % i, [128, 4], F32) for i in range(2)]; bssq = [Buf(), Buf()]
        sg = T("sg", [128, 512], F32); bsg = Buf()
        t1 = T("t1", [128, 512], F32); bt1 = Buf()
        ob = [T("ob%d" % i, [128, 512], BF16) for i in range(2)]; bob = [Buf(), Buf()]
        mt = [T("mt%d" % i, [128, 4, 512], BF16) for i in range(2)]; bmt = [Buf(), Buf()]
        c.dma('sp', lambda e: e.dma_start(out=gA[:], in_=gain_ap.partition_broadcast(128)), writes=[bgA])
        ptb, bptb = g.psb
        for i in range(NT):
            oa_, boa_ = oa[i % 2], boa[i % 2]
            ga_, bga_ = ga[i % 2], bga[i % 2]
            ss_, bss_ = ssq[i % 2], bssq[i % 2]
            ob_, bob_ = ob[i % 2], bob[i % 2]
            mt_, bmt_ = mt[(i // 4) % 2], bmt[(i // 4) % 2]
            c.dma('sp', lambda e: e.dma_start(out=oa_[:], in_=OA[i * 128:(i + 1) * 128, :]), reads=bOAs, writes=[boa_])
            c.dma('act', lambda e: e.dma_start(out=ga_[:], in_=PV[i * 128:(i + 1) * 128, gcol0:gcol0 + 512]), reads=[bPV], writes=[bga_])
            if OA2 is not None:
                o2_, bo2_ = oa2[i % 2], boa2[i % 2]
                c.dma('act', lambda e: e.dma_start(out=o2_[:], in_=OA2[i * 128:(i + 1) * 128, :]), reads=list(bOA2s), writes=[bo2_])
                c.op('dve', lambda e: e.tensor_tensor(out=oa_[:], in0=oa_[:], in1=o2_[:], op=ALU.add), reads=[boa_, bo2_], writes=[boa_])
            c.op('act', lambda e: e.activation(out=sq[:], in_=oa_[:], func=AF.Square), reads=[boa_], writes=[bsq])
            c.op('dve', lambda e: e.tensor_reduce(out=ss_[:], in_=sq[:].rearrange("p (h d) -> p h d", h=4), axis=AX.X, op=ALU.add), reads=[bsq], writes=[bss_])
            c.op('dve', lambda e: e.tensor_scalar(out=ss_[:], in0=ss_[:], scalar1=1.0 / 128, scalar2=1e-6, op0=ALU.mult, op1=ALU.add), reads=[bss_], writes=[bss_])
            c.op('pool', lambda e: e.tensor_tensor(out=ss_[:], in0=ss_[:], in1=g.neghalf[:, 0:1].broadcast_to([128, 4]), op=ALU.pow), reads=[bss_, g.b_neghalf], writes=[bss_])
            c.op('act', lambda e: e.activation(out=sg[:], in_=ga_[:], func=AF.Silu), reads=[bga_], writes=[bsg])
            c.op('dve', lambda e: e.tensor_tensor(out=t1[:].rearrange("p (h d) -> p h d", h=4), in0=oa_[:].rearrange("p (h d) -> p h d", h=4),
                                                  in1=ss_[:].unsqueeze(2).broadcast_to([128, 4, 128]), op=ALU.mult), reads=[boa_, bss_], writes=[bt1])
            c.op('dve', lambda e: e.tensor_tensor(out=t1[:].rearrange("p (h d) -> p h d", h=4), in0=t1[:].rearrange("p (h d) -> p h d", h=4),
                                                   in1=gA[:].unsqueeze(1).broadcast_to([128, 4, 128]), op=ALU.mult), reads=[bt1, bgA], writes=[bt1])
            c.op('dve', lambda e: e.tensor_tensor(out=ob_[:], in0=t1[:], in1=sg[:], op=ALU.mult), reads=[bt1, bsg], writes=[bob_])
            for k in range(4):
                c.op('pe', lambda e: e.transpose(out=ptb[:, k * 128:(k + 1) * 128], in_=ob_[:, k * 128:(k + 1) * 128], identity=g.identb[:]),
                     reads=[bob_, g.b_identb], writes=[bptb])
            c.op('act', lambda e: e.activation(out=mt_[:, :, (i % 4) * 128:(i % 4 + 1) * 128], in_=ptb[:, 0:512].rearrange("p (k s) -> p k s", k=4), func=AF.Copy),
                 reads=[bptb], writes=[bmt_])
            if i % 4 == 3:
                t0 = (i // 4) * 512
                c.dma('sp', lambda e: e.dma_start(out=MT[row0:row0 + 512, t0:t0 + 512].rearrange("(k p) t -> p k t", p=128), in_=mt_[:]), reads=[bmt_], pwrites=[bMT])
        barrier(c)


def gdn_phase(c, g, PT, bPT, PV, bPV, conv_w, a_log, dt_bias, out_gain, GQ, bGQ, GR, bGR, OD, bODs, OD2, bOD2s, MT, bMT, stage=4):
    nc = c.nc
    ptb, bptb = g.psb
    NB = TBK
    with ExitStack() as es:
        def T(name, shape, dt):
            return es.enter_context(nc.sbuf_tensor(uniq(name), shape, dt))
        cw = T("gd_cw", [128, 12, 5], F32); bcw = Buf()
        onesb = T("gd_onesb", [128, 128], BF16); bonesb = Buf()
        xin = [T("gd_xin%d" % i, [128, NB + 4], F32) for i in range(2)]; bxin = [Buf(), Buf()]
        y = T("gd_y", [128, NB], F32); by = Buf()
        s = T("gd_s", [128, NB], F32); bs = Buf()
        sqb = T("gd_sqb", [128, NB], BF16); bsqb = Buf()
        rst = T("gd_rst", [128, NB], F32); brst = Buf()
        ob = [T("gd_ob%d" % i, [128, NB], BF16) for i in range(2)]; bob = [Buf(), Buf()]
        with nc.allow_non_contiguous_dma(reason="small params"):
            for j in range(5):
                c.dma('sp', lambda e: e.dma_start(out=cw[:, :, j], in_=conv_w[j, :].rearrange("(k p) -> p k", p=128)), pwrites=[bcw])
        bcw.seal()
        c.op('pool', lambda e: e.memset(onesb[:], 1.0), writes=[bonesb])
        n = 0
        for cbk in range(12):
            for tb in range(L // NB):
                x_, bx_ = xin[n % 2], bxin[n % 2]
                o_, bo_ = ob[n % 2], bob[n % 2]
                n += 1
                t0 = tb * NB
                lo = max(t0 - 2, 0); hi = min(t0 + NB + 2, L)
                if tb == 0:
                    c.op('pool', lambda e: e.memset(x_[:, 0:2], 0.0), writes=[bx_])
                if tb == L // NB - 1:
                    c.op('pool', lambda e: e.memset(x_[:, NB + 2:NB + 4], 0.0), writes=[bx_])
                c.dma('sp', lambda e: e.dma_start(out=x_[:, lo - (t0 - 2):hi - (t0 - 2)], in_=PT[cbk * 128:(cbk + 1) * 128, lo:hi]), reads=[bPT, bx_], writes=[bx_])
                c.op('dve', lambda e: e.tensor_scalar(out=y[:], in0=x_[:, 0:NB], scalar1=cw[:, cbk, 0:1], scalar2=None, op0=ALU.mult), reads=[bx_, bcw], writes=[by])
                for j in range(1, 5):
                    c.op('dve', lambda e: e.scalar_tensor_tensor(out=y[:], in0=x_[:, j:j + NB], scalar=cw[:, cbk, j:j + 1], in1=y[:], op0=ALU.mult, op1=ALU.add),
                         reads=[bx_, bcw, by], writes=[by])
                c.op('act', lambda e: e.activation(out=s[:], in_=y[:], func=AF.Silu), reads=[by], writes=[bs])
                if cbk < 8:
                    c.op('act', lambda e: e.activation(out=sqb[:], in_=s[:], func=AF.Square), reads=[bs], writes=[bsqb])
                    for hf in range(NB // 512):
                        ps, bps = g.ps[hf % 4]
                        c.op('pe', lambda e: e.matmul(ps[:, :], lhsT=onesb[:], rhs=sqb[:, hf * 512:(hf + 1) * 512], start=True, stop=True), reads=[bonesb, bsqb], writes=[bps])
                        c.op('dve', lambda e: e.tensor_scalar(out=rst[:, hf * 512:(hf + 1) * 512], in0=ps[:, :], scalar1=1e-6, scalar2=None, op0=ALU.add), reads=[bps, brst], writes=[brst])
                    c.op('act', lambda e: e.activation(out=rst[:], in_=rst[:], func=AF.Ln), reads=[brst], writes=[brst])
                    c.op('act', lambda e: e.activation(out=rst[:], in_=rst[:], func=AF.Exp, scale=-0.5), reads=[brst], writes=[brst])
                    sc = (128.0 ** -0.5) if cbk < 4 else 1.0
                    c.op('dve', lambda e: e.scalar_tensor_tensor(out=o_[:], in0=s[:], scalar=sc, in1=rst[:], op0=ALU.mult, op1=ALU.mult), reads=[bs, brst], writes=[bo_])
                else:
                    c.op('act', lambda e: e.activation(out=o_[:], in_=s[:], func=AF.Copy), reads=[bs], writes=[bo_])
                c.dma('act', lambda e: e.dma_start(out=GQ[cbk, :, t0:t0 + NB], in_=o_[:]), reads=[bo_], pwrites=[bGQ])
        bGQ.seal()
        barrier(c)
    if stage < 2:
        return
    with ExitStack() as es0:
        def T0(name, shape, dt):
            return es0.enter_context(nc.sbuf_tensor(uniq(name), shape, dt))
        NQ = 5
        cols = [T0("gd_cols%d" % d, [128, 64, 4 * NQ], F32) for d in range(2)]; bcols = [Buf(), Buf()]
        sel = T0("gd_sel", [4, 4, 128], F32); bsel = Buf()
        with ExitStack() as es:
            def T(name, shape, dt):
                return es.enter_context(nc.sbuf_tensor(uniq(name), shape, dt))
            GP = 2048
            ar = T("gd_ar", [4, GP], F32); bar_ = Buf()
            br = T("gd_br", [4, GP], F32); bbr = Buf()
            w1 = T("gd_w1", [4, GP], F32); bw1 = Buf()
            w2 = T("gd_w2", [4, GP], F32); bw2 = Buf()
            gam = T("gd_gam", [4, GP], F32); bet = T("gd_bet", [4, GP], F32); egam = T("gd_egam", [4, GP], F32); brw = Buf()
            q3 = T("gd_q3", [4, GP], F32); bq3 = Buf()
            q4 = T("gd_q4", [4, GP], F32); bq4 = Buf()
            q5 = T("gd_q5", [4, GP], F32); bq5 = Buf()
            msk = T("gd_msk", [4, GP], F32); bmsk = Buf()
            pc = T("gd_pc", [4, 4], F32); bpc = Buf()
            c.op('pool', lambda e: e.memset(msk[:], 1.0), writes=[bmsk])
            c.op('pool', lambda e: e.memset(msk[:].rearrange("p (c j) -> p c j", j=64)[:, :, 0:1], 0.0), reads=[bmsk], writes=[bmsk])
            c.op('pool', lambda e: e.memset(sel[:], 0.0), writes=[bsel])
            c.op('pool', lambda e: e.affine_select(out=sel[:], in_=sel[:], pattern=[[-1, 4], [0, 128]], compare_op=ALU.not_equal, fill=1.0, base=0, channel_multiplier=1),
                 reads=[bsel], writes=[bsel])
            for d in range(2):
                with nc.allow_non_contiguous_dma(reason="small params"):
                    c.dma('sp', lambda e: e.dma_start(out=pc[:, 0:1], in_=dt_bias[d, :].rearrange("(h o) -> h o", o=1)), reads=[bpc], writes=[bpc])
                    c.dma('sp', lambda e: e.dma_start(out=pc[:, 1:2], in_=a_log[d, :].rearrange("(h o) -> h o", o=1)), reads=[bpc], writes=[bpc])
                c.op('act', lambda e: e.activation(out=pc[:, 2:3], in_=pc[:, 1:2], func=AF.Exp), reads=[bpc], writes=[bpc])
                c.op('dve', lambda e: e.tensor_scalar(out=pc[:, 2:3], in0=pc[:, 2:3], scalar1=-1.0, scalar2=None, op0=ALU.mult), reads=[bpc], writes=[bpc])
                for tp in range(L // GP):
                    nbp = tp if d == 0 else L // GP - 1 - tp
                    c.dma('sp', lambda e: e.dma_start(out=ar[:], in_=PT[1536 + 4 * d:1540 + 4 * d, nbp * GP:(nbp + 1) * GP]), reads=[bPT, bar_], writes=[bar_])
                    c.dma('act', lambda e: e.dma_start(out=br[:], in_=PT[1544 + 4 * d:1548 + 4 * d, nbp * GP:(nbp + 1) * GP]), reads=[bPT, bbr], writes=[bbr])
                    asrc = ar[:, ::-1] if d else ar[:, :]
                    bsrc = br[:, ::-1] if d else br[:, :]
                    c.op('dve', lambda e: e.tensor_scalar(out=w1[:], in0=asrc, scalar1=pc[:, 0:1], scalar2=None, op0=ALU.add), reads=[bar_, bpc], writes=[bw1])
                    c.op('dve', lambda e: e.tensor_scalar(out=w2[:], in0=w1[:], scalar1=-1.0, scalar2=None, op0=ALU.mult), reads=[bw1], writes=[bw2])
                    c.op('dve', lambda e: e.tensor_tensor(out=w2[:], in0=w2[:], in1=w1[:], op=ALU.min), reads=[bw1, bw2], writes=[bw2])
                    c.op('act', lambda e: e.activation(out=w2[:], in_=w2[:], func=AF.Exp), reads=[bw2], writes=[bw2])
                    c.op('act', lambda e: e.activation(out=w2[:], in_=w2[:], func=AF.Ln, bias=1.0, scale=1.0), reads=[bw2], writes=[bw2])
                    c.op('dve', lambda e: e.scalar_tensor_tensor(out=w1[:], in0=w1[:], scalar=0.0, in1=w2[:], op0=ALU.max, op1=ALU.add), reads=[bw1, bw2], writes=[bw1])
                    c.op('dve', lambda e: e.tensor_scalar(out=w1[:], in0=w1[:], scalar1=pc[:, 2:3], scalar2=None, op0=ALU.mult), reads=[bw1, bpc], writes=[bw1])
                    c.op('dve', lambda e: e.tensor_tensor_scan(out=gam[:], data0=msk[:], data1=w1[:], initial=0.0, op0=ALU.mult, op1=ALU.add), reads=[bmsk, bw1, brw], writes=[brw])
                    c.op('act', lambda e: e.activation(out=bet[:], in_=bsrc, func=AF.Sigmoid), reads=[bbr, brw], writes=[brw])
                    c.op('act', lambda e: e.activation(out=egam[:], in_=gam[:], func=AF.Exp), reads=[brw], writes=[brw])
                    c.op('dve', lambda e: e.tensor_tensor(out=q3[:], in0=bet[:], in1=egam[:], op=ALU.mult), reads=[brw, bq3], writes=[bq3])
                    g3 = gam[:].rearrange("p (c j) -> p c j", j=64)
                    c.op('dve', lambda e: e.tensor_tensor(out=q4[:].rearrange("p (c j) -> p c j", j=64), in0=g3[:, :, 63:64].broadcast_to([4, GP // 64, 64]), in1=g3, op=ALU.subtract),
                         reads=[brw, bq4], writes=[bq4])
                    c.op('act', lambda e: e.activation(out=q4[:], in_=q4[:], func=AF.Exp), reads=[bq4], writes=[bq4])
                    c.op('dve', lambda e: e.tensor_scalar(out=q5[:], in0=gam[:], scalar1=-1.0, scalar2=None, op0=ALU.mult), reads=[brw, bq5], writes=[bq5])
                    quants = [(gam, brw), (bet, brw), (q3, bq3), (q4, bq4), (q5, bq5)]
                    for bl in range(GP // 128):
                        blk = tp * (GP // 128) + bl
                        pc_, bpc_ = g.ps[blk % 2]
                        for qi, (qt_, bq_) in enumerate(quants):
                            c.op('pe', lambda e: e.transpose(out=pc_[:, qi * 4:(qi + 1) * 4], in_=qt_[0:4, bl * 128:(bl + 1) * 128], identity=g.ident32[0:4, 0:4]),
                                 reads=[bq_, g.b_ident32], writes=[bpc_])
                        c.op('act', lambda e: e.activation(out=cols[d][:, blk, :], in_=pc_[:, 0:4 * NQ], func=AF.Copy), reads=[bpc_, bcols[d]], writes=[bcols[d]])
                    for qi, rt in enumerate((gam, bet, egam)):
                        c.dma('sp', lambda e: e.dma_start(out=GR[d, qi, :, tp * GP:(tp + 1) * GP], in_=rt[:]), reads=[brw], pwrites=[bGR])
            bGR.seal()
            barrier(c)
        if stage < 3:
            return
        with ExitStack() as es:
            def T(name, shape, dt):
                return es.enter_context(nc.sbuf_tensor(uniq(name), shape, dt))
            nm_le = T("gm_nmle", [128, 128], F32)
            nm_geT = T("gm_nmgeT", [128, 128], F32)
            m_stT = T("gm_mstT", [128, 128], F32)
            bmk = Buf()
            c.op('pool', lambda e: e.memset(nm_le[:], 0.0), writes=[bmk])
            c.op('pool', lambda e: e.affine_select(out=nm_le[:], in_=nm_le[:], pattern=[[-1, 128]], compare_op=ALU.is_gt, fill=-30000.0, base=0, channel_multiplier=1), reads=[bmk], writes=[bmk])
            c.op('pool', lambda e: e.memset(nm_le[64:128, 0:64], -30000.0), reads=[bmk], writes=[bmk])
            c.op('pool', lambda e: e.memset(nm_geT[:], 0.0), reads=[bmk], writes=[bmk])
            c.op('pool', lambda e: e.affine_select(out=nm_geT[:], in_=nm_geT[:], pattern=[[1, 128]], compare_op=ALU.is_ge, fill=-30000.0, base=0, channel_multiplier=-1), reads=[bmk], writes=[bmk])
            c.op('pool', lambda e: e.memset(nm_geT[0:64, 64:128], -30000.0), reads=[bmk], writes=[bmk])
            c.op('pool', lambda e: e.memset(m_stT[:], 1.0), reads=[bmk], writes=[bmk])
            c.op('pool', lambda e: e.affine_select(out=m_stT[:], in_=m_stT[:], pattern=[[1, 128]], compare_op=ALU.is_gt, fill=0.0, base=0, channel_multiplier=-1), reads=[bmk], writes=[bmk])

            class CH:
                pass
            chs = []
            for d in range(2):
                ch = CH(); chs.append(ch)
                ch.d = d

                def TT(name, shape, dt, d=d):
                    return (T("gm%d_%s" % (d, name), shape, dt), Buf())
                ch.nat = [TT("nat%d" % i, [128, GTB], BF16) for i in range(3)]
                ch.arr = [[TT("arr%d_%d" % (i, j), [128, GTB], BF16) for j in range(2)] for i in range(3)]
                ch.rts = [[TT("rt%d_%d" % (q, j), [4, GTB], F32) for j in range(2)] for q in range(3)]
                ch.S32 = TT("S32", [128, 128], F32); ch.Sb = TT("Sb", [128, 128], BF16)
                ch.tmpD = TT("tmpD", [128, 128], F32); ch.Dst = TT("Dst", [128, 128], F32); ch.DTi = TT("DTi", [128, 128], F32); ch.DTs = TT("DTs", [128, 128], F32)
                ch.A_ = TT("A", [128, 128], BF16); ch.AT_ = TT("AT", [128, 128], BF16); ch.atT = TT("attnT", [128, 128], BF16)
                ch.Pm = [TT("P%d" % i, [128, 128], BF16) for i in range(6)]
                ch.Qm = [TT("Q%d" % i, [128, 128], BF16) for i in range(5)]
                ch.W32 = TT("W32", [128, 256], F32); ch.Wb = TT("Wb", [128, 256], BF16)
                ch.kdec = TT("kdec", [128, 128], BF16); ch.kcT = TT("kcT", [128, 128], BF16); ch.qdec = TT("qdec", [128, 128], BF16); ch.vnew = TT("vnew", [128, 128], BF16)
                ch.elc = TT("elc", [128, 2], F32)
                ch.osb = [TT("osb%d" % i, [128, 128], F32) for i in range(2)]
                ch.osf = [TT("osf%d" % i, [128, 128], F32) for i in range(2)]
                ch.bA = g.ps[3 * d + 0]; ch.bB = g.ps[3 * d + 1]; ch.bC = g.ps[3 * d + 2]
                ch.pb0 = 512 * d
                ch.nblk = 0

            def block_gen(ch, h, blk, b, cur, rcur):
                d = ch.d
                (qA, bqA), (kA, bkA), (vA, bvA) = cur
                bs_ = slice(b * 128, (b + 1) * 128)
                cl = cols[d][:, blk, :]
                gcol = cl[:, 0 + h:0 + h + 1]; bcol = cl[:, 4 + h:4 + h + 1]; begcol = cl[:, 8 + h:8 + h + 1]
                ekdcol = cl[:, 12 + h:12 + h + 1]; ngcol = cl[:, 16 + h:16 + h + 1]
                pA, bpA = ch.bA; pB, bpB = ch.bB; pC, bpC = ch.bC
                pb0 = ch.pb0
                tmpD, btmpD = ch.tmpD; Dst, bDst = ch.Dst; DTi, bDTi = ch.DTi; DTs, bDTs = ch.DTs
                A_, bA_ = ch.A_; AT_, bAT_ = ch.AT_; atT, batT = ch.atT
                W32, bW32 = ch.W32; Wb, bWb = ch.Wb; kdec, bkdec = ch.kdec; kcT, bkcT = ch.kcT; qdec, bqdec = ch.qdec; vnew, bvnew = ch.vnew
                elc, belc = ch.elc; S32, bS32 = ch.S32; Sb, bSb = ch.Sb
                for qi, (rt, brt) in enumerate(rcur):
                    c.op('pe', lambda e: e.matmul(pA[:, qi * 128:(qi + 1) * 128], lhsT=sel[:, h, :], rhs=rt[0:4, bs_], start=True, stop=True, skip_group_check=True),
                         reads=[bsel, brt], writes=[bpA])
                    yield
                c.op('pe', lambda e: e.matmul(pB[:, 0:128], lhsT=kA[:, bs_], rhs=kA[:, bs_], start=True, stop=True, skip_group_check=True), reads=[bkA], writes=[bpB]); yield
                c.op('pe', lambda e: e.matmul(pB[:, 128:256], lhsT=kA[:, bs_], rhs=qA[:, bs_], start=True, stop=True, skip_group_check=True), reads=[bkA, bqA], writes=[bpB]); yield
                c.op('pe', lambda e: e.transpose(out=ptb[:, pb0:pb0 + 128], in_=vA[:, bs_], identity=g.identb[:]), reads=[bvA, g.b_identb], writes=[bptb]); yield
                c.op('pe', lambda e: e.transpose(out=ptb[:, pb0 + 128:pb0 + 256], in_=kA[:, bs_], identity=g.identb[:]), reads=[bkA, g.b_identb], writes=[bptb]); yield
                c.op('dve', lambda e: e.scalar_tensor_tensor(out=tmpD[:], in0=pA[:, 0:128], scalar=-1.0, in1=nm_le[:], op0=ALU.mult, op1=ALU.add), reads=[bpA, bmk], writes=[btmpD]); yield
                c.op('act', lambda e: e.activation(out=Dst[:], in_=tmpD[:], func=AF.Exp, bias=gcol, scale=1.0), reads=[btmpD, bcols[d]], writes=[bDst]); yield
                c.op('dve', lambda e: e.tensor_tensor(out=tmpD[:], in0=pA[:, 0:128], in1=nm_geT[:], op=ALU.add), reads=[bpA, bmk, btmpD], writes=[btmpD]); yield
                c.op('act', lambda e: e.activation(out=DTi[:], in_=tmpD[:], func=AF.Exp, bias=ngcol, scale=1.0), reads=[btmpD, bcols[d]], writes=[bDTi]); yield
                c.op('dve', lambda e: e.tensor_tensor(out=DTs[:], in0=DTi[:], in1=m_stT[:], op=ALU.mult), reads=[bDTi, bmk], writes=[bDTs]); yield
                c.op('dve', lambda e: e.tensor_tensor(out=DTs[:], in0=pA[:, 128:256], in1=DTs[:], op=ALU.mult), reads=[bpA, bDTs], writes=[bDTs]); yield
                c.op('dve', lambda e: e.scalar_tensor_tensor(out=A_[:], in0=pB[:, 0:128], scalar=bcol, in1=Dst[:], op0=ALU.mult, op1=ALU.mult), reads=[bpB, bcols[d], bDst], writes=[bA_]); yield
                c.op('dve', lambda e: e.tensor_tensor(out=AT_[:], in0=pB[:, 0:128], in1=DTs[:], op=ALU.mult), reads=[bpB, bDTs], writes=[bAT_]); yield
                c.op('dve', lambda e: e.tensor_tensor(out=atT[:], in0=pB[:, 128:256], in1=DTi[:], op=ALU.mult), reads=[bpB, bDTi], writes=[batT]); yield
                c.op('dve', lambda e: e.tensor_scalar(out=Wb[:, 0:128], in0=ptb[:, pb0:pb0 + 128], scalar1=bcol, scalar2=None, op0=ALU.mult), reads=[bptb, bcols[d], bWb], writes=[bWb]); yield
                c.op('dve', lambda e: e.tensor_scalar(out=Wb[:, 128:256], in0=ptb[:, pb0 + 128:pb0 + 256], scalar1=begcol, scalar2=None, op0=ALU.mult), reads=[bptb, bcols[d], bWb], writes=[bWb]); yield
                c.op('act', lambda e: e.activation(out=kdec[:], in_=ptb[:, pb0 + 128:pb0 + 256], func=AF.Copy, scale=ekdcol), reads=[bptb, bcols[d]], writes=[bkdec]); yield
                c.op('dve', lambda e: e.tensor_tensor(out=qdec[:], in0=pA[:, 256:384], in1=qA[:, bs_], op=ALU.mult), reads=[bpA, bqA], writes=[bqdec]); yield
                c.op('act', lambda e: e.activation(out=elc[:], in_=pA[:, 256:384].rearrange("p (c j) -> p c j", j=64)[:, :, 63], func=AF.Copy), reads=[bpA], writes=[belc]); yield
                Pc, bPc = AT_, bAT_
                Qc, bQc = A_, bA_
                for lev in range(6):
                    c.op('pe', lambda e: e.matmul(pC[:, 0:256], lhsT=Pc[:], rhs=Wb[:], start=True, stop=True, skip_group_check=True), reads=[bPc, bWb], writes=[bpC]); yield
                    c.op('dve', lambda e: e.tensor_tensor(out=Wb[:], in0=Wb[:], in1=pC[:, 0:256], op=(ALU.subtract if lev == 0 else ALU.add)), reads=[bWb, bpC], writes=[bWb]); yield
                    if lev < 5:
                        Pn, bPn = ch.Pm[lev + 1]
                        c.op('pe', lambda e: e.matmul(pB[:, 256:384], lhsT=Qc[:], rhs=Pc[:], start=True, stop=True, skip_group_check=True), reads=[bQc, bPc], writes=[bpB]); yield
                        if lev < 4:
                            Qn, bQn = ch.Qm[lev + 1]
                            c.op('pe', lambda e: e.matmul(pB[:, 384:512], lhsT=Pc[:], rhs=Qc[:], start=True, stop=True, skip_group_check=True), reads=[bQc, bPc], writes=[bpB]); yield
                            c.op('dve', lambda e: e.tensor_copy(out=Qn[:], in_=pB[:, 384:512]), reads=[bpB], writes=[bQn]); yield
                        c.op('act', lambda e: e.activation(out=Pn[:], in_=pB[:, 256:384], func=AF.Copy), reads=[bpB], writes=[bPn]); yield
                        Pc, bPc = Pn, bPn
                        if lev < 4:
                            Qc, bQc = Qn, bQn
                c.op('pe', lambda e: e.transpose(out=ptb[:, pb0 + 256:pb0 + 384], in_=Wb[:, 128:256], identity=g.identb[:]), reads=[bWb, g.b_identb], writes=[bptb]); yield
                c.op('act', lambda e: e.activation(out=kcT[:], in_=ptb[:, pb0 + 256:pb0 + 384], func=AF.Copy), reads=[bptb], writes=[bkcT]); yield
                for ci in range(2):
                    r0 = 64 * ci
                    c.op('pe', lambda e: e.matmul(pC[r0:r0 + 64, 256:384], lhsT=kcT[:, r0:r0 + 64], rhs=Sb[:, :], start=True, stop=True, skip_group_check=True), reads=[bkcT, bSb], writes=[bpC]); yield
                    c.op('dve', lambda e: e.tensor_tensor(out=vnew[r0:r0 + 64, :], in0=Wb[r0:r0 + 64, 0:128], in1=pC[r0:r0 + 64, 256:384], op=ALU.subtract), reads=[bWb, bpC, bvnew], writes=[bvnew]); yield
                    c.op('pe', lambda e: e.matmul(pA[r0:r0 + 64, 384:512], lhsT=qdec[:, r0:r0 + 64], rhs=Sb[:, :], start=True, stop=False, skip_group_check=True), reads=[bqdec, bSb], writes=[bpA])
                    c.op('pe', lambda e: e.matmul(pA[r0:r0 + 64, 384:512], lhsT=atT[r0:r0 + 64, r0:r0 + 64], rhs=vnew[r0:r0 + 64, :], start=False, stop=True, skip_group_check=True), reads=[batT, bvnew], writes=[bpA]); yield
                    c.op('pe', lambda e: e.matmul(pC[:, 384:512], lhsT=kdec[r0:r0 + 64, :], rhs=vnew[r0:r0 + 64, :], start=True, stop=True, skip_group_check=True), reads=[bkdec, bvnew], writes=[bpC]); yield
                    c.op('dve', lambda e: e.scalar_tensor_tensor(out=Sb[:], in0=S32[:], scalar=elc[:, ci:ci + 1], in1=pC[:, 384:512], op0=ALU.mult, op1=ALU.add), reads=[bS32, belc, bpC], writes=[bSb]); yield
                    c.op('dve', lambda e: e.scalar_tensor_tensor(out=S32[:], in0=S32[:], scalar=elc[:, ci:ci + 1], in1=pC[:, 384:512], op0=ALU.mult, op1=ALU.add), reads=[bS32, belc, bpC], writes=[bS32]); yield
                os_, bos_ = ch.osb[ch.nblk % 2]
                of_, bof_ = ch.osf[ch.nblk % 2]
                ch.nblk += 1
                c.op('act', lambda e: e.activation(out=os_[:], in_=pA[:, 384:512], func=AF.Copy), reads=[bpA], writes=[bos_]); yield
                if d == 0:
                    c.dma('sp', lambda e: e.dma_start(out=OD[blk * 128:(blk + 1) * 128, h * 128:(h + 1) * 128], in_=os_[:]), reads=[bos_], pwrites=[bODs[h]]); yield
                else:
                    pf, bpf = g.ps[6]
                    c.op('pe', lambda e: e.matmul(pf[:, 0:128], lhsT=g.J32[:], rhs=os_[:], start=True, stop=True), reads=[g.b_J32, bos_], writes=[bpf]); yield
                    c.op('act', lambda e: e.activation(out=of_[:], in_=pf[:, 0:128], func=AF.Copy), reads=[bpf], writes=[bof_]); yield
                    c.dma('act', lambda e: e.dma_start(out=OD2[L - (blk + 1) * 128:L - blk * 128, h * 128:(h + 1) * 128], in_=of_[:]), reads=[bof_], pwrites=[bOD2s[h]]); yield

            for h in range(4):
                for ch in chs:
                    S32, bS32 = ch.S32; Sb, bSb = ch.Sb
                    c.op('pool', lambda e: e.memset(S32[:], 0.0), reads=[bS32], writes=[bS32])
                    c.op('pool', lambda e: e.memset(Sb[:], 0.0), reads=[bSb], writes=[bSb])
                for tb in range(L // GTB):
                    curs = []; rcurs = []
                    for ch in chs:
                        d = ch.d
                        cur = []
                        for ai in range(3):
                            a_, ba_ = ch.arr[ai][tb % 2]
                            if d == 0:
                                c.dma('sp' if ai % 2 else 'act', lambda e: e.dma_start(out=a_[:], in_=GQ[ai * 4 + h, :, tb * GTB:(tb + 1) * GTB]), reads=[bGQ], writes=[ba_])
                            else:
                                n_, bn_ = ch.nat[ai]
                                c.dma('sp' if ai % 2 else 'act', lambda e: e.dma_start(out=n_[:], in_=GQ[ai * 4 + h, :, L - (tb + 1) * GTB:L - tb * GTB]), reads=[bGQ], writes=[bn_])
                                c.op('pool', lambda e: e.tensor_copy(out=a_[:], in_=n_[:, ::-1]), reads=[bn_], writes=[ba_])
                            cur.append((a_, ba_))
                        rcur = []
                        for qi in range(3):
                            r_, br_ = ch.rts[qi][tb % 2]
                            c.dma('sp', lambda e: e.dma_start(out=r_[:], in_=GR[d, qi, :, tb * GTB:(tb + 1) * GTB]), reads=[bGR], writes=[br_])
                            rcur.append((r_, br_))
                        curs.append(cur); rcurs.append(rcur)
                    for b in range(GTB // 128):
                        blk = tb * (GTB // 128) + b
                        gens = [block_gen(ch, h, blk, b, curs[i], rcurs[i]) for i, ch in enumerate(chs)]
                        alive = list(gens)
                        while alive:
                            for gnr in list(alive):
                                try:
                                    next(gnr)
                                except StopIteration:
                                    alive.remove(gnr)
                bODs[h].seal(); bOD2s[h].seal()
            barrier(c)
    if stage < 4:
        return
    gated_norm_finalize(c, g, OD, bODs, PV, bPV, 1536, out_gain, MT, bMT, 512, "gf_", OA2=OD2, bOA2s=bOD2s)


N_ACTIVE = 4
DEPTH = 4

PARAM_NAMES = ['mix_norm', 'ffn_norm', 'ev_w_in', 'ev_w_out', 'a_lb_logits', 'a_out_norm', 's5_lambda_re', 's5_lambda_im',
               's5_log_step', 's5_b_re', 's5_b_im', 's5_c_re', 's5_c_im', 's5_d', 's5_glu_w', 's5_glu_b', 'od_w_in', 'od_w_out',
               'c_q_norm', 'c_k_norm', 'c_lambda', 'c_out_norm', 'rel_bias', 'd_conv_w', 'd_a_log', 'd_dt_bias', 'd_out_norm',
               'moe_router', 'moe_w_gate', 'moe_w_up', 'moe_w_down']

PARAM_SHAPES = {
    'mix_norm': (4, 1024), 'ffn_norm': (4, 1024), 'ev_w_in': (2, 1024, 3072), 'ev_w_out': (2, 1024, 1024), 'a_lb_logits': (2, 2, 512),
    'a_out_norm': (2, 128), 's5_lambda_re': (2, 2, 32, 64), 's5_lambda_im': (2, 2, 32, 64), 's5_log_step': (2, 2, 32),
    's5_b_re': (2, 2, 32, 64, 16), 's5_b_im': (2, 2, 32, 64, 16), 's5_c_re': (2, 2, 32, 16, 64), 's5_c_im': (2, 2, 32, 16, 64),
    's5_d': (2, 512), 's5_glu_w': (2, 512, 512), 's5_glu_b': (2, 512), 'od_w_in': (2, 1024, 3600), 'od_w_out': (2, 1024, 1024),
    'c_q_norm': (2, 64), 'c_k_norm': (2, 64), 'c_lambda': (2, 4, 64), 'c_out_norm': (2, 128), 'rel_bias': (32, 4),
    'd_conv_w': (2, 5, 1536), 'd_a_log': (2, 2, 4), 'd_dt_bias': (2, 2, 4), 'd_out_norm': (2, 128), 'moe_router': (4, 1024, 16),
    'moe_w_gate': (4, 16, 1024, 2048), 'moe_w_up': (4, 16, 1024, 2048), 'moe_w_down': (4, 16, 2048, 1024)}


def build_program(layers=range(DEPTH), do_mixer=True, do_moe=True):
    nc = bass.Bass('TRN2', target_bir_lowering=False)
    xin = nc.dram_tensor("x", [L, D], F32, kind="ExternalInput").ap()
    P = {n: nc.dram_tensor(n, list(PARAM_SHAPES[n]), F32, kind="ExternalInput").ap() for n in PARAM_NAMES}
    onehot = nc.dram_tensor("t5_onehot", [32, 512], F32, kind="ExternalInput").ap()
    X = nc.dram_tensor("y", [L, D], F32, kind="ExternalOutput").ap()
    PT = nc.dram_tensor("PT", [2048, L], F32).ap()
    PV = nc.dram_tensor("PV", [L, 2048], BF16).ap()
    QK = nc.dram_tensor("QK", [16, 128, L], BF16).ap()
    QK5 = QK.rearrange("(h r w) p t -> h r w p t", h=4, r=2)
    OA = nc.dram_tensor("OA", [L, 512], F32).ap()
    YT = nc.dram_tensor("YT", [512, L], F32).ap()
    MT = nc.dram_tensor("MT", [1024, L], BF16).ap()
    HB = nc.dram_tensor("HB", [L, D], BF16).ap()
    GQ = nc.dram_tensor("GQ", [12, 128, L], BF16).ap()
    GR = nc.dram_tensor("GR", [2, 3, 4, L], F32).ap()
    OD2 = nc.dram_tensor("OD2", [L, 512], F32).ap()
    FV = nc.dram_tensor("FV", [4, 512], F32).ap()
    c = Ctx(nc); g = G()
    setup_consts(c, g)
    bX = Buf('X'); bPT = Buf(); bPV = Buf(); bQK = Buf(); bOAs = [Buf() for _ in range(4)]; bYT = Buf(); bMT = Buf()
    bHB = Buf(); bGQ = Buf(); bGR = Buf(); bFV = Buf(); bOD2s = [Buf() for _ in range(4)]
    for r in range(0, L, 512):
        c.dma('sp', lambda e: e.dma_start(out=X[r:r + 512, :], in_=xin[r:r + 512, :]), pwrites=[bX])
    bX.seal()
    for layer in layers:
        j = layer // 2
        if do_mixer:
            if layer % 2 == 0:
                spec = [(0, 512, 'F', 0), (512, 512, 'F', 512), (1024, 512, 'F', 1024), (2560, 512, 'F', 1536), (1536, 512, 'T', 0), (2048, 512, 'T', 512)]
                proj_phase(c, g, X, bX, P['mix_norm'][layer], P['ev_w_in'][j], 3072, spec, PT, bPT, PV, bPV)
                hgrn2_phase(c, g, PT, bPT, PV, bPV, P['a_lb_logits'], j, P['a_out_norm'][j], QK5, bQK, OA, bOAs, MT, bMT)
                s5_phase(c, g, PT, bPT, P['s5_lambda_re'][j], P['s5_lambda_im'][j], P['s5_log_step'][j], P['s5_b_re'][j], P['s5_b_im'][j],
                         P['s5_c_re'][j], P['s5_c_im'][j], P['s5_d'][j], P['s5_glu_w'][j], P['s5_glu_b'][j], YT, bYT, MT, bMT)
                bMT.seal()
                bX = outproj_phase(c, g, MT, bMT, P['ev_w_out'][j], X, bX)
            else:
                spec = [(0, 512, 'T', 0), (512, 512, 'T', 512), (1024, 512, 'T', 1024), (1536, 1536, 'F', 0), (3072, 16, 'F', 1536), (3088, 512, 'T', 1536)]
                proj_phase(c, g, X, bX, P['mix_norm'][layer], P['od_w_in'][j], 3600, spec, PT, bPT, PV, bPV)
                attn_phase(c, g, PV, bPV, P['c_q_norm'][j], P['c_k_norm'][j], P['c_lambda'][j], P['c_out_norm'][j], P['rel_bias'], onehot, layer,
                           QK, bQK, FV, bFV, MT, bMT)
                gdn_phase(c, g, PT, bPT, PV, bPV, P['d_conv_w'][j], P['d_a_log'][j], P['d_dt_bias'][j], P['d_out_norm'][j], GQ, bGQ, GR, bGR, OA, bOAs, OD2, bOD2s, MT, bMT)
                bMT.seal()
                bX = outproj_phase(c, g, MT, bMT, P['od_w_out'][j], X, bX)
        if do_moe:
            moe_layer(c, g, X, bX, HB, bHB, P['ffn_norm'][layer], P['moe_router'][layer], P['moe_w_gate'][layer], P['moe_w_up'][layer], P['moe_w_down'][layer])
    barrier(c)
    c.finish([bX])
    return nc, c


def kernel(**inputs):
    x = np.ascontiguousarray(np.asarray(inputs['x'], dtype=np.float32))
    B = x.shape[0]
    assert B == N_ACTIVE and x.shape[1] == L and x.shape[2] == D
    nc, c = build_program()
    params = {n: np.ascontiguousarray(np.asarray(inputs[n], dtype=np.float32)) for n in PARAM_NAMES}
    oh = t5_onehot()
    in_maps = []
    for b in range(N_ACTIVE):
        m = {"x": x[b], "t5_onehot": oh}
        m.update(params)
        in_maps.append(m)
    res = run_bass_kernel_spmd(nc, in_maps, core_ids=list(range(N_ACTIVE)))
    out = np.stack([np.asarray(res.results[b]["y"], dtype=np.float32) for b in range(N_ACTIVE)], axis=0)
    return out
```

```python
import math
from contextlib import ExitStack

import numpy as np
import concourse.bass as bass
import concourse.mybir as mybir
from concourse.bass_utils import run_bass_kernel_spmd

F32 = mybir.dt.float32
BF16 = mybir.dt.bfloat16
U32 = mybir.dt.uint32
I32 = mybir.dt.int32
AF = mybir.ActivationFunctionType
ALU = mybir.AluOpType
AX = mybir.AxisListType


class Buf:
    __slots__ = ("name", "w", "r", "pw", "psum")

    def __init__(self, name="", psum=False):
        self.name = name
        self.psum = psum
        self.w = {}
        self.r = {}
        self.pw = {}

    def seal(self):
        for k, v in self.pw.items():
            if self.w.get(k, 0) < v:
                self.w[k] = v
        self.pw = {}


class Ctx:
    NDMA = 8

    def __init__(self, nc, same_engine_sync=True):
        self.nc = nc
        self.E = dict(pe=nc.tensor, dve=nc.vector, act=nc.scalar, pool=nc.gpsimd, sp=nc.sync)
        self.sem = {}
        self.cnt = {}
        for k in self.E:
            self.sem[k] = nc.alloc_semaphore("c_" + k)
            self.cnt[k] = 0
        self.dslot = {}
        for q in ("sp", "act", "pool"):
            for i in range(self.NDMA):
                key = "d_%s%d" % (q, i)
                self.sem[key] = nc.alloc_semaphore(key)
                self.cnt[key] = 0
            self.dslot[q] = 0
        self.seen = {k: {} for k in self.E}
        self.same = same_engine_sync
        self.ninst = 0

    def _wait(self, eng, tok):
        if tok is None:
            return
        key, val = tok
        if key == eng and (eng == "pe" or not self.same):
            return
        if self.seen[eng].get(key, 0) >= val:
            return
        self.E[eng].wait_ge(self.sem[key], val)
        self.seen[eng][key] = val

    def _deps(self, eng, reads, writes, pwrites=()):
        for b in reads:
            for k, v in b.w.items():
                self._wait(eng, (k, v))
            for k, v in b.pw.items():
                self._wait(eng, (k, v))
            if b.psum:
                for k, v in b.r.items():
                    if k != eng:
                        self._wait(eng, (k, v))
        for b in writes:
            for d in (b.w, b.pw, b.r):
                for k, v in d.items():
                    self._wait(eng, (k, v))
        for b in pwrites:
            for d in (b.w, b.r):
                for k, v in d.items():
                    self._wait(eng, (k, v))

    def _commit(self, tok, reads, writes, pwrites=()):
        k, v = tok
        for b in writes:
            b.w = {k: v}
            b.pw = {}
            b.r = {}
        for b in pwrites:
            if b.pw.get(k, 0) < v:
                b.pw[k] = v
        for b in reads:
            if b.r.get(k, 0) < v:
                b.r[k] = v

    def op(self, eng, fn, reads=(), writes=(), pwrites=()):
        self._deps(eng, reads, writes, pwrites)
        inst = fn(self.E[eng])
        self.cnt[eng] += 1
        tok = (eng, self.cnt[eng])
        inst.then_inc(self.sem[eng], 1)
        self._commit(tok, reads, writes, pwrites)
        self.ninst += 1
        return tok

    def dma(self, q, fn, reads=(), writes=(), pwrites=()):
        self._deps(q, reads, writes, pwrites)
        i = self.dslot[q]
        self.dslot[q] = (i + 1) % self.NDMA
        key = "d_%s%d" % (q, i)
        if self.cnt[key] > 0:
            self._wait(q, (key, self.cnt[key]))
        inst = fn(self.E[q])
        self.cnt[key] += 16
        tok = (key, self.cnt[key])
        inst.then_inc(self.sem[key], 16)
        self._commit(tok, reads, writes, pwrites)
        self.ninst += 1
        return tok

    def finish(self, bufs):
        for b in bufs:
            for d in (b.w, b.pw):
                for k, v in d.items():
                    self._wait("sp", (k, v))


def barrier(c):
    toks = [(k, v) for k, v in c.cnt.items() if v > 0]
    for eng in c.E:
        for tok in toks:
            c._wait(eng, tok)


_UNIQ = [0]


def uniq(name):
    _UNIQ[0] += 1
    return "%s_u%d" % (name, _UNIQ[0])


L = 8192
D = 1024
NE = 16
FF = 2048
CAP = 1024
NT = L // 128


class G:
    pass


def alloc_T(nc, name, shape, dtype, n=1, es=None):
    if es is None:
        return [(nc.alloc_sbuf_tensor("%s_%d" % (name, i), shape, dtype), Buf(name)) for i in range(n)]
    return [(es.enter_context(nc.sbuf_tensor(uniq("%s_%d" % (name, i)), shape, dtype)), Buf(name)) for i in range(n)]


def setup_consts(c, g):
    nc = c.nc
    g.ident32 = nc.alloc_sbuf_tensor("ident32", [128, 128], F32); g.b_ident32 = Buf()
    g.identb = nc.alloc_sbuf_tensor("identb", [128, 128], BF16); g.b_identb = Buf()
    g.ones32 = nc.alloc_sbuf_tensor("ones32", [1, 128], F32); g.b_ones32 = Buf()
    g.neghalf = nc.alloc_sbuf_tensor("neghalf", [128, 1], F32); g.b_neghalf = Buf()
    for t, b in ((g.ident32, g.b_ident32), (g.identb, g.b_identb)):
        c.op('pool', lambda e: e.memset(t[:], 0.0), writes=[b])
        c.op('pool', lambda e: e.affine_select(out=t[:], in_=t[:], pattern=[[-1, 128]], compare_op=ALU.not_equal,
                                               fill=1.0, base=0, channel_multiplier=1), reads=[b], writes=[b])
    g.J32 = nc.alloc_sbuf_tensor("J32", [128, 128], F32); g.b_J32 = Buf()
    g.Jb = nc.alloc_sbuf_tensor("Jb", [128, 128], BF16); g.b_Jb = Buf()
    for t, b in ((g.J32, g.b_J32), (g.Jb, g.b_Jb)):
        c.op('pool', lambda e: e.memset(t[:], 0.0), writes=[b])
        c.op('pool', lambda e: e.affine_select(out=t[:], in_=t[:], pattern=[[1, 128]], compare_op=ALU.not_equal,
                                               fill=1.0, base=-127, channel_multiplier=1), reads=[b], writes=[b])
    c.op('pool', lambda e: e.memset(g.ones32[:], 1.0), writes=[g.b_ones32])
    c.op('pool', lambda e: e.memset(g.neghalf[:], -0.5), writes=[g.b_neghalf])
    g.ps = []
    for i in range(7):
        g.ps.append((nc.alloc_psum_tensor("ps%d" % i, [128, 512], F32), Buf("ps%d" % i, psum=True)))
    g.psb = (nc.alloc_psum_tensor("psb", [128, 1024], BF16), Buf("psb", psum=True))


def bcast_row(c, g, dst, bdst, src_ap, n, tmp, btmp, psi=6):
    c.dma('sp', lambda e: e.dma_start(out=tmp[0:1, 0:n], in_=src_ap.rearrange("(o n) -> o n", o=1)), writes=[btmp])
    ps, bps = g.ps[psi]
    for h in range(0, n, 512):
        w = min(512, n - h)
        c.op('pe', lambda e: e.matmul(ps[:, 0:w], lhsT=g.ones32[0:1, :], rhs=tmp[0:1, h:h + w], start=True, stop=True),
             reads=[g.b_ones32, btmp], writes=[bps])
        c.op('dve', lambda e: e.tensor_copy(out=dst[:, h:h + w], in_=ps[:, 0:w]), reads=[bps], writes=[bdst])


def rmsnorm_tile(c, g, xt, bxt, gB, bgB, h32, bh32, junk, bjunk, ss, bss):
    c.op('dve', lambda e: e.scalar_tensor_tensor(out=junk[:], in0=xt[:], scalar=1.0, in1=xt[:], op0=ALU.mult, op1=ALU.mult,
                                                 accum_out=ss[:, 0:1]), reads=[bxt], writes=[bjunk, bss])
    c.op('dve', lambda e: e.tensor_scalar(out=ss[:, 0:1], in0=ss[:, 0:1], scalar1=1.0 / D, scalar2=1e-6, op0=ALU.mult, op1=ALU.add),
         reads=[bss], writes=[bss])
    c.op('act', lambda e: e.activation(out=ss[:, 0:1], in_=ss[:, 0:1], func=AF.Ln), reads=[bss], writes=[bss])
    c.op('act', lambda e: e.activation(out=ss[:, 0:1], in_=ss[:, 0:1], func=AF.Exp, scale=-0.5), reads=[bss], writes=[bss])
    c.op('dve', lambda e: e.scalar_tensor_tensor(out=h32[:], in0=xt[:], scalar=ss[:, 0:1], in1=gB[:], op0=ALU.mult, op1=ALU.mult),
         reads=[bxt, bss, bgB], writes=[bh32])


def moe_phase(c, g, X, bX, HB, bHB, ffn_g, w_router, w_gate, w_up, w_down, sb, stage=3):
    nc = c.nc
    gB, bgB = sb['gB']
    tmpr, btmpr = sb['tmprow']
    bcast_row(c, g, gB, bgB, ffn_g, D, tmpr, btmpr)
    wr, bwr = sb['wr']
    c.dma('sp', lambda e: e.dma_start(out=wr[:], in_=w_router.rearrange("(k p) e -> p k e", p=128)), writes=[bwr])
    affT, baffT = sb['affT']
    for i in range(NT):
        xt, bxt = sb['xt'][i % 2]
        h32, bh32 = sb['h32'][i % 2]
        hb, bhb = sb['hb'][i % 2]
        junk, bjunk = sb['junk']
        ss, bss = sb['ss'][i % 2]
        c.dma('sp', lambda e: e.dma_start(out=xt[:], in_=X[i * 128:(i + 1) * 128, :]), reads=[bX], writes=[bxt])
        rmsnorm_tile(c, g, xt, bxt, gB, bgB, h32, bh32, junk, bjunk, ss, bss)
        c.op('act', lambda e: e.activation(out=hb[:], in_=h32[:], func=AF.Copy), reads=[bh32], writes=[bhb])
        c.dma('act', lambda e: e.dma_start(out=HB[i * 128:(i + 1) * 128, :], in_=hb[:]), reads=[bhb], pwrites=[bHB])
        hT, bhT = sb['hT32'][i % 2]
        for hh in range(2):
            ps, bps = g.ps[hh]
            for k in range(4):
                kk = hh * 4 + k
                c.op('pe', lambda e: e.transpose(out=ps[:, k * 128:(k + 1) * 128], in_=h32[:, kk * 128:(kk + 1) * 128],
                                                 identity=g.ident32[:]), reads=[bh32, g.b_ident32], writes=[bps])
            c.op('act', lambda e: e.activation(out=hT[:, hh * 512:(hh + 1) * 512], in_=ps[:, :], func=AF.Copy),
                 reads=[bps], writes=[bhT])
        pl, bpl = g.ps[2 + (i % 2)]
        for k in range(8):
            c.op('pe', lambda e: e.matmul(pl[:, 0:NE], lhsT=hT[:, k * 128:(k + 1) * 128], rhs=wr[:, k, :], start=(k == 0), stop=(k == 7)),
                 reads=[bhT, bwr], writes=[bpl])
        sm, bsm = sb['sm'][i % 2]
        ex, bex = sb['ex'][i % 2]
        c.op('dve', lambda e: e.tensor_reduce(out=sm[:, 0:1], in_=pl[:, 0:NE], axis=AX.X, op=ALU.max), reads=[bpl], writes=[bsm])
        c.op('dve', lambda e: e.tensor_scalar(out=sm[:, 0:1], in0=sm[:, 0:1], scalar1=-1.0, scalar2=None, op0=ALU.mult), reads=[bsm], writes=[bsm])
        c.op('act', lambda e: e.activation(out=ex[:], in_=pl[:, 0:NE], func=AF.Exp, bias=sm[:, 0:1], scale=1.0, accum_out=sm[:, 1:2]),
             reads=[bpl, bsm], writes=[bex, bsm])
        c.op('dve', lambda e: e.reciprocal(out=sm[:, 2:3], in_=sm[:, 1:2]), reads=[bsm], writes=[bsm])
        c.op('dve', lambda e: e.tensor_scalar(out=ex[:], in0=ex[:], scalar1=sm[:, 2:3], scalar2=None, op0=ALU.mult), reads=[bex, bsm], writes=[bex])
        pt, bpt = g.ps[4 + (i % 2)]
        c.op('pe', lambda e: e.transpose(out=pt[0:NE, 0:128], in_=ex[:, 0:NE], identity=g.ident32[:]), reads=[bex, g.b_ident32], writes=[bpt])
        c.op('act', lambda e: e.activation(out=affT[0:NE, i * 128:(i + 1) * 128], in_=pt[0:NE, 0:128], func=AF.Copy), reads=[bpt], writes=[baffT])
    bHB.seal()
    if stage < 2:
        return
    vals, bvals = sb['vals']
    idxu, bidxu = sb['idxu']
    for it in range(CAP // 8):
        sl = slice(it * 8, it * 8 + 8)
        c.op('dve', lambda e: e.max(out=vals[:, sl], in_=affT[:, :]), reads=[baffT], writes=[bvals])
        c.op('dve', lambda e: e.max_index(out=idxu[:, sl], in_max=vals[:, sl], in_values=affT[:, :]), reads=[baffT, bvals], writes=[bidxu])
        c.op('dve', lambda e: e.match_replace(out=affT[:, :], in_to_replace=vals[:, sl], in_values=affT[:, :], imm_value=-1.0),
             reads=[bvals, baffT], writes=[baffT])
    idxf, bidxf = sb['idxf']
    c.op('dve', lambda e: e.tensor_copy(out=idxf[:], in_=idxu[:]), reads=[bidxu], writes=[bidxf])
    idxT, bidxT = sb['idxT']
    gateT, bgateT = sb['gateT']
    for j in range(8):
        pt, bpt = g.ps[4 + (j % 2)]
        c.op('pe', lambda e: e.transpose(out=pt[:, 0:NE], in_=idxf[0:NE, j * 128:(j + 1) * 128], identity=g.ident32[0:NE, 0:NE]),
             reads=[bidxf, g.b_ident32], writes=[bpt])
        c.op('dve', lambda e: e.tensor_copy(out=idxT[:, j, :], in_=pt[:, 0:NE]), reads=[bpt], writes=[bidxT])
        pt2, bpt2 = g.ps[2 + (j % 2)]
        c.op('pe', lambda e: e.transpose(out=pt2[:, 0:NE], in_=vals[0:NE, j * 128:(j + 1) * 128], identity=g.ident32[0:NE, 0:NE]),
             reads=[bvals, g.b_ident32], writes=[bpt2])
        c.op('act', lambda e: e.activation(out=gateT[:, j, :], in_=pt2[:, 0:NE], func=AF.Copy), reads=[bpt2], writes=[bgateT])
    if stage < 3:
        return
    xsT, bxsT = sb['xsT']
    hidT, bhidT = sb['hidT']
    yacc, byacc = sb['yacc']
    ptb, bptb = g.psb
    qi = 0
    for ex_i in range(NE):
        for j in range(8):
            xs, bxs = sb['xs'][j % 2]
            c.dma('pool', lambda e: e.indirect_dma_start(out=xs[:, :], out_offset=None, in_=HB[:, :],
                                                        in_offset=bass.IndirectOffsetOnAxis(ap=idxT[:, j, ex_i:ex_i + 1], axis=0)),
                  reads=[bHB, bidxT], writes=[bxs])
            for k in range(8):
                c.op('pe', lambda e: e.transpose(out=ptb[:, k * 128:(k + 1) * 128], in_=xs[:, k * 128:(k + 1) * 128], identity=g.identb[:]),
                     reads=[bxs, g.b_identb], writes=[bptb])
            c.op('dve', lambda e: e.tensor_copy(out=xsT[:, :, j * 128:(j + 1) * 128], in_=ptb[:, :].rearrange("p (k s) -> p k s", k=8)),
                 reads=[bptb], writes=[bxsT])
        for q in range(4):
            wg, bwg = sb['wg'][qi % 2]
            wu, bwu = sb['wu'][qi % 2]
            wd, bwd = sb['wd'][qi % 2]
            qi += 1
            f0 = q * 512
            c.dma('pool', lambda e: e.dma_start(out=wg[:], in_=w_gate[ex_i, :, f0:f0 + 512].rearrange("(k p) f -> p k f", p=128)), writes=[bwg])
            c.dma('pool', lambda e: e.dma_start(out=wu[:], in_=w_up[ex_i, :, f0:f0 + 512].rearrange("(k p) f -> p k f", p=128)), writes=[bwu])
            c.dma('pool', lambda e: e.dma_start(out=wd[:], in_=w_down[ex_i, f0:f0 + 512, :].rearrange("(k p) d -> p k d", p=128)), writes=[bwd])
            n = 0
            for fc in range(4):
                for sh in range(2):
                    pg, bpg = g.ps[0 + (n % 2)]
                    pu, bpu = g.ps[2 + (n % 2)]
                    sg, bsg = sb['sg'][n % 2]
                    n += 1
                    for k in range(8):
                        c.op('pe', lambda e: e.matmul(pg[:, :], lhsT=wg[:, k, fc * 128:(fc + 1) * 128], rhs=xsT[:, k, sh * 512:(sh + 1) * 512],
                                                      start=(k == 0), stop=(k == 7)), reads=[bwg, bxsT], writes=[bpg])
                    for k in range(8):
                        c.op('pe', lambda e: e.matmul(pu[:, :], lhsT=wu[:, k, fc * 128:(fc + 1) * 128], rhs=xsT[:, k, sh * 512:(sh + 1) * 512],
                                                      start=(k == 0), stop=(k == 7)), reads=[bwu, bxsT], writes=[bpu])
                    c.op('act', lambda e: e.activation(out=sg[:], in_=pg[:, :], func=AF.Silu), reads=[bpg], writes=[bsg])
                    c.op('dve', lambda e: e.tensor_tensor(out=hidT[:, fc, sh * 512:(sh + 1) * 512], in0=sg[:], in1=pu[:, :], op=ALU.mult),
                         reads=[bsg, bpu], writes=[bhidT])
            m = 0
            for j in range(8):
                for dh in range(2):
                    py, bpy = g.ps[4 + (m % 2)]
                    m += 1
                    for fc in range(4):
                        c.op('pe', lambda e: e.matmul(py[:, :], lhsT=hidT[:, fc, j * 128:(j + 1) * 128], rhs=wd[:, fc, dh * 512:(dh + 1) * 512],
                                                      start=(fc == 0), stop=(fc == 3)), reads=[bhidT, bwd], writes=[bpy])
                    ysl = yacc[:, j, dh * 512:(dh + 1) * 512]
                    gsc = gateT[:, j, ex_i:ex_i + 1]
                    if q == 0:
                        c.op('dve', lambda e: e.tensor_scalar(out=ysl, in0=py[:, :], scalar1=gsc, scalar2=None, op0=ALU.mult),
                             reads=[bpy, bgateT], writes=[byacc])
                    else:
                        c.op('dve', lambda e: e.scalar_tensor_tensor(out=ysl, in0=py[:, :], scalar=gsc, in1=ysl, op0=ALU.mult, op1=ALU.add),
                             reads=[bpy, bgateT, byacc], writes=[byacc])
        for j in range(8):
            c.dma('pool', lambda e: e.indirect_dma_start(out=X[:, :], out_offset=bass.IndirectOffsetOnAxis(ap=idxT[:, j, ex_i:ex_i + 1], axis=0),
                                                        in_=yacc[:, j, :], in_offset=None, compute_op=ALU.add),
                  reads=[byacc, bidxT], pwrites=[bX])
        bX.seal()


def moe_alloc(nc, es=None):
    sb = {}
    sb['gB'] = alloc_T(nc, 'gB', [128, D], F32, 1, es=es)[0]
    sb['tmprow'] = alloc_T(nc, 'tmprow', [1, 1024], F32, 1, es=es)[0]
    sb['wr'] = alloc_T(nc, 'wr', [128, 8, NE], F32, 1, es=es)[0]
    big = es.enter_context(nc.sbuf_tensor(uniq('big'), [128, L], F32)) if es is not None else nc.alloc_sbuf_tensor('big', [128, L], F32); bbig = Buf('big')
    sb['affT'] = (big[0:NE, :], bbig)
    sb['xt'] = alloc_T(nc, 'xt', [128, D], F32, 2, es=es)
    sb['h32'] = alloc_T(nc, 'h32', [128, D], F32, 2, es=es)
    sb['hb'] = alloc_T(nc, 'hb', [128, D], BF16, 2, es=es)
    sb['junk'] = alloc_T(nc, 'junk', [128, D], F32, 1, es=es)[0]
    sb['ss'] = alloc_T(nc, 'ss', [128, 4], F32, 2, es=es)
    sb['hT32'] = alloc_T(nc, 'hT32', [128, D], F32, 2, es=es)
    sb['sm'] = alloc_T(nc, 'sm', [128, 4], F32, 2, es=es)
    sb['ex'] = alloc_T(nc, 'ex', [128, NE], F32, 2, es=es)
    sb['vals'] = alloc_T(nc, 'vals', [NE, CAP], F32, 1, es=es)[0]
    sb['idxu'] = alloc_T(nc, 'idxu', [NE, CAP], U32, 1, es=es)[0]
    sb['idxf'] = alloc_T(nc, 'idxf', [NE, CAP], F32, 1, es=es)[0]
    sb['idxT'] = alloc_T(nc, 'idxT', [128, 8, NE], U32, 1, es=es)[0]
    sb['gateT'] = alloc_T(nc, 'gateT', [128, 8, NE], F32, 1, es=es)[0]
    sb['xsT'] = alloc_T(nc, 'xsT', [128, 8, CAP], BF16, 1, es=es)[0]
    sb['hidT'] = alloc_T(nc, 'hidT', [128, 4, CAP], BF16, 1, es=es)[0]
    sb['yacc'] = (big[:, :].rearrange('p (j d) -> p j d', j=8), bbig)
    sb['xs'] = alloc_T(nc, 'xs', [128, D], BF16, 2, es=es)
    sb['wg'] = alloc_T(nc, 'wg', [128, 8, 512], BF16, 2, es=es)
    sb['wu'] = alloc_T(nc, 'wu', [128, 8, 512], BF16, 2, es=es)
    sb['wd'] = alloc_T(nc, 'wd', [128, 4, D], BF16, 2, es=es)
    sb['sg'] = alloc_T(nc, 'sg', [128, 512], BF16, 2, es=es)
    return sb


def moe_layer(c, g, X, bX, HB, bHB, ffn_g, w_router, w_gate, w_up, w_down):
    with ExitStack() as es:
        sb = moe_alloc(c.nc, es)
        moe_phase(c, g, X, bX, HB, bHB, ffn_g, w_router, w_gate, w_up, w_down, sb)
        barrier(c)


def proj_phase(c, g, X, bX, gain_ap, w_ap, nout, spec, PT, bPT, PV, bPV):
    nc = c.nc
    with ExitStack() as es:
        def T(name, shape, dt):
            return es.enter_context(nc.sbuf_tensor(uniq(name), shape, dt))
        wsb = T("pj_w", [128, 8, nout], BF16); bw = Buf()
        gB = T("pj_gB", [128, D], F32); bgB = Buf()
        tmpr = T("pj_tmpr", [1, D], F32); btmpr = Buf()
        xt = [T("pj_xt%d" % i, [128, 4, D], F32) for i in range(2)]; bxt = [Buf(), Buf()]
        junk = T("pj_junk", [128, D], F32); bjunk = Buf()
        ss = [T("pj_ss%d" % i, [128, 4], F32) for i in range(2)]; bss = [Buf(), Buf()]
        hb = [T("pj_hb%d" % i, [128, D], BF16) for i in range(2)]; bhb = [Buf(), Buf()]
        hT = [T("pj_hT%d" % i, [128, 8, 512], BF16) for i in range(2)]; bhT = [Buf(), Buf()]
        stF = [T("pj_stF%d" % i, [128, 512], F32) for i in range(3)]; bstF = [Buf() for _ in range(3)]
        stT = [T("pj_stT%d" % i, [128, 512], BF16) for i in range(3)]; bstT = [Buf() for _ in range(3)]
        bcast_row(c, g, gB, bgB, gain_ap, D, tmpr, btmpr)
        for c0 in range(0, nout, 512):
            wd = min(512, nout - c0)
            c.dma('pool', lambda e: e.dma_start(out=wsb[:, :, c0:c0 + wd], in_=w_ap[:, c0:c0 + wd].rearrange("(k p) f -> p k f", p=128)), writes=[bw])
        ptb, bptb = g.psb
        nF = 0; nT = 0; npz = 0
        for it in range(L // 512):
            t0 = it * 512
            x_, bx_ = xt[it % 2], bxt[it % 2]
            c.dma('sp', lambda e: e.dma_start(out=x_[:], in_=X[t0:t0 + 512, :].rearrange("(j p) d -> p j d", p=128)), reads=[bX], writes=[bx_])
            hT_, bhT_ = hT[it % 2], bhT[it % 2]
            for j in range(4):
                s_, bs_ = ss[j % 2], bss[j % 2]
                h_, bh_ = hb[j % 2], bhb[j % 2]
                c.op('dve', lambda e: e.scalar_tensor_tensor(out=junk[:], in0=x_[:, j, :], scalar=1.0, in1=x_[:, j, :], op0=ALU.mult, op1=ALU.mult,
                                                             accum_out=s_[:, 0:1]), reads=[bx_], writes=[bjunk, bs_])
                c.op('dve', lambda e: e.tensor_scalar(out=s_[:, 0:1], in0=s_[:, 0:1], scalar1=1.0 / D, scalar2=1e-6, op0=ALU.mult, op1=ALU.add),
                     reads=[bs_], writes=[bs_])
                c.op('act', lambda e: e.activation(out=s_[:, 0:1], in_=s_[:, 0:1], func=AF.Ln), reads=[bs_], writes=[bs_])
                c.op('act', lambda e: e.activation(out=s_[:, 0:1], in_=s_[:, 0:1], func=AF.Exp, scale=-0.5), reads=[bs_], writes=[bs_])
                c.op('dve', lambda e: e.scalar_tensor_tensor(out=h_[:], in0=x_[:, j, :], scalar=s_[:, 0:1], in1=gB[:], op0=ALU.mult, op1=ALU.mult),
                     reads=[bx_, bs_, bgB], writes=[bh_])
                for k in range(8):
                    c.op('pe', lambda e: e.transpose(out=ptb[:, k * 128:(k + 1) * 128], in_=h_[:, k * 128:(k + 1) * 128], identity=g.identb[:]),
                         reads=[bh_, g.b_identb], writes=[bptb])
                c.op('act', lambda e: e.activation(out=hT_[:, :, j * 128:(j + 1) * 128], in_=ptb[:, :].rearrange("p (k s) -> p k s", k=8), func=AF.Copy),
                     reads=[bptb], writes=[bhT_])
            for (col0, ncols, mode, dst0) in spec:
                if mode == 'F':
                    for f0 in range(0, ncols, 128):
                        fw = min(128, ncols - f0)
                        ps, bps = g.ps[npz % 4]; npz += 1
                        for k in range(8):
                            c.op('pe', lambda e: e.matmul(ps[0:fw, :], lhsT=wsb[:, k, col0 + f0:col0 + f0 + fw], rhs=hT_[:, k, :], start=(k == 0), stop=(k == 7)),
                                 reads=[bw, bhT_], writes=[bps])
                        st, bst = stF[nF % 3], bstF[nF % 3]; nF += 1
                        eng = 'act' if nF % 2 else 'dve'
                        if eng == 'act':
                            c.op('act', lambda e: e.activation(out=st[0:fw, :], in_=ps[0:fw, :], func=AF.Copy), reads=[bps], writes=[bst])
                        else:
                            c.op('dve', lambda e: e.tensor_copy(out=st[0:fw, :], in_=ps[0:fw, :]), reads=[bps], writes=[bst])
                        c.dma('sp' if nF % 2 else 'act', lambda e: e.dma_start(out=PT[dst0 + f0:dst0 + f0 + fw, t0:t0 + 512], in_=st[0:fw, :]), reads=[bst], pwrites=[bPT])
                else:
                    for j in range(4):
                        for c0 in range(0, ncols, 512):
                            cw = min(512, ncols - c0)
                            ps, bps = g.ps[npz % 4]; npz += 1
                            for k in range(8):
                                c.op('pe', lambda e: e.matmul(ps[:, 0:cw], lhsT=hT_[:, k, j * 128:(j + 1) * 128], rhs=wsb[:, k, col0 + c0:col0 + c0 + cw], start=(k == 0), stop=(k == 7)),
                                     reads=[bw, bhT_], writes=[bps])
                            st, bst = stT[nT % 3], bstT[nT % 3]; nT += 1
                            eng = 'act' if nT % 2 else 'dve'
                            if eng == 'act':
                                c.op('act', lambda e: e.activation(out=st[:, 0:cw], in_=ps[:, 0:cw], func=AF.Copy), reads=[bps], writes=[bst])
                            else:
                                c.op('dve', lambda e: e.tensor_copy(out=st[:, 0:cw], in_=ps[:, 0:cw]), reads=[bps], writes=[bst])
                            c.dma('sp' if nT % 2 else 'act', lambda e: e.dma_start(out=PV[t0 + j * 128:t0 + (j + 1) * 128, dst0 + c0:dst0 + c0 + cw], in_=st[:, 0:cw]),
                                  reads=[bst], pwrites=[bPV])
        bPT.seal(); bPV.seal()
        barrier(c)


def gated_norm_finalize(c, g, OA, bOAs, PV, bPV, gcol0, gain_ap, MT, bMT, row0, pfx, OA2=None, bOA2s=()):
    nc = c.nc
    with ExitStack() as es:
        def T(name, shape, dt):
            return es.enter_context(nc.sbuf_tensor(uniq(pfx + name), shape, dt))
        gA = T("gA", [128, 128], F32); bgA = Buf()
        oa = [T("oa%d" % i, [128, 512], F32) for i in range(2)]; boa = [Buf(), Buf()]
        oa2 = [T("oa2%d" % i, [128, 512], F32) for i in range(2)]; boa2 = [Buf(), Buf()]
        ga = [T("ga%d" % i, [128, 512], BF16) for i in range(2)]; bga = [Buf(), Buf()]
        sq = T("sq", [128, 512], F32); bsq = Buf()
        ssq = [T("ssq%d" % i, [128, 4], F32) for i in range(2)]; bssq = [Buf(), Buf()]
        sg = T("sg", [128, 512], F32); bsg = Buf()
        t1 = T("t1", [128, 512], F32); bt1 = Buf()
        ob = [T("ob%d" % i, [128, 512], BF16) for i in range(2)]; bob = [Buf(), Buf()]
        mt = [T("mt%d" % i, [128, 4, 512], BF16) for i in range(2)]; bmt = [Buf(), Buf()]
        c.dma('sp', lambda e: e.dma_start(out=gA[:], in_=gain_ap.partition_broadcast(128)), writes=[bgA])
        ptb, bptb = g.psb
        for i in range(NT):
            oa_, boa_ = oa[i % 2], boa[i % 2]
            ga_, bga_ = ga[i % 2], bga[i % 2]
            ss_, bss_ = ssq[i % 2], bssq[i % 2]
            ob_, bob_ = ob[i % 2], bob[i % 2]
            mt_, bmt_ = mt[(i // 4) % 2], bmt[(i // 4) % 2]
            c.dma('sp', lambda e: e.dma_start(out=oa_[:], in_=OA[i * 128:(i + 1) * 128, :]), reads=bOAs, writes=[boa_])
            c.dma('act', lambda e: e.dma_start(out=ga_[:], in_=PV[i * 128:(i + 1) * 128, gcol0:gcol0 + 512]), reads=[bPV], writes=[bga_])
            if OA2 is not None:
                o2_, bo2_ = oa2[i % 2], boa2[i % 2]
                c.dma('act', lambda e: e.dma_start(out=o2_[:], in_=OA2[i * 128:(i + 1) * 128, :]), reads=list(bOA2s), writes=[bo2_])
                c.op('dve', lambda e: e.tensor_tensor(out=oa_[:], in0=oa_[:], in1=o2_[:], op=ALU.add), reads=[boa_, bo2_], writes=[boa_])
            c.op('act', lambda e: e.activation(out=sq[:], in_=oa_[:], func=AF.Square), reads=[boa_], writes=[bsq])
            c.op('dve', lambda e: e.tensor_reduce(out=ss_[:], in_=sq[:].rearrange("p (h d) -> p h d", h=4), axis=AX.X, op=ALU.add), reads=[bsq], writes=[bss_])
            c.op('dve', lambda e: e.tensor_scalar(out=ss_[:], in0=ss_[:], scalar1=1.0 / 128, scalar2=1e-6, op0=ALU.mult, op1=ALU.add), reads=[bss_], writes=[bss_])
            c.op('pool', lambda e: e.tensor_tensor(out=ss_[:], in0=ss_[:], in1=g.neghalf[:, 0:1].broadcast_to([128, 4]), op=ALU.pow), reads=[bss_, g.b_neghalf], writes=[bss_])
            c.op('act', lambda e: e.activation(out=sg[:], in_=ga_[:], func=AF.Silu), reads=[bga_], writes=[bsg])
            c.op('dve', lambda e: e.tensor_tensor(out=t1[:].rearrange("p (h d) -> p h d", h=4), in0=oa_[:].rearrange("p (h d) -> p h d", h=4),
                                                  in1=ss_[:].unsqueeze(2).broadcast_to([128, 4, 128]), op=ALU.mult), reads=[boa_, bss_], writes=[bt1])
            c.op('dve', lambda e: e.tensor_tensor(out=t1[:].rearrange("p (h d) -> p h d", h=4), in0=t1[:].rearrange("p (h d) -> p h d", h=4),
                                                   in1=gA[:].unsqueeze(1).broadcast_to([128, 4, 128]), op=ALU.mult), reads=[bt1, bgA], writes=[bt1])
            c.op('dve', lambda e: e.tensor_tensor(out=ob_[:], in0=t1[:], in1=sg[:], op=ALU.mult), reads=[bt1, bsg], writes=[bob_])
            for k in range(4):
                c.op('pe', lambda e: e.transpose(out=ptb[:, k * 128:(k + 1) * 128], in_=ob_[:, k * 128:(k + 1) * 128], identity=g.identb[:]),
                     reads=[bob_, g.b_identb], writes=[bptb])
            c.op('act', lambda e: e.activation(out=mt_[:, :, (i % 4) * 128:(i % 4 + 1) * 128], in_=ptb[:, 0:512].rearrange("p (k s) -> p k s", k=4), func=AF.Copy),
                 reads=[bptb], writes=[bmt_])
            if i % 4 == 3:
                t0 = (i // 4) * 512
                c.dma('sp', lambda e: e.dma_start(out=MT[row0:row0 + 512, t0:t0 + 512].rearrange("(k p) t -> p k t", p=128), in_=mt_[:]), reads=[bmt_], pwrites=[bMT])
        barrier(c)


TBK = 2048
NTB = L // TBK


def hgrn2_phase(c, g, PT, bPT, PV, bPV, lb_logits, jl, a_out_norm, QK, bQK, OA, bOAs, OA2, bOA2s, MT, bMT, stage=3):
    nc = c.nc
    with ExitStack() as es0:
        def T0(name, shape, dt):
            return es0.enter_context(nc.sbuf_tensor(uniq(name), shape, dt))
        mcols = T0("hg_mcols", [128, 8, 128], F32); bmcols = Buf()
        with ExitStack() as es:
            def T(name, shape, dt):
                return es.enter_context(nc.sbuf_tensor(uniq(name), shape, dt))
            lbt = T("hg_lbt", [128, 2, 2, 4], F32); blbt = Buf()
            lbc = T("hg_lbc", [128, 8], F32); blbc = Buf()
            oml = T("hg_oml", [128, 8], F32); boml = Buf()
            noml = T("hg_noml", [128, 8], F32); bnoml = Buf()
            msk = T("hg_msk", [128, TBK], F32); bmsk = Buf()
            bmid = T("hg_bmid", [128, 128], F32); bbmid = Buf()
            blast = T("hg_blast", [128, 128], F32); bblast = Buf()
            zq = [T("hg_zq%d" % i, [128, TBK], F32) for i in range(2)]; bzq = [Buf(), Buf()]
            zf = [T("hg_zf%d" % i, [128, TBK], F32) for i in range(2)]; bzf = [Buf(), Buf()]
            q_ = T("hg_q", [128, TBK], F32); bq_ = Buf()
            sig = T("hg_sig", [128, TBK], F32); bsig = Buf()
            f_ = T("hg_f", [128, TBK], F32); bf_ = Buf()
            kk = T("hg_kk", [128, TBK], F32); bkk = Buf()
            b_ = T("hg_b", [128, TBK], F32); bb_ = Buf()
            e1 = T("hg_e1", [128, TBK], F32); be1 = Buf()
            eq = T("hg_eq", [128, TBK], F32); beq = Buf()
            ek = T("hg_ek", [128, TBK], F32); bek = Buf()
            qt = [T("hg_qt%d" % i, [128, TBK], BF16) for i in range(2)]; bqt = [Buf(), Buf()]
            kt = [T("hg_kt%d" % i, [128, TBK], BF16) for i in range(2)]; bkt = [Buf(), Buf()]
            if jl == 0:
                c.op('pool', lambda e: e.memset(lbc[:], 0.0), writes=[blbc])
            else:
                with nc.allow_non_contiguous_dma(reason="tiny"):
                    for j_ in range(2):
                        for r_ in range(2):
                            c.dma('sp', lambda e: e.dma_start(out=lbt[:, j_, r_, :], in_=lb_logits[j_, r_, :].rearrange("(h p) -> p h", p=128)), pwrites=[blbt])
                blbt.seal()
                c.op('dve', lambda e: e.tensor_tensor(out=lbc[:].rearrange("p (r h) -> p r h", r=2), in0=lbt[:, 1, :, :], in1=lbt[:, 0, :, :], op=ALU.subtract),
                     reads=[blbt], writes=[blbc])
                c.op('act', lambda e: e.activation(out=lbc[:], in_=lbc[:], func=AF.Sigmoid), reads=[blbc], writes=[blbc])
            c.op('dve', lambda e: e.tensor_scalar(out=oml[:], in0=lbc[:], scalar1=-1.0, scalar2=1.0, op0=ALU.mult, op1=ALU.add), reads=[blbc], writes=[boml])
            c.op('dve', lambda e: e.tensor_scalar(out=noml[:], in0=oml[:], scalar1=-1.0, scalar2=None, op0=ALU.mult), reads=[boml], writes=[bnoml])
            c.op('pool', lambda e: e.memset(msk[:], 1.0), writes=[bmsk])
            c.op('pool', lambda e: e.memset(msk[:].rearrange("p (c j) -> p c j", j=64)[:, :, 0:1], 0.0), writes=[bmsk])
            n = 0
            for h in range(4):
                for r in range(2):
                    hr = r * 4 + h
                    for tb in range(NTB):
                        nb = tb if r == 0 else NTB - 1 - tb
                        zq_, bzq_ = zq[n % 2], bzq[n % 2]
                        zf_, bzf_ = zf[n % 2], bzf[n % 2]
                        qt_, bqt_ = qt[n % 2], bqt[n % 2]
                        kt_, bkt_ = kt[n % 2], bkt[n % 2]
                        n += 1
                        c.dma('sp', lambda e: e.dma_start(out=zq_[:], in_=PT[h * 128:(h + 1) * 128, nb * TBK:(nb + 1) * TBK]), reads=[bPT], writes=[bzq_])
                        fr = 512 + r * 512 + h * 128
                        c.dma('act', lambda e: e.dma_start(out=zf_[:], in_=PT[fr:fr + 128, nb * TBK:(nb + 1) * TBK]), reads=[bPT], writes=[bzf_])
                        zqs = zq_[:, ::-1] if r else zq_[:, :]
                        zfs = zf_[:, ::-1] if r else zf_[:, :]
                        c.op('act', lambda e: e.activation(out=q_[:], in_=zqs, func=AF.Silu), reads=[bzq_], writes=[bq_])
                        c.op('act', lambda e: e.activation(out=sig[:], in_=zfs, func=AF.Sigmoid), reads=[bzf_], writes=[bsig])
                        c.op('dve', lambda e: e.tensor_scalar(out=f_[:], in0=sig[:], scalar1=oml[:, hr:hr + 1], scalar2=lbc[:, hr:hr + 1], op0=ALU.mult, op1=ALU.add),
                             reads=[bsig, boml, blbc], writes=[bf_])
                        c.op('act', lambda e: e.activation(out=f_[:], in_=f_[:], func=AF.Ln), reads=[bf_], writes=[bf_])
                        c.op('dve', lambda e: e.tensor_scalar(out=kk[:], in0=sig[:], scalar1=noml[:, hr:hr + 1], scalar2=oml[:, hr:hr + 1], op0=ALU.mult, op1=ALU.add),
                             reads=[bsig, bnoml, boml], writes=[bkk])
                        c.op('dve', lambda e: e.tensor_tensor_scan(out=b_[:], data0=msk[:], data1=f_[:], initial=0.0, op0=ALU.mult, op1=ALU.add),
                             reads=[bmsk, bf_], writes=[bb_])
                        b3 = b_[:].rearrange("p (c j) -> p c j", j=64)
                        c.op('dve', lambda e: e.tensor_tensor(out=e1[:].rearrange("p (c j) -> p c j", j=64), in0=b3, in1=b3[:, :, 31:32].broadcast_to([128, TBK // 64, 64]), op=ALU.subtract),
                             reads=[bb_], writes=[be1])
                        c.op('act', lambda e: e.activation(out=eq[:], in_=e1[:], func=AF.Exp), reads=[be1], writes=[beq])
                        c.op('act', lambda e: e.activation(out=ek[:], in_=e1[:], func=AF.Exp, scale=-1.0), reads=[be1], writes=[bek])
                        c.op('dve', lambda e: e.tensor_tensor(out=qt_[:], in0=q_[:], in1=eq[:], op=ALU.mult), reads=[bq_, beq], writes=[bqt_])
                        c.op('dve', lambda e: e.tensor_tensor(out=kt_[:], in0=kk[:], in1=ek[:], op=ALU.mult), reads=[bkk, bek], writes=[bkt_])
                        nch = TBK // 64
                        c.op('act', lambda e: e.activation(out=bmid[:, tb * nch:(tb + 1) * nch], in_=b3[:, :, 31], func=AF.Copy), reads=[bb_], writes=[bbmid])
                        c.op('act', lambda e: e.activation(out=blast[:, tb * nch:(tb + 1) * nch], in_=b3[:, :, 63], func=AF.Copy), reads=[bb_], writes=[bblast])
                        c.dma('sp', lambda e: e.dma_start(out=QK[h, r, 0, :, tb * TBK:(tb + 1) * TBK], in_=qt_[:]), reads=[bqt_], pwrites=[bQK])
                        c.dma('act', lambda e: e.dma_start(out=QK[h, r, 1, :, tb * TBK:(tb + 1) * TBK], in_=kt_[:]), reads=[bkt_], pwrites=[bQK])
                    c.op('dve', lambda e: e.tensor_tensor(out=blast[:], in0=blast[:], in1=bmid[:], op=ALU.subtract), reads=[bblast, bbmid], writes=[bblast])
                    c.op('dve', lambda e: e.tensor_tensor(out=blast[:, 0:127], in0=blast[:, 0:127], in1=bmid[:, 1:128], op=ALU.add), reads=[bblast, bbmid], writes=[bblast])
                    c.op('act', lambda e: e.activation(out=mcols[:, hr, :], in_=blast[:], func=AF.Exp), reads=[bblast], writes=[bmcols])
            bQK.seal()
            barrier(c)
        if stage < 2:
            return
        with ExitStack() as es:
            def T(name, shape, dt):
                return es.enter_context(nc.sbuf_tensor(uniq(name), shape, dt))
            mask = T("hr_mask", [128, 128], F32); bmask = Buf()
            c.op('pool', lambda e: e.memset(mask[:], 1.0), writes=[bmask])
            c.op('pool', lambda e: e.affine_select(out=mask[:], in_=mask[:], pattern=[[1, 128]], compare_op=ALU.is_ge, fill=0.0, base=0, channel_multiplier=-1),
                 reads=[bmask], writes=[bmask])
            c.op('pool', lambda e: e.memset(mask[0:64, 64:128], 0.0), reads=[bmask], writes=[bmask])
            ptb, bptb = g.psb

            class CH:
                pass
            chs = []
            for r in range(2):
                ch = CH(); chs.append(ch); ch.r = r

                def TT(name, shape, dt, r=r):
                    return (T("hr%d_%s" % (r, name), shape, dt), Buf())
                ch.qb = [TT("qb%d" % i, [128, TBK], BF16) for i in range(2)]
                ch.kb = [TT("kb%d" % i, [128, TBK], BF16) for i in range(2)]
                ch.vb = [TT("vb%d" % i, [128, TBK // 128, 128], BF16) for i in range(2)]
                ch.vnat = TT("vnat", [128, TBK // 128, 128], BF16)
                ch.attnT = [TT("attn%d" % i, [128, 128], BF16) for i in range(2)]
                ch.ktok = [TT("ktok%d" % i, [128, 128], BF16) for i in range(2)]
                ch.M32 = TT("M32", [128, 128], F32); ch.Mb = TT("Mb", [128, 128], BF16); ch.tmp32 = TT("tmp32", [128, 128], F32)
                ch.osb = [TT("osb%d" % i, [128, 128], F32) for i in range(3)]
                ch.osf = [TT("osf%d" % i, [128, 128], F32) for i in range(3)]
                ch.pa = g.ps[3 * r]; ch.po = g.ps[3 * r + 1]; ch.pk = g.ps[3 * r + 2]
                ch.nblk = 0

            def blk_gen(ch, h, tb, b, qb_, bqb_, kb_, bkb_, vb_, bvb_):
                r = ch.r; hr = r * 4 + h
                blk = tb * (TBK // 128) + b
                at_, bat_ = ch.attnT[ch.nblk % 2]; kt_, bkt_ = ch.ktok[ch.nblk % 2]
                os_, bos_ = ch.osb[ch.nblk % 3]; of_, bof_ = ch.osf[ch.nblk % 3]
                ch.nblk += 1
                pa, bpa = ch.pa; po, bpo = ch.po; pk, bpk = ch.pk
                M32, bM32 = ch.M32; Mb, bMb = ch.Mb; tmp32, btmp32 = ch.tmp32
                pcol = 128 * r
                bs = slice(b * 128, (b + 1) * 128)
                c.op('pe', lambda e: e.matmul(pa[:, 0:128], lhsT=kb_[:, bs], rhs=qb_[:, bs], start=True, stop=True), reads=[bkb_, bqb_], writes=[bpa]); yield
                c.op('dve', lambda e: e.tensor_tensor(out=at_[:], in0=pa[:, 0:128], in1=mask[:], op=ALU.mult), reads=[bpa, bmask], writes=[bat_]); yield
                c.op('pe', lambda e: e.transpose(out=ptb[:, pcol:pcol + 128], in_=kb_[:, bs], identity=g.identb[:]), reads=[bkb_, g.b_identb], writes=[bptb]); yield
                c.op('act', lambda e: e.activation(out=kt_[:], in_=ptb[:, pcol:pcol + 128], func=AF.Copy), reads=[bptb], writes=[bkt_]); yield
                for ci in range(2):
                    r0 = 64 * ci
                    cidx = 2 * blk + ci
                    c.op('pe', lambda e: e.matmul(po[r0:r0 + 64, 0:128], lhsT=at_[r0:r0 + 64, r0:r0 + 64], rhs=vb_[r0:r0 + 64, b, :], start=True, stop=False),
                         reads=[bat_, bvb_], writes=[bpo])
                    c.op('pe', lambda e: e.matmul(po[r0:r0 + 64, 0:128], lhsT=qb_[:, b * 128 + r0:b * 128 + r0 + 64], rhs=Mb[:, :], start=False, stop=True),
                         reads=[bqb_, bMb], writes=[bpo]); yield
                    if cidx < 127:
                        c.op('pe', lambda e: e.matmul(pk[:, 0:128], lhsT=kt_[r0:r0 + 64, :], rhs=vb_[r0:r0 + 64, b, :], start=True, stop=True), reads=[bkt_, bvb_], writes=[bpk]); yield
                        c.op('dve', lambda e: e.tensor_tensor(out=tmp32[:], in0=pk[:, 0:128], in1=M32[:], op=ALU.add), reads=[bpk, bM32], writes=[btmp32]); yield
                        c.op('dve', lambda e: e.tensor_scalar(out=Mb[:], in0=tmp32[:], scalar1=mcols[:, hr, cidx:cidx + 1], scalar2=None, op0=ALU.mult), reads=[btmp32, bmcols], writes=[bMb]); yield
                        c.op('dve', lambda e: e.tensor_scalar(out=M32[:], in0=tmp32[:], scalar1=mcols[:, hr, cidx:cidx + 1], scalar2=None, op0=ALU.mult), reads=[btmp32, bmcols], writes=[bM32]); yield
                c.op('act', lambda e: e.activation(out=os_[:], in_=po[:, 0:128], func=AF.Copy), reads=[bpo], writes=[bos_]); yield
                if r == 0:
                    c.dma('sp', lambda e: e.dma_start(out=OA[blk * 128:(blk + 1) * 128, h * 128:(h + 1) * 128], in_=os_[:]), reads=[bos_], pwrites=[bOAs[h]]); yield
                else:
                    pf, bpf = g.ps[6]
                    c.op('pe', lambda e: e.matmul(pf[:, 0:128], lhsT=g.J32[:], rhs=os_[:], start=True, stop=True), reads=[g.b_J32, bos_], writes=[bpf]); yield
                    c.op('act', lambda e: e.activation(out=of_[:], in_=pf[:, 0:128], func=AF.Copy), reads=[bpf], writes=[bof_]); yield
                    c.dma('act', lambda e: e.dma_start(out=OA2[L - (blk + 1) * 128:L - blk * 128, h * 128:(h + 1) * 128], in_=of_[:]), reads=[bof_], pwrites=[bOA2s[h]]); yield

            for h in range(4):
                for ch in chs:
                    M32, bM32 = ch.M32; Mb, bMb = ch.Mb
                    c.op('pool', lambda e: e.memset(M32[:], 0.0), reads=[bM32], writes=[bM32])
                    c.op('pool', lambda e: e.memset(Mb[:], 0.0), reads=[bMb], writes=[bMb])
                for tb in range(NTB):
                    loaded = []
                    for ch in chs:
                        r = ch.r
                        qb_, bqb_ = ch.qb[tb % 2]; kb_, bkb_ = ch.kb[tb % 2]; vb_, bvb_ = ch.vb[tb % 2]
                        c.dma('sp', lambda e: e.dma_start(out=qb_[:], in_=QK[h, r, 0, :, tb * TBK:(tb + 1) * TBK]), reads=[bQK], writes=[bqb_])
                        c.dma('act', lambda e: e.dma_start(out=kb_[:], in_=QK[h, r, 1, :, tb * TBK:(tb + 1) * TBK]), reads=[bQK], writes=[bkb_])
                        if r == 0:
                            vsrc = PV[tb * TBK:(tb + 1) * TBK, h * 128:(h + 1) * 128].rearrange("(b p) d -> p b d", p=128)
                            c.dma('sp', lambda e: e.dma_start(out=vb_[:], in_=vsrc), reads=[bPV], writes=[bvb_])
                        else:
                            vnat, bvnat = ch.vnat
                            vsrc = PV[L - (tb + 1) * TBK:L - tb * TBK, h * 128:(h + 1) * 128].rearrange("(b p) d -> p b d", p=128)
                            c.dma('sp', lambda e: e.dma_start(out=vnat[:], in_=vsrc), reads=[bPV], writes=[bvnat])
                            nbk = TBK // 128
                            for b4 in range(0, nbk, 4):
                                pf, bpf = g.ps[6]
                                c.op('pe', lambda e: e.matmul(pf[:, :], lhsT=g.Jb[:], rhs=vnat[:, b4:b4 + 4, :], start=True, stop=True), reads=[g.b_Jb, bvnat], writes=[bpf])
                                for bb in range(4):
                                    c.op('act', lambda e: e.activation(out=vb_[:, nbk - 1 - (b4 + bb), :], in_=pf[:, bb * 128:(bb + 1) * 128], func=AF.Copy), reads=[bpf, bvb_], writes=[bvb_])
                        loaded.append((qb_, bqb_, kb_, bkb_, vb_, bvb_))
                    for b in range(TBK // 128):
                        gens = [blk_gen(ch, h, tb, b, *loaded[i]) for i, ch in enumerate(chs)]
                        alive = list(gens)
                        while alive:
                            for gnr in list(alive):
                                try:
                                    next(gnr)
                                except StopIteration:
                                    alive.remove(gnr)
                bOAs[h].seal(); bOA2s[h].seal()
            barrier(c)
        if stage < 3:
            return
    gated_norm_finalize(c, g, OA, bOAs, PV, bPV, 512, a_out_norm, MT, bMT, 0, "hf_", OA2=OA2, bOA2s=bOA2s)


TB5 = 1024
NTB5 = L // TB5
TWO_PI = 2.0 * math.pi


def s5_phase(c, g, PT, bPT, lam_re, lam_im, log_step, b_re, b_im, c_re, c_im, d_skip, glu_w, glu_b, YT, bYT, MT, bMT, stage=3):
    nc = c.nc
    U0 = 1536
    with ExitStack() as es0:
        def T0(name, shape, dt):
            return es0.enter_context(nc.sbuf_tensor(uniq(name), shape, dt))
        WB = [T0("s5_WB%d" % p, [128, 2, 4, 128], BF16) for p in range(2)]; bWB = Buf()
        WC = [T0("s5_WC%d" % p, [128, 2, 4, 128], BF16) for p in range(2)]; bWC = Buf()
        WBx = [T0("s5_WBx%d" % p, [128, 2, 4, 128], BF16) for p in range(2)]
        WCx = [T0("s5_WCx%d" % p, [128, 2, 4, 64], BF16) for p in range(2)]
        mag = T0("s5_mag", [128, 32], F32); bmag = Buf()
        pwc = T0("s5_pwc", [128, 11, 32], F32); bpw = Buf()
        pws = T0("s5_pws", [128, 11, 32], F32)
        with ExitStack() as es:
            def T(name, shape, dt):
                return es.enter_context(nc.sbuf_tensor(uniq(name), shape, dt))
            n_ = [0]

            def S(shape=[128, 32], dt=F32):
                n_[0] += 1
                return T("s5_t%d" % n_[0], shape, dt), Buf()
            lamre, blamre = S(); lamim, blamim = S()
            lsB, blsB = S([128, 64]); ls, bls = S()
            with nc.allow_non_contiguous_dma(reason="small params"):
                for r_ in range(2):
                    for g4 in range(0, 16, 4):
                        c.dma('sp', lambda e: e.dma_start(out=lamre[:, r_ * 16 + g4:r_ * 16 + g4 + 4], in_=lam_re[r_, 2 * g4:2 * g4 + 8, :].rearrange("(gp gl) n -> (gl n) gp", gl=2)), pwrites=[blamre])
                        c.dma('act', lambda e: e.dma_start(out=lamim[:, r_ * 16 + g4:r_ * 16 + g4 + 4], in_=lam_im[r_, 2 * g4:2 * g4 + 8, :].rearrange("(gp gl) n -> (gl n) gp", gl=2)), pwrites=[blamim])
                blamre.seal(); blamim.seal()
                c.dma('sp', lambda e: e.dma_start(out=lsB[:], in_=log_step.rearrange("r g -> (r g)").partition_broadcast(128)), writes=[blsB])
            lsv = lsB[:].rearrange("p (r gp gl) -> p r gp gl", r=2, gl=2)
            c.op('dve', lambda e: e.tensor_copy(out=ls[0:64, :].rearrange("p (r gp) -> p r gp", r=2), in_=lsv[0:64, :, :, 0]), reads=[blsB], writes=[bls])
            c.op('dve', lambda e: e.tensor_copy(out=ls[64:128, :].rearrange("p (r gp) -> p r gp", r=2), in_=lsv[64:128, :, :, 1]), reads=[blsB], writes=[bls])
            step, bstep = S()
            c.op('act', lambda e: e.activation(out=step[:], in_=ls[:], func=AF.Exp), reads=[bls], writes=[bstep])
            lrs, blrs = S(); ang, bang = S()
            c.op('dve', lambda e: e.tensor_tensor(out=lrs[:], in0=lamre[:], in1=step[:], op=ALU.mult), reads=[blamre, bstep], writes=[blrs])
            c.op('act', lambda e: e.activation(out=mag[:], in_=lrs[:], func=AF.Exp), reads=[blrs], writes=[bmag])
            c.op('dve', lambda e: e.tensor_tensor(out=ang[:], in0=lamim[:], in1=step[:], op=ALU.mult), reads=[blamim, bstep], writes=[bang])

            def sin_of(src, bsrc, offset, dst, bdst):
                q, bq = S(); qi, bqi = S(dt=I32); r, br = S(); m, bm = S()
                c.op('dve', lambda e: e.tensor_scalar(out=q[:], in0=src[:], scalar1=offset, scalar2=1.0 / TWO_PI, op0=ALU.add, op1=ALU.mult), reads=[bsrc], writes=[bq])
                c.op('dve', lambda e: e.tensor_copy(out=qi[:], in_=q[:]), reads=[bq], writes=[bqi])
                c.op('dve', lambda e: e.tensor_copy(out=q[:], in_=qi[:]), reads=[bqi], writes=[bq])
                c.op('dve', lambda e: e.scalar_tensor_tensor(out=r[:], in0=q[:], scalar=-TWO_PI, in1=src[:], op0=ALU.mult, op1=ALU.add), reads=[bq, bsrc], writes=[br])
                if offset != 0.0:
                    c.op('dve', lambda e: e.tensor_scalar(out=r[:], in0=r[:], scalar1=offset, scalar2=None, op0=ALU.add), reads=[br], writes=[br])
                c.op('dve', lambda e: e.tensor_scalar(out=m[:], in0=r[:], scalar1=math.pi, scalar2=-TWO_PI, op0=ALU.is_gt, op1=ALU.mult), reads=[br], writes=[bm])
                c.op('dve', lambda e: e.tensor_tensor(out=r[:], in0=r[:], in1=m[:], op=ALU.add), reads=[br, bm], writes=[br])
                c.op('dve', lambda e: e.tensor_scalar(out=m[:], in0=r[:], scalar1=-math.pi, scalar2=TWO_PI, op0=ALU.is_lt, op1=ALU.mult), reads=[br], writes=[bm])
                c.op('dve', lambda e: e.tensor_tensor(out=r[:], in0=r[:], in1=m[:], op=ALU.add), reads=[br, bm], writes=[br])
                c.op('dve', lambda e: e.tensor_scalar(out=r[:], in0=r[:], scalar1=math.pi, scalar2=-math.pi, op0=ALU.min, op1=ALU.max), reads=[br], writes=[br])
                c.op('act', lambda e: e.activation(out=dst, in_=r[:], func=AF.Sin), reads=[br], writes=[bdst])
            sin_of(ang, bang, 0.0, pws[:, 0, :], bpw)
            sin_of(ang, bang, math.pi / 2, pwc[:, 0, :], bpw)
            tq, btq = S(); tq2, btq2 = S()
            for k in range(10):
                c.op('dve', lambda e: e.tensor_tensor(out=tq[:], in0=pwc[:, k, :], in1=pwc[:, k, :], op=ALU.mult), reads=[bpw], writes=[btq])
                c.op('dve', lambda e: e.tensor_tensor(out=tq2[:], in0=pws[:, k, :], in1=pws[:, k, :], op=ALU.mult), reads=[bpw], writes=[btq2])
                c.op('dve', lambda e: e.tensor_tensor(out=pwc[:, k + 1, :], in0=tq[:], in1=tq2[:], op=ALU.subtract), reads=[btq, btq2, bpw], writes=[bpw])
                c.op('dve', lambda e: e.tensor_tensor(out=tq[:], in0=pws[:, k, :], in1=pwc[:, k, :], op=ALU.mult), reads=[bpw], writes=[btq])
                c.op('dve', lambda e: e.tensor_scalar(out=pws[:, k + 1, :], in0=tq[:], scalar1=2.0, scalar2=None, op0=ALU.mult), reads=[btq, bpw], writes=[bpw])
            are, bare = S(); aim, baim = S(); den, bden = S(); am1, bam1 = S(); fr, bfr = S(); fi, bfi = S(); tt, btt = S()
            c.op('dve', lambda e: e.tensor_tensor(out=are[:], in0=mag[:], in1=pwc[:, 0, :], op=ALU.mult), reads=[bmag, bpw], writes=[bare])
            c.op('dve', lambda e: e.tensor_tensor(out=aim[:], in0=mag[:], in1=pws[:, 0, :], op=ALU.mult), reads=[bmag, bpw], writes=[baim])
            c.op('dve', lambda e: e.tensor_tensor(out=den[:], in0=lamre[:], in1=lamre[:], op=ALU.mult), reads=[blamre], writes=[bden])
            c.op('dve', lambda e: e.tensor_tensor(out=tt[:], in0=lamim[:], in1=lamim[:], op=ALU.mult), reads=[blamim], writes=[btt])
            c.op('dve', lambda e: e.tensor_tensor(out=den[:], in0=den[:], in1=tt[:], op=ALU.add), reads=[bden, btt], writes=[bden])
            c.op('dve', lambda e: e.reciprocal(out=den[:], in_=den[:]), reads=[bden], writes=[bden])
            c.op('dve', lambda e: e.tensor_scalar(out=am1[:], in0=are[:], scalar1=-1.0, scalar2=None, op0=ALU.add), reads=[bare], writes=[bam1])
            c.op('dve', lambda e: e.tensor_tensor(out=fr[:], in0=am1[:], in1=lamre[:], op=ALU.mult), reads=[bam1, blamre], writes=[bfr])
            c.op('dve', lambda e: e.tensor_tensor(out=tt[:], in0=aim[:], in1=lamim[:], op=ALU.mult), reads=[baim, blamim], writes=[btt])
            c.op('dve', lambda e: e.tensor_tensor(out=fr[:], in0=fr[:], in1=tt[:], op=ALU.add), reads=[bfr, btt], writes=[bfr])
            c.op('dve', lambda e: e.tensor_tensor(out=fr[:], in0=fr[:], in1=den[:], op=ALU.mult), reads=[bfr, bden], writes=[bfr])
            c.op('dve', lambda e: e.tensor_tensor(out=fi[:], in0=aim[:], in1=lamre[:], op=ALU.mult), reads=[baim, blamre], writes=[bfi])
            c.op('dve', lambda e: e.tensor_tensor(out=tt[:], in0=am1[:], in1=lamim[:], op=ALU.mult), reads=[bam1, blamim], writes=[btt])
            c.op('dve', lambda e: e.tensor_tensor(out=fi[:], in0=fi[:], in1=tt[:], op=ALU.subtract), reads=[bfi, btt], writes=[bfi])
            c.op('dve', lambda e: e.tensor_tensor(out=fi[:], in0=fi[:], in1=den[:], op=ALU.mult), reads=[bfi, bden], writes=[bfi])
            mk, bmk = S([128, 2])
            c.op('pool', lambda e: e.memset(mk[:], 0.0), writes=[bmk])
            c.op('pool', lambda e: e.memset(mk[0:64, 0:1], 1.0), reads=[bmk], writes=[bmk])
            c.op('pool', lambda e: e.memset(mk[64:128, 1:2], 1.0), reads=[bmk], writes=[bmk])
            Bn = [S([128, 2, 16, 16]) for _ in range(2)]
            with nc.allow_non_contiguous_dma(reason="small params"):
                for r_ in range(2):
                    for g4 in range(0, 16, 4):
                        c.dma('sp', lambda e: e.dma_start(out=Bn[0][0][:, r_, g4:g4 + 4, :], in_=b_re[r_, 2 * g4:2 * g4 + 8].rearrange("(gp gl) n p -> (gl n) gp p", gl=2)), pwrites=[Bn[0][1]])
                        c.dma('act', lambda e: e.dma_start(out=Bn[1][0][:, r_, g4:g4 + 4, :], in_=b_im[r_, 2 * g4:2 * g4 + 8].rearrange("(gp gl) n p -> (gl n) gp p", gl=2)), pwrites=[Bn[1][1]])
                Bn[0][1].seal(); Bn[1][1].seal()
            frb = fr[:].rearrange("p (r gp) -> p r gp", r=2).unsqueeze(3).broadcast_to([128, 2, 16, 16])
            fib = fi[:].rearrange("p (r gp) -> p r gp", r=2).unsqueeze(3).broadcast_to([128, 2, 16, 16])
            bbr, bbbr = S([128, 2, 16, 16]); bbi, bbbi = S([128, 2, 16, 16]); t5, bt5 = S([128, 2, 16, 16])
            c.op('dve', lambda e: e.tensor_tensor(out=bbr[:], in0=Bn[0][0][:], in1=frb, op=ALU.mult), reads=[Bn[0][1], bfr], writes=[bbbr])
            c.op('dve', lambda e: e.tensor_tensor(out=t5[:], in0=Bn[1][0][:], in1=fib, op=ALU.mult), reads=[Bn[1][1], bfi], writes=[bt5])
            c.op('dve', lambda e: e.tensor_tensor(out=bbr[:], in0=bbr[:], in1=t5[:], op=ALU.subtract), reads=[bbbr, bt5], writes=[bbbr])
            c.op('dve', lambda e: e.tensor_tensor(out=bbi[:], in0=Bn[1][0][:], in1=frb, op=ALU.mult), reads=[Bn[1][1], bfr], writes=[bbbi])
            c.op('dve', lambda e: e.tensor_tensor(out=t5[:], in0=Bn[0][0][:], in1=fib, op=ALU.mult), reads=[Bn[0][1], bfi], writes=[bt5])
            c.op('dve', lambda e: e.tensor_tensor(out=bbi[:], in0=bbi[:], in1=t5[:], op=ALU.add), reads=[bbbi, bt5], writes=[bbbi])
            BBm, bBBm = S([128, 2, 16, 2, 16], BF16)
            ptb, bptb = g.psb
            for part, (src, bsrc) in enumerate(((bbr, bbbr), (bbi, bbbi))):
                for gl in range(2):
                    c.op('dve', lambda e: e.tensor_scalar(out=BBm[:, :, :, gl, :], in0=src[:], scalar1=mk[:, gl:gl + 1], scalar2=None, op0=ALU.mult), reads=[bsrc, bmk, bBBm], writes=[bBBm])
                for r in range(2):
                    for cb in range(4):
                        c.op('pe', lambda e: e.transpose(out=ptb[:, 0:128], in_=BBm[:, r, 4 * cb:4 * cb + 4, :, :].rearrange("p a b c -> p (a b c)"), identity=g.identb[:]),
                             reads=[bBBm, g.b_identb], writes=[bptb])
                        c.op('act', lambda e: e.activation(out=WB[part][:, r, cb, :], in_=ptb[:, 0:128], func=AF.Copy), reads=[bptb], writes=[bWB])
            Cn = [S([128, 2, 4, 64]) for _ in range(2)]
            c.dma('sp', lambda e: e.dma_start(out=Cn[0][0][:], in_=c_re.rearrange("r (cb g8) p n -> (g8 p) r cb n", g8=8)), writes=[Cn[0][1]])
            c.dma('act', lambda e: e.dma_start(out=Cn[1][0][:], in_=c_im.rearrange("r (cb g8) p n -> (g8 p) r cb n", g8=8)), writes=[Cn[1][1]])
            Cd, bCd = S([128, 2, 4, 2, 64], BF16)
            mkb = mk[:].unsqueeze(1).unsqueeze(3).broadcast_to([128, 4, 2, 16])
            for part in range(2):
                sc = 1.0 if part == 0 else -1.0
                for x in range(2):
                    c.op('dve', lambda e: e.tensor_scalar(out=Cd[:, :, :, x, :], in0=Cn[part][0][:], scalar1=sc, scalar2=None, op0=ALU.mult), reads=[Cn[part][1], bCd], writes=[bCd])
                for r in range(2):
                    for cb in range(4):
                        c.op('pe', lambda e: e.transpose(out=ptb[:, 0:128], in_=Cd[:, r, cb, :, :].rearrange("p a b -> p (a b)"), identity=g.identb[:]),
                             reads=[bCd, g.b_identb], writes=[bptb])
                        c.op('dve', lambda e: e.tensor_tensor(out=WC[part][:, r, cb, :].rearrange("p (k a b) -> p k a b", k=4, a=2), in0=ptb[:, 0:128].rearrange("p (k a b) -> p k a b", k=4, a=2),
                                                              in1=mkb, op=ALU.mult), reads=[bptb, bmk], writes=[bWC])
            for part in range(2):
                c.op('act', lambda e: e.activation(out=WBx[part][64:128], in_=WB[part][64:128], func=AF.Copy), reads=[bWB], writes=[bWB])
                c.op('pool', lambda e: e.memset(WBx[part][64:96], 0.0), reads=[bWB], writes=[bWB])
                c.op('act', lambda e: e.activation(out=WCx[part][:], in_=WC[part][:, :, :, 64:128], func=AF.Copy), reads=[bWC], writes=[bWC])
                c.op('pool', lambda e: e.memset(WCx[part][:, :, :, 0:32], 0.0), reads=[bWC], writes=[bWC])
            barrier(c)
        if stage < 2:
            return
        with ExitStack() as es:
            def T(name, shape, dt):
                return es.enter_context(nc.sbuf_tensor(uniq(name), shape, dt))
            uf = T("s5_uf", [128, TB5 * 2], F32); buf_ = Buf()
            ub = [T("s5_ub%d" % r, [128, L], BF16) for r in range(2)]; bub = [Buf(), Buf()]
            Xa = [[T("s5_X%d%d" % (p, r), [128, L], BF16) for r in range(2)] for p in range(2)]
            bXa = [[Buf(), Buf()], [Buf(), Buf()]]
            tcos = T("s5_cos", [128, TB5], F32); tsin = T("s5_sin", [128, TB5], F32); btab = Buf()
            BUs = [T("s5_BU%d" % p, [128, TB5], F32) for p in range(2)]; bBUs = [Buf(), Buf()]
            t = [T("s5_w%d" % i, [128, TB5], F32) for i in range(4)]; bt = [Buf() for _ in range(4)]
            ini = T("s5_ini", [128, 4], F32); bini = Buf()
            yst = [T("s5_yst%d" % i, [128, 512], F32) for i in range(2)]; byst = [Buf(), Buf()]
            npz = 0
            for cb in range(4):
                for r in range(2):
                    for hh in range(L // (2 * TB5)):
                        nb = hh if r == 0 else L // (2 * TB5) - 1 - hh
                        c.dma('sp', lambda e: e.dma_start(out=uf[:], in_=PT[U0 + cb * 128:U0 + (cb + 1) * 128, nb * 2 * TB5:(nb + 1) * 2 * TB5]), reads=[bPT], writes=[buf_])
                        src = uf[:, ::-1] if r else uf[:, :]
                        c.op('act', lambda e: e.activation(out=ub[r][:, hh * 2 * TB5:(hh + 1) * 2 * TB5], in_=src, func=AF.Copy), reads=[buf_], writes=[bub[r]])
                for k in range(4):
                    gp = cb * 4 + k
                    for r in range(2):
                        col = r * 16 + gp
                        c.op('pool', lambda e: e.memset(tcos[:, 0:1], 1.0), writes=[btab])
                        c.op('pool', lambda e: e.memset(tsin[:, 0:1], 0.0), reads=[btab], writes=[btab])
                        n = 1
                        kk = 0
                        while n < TB5:
                            cr = pwc[:, kk, col:col + 1]; ci = pws[:, kk, col:col + 1]
                            c.op('dve', lambda e: e.tensor_scalar(out=t[0][:, 0:n], in0=tsin[:, 0:n], scalar1=ci, scalar2=None, op0=ALU.mult), reads=[btab, bpw], writes=[bt[0]])
                            c.op('dve', lambda e: e.tensor_scalar(out=t[1][:, 0:n], in0=tsin[:, 0:n], scalar1=cr, scalar2=None, op0=ALU.mult), reads=[btab, bpw], writes=[bt[1]])
                            c.op('dve', lambda e: e.scalar_tensor_tensor(out=tsin[:, n:2 * n], in0=tcos[:, 0:n], scalar=ci, in1=t[1][:, 0:n], op0=ALU.mult, op1=ALU.add),
                                 reads=[btab, bpw, bt[1]], writes=[btab])
                            c.op('dve', lambda e: e.scalar_tensor_tensor(out=tcos[:, n:2 * n], in0=tcos[:, 0:n], scalar=cr, in1=t[0][:, 0:n], op0=ALU.mult, op1=ALU.subtract),
                                 reads=[btab, bpw, bt[0]], writes=[btab])
                            n *= 2; kk += 1
                        cTB = pwc[:, kk, col:col + 1]; sTB = pws[:, kk, col:col + 1]
                        rho = mag[:, col:col + 1]
                        c.op('pool', lambda e: e.memset(ini[:], 0.0), writes=[bini])
                        for tb in range(NTB5):
                            ts0 = tb * TB5
                            for part in range(2):
                                for hf in range(TB5 // 512):
                                    ps, bps = g.ps[npz % 4]; npz += 1
                                    if k < 3:
                                        lh = WB[part][32 * k:32 * k + 32, r, cb, :]; rh = ub[r][32 * k:32 * k + 32, ts0 + hf * 512:ts0 + (hf + 1) * 512]
                                    else:
                                        lh = WBx[part][64:128, r, cb, :]; rh = ub[r][64:128, ts0 + hf * 512:ts0 + (hf + 1) * 512]
                                    c.op('pe', lambda e: e.matmul(ps[:, :], lhsT=lh, rhs=rh, start=True, stop=True),
                                         reads=[bWB, bub[r]], writes=[bps])
                                    c.op('act', lambda e: e.activation(out=BUs[part][:, hf * 512:(hf + 1) * 512], in_=ps[:, :], func=AF.Copy), reads=[bps], writes=[bBUs[part]])
                            c.op('dve', lambda e: e.tensor_tensor(out=t[0][:], in0=BUs[0][:], in1=tcos[:], op=ALU.mult), reads=[bBUs[0], btab], writes=[bt[0]])
                            c.op('dve', lambda e: e.tensor_tensor(out=t[1][:], in0=BUs[1][:], in1=tsin[:], op=ALU.mult), reads=[bBUs[1], btab], writes=[bt[1]])
                            c.op('dve', lambda e: e.tensor_tensor(out=t[0][:], in0=t[0][:], in1=t[1][:], op=ALU.add), reads=[bt[0], bt[1]], writes=[bt[0]])
                            c.op('pool', lambda e: e.tensor_tensor(out=t[2][:], in0=BUs[1][:], in1=tcos[:], op=ALU.mult), reads=[bBUs[1], btab], writes=[bt[2]])
                            c.op('pool', lambda e: e.tensor_tensor(out=t[3][:], in0=BUs[0][:], in1=tsin[:], op=ALU.mult), reads=[bBUs[0], btab], writes=[bt[3]])
                            c.op('dve', lambda e: e.tensor_tensor(out=t[2][:], in0=t[2][:], in1=t[3][:], op=ALU.subtract), reads=[bt[2], bt[3]], writes=[bt[2]])
                            c.op('dve', lambda e: e.tensor_tensor_scan(out=t[1][:], data0=rho.broadcast_to([128, TB5]), data1=t[0][:], initial=ini[:, 0:1], op0=ALU.mult, op1=ALU.add),
                                 reads=[bmag, bt[0], bini], writes=[bt[1]])
                            c.op('dve', lambda e: e.tensor_tensor_scan(out=t[3][:], data0=rho.broadcast_to([128, TB5]), data1=t[2][:], initial=ini[:, 1:2], op0=ALU.mult, op1=ALU.add),
                                 reads=[bmag, bt[2], bini], writes=[bt[3]])
                            if tb < NTB5 - 1:
                                xr = t[1][:, TB5 - 1:TB5]; xi = t[3][:, TB5 - 1:TB5]
                                c.op('dve', lambda e: e.tensor_scalar(out=ini[:, 2:3], in0=xi, scalar1=sTB, scalar2=None, op0=ALU.mult), reads=[bt[3], bpw], writes=[bini])
                                c.op('dve', lambda e: e.scalar_tensor_tensor(out=ini[:, 0:1], in0=xr, scalar=cTB, in1=ini[:, 2:3], op0=ALU.mult, op1=ALU.subtract), reads=[bt[1], bpw, bini], writes=[bini])
                                c.op('dve', lambda e: e.tensor_scalar(out=ini[:, 3:4], in0=xi, scalar1=cTB, scalar2=None, op0=ALU.mult), reads=[bt[3], bpw], writes=[bini])
                                c.op('dve', lambda e: e.scalar_tensor_tensor(out=ini[:, 1:2], in0=xr, scalar=sTB, in1=ini[:, 3:4], op0=ALU.mult, op1=ALU.add), reads=[bt[1], bpw, bini], writes=[bini])
                            if r == 0:
                                oslc = slice(ts0, ts0 + TB5)
                                xo_re = Xa[0][r][:, oslc]; xo_im = Xa[1][r][:, oslc]
                            else:
                                lo = L - ts0 - TB5
                                xo_re = Xa[0][r][:, lo:lo + TB5][:, ::-1]; xo_im = Xa[1][r][:, lo:lo + TB5][:, ::-1]
                            c.op('dve', lambda e: e.tensor_tensor(out=t[0][:], in0=t[1][:], in1=tcos[:], op=ALU.mult), reads=[bt[1], btab], writes=[bt[0]])
                            c.op('pool', lambda e: e.tensor_tensor(out=t[2][:], in0=t[3][:], in1=tsin[:], op=ALU.mult), reads=[bt[3], btab], writes=[bt[2]])
                            c.op('dve', lambda e: e.tensor_tensor(out=xo_re, in0=t[0][:], in1=t[2][:], op=ALU.subtract), reads=[bt[0], bt[2]], pwrites=[bXa[0][r]])
                            c.op('pool', lambda e: e.tensor_tensor(out=t[0][:], in0=t[1][:], in1=tsin[:], op=ALU.mult), reads=[bt[1], btab], writes=[bt[0]])
                            c.op('dve', lambda e: e.tensor_tensor(out=t[2][:], in0=t[3][:], in1=tcos[:], op=ALU.mult), reads=[bt[3], btab], writes=[bt[2]])
                            c.op('dve', lambda e: e.tensor_tensor(out=xo_im, in0=t[0][:], in1=t[2][:], op=ALU.add), reads=[bt[0], bt[2]], pwrites=[bXa[1][r]])
                        bXa[0][r].seal(); bXa[1][r].seal()
                    for it in range(L // 512):
                        ps, bps = g.ps[4 + it % 2]
                        i = 0
                        for r in range(2):
                            for part in range(2):
                                if k < 3:
                                    po = ps[32 * k:32 * k + 32, :]; lh = WC[part][:, r, cb, 32 * k:32 * k + 32]
                                else:
                                    po = ps[64:128, :]; lh = WCx[part][:, r, cb, :]
                                c.op('pe', lambda e: e.matmul(po, lhsT=lh, rhs=Xa[part][r][:, it * 512:(it + 1) * 512], start=(i == 0), stop=(i == 3)),
                                     reads=[bWC, bXa[part][r]], writes=[bps])
                                i += 1
                        ys, bys = yst[it % 2], byst[it % 2]
                        e0 = 32 * k if k < 3 else 64
                        c.op('act', lambda e: e.activation(out=ys[e0:32 * k + 32, :], in_=ps[e0:32 * k + 32, :], func=AF.Copy), reads=[bps], writes=[bys])
                        c.dma('sp', lambda e: e.dma_start(out=YT[cb * 128 + 32 * k:cb * 128 + 32 * k + 32, it * 512:(it + 1) * 512], in_=ys[32 * k:32 * k + 32, :]), reads=[bys], pwrites=[bYT])
            bYT.seal()
            barrier(c)
        if stage < 3:
            return
        with ExitStack() as es:
            def T(name, shape, dt):
                return es.enter_context(nc.sbuf_tensor(uniq(name), shape, dt))
            gw = T("s5_gw", [128, 4, 512], BF16); bgw = Buf()
            dcol = T("s5_dcol", [128, 4], F32); bdcol = Buf()
            gbc = T("s5_gbc", [128, 4], F32); bgbc = Buf()
            yt = [T("s5_yt%d" % i, [128, 4, 512], F32) for i in range(2)]; byt = [Buf(), Buf()]
            ut = [T("s5_ut%d" % i, [128, 4, 512], F32) for i in range(2)]; but = [Buf(), Buf()]
            sq = T("s5_sq", [128, 4, 512], F32); bsq = Buf()
            gy = T("s5_gy", [128, 4, 512], F32); bgy = Buf()
            gyb = T("s5_gyb", [128, 4, 512], BF16); bgyb = Buf()
            sg = [T("s5_sg%d" % i, [128, 512], F32) for i in range(2)]; bsg = [Buf(), Buf()]
            ob = [T("s5_ob%d" % i, [128, 4, 512], BF16) for i in range(2)]; bob = [Buf(), Buf()]
            c.dma('pool', lambda e: e.dma_start(out=gw[:], in_=glu_w.rearrange("(k p) f -> p k f", p=128)), writes=[bgw])
            with nc.allow_non_contiguous_dma(reason="small params"):
                c.dma('sp', lambda e: e.dma_start(out=dcol[:], in_=d_skip.rearrange("(k p) -> p k", p=128)), writes=[bdcol])
                c.dma('sp', lambda e: e.dma_start(out=gbc[:], in_=glu_b.rearrange("(k p) -> p k", p=128)), writes=[bgbc])
            GC = 1.5957691216057308
            for it in range(L // 512):
                yt_, byt_ = yt[it % 2], byt[it % 2]
                ut_, but_ = ut[it % 2], but[it % 2]
                ob_, bob_ = ob[it % 2], bob[it % 2]
                tsl = slice(it * 512, (it + 1) * 512)
                c.dma('sp', lambda e: e.dma_start(out=yt_[:], in_=YT[:, tsl].rearrange("(k p) t -> p k t", p=128)), reads=[bYT], writes=[byt_])
                c.dma('act', lambda e: e.dma_start(out=ut_[:], in_=PT[U0:U0 + 512, tsl].rearrange("(k p) t -> p k t", p=128)), reads=[bPT], writes=[but_])
                for k in range(4):
                    c.op('dve', lambda e: e.scalar_tensor_tensor(out=yt_[:, k, :], in0=ut_[:, k, :], scalar=dcol[:, k:k + 1], in1=yt_[:, k, :], op0=ALU.mult, op1=ALU.add),
                         reads=[but_, bdcol, byt_], writes=[byt_])
                c.op('act', lambda e: e.activation(out=sq[:], in_=yt_[:], func=AF.Square), reads=[byt_], writes=[bsq])
                c.op('dve', lambda e: e.tensor_scalar(out=sq[:], in0=sq[:], scalar1=0.044715, scalar2=1.0, op0=ALU.mult, op1=ALU.add), reads=[bsq], writes=[bsq])
                c.op('dve', lambda e: e.tensor_tensor(out=sq[:], in0=sq[:], in1=yt_[:], op=ALU.mult), reads=[bsq, byt_], writes=[bsq])
                c.op('act', lambda e: e.activation(out=sq[:], in_=sq[:], func=AF.Sigmoid, scale=GC), reads=[bsq], writes=[bsq])
                c.op('dve', lambda e: e.tensor_tensor(out=gy[:], in0=sq[:], in1=yt_[:], op=ALU.mult), reads=[bsq, byt_], writes=[bgy])
                c.op('act', lambda e: e.activation(out=gyb[:], in_=gy[:], func=AF.Copy), reads=[bgy], writes=[bgyb])
                for co in range(4):
                    ps, bps = g.ps[co % 4]
                    for k in range(4):
                        c.op('pe', lambda e: e.matmul(ps[:, :], lhsT=gw[:, k, co * 128:(co + 1) * 128], rhs=gyb[:, k, :], start=(k == 0), stop=(k == 3)),
                             reads=[bgw, bgyb], writes=[bps])
                    sg_, bsg_ = sg[co % 2], bsg[co % 2]
                    c.op('act', lambda e: e.activation(out=sg_[:], in_=ps[:, :], func=AF.Sigmoid, bias=gbc[:, co:co + 1], scale=1.0), reads=[bps, bgbc], writes=[bsg_])
                    c.op('dve', lambda e: e.tensor_tensor(out=ob_[:, co, :], in0=gy[:, co, :], in1=sg_[:], op=ALU.mult), reads=[bgy, bsg_, bob_], writes=[bob_])
                c.dma('sp', lambda e: e.dma_start(out=MT[512:1024, tsl].rearrange("(k p) t -> p k t", p=128), in_=ob_[:]), reads=[bob_], pwrites=[bMT])
            barrier(c)


def outproj_phase(c, g, MT, bMT, w_out, X, bX):
    nc = c.nc
    bXn = Buf('Xn')
    with ExitStack() as es:
        def T(name, shape, dt):
            return es.enter_context(nc.sbuf_tensor(uniq(name), shape, dt))
        wsb = T("op_w", [128, 8, D], BF16); bw = Buf()
        mt = [T("op_mt%d" % i, [128, 8, 512], BF16) for i in range(2)]; bmt = [Buf(), Buf()]
        xt = [T("op_xt%d" % i, [128, 4, D], F32) for i in range(2)]; bxt = [Buf(), Buf()]
        xo = [T("op_xo%d" % i, [128, 4, D], F32) for i in range(2)]; bxo = [Buf(), Buf()]
        for c0 in range(0, D, 512):
            c.dma('pool', lambda e: e.dma_start(out=wsb[:, :, c0:c0 + 512], in_=w_out[:, c0:c0 + 512].rearrange("(k p) f -> p k f", p=128)), pwrites=[bw])
        bw.seal()
        n = 0
        for it in range(L // 512):
            t0 = it * 512
            mt_, bmt_ = mt[it % 2], bmt[it % 2]
            xt_, bxt_ = xt[it % 2], bxt[it % 2]
            xo_, bxo_ = xo[it % 2], bxo[it % 2]
            c.dma('sp', lambda e: e.dma_start(out=mt_[:], in_=MT[:, t0:t0 + 512].rearrange("(k p) t -> p k t", p=128)), reads=[bMT], writes=[bmt_])
            c.dma('act', lambda e: e.dma_start(out=xt_[:], in_=X[t0:t0 + 512, :].rearrange("(j p) d -> p j d", p=128)), reads=[bX], writes=[bxt_])
            for j in range(4):
                for dh in range(2):
                    ps, bps = g.ps[n % 4]; n += 1
                    for k in range(8):
                        c.op('pe', lambda e: e.matmul(ps[:, :], lhsT=mt_[:, k, j * 128:(j + 1) * 128], rhs=wsb[:, k, dh * 512:(dh + 1) * 512], start=(k == 0), stop=(k == 7)),
                             reads=[bmt_, bw], writes=[bps])
                    c.op('dve', lambda e: e.tensor_tensor(out=xo_[:, j, dh * 512:(dh + 1) * 512], in0=ps[:, :], in1=xt_[:, j, dh * 512:(dh + 1) * 512], op=ALU.add),
                         reads=[bps, bxt_, bxo_], writes=[bxo_])
            c.dma('sp', lambda e: e.dma_start(out=X[t0:t0 + 512, :].rearrange("(j p) d -> p j d", p=128), in_=xo_[:]), reads=[bxo_], pwrites=[bXn])
        bXn.seal()
        barrier(c)
    return bXn


def t5_onehot():
    half = 16; max_exact = 8
    rel = np.arange(-255, 256)
    n = np.abs(rel)
    nf = np.maximum(n, 1).astype(np.float32)
    large = max_exact + (np.log(nf / np.float32(max_exact)) / np.float32(math.log(128 / max_exact)) * np.float32(half - max_exact)).astype(np.int32)
    large = np.minimum(large, half - 1)
    b = np.where(rel > 0, half, 0) + np.where(n < max_exact, n, large)
    oh = np.zeros((32, 512), np.float32)
    oh[b, np.arange(511)] = 1.0
    return oh


def attn_phase(c, g, PV, bPV, q_gain, k_gain, c_lambda, out_gain, rel_bias, onehot, layer_idx, QKT, bQKT, FV, bFV, MT, bMT, stage=3):
    nc = c.nc
    lam_init = 0.8 - 0.6 * math.exp(-0.3 * layer_idx)
    ptb, bptb = g.psb
    with ExitStack() as es:
        def T(name, shape, dt):
            return es.enter_context(nc.sbuf_tensor(uniq(name), shape, dt))
        g64 = T("at_g64", [128, 2, 64], F32); bg64 = Buf()
        gQK = T("at_gQK", [128, 16, 64], F32); bgQK = Buf()
        xq = [T("at_xq%d" % i, [128, 1024], BF16) for i in range(2)]; bxq = [Buf(), Buf()]
        sq = T("at_sq", [128, 1024], F32); bsq = Buf()
        ss = [T("at_ss%d" % i, [128, 16], F32) for i in range(2)]; bss = [Buf(), Buf()]
        xn = T("at_xn", [128, 1024], F32); bxn = Buf()
        xb = [T("at_xb%d" % i, [128, 1024], BF16) for i in range(2)]; bxb = [Buf(), Buf()]
        st = [T("at_st%d" % i, [128, 8, 512], BF16) for i in range(2)]; bst = [Buf(), Buf()]
        c.dma('sp', lambda e: e.dma_start(out=g64[:, 0, :], in_=q_gain.partition_broadcast(128)), pwrites=[bg64])
        c.dma('sp', lambda e: e.dma_start(out=g64[:, 1, :], in_=k_gain.partition_broadcast(128)), pwrites=[bg64])
        bg64.seal()
        c.op('dve', lambda e: e.tensor_scalar(out=gQK[:, 0:8, :], in0=g64[:, 0:1, :].broadcast_to([128, 8, 64]), scalar1=0.125, scalar2=None, op0=ALU.mult), reads=[bg64], writes=[bgQK])
        c.op('dve', lambda e: e.tensor_copy(out=gQK[:, 8:16, :], in_=g64[:, 1:2, :].broadcast_to([128, 8, 64])), reads=[bg64, bgQK], writes=[bgQK])
        for i in range(NT):
            xq_, bxq_ = xq[i % 2], bxq[i % 2]
            ss_, bss_ = ss[i % 2], bss[i % 2]
            xb_, bxb_ = xb[i % 2], bxb[i % 2]
            st_, bst_ = st[(i // 4) % 2], bst[(i // 4) % 2]
            c.dma('sp', lambda e: e.dma_start(out=xq_[:], in_=PV[i * 128:(i + 1) * 128, 0:1024]), reads=[bPV], writes=[bxq_])
            c.op('act', lambda e: e.activation(out=sq[:], in_=xq_[:], func=AF.Square), reads=[bxq_], writes=[bsq])
            c.op('dve', lambda e: e.tensor_reduce(out=ss_[:], in_=sq[:].rearrange("p (a d) -> p a d", d=64), axis=AX.X, op=ALU.add), reads=[bsq], writes=[bss_])
            c.op('dve', lambda e: e.tensor_scalar(out=ss_[:], in0=ss_[:], scalar1=1.0 / 64, scalar2=1e-6, op0=ALU.mult, op1=ALU.add), reads=[bss_], writes=[bss_])
            c.op('pool', lambda e: e.tensor_tensor(out=ss_[:], in0=ss_[:], in1=g.neghalf[:, 0:1].broadcast_to([128, 16]), op=ALU.pow), reads=[bss_, g.b_neghalf], writes=[bss_])
            c.op('dve', lambda e: e.tensor_tensor(out=xn[:].rearrange("p (a d) -> p a d", d=64), in0=xq_[:].rearrange("p (a d) -> p a d", d=64),
                                                  in1=ss_[:].unsqueeze(2).broadcast_to([128, 16, 64]), op=ALU.mult), reads=[bxq_, bss_], writes=[bxn])
            c.op('dve', lambda e: e.tensor_tensor(out=xb_[:], in0=xn[:], in1=gQK[:].rearrange("p a d -> p (a d)"), op=ALU.mult), reads=[bxn, bgQK], writes=[bxb_])
            for a in range(8):
                c.op('pe', lambda e: e.transpose(out=ptb[:, a * 128:(a + 1) * 128], in_=xb_[:, a * 128:(a + 1) * 128], identity=g.identb[:]), reads=[bxb_, g.b_identb], writes=[bptb])
            c.op('act', lambda e: e.activation(out=st_[:, :, (i % 4) * 128:(i % 4 + 1) * 128], in_=ptb[:, :].rearrange("p (a s) -> p a s", a=8), func=AF.Copy), reads=[bptb], writes=[bst_])
            if i % 4 == 3:
                t0 = (i // 4) * 512
                for a in range(8):
                    c.dma('sp' if a % 2 else 'act', lambda e: e.dma_start(out=QKT[a, :, t0:t0 + 512], in_=st_[:, a, :]), reads=[bst_], pwrites=[bQKT])
        bQKT.seal()
        barrier(c)
    if stage < 2:
        return
    with ExitStack() as es:
        def T(name, shape, dt):
            return es.enter_context(nc.sbuf_tensor(uniq(name), shape, dt))
        KT = T("at_KT", [128, L], BF16); bKT = Buf()
        Va = T("at_Va", [128, 64, 130], BF16); bVa = Buf()
        QT = [T("at_QT%d" % i, [128, 512], BF16) for i in range(2)]; bQT = [Buf(), Buf()]
        Pt = [T("at_P%d" % i, [128, 512], BF16) for i in range(4)]; bPt = [Buf() for _ in range(4)]
        tmp = [T("at_tmp%d" % i, [128, 512], F32) for i in range(2)]; btmp = [Buf(), Buf()]
        biasT = T("at_bias", [128, 4, 3, 128], F32); bbias = Buf()
        hank = T("at_hank", [128, 128], F32); bhank = Buf()
        cfar = T("at_cfar", [128, 4, 2], F32); bcfar = Buf()
        tab = T("at_tab", [32, 4], F32); btab = Buf()
        oh = T("at_oh", [32, 512], F32); boh = Buf()
        fv = T("at_fv", [4, 512], F32); bfv = Buf()
        lamt = T("at_lamt", [128, 4, 64], F32); blamt = Buf()
        lam = T("at_lam", [128, 8], F32); blam = Buf()
        gO = T("at_gO", [128, 128], F32); bgO = Buf()
        rs = [T("at_rs%d" % i, [128, 4], F32) for i in range(2)]; brs = [Buf(), Buf()]
        t1 = T("at_t1", [128, 128], F32); bt1 = Buf()
        w_ = T("at_w", [128, 128], F32); bw_ = Buf()
        junk = T("at_junk", [128, 128], F32); bjunk = Buf()
        wb = [T("at_wb%d" % i, [128, 128], BF16) for i in range(2)]; bwb = [Buf(), Buf()]
        ost = [T("at_ost%d" % i, [128, 512], BF16) for i in range(2)]; bost = [Buf(), Buf()]
        c.dma('sp', lambda e: e.dma_start(out=lamt[:].rearrange("p a d -> p (a d)"), in_=c_lambda.rearrange("a d -> (a d)").partition_broadcast(128)), writes=[blamt])
        c.op('dve', lambda e: e.tensor_tensor(out=lamt[:, 0, :], in0=lamt[:, 0, :], in1=lamt[:, 1, :], op=ALU.mult), reads=[blamt], writes=[blamt])
        c.op('dve', lambda e: e.tensor_tensor(out=lamt[:, 2, :], in0=lamt[:, 2, :], in1=lamt[:, 3, :], op=ALU.mult), reads=[blamt], writes=[blamt])
        c.op('dve', lambda e: e.tensor_reduce(out=lam[:, 0:1], in_=lamt[:, 0, :], axis=AX.X, op=ALU.add), reads=[blamt], writes=[blam])
        c.op('dve', lambda e: e.tensor_reduce(out=lam[:, 1:2], in_=lamt[:, 2, :], axis=AX.X, op=ALU.add), reads=[blamt, blam], writes=[blam])
        c.op('act', lambda e: e.activation(out=lam[:, 2:4], in_=lam[:, 0:2], func=AF.Exp), reads=[blam], writes=[blam])
        c.op('dve', lambda e: e.tensor_tensor(out=lam[:, 4:5], in0=lam[:, 3:4], in1=lam[:, 2:3], op=ALU.subtract), reads=[blam], writes=[blam])
        c.op('dve', lambda e: e.tensor_scalar(out=lam[:, 4:5], in0=lam[:, 4:5], scalar1=-lam_init, scalar2=None, op0=ALU.add), reads=[blam], writes=[blam])
        c.dma('sp', lambda e: e.dma_start(out=gO[:], in_=out_gain.partition_broadcast(128)), writes=[bgO])
        c.op('dve', lambda e: e.tensor_scalar(out=gO[:], in0=gO[:], scalar1=1.0 - lam_init, scalar2=None, op0=ALU.mult), reads=[bgO], writes=[bgO])
        c.dma('sp', lambda e: e.dma_start(out=tab[:], in_=rel_bias), writes=[btab])
        c.dma('act', lambda e: e.dma_start(out=oh[:], in_=onehot), writes=[boh])
        ps6, bps6 = g.ps[6]
        c.op('pe', lambda e: e.matmul(ps6[0:4, :], lhsT=tab[:, :], rhs=oh[:, :], start=True, stop=True), reads=[btab, boh], writes=[bps6])
        c.op('dve', lambda e: e.tensor_copy(out=fv[:], in_=ps6[0:4, :]), reads=[bps6], writes=[bfv])
        c.dma('sp', lambda e: e.dma_start(out=FV, in_=fv[:]), reads=[bfv], writes=[bFV])
        for h in range(4):
            for o in (-1, 0, 1):
                off = h * 512 + 128 * o + 128
                src = bass.AP(FV.tensor, off, [[1, 128], [1, 128]])
                c.dma('sp', lambda e: e.dma_start(out=hank[:], in_=src), reads=[bFV], writes=[bhank])
                c.op('dve', lambda e: e.tensor_copy(out=biasT[:, h, o + 1, :], in_=hank[:, ::-1]), reads=[bhank, bbias], writes=[bbias])
            c.dma('sp', lambda e: e.dma_start(out=cfar[:, h, 0:1], in_=bass.AP(FV.tensor, h * 512 + 0, [[0, 128], [1, 1]])), reads=[bFV], pwrites=[bcfar])
            c.dma('sp', lambda e: e.dma_start(out=cfar[:, h, 1:2], in_=bass.AP(FV.tensor, h * 512 + 510, [[0, 128], [1, 1]])), reads=[bFV], pwrites=[bcfar])
        bcfar.seal()
        ones_col_done = False
        nS = 0; nP = 0; nq = 0; ntmp = 0; nout = 0
        for h in range(4):
            c.dma('sp', lambda e: e.dma_start(out=KT[:], in_=QKT[4 + h, :, :]), reads=[bQKT], writes=[bKT])
            for half in range(2):
                c.dma('act', lambda e: e.dma_start(out=Va[:, half * 32:(half + 1) * 32, 0:128], in_=PV[half * 4096:(half + 1) * 4096, 1024 + h * 128:1024 + (h + 1) * 128].rearrange("(b p) d -> p b d", p=128)),
                      reads=[bPV], writes=[bVa])
            c.op('pool', lambda e: e.memset(Va[:, :, 128:129], 1.0), reads=[bVa], writes=[bVa])
            for qt in range(16):
                QT_, bQT_ = QT[nq % 2], bQT[nq % 2]; nq += 1
                c.dma('sp', lambda e: e.dma_start(out=QT_[:], in_=QKT[h, :, qt * 512:(qt + 1) * 512]), reads=[bQKT], writes=[bQT_])
                steps = [(comp, kb) for kb in range(64) for comp in range(2)]
                Sbank = {}

                SB = [0, 1, 2, 6]

                def emit_S(i):
                    comp, kb = steps[i]
                    S, bS = g.ps[SB[i % 4]]
                    c.op('pe', lambda e: e.matmul(S[:, :], lhsT=KT[64 * comp:64 * comp + 64, kb * 128:(kb + 1) * 128], rhs=QT_[64 * comp:64 * comp + 64, :], start=True, stop=True),
                         reads=[bKT, bQT_], writes=[bS])

                def emit_exp(i):
                    comp, kb = steps[i]
                    S, bS = g.ps[SB[i % 4]]
                    P_, bP_ = Pt[i % 4], bPt[i % 4]
                    near = (4 * qt - 1 <= kb <= 4 * qt + 4)
                    if not near:
                        col = cfar[:, h, 0:1] if kb < 4 * qt else cfar[:, h, 1:2]
                        c.op('act', lambda e: e.activation(out=P_[:], in_=S[:, :], func=AF.Exp, bias=col, scale=1.0), reads=[bS, bcfar], writes=[bP_])
                    else:
                        tm, btm = tmp[i % 2], btmp[i % 2]
                        for qs in range(4):
                            o = kb - (4 * qt + qs)
                            sl = slice(qs * 128, (qs + 1) * 128)
                            if abs(o) <= 1:
                                c.op('dve', lambda e: e.tensor_tensor(out=tm[:, sl], in0=S[:, sl], in1=biasT[:, h, o + 1, :], op=ALU.add), reads=[bS, bbias, btm], writes=[btm])
                            else:
                                col = cfar[:, h, 0:1] if o < 0 else cfar[:, h, 1:2]
                                c.op('dve', lambda e: e.tensor_scalar(out=tm[:, sl], in0=S[:, sl], scalar1=col, scalar2=None, op0=ALU.add), reads=[bS, bcfar, btm], writes=[btm])
                        c.op('act', lambda e: e.activation(out=P_[:], in_=tm[:], func=AF.Exp), reads=[btm], writes=[bP_])

                def emit_PV(i):
                    comp, kb = steps[i]
                    P_, bP_ = Pt[i % 4], bPt[i % 4]
                    for qs in range(4):
                        a = comp * 4 + qs
                        acc, bacc = g.ps[3 + a // 3]
                        c0 = (a % 3) * 130
                        first = (kb == 0) and ((comp == 0 and a in (0, 3)) or (comp == 1 and a == 6))
                        c.op('pe', lambda e: e.matmul(acc[:, c0:c0 + 129], lhsT=P_[:, qs * 128:(qs + 1) * 128], rhs=Va[:, kb, 0:129], start=first, stop=(kb == 63), skip_group_check=True),
                             reads=[bP_, bVa], writes=[bacc])
                npair = len(steps) // 2
                emit_S(0); emit_S(1); emit_S(2); emit_S(3)
                for j in range(npair):
                    emit_exp(2 * j); emit_exp(2 * j + 1)
                    if j + 2 < npair:
                        emit_S(2 * j + 4); emit_S(2 * j + 5)
                    emit_PV(2 * j); emit_PV(2 * j + 1)
                os_, bos_ = ost[nout % 2], bost[nout % 2]; nout += 1
                for qs in range(4):
                    a0 = qs; a1 = 4 + qs
                    acc0, bacc0 = g.ps[3 + a0 // 3]; o0 = (a0 % 3) * 130
                    acc1, bacc1 = g.ps[3 + a1 // 3]; o1 = (a1 % 3) * 130
                    rs_, brs_ = rs[qs % 2], brs[qs % 2]
                    wb_, bwb_ = wb[qs % 2], bwb[qs % 2]
                    c.op('dve', lambda e: e.reciprocal(out=rs_[:, 0:1], in_=acc0[:, o0 + 128:o0 + 129]), reads=[bacc0], writes=[brs_])
                    c.op('dve', lambda e: e.reciprocal(out=rs_[:, 1:2], in_=acc1[:, o1 + 128:o1 + 129]), reads=[bacc1, brs_], writes=[brs_])
                    c.op('dve', lambda e: e.tensor_tensor(out=rs_[:, 1:2], in0=rs_[:, 1:2], in1=lam[:, 4:5], op=ALU.mult), reads=[brs_, blam], writes=[brs_])
                    c.op('dve', lambda e: e.tensor_scalar(out=t1[:], in0=acc1[:, o1:o1 + 128], scalar1=rs_[:, 1:2], scalar2=None, op0=ALU.mult), reads=[bacc1, brs_], writes=[bt1])
                    c.op('dve', lambda e: e.scalar_tensor_tensor(out=w_[:], in0=acc0[:, o0:o0 + 128], scalar=rs_[:, 0:1], in1=t1[:], op0=ALU.mult, op1=ALU.add), reads=[bacc0, brs_, bt1], writes=[bw_])
                    c.op('dve', lambda e: e.scalar_tensor_tensor(out=junk[:], in0=w_[:], scalar=1.0, in1=w_[:], op0=ALU.mult, op1=ALU.mult, accum_out=rs_[:, 2:3]), reads=[bw_, brs_], writes=[bjunk, brs_])
                    c.op('dve', lambda e: e.tensor_scalar(out=rs_[:, 2:3], in0=rs_[:, 2:3], scalar1=1.0 / 128, scalar2=1e-6, op0=ALU.mult, op1=ALU.add), reads=[brs_], writes=[brs_])
                    c.op('pool', lambda e: e.tensor_tensor(out=rs_[:, 2:3], in0=rs_[:, 2:3], in1=g.neghalf[:, 0:1], op=ALU.pow), reads=[brs_, g.b_neghalf], writes=[brs_])
                    c.op('dve', lambda e: e.scalar_tensor_tensor(out=wb_[:], in0=w_[:], scalar=rs_[:, 2:3], in1=gO[:], op0=ALU.mult, op1=ALU.mult), reads=[bw_, brs_, bgO], writes=[bwb_])
                    c.op('pe', lambda e: e.transpose(out=ptb[:, qs * 128:(qs + 1) * 128], in_=wb_[:], identity=g.identb[:]), reads=[bwb_, g.b_identb], writes=[bptb])
                c.op('act', lambda e: e.activation(out=os_[:], in_=ptb[:, 0:512], func=AF.Copy), reads=[bptb], writes=[bos_])
                c.dma('sp', lambda e: e.dma_start(out=MT[h * 128:(h + 1) * 128, qt * 512:(qt + 1) * 512], in_=os_[:]), reads=[bos_], pwrites=[bMT])
        barrier(c)


GTB = 512


def gdn_phase(c, g, PT, bPT, PV, bPV, conv_w, a_log, dt_bias, out_gain, GQ, bGQ, GR, bGR, OD, bODs, OD2, bOD2s, MT, bMT, stage=4):
    nc = c.nc
    ptb, bptb = g.psb
    NB = TBK
    with ExitStack() as es:
        def T(name, shape, dt):
            return es.enter_context(nc.sbuf_tensor(uniq(name), shape, dt))
        cw = T("gd_cw", [128, 12, 5], F32); bcw = Buf()
        onesb = T("gd_onesb", [128, 128], BF16); bonesb = Buf()
        xin = [T("gd_xin%d" % i, [128, NB + 4], F32) for i in range(2)]; bxin = [Buf(), Buf()]
        y = T("gd_y", [128, NB], F32); by = Buf()
        s = T("gd_s", [128, NB], F32); bs = Buf()
        sqb = T("gd_sqb", [128, NB], BF16); bsqb = Buf()
        rst = T("gd_rst", [128, NB], F32); brst = Buf()
        ob = [T("gd_ob%d" % i, [128, NB], BF16) for i in range(2)]; bob = [Buf(), Buf()]
        with nc.allow_non_contiguous_dma(reason="small params"):
            for j in range(5):
                c.dma('sp', lambda e: e.dma_start(out=cw[:, :, j], in_=conv_w[j, :].rearrange("(k p) -> p k", p=128)), pwrites=[bcw])
        bcw.seal()
        c.op('pool', lambda e: e.memset(onesb[:], 1.0), writes=[bonesb])
        n = 0
        for cbk in range(12):
            for tb in range(L // NB):
                x_, bx_ = xin[n % 2], bxin[n % 2]
                o_, bo_ = ob[n % 2], bob[n % 2]
                n += 1
                t0 = tb * NB
                lo = max(t0 - 2, 0); hi = min(t0 + NB + 2, L)
                if tb == 0:
                    c.op('pool', lambda e: e.memset(x_[:, 0:2], 0.0), writes=[bx_])
                if tb == L // NB - 1:
                    c.op('pool', lambda e: e.memset(x_[:, NB + 2:NB + 4], 0.0), writes=[bx_])
                c.dma('sp', lambda e: e.dma_start(out=x_[:, lo - (t0 - 2):hi - (t0 - 2)], in_=PT[cbk * 128:(cbk + 1) * 128, lo:hi]), reads=[bPT, bx_], writes=[bx_])
                c.op('dve', lambda e: e.tensor_scalar(out=y[:], in0=x_[:, 0:NB], scalar1=cw[:, cbk, 0:1], scalar2=None, op0=ALU.mult), reads=[bx_, bcw], writes=[by])
                for j in range(1, 5):
                    c.op('dve', lambda e: e.scalar_tensor_tensor(out=y[:], in0=x_[:, j:j + NB], scalar=cw[:, cbk, j:j + 1], in1=y[:], op0=ALU.mult, op1=ALU.add),
                         reads=[bx_, bcw, by], writes=[by])
                c.op('act', lambda e: e.activation(out=s[:], in_=y[:], func=AF.Silu), reads=[by], writes=[bs])
                if cbk < 8:
                    c.op('act', lambda e: e.activation(out=sqb[:], in_=s[:], func=AF.Square), reads=[bs], writes=[bsqb])
                    for hf in range(NB // 512):
                        ps, bps = g.ps[hf % 4]
                        c.op('pe', lambda e: e.matmul(ps[:, :], lhsT=onesb[:], rhs=sqb[:, hf * 512:(hf + 1) * 512], start=True, stop=True), reads=[bonesb, bsqb], writes=[bps])
                        c.op('dve', lambda e: e.tensor_scalar(out=rst[:, hf * 512:(hf + 1) * 512], in0=ps[:, :], scalar1=1e-6, scalar2=None, op0=ALU.add), reads=[bps, brst], writes=[brst])
                    c.op('act', lambda e: e.activation(out=rst[:], in_=rst[:], func=AF.Ln), reads=[brst], writes=[brst])
                    c.op('act', lambda e: e.activation(out=rst[:], in_=rst[:], func=AF.Exp, scale=-0.5), reads=[brst], writes=[brst])
                    sc = (128.0 ** -0.5) if cbk < 4 else 1.0
                    c.op('dve', lambda e: e.scalar_tensor_tensor(out=o_[:], in0=s[:], scalar=sc, in1=rst[:], op0=ALU.mult, op1=ALU.mult), reads=[bs, brst], writes=[bo_])
                else:
                    c.op('act', lambda e: e.activation(out=o_[:], in_=s[:], func=AF.Copy), reads=[bs], writes=[bo_])
                c.dma('act', lambda e: e.dma_start(out=GQ[cbk, :, t0:t0 + NB], in_=o_[:]), reads=[bo_], pwrites=[bGQ])
        bGQ.seal()
        barrier(c)
    if stage < 2:
        return
    with ExitStack() as es0:
        def T0(name, shape, dt):
            return es0.enter_context(nc.sbuf_tensor(uniq(name), shape, dt))
        NQ = 5
        cols = [T0("gd_cols%d" % d, [128, 64, 4 * NQ], F32) for d in range(2)]; bcols = [Buf(), Buf()]
        sel = T0("gd_sel", [4, 4, 128], F32); bsel = Buf()
        with ExitStack() as es:
            def T(name, shape, dt):
                return es.enter_context(nc.sbuf_tensor(uniq(name), shape, dt))
            GP = 2048
            ar = T("gd_ar", [4, GP], F32); bar_ = Buf()
            br = T("gd_br", [4, GP], F32); bbr = Buf()
            w1 = T("gd_w1", [4, GP], F32); bw1 = Buf()
            w2 = T("gd_w2", [4, GP], F32); bw2 = Buf()
            gam = T("gd_gam", [4, GP], F32); bet = T("gd_bet", [4, GP], F32); egam = T("gd_egam", [4, GP], F32); brw = Buf()
            q3 = T("gd_q3", [4, GP], F32); bq3 = Buf()
            q4 = T("gd_q4", [4, GP], F32); bq4 = Buf()
            q5 = T("gd_q5", [4, GP], F32); bq5 = Buf()
            msk = T("gd_msk", [4, GP], F32); bmsk = Buf()
            pc = T("gd_pc", [4, 4], F32); bpc = Buf()
            c.op('pool', lambda e: e.memset(msk[:], 1.0), writes=[bmsk])
            c.op('pool', lambda e: e.memset(msk[:].rearrange("p (c j) -> p c j", j=64)[:, :, 0:1], 0.0), reads=[bmsk], writes=[bmsk])
            c.op('pool', lambda e: e.memset(sel[:], 0.0), writes=[bsel])
            c.op('pool', lambda e: e.affine_select(out=sel[:], in_=sel[:], pattern=[[-1, 4], [0, 128]], compare_op=ALU.not_equal, fill=1.0, base=0, channel_multiplier=1),
                 reads=[bsel], writes=[bsel])
            for d in range(2):
                with nc.allow_non_contiguous_dma(reason="small params"):
                    c.dma('sp', lambda e: e.dma_start(out=pc[:, 0:1], in_=dt_bias[d, :].rearrange("(h o) -> h o", o=1)), reads=[bpc], writes=[bpc])
                    c.dma('sp', lambda e: e.dma_start(out=pc[:, 1:2], in_=a_log[d, :].rearrange("(h o) -> h o", o=1)), reads=[bpc], writes=[bpc])
                c.op('act', lambda e: e.activation(out=pc[:, 2:3], in_=pc[:, 1:2], func=AF.Exp), reads=[bpc], writes=[bpc])
                c.op('dve', lambda e: e.tensor_scalar(out=pc[:, 2:3], in0=pc[:, 2:3], scalar1=-1.0, scalar2=None, op0=ALU.mult), reads=[bpc], writes=[bpc])
                for tp in range(L // GP):
                    nbp = tp if d == 0 else L // GP - 1 - tp
                    c.dma('sp', lambda e: e.dma_start(out=ar[:], in_=PT[1536 + 4 * d:1540 + 4 * d, nbp * GP:(nbp + 1) * GP]), reads=[bPT, bar_], writes=[bar_])
                    c.dma('act', lambda e: e.dma_start(out=br[:], in_=PT[1544 + 4 * d:1548 + 4 * d, nbp * GP:(nbp + 1) * GP]), reads=[bPT, bbr], writes=[bbr])
                    asrc = ar[:, ::-1] if d else ar[:, :]
                    bsrc = br[:, ::-1] if d else br[:, :]
                    c.op('dve', lambda e: e.tensor_scalar(out=w1[:], in0=asrc, scalar1=pc[:, 0:1], scalar2=None, op0=ALU.add), reads=[bar_, bpc], writes=[bw1])
                    c.op('dve', lambda e: e.tensor_scalar(out=w2[:], in0=w1[:], scalar1=-1.0, scalar2=None, op0=ALU.mult), reads=[bw1], writes=[bw2])
                    c.op('dve', lambda e: e.tensor_tensor(out=w2[:], in0=w2[:], in1=w1[:], op=ALU.min), reads=[bw1, bw2], writes=[bw2])
                    c.op('act', lambda e: e.activation(out=w2[:], in_=w2[:], func=AF.Exp), reads=[bw2], writes=[bw2])
                    c.op('act', lambda e: e.activation(out=w2[:], in_=w2[:], func=AF.Ln, bias=1.0, scale=1.0), reads=[bw2], writes=[bw2])
                    c.op('dve', lambda e: e.scalar_tensor_tensor(out=w1[:], in0=w1[:], scalar=0.0, in1=w2[:], op0=ALU.max, op1=ALU.add), reads=[bw1, bw2], writes=[bw1])
                    c.op('dve', lambda e: e.tensor_scalar(out=w1[:], in0=w1[:], scalar1=pc[:, 2:3], scalar2=None, op0=ALU.mult), reads=[bw1, bpc], writes=[bw1])
                    c.op('dve', lambda e: e.tensor_tensor_scan(out=gam[:], data0=msk[:], data1=w1[:], initial=0.0, op0=ALU.mult, op1=ALU.add), reads=[bmsk, bw1, brw], writes=[brw])
                    c.op('act', lambda e: e.activation(out=bet[:], in_=bsrc, func=AF.Sigmoid), reads=[bbr, brw], writes=[brw])
                    c.op('act', lambda e: e.activation(out=egam[:], in_=gam[:], func=AF.Exp), reads=[brw], writes=[brw])
                    c.op('dve', lambda e: e.tensor_tensor(out=q3[:], in0=bet[:], in1=egam[:], op=ALU.mult), reads=[brw, bq3], writes=[bq3])
                    g3 = gam[:].rearrange("p (c j) -> p c j", j=64)
                    c.op('dve', lambda e: e.tensor_tensor(out=q4[:].rearrange("p (c j) -> p c j", j=64), in0=g3[:, :, 63:64].broadcast_to([4, GP // 64, 64]), in1=g3, op=ALU.subtract),
                         reads=[brw, bq4], writes=[bq4])
                    c.op('act', lambda e: e.activation(out=q4[:], in_=q4[:], func=AF.Exp), reads=[bq4], writes=[bq4])
                    c.op('dve', lambda e: e.tensor_scalar(out=q5[:], in0=gam[:], scalar1=-1.0, scalar2=None, op0=ALU.mult), reads=[brw, bq5], writes=[bq5])
                    quants = [(gam, brw), (bet, brw), (q3, bq3), (q4, bq4), (q5, bq5)]
                    for bl in range(GP // 128):
                        blk = tp * (GP // 128) + bl
                        pc_, bpc_ = g.ps[blk % 2]
                        for qi, (qt_, bq_) in enumerate(quants):
                            c.op('pe', lambda e: e.transpose(out=pc_[:, qi * 4:(qi + 1) * 4], in_=qt_[0:4, bl * 128:(bl + 1) * 128], identity=g.ident32[0:4, 0:4]),
                                 reads=[bq_, g.b_ident32], writes=[bpc_])
                        c.op('act', lambda e: e.activation(out=cols[d][:, blk, :], in_=pc_[:, 0:4 * NQ], func=AF.Copy), reads=[bpc_, bcols[d]], writes=[bcols[d]])
                    for qi, rt in enumerate((gam, bet, egam)):
                        c.dma('sp', lambda e: e.dma_start(out=GR[d, qi, :, tp * GP:(tp + 1) * GP], in_=rt[:]), reads=[brw], pwrites=[bGR])
            bGR.seal()
            barrier(c)
        if stage < 3:
            return
        with ExitStack() as es:
            def T(name, shape, dt):
                return es.enter_context(nc.sbuf_tensor(uniq(name), shape, dt))
            nm_le = T("gm_nmle", [128, 128], F32)
            nm_geT = T("gm_nmgeT", [128, 128], F32)
            m_stT = T("gm_mstT", [128, 128], F32)
            bmk = Buf()
            c.op('pool', lambda e: e.memset(nm_le[:], 0.0), writes=[bmk])
            c.op('pool', lambda e: e.affine_select(out=nm_le[:], in_=nm_le[:], pattern=[[-1, 128]], compare_op=ALU.is_gt, fill=-30000.0, base=0, channel_multiplier=1), reads=[bmk], writes=[bmk])
            c.op('pool', lambda e: e.memset(nm_le[64:128, 0:64], -30000.0), reads=[bmk], writes=[bmk])
            c.op('pool', lambda e: e.memset(nm_geT[:], 0.0), reads=[bmk], writes=[bmk])
            c.op('pool', lambda e: e.affine_select(out=nm_geT[:], in_=nm_geT[:], pattern=[[1, 128]], compare_op=ALU.is_ge, fill=-30000.0, base=0, channel_multiplier=-1), reads=[bmk], writes=[bmk])
            c.op('pool', lambda e: e.memset(nm_geT[0:64, 64:128], -30000.0), reads=[bmk], writes=[bmk])
            c.op('pool', lambda e: e.memset(m_stT[:], 1.0), reads=[bmk], writes=[bmk])
            c.op('pool', lambda e: e.affine_select(out=m_stT[:], in_=m_stT[:], pattern=[[1, 128]], compare_op=ALU.is_gt, fill=0.0, base=0, channel_multiplier=-1), reads=[bmk], writes=[bmk])

            class CH:
                pass
            chs = []
            for d in range(2):
                ch = CH(); chs.append(ch)
                ch.d = d

                def TT(name, shape, dt, d=d):
                    return (T("gm%d_%s" % (d, name), shape, dt), Buf())
                ch.nat = [TT("nat%d" % i, [128, GTB], BF16) for i in range(3)]
                ch.arr = [[TT("arr%d_%d" % (i, j), [128, GTB], BF16) for j in range(2)] for i in range(3)]
                ch.rts = [[TT("rt%d_%d" % (q, j), [4, GTB], F32) for j in range(2)] for q in range(3)]
                ch.S32 = TT("S32", [128, 128], F32); ch.Sb = TT("Sb", [128, 128], BF16)
                ch.tmpD = TT("tmpD", [128, 128], F32); ch.Dst = TT("Dst", [128, 128], F32); ch.DTi = TT("DTi", [128, 128], F32); ch.DTs = TT("DTs", [128, 128], F32)
                ch.A_ = TT("A", [128, 128], BF16); ch.AT_ = TT("AT", [128, 128], BF16); ch.atT = TT("attnT", [128, 128], BF16)
                ch.Pm = [TT("P%d" % i, [128, 128], BF16) for i in range(6)]
                ch.Qm = [TT("Q%d" % i, [128, 128], BF16) for i in range(5)]
                ch.W32 = TT("W32", [128, 256], F32); ch.Wb = TT("Wb", [128, 256], BF16)
                ch.kdec = TT("kdec", [128, 128], BF16); ch.kcT = TT("kcT", [128, 128], BF16); ch.qdec = TT("qdec", [128, 128], BF16); ch.vnew = TT("vnew", [128, 128], BF16)
                ch.elc = TT("elc", [128, 2], F32)
                ch.osb = [TT("osb%d" % i, [128, 128], F32) for i in range(2)]
                ch.osf = [TT("osf%d" % i, [128, 128], F32) for i in range(2)]
                ch.bA = g.ps[3 * d + 0]; ch.bB = g.ps[3 * d + 1]; ch.bC = g.ps[3 * d + 2]
                ch.pb0 = 512 * d
                ch.nblk = 0

            def block_gen(ch, h, blk, b, cur, rcur):
                d = ch.d
                (qA, bqA), (kA, bkA), (vA, bvA) = cur
                bs_ = slice(b * 128, (b + 1) * 128)
                cl = cols[d][:, blk, :]
                gcol = cl[:, 0 + h:0 + h + 1]; bcol = cl[:, 4 + h:4 + h + 1]; begcol = cl[:, 8 + h:8 + h + 1]
                ekdcol = cl[:, 12 + h:12 + h + 1]; ngcol = cl[:, 16 + h:16 + h + 1]
                pA, bpA = ch.bA; pB, bpB = ch.bB; pC, bpC = ch.bC
                pb0 = ch.pb0
                tmpD, btmpD = ch.tmpD; Dst, bDst = ch.Dst; DTi, bDTi = ch.DTi; DTs, bDTs = ch.DTs
                A_, bA_ = ch.A_; AT_, bAT_ = ch.AT_; atT, batT = ch.atT
                W32, bW32 = ch.W32; Wb, bWb = ch.Wb; kdec, bkdec = ch.kdec; kcT, bkcT = ch.kcT; qdec, bqdec = ch.qdec; vnew, bvnew = ch.vnew
                elc, belc = ch.elc; S32, bS32 = ch.S32; Sb, bSb = ch.Sb
                for qi, (rt, brt) in enumerate(rcur):
                    c.op('pe', lambda e: e.matmul(pA[:, qi * 128:(qi + 1) * 128], lhsT=sel[:, h, :], rhs=rt[0:4, bs_], start=True, stop=True, skip_group_check=True),
                         reads=[bsel, brt], writes=[bpA])
                    yield
                c.op('pe', lambda e: e.matmul(pB[:, 0:128], lhsT=kA[:, bs_], rhs=kA[:, bs_], start=True, stop=True, skip_group_check=True), reads=[bkA], writes=[bpB]); yield
                c.op('pe', lambda e: e.matmul(pB[:, 128:256], lhsT=kA[:, bs_], rhs=qA[:, bs_], start=True, stop=True, skip_group_check=True), reads=[bkA, bqA], writes=[bpB]); yield
                c.op('pe', lambda e: e.transpose(out=ptb[:, pb0:pb0 + 128], in_=vA[:, bs_], identity=g.identb[:]), reads=[bvA, g.b_identb], writes=[bptb]); yield
                c.op('pe', lambda e: e.transpose(out=ptb[:, pb0 + 128:pb0 + 256], in_=kA[:, bs_], identity=g.identb[:]), reads=[bkA, g.b_identb], writes=[bptb]); yield
                c.op('dve', lambda e: e.scalar_tensor_tensor(out=tmpD[:], in0=pA[:, 0:128], scalar=-1.0, in1=nm_le[:], op0=ALU.mult, op1=ALU.add), reads=[bpA, bmk], writes=[btmpD]); yield
                c.op('act', lambda e: e.activation(out=Dst[:], in_=tmpD[:], func=AF.Exp, bias=gcol, scale=1.0), reads=[btmpD, bcols[d]], writes=[bDst]); yield
                c.op('dve', lambda e: e.tensor_tensor(out=tmpD[:], in0=pA[:, 0:128], in1=nm_geT[:], op=ALU.add), reads=[bpA, bmk, btmpD], writes=[btmpD]); yield
                c.op('act', lambda e: e.activation(out=DTi[:], in_=tmpD[:], func=AF.Exp, bias=ngcol, scale=1.0), reads=[btmpD, bcols[d]], writes=[bDTi]); yield
                c.op('dve', lambda e: e.tensor_tensor(out=DTs[:], in0=DTi[:], in1=m_stT[:], op=ALU.mult), reads=[bDTi, bmk], writes=[bDTs]); yield
                c.op('dve', lambda e: e.tensor_tensor(out=DTs[:], in0=pA[:, 128:256], in1=DTs[:], op=ALU.mult), reads=[bpA, bDTs], writes=[bDTs]); yield
                c.op('dve', lambda e: e.scalar_tensor_tensor(out=A_[:], in0=pB[:, 0:128], scalar=bcol, in1=Dst[:], op0=ALU.mult, op1=ALU.mult), reads=[bpB, bcols[d], bDst], writes=[bA_]); yield
                c.op('dve', lambda e: e.tensor_tensor(out=AT_[:], in0=pB[:, 0:128], in1=DTs[:], op=ALU.mult), reads=[bpB, bDTs], writes=[bAT_]); yield
                c.op('dve', lambda e: e.tensor_tensor(out=atT[:], in0=pB[:, 128:256], in1=DTi[:], op=ALU.mult), reads=[bpB, bDTi], writes=[batT]); yield
                c.op('dve', lambda e: e.tensor_scalar(out=Wb[:, 0:128], in0=ptb[:, pb0:pb0 + 128], scalar1=bcol, scalar2=None, op0=ALU.mult), reads=[bptb, bcols[d], bWb], writes=[bWb]); yield
                c.op('dve', lambda e: e.tensor_scalar(out=Wb[:, 128:256], in0=ptb[:, pb0 + 128:pb0 + 256], scalar1=begcol, scalar2=None, op0=ALU.mult), reads=[bptb, bcols[d], bWb], writes=[bWb]); yield
                c.op('act', lambda e: e.activation(out=kdec[:], in_=ptb[:, pb0 + 128:pb0 + 256], func=AF.Copy, scale=ekdcol), reads=[bptb, bcols[d]], writes=[bkdec]); yield
                c.op('dve', lambda e: e.tensor_tensor(out=qdec[:], in0=pA[:, 256:384], in1=qA[:, bs_], op=ALU.mult), reads=[bpA, bqA], writes=[bqdec]); yield
                c.op('act', lambda e: e.activation(out=elc[:], in_=pA[:, 256:384].rearrange("p (c j) -> p c j", j=64)[:, :, 63], func=AF.Copy), reads=[bpA], writes=[belc]); yield
                Pc, bPc = AT_, bAT_
                Qc, bQc = A_, bA_
                for lev in range(6):
                    c.op('pe', lambda e: e.matmul(pC[:, 0:256], lhsT=Pc[:], rhs=Wb[:], start=True, stop=True, skip_group_check=True), reads=[bPc, bWb], writes=[bpC]); yield
                    c.op('dve', lambda e: e.tensor_tensor(out=Wb[:], in0=Wb[:], in1=pC[:, 0:256], op=(ALU.subtract if lev == 0 else ALU.add)), reads=[bWb, bpC], writes=[bWb]); yield
                    if lev < 5:
                        Pn, bPn = ch.Pm[lev + 1]
                        c.op('pe', lambda e: e.matmul(pB[:, 256:384], lhsT=Qc[:], rhs=Pc[:], start=True, stop=True, skip_group_check=True), reads=[bQc, bPc], writes=[bpB]); yield
                        if lev < 4:
                            Qn, bQn = ch.Qm[lev + 1]
                            c.op('pe', lambda e: e.matmul(pB[:, 384:512], lhsT=Pc[:], rhs=Qc[:], start=True, stop=True, skip_group_check=True), reads=[bQc, bPc], writes=[bpB]); yield
                            c.op('dve', lambda e: e.tensor_copy(out=Qn[:], in_=pB[:, 384:512]), reads=[bpB], writes=[bQn]); yield
                        c.op('act', lambda e: e.activation(out=Pn[:], in_=pB[:, 256:384], func=AF.Copy), reads=[bpB], writes=[bPn]); yield
                        Pc, bPc = Pn, bPn
                        if lev < 4:
                            Qc, bQc = Qn, bQn
                c.op('pe', lambda e: e.transpose(out=ptb[:, pb0 + 256:pb0 + 384], in_=Wb[:, 128:256], identity=g.identb[:]), reads=[bWb, g.b_identb], writes=[bptb]); yield
                c.op('act', lambda e: e.activation(out=kcT[:], in_=ptb[:, pb0 + 256:pb0 + 384], func=AF.Copy), reads=[bptb], writes=[bkcT]); yield
                for ci in range(2):
                    r0 = 64 * ci
                    c.op('pe', lambda e: e.matmul(pC[r0:r0 + 64, 256:384], lhsT=kcT[:, r0:r0 + 64], rhs=Sb[:, :], start=True, stop=True, skip_group_check=True), reads=[bkcT, bSb], writes=[bpC]); yield
                    c.op('dve', lambda e: e.tensor_tensor(out=vnew[r0:r0 + 64, :], in0=Wb[r0:r0 + 64, 0:128], in1=pC[r0:r0 + 64, 256:384], op=ALU.subtract), reads=[bWb, bpC, bvnew], writes=[bvnew]); yield
                    c.op('pe', lambda e: e.matmul(pA[r0:r0 + 64, 384:512], lhsT=qdec[:, r0:r0 + 64], rhs=Sb[:, :], start=True, stop=False, skip_group_check=True), reads=[bqdec, bSb], writes=[bpA])
                    c.op('pe', lambda e: e.matmul(pA[r0:r0 + 64, 384:512], lhsT=atT[r0:r0 + 64, r0:r0 + 64], rhs=vnew[r0:r0 + 64, :], start=False, stop=True, skip_group_check=True), reads=[batT, bvnew], writes=[bpA]); yield
                    c.op('pe', lambda e: e.matmul(pC[:, 384:512], lhsT=kdec[r0:r0 + 64, :], rhs=vnew[r0:r0 + 64, :], start=True, stop=True, skip_group_check=True), reads=[bkdec, bvnew], writes=[bpC]); yield
                    c.op('dve', lambda e: e.scalar_tensor_tensor(out=Sb[:], in0=S32[:], scalar=elc[:, ci:ci + 1], in1=pC[:, 384:512], op0=ALU.mult, op1=ALU.add), reads=[bS32, belc, bpC], writes=[bSb]); yield
                    c.op('dve', lambda e: e.scalar_tensor_tensor(out=S32[:], in0=S32[:], scalar=elc[:, ci:ci + 1], in1=pC[:, 384:512], op0=ALU.mult, op1=ALU.add), reads=[bS32, belc, bpC], writes=[bS32]); yield
                os_, bos_ = ch.osb[ch.nblk % 2]
                of_, bof_ = ch.osf[ch.nblk % 2]
                ch.nblk += 1
                c.op('act', lambda e: e.activation(out=os_[:], in_=pA[:, 384:512], func=AF.Copy), reads=[bpA], writes=[bos_]); yield
                if d == 0:
                    c.dma('sp', lambda e: e.dma_start(out=OD[blk * 128:(blk + 1) * 128, h * 128:(h + 1) * 128], in_=os_[:]), reads=[bos_], pwrites=[bODs[h]]); yield
                else:
                    pf, bpf = g.ps[6]
                    c.op('pe', lambda e: e.matmul(pf[:, 0:128], lhsT=g.J32[:], rhs=os_[:], start=True, stop=True), reads=[g.b_J32, bos_], writes=[bpf]); yield
                    c.op('act', lambda e: e.activation(out=of_[:], in_=pf[:, 0:128], func=AF.Copy), reads=[bpf], writes=[bof_]); yield
                    c.dma('act', lambda e: e.dma_start(out=OD2[L - (blk + 1) * 128:L - blk * 128, h * 128:(h + 1) * 128], in_=of_[:]), reads=[bof_], pwrites=[bOD2s[h]]); yield

            for h in range(4):
                for ch in chs:
                    S32, bS32 = ch.S32; Sb, bSb = ch.Sb
                    c.op('pool', lambda e: e.memset(S32[:], 0.0), reads=[bS32], writes=[bS32])
                    c.op('pool', lambda e: e.memset(Sb[:], 0.0), reads=[bSb], writes=[bSb])
                for tb in range(L // GTB):
                    curs = []; rcurs = []
                    for ch in chs:
                        d = ch.d
                        cur = []
                        for ai in range(3):
                            a_, ba_ = ch.arr[ai][tb % 2]
                            if d == 0:
                                c.dma('sp' if ai % 2 else 'act', lambda e: e.dma_start(out=a_[:], in_=GQ[ai * 4 + h, :, tb * GTB:(tb + 1) * GTB]), reads=[bGQ], writes=[ba_])
                            else:
                                n_, bn_ = ch.nat[ai]
                                c.dma('sp' if ai % 2 else 'act', lambda e: e.dma_start(out=n_[:], in_=GQ[ai * 4 + h, :, L - (tb + 1) * GTB:L - tb * GTB]), reads=[bGQ], writes=[bn_])
                                c.op('pool', lambda e: e.tensor_copy(out=a_[:], in_=n_[:, ::-1]), reads=[bn_], writes=[ba_])
                            cur.append((a_, ba_))
                        rcur = []
                        for qi in range(3):
                            r_, br_ = ch.rts[qi][tb % 2]
                            c.dma('sp', lambda e: e.dma_start(out=r_[:], in_=GR[d, qi, :, tb * GTB:(tb + 1) * GTB]), reads=[bGR], writes=[br_])
                            rcur.append((r_, br_))
                        curs.append(cur); rcurs.append(rcur)
                    for b in range(GTB // 128):
                        blk = tb * (GTB // 128) + b
                        gens = [block_gen(ch, h, blk, b, curs[i], rcurs[i]) for i, ch in enumerate(chs)]
                        alive = list(gens)
                        while alive:
                            for gnr in list(alive):
                                try:
                                    next(gnr)
                                except StopIteration:
                                    alive.remove(gnr)
                bODs[h].seal(); bOD2s[h].seal()
            barrier(c)
    if stage < 4:
        return
    gated_norm_finalize(c, g, OD, bODs, PV, bPV, 1536, out_gain, MT, bMT, 512, "gf_", OA2=OD2, bOA2s=bOD2s)


N_ACTIVE = 4
DEPTH = 4

PARAM_NAMES = ['mix_norm', 'ffn_norm', 'ev_w_in', 'ev_w_out', 'a_lb_logits', 'a_out_norm', 's5_lambda_re', 's5_lambda_im',
               's5_log_step', 's5_b_re', 's5_b_im', 's5_c_re', 's5_c_im', 's5_d', 's5_glu_w', 's5_glu_b', 'od_w_in', 'od_w_out',
               'c_q_norm', 'c_k_norm', 'c_lambda', 'c_out_norm', 'rel_bias', 'd_conv_w', 'd_a_log', 'd_dt_bias', 'd_out_norm',
               'moe_router', 'moe_w_gate', 'moe_w_up', 'moe_w_down']

PARAM_SHAPES = {
    'mix_norm': (4, 1024), 'ffn_norm': (4, 1024), 'ev_w_in': (2, 1024, 3072), 'ev_w_out': (2, 1024, 1024), 'a_lb_logits': (2, 2, 512),
    'a_out_norm': (2, 128), 's5_lambda_re': (2, 2, 32, 64), 's5_lambda_im': (2, 2, 32, 64), 's5_log_step': (2, 2, 32),
    's5_b_re': (2, 2, 32, 64, 16), 's5_b_im': (2, 2, 32, 64, 16), 's5_c_re': (2, 2, 32, 16, 64), 's5_c_im': (2, 2, 32, 16, 64),
    's5_d': (2, 512), 's5_glu_w': (2, 512, 512), 's5_glu_b': (2, 512), 'od_w_in': (2, 1024, 3600), 'od_w_out': (2, 1024, 1024),
    'c_q_norm': (2, 64), 'c_k_norm': (2, 64), 'c_lambda': (2, 4, 64), 'c_out_norm': (2, 128), 'rel_bias': (32, 4),
    'd_conv_w': (2, 5, 1536), 'd_a_log': (2, 2, 4), 'd_dt_bias': (2, 2, 4), 'd_out_norm': (2, 128), 'moe_router': (4, 1024, 16),
    'moe_w_gate': (4, 16, 1024, 2048), 'moe_w_up': (4, 16, 1024, 2048), 'moe_w_down': (4, 16, 2048, 1024)}


def build_program(layers=range(DEPTH), do_mixer=True, do_moe=True):
    nc = bass.Bass('TRN2', target_bir_lowering=False)
    xin = nc.dram_tensor("x", [L, D], F32, kind="ExternalInput").ap()
    P = {n: nc.dram_tensor(n, list(PARAM_SHAPES[n]), F32, kind="ExternalInput").ap() for n in PARAM_NAMES}
    onehot = nc.dram_tensor("t5_onehot", [32, 512], F32, kind="ExternalInput").ap()
    X = nc.dram_tensor("y", [L, D], F32, kind="ExternalOutput").ap()
    PT = nc.dram_tensor("PT", [2048, L], F32).ap()
    PV = nc.dram_tensor("PV", [L, 2048], BF16).ap()
    QK = nc.dram_tensor("QK", [16, 128, L], BF16).ap()
    QK5 = QK.rearrange("(h r w) p t -> h r w p t", h=4, r=2)
    OA = nc.dram_tensor("OA", [L, 512], F32).ap()
    YT = nc.dram_tensor("YT", [512, L], F32).ap()
    MT = nc.dram_tensor("MT", [1024, L], BF16).ap()
    HB = nc.dram_tensor("HB", [L, D], BF16).ap()
    GQ = nc.dram_tensor("GQ", [12, 128, L], BF16).ap()
    GR = nc.dram_tensor("GR", [2, 3, 4, L], F32).ap()
    OD2 = nc.dram_tensor("OD2", [L, 512], F32).ap()
    FV = nc.dram_tensor("FV", [4, 512], F32).ap()
    c = Ctx(nc); g = G()
    setup_consts(c, g)
    bX = Buf('X'); bPT = Buf(); bPV = Buf(); bQK = Buf(); bOAs = [Buf() for _ in range(4)]; bYT = Buf(); bMT = Buf()
    bHB = Buf(); bGQ = Buf(); bGR = Buf(); bFV = Buf(); bOD2s = [Buf() for _ in range(4)]
    for r in range(0, L, 512):
        c.dma('sp', lambda e: e.dma_start(out=X[r:r + 512, :], in_=xin[r:r + 512, :]), pwrites=[bX])
    bX.seal()
    for layer in layers:
        j = layer // 2
        if do_mixer:
            if layer % 2 == 0:
                spec = [(0, 512, 'F', 0), (512, 512, 'F', 512), (1024, 512, 'F', 1024), (2560, 512, 'F', 1536), (1536, 512, 'T', 0), (2048, 512, 'T', 512)]
                proj_phase(c, g, X, bX, P['mix_norm'][layer], P['ev_w_in'][j], 3072, spec, PT, bPT, PV, bPV)
                hgrn2_phase(c, g, PT, bPT, PV, bPV, P['a_lb_logits'], j, P['a_out_norm'][j], QK5, bQK, OA, bOAs, OD2, bOD2s, MT, bMT)
                s5_phase(c, g, PT, bPT, P['s5_lambda_re'][j], P['s5_lambda_im'][j], P['s5_log_step'][j], P['s5_b_re'][j], P['s5_b_im'][j],
                         P['s5_c_re'][j], P['s5_c_im'][j], P['s5_d'][j], P['s5_glu_w'][j], P['s5_glu_b'][j], YT, bYT, MT, bMT)
                bMT.seal()
                bX = outproj_phase(c, g, MT, bMT, P['ev_w_out'][j], X, bX)
            else:
                spec = [(0, 512, 'T', 0), (512, 512, 'T', 512), (1024, 512, 'T', 1024), (1536, 1536, 'F', 0), (3072, 16, 'F', 1536), (3088, 512, 'T', 1536)]
                proj_phase(c, g, X, bX, P['mix_norm'][layer], P['od_w_in'][j], 3600, spec, PT, bPT, PV, bPV)
                attn_phase(c, g, PV, bPV, P['c_q_norm'][j], P['c_k_norm'][j], P['c_lambda'][j], P['c_out_norm'][j], P['rel_bias'], onehot, layer,
                           QK, bQK, FV, bFV, MT, bMT)
                gdn_phase(c, g, PT, bPT, PV, bPV, P['d_conv_w'][j], P['d_a_log'][j], P['d_dt_bias'][j], P['d_out_norm'][j], GQ, bGQ, GR, bGR, OA, bOAs, OD2, bOD2s, MT, bMT)
                bMT.seal()
                bX = outproj_phase(c, g, MT, bMT, P['od_w_out'][j], X, bX)
        if do_moe:
            moe_layer(c, g, X, bX, HB, bHB, P['ffn_norm'][layer], P['moe_router'][layer], P['moe_w_gate'][layer], P['moe_w_up'][layer], P['moe_w_down'][layer])
    barrier(c)
    c.finish([bX])
    return nc, c


def kernel(**inputs):
    x = np.ascontiguousarray(np.asarray(inputs['x'], dtype=np.float32))
    B = x.shape[0]
    assert B == N_ACTIVE and x.shape[1] == L and x.shape[2] == D
    nc, c = build_program()
    params = {n: np.ascontiguousarray(np.asarray(inputs[n], dtype=np.float32)) for n in PARAM_NAMES}
    oh = t5_onehot()
    in_maps = []
    for b in range(N_ACTIVE):
        m = {"x": x[b], "t5_onehot": oh}
        m.update(params)
        in_maps.append(m)
    res = run_bass_kernel_spmd(nc, in_maps, core_ids=list(range(N_ACTIVE)))
    out = np.stack([np.asarray(res.results[b]["y"], dtype=np.float32) for b in range(N_ACTIVE)], axis=0)
    return out
```

```python
import math
from contextlib import ExitStack

import numpy as np
import concourse.bass as bass
import concourse.mybir as mybir
from concourse.bass_utils import run_bass_kernel_spmd

F32 = mybir.dt.float32
BF16 = mybir.dt.bfloat16
U32 = mybir.dt.uint32
I32 = mybir.dt.int32
AF = mybir.ActivationFunctionType
ALU = mybir.AluOpType
AX = mybir.AxisListType


class Buf:
    __slots__ = ("name", "w", "r", "pw", "psum")

    def __init__(self, name="", psum=False):
        self.name = name
        self.psum = psum
        self.w = {}
        self.r = {}
        self.pw = {}

    def seal(self):
        for k, v in self.pw.items():
            if self.w.get(k, 0) < v:
                self.w[k] = v
        self.pw = {}


class Ctx:
    NDMA = 8

    def __init__(self, nc, same_engine_sync=True):
        self.nc = nc
        self.E = dict(pe=nc.tensor, dve=nc.vector, act=nc.scalar, pool=nc.gpsimd, sp=nc.sync)
        self.sem = {}
        self.cnt = {}
        for k in self.E:
            self.sem[k] = nc.alloc_semaphore("c_" + k)
            self.cnt[k] = 0
        self.dslot = {}
        for q in ("sp", "act", "pool"):
            for i in range(self.NDMA):
                key = "d_%s%d" % (q, i)
                self.sem[key] = nc.alloc_semaphore(key)
                self.cnt[key] = 0
            self.dslot[q] = 0
        self.seen = {k: {} for k in self.E}
        self.same = same_engine_sync
        self.ninst = 0

    def _wait(self, eng, tok):
        if tok is None:
            return
        key, val = tok
        if key == eng and (eng == "pe" or not self.same):
            return
        if self.seen[eng].get(key, 0) >= val:
            return
        self.E[eng].wait_ge(self.sem[key], val)
        self.seen[eng][key] = val

    def _deps(self, eng, reads, writes, pwrites=()):
        for b in reads:
            for k, v in b.w.items():
                self._wait(eng, (k, v))
            for k, v in b.pw.items():
                self._wait(eng, (k, v))
            if b.psum:
                for k, v in b.r.items():
                    if k != eng:
                        self._wait(eng, (k, v))
        for b in writes:
            for d in (b.w, b.pw, b.r):
                for k, v in d.items():
                    self._wait(eng, (k, v))
        for b in pwrites:
            for d in (b.w, b.r):
                for k, v in d.items():
                    self._wait(eng, (k, v))

    def _commit(self, tok, reads, writes, pwrites=()):
        k, v = tok
        for b in writes:
            b.w = {k: v}
            b.pw = {}
            b.r = {}
        for b in pwrites:
            if b.pw.get(k, 0) < v:
                b.pw[k] = v
        for b in reads:
            if b.r.get(k, 0) < v:
                b.r[k] = v

    def op(self, eng, fn, reads=(), writes=(), pwrites=()):
        self._deps(eng, reads, writes, pwrites)
        inst = fn(self.E[eng])
        self.cnt[eng] += 1
        tok = (eng, self.cnt[eng])
        inst.then_inc(self.sem[eng], 1)
        self._commit(tok, reads, writes, pwrites)
        self.ninst += 1
        return tok

    def dma(self, q, fn, reads=(), writes=(), pwrites=()):
        self._deps(q, reads, writes, pwrites)
        i = self.dslot[q]
        self.dslot[q] = (i + 1) % self.NDMA
        key = "d_%s%d" % (q, i)
        if self.cnt[key] > 0:
            self._wait(q, (key, self.cnt[key]))
        inst = fn(self.E[q])
        self.cnt[key] += 16
        tok = (key, self.cnt[key])
        inst.then_inc(self.sem[key], 16)
        self._commit(tok, reads, writes, pwrites)
        self.ninst += 1
        return tok

    def finish(self, bufs):
        for b in bufs:
            for d in (b.w, b.pw):
                for k, v in d.items():
                    self._wait("sp", (k, v))


def barrier(c):
    toks = [(k, v) for k, v in c.cnt.items() if v > 0]
    for eng in c.E:
        for tok in toks:
            c._wait(eng, tok)


_UNIQ = [0]


def uniq(name):
    _UNIQ[0] += 1
    return "%s_u%d" % (name, _UNIQ[0])


L = 8192
D = 1024
NE = 16
FF = 2048
CAP = 1024
NT = L // 128


class G:
    pass


def alloc_T(nc, name, shape, dtype, n=1, es=None):
    if es is None:
        return [(nc.alloc_sbuf_tensor("%s_%d" % (name, i), shape, dtype), Buf(name)) for i in range(n)]
    return [(es.enter_context(nc.sbuf_tensor(uniq("%s_%d" % (name, i)), shape, dtype)), Buf(name)) for i in range(n)]


def setup_consts(c, g):
    nc = c.nc
    g.ident32 = nc.alloc_sbuf_tensor("ident32", [128, 128], F32); g.b_ident32 = Buf()
    g.identb = nc.alloc_sbuf_tensor("identb", [128, 128], BF16); g.b_identb = Buf()
    g.ones32 = nc.alloc_sbuf_tensor("ones32", [1, 128], F32); g.b_ones32 = Buf()
    g.neghalf = nc.alloc_sbuf_tensor("neghalf", [128, 1], F32); g.b_neghalf = Buf()
    for t, b in ((g.ident32, g.b_ident32), (g.identb, g.b_identb)):
        c.op('pool', lambda e: e.memset(t[:], 0.0), writes=[b])
        c.op('pool', lambda e: e.affine_select(out=t[:], in_=t[:], pattern=[[-1, 128]], compare_op=ALU.not_equal,
                                               fill=1.0, base=0, channel_multiplier=1), reads=[b], writes=[b])
    g.J32 = nc.alloc_sbuf_tensor("J32", [128, 128], F32); g.b_J32 = Buf()
    g.Jb = nc.alloc_sbuf_tensor("Jb", [128, 128], BF16); g.b_Jb = Buf()
    for t, b in ((g.J32, g.b_J32), (g.Jb, g.b_Jb)):
        c.op('pool', lambda e: e.memset(t[:], 0.0), writes=[b])
        c.op('pool', lambda e: e.affine_select(out=t[:], in_=t[:], pattern=[[1, 128]], compare_op=ALU.not_equal,
                                               fill=1.0, base=-127, channel_multiplier=1), reads=[b], writes=[b])
    c.op('pool', lambda e: e.memset(g.ones32[:], 1.0), writes=[g.b_ones32])
    c.op('pool', lambda e: e.memset(g.neghalf[:], -0.5), writes=[g.b_neghalf])
    g.ps = []
    for i in range(7):
        g.ps.append((nc.alloc_psum_tensor("ps%d" % i, [128, 512], F32), Buf("ps%d" % i, psum=True)))
    g.psb = (nc.alloc_psum_tensor("psb", [128, 1024], BF16), Buf("psb", psum=True))


def bcast_row(c, g, dst, bdst, src_ap, n, tmp, btmp, psi=6):
    c.dma('sp', lambda e: e.dma_start(out=tmp[0:1, 0:n], in_=src_ap.rearrange("(o n) -> o n", o=1)), writes=[btmp])
    ps, bps = g.ps[psi]
    for h in range(0, n, 512):
        w = min(512, n - h)
        c.op('pe', lambda e: e.matmul(ps[:, 0:w], lhsT=g.ones32[0:1, :], rhs=tmp[0:1, h:h + w], start=True, stop=True),
             reads=[g.b_ones32, btmp], writes=[bps])
        c.op('dve', lambda e: e.tensor_copy(out=dst[:, h:h + w], in_=ps[:, 0:w]), reads=[bps], writes=[bdst])


def rmsnorm_tile(c, g, xt, bxt, gB, bgB, h32, bh32, junk, bjunk, ss, bss):
    c.op('dve', lambda e: e.scalar_tensor_tensor(out=junk[:], in0=xt, scalar=1.0, in1=xt, op0=ALU.mult, op1=ALU.mult,
                                                 accum_out=ss[:, 0:1]), reads=[bxt], writes=[bjunk, bss])
    c.op('dve', lambda e: e.tensor_scalar(out=ss[:, 0:1], in0=ss[:, 0:1], scalar1=1.0 / D, scalar2=1e-6, op0=ALU.mult, op1=ALU.add),
         reads=[bss], writes=[bss])
    c.op('act', lambda e: e.activation(out=ss[:, 0:1], in_=ss[:, 0:1], func=AF.Ln), reads=[bss], writes=[bss])
    c.op('act', lambda e: e.activation(out=ss[:, 0:1], in_=ss[:, 0:1], func=AF.Exp, scale=-0.5), reads=[bss], writes=[bss])
    c.op('dve', lambda e: e.scalar_tensor_tensor(out=h32[:], in0=xt, scalar=ss[:, 0:1], in1=gB[:], op0=ALU.mult, op1=ALU.mult),
         reads=[bxt, bss, bgB], writes=[bh32])


def moe_prep(c, g, sb, ffn_g, w_router):
    nc = c.nc
    gB, bgB = sb['gB']
    tmpr, btmpr = sb['tmprow']
    bcast_row(c, g, gB, bgB, ffn_g, D, tmpr, btmpr)
    wr, bwr = sb['wr']
    c.dma('sp', lambda e: e.dma_start(out=wr[:], in_=w_router.rearrange("(k p) e -> p k e", p=128)), writes=[bwr])


def moe_step1_tile(c, g, sb, i, xt, bxt, HB, bHB):
    affT, baffT = sb['affT']
    gB, bgB = sb['gB']
    wr, bwr = sb['wr']
    h32, bh32 = sb['h32'][i % 2]
    hb, bhb = sb['hb'][i % 2]
    junk, bjunk = sb['junk']
    ss, bss = sb['ss'][i % 2]
    rmsnorm_tile(c, g, xt, bxt, gB, bgB, h32, bh32, junk, bjunk, ss, bss)
    c.op('act', lambda e: e.activation(out=hb[:], in_=h32[:], func=AF.Copy), reads=[bh32], writes=[bhb])
    c.dma('act', lambda e: e.dma_start(out=HB[i * 128:(i + 1) * 128, :], in_=hb[:]), reads=[bhb], pwrites=[bHB])
    hT, bhT = sb['hT32'][i % 2]
    for hh in range(2):
        ps, bps = g.ps[4 + hh]
        for k in range(4):
            kk = hh * 4 + k
            c.op('pe', lambda e: e.transpose(out=ps[:, k * 128:(k + 1) * 128], in_=h32[:, kk * 128:(kk + 1) * 128],
                                             identity=g.ident32[:]), reads=[bh32, g.b_ident32], writes=[bps])
        c.op('act', lambda e: e.activation(out=hT[:, hh * 512:(hh + 1) * 512], in_=ps[:, :], func=AF.Copy),
             reads=[bps], writes=[bhT])
    pl, bpl = g.ps[6]
    for k in range(8):
        c.op('pe', lambda e: e.matmul(pl[:, 0:NE], lhsT=hT[:, k * 128:(k + 1) * 128], rhs=wr[:, k, :], start=(k == 0), stop=(k == 7)),
             reads=[bhT, bwr], writes=[bpl])
    sm, bsm = sb['sm'][i % 2]
    ex, bex = sb['ex'][i % 2]
    c.op('dve', lambda e: e.tensor_reduce(out=sm[:, 0:1], in_=pl[:, 0:NE], axis=AX.X, op=ALU.max), reads=[bpl], writes=[bsm])
    c.op('dve', lambda e: e.tensor_scalar(out=sm[:, 0:1], in0=sm[:, 0:1], scalar1=-1.0, scalar2=None, op0=ALU.mult), reads=[bsm], writes=[bsm])
    c.op('act', lambda e: e.activation(out=ex[:], in_=pl[:, 0:NE], func=AF.Exp, bias=sm[:, 0:1], scale=1.0, accum_out=sm[:, 1:2]),
         reads=[bpl, bsm], writes=[bex, bsm])
    c.op('dve', lambda e: e.reciprocal(out=sm[:, 2:3], in_=sm[:, 1:2]), reads=[bsm], writes=[bsm])
    c.op('dve', lambda e: e.tensor_scalar(out=ex[:], in0=ex[:], scalar1=sm[:, 2:3], scalar2=None, op0=ALU.mult), reads=[bex, bsm], writes=[bex])
    pt, bpt = g.ps[6]
    c.op('pe', lambda e: e.transpose(out=pt[0:NE, 0:128], in_=ex[:, 0:NE], identity=g.ident32[:]), reads=[bex, g.b_ident32], writes=[bpt])
    c.op('act', lambda e: e.activation(out=affT[0:NE, i * 128:(i + 1) * 128], in_=pt[0:NE, 0:128], func=AF.Copy), reads=[bpt], writes=[baffT])


def moe_rest(c, g, sb, X, bX, HB, bHB, w_gate, w_up, w_down, stage=3):
    nc = c.nc
    affT, baffT = sb['affT']
    bHB.seal()
    if stage < 2:
        return
    vals, bvals = sb['vals']
    idxu, bidxu = sb['idxu']
    for it in range(CAP // 8):
        sl = slice(it * 8, it * 8 + 8)
        c.op('dve', lambda e: e.max(out=vals[:, sl], in_=affT[:, :]), reads=[baffT], writes=[bvals])
        c.op('dve', lambda e: e.max_index(out=idxu[:, sl], in_max=vals[:, sl], in_values=affT[:, :]), reads=[baffT, bvals], writes=[bidxu])
        c.op('dve', lambda e: e.match_replace(out=affT[:, :], in_to_replace=vals[:, sl], in_values=affT[:, :], imm_value=-1.0),
             reads=[bvals, baffT], writes=[baffT])
    idxf, bidxf = sb['idxf']
    c.op('dve', lambda e: e.tensor_copy(out=idxf[:], in_=idxu[:]), reads=[bidxu], writes=[bidxf])
    idxT, bidxT = sb['idxT']
    gateT, bgateT = sb['gateT']
    for j in range(8):
        pt, bpt = g.ps[4 + (j % 2)]
        c.op('pe', lambda e: e.transpose(out=pt[:, 0:NE], in_=idxf[0:NE, j * 128:(j + 1) * 128], identity=g.ident32[0:NE, 0:NE]),
             reads=[bidxf, g.b_ident32], writes=[bpt])
        c.op('dve', lambda e: e.tensor_copy(out=idxT[:, j, :], in_=pt[:, 0:NE]), reads=[bpt], writes=[bidxT])
        pt2, bpt2 = g.ps[2 + (j % 2)]
        c.op('pe', lambda e: e.transpose(out=pt2[:, 0:NE], in_=vals[0:NE, j * 128:(j + 1) * 128], identity=g.ident32[0:NE, 0:NE]),
             reads=[bvals, g.b_ident32], writes=[bpt2])
        c.op('act', lambda e: e.activation(out=gateT[:, j, :], in_=pt2[:, 0:NE], func=AF.Copy), reads=[bpt2], writes=[bgateT])
    if stage < 3:
        return
    xsT, bxsT = sb['xsT']
    hidT, bhidT = sb['hidT']
    yacc, byacc = sb['yacc']
    ptb, bptb = g.psb
    qi = 0
    for ex_i in range(NE):
        for j in range(8):
            xs, bxs = sb['xs'][j % 2]
            c.dma('pool', lambda e: e.indirect_dma_start(out=xs[:, :], out_offset=None, in_=HB[:, :],
                                                        in_offset=bass.IndirectOffsetOnAxis(ap=idxT[:, j, ex_i:ex_i + 1], axis=0)),
                  reads=[bHB, bidxT], writes=[bxs])
            for k in range(8):
                c.op('pe', lambda e: e.transpose(out=ptb[:, k * 128:(k + 1) * 128], in_=xs[:, k * 128:(k + 1) * 128], identity=g.identb[:]),
                     reads=[bxs, g.b_identb], writes=[bptb])
            c.op('dve', lambda e: e.tensor_copy(out=xsT[:, :, j * 128:(j + 1) * 128], in_=ptb[:, :].rearrange("p (k s) -> p k s", k=8)),
                 reads=[bptb], writes=[bxsT])
        for q in range(4):
            wg, bwg = sb['wg'][qi % 2]
            wu, bwu = sb['wu'][qi % 2]
            wd, bwd = sb['wd'][qi % 2]
            qi += 1
            f0 = q * 512
            c.dma('pool', lambda e: e.dma_start(out=wg[:], in_=w_gate[ex_i, :, f0:f0 + 512].rearrange("(k p) f -> p k f", p=128)), writes=[bwg])
            c.dma('pool', lambda e: e.dma_start(out=wu[:], in_=w_up[ex_i, :, f0:f0 + 512].rearrange("(k p) f -> p k f", p=128)), writes=[bwu])
            c.dma('pool', lambda e: e.dma_start(out=wd[:], in_=w_down[ex_i, f0:f0 + 512, :].rearrange("(k p) d -> p k d", p=128)), writes=[bwd])
            n = 0
            for fc in range(4):
                for sh in range(2):
                    pg, bpg = g.ps[0 + (n % 2)]
                    pu, bpu = g.ps[2 + (n % 2)]
                    sg, bsg = sb['sg'][n % 2]
                    n += 1
                    for k in range(8):
                        c.op('pe', lambda e: e.matmul(pg[:, :], lhsT=wg[:, k, fc * 128:(fc + 1) * 128], rhs=xsT[:, k, sh * 512:(sh + 1) * 512],
                                                      start=(k == 0), stop=(k == 7)), reads=[bwg, bxsT], writes=[bpg])
                    for k in range(8):
                        c.op('pe', lambda e: e.matmul(pu[:, :], lhsT=wu[:, k, fc * 128:(fc + 1) * 128], rhs=xsT[:, k, sh * 512:(sh + 1) * 512],
                                                      start=(k == 0), stop=(k == 7)), reads=[bwu, bxsT], writes=[bpu])
                    c.op('act', lambda e: e.activation(out=sg[:], in_=pg[:, :], func=AF.Silu), reads=[bpg], writes=[bsg])
                    c.op('dve', lambda e: e.tensor_tensor(out=hidT[:, fc, sh * 512:(sh + 1) * 512], in0=sg[:], in1=pu[:, :], op=ALU.mult),
                         reads=[bsg, bpu], writes=[bhidT])
            m = 0
            for j in range(8):
                for dh in range(2):
                    py, bpy = g.ps[4 + (m % 2)]
                    m += 1
                    for fc in range(4):
                        c.op('pe', lambda e: e.matmul(py[:, :], lhsT=hidT[:, fc, j * 128:(j + 1) * 128], rhs=wd[:, fc, dh * 512:(dh + 1) * 512],
                                                      start=(fc == 0), stop=(fc == 3)), reads=[bhidT, bwd], writes=[bpy])
                    ysl = yacc[:, j, dh * 512:(dh + 1) * 512]
                    gsc = gateT[:, j, ex_i:ex_i + 1]
                    if q == 0:
                        c.op('dve', lambda e: e.tensor_scalar(out=ysl, in0=py[:, :], scalar1=gsc, scalar2=None, op0=ALU.mult),
                             reads=[bpy, bgateT], writes=[byacc])
                    else:
                        c.op('dve', lambda e: e.scalar_tensor_tensor(out=ysl, in0=py[:, :], scalar=gsc, in1=ysl, op0=ALU.mult, op1=ALU.add),
                             reads=[bpy, bgateT, byacc], writes=[byacc])
        for j in range(8):
            c.dma('pool', lambda e: e.indirect_dma_start(out=X[:, :], out_offset=bass.IndirectOffsetOnAxis(ap=idxT[:, j, ex_i:ex_i + 1], axis=0),
                                                        in_=yacc[:, j, :], in_offset=None, compute_op=ALU.add),
                  reads=[byacc, bidxT], pwrites=[bX])
        bX.seal()


def moe_phase(c, g, X, bX, HB, bHB, ffn_g, w_router, w_gate, w_up, w_down, sb, stage=3):
    moe_prep(c, g, sb, ffn_g, w_router)
    for i in range(NT):
        xt, bxt = sb['xt'][i % 2]
        c.dma('sp', lambda e: e.dma_start(out=xt[:], in_=X[i * 128:(i + 1) * 128, :]), reads=[bX], writes=[bxt])
        moe_step1_tile(c, g, sb, i, xt[:], bxt, HB, bHB)
    moe_rest(c, g, sb, X, bX, HB, bHB, w_gate, w_up, w_down, stage=stage)


def moe_alloc(nc, es=None, part=0, sb=None):
    sb = {} if sb is None else sb
    if part in (0, 1):
        sb['gB'] = alloc_T(nc, 'gB', [128, D], F32, 1, es=es)[0]
        sb['tmprow'] = alloc_T(nc, 'tmprow', [1, 1024], F32, 1, es=es)[0]
        sb['wr'] = alloc_T(nc, 'wr', [128, 8, NE], F32, 1, es=es)[0]
        big = es.enter_context(nc.sbuf_tensor(uniq('big'), [128, L], F32)) if es is not None else nc.alloc_sbuf_tensor('big', [128, L], F32); bbig = Buf('big')
        sb['affT'] = (big[0:NE, :], bbig)
        sb['yacc'] = (big[:, :].rearrange('p (j d) -> p j d', j=8), bbig)
        if part == 0:
            sb['xt'] = alloc_T(nc, 'xt', [128, D], F32, 2, es=es)
        sb['h32'] = alloc_T(nc, 'h32', [128, D], F32, 2, es=es)
        sb['hb'] = alloc_T(nc, 'hb', [128, D], BF16, 2, es=es)
        sb['junk'] = alloc_T(nc, 'junk', [128, D], F32, 1, es=es)[0]
        sb['ss'] = alloc_T(nc, 'ss', [128, 4], F32, 2, es=es)
        sb['hT32'] = alloc_T(nc, 'hT32', [128, D], F32, 2, es=es)
        sb['sm'] = alloc_T(nc, 'sm', [128, 4], F32, 2, es=es)
        sb['ex'] = alloc_T(nc, 'ex', [128, NE], F32, 2, es=es)
    if part in (0, 3):
        sb['vals'] = alloc_T(nc, 'vals', [NE, CAP], F32, 1, es=es)[0]
        sb['idxu'] = alloc_T(nc, 'idxu', [NE, CAP], U32, 1, es=es)[0]
        sb['idxf'] = alloc_T(nc, 'idxf', [NE, CAP], F32, 1, es=es)[0]
        sb['idxT'] = alloc_T(nc, 'idxT', [128, 8, NE], U32, 1, es=es)[0]
        sb['gateT'] = alloc_T(nc, 'gateT', [128, 8, NE], F32, 1, es=es)[0]
        sb['xsT'] = alloc_T(nc, 'xsT', [128, 8, CAP], BF16, 1, es=es)[0]
        sb['hidT'] = alloc_T(nc, 'hidT', [128, 4, CAP], BF16, 1, es=es)[0]
        sb['xs'] = alloc_T(nc, 'xs', [128, D], BF16, 2, es=es)
        sb['wg'] = alloc_T(nc, 'wg', [128, 8, 512], BF16, 2, es=es)
        sb['wu'] = alloc_T(nc, 'wu', [128, 8, 512], BF16, 2, es=es)
        sb['wd'] = alloc_T(nc, 'wd', [128, 4, D], BF16, 2, es=es)
        sb['sg'] = alloc_T(nc, 'sg', [128, 512], BF16, 2, es=es)
    return sb


def moe_layer(c, g, X, bX, HB, bHB, ffn_g, w_router, w_gate, w_up, w_down):
    with ExitStack() as es:
        sb = moe_alloc(c.nc, es)
        moe_phase(c, g, X, bX, HB, bHB, ffn_g, w_router, w_gate, w_up, w_down, sb)
        barrier(c)


def proj_phase(c, g, X, bX, gain_ap, w_ap, nout, spec, PT, bPT, PV, bPV):
    nc = c.nc
    with ExitStack() as es:
        def T(name, shape, dt):
            return es.enter_context(nc.sbuf_tensor(uniq(name), shape, dt))
        wsb = T("pj_w", [128, 8, nout], BF16); bw = Buf()
        gB = T("pj_gB", [128, D], F32); bgB = Buf()
        tmpr = T("pj_tmpr", [1, D], F32); btmpr = Buf()
        xt = [T("pj_xt%d" % i, [128, 4, D], F32) for i in range(2)]; bxt = [Buf(), Buf()]
        junk = T("pj_junk", [128, D], F32); bjunk = Buf()
        ss = [T("pj_ss%d" % i, [128, 4], F32) for i in range(2)]; bss = [Buf(), Buf()]
        hb = [T("pj_hb%d" % i, [128, D], BF16) for i in range(2)]; bhb = [Buf(), Buf()]
        hT = [T("pj_hT%d" % i, [128, 8, 512], BF16) for i in range(2)]; bhT = [Buf(), Buf()]
        stF = [T("pj_stF%d" % i, [128, 512], F32) for i in range(3)]; bstF = [Buf() for _ in range(3)]
        stT = [T("pj_stT%d" % i, [128, 512], BF16) for i in range(3)]; bstT = [Buf() for _ in range(3)]
        bcast_row(c, g, gB, bgB, gain_ap, D, tmpr, btmpr)
        for c0 in range(0, nout, 512):
            wd = min(512, nout - c0)
            c.dma('pool', lambda e: e.dma_start(out=wsb[:, :, c0:c0 + wd], in_=w_ap[:, c0:c0 + wd].rearrange("(k p) f -> p k f", p=128)), writes=[bw])
        ptb, bptb = g.psb
        nF = 0; nT = 0; npz = 0
        for it in range(L // 512):
            t0 = it * 512
            x_, bx_ = xt[it % 2], bxt[it % 2]
            c.dma('sp', lambda e: e.dma_start(out=x_[:], in_=X[t0:t0 + 512, :].rearrange("(j p) d -> p j d", p=128)), reads=[bX], writes=[bx_])
            hT_, bhT_ = hT[it % 2], bhT[it % 2]
            for j in range(4):
                s_, bs_ = ss[j % 2], bss[j % 2]
                h_, bh_ = hb[j % 2], bhb[j % 2]
                c.op('dve', lambda e: e.scalar_tensor_tensor(out=junk[:], in0=x_[:, j, :], scalar=1.0, in1=x_[:, j, :], op0=ALU.mult, op1=ALU.mult,
                                                             accum_out=s_[:, 0:1]), reads=[bx_], writes=[bjunk, bs_])
                c.op('dve', lambda e: e.tensor_scalar(out=s_[:, 0:1], in0=s_[:, 0:1], scalar1=1.0 / D, scalar2=1e-6, op0=ALU.mult, op1=ALU.add),
                     reads=[bs_], writes=[bs_])
                c.op('act', lambda e: e.activation(out=s_[:, 0:1], in_=s_[:, 0:1], func=AF.Ln), reads=[bs_], writes=[bs_])
                c.op('act', lambda e: e.activation(out=s_[:, 0:1], in_=s_[:, 0:1], func=AF.Exp, scale=-0.5), reads=[bs_], writes=[bs_])
                c.op('dve', lambda e: e.scalar_tensor_tensor(out=h_[:], in0=x_[:, j, :], scalar=s_[:, 0:1], in1=gB[:], op0=ALU.mult, op1=ALU.mult),
                     reads=[bx_, bs_, bgB], writes=[bh_])
                for k in range(8):
                    c.op('pe', lambda e: e.transpose(out=ptb[:, k * 128:(k + 1) * 128], in_=h_[:, k * 128:(k + 1) * 128], identity=g.identb[:]),
                         reads=[bh_, g.b_identb], writes=[bptb])
                c.op('act', lambda e: e.activation(out=hT_[:, :, j * 128:(j + 1) * 128], in_=ptb[:, :].rearrange("p (k s) -> p k s", k=8), func=AF.Copy),
                     reads=[bptb], writes=[bhT_])
            for (col0, ncols, mode, dst0) in spec:
                if mode == 'F':
                    for f0 in range(0, ncols, 128):
                        fw = min(128, ncols - f0)
                        ps, bps = g.ps[npz % 4]; npz += 1
                        for k in range(8):
                            c.op('pe', lambda e: e.matmul(ps[0:fw, :], lhsT=wsb[:, k, col0 + f0:col0 + f0 + fw], rhs=hT_[:, k, :], start=(k == 0), stop=(k == 7)),
                                 reads=[bw, bhT_], writes=[bps])
                        st, bst = stF[nF % 3], bstF[nF % 3]; nF += 1
                        eng = 'act' if nF % 2 else 'dve'
                        if eng == 'act':
                            c.op('act', lambda e: e.activation(out=st[0:fw, :], in_=ps[0:fw, :], func=AF.Copy), reads=[bps], writes=[bst])
                        else:
                            c.op('dve', lambda e: e.tensor_copy(out=st[0:fw, :], in_=ps[0:fw, :]), reads=[bps], writes=[bst])
                        c.dma('sp' if nF % 2 else 'act', lambda e: e.dma_start(out=PT[dst0 + f0:dst0 + f0 + fw, t0:t0 + 512], in_=st[0:fw, :]), reads=[bst], pwrites=[bPT])
                else:
                    for j in range(4):
                        for c0 in range(0, ncols, 512):
                            cw = min(512, ncols - c0)
                            ps, bps = g.ps[npz % 4]; npz += 1
                            for k in range(8):
                                c.op('pe', lambda e: e.matmul(ps[:, 0:cw], lhsT=hT_[:, k, j * 128:(j + 1) * 128], rhs=wsb[:, k, col0 + c0:col0 + c0 + cw], start=(k == 0), stop=(k == 7)),
                                     reads=[bw, bhT_], writes=[bps])
                            st, bst = stT[nT % 3], bstT[nT % 3]; nT += 1
                            eng = 'act' if nT % 2 else 'dve'
                            if eng == 'act':
                                c.op('act', lambda e: e.activation(out=st[:, 0:cw], in_=ps[:, 0:cw], func=AF.Copy), reads=[bps], writes=[bst])
                            else:
                                c.op('dve', lambda e: e.tensor_copy(out=st[:, 0:cw], in_=ps[:, 0:cw]), reads=[bps], writes=[bst])
                            c.dma('sp' if nT % 2 else 'act', lambda e: e.dma_start(out=PV[t0 + j * 128:t0 + (j + 1) * 128, dst0 + c0:dst0 + c0 + cw], in_=st[:, 0:cw]),
                                  reads=[bst], pwrites=[bPV])
        bPT.seal(); bPV.seal()
        barrier(c)


def gated_norm_finalize(c, g, OA, bOAs, PV, bPV, gcol0, gain_ap, MT, bMT, row0, pfx, OA2=None, bOA2s=()):
    nc = c.nc
    with ExitStack() as es:
        def T(name, shape, dt):
            return es.enter_context(nc.sbuf_tensor(uniq(pfx + name), shape, dt))
        gA = T("gA", [128, 128], F32); bgA = Buf()
        oa = [T("oa%d" % i, [128, 512], F32) for i in range(2)]; boa = [Buf(), Buf()]
        oa2 = [T("oa2%d" % i, [128, 512], F32) for i in range(2)]; boa2 = [Buf(), Buf()]
        ga = [T("ga%d" % i, [128, 512], BF16) for i in range(2)]; bga = [Buf(), Buf()]
        sq = T("sq", [128, 512], F32); bsq = Buf()
        ssq = [T("ssq%d" % i, [128, 4], F32) for i in range(2)]; bssq = [Buf(), Buf()]
        sg = T("sg", [128, 512], F32); bsg = Buf()
        t1 = T("t1", [128, 512], F32); bt1 = Buf()
        ob = [T("ob%d" % i, [128, 512], BF16) for i in range(2)]; bob = [Buf(), Buf()]
        mt = [T("mt%d" % i, [128, 4, 512], BF16) for i in range(2)]; bmt = [Buf(), Buf()]
        c.dma('sp', lambda e: e.dma_start(out=gA[:], in_=gain_ap.partition_broadcast(128)), writes=[bgA])
        ptb, bptb = g.psb
        for i in range(NT):
            oa_, boa_ = oa[i % 2], boa[i % 2]
            ga_, bga_ = ga[i % 2], bga[i % 2]
            ss_, bss_ = ssq[i % 2], bssq[i % 2]
            ob_, bob_ = ob[i % 2], bob[i % 2]
            mt_, bmt_ = mt[(i // 4) % 2], bmt[(i // 4) % 2]
            c.dma('sp', lambda e: e.dma_start(out=oa_[:], in_=OA[i * 128:(i + 1) * 128, :]), reads=bOAs, writes=[boa_])
            c.dma('act', lambda e: e.dma_start(out=ga_[:], in_=PV[i * 128:(i + 1) * 128, gcol0:gcol0 + 512]), reads=[bPV], writes=[bga_])
            if OA2 is not None:
                o2_, bo2_ = oa2[i % 2], boa2[i % 2]
                c.dma('act', lambda e: e.dma_start(out=o2_[:], in_=OA2[i * 128:(i + 1) * 128, :]), reads=list(bOA2s), writes=[bo2_])
                c.op('dve', lambda e: e.tensor_tensor(out=oa_[:], in0=oa_[:], in1=o2_[:], op=ALU.add), reads=[boa_, bo2_], writes=[boa_])
            c.op('act', lambda e: e.activation(out=sq[:], in_=oa_[:], func=AF.Square), reads=[boa_], writes=[bsq])
            c.op('dve', lambda e: e.tensor_reduce(out=ss_[:], in_=sq[:].rearrange("p (h d) -> p h d", h=4), axis=AX.X, op=ALU.add), reads=[bsq], writes=[bss_])
            c.op('dve', lambda e: e.tensor_scalar(out=ss_[:], in0=ss_[:], scalar1=1.0 / 128, scalar2=1e-6, op0=ALU.mult, op1=ALU.add), reads=[bss_], writes=[bss_])
            c.op('pool', lambda e: e.tensor_tensor(out=ss_[:], in0=ss_[:], in1=g.neghalf[:, 0:1].broadcast_to([128, 4]), op=ALU.pow), reads=[bss_, g.b_neghalf], writes=[bss_])
            c.op('act', lambda e: e.activation(out=sg[:], in_=ga_[:], func=AF.Silu), reads=[bga_], writes=[bsg])
            c.op('dve', lambda e: e.tensor_tensor(out=t1[:].rearrange("p (h d) -> p h d", h=4), in0=oa_[:].rearrange("p (h d) -> p h d", h=4),
                                                  in1=ss_[:].unsqueeze(2).broadcast_to([128, 4, 128]), op=ALU.mult), reads=[boa_, bss_], writes=[bt1])
            c.op('dve', lambda e: e.tensor_tensor(out=t1[:].rearrange("p (h d) -> p h d", h=4), in0=t1[:].rearrange("p (h d) -> p h d", h=4),
                                                   in1=gA[:].unsqueeze(1).broadcast_to([128, 4, 128]), op=ALU.mult), reads=[bt1, bgA], writes=[bt1])
            c.op('dve', lambda e: e.tensor_tensor(out=ob_[:], in0=t1[:], in1=sg[:], op=ALU.mult), reads=[bt1, bsg], writes=[bob_])
            for k in range(4):
                c.op('pe', lambda e: e.transpose(out=ptb[:, k * 128:(k + 1) * 128], in_=ob_[:, k * 128:(k + 1) * 128], identity=g.identb[:]),
                     reads=[bob_, g.b_identb], writes=[bptb])
            c.op('act', lambda e: e.activation(out=mt_[:, :, (i % 4) * 128:(i % 4 + 1) * 128], in_=ptb[:, 0:512].rearrange("p (k s) -> p k s", k=4), func=AF.Copy),
                 reads=[bptb], writes=[bmt_])
            if i % 4 == 3:
                t0 = (i // 4) * 512
                c.dma('sp', lambda e: e.dma_start(out=MT[row0:row0 + 512, t0:t0 + 512].rearrange("(k p) t -> p k t", p=128), in_=mt_[:]), reads=[bmt_], pwrites=[bMT])
        barrier(c)


TBK = 2048
NTB = L // TBK


def hgrn2_phase(c, g, PT, bPT, PV, bPV, lb_logits, jl, a_out_norm, QK, bQK, OA, bOAs, OA2, bOA2s, MT, bMT, stage=3):
    nc = c.nc
    with ExitStack() as es0:
        def T0(name, shape, dt):
            return es0.enter_context(nc.sbuf_tensor(uniq(name), shape, dt))
        mcols = T0("hg_mcols", [128, 8, 128], F32); bmcols = Buf()
        with ExitStack() as es:
            def T(name, shape, dt):
                return es.enter_context(nc.sbuf_tensor(uniq(name), shape, dt))
            lbt = T("hg_lbt", [128, 2, 2, 4], F32); blbt = Buf()
            lbc = T("hg_lbc", [128, 8], F32); blbc = Buf()
            oml = T("hg_oml", [128, 8], F32); boml = Buf()
            noml = T("hg_noml", [128, 8], F32); bnoml = Buf()
            msk = T("hg_msk", [128, TBK], F32); bmsk = Buf()
            bmid = T("hg_bmid", [128, 128], F32); bbmid = Buf()
            blast = T("hg_blast", [128, 128], F32); bblast = Buf()
            zq = [T("hg_zq%d" % i, [128, TBK], F32) for i in range(2)]; bzq = [Buf(), Buf()]
            zf = [T("hg_zf%d" % i, [128, TBK], F32) for i in range(2)]; bzf = [Buf(), Buf()]
            q_ = T("hg_q", [128, TBK], F32); bq_ = Buf()
            sig = T("hg_sig", [128, TBK], F32); bsig = Buf()
            f_ = T("hg_f", [128, TBK], F32); bf_ = Buf()
            kk = T("hg_kk", [128, TBK], F32); bkk = Buf()
            b_ = T("hg_b", [128, TBK], F32); bb_ = Buf()
            e1 = T("hg_e1", [128, TBK], F32); be1 = Buf()
            eq = T("hg_eq", [128, TBK], F32); beq = Buf()
            ek = T("hg_ek", [128, TBK], F32); bek = Buf()
            qt = [T("hg_qt%d" % i, [128, TBK], BF16) for i in range(2)]; bqt = [Buf(), Buf()]
            kt = [T("hg_kt%d" % i, [128, TBK], BF16) for i in range(2)]; bkt = [Buf(), Buf()]
            if jl == 0:
                c.op('pool', lambda e: e.memset(lbc[:], 0.0), writes=[blbc])
            else:
                with nc.allow_non_contiguous_dma(reason="tiny"):
                    for j_ in range(2):
                        for r_ in range(2):
                            c.dma('sp', lambda e: e.dma_start(out=lbt[:, j_, r_, :], in_=lb_logits[j_, r_, :].rearrange("(h p) -> p h", p=128)), pwrites=[blbt])
                blbt.seal()
                c.op('dve', lambda e: e.tensor_tensor(out=lbc[:].rearrange("p (r h) -> p r h", r=2), in0=lbt[:, 1, :, :], in1=lbt[:, 0, :, :], op=ALU.subtract),
                     reads=[blbt], writes=[blbc])
                c.op('act', lambda e: e.activation(out=lbc[:], in_=lbc[:], func=AF.Sigmoid), reads=[blbc], writes=[blbc])
            c.op('dve', lambda e: e.tensor_scalar(out=oml[:], in0=lbc[:], scalar1=-1.0, scalar2=1.0, op0=ALU.mult, op1=ALU.add), reads=[blbc], writes=[boml])
            c.op('dve', lambda e: e.tensor_scalar(out=noml[:], in0=oml[:], scalar1=-1.0, scalar2=None, op0=ALU.mult), reads=[boml], writes=[bnoml])
            c.op('pool', lambda e: e.memset(msk[:], 1.0), writes=[bmsk])
            c.op('pool', lambda e: e.memset(msk[:].rearrange("p (c j) -> p c j", j=64)[:, :, 0:1], 0.0), writes=[bmsk])
            n = 0
            for h in range(4):
                for r in range(2):
                    hr = r * 4 + h
                    for tb in range(NTB):
                        nb = tb if r == 0 else NTB - 1 - tb
                        zq_, bzq_ = zq[n % 2], bzq[n % 2]
                        zf_, bzf_ = zf[n % 2], bzf[n % 2]
                        qt_, bqt_ = qt[n % 2], bqt[n % 2]
                        kt_, bkt_ = kt[n % 2], bkt[n % 2]
                        n += 1
                        c.dma('sp', lambda e: e.dma_start(out=zq_[:], in_=PT[h * 128:(h + 1) * 128, nb * TBK:(nb + 1) * TBK]), reads=[bPT], writes=[bzq_])
                        fr = 512 + r * 512 + h * 128
                        c.dma('act', lambda e: e.dma_start(out=zf_[:], in_=PT[fr:fr + 128, nb * TBK:(nb + 1) * TBK]), reads=[bPT], writes=[bzf_])
                        zqs = zq_[:, ::-1] if r else zq_[:, :]
                        zfs = zf_[:, ::-1] if r else zf_[:, :]
                        c.op('act', lambda e: e.activation(out=q_[:], in_=zqs, func=AF.Silu), reads=[bzq_], writes=[bq_])
                        c.op('act', lambda e: e.activation(out=sig[:], in_=zfs, func=AF.Sigmoid), reads=[bzf_], writes=[bsig])
                        c.op('dve', lambda e: e.tensor_scalar(out=f_[:], in0=sig[:], scalar1=oml[:, hr:hr + 1], scalar2=lbc[:, hr:hr + 1], op0=ALU.mult, op1=ALU.add),
                             reads=[bsig, boml, blbc], writes=[bf_])
                        c.op('act', lambda e: e.activation(out=f_[:], in_=f_[:], func=AF.Ln), reads=[bf_], writes=[bf_])
                        c.op('dve', lambda e: e.tensor_scalar(out=kk[:], in0=sig[:], scalar1=noml[:, hr:hr + 1], scalar2=oml[:, hr:hr + 1], op0=ALU.mult, op1=ALU.add),
                             reads=[bsig, bnoml, boml], writes=[bkk])
                        c.op('dve', lambda e: e.tensor_tensor_scan(out=b_[:], data0=msk[:], data1=f_[:], initial=0.0, op0=ALU.mult, op1=ALU.add),
                             reads=[bmsk, bf_], writes=[bb_])
                        b3 = b_[:].rearrange("p (c j) -> p c j", j=64)
                        c.op('dve', lambda e: e.tensor_tensor(out=e1[:].rearrange("p (c j) -> p c j", j=64), in0=b3, in1=b3[:, :, 31:32].broadcast_to([128, TBK // 64, 64]), op=ALU.subtract),
                             reads=[bb_], writes=[be1])
                        c.op('act', lambda e: e.activation(out=eq[:], in_=e1[:], func=AF.Exp), reads=[be1], writes=[beq])
                        c.op('act', lambda e: e.activation(out=ek[:], in_=e1[:], func=AF.Exp, scale=-1.0), reads=[be1], writes=[bek])
                        c.op('dve', lambda e: e.tensor_tensor(out=qt_[:], in0=q_[:], in1=eq[:], op=ALU.mult), reads=[bq_, beq], writes=[bqt_])
                        c.op('dve', lambda e: e.tensor_tensor(out=kt_[:], in0=kk[:], in1=ek[:], op=ALU.mult), reads=[bkk, bek], writes=[bkt_])
                        nch = TBK // 64
                        c.op('act', lambda e: e.activation(out=bmid[:, tb * nch:(tb + 1) * nch], in_=b3[:, :, 31], func=AF.Copy), reads=[bb_], writes=[bbmid])
                        c.op('act', lambda e: e.activation(out=blast[:, tb * nch:(tb + 1) * nch], in_=b3[:, :, 63], func=AF.Copy), reads=[bb_], writes=[bblast])
                        c.dma('sp', lambda e: e.dma_start(out=QK[h, r, 0, :, tb * TBK:(tb + 1) * TBK], in_=qt_[:]), reads=[bqt_], pwrites=[bQK])
                        c.dma('act', lambda e: e.dma_start(out=QK[h, r, 1, :, tb * TBK:(tb + 1) * TBK], in_=kt_[:]), reads=[bkt_], pwrites=[bQK])
                    c.op('dve', lambda e: e.tensor_tensor(out=blast[:], in0=blast[:], in1=bmid[:], op=ALU.subtract), reads=[bblast, bbmid], writes=[bblast])
                    c.op('dve', lambda e: e.tensor_tensor(out=blast[:, 0:127], in0=blast[:, 0:127], in1=bmid[:, 1:128], op=ALU.add), reads=[bblast, bbmid], writes=[bblast])
                    c.op('act', lambda e: e.activation(out=mcols[:, hr, :], in_=blast[:], func=AF.Exp), reads=[bblast], writes=[bmcols])
            bQK.seal()
            barrier(c)
        if stage < 2:
            return
        with ExitStack() as es:
            def T(name, shape, dt):
                return es.enter_context(nc.sbuf_tensor(uniq(name), shape, dt))
            mask = T("hr_mask", [128, 128], F32); bmask = Buf()
            c.op('pool', lambda e: e.memset(mask[:], 1.0), writes=[bmask])
            c.op('pool', lambda e: e.affine_select(out=mask[:], in_=mask[:], pattern=[[1, 128]], compare_op=ALU.is_ge, fill=0.0, base=0, channel_multiplier=-1),
                 reads=[bmask], writes=[bmask])
            c.op('pool', lambda e: e.memset(mask[0:64, 64:128], 0.0), reads=[bmask], writes=[bmask])
            ptb, bptb = g.psb

            class CH:
                pass
            chs = []
            for r in range(2):
                ch = CH(); chs.append(ch); ch.r = r

                def TT(name, shape, dt, r=r):
                    return (T("hr%d_%s" % (r, name), shape, dt), Buf())
                ch.qb = [TT("qb%d" % i, [128, TBK], BF16) for i in range(2)]
                ch.kb = [TT("kb%d" % i, [128, TBK], BF16) for i in range(2)]
                ch.vb = [TT("vb%d" % i, [128, TBK // 128, 128], BF16) for i in range(2)]
                ch.vnat = TT("vnat", [128, TBK // 128, 128], BF16)
                ch.attnT = [TT("attn%d" % i, [128, 128], BF16) for i in range(2)]
                ch.ktok = [TT("ktok%d" % i, [128, 128], BF16) for i in range(2)]
                ch.M32 = TT("M32", [128, 128], F32); ch.Mb = TT("Mb", [128, 128], BF16); ch.tmp32 = TT("tmp32", [128, 128], F32)
                ch.osb = [TT("osb%d" % i, [128, 128], F32) for i in range(3)]
                ch.osf = [TT("osf%d" % i, [128, 128], F32) for i in range(3)]
                ch.pa = g.ps[3 * r]; ch.po = g.ps[3 * r + 1]; ch.pk = g.ps[3 * r + 2]
                ch.nblk = 0

            def blk_gen(ch, h, tb, b, qb_, bqb_, kb_, bkb_, vb_, bvb_):
                r = ch.r; hr = r * 4 + h
                blk = tb * (TBK // 128) + b
                at_, bat_ = ch.attnT[ch.nblk % 2]; kt_, bkt_ = ch.ktok[ch.nblk % 2]
                os_, bos_ = ch.osb[ch.nblk % 3]; of_, bof_ = ch.osf[ch.nblk % 3]
                ch.nblk += 1
                pa, bpa = ch.pa; po, bpo = ch.po; pk, bpk = ch.pk
                M32, bM32 = ch.M32; Mb, bMb = ch.Mb; tmp32, btmp32 = ch.tmp32
                pcol = 128 * r
                bs = slice(b * 128, (b + 1) * 128)
                c.op('pe', lambda e: e.matmul(pa[:, 0:128], lhsT=kb_[:, bs], rhs=qb_[:, bs], start=True, stop=True), reads=[bkb_, bqb_], writes=[bpa]); yield
                c.op('dve', lambda e: e.tensor_tensor(out=at_[:], in0=pa[:, 0:128], in1=mask[:], op=ALU.mult), reads=[bpa, bmask], writes=[bat_]); yield
                c.op('pe', lambda e: e.transpose(out=ptb[:, pcol:pcol + 128], in_=kb_[:, bs], identity=g.identb[:]), reads=[bkb_, g.b_identb], writes=[bptb]); yield
                c.op('act', lambda e: e.activation(out=kt_[:], in_=ptb[:, pcol:pcol + 128], func=AF.Copy), reads=[bptb], writes=[bkt_]); yield
                for ci in range(2):
                    r0 = 64 * ci
                    cidx = 2 * blk + ci
                    c.op('pe', lambda e: e.matmul(po[r0:r0 + 64, 0:128], lhsT=at_[r0:r0 + 64, r0:r0 + 64], rhs=vb_[r0:r0 + 64, b, :], start=True, stop=False),
                         reads=[bat_, bvb_], writes=[bpo])
                    c.op('pe', lambda e: e.matmul(po[r0:r0 + 64, 0:128], lhsT=qb_[:, b * 128 + r0:b * 128 + r0 + 64], rhs=Mb[:, :], start=False, stop=True),
                         reads=[bqb_, bMb], writes=[bpo]); yield
                    if cidx < 127:
                        c.op('pe', lambda e: e.matmul(pk[:, 0:128], lhsT=kt_[r0:r0 + 64, :], rhs=vb_[r0:r0 + 64, b, :], start=True, stop=True), reads=[bkt_, bvb_], writes=[bpk]); yield
                        c.op('dve', lambda e: e.tensor_tensor(out=tmp32[:], in0=pk[:, 0:128], in1=M32[:], op=ALU.add), reads=[bpk, bM32], writes=[btmp32]); yield
                        c.op('dve', lambda e: e.tensor_scalar(out=Mb[:], in0=tmp32[:], scalar1=mcols[:, hr, cidx:cidx + 1], scalar2=None, op0=ALU.mult), reads=[btmp32, bmcols], writes=[bMb]); yield
                        c.op('dve', lambda e: e.tensor_scalar(out=M32[:], in0=tmp32[:], scalar1=mcols[:, hr, cidx:cidx + 1], scalar2=None, op0=ALU.mult), reads=[btmp32, bmcols], writes=[bM32]); yield
                c.op('act', lambda e: e.activation(out=os_[:], in_=po[:, 0:128], func=AF.Copy), reads=[bpo], writes=[bos_]); yield
                if r == 0:
                    c.dma('sp', lambda e: e.dma_start(out=OA[blk * 128:(blk + 1) * 128, h * 128:(h + 1) * 128], in_=os_[:]), reads=[bos_], pwrites=[bOAs[h]]); yield
                else:
                    pf, bpf = g.ps[6]
                    c.op('pe', lambda e: e.matmul(pf[:, 0:128], lhsT=g.J32[:], rhs=os_[:], start=True, stop=True), reads=[g.b_J32, bos_], writes=[bpf]); yield
                    c.op('act', lambda e: e.activation(out=of_[:], in_=pf[:, 0:128], func=AF.Copy), reads=[bpf], writes=[bof_]); yield
                    c.dma('act', lambda e: e.dma_start(out=OA2[L - (blk + 1) * 128:L - blk * 128, h * 128:(h + 1) * 128], in_=of_[:]), reads=[bof_], pwrites=[bOA2s[h]]); yield

            for h in range(4):
                for ch in chs:
                    M32, bM32 = ch.M32; Mb, bMb = ch.Mb
                    c.op('pool', lambda e: e.memset(M32[:], 0.0), reads=[bM32], writes=[bM32])
                    c.op('pool', lambda e: e.memset(Mb[:], 0.0), reads=[bMb], writes=[bMb])
                for tb in range(NTB):
                    loaded = []
                    for ch in chs:
                        r = ch.r
                        qb_, bqb_ = ch.qb[tb % 2]; kb_, bkb_ = ch.kb[tb % 2]; vb_, bvb_ = ch.vb[tb % 2]
                        c.dma('sp', lambda e: e.dma_start(out=qb_[:], in_=QK[h, r, 0, :, tb * TBK:(tb + 1) * TBK]), reads=[bQK], writes=[bqb_])
                        c.dma('act', lambda e: e.dma_start(out=kb_[:], in_=QK[h, r, 1, :, tb * TBK:(tb + 1) * TBK]), reads=[bQK], writes=[bkb_])
                        if r == 0:
                            vsrc = PV[tb * TBK:(tb + 1) * TBK, h * 128:(h + 1) * 128].rearrange("(b p) d -> p b d", p=128)
                            c.dma('sp', lambda e: e.dma_start(out=vb_[:], in_=vsrc), reads=[bPV], writes=[bvb_])
                        else:
                            vnat, bvnat = ch.vnat
                            vsrc = PV[L - (tb + 1) * TBK:L - tb * TBK, h * 128:(h + 1) * 128].rearrange("(b p) d -> p b d", p=128)
                            c.dma('sp', lambda e: e.dma_start(out=vnat[:], in_=vsrc), reads=[bPV], writes=[bvnat])
                            nbk = TBK // 128
                            for b4 in range(0, nbk, 4):
                                pf, bpf = g.ps[6]
                                c.op('pe', lambda e: e.matmul(pf[:, :], lhsT=g.Jb[:], rhs=vnat[:, b4:b4 + 4, :], start=True, stop=True), reads=[g.b_Jb, bvnat], writes=[bpf])
                                for bb in range(4):
                                    c.op('act', lambda e: e.activation(out=vb_[:, nbk - 1 - (b4 + bb), :], in_=pf[:, bb * 128:(bb + 1) * 128], func=AF.Copy), reads=[bpf, bvb_], writes=[bvb_])
                        loaded.append((qb_, bqb_, kb_, bkb_, vb_, bvb_))
                    for b in range(TBK // 128):
                        gens = [blk_gen(ch, h, tb, b, *loaded[i]) for i, ch in enumerate(chs)]
                        alive = list(gens)
                        while alive:
                            for gnr in list(alive):
                                try:
                                    next(gnr)
                                except StopIteration:
                                    alive.remove(gnr)
                bOAs[h].seal(); bOA2s[h].seal()
            barrier(c)
        if stage < 3:
            return
    gated_norm_finalize(c, g, OA, bOAs, PV, bPV, 512, a_out_norm, MT, bMT, 0, "hf_", OA2=OA2, bOA2s=bOA2s)


TB5 = 1024
NTB5 = L // TB5
TWO_PI = 2.0 * math.pi


def s5_phase(c, g, PT, bPT, lam_re, lam_im, log_step, b_re, b_im, c_re, c_im, d_skip, glu_w, glu_b, YT, bYT, MT, bMT, stage=3):
    nc = c.nc
    U0 = 1536
    with ExitStack() as es0:
        def T0(name, shape, dt):
            return es0.enter_context(nc.sbuf_tensor(uniq(name), shape, dt))
        WB = [T0("s5_WB%d" % p, [128, 2, 4, 128], BF16) for p in range(2)]; bWB = Buf()
        WC = [T0("s5_WC%d" % p, [128, 2, 4, 128], BF16) for p in range(2)]; bWC = Buf()
        WBx = [T0("s5_WBx%d" % p, [128, 2, 4, 128], BF16) for p in range(2)]
        WCx = [T0("s5_WCx%d" % p, [128, 2, 4, 64], BF16) for p in range(2)]
        mag = T0("s5_mag", [128, 32], F32); bmag = Buf()
        pwc = T0("s5_pwc", [128, 11, 32], F32); bpw = Buf()
        pws = T0("s5_pws", [128, 11, 32], F32)
        with ExitStack() as es:
            def T(name, shape, dt):
                return es.enter_context(nc.sbuf_tensor(uniq(name), shape, dt))
            n_ = [0]

            def S(shape=[128, 32], dt=F32):
                n_[0] += 1
                return T("s5_t%d" % n_[0], shape, dt), Buf()
            lamre, blamre = S(); lamim, blamim = S()
            lsB, blsB = S([128, 64]); ls, bls = S()
            with nc.allow_non_contiguous_dma(reason="small params"):
                for r_ in range(2):
                    for g4 in range(0, 16, 4):
                        c.dma('sp', lambda e: e.dma_start(out=lamre[:, r_ * 16 + g4:r_ * 16 + g4 + 4], in_=lam_re[r_, 2 * g4:2 * g4 + 8, :].rearrange("(gp gl) n -> (gl n) gp", gl=2)), pwrites=[blamre])
                        c.dma('act', lambda e: e.dma_start(out=lamim[:, r_ * 16 + g4:r_ * 16 + g4 + 4], in_=lam_im[r_, 2 * g4:2 * g4 + 8, :].rearrange("(gp gl) n -> (gl n) gp", gl=2)), pwrites=[blamim])
                blamre.seal(); blamim.seal()
                c.dma('sp', lambda e: e.dma_start(out=lsB[:], in_=log_step.rearrange("r g -> (r g)").partition_broadcast(128)), writes=[blsB])
            lsv = lsB[:].rearrange("p (r gp gl) -> p r gp gl", r=2, gl=2)
            c.op('dve', lambda e: e.tensor_copy(out=ls[0:64, :].rearrange("p (r gp) -> p r gp", r=2), in_=lsv[0:64, :, :, 0]), reads=[blsB], writes=[bls])
            c.op('dve', lambda e: e.tensor_copy(out=ls[64:128, :].rearrange("p (r gp) -> p r gp", r=2), in_=lsv[64:128, :, :, 1]), reads=[blsB], writes=[bls])
            step, bstep = S()
            c.op('act', lambda e: e.activation(out=step[:], in_=ls[:], func=AF.Exp), reads=[bls], writes=[bstep])
            lrs, blrs = S(); ang, bang = S()
            c.op('dve', lambda e: e.tensor_tensor(out=lrs[:], in0=lamre[:], in1=step[:], op=ALU.mult), reads=[blamre, bstep], writes=[blrs])
            c.op('act', lambda e: e.activation(out=mag[:], in_=lrs[:], func=AF.Exp), reads=[blrs], writes=[bmag])
            c.op('dve', lambda e: e.tensor_tensor(out=ang[:], in0=lamim[:], in1=step[:], op=ALU.mult), reads=[blamim, bstep], writes=[bang])

            def sin_of(src, bsrc, offset, dst, bdst):
                q, bq = S(); qi, bqi = S(dt=I32); r, br = S(); m, bm = S()
                c.op('dve', lambda e: e.tensor_scalar(out=q[:], in0=src[:], scalar1=offset, scalar2=1.0 / TWO_PI, op0=ALU.add, op1=ALU.mult), reads=[bsrc], writes=[bq])
                c.op('dve', lambda e: e.tensor_copy(out=qi[:], in_=q[:]), reads=[bq], writes=[bqi])
                c.op('dve', lambda e: e.tensor_copy(out=q[:], in_=qi[:]), reads=[bqi], writes=[bq])
                c.op('dve', lambda e: e.scalar_tensor_tensor(out=r[:], in0=q[:], scalar=-TWO_PI, in1=src[:], op0=ALU.mult, op1=ALU.add), reads=[bq, bsrc], writes=[br])
                if offset != 0.0:
                    c.op('dve', lambda e: e.tensor_scalar(out=r[:], in0=r[:], scalar1=offset, scalar2=None, op0=ALU.add), reads=[br], writes=[br])
                c.op('dve', lambda e: e.tensor_scalar(out=m[:], in0=r[:], scalar1=math.pi, scalar2=-TWO_PI, op0=ALU.is_gt, op1=ALU.mult), reads=[br], writes=[bm])
                c.op('dve', lambda e: e.tensor_tensor(out=r[:], in0=r[:], in1=m[:], op=ALU.add), reads=[br, bm], writes=[br])
                c.op('dve', lambda e: e.tensor_scalar(out=m[:], in0=r[:], scalar1=-math.pi, scalar2=TWO_PI, op0=ALU.is_lt, op1=ALU.mult), reads=[br], writes=[bm])
                c.op('dve', lambda e: e.tensor_tensor(out=r[:], in0=r[:], in1=m[:], op=ALU.add), reads=[br, bm], writes=[br])
                c.op('dve', lambda e: e.tensor_scalar(out=r[:], in0=r[:], scalar1=math.pi, scalar2=-math.pi, op0=ALU.min, op1=ALU.max), reads=[br], writes=[br])
                c.op('act', lambda e: e.activation(out=dst, in_=r[:], func=AF.Sin), reads=[br], writes=[bdst])
            sin_of(ang, bang, 0.0, pws[:, 0, :], bpw)
            sin_of(ang, bang, math.pi / 2, pwc[:, 0, :], bpw)
            tq, btq = S(); tq2, btq2 = S()
            for k in range(10):
                c.op('dve', lambda e: e.tensor_tensor(out=tq[:], in0=pwc[:, k, :], in1=pwc[:, k, :], op=ALU.mult), reads=[bpw], writes=[btq])
                c.op('dve', lambda e: e.tensor_tensor(out=tq2[:], in0=pws[:, k, :], in1=pws[:, k, :], op=ALU.mult), reads=[bpw], writes=[btq2])
                c.op('dve', lambda e: e.tensor_tensor(out=pwc[:, k + 1, :], in0=tq[:], in1=tq2[:], op=ALU.subtract), reads=[btq, btq2, bpw], writes=[bpw])
                c.op('dve', lambda e: e.tensor_tensor(out=tq[:], in0=pws[:, k, :], in1=pwc[:, k, :], op=ALU.mult), reads=[bpw], writes=[btq])
                c.op('dve', lambda e: e.tensor_scalar(out=pws[:, k + 1, :], in0=tq[:], scalar1=2.0, scalar2=None, op0=ALU.mult), reads=[btq, bpw], writes=[bpw])
            are, bare = S(); aim, baim = S(); den, bden = S(); am1, bam1 = S(); fr, bfr = S(); fi, bfi = S(); tt, btt = S()
            c.op('dve', lambda e: e.tensor_tensor(out=are[:], in0=mag[:], in1=pwc[:, 0, :], op=ALU.mult), reads=[bmag, bpw], writes=[bare])
            c.op('dve', lambda e: e.tensor_tensor(out=aim[:], in0=mag[:], in1=pws[:, 0, :], op=ALU.mult), reads=[bmag, bpw], writes=[baim])
            c.op('dve', lambda e: e.tensor_tensor(out=den[:], in0=lamre[:], in1=lamre[:], op=ALU.mult), reads=[blamre], writes=[bden])
            c.op('dve', lambda e: e.tensor_tensor(out=tt[:], in0=lamim[:], in1=lamim[:], op=ALU.mult), reads=[blamim], writes=[btt])
            c.op('dve', lambda e: e.tensor_tensor(out=den[:], in0=den[:], in1=tt[:], op=ALU.add), reads=[bden, btt], writes=[bden])
            c.op('dve', lambda e: e.reciprocal(out=den[:], in_=den[:]), reads=[bden], writes=[bden])
            c.op('dve', lambda e: e.tensor_scalar(out=am1[:], in0=are[:], scalar1=-1.0, scalar2=None, op0=ALU.add), reads=[bare], writes=[bam1])
            c.op('dve', lambda e: e.tensor_tensor(out=fr[:], in0=am1[:], in1=lamre[:], op=ALU.mult), reads=[bam1, blamre], writes=[bfr])
            c.op('dve', lambda e: e.tensor_tensor(out=tt[:], in0=aim[:], in1=lamim[:], op=ALU.mult), reads=[baim, blamim], writes=[btt])
            c.op('dve', lambda e: e.tensor_tensor(out=fr[:], in0=fr[:], in1=tt[:], op=ALU.add), reads=[bfr, btt], writes=[bfr])
            c.op('dve', lambda e: e.tensor_tensor(out=fr[:], in0=fr[:], in1=den[:], op=ALU.mult), reads=[bfr, bden], writes=[bfr])
            c.op('dve', lambda e: e.tensor_tensor(out=fi[:], in0=aim[:], in1=lamre[:], op=ALU.mult), reads=[baim, blamre], writes=[bfi])
            c.op('dve', lambda e: e.tensor_tensor(out=tt[:], in0=am1[:], in1=lamim[:], op=ALU.mult), reads=[bam1, blamim], writes=[btt])
            c.op('dve', lambda e: e.tensor_tensor(out=fi[:], in0=fi[:], in1=tt[:], op=ALU.subtract), reads=[bfi, btt], writes=[bfi])
            c.op('dve', lambda e: e.tensor_tensor(out=fi[:], in0=fi[:], in1=den[:], op=ALU.mult), reads=[bfi, bden], writes=[bfi])
            mk, bmk = S([128, 2])
            c.op('pool', lambda e: e.memset(mk[:], 0.0), writes=[bmk])
            c.op('pool', lambda e: e.memset(mk[0:64, 0:1], 1.0), reads=[bmk], writes=[bmk])
            c.op('pool', lambda e: e.memset(mk[64:128, 1:2], 1.0), reads=[bmk], writes=[bmk])
            Bn = [S([128, 2, 16, 16]) for _ in range(2)]
            with nc.allow_non_contiguous_dma(reason="small params"):
                for r_ in range(2):
                    for g4 in range(0, 16, 4):
                        c.dma('sp', lambda e: e.dma_start(out=Bn[0][0][:, r_, g4:g4 + 4, :], in_=b_re[r_, 2 * g4:2 * g4 + 8].rearrange("(gp gl) n p -> (gl n) gp p", gl=2)), pwrites=[Bn[0][1]])
                        c.dma('act', lambda e: e.dma_start(out=Bn[1][0][:, r_, g4:g4 + 4, :], in_=b_im[r_, 2 * g4:2 * g4 + 8].rearrange("(gp gl) n p -> (gl n) gp p", gl=2)), pwrites=[Bn[1][1]])
                Bn[0][1].seal(); Bn[1][1].seal()
            frb = fr[:].rearrange("p (r gp) -> p r gp", r=2).unsqueeze(3).broadcast_to([128, 2, 16, 16])
            fib = fi[:].rearrange("p (r gp) -> p r gp", r=2).unsqueeze(3).broadcast_to([128, 2, 16, 16])
            bbr, bbbr = S([128, 2, 16, 16]); bbi, bbbi = S([128, 2, 16, 16]); t5, bt5 = S([128, 2, 16, 16])
            c.op('dve', lambda e: e.tensor_tensor(out=bbr[:], in0=Bn[0][0][:], in1=frb, op=ALU.mult), reads=[Bn[0][1], bfr], writes=[bbbr])
            c.op('dve', lambda e: e.tensor_tensor(out=t5[:], in0=Bn[1][0][:], in1=fib, op=ALU.mult), reads=[Bn[1][1], bfi], writes=[bt5])
            c.op('dve', lambda e: e.tensor_tensor(out=bbr[:], in0=bbr[:], in1=t5[:], op=ALU.subtract), reads=[bbbr, bt5], writes=[bbbr])
            c.op('dve', lambda e: e.tensor_tensor(out=bbi[:], in0=Bn[1][0][:], in1=frb, op=ALU.mult), reads=[Bn[1][1], bfr], writes=[bbbi])
            c.op('dve', lambda e: e.tensor_tensor(out=t5[:], in0=Bn[0][0][:], in1=fib, op=ALU.mult), reads=[Bn[0][1], bfi], writes=[bt5])
            c.op('dve', lambda e: e.tensor_tensor(out=bbi[:], in0=bbi[:], in1=t5[:], op=ALU.add), reads=[bbbi, bt5], writes=[bbbi])
            BBm, bBBm = S([128, 2, 16, 2, 16], BF16)
            ptb, bptb = g.psb
            for part, (src, bsrc) in enumerate(((bbr, bbbr), (bbi, bbbi))):
                for gl in range(2):
                    c.op('dve', lambda e: e.tensor_scalar(out=BBm[:, :, :, gl, :], in0=src[:], scalar1=mk[:, gl:gl + 1], scalar2=None, op0=ALU.mult), reads=[bsrc, bmk, bBBm], writes=[bBBm])
                for r in range(2):
                    for cb in range(4):
                        c.op('pe', lambda e: e.transpose(out=ptb[:, 0:128], in_=BBm[:, r, 4 * cb:4 * cb + 4, :, :].rearrange("p a b c -> p (a b c)"), identity=g.identb[:]),
                             reads=[bBBm, g.b_identb], writes=[bptb])
                        c.op('act', lambda e: e.activation(out=WB[part][:, r, cb, :], in_=ptb[:, 0:128], func=AF.Copy), reads=[bptb], writes=[bWB])
            Cn = [S([128, 2, 4, 64]) for _ in range(2)]
            c.dma('sp', lambda e: e.dma_start(out=Cn[0][0][:], in_=c_re.rearrange("r (cb g8) p n -> (g8 p) r cb n", g8=8)), writes=[Cn[0][1]])
            c.dma('act', lambda e: e.dma_start(out=Cn[1][0][:], in_=c_im.rearrange("r (cb g8) p n -> (g8 p) r cb n", g8=8)), writes=[Cn[1][1]])
            Cd, bCd = S([128, 2, 4, 2, 64], BF16)
            mkb = mk[:].unsqueeze(1).unsqueeze(3).broadcast_to([128, 4, 2, 16])
            for part in range(2):
                sc = 1.0 if part == 0 else -1.0
                for x in range(2):
                    c.op('dve', lambda e: e.tensor_scalar(out=Cd[:, :, :, x, :], in0=Cn[part][0][:], scalar1=sc, scalar2=None, op0=ALU.mult), reads=[Cn[part][1], bCd], writes=[bCd])
                for r in range(2):
                    for cb in range(4):
                        c.op('pe', lambda e: e.transpose(out=ptb[:, 0:128], in_=Cd[:, r, cb, :, :].rearrange("p a b -> p (a b)"), identity=g.identb[:]),
                             reads=[bCd, g.b_identb], writes=[bptb])
                        c.op('dve', lambda e: e.tensor_tensor(out=WC[part][:, r, cb, :].rearrange("p (k a b) -> p k a b", k=4, a=2), in0=ptb[:, 0:128].rearrange("p (k a b) -> p k a b", k=4, a=2),
                                                              in1=mkb, op=ALU.mult), reads=[bptb, bmk], writes=[bWC])
            for part in range(2):
                c.op('act', lambda e: e.activation(out=WBx[part][64:128], in_=WB[part][64:128], func=AF.Copy), reads=[bWB], writes=[bWB])
                c.op('pool', lambda e: e.memset(WBx[part][64:96], 0.0), reads=[bWB], writes=[bWB])
                c.op('act', lambda e: e.activation(out=WCx[part][:], in_=WC[part][:, :, :, 64:128], func=AF.Copy), reads=[bWC], writes=[bWC])
                c.op('pool', lambda e: e.memset(WCx[part][:, :, :, 0:32], 0.0), reads=[bWC], writes=[bWC])
            barrier(c)
        if stage < 2:
            return
        with ExitStack() as es:
            def T(name, shape, dt):
                return es.enter_context(nc.sbuf_tensor(uniq(name), shape, dt))
            uf = T("s5_uf", [128, TB5 * 2], F32); buf_ = Buf()
            ub = [T("s5_ub%d" % r, [128, L], BF16) for r in range(2)]; bub = [Buf(), Buf()]
            Xa = [[T("s5_X%d%d" % (p, r), [128, L], BF16) for r in range(2)] for p in range(2)]
            bXa = [[Buf(), Buf()], [Buf(), Buf()]]
            tcos = T("s5_cos", [128, TB5], F32); tsin = T("s5_sin", [128, TB5], F32); btab = Buf()
            BUs = [T("s5_BU%d" % p, [128, TB5], F32) for p in range(2)]; bBUs = [Buf(), Buf()]
            t = [T("s5_w%d" % i, [128, TB5], F32) for i in range(4)]; bt = [Buf() for _ in range(4)]
            ini = T("s5_ini", [128, 4], F32); bini = Buf()
            yst = [T("s5_yst%d" % i, [128, 512], F32) for i in range(2)]; byst = [Buf(), Buf()]
            npz = 0
            for cb in range(4):
                for r in range(2):
                    for hh in range(L // (2 * TB5)):
                        nb = hh if r == 0 else L // (2 * TB5) - 1 - hh
                        c.dma('sp', lambda e: e.dma_start(out=uf[:], in_=PT[U0 + cb * 128:U0 + (cb + 1) * 128, nb * 2 * TB5:(nb + 1) * 2 * TB5]), reads=[bPT], writes=[buf_])
                        src = uf[:, ::-1] if r else uf[:, :]
                        c.op('act', lambda e: e.activation(out=ub[r][:, hh * 2 * TB5:(hh + 1) * 2 * TB5], in_=src, func=AF.Copy), reads=[buf_], writes=[bub[r]])
                for k in range(4):
                    gp = cb * 4 + k
                    for r in range(2):
                        col = r * 16 + gp
                        c.op('pool', lambda e: e.memset(tcos[:, 0:1], 1.0), writes=[btab])
                        c.op('pool', lambda e: e.memset(tsin[:, 0:1], 0.0), reads=[btab], writes=[btab])
                        n = 1
                        kk = 0
                        while n < TB5:
                            cr = pwc[:, kk, col:col + 1]; ci = pws[:, kk, col:col + 1]
                            c.op('dve', lambda e: e.tensor_scalar(out=t[0][:, 0:n], in0=tsin[:, 0:n], scalar1=ci, scalar2=None, op0=ALU.mult), reads=[btab, bpw], writes=[bt[0]])
                            c.op('dve', lambda e: e.tensor_scalar(out=t[1][:, 0:n], in0=tsin[:, 0:n], scalar1=cr, scalar2=None, op0=ALU.mult), reads=[btab, bpw], writes=[bt[1]])
                            c.op('dve', lambda e: e.scalar_tensor_tensor(out=tsin[:, n:2 * n], in0=tcos[:, 0:n], scalar=ci, in1=t[1][:, 0:n], op0=ALU.mult, op1=ALU.add),
                                 reads=[btab, bpw, bt[1]], writes=[btab])
                            c.op('dve', lambda e: e.scalar_tensor_tensor(out=tcos[:, n:2 * n], in0=tcos[:, 0:n], scalar=cr, in1=t[0][:, 0:n], op0=ALU.mult, op1=ALU.subtract),
                                 reads=[btab, bpw, bt[0]], writes=[btab])
                            n *= 2; kk += 1
                        cTB = pwc[:, kk, col:col + 1]; sTB = pws[:, kk, col:col + 1]
                        rho = mag[:, col:col + 1]
                        c.op('pool', lambda e: e.memset(ini[:], 0.0), writes=[bini])
                        for tb in range(NTB5):
                            ts0 = tb * TB5
                            for part in range(2):
                                for hf in range(TB5 // 512):
                                    ps, bps = g.ps[npz % 4]; npz += 1
                                    if k < 3:
                                        lh = WB[part][32 * k:32 * k + 32, r, cb, :]; rh = ub[r][32 * k:32 * k + 32, ts0 + hf * 512:ts0 + (hf + 1) * 512]
                                    else:
                                        lh = WBx[part][64:128, r, cb, :]; rh = ub[r][64:128, ts0 + hf * 512:ts0 + (hf + 1) * 512]
                                    c.op('pe', lambda e: e.matmul(ps[:, :], lhsT=lh, rhs=rh, start=True, stop=True),
                                         reads=[bWB, bub[r]], writes=[bps])
                                    c.op('act', lambda e: e.activation(out=BUs[part][:, hf * 512:(hf + 1) * 512], in_=ps[:, :], func=AF.Copy), reads=[bps], writes=[bBUs[part]])
                            c.op('dve', lambda e: e.tensor_tensor(out=t[0][:], in0=BUs[0][:], in1=tcos[:], op=ALU.mult), reads=[bBUs[0], btab], writes=[bt[0]])
                            c.op('dve', lambda e: e.tensor_tensor(out=t[1][:], in0=BUs[1][:], in1=tsin[:], op=ALU.mult), reads=[bBUs[1], btab], writes=[bt[1]])
                            c.op('dve', lambda e: e.tensor_tensor(out=t[0][:], in0=t[0][:], in1=t[1][:], op=ALU.add), reads=[bt[0], bt[1]], writes=[bt[0]])
                            c.op('pool', lambda e: e.tensor_tensor(out=t[2][:], in0=BUs[1][:], in1=tcos[:], op=ALU.mult), reads=[bBUs[1], btab], writes=[bt[2]])
                            c.op('pool', lambda e: e.tensor_tensor(out=t[3][:], in0=BUs[0][:], in1=tsin[:], op=ALU.mult), reads=[bBUs[0], btab], writes=[bt[3]])
                            c.op('dve', lambda e: e.tensor_tensor(out=t[2][:], in0=t[2][:], in1=t[3][:], op=ALU.subtract), reads=[bt[2], bt[3]], writes=[bt[2]])
                            c.op('dve', lambda e: e.tensor_tensor_scan(out=t[1][:], data0=rho.broadcast_to([128, TB5]), data1=t[0][:], initial=ini[:, 0:1], op0=ALU.mult, op1=ALU.add),
                                 reads=[bmag, bt[0], bini], writes=[bt[1]])
                            c.op('dve', lambda e: e.tensor_tensor_scan(out=t[3][:], data0=rho.broadcast_to([128, TB5]), data1=t[2][:], initial=ini[:, 1:2], op0=ALU.mult, op1=ALU.add),
                                 reads=[bmag, bt[2], bini], writes=[bt[3]])
                            if tb < NTB5 - 1:
                                xr = t[1][:, TB5 - 1:TB5]; xi = t[3][:, TB5 - 1:TB5]
                                c.op('dve', lambda e: e.tensor_scalar(out=ini[:, 2:3], in0=xi, scalar1=sTB, scalar2=None, op0=ALU.mult), reads=[bt[3], bpw], writes=[bini])
                                c.op('dve', lambda e: e.scalar_tensor_tensor(out=ini[:, 0:1], in0=xr, scalar=cTB, in1=ini[:, 2:3], op0=ALU.mult, op1=ALU.subtract), reads=[bt[1], bpw, bini], writes=[bini])
                                c.op('dve', lambda e: e.tensor_scalar(out=ini[:, 3:4], in0=xi, scalar1=cTB, scalar2=None, op0=ALU.mult), reads=[bt[3], bpw], writes=[bini])
                                c.op('dve', lambda e: e.scalar_tensor_tensor(out=ini[:, 1:2], in0=xr, scalar=sTB, in1=ini[:, 3:4], op0=ALU.mult, op1=ALU.add), reads=[bt[1], bpw, bini], writes=[bini])
                            if r == 0:
                                oslc = slice(ts0, ts0 + TB5)
                                xo_re = Xa[0][r][:, oslc]; xo_im = Xa[1][r][:, oslc]
                            else:
                                lo = L - ts0 - TB5
                                xo_re = Xa[0][r][:, lo:lo + TB5][:, ::-1]; xo_im = Xa[1][r][:, lo:lo + TB5][:, ::-1]
                            c.op('dve', lambda e: e.tensor_tensor(out=t[0][:], in0=t[1][:], in1=tcos[:], op=ALU.mult), reads=[bt[1], btab], writes=[bt[0]])
                            c.op('pool', lambda e: e.tensor_tensor(out=t[2][:], in0=t[3][:], in1=tsin[:], op=ALU.mult), reads=[bt[3], btab], writes=[bt[2]])
                            c.op('dve', lambda e: e.tensor_tensor(out=xo_re, in0=t[0][:], in1=t[2][:], op=ALU.subtract), reads=[bt[0], bt[2]], pwrites=[bXa[0][r]])
                            c.op('pool', lambda e: e.tensor_tensor(out=t[0][:], in0=t[1][:], in1=tsin[:], op=ALU.mult), reads=[bt[1], btab], writes=[bt[0]])
                            c.op('dve', lambda e: e.tensor_tensor(out=t[2][:], in0=t[3][:], in1=tcos[:], op=ALU.mult), reads=[bt[3], btab], writes=[bt[2]])
                            c.op('dve', lambda e: e.tensor_tensor(out=xo_im, in0=t[0][:], in1=t[2][:], op=ALU.add), reads=[bt[0], bt[2]], pwrites=[bXa[1][r]])
                        bXa[0][r].seal(); bXa[1][r].seal()
                    for it in range(L // 512):
                        ps, bps = g.ps[4 + it % 2]
                        i = 0
                        for r in range(2):
                            for part in range(2):
                                if k < 3:
                                    po = ps[32 * k:32 * k + 32, :]; lh = WC[part][:, r, cb, 32 * k:32 * k + 32]
                                else:
                                    po = ps[64:128, :]; lh = WCx[part][:, r, cb, :]
                                c.op('pe', lambda e: e.matmul(po, lhsT=lh, rhs=Xa[part][r][:, it * 512:(it + 1) * 512], start=(i == 0), stop=(i == 3)),
                                     reads=[bWC, bXa[part][r]], writes=[bps])
                                i += 1
                        ys, bys = yst[it % 2], byst[it % 2]
                        e0 = 32 * k if k < 3 else 64
                        c.op('act', lambda e: e.activation(out=ys[e0:32 * k + 32, :], in_=ps[e0:32 * k + 32, :], func=AF.Copy), reads=[bps], writes=[bys])
                        c.dma('sp', lambda e: e.dma_start(out=YT[cb * 128 + 32 * k:cb * 128 + 32 * k + 32, it * 512:(it + 1) * 512], in_=ys[32 * k:32 * k + 32, :]), reads=[bys], pwrites=[bYT])
            bYT.seal()
            barrier(c)
        if stage < 3:
            return
        with ExitStack() as es:
            def T(name, shape, dt):
                return es.enter_context(nc.sbuf_tensor(uniq(name), shape, dt))
            gw = T("s5_gw", [128, 4, 512], BF16); bgw = Buf()
            dcol = T("s5_dcol", [128, 4], F32); bdcol = Buf()
            gbc = T("s5_gbc", [128, 4], F32); bgbc = Buf()
            yt = [T("s5_yt%d" % i, [128, 4, 512], F32) for i in range(2)]; byt = [Buf(), Buf()]
            ut = [T("s5_ut%d" % i, [128, 4, 512], F32) for i in range(2)]; but = [Buf(), Buf()]
            sq = T("s5_sq", [128, 4, 512], F32); bsq = Buf()
            gy = T("s5_gy", [128, 4, 512], F32); bgy = Buf()
            gyb = T("s5_gyb", [128, 4, 512], BF16); bgyb = Buf()
            sg = [T("s5_sg%d" % i, [128, 512], F32) for i in range(2)]; bsg = [Buf(), Buf()]
            ob = [T("s5_ob%d" % i, [128, 4, 512], BF16) for i in range(2)]; bob = [Buf(), Buf()]
            c.dma('pool', lambda e: e.dma_start(out=gw[:], in_=glu_w.rearrange("(k p) f -> p k f", p=128)), writes=[bgw])
            with nc.allow_non_contiguous_dma(reason="small params"):
                c.dma('sp', lambda e: e.dma_start(out=dcol[:], in_=d_skip.rearrange("(k p) -> p k", p=128)), writes=[bdcol])
                c.dma('sp', lambda e: e.dma_start(out=gbc[:], in_=glu_b.rearrange("(k p) -> p k", p=128)), writes=[bgbc])
            GC = 1.5957691216057308
            for it in range(L // 512):
                yt_, byt_ = yt[it % 2], byt[it % 2]
                ut_, but_ = ut[it % 2], but[it % 2]
                ob_, bob_ = ob[it % 2], bob[it % 2]
                tsl = slice(it * 512, (it + 1) * 512)
                c.dma('sp', lambda e: e.dma_start(out=yt_[:], in_=YT[:, tsl].rearrange("(k p) t -> p k t", p=128)), reads=[bYT], writes=[byt_])
                c.dma('act', lambda e: e.dma_start(out=ut_[:], in_=PT[U0:U0 + 512, tsl].rearrange("(k p) t -> p k t", p=128)), reads=[bPT], writes=[but_])
                for k in range(4):
                    c.op('dve', lambda e: e.scalar_tensor_tensor(out=yt_[:, k, :], in0=ut_[:, k, :], scalar=dcol[:, k:k + 1], in1=yt_[:, k, :], op0=ALU.mult, op1=ALU.add),
                         reads=[but_, bdcol, byt_], writes=[byt_])
                c.op('act', lambda e: e.activation(out=sq[:], in_=yt_[:], func=AF.Square), reads=[byt_], writes=[bsq])
                c.op('dve', lambda e: e.tensor_scalar(out=sq[:], in0=sq[:], scalar1=0.044715, scalar2=1.0, op0=ALU.mult, op1=ALU.add), reads=[bsq], writes=[bsq])
                c.op('dve', lambda e: e.tensor_tensor(out=sq[:], in0=sq[:], in1=yt_[:], op=ALU.mult), reads=[bsq, byt_], writes=[bsq])
                c.op('act', lambda e: e.activation(out=sq[:], in_=sq[:], func=AF.Sigmoid, scale=GC), reads=[bsq], writes=[bsq])
                c.op('dve', lambda e: e.tensor_tensor(out=gy[:], in0=sq[:], in1=yt_[:], op=ALU.mult), reads=[bsq, byt_], writes=[bgy])
                c.op('act', lambda e: e.activation(out=gyb[:], in_=gy[:], func=AF.Copy), reads=[bgy], writes=[bgyb])
                for co in range(4):
                    ps, bps = g.ps[co % 4]
                    for k in range(4):
                        c.op('pe', lambda e: e.matmul(ps[:, :], lhsT=gw[:, k, co * 128:(co + 1) * 128], rhs=gyb[:, k, :], start=(k == 0), stop=(k == 3)),
                             reads=[bgw, bgyb], writes=[bps])
                    sg_, bsg_ = sg[co % 2], bsg[co % 2]
                    c.op('act', lambda e: e.activation(out=sg_[:], in_=ps[:, :], func=AF.Sigmoid, bias=gbc[:, co:co + 1], scale=1.0), reads=[bps, bgbc], writes=[bsg_])
                    c.op('dve', lambda e: e.tensor_tensor(out=ob_[:, co, :], in0=gy[:, co, :], in1=sg_[:], op=ALU.mult), reads=[bgy, bsg_, bob_], writes=[bob_])
                c.dma('sp', lambda e: e.dma_start(out=MT[512:1024, tsl].rearrange("(k p) t -> p k t", p=128), in_=ob_[:]), reads=[bob_], pwrites=[bMT])
            barrier(c)


def outproj_phase(c, g, MT, bMT, w_out, X, bX, tile_cb=None):
    nc = c.nc
    bXn = Buf('Xn')
    with ExitStack() as es:
        def T(name, shape, dt):
            return es.enter_context(nc.sbuf_tensor(uniq(name), shape, dt))
        wsb = T("op_w", [128, 8, D], BF16); bw = Buf()
        mt = [T("op_mt%d" % i, [128, 8, 512], BF16) for i in range(2)]; bmt = [Buf(), Buf()]
        xt = [T("op_xt%d" % i, [128, 4, D], F32) for i in range(2)]; bxt = [Buf(), Buf()]
        xo = [T("op_xo%d" % i, [128, 4, D], F32) for i in range(2)]; bxo = [Buf(), Buf()]
        for c0 in range(0, D, 512):
            c.dma('pool', lambda e: e.dma_start(out=wsb[:, :, c0:c0 + 512], in_=w_out[:, c0:c0 + 512].rearrange("(k p) f -> p k f", p=128)), pwrites=[bw])
        bw.seal()
        n = 0
        for it in range(L // 512):
            t0 = it * 512
            mt_, bmt_ = mt[it % 2], bmt[it % 2]
            xt_, bxt_ = xt[it % 2], bxt[it % 2]
            xo_, bxo_ = xo[it % 2], bxo[it % 2]
            c.dma('sp', lambda e: e.dma_start(out=mt_[:], in_=MT[:, t0:t0 + 512].rearrange("(k p) t -> p k t", p=128)), reads=[bMT], writes=[bmt_])
            c.dma('act', lambda e: e.dma_start(out=xt_[:], in_=X[t0:t0 + 512, :].rearrange("(j p) d -> p j d", p=128)), reads=[bX], writes=[bxt_])
            for j in range(4):
                for dh in range(2):
                    ps, bps = g.ps[n % 4]; n += 1
                    for k in range(8):
                        c.op('pe', lambda e: e.matmul(ps[:, :], lhsT=mt_[:, k, j * 128:(j + 1) * 128], rhs=wsb[:, k, dh * 512:(dh + 1) * 512], start=(k == 0), stop=(k == 7)),
                             reads=[bmt_, bw], writes=[bps])
                    c.op('dve', lambda e: e.tensor_tensor(out=xo_[:, j, dh * 512:(dh + 1) * 512], in0=ps[:, :], in1=xt_[:, j, dh * 512:(dh + 1) * 512], op=ALU.add),
                         reads=[bps, bxt_, bxo_], writes=[bxo_])
            c.dma('sp', lambda e: e.dma_start(out=X[t0:t0 + 512, :].rearrange("(j p) d -> p j d", p=128), in_=xo_[:]), reads=[bxo_], pwrites=[bXn])
            if tile_cb is not None:
                for j in range(4):
                    tile_cb(it * 4 + j, xo_[:, j, :], bxo_)
        bXn.seal()
        barrier(c)
    return bXn


def outproj_moe(c, g, MT, bMT, w_out, X, bX, HB, bHB, ffn_g, w_router, w_gate, w_up, w_down):
    with ExitStack() as es:
        sb = moe_alloc(c.nc, es, part=1)
        moe_prep(c, g, sb, ffn_g, w_router)
        bXn = outproj_phase(c, g, MT, bMT, w_out, X, bX, tile_cb=lambda i, xt, bxt: moe_step1_tile(c, g, sb, i, xt, bxt, HB, bHB))
        moe_alloc(c.nc, es, part=3, sb=sb)
        moe_rest(c, g, sb, X, bXn, HB, bHB, w_gate, w_up, w_down)
        barrier(c)
    return bXn


def t5_onehot():
    half = 16; max_exact = 8
    rel = np.arange(-255, 256)
    n = np.abs(rel)
    nf = np.maximum(n, 1).astype(np.float32)
    large = max_exact + (np.log(nf / np.float32(max_exact)) / np.float32(math.log(128 / max_exact)) * np.float32(half - max_exact)).astype(np.int32)
    large = np.minimum(large, half - 1)
    b = np.where(rel > 0, half, 0) + np.where(n < max_exact, n, large)
    oh = np.zeros((32, 512), np.float32)
    oh[b, np.arange(511)] = 1.0
    return oh


def attn_phase(c, g, PV, bPV, q_gain, k_gain, c_lambda, out_gain, rel_bias, onehot, layer_idx, QKT, bQKT, FV, bFV, MT, bMT, stage=3):
    nc = c.nc
    lam_init = 0.8 - 0.6 * math.exp(-0.3 * layer_idx)
    ptb, bptb = g.psb
    with ExitStack() as es:
        def T(name, shape, dt):
            return es.enter_context(nc.sbuf_tensor(uniq(name), shape, dt))
        g64 = T("at_g64", [128, 2, 64], F32); bg64 = Buf()
        gQK = T("at_gQK", [128, 16, 64], F32); bgQK = Buf()
        xq = [T("at_xq%d" % i, [128, 1024], BF16) for i in range(2)]; bxq = [Buf(), Buf()]
        sq = T("at_sq", [128, 1024], F32); bsq = Buf()
        ss = [T("at_ss%d" % i, [128, 16], F32) for i in range(2)]; bss = [Buf(), Buf()]
        xn = T("at_xn", [128, 1024], F32); bxn = Buf()
        xb = [T("at_xb%d" % i, [128, 1024], BF16) for i in range(2)]; bxb = [Buf(), Buf()]
        st = [T("at_st%d" % i, [128, 8, 512], BF16) for i in range(2)]; bst = [Buf(), Buf()]
        c.dma('sp', lambda e: e.dma_start(out=g64[:, 0, :], in_=q_gain.partition_broadcast(128)), pwrites=[bg64])
        c.dma('sp', lambda e: e.dma_start(out=g64[:, 1, :], in_=k_gain.partition_broadcast(128)), pwrites=[bg64])
        bg64.seal()
        c.op('dve', lambda e: e.tensor_scalar(out=gQK[:, 0:8, :], in0=g64[:, 0:1, :].broadcast_to([128, 8, 64]), scalar1=0.125, scalar2=None, op0=ALU.mult), reads=[bg64], writes=[bgQK])
        c.op('dve', lambda e: e.tensor_copy(out=gQK[:, 8:16, :], in_=g64[:, 1:2, :].broadcast_to([128, 8, 64])), reads=[bg64, bgQK], writes=[bgQK])
        for i in range(NT):
            xq_, bxq_ = xq[i % 2], bxq[i % 2]
            ss_, bss_ = ss[i % 2], bss[i % 2]
            xb_, bxb_ = xb[i % 2], bxb[i % 2]
            st_, bst_ = st[(i // 4) % 2], bst[(i // 4) % 2]
            c.dma('sp', lambda e: e.dma_start(out=xq_[:], in_=PV[i * 128:(i + 1) * 128, 0:1024]), reads=[bPV], writes=[bxq_])
            c.op('act', lambda e: e.activation(out=sq[:], in_=xq_[:], func=AF.Square), reads=[bxq_], writes=[bsq])
            c.op('dve', lambda e: e.tensor_reduce(out=ss_[:], in_=sq[:].rearrange("p (a d) -> p a d", d=64), axis=AX.X, op=ALU.add), reads=[bsq], writes=[bss_])
            c.op('dve', lambda e: e.tensor_scalar(out=ss_[:], in0=ss_[:], scalar1=1.0 / 64, scalar2=1e-6, op0=ALU.mult, op1=ALU.add), reads=[bss_], writes=[bss_])
            c.op('pool', lambda e: e.tensor_tensor(out=ss_[:], in0=ss_[:], in1=g.neghalf[:, 0:1].broadcast_to([128, 16]), op=ALU.pow), reads=[bss_, g.b_neghalf], writes=[bss_])
            c.op('dve', lambda e: e.tensor_tensor(out=xn[:].rearrange("p (a d) -> p a d", d=64), in0=xq_[:].rearrange("p (a d) -> p a d", d=64),
                                                  in1=ss_[:].unsqueeze(2).broadcast_to([128, 16, 64]), op=ALU.mult), reads=[bxq_, bss_], writes=[bxn])
            c.op('dve', lambda e: e.tensor_tensor(out=xb_[:], in0=xn[:], in1=gQK[:].rearrange("p a d -> p (a d)"), op=ALU.mult), reads=[bxn, bgQK], writes=[bxb_])
            for a in range(8):
                c.op('pe', lambda e: e.transpose(out=ptb[:, a * 128:(a + 1) * 128], in_=xb_[:, a * 128:(a + 1) * 128], identity=g.identb[:]), reads=[bxb_, g.b_identb], writes=[bptb])
            c.op('act', lambda e: e.activation(out=st_[:, :, (i % 4) * 128:(i % 4 + 1) * 128], in_=ptb[:, :].rearrange("p (a s) -> p a s", a=8), func=AF.Copy), reads=[bptb], writes=[bst_])
            if i % 4 == 3:
                t0 = (i // 4) * 512
                for a in range(8):
                    c.dma('sp' if a % 2 else 'act', lambda e: e.dma_start(out=QKT[a, :, t0:t0 + 512], in_=st_[:, a, :]), reads=[bst_], pwrites=[bQKT])
        bQKT.seal()
        barrier(c)
    if stage < 2:
        return
    with ExitStack() as es:
        def T(name, shape, dt):
            return es.enter_context(nc.sbuf_tensor(uniq(name), shape, dt))
        KT = T("at_KT", [128, L], BF16); bKT = Buf()
        Va = T("at_Va", [128, 64, 130], BF16); bVa = Buf()
        QT = [T("at_QT%d" % i, [128, 512], BF16) for i in range(2)]; bQT = [Buf(), Buf()]
        Pt = [T("at_P%d" % i, [128, 512], BF16) for i in range(4)]; bPt = [Buf() for _ in range(4)]
        tmp = [T("at_tmp%d" % i, [128, 512], F32) for i in range(2)]; btmp = [Buf(), Buf()]
        biasT = T("at_bias", [128, 4, 3, 128], F32); bbias = Buf()
        hank = T("at_hank", [128, 128], F32); bhank = Buf()
        cfar = T("at_cfar", [128, 4, 2], F32); bcfar = Buf()
        tab = T("at_tab", [32, 4], F32); btab = Buf()
        oh = T("at_oh", [32, 512], F32); boh = Buf()
        fv = T("at_fv", [4, 512], F32); bfv = Buf()
        lamt = T("at_lamt", [128, 4, 64], F32); blamt = Buf()
        lam = T("at_lam", [128, 8], F32); blam = Buf()
        gO = T("at_gO", [128, 128], F32); bgO = Buf()
        rs = [T("at_rs%d" % i, [128, 4], F32) for i in range(2)]; brs = [Buf(), Buf()]
        t1 = T("at_t1", [128, 128], F32); bt1 = Buf()
        w_ = T("at_w", [128, 128], F32); bw_ = Buf()
        junk = T("at_junk", [128, 128], F32); bjunk = Buf()
        wb = [T("at_wb%d" % i, [128, 128], BF16) for i in range(2)]; bwb = [Buf(), Buf()]
        ost = [T("at_ost%d" % i, [128, 512], BF16) for i in range(2)]; bost = [Buf(), Buf()]
        c.dma('sp', lambda e: e.dma_start(out=lamt[:].rearrange("p a d -> p (a d)"), in_=c_lambda.rearrange("a d -> (a d)").partition_broadcast(128)), writes=[blamt])
        c.op('dve', lambda e: e.tensor_tensor(out=lamt[:, 0, :], in0=lamt[:, 0, :], in1=lamt[:, 1, :], op=ALU.mult), reads=[blamt], writes=[blamt])
        c.op('dve', lambda e: e.tensor_tensor(out=lamt[:, 2, :], in0=lamt[:, 2, :], in1=lamt[:, 3, :], op=ALU.mult), reads=[blamt], writes=[blamt])
        c.op('dve', lambda e: e.tensor_reduce(out=lam[:, 0:1], in_=lamt[:, 0, :], axis=AX.X, op=ALU.add), reads=[blamt], writes=[blam])
        c.op('dve', lambda e: e.tensor_reduce(out=lam[:, 1:2], in_=lamt[:, 2, :], axis=AX.X, op=ALU.add), reads=[blamt, blam], writes=[blam])
        c.op('act', lambda e: e.activation(out=lam[:, 2:4], in_=lam[:, 0:2], func=AF.Exp), reads=[blam], writes=[blam])
        c.op('dve', lambda e: e.tensor_tensor(out=lam[:, 4:5], in0=lam[:, 3:4], in1=lam[:, 2:3], op=ALU.subtract), reads=[blam], writes=[blam])
        c.op('dve', lambda e: e.tensor_scalar(out=lam[:, 4:5], in0=lam[:, 4:5], scalar1=-lam_init, scalar2=None, op0=ALU.add), reads=[blam], writes=[blam])
        c.dma('sp', lambda e: e.dma_start(out=gO[:], in_=out_gain.partition_broadcast(128)), writes=[bgO])
        c.op('dve', lambda e: e.tensor_scalar(out=gO[:], in0=gO[:], scalar1=1.0 - lam_init, scalar2=None, op0=ALU.mult), reads=[bgO], writes=[bgO])
        c.dma('sp', lambda e: e.dma_start(out=tab[:], in_=rel_bias), writes=[btab])
        c.dma('act', lambda e: e.dma_start(out=oh[:], in_=onehot), writes=[boh])
        ps6, bps6 = g.ps[6]
        c.op('pe', lambda e: e.matmul(ps6[0:4, :], lhsT=tab[:, :], rhs=oh[:, :], start=True, stop=True), reads=[btab, boh], writes=[bps6])
        c.op('dve', lambda e: e.tensor_copy(out=fv[:], in_=ps6[0:4, :]), reads=[bps6], writes=[bfv])
        c.dma('sp', lambda e: e.dma_start(out=FV, in_=fv[:]), reads=[bfv], writes=[bFV])
        for h in range(4):
            for o in (-1, 0, 1):
                off = h * 512 + 128 * o + 128
                src = bass.AP(FV.tensor, off, [[1, 128], [1, 128]])
                c.dma('sp', lambda e: e.dma_start(out=hank[:], in_=src), reads=[bFV], writes=[bhank])
                c.op('dve', lambda e: e.tensor_copy(out=biasT[:, h, o + 1, :], in_=hank[:, ::-1]), reads=[bhank, bbias], writes=[bbias])
            c.dma('sp', lambda e: e.dma_start(out=cfar[:, h, 0:1], in_=bass.AP(FV.tensor, h * 512 + 0, [[0, 128], [1, 1]])), reads=[bFV], pwrites=[bcfar])
            c.dma('sp', lambda e: e.dma_start(out=cfar[:, h, 1:2], in_=bass.AP(FV.tensor, h * 512 + 510, [[0, 128], [1, 1]])), reads=[bFV], pwrites=[bcfar])
        bcfar.seal()
        ones_col_done = False
        nS = 0; nP = 0; nq = 0; ntmp = 0; nout = 0
        for h in range(4):
            c.dma('sp', lambda e: e.dma_start(out=KT[:], in_=QKT[4 + h, :, :]), reads=[bQKT], writes=[bKT])
            for half in range(2):
                c.dma('act', lambda e: e.dma_start(out=Va[:, half * 32:(half + 1) * 32, 0:128], in_=PV[half * 4096:(half + 1) * 4096, 1024 + h * 128:1024 + (h + 1) * 128].rearrange("(b p) d -> p b d", p=128)),
                      reads=[bPV], writes=[bVa])
            c.op('pool', lambda e: e.memset(Va[:, :, 128:129], 1.0), reads=[bVa], writes=[bVa])
            for qt in range(16):
                QT_, bQT_ = QT[nq % 2], bQT[nq % 2]; nq += 1
                c.dma('sp', lambda e: e.dma_start(out=QT_[:], in_=QKT[h, :, qt * 512:(qt + 1) * 512]), reads=[bQKT], writes=[bQT_])
                steps = [(comp, kb) for kb in range(64) for comp in range(2)]
                Sbank = {}

                SB = [0, 1, 2, 6]

                def emit_S(i):
                    comp, kb = steps[i]
                    S, bS = g.ps[SB[i % 4]]
                    c.op('pe', lambda e: e.matmul(S[:, :], lhsT=KT[64 * comp:64 * comp + 64, kb * 128:(kb + 1) * 128], rhs=QT_[64 * comp:64 * comp + 64, :], start=True, stop=True),
                         reads=[bKT, bQT_], writes=[bS])

                def emit_exp(i):
                    comp, kb = steps[i]
                    S, bS = g.ps[SB[i % 4]]
                    P_, bP_ = Pt[i % 4], bPt[i % 4]
                    near = (4 * qt - 1 <= kb <= 4 * qt + 4)
                    if not near:
                        col = cfar[:, h, 0:1] if kb < 4 * qt else cfar[:, h, 1:2]
                        c.op('act', lambda e: e.activation(out=P_[:], in_=S[:, :], func=AF.Exp, bias=col, scale=1.0), reads=[bS, bcfar], writes=[bP_])
                    else:
                        tm, btm = tmp[i % 2], btmp[i % 2]
                        for qs in range(4):
                            o = kb - (4 * qt + qs)
                            sl = slice(qs * 128, (qs + 1) * 128)
                            if abs(o) <= 1:
                                c.op('dve', lambda e: e.tensor_tensor(out=tm[:, sl], in0=S[:, sl], in1=biasT[:, h, o + 1, :], op=ALU.add), reads=[bS, bbias, btm], writes=[btm])
                            else:
                                col = cfar[:, h, 0:1] if o < 0 else cfar[:, h, 1:2]
                                c.op('dve', lambda e: e.tensor_scalar(out=tm[:, sl], in0=S[:, sl], scalar1=col, scalar2=None, op0=ALU.add), reads=[bS, bcfar, btm], writes=[btm])
                        c.op('act', lambda e: e.activation(out=P_[:], in_=tm[:], func=AF.Exp), reads=[btm], writes=[bP_])

                def emit_PV(i):
                    comp, kb = steps[i]
                    P_, bP_ = Pt[i % 4], bPt[i % 4]
                    for qs in range(4):
                        a = comp * 4 + qs
                        acc, bacc = g.ps[3 + a // 3]
                        c0 = (a % 3) * 130
                        first = (kb == 0) and ((comp == 0 and a in (0, 3)) or (comp == 1 and a == 6))
                        c.op('pe', lambda e: e.matmul(acc[:, c0:c0 + 129], lhsT=P_[:, qs * 128:(qs + 1) * 128], rhs=Va[:, kb, 0:129], start=first, stop=(kb == 63), skip_group_check=True),
                             reads=[bP_, bVa], writes=[bacc])
                npair = len(steps) // 2
                emit_S(0); emit_S(1); emit_S(2); emit_S(3)
                for j in range(npair):
                    emit_exp(2 * j); emit_exp(2 * j + 1)
                    if j + 2 < npair:
                        emit_S(2 * j + 4); emit_S(2 * j + 5)
                    emit_PV(2 * j); emit_PV(2 * j + 1)
                os_, bos_ = ost[nout % 2], bost[nout % 2]; nout += 1
                for qs in range(4):
                    a0 = qs; a1 = 4 + qs
                    acc0, bacc0 = g.ps[3 + a0 // 3]; o0 = (a0 % 3) * 130
                    acc1, bacc1 = g.ps[3 + a1 // 3]; o1 = (a1 % 3) * 130
                    rs_, brs_ = rs[qs % 2], brs[qs % 2]
                    wb_, bwb_ = wb[qs % 2], bwb[qs % 2]
                    c.op('dve', lambda e: e.reciprocal(out=rs_[:, 0:1], in_=acc0[:, o0 + 128:o0 + 129]), reads=[bacc0], writes=[brs_])
                    c.op('dve', lambda e: e.reciprocal(out=rs_[:, 1:2], in_=acc1[:, o1 + 128:o1 + 129]), reads=[bacc1, brs_], writes=[brs_])
                    c.op('dve', lambda e: e.tensor_tensor(out=rs_[:, 1:2], in0=rs_[:, 1:2], in1=lam[:, 4:5], op=ALU.mult), reads=[brs_, blam], writes=[brs_])
                    c.op('dve', lambda e: e.tensor_scalar(out=t1[:], in0=acc1[:, o1:o1 + 128], scalar1=rs_[:, 1:2], scalar2=None, op0=ALU.mult), reads=[bacc1, brs_], writes=[bt1])
                    c.op('dve', lambda e: e.scalar_tensor_tensor(out=w_[:], in0=acc0[:, o0:o0 + 128], scalar=rs_[:, 0:1], in1=t1[:], op0=ALU.mult, op1=ALU.add), reads=[bacc0, brs_, bt1], writes=[bw_])
                    c.op('dve', lambda e: e.scalar_tensor_tensor(out=junk[:], in0=w_[:], scalar=1.0, in1=w_[:], op0=ALU.mult, op1=ALU.mult, accum_out=rs_[:, 2:3]), reads=[bw_, brs_], writes=[bjunk, brs_])
                    c.op('dve', lambda e: e.tensor_scalar(out=rs_[:, 2:3], in0=rs_[:, 2:3], scalar1=1.0 / 128, scalar2=1e-6, op0=ALU.mult, op1=ALU.add), reads=[brs_], writes=[brs_])
                    c.op('pool', lambda e: e.tensor_tensor(out=rs_[:, 2:3], in0=rs_[:, 2:3], in1=g.neghalf[:, 0:1], op=ALU.pow), reads=[brs_, g.b_neghalf], writes=[brs_])
                    c.op('dve', lambda e: e.scalar_tensor_tensor(out=wb_[:], in0=w_[:], scalar=rs_[:, 2:3], in1=gO[:], op0=ALU.mult, op1=ALU.mult), reads=[bw_, brs_, bgO], writes=[bwb_])
                    c.op('pe', lambda e: e.transpose(out=ptb[:, qs * 128:(qs + 1) * 128], in_=wb_[:], identity=g.identb[:]), reads=[bwb_, g.b_identb], writes=[bptb])
                c.op('act', lambda e: e.activation(out=os_[:], in_=ptb[:, 0:512], func=AF.Copy), reads=[bptb], writes=[bos_])
                c.dma('sp', lambda e: e.dma_start(out=MT[h * 128:(h + 1) * 128, qt * 512:(qt + 1) * 512], in_=os_[:]), reads=[bos_], pwrites=[bMT])
        barrier(c)


GTB = 512


def gdn_phase(c, g, PT, bPT, PV, bPV, conv_w, a_log, dt_bias, out_gain, GQ, bGQ, GR, bGR, OD, bODs, OD2, bOD2s, MT, bMT, stage=4):
    nc = c.nc
    ptb, bptb = g.psb
    NB = TBK
    with ExitStack() as es:
        def T(name, shape, dt):
            return es.enter_context(nc.sbuf_tensor(uniq(name), shape, dt))
        cw = T("gd_cw", [128, 12, 5], F32); bcw = Buf()
        onesb = T("gd_onesb", [128, 128], BF16); bonesb = Buf()
        xin = [T("gd_xin%d" % i, [128, NB + 4], F32) for i in range(2)]; bxin = [Buf(), Buf()]
        y = T("gd_y", [128, NB], F32); by = Buf()
        s = T("gd_s", [128, NB], F32); bs = Buf()
        sqb = T("gd_sqb", [128, NB], BF16); bsqb = Buf()
        rst = T("gd_rst", [128, NB], F32); brst = Buf()
        ob = [T("gd_ob%d" % i, [128, NB], BF16) for i in range(2)]; bob = [Buf(), Buf()]
        with nc.allow_non_contiguous_dma(reason="small params"):
            for j in range(5):
                c.dma('sp', lambda e: e.dma_start(out=cw[:, :, j], in_=conv_w[j, :].rearrange("(k p) -> p k", p=128)), pwrites=[bcw])
        bcw.seal()
        c.op('pool', lambda e: e.memset(onesb[:], 1.0), writes=[bonesb])
        n = 0
        for cbk in range(12):
            for tb in range(L // NB):
                x_, bx_ = xin[n % 2], bxin[n % 2]
                o_, bo_ = ob[n % 2], bob[n % 2]
                n += 1
                t0 = tb * NB
                lo = max(t0 - 2, 0); hi = min(t0 + NB + 2, L)
                if tb == 0:
                    c.op('pool', lambda e: e.memset(x_[:, 0:2], 0.0), writes=[bx_])
                if tb == L // NB - 1:
                    c.op('pool', lambda e: e.memset(x_[:, NB + 2:NB + 4], 0.0), writes=[bx_])
                c.dma('sp', lambda e: e.dma_start(out=x_[:, lo - (t0 - 2):hi - (t0 - 2)], in_=PT[cbk * 128:(cbk + 1) * 128, lo:hi]), reads=[bPT, bx_], writes=[bx_])
                c.op('dve', lambda e: e.tensor_scalar(out=y[:], in0=x_[:, 0:NB], scalar1=cw[:, cbk, 0:1], scalar2=None, op0=ALU.mult), reads=[bx_, bcw], writes=[by])
                for j in range(1, 5):
                    c.op('dve', lambda e: e.scalar_tensor_tensor(out=y[:], in0=x_[:, j:j + NB], scalar=cw[:, cbk, j:j + 1], in1=y[:], op0=ALU.mult, op1=ALU.add),
                         reads=[bx_, bcw, by], writes=[by])
                c.op('act', lambda e: e.activation(out=s[:], in_=y[:], func=AF.Silu), reads=[by], writes=[bs])
                if cbk < 8:
                    c.op('act', lambda e: e.activation(out=sqb[:], in_=s[:], func=AF.Square), reads=[bs], writes=[bsqb])
                    for hf in range(NB // 512):
                        ps, bps = g.ps[hf % 4]
                        c.op('pe', lambda e: e.matmul(ps[:, :], lhsT=onesb[:], rhs=sqb[:, hf * 512:(hf + 1) * 512], start=True, stop=True), reads=[bonesb, bsqb], writes=[bps])
                        c.op('dve', lambda e: e.tensor_scalar(out=rst[:, hf * 512:(hf + 1) * 512], in0=ps[:, :], scalar1=1e-6, scalar2=None, op0=ALU.add), reads=[bps, brst], writes=[brst])
                    c.op('act', lambda e: e.activation(out=rst[:], in_=rst[:], func=AF.Ln), reads=[brst], writes=[brst])
                    c.op('act', lambda e: e.activation(out=rst[:], in_=rst[:], func=AF.Exp, scale=-0.5), reads=[brst], writes=[brst])
                    sc = (128.0 ** -0.5) if cbk < 4 else 1.0
                    c.op('dve', lambda e: e.scalar_tensor_tensor(out=o_[:], in0=s[:], scalar=sc, in1=rst[:], op0=ALU.mult, op1=ALU.mult), reads=[bs, brst], writes=[bo_])
                else:
                    c.op('act', lambda e: e.activation(out=o_[:], in_=s[:], func=AF.Copy), reads=[bs], writes=[bo_])
                c.dma('act', lambda e: e.dma_start(out=GQ[cbk, :, t0:t0 + NB], in_=o_[:]), reads=[bo_], pwrites=[bGQ])
        bGQ.seal()
        barrier(c)
    if stage < 2:
        return
    with ExitStack() as es0:
        def T0(name, shape, dt):
            return es0.enter_context(nc.sbuf_tensor(uniq(name), shape, dt))
        NQ = 5
        cols = [T0("gd_cols%d" % d, [128, 64, 4 * NQ], F32) for d in range(2)]; bcols = [Buf(), Buf()]
        sel = T0("gd_sel", [4, 4, 128], F32); bsel = Buf()
        with ExitStack() as es:
            def T(name, shape, dt):
                return es.enter_context(nc.sbuf_tensor(uniq(name), shape, dt))
            GP = 2048
            ar = T("gd_ar", [4, GP], F32); bar_ = Buf()
            br = T("gd_br", [4, GP], F32); bbr = Buf()
            w1 = T("gd_w1", [4, GP], F32); bw1 = Buf()
            w2 = T("gd_w2", [4, GP], F32); bw2 = Buf()
            gam = T("gd_gam", [4, GP], F32); bet = T("gd_bet", [4, GP], F32); egam = T("gd_egam", [4, GP], F32); brw = Buf()
            q3 = T("gd_q3", [4, GP], F32); bq3 = Buf()
            q4 = T("gd_q4", [4, GP], F32); bq4 = Buf()
            q5 = T("gd_q5", [4, GP], F32); bq5 = Buf()
            msk = T("gd_msk", [4, GP], F32); bmsk = Buf()
            pc = T("gd_pc", [4, 4], F32); bpc = Buf()
            c.op('pool', lambda e: e.memset(msk[:], 1.0), writes=[bmsk])
            c.op('pool', lambda e: e.memset(msk[:].rearrange("p (c j) -> p c j", j=64)[:, :, 0:1], 0.0), reads=[bmsk], writes=[bmsk])
            c.op('pool', lambda e: e.memset(sel[:], 0.0), writes=[bsel])
            c.op('pool', lambda e: e.affine_select(out=sel[:], in_=sel[:], pattern=[[-1, 4], [0, 128]], compare_op=ALU.not_equal, fill=1.0, base=0, channel_multiplier=1),
                 reads=[bsel], writes=[bsel])
            for d in range(2):
                with nc.allow_non_contiguous_dma(reason="small params"):
                    c.dma('sp', lambda e: e.dma_start(out=pc[:, 0:1], in_=dt_bias[d, :].rearrange("(h o) -> h o", o=1)), reads=[bpc], writes=[bpc])
                    c.dma('sp', lambda e: e.dma_start(out=pc[:, 1:2], in_=a_log[d, :].rearrange("(h o) -> h o", o=1)), reads=[bpc], writes=[bpc])
                c.op('act', lambda e: e.activation(out=pc[:, 2:3], in_=pc[:, 1:2], func=AF.Exp), reads=[bpc], writes=[bpc])
                c.op('dve', lambda e: e.tensor_scalar(out=pc[:, 2:3], in0=pc[:, 2:3], scalar1=-1.0, scalar2=None, op0=ALU.mult), reads=[bpc], writes=[bpc])
                for tp in range(L // GP):
                    nbp = tp if d == 0 else L // GP - 1 - tp
                    c.dma('sp', lambda e: e.dma_start(out=ar[:], in_=PT[1536 + 4 * d:1540 + 4 * d, nbp * GP:(nbp + 1) * GP]), reads=[bPT, bar_], writes=[bar_])
                    c.dma('act', lambda e: e.dma_start(out=br[:], in_=PT[1544 + 4 * d:1548 + 4 * d, nbp * GP:(nbp + 1) * GP]), reads=[bPT, bbr], writes=[bbr])
                    asrc = ar[:, ::-1] if d else ar[:, :]
                    bsrc = br[:, ::-1] if d else br[:, :]
                    c.op('dve', lambda e: e.tensor_scalar(out=w1[:], in0=asrc, scalar1=pc[:, 0:1], scalar2=None, op0=ALU.add), reads=[bar_, bpc], writes=[bw1])
                    c.op('dve', lambda e: e.tensor_scalar(out=w2[:], in0=w1[:], scalar1=-1.0, scalar2=None, op0=ALU.mult), reads=[bw1], writes=[bw2])
                    c.op('dve', lambda e: e.tensor_tensor(out=w2[:], in0=w2[:], in1=w1[:], op=ALU.min), reads=[bw1, bw2], writes=[bw2])
                    c.op('act', lambda e: e.activation(out=w2[:], in_=w2[:], func=AF.Exp), reads=[bw2], writes=[bw2])
                    c.op('act', lambda e: e.activation(out=w2[:], in_=w2[:], func=AF.Ln, bias=1.0, scale=1.0), reads=[bw2], writes=[bw2])
                    c.op('dve', lambda e: e.scalar_tensor_tensor(out=w1[:], in0=w1[:], scalar=0.0, in1=w2[:], op0=ALU.max, op1=ALU.add), reads=[bw1, bw2], writes=[bw1])
                    c.op('dve', lambda e: e.tensor_scalar(out=w1[:], in0=w1[:], scalar1=pc[:, 2:3], scalar2=None, op0=ALU.mult), reads=[bw1, bpc], writes=[bw1])
                    c.op('dve', lambda e: e.tensor_tensor_scan(out=gam[:], data0=msk[:], data1=w1[:], initial=0.0, op0=ALU.mult, op1=ALU.add), reads=[bmsk, bw1, brw], writes=[brw])
                    c.op('act', lambda e: e.activation(out=bet[:], in_=bsrc, func=AF.Sigmoid), reads=[bbr, brw], writes=[brw])
                    c.op('act', lambda e: e.activation(out=egam[:], in_=gam[:], func=AF.Exp), reads=[brw], writes=[brw])
                    c.op('dve', lambda e: e.tensor_tensor(out=q3[:], in0=bet[:], in1=egam[:], op=ALU.mult), reads=[brw, bq3], writes=[bq3])
                    g3 = gam[:].rearrange("p (c j) -> p c j", j=64)
                    c.op('dve', lambda e: e.tensor_tensor(out=q4[:].rearrange("p (c j) -> p c j", j=64), in0=g3[:, :, 63:64].broadcast_to([4, GP // 64, 64]), in1=g3, op=ALU.subtract),
                         reads=[brw, bq4], writes=[bq4])
                    c.op('act', lambda e: e.activation(out=q4[:], in_=q4[:], func=AF.Exp), reads=[bq4], writes=[bq4])
                    c.op('dve', lambda e: e.tensor_scalar(out=q5[:], in0=gam[:], scalar1=-1.0, scalar2=None, op0=ALU.mult), reads=[brw, bq5], writes=[bq5])
                    quants = [(gam, brw), (bet, brw), (q3, bq3), (q4, bq4), (q5, bq5)]
                    for bl in range(GP // 128):
                        blk = tp * (GP // 128) + bl
                        pc_, bpc_ = g.ps[blk % 2]
                        for qi, (qt_, bq_) in enumerate(quants):
                            c.op('pe', lambda e: e.transpose(out=pc_[:, qi * 4:(qi + 1) * 4], in_=qt_[0:4, bl * 128:(bl + 1) * 128], identity=g.ident32[0:4, 0:4]),
                                 reads=[bq_, g.b_ident32], writes=[bpc_])
                        c.op('act', lambda e: e.activation(out=cols[d][:, blk, :], in_=pc_[:, 0:4 * NQ], func=AF.Copy), reads=[bpc_, bcols[d]], writes=[bcols[d]])
                    for qi, rt in enumerate((gam, bet, egam)):
                        c.dma('sp', lambda e: e.dma_start(out=GR[d, qi, :, tp * GP:(tp + 1) * GP], in_=rt[:]), reads=[brw], pwrites=[bGR])
            bGR.seal()
            barrier(c)
        if stage < 3:
            return
        with ExitStack() as es:
            def T(name, shape, dt):
                return es.enter_context(nc.sbuf_tensor(uniq(name), shape, dt))
            nm_le = T("gm_nmle", [128, 128], F32)
            nm_geT = T("gm_nmgeT", [128, 128], F32)
            m_stT = T("gm_mstT", [128, 128], F32)
            bmk = Buf()
            c.op('pool', lambda e: e.memset(nm_le[:], 0.0), writes=[bmk])
            c.op('pool', lambda e: e.affine_select(out=nm_le[:], in_=nm_le[:], pattern=[[-1, 128]], compare_op=ALU.is_gt, fill=-30000.0, base=0, channel_multiplier=1), reads=[bmk], writes=[bmk])
            c.op('pool', lambda e: e.memset(nm_le[64:128, 0:64], -30000.0), reads=[bmk], writes=[bmk])
            c.op('pool', lambda e: e.memset(nm_geT[:], 0.0), reads=[bmk], writes=[bmk])
            c.op('pool', lambda e: e.affine_select(out=nm_geT[:], in_=nm_geT[:], pattern=[[1, 128]], compare_op=ALU.is_ge, fill=-30000.0, base=0, channel_multiplier=-1), reads=[bmk], writes=[bmk])
            c.op('pool', lambda e: e.memset(nm_geT[0:64, 64:128], -30000.0), reads=[bmk], writes=[bmk])
            c.op('pool', lambda e: e.memset(m_stT[:], 1.0), reads=[bmk], writes=[bmk])
            c.op('pool', lambda e: e.affine_select(out=m_stT[:], in_=m_stT[:], pattern=[[1, 128]], compare_op=ALU.is_gt, fill=0.0, base=0, channel_multiplier=-1), reads=[bmk], writes=[bmk])

            class CH:
                pass
            chs = []
            for d in range(2):
                ch = CH(); chs.append(ch)
                ch.d = d

                def TT(name, shape, dt, d=d):
                    return (T("gm%d_%s" % (d, name), shape, dt), Buf())
                ch.nat = [TT("nat%d" % i, [128, GTB], BF16) for i in range(3)]
                ch.arr = [[TT("arr%d_%d" % (i, j), [128, GTB], BF16) for j in range(2)] for i in range(3)]
                ch.rts = [[TT("rt%d_%d" % (q, j), [4, GTB], F32) for j in range(2)] for q in range(3)]
                ch.S32 = TT("S32", [128, 128], F32); ch.Sb = TT("Sb", [128, 128], BF16)
                ch.tmpD = TT("tmpD", [128, 128], F32); ch.Dst = TT("Dst", [128, 128], F32); ch.DTi = TT("DTi", [128, 128], F32); ch.DTs = TT("DTs", [128, 128], F32)
                ch.A_ = TT("A", [128, 128], BF16); ch.AT_ = TT("AT", [128, 128], BF16); ch.atT = TT("attnT", [128, 128], BF16)
                ch.Pm = [TT("P%d" % i, [128, 128], BF16) for i in range(6)]
                ch.Qm = [TT("Q%d" % i, [128, 128], BF16) for i in range(5)]
                ch.W32 = TT("W32", [128, 256], F32); ch.Wb = TT("Wb", [128, 256], BF16)
                ch.kdec = TT("kdec", [128, 128], BF16); ch.kcT = TT("kcT", [128, 128], BF16); ch.qdec = TT("qdec", [128, 128], BF16); ch.vnew = TT("vnew", [128, 128], BF16)
                ch.elc = TT("elc", [128, 2], F32)
                ch.osb = [TT("osb%d" % i, [128, 128], F32) for i in range(2)]
                ch.osf = [TT("osf%d" % i, [128, 128], F32) for i in range(2)]
                ch.bA = g.ps[3 * d + 0]; ch.bB = g.ps[3 * d + 1]; ch.bC = g.ps[3 * d + 2]
                ch.pb0 = 512 * d
                ch.nblk = 0

            def block_gen(ch, h, blk, b, cur, rcur):
                d = ch.d
                (qA, bqA), (kA, bkA), (vA, bvA) = cur
                bs_ = slice(b * 128, (b + 1) * 128)
                cl = cols[d][:, blk, :]
                gcol = cl[:, 0 + h:0 + h + 1]; bcol = cl[:, 4 + h:4 + h + 1]; begcol = cl[:, 8 + h:8 + h + 1]
                ekdcol = cl[:, 12 + h:12 + h + 1]; ngcol = cl[:, 16 + h:16 + h + 1]
                pA, bpA = ch.bA; pB, bpB = ch.bB; pC, bpC = ch.bC
                pb0 = ch.pb0
                tmpD, btmpD = ch.tmpD; Dst, bDst = ch.Dst; DTi, bDTi = ch.DTi; DTs, bDTs = ch.DTs
                A_, bA_ = ch.A_; AT_, bAT_ = ch.AT_; atT, batT = ch.atT
                W32, bW32 = ch.W32; Wb, bWb = ch.Wb; kdec, bkdec = ch.kdec; kcT, bkcT = ch.kcT; qdec, bqdec = ch.qdec; vnew, bvnew = ch.vnew
                elc, belc = ch.elc; S32, bS32 = ch.S32; Sb, bSb = ch.Sb
                for qi, (rt, brt) in enumerate(rcur):
                    c.op('pe', lambda e: e.matmul(pA[:, qi * 128:(qi + 1) * 128], lhsT=sel[:, h, :], rhs=rt[0:4, bs_], start=True, stop=True, skip_group_check=True),
                         reads=[bsel, brt], writes=[bpA])
                    yield
                c.op('pe', lambda e: e.matmul(pB[:, 0:128], lhsT=kA[:, bs_], rhs=kA[:, bs_], start=True, stop=True, skip_group_check=True), reads=[bkA], writes=[bpB]); yield
                c.op('pe', lambda e: e.matmul(pB[:, 128:256], lhsT=kA[:, bs_], rhs=qA[:, bs_], start=True, stop=True, skip_group_check=True), reads=[bkA, bqA], writes=[bpB]); yield
                c.op('pe', lambda e: e.transpose(out=ptb[:, pb0:pb0 + 128], in_=vA[:, bs_], identity=g.identb[:]), reads=[bvA, g.b_identb], writes=[bptb]); yield
                c.op('pe', lambda e: e.transpose(out=ptb[:, pb0 + 128:pb0 + 256], in_=kA[:, bs_], identity=g.identb[:]), reads=[bkA, g.b_identb], writes=[bptb]); yield
                c.op('dve', lambda e: e.scalar_tensor_tensor(out=tmpD[:], in0=pA[:, 0:128], scalar=-1.0, in1=nm_le[:], op0=ALU.mult, op1=ALU.add), reads=[bpA, bmk], writes=[btmpD]); yield
                c.op('act', lambda e: e.activation(out=Dst[:], in_=tmpD[:], func=AF.Exp, bias=gcol, scale=1.0), reads=[btmpD, bcols[d]], writes=[bDst]); yield
                c.op('dve', lambda e: e.tensor_tensor(out=tmpD[:], in0=pA[:, 0:128], in1=nm_geT[:], op=ALU.add), reads=[bpA, bmk, btmpD], writes=[btmpD]); yield
                c.op('act', lambda e: e.activation(out=DTi[:], in_=tmpD[:], func=AF.Exp, bias=ngcol, scale=1.0), reads=[btmpD, bcols[d]], writes=[bDTi]); yield
                c.op('dve', lambda e: e.tensor_tensor(out=DTs[:], in0=DTi[:], in1=m_stT[:], op=ALU.mult), reads=[bDTi, bmk], writes=[bDTs]); yield
                c.op('dve', lambda e: e.tensor_tensor(out=DTs[:], in0=pA[:, 128:256], in1=DTs[:], op=ALU.mult), reads=[bpA, bDTs], writes=[bDTs]); yield
                c.op('dve', lambda e: e.scalar_tensor_tensor(out=A_[:], in0=pB[:, 0:128], scalar=bcol, in1=Dst[:], op0=ALU.mult, op1=ALU.mult), reads=[bpB, bcols[d], bDst], writes=[bA_]); yield
                c.op('dve', lambda e: e.tensor_tensor(out=AT_[:], in0=pB[:, 0:128], in1=DTs[:], op=ALU.mult), reads=[bpB, bDTs], writes=[bAT_]); yield
                c.op('dve', lambda e: e.tensor_tensor(out=atT[:], in0=pB[:, 128:256], in1=DTi[:], op=ALU.mult), reads=[bpB, bDTi], writes=[batT]); yield
                c.op('dve', lambda e: e.tensor_scalar(out=Wb[:, 0:128], in0=ptb[:, pb0:pb0 + 128], scalar1=bcol, scalar2=None, op0=ALU.mult), reads=[bptb, bcols[d], bWb], writes=[bWb]); yield
                c.op('dve', lambda e: e.tensor_scalar(out=Wb[:, 128:256], in0=ptb[:, pb0 + 128:pb0 + 256], scalar1=begcol, scalar2=None, op0=ALU.mult), reads=[bptb, bcols[d], bWb], writes=[bWb]); yield
                c.op('act', lambda e: e.activation(out=kdec[:], in_=ptb[:, pb0 + 128:pb0 + 256], func=AF.Copy, scale=ekdcol), reads=[bptb, bcols[d]], writes=[bkdec]); yield
                c.op('dve', lambda e: e.tensor_tensor(out=qdec[:], in0=pA[:, 256:384], in1=qA[:, bs_], op=ALU.mult), reads=[bpA, bqA], writes=[bqdec]); yield
                c.op('act', lambda e: e.activation(out=elc[:], in_=pA[:, 256:384].rearrange("p (c j) -> p c j", j=64)[:, :, 63], func=AF.Copy), reads=[bpA], writes=[belc]); yield
                Pc, bPc = AT_, bAT_
                Qc, bQc = A_, bA_
                for lev in range(6):
                    c.op('pe', lambda e: e.matmul(pC[:, 0:256], lhsT=Pc[:], rhs=Wb[:], start=True, stop=True, skip_group_check=True), reads=[bPc, bWb], writes=[bpC]); yield
                    c.op('dve', lambda e: e.tensor_tensor(out=Wb[:], in0=Wb[:], in1=pC[:, 0:256], op=(ALU.subtract if lev == 0 else ALU.add)), reads=[bWb, bpC], writes=[bWb]); yield
                    if lev < 5:
                        Pn, bPn = ch.Pm[lev + 1]
                        c.op('pe', lambda e: e.matmul(pB[:, 256:384], lhsT=Qc[:], rhs=Pc[:], start=True, stop=True, skip_group_check=True), reads=[bQc, bPc], writes=[bpB]); yield
                        if lev < 4:
                            Qn, bQn = ch.Qm[lev + 1]
                            c.op('pe', lambda e: e.matmul(pB[:, 384:512], lhsT=Pc[:], rhs=Qc[:], start=True, stop=True, skip_group_check=True), reads=[bQc, bPc], writes=[bpB]); yield
                            c.op('dve', lambda e: e.tensor_copy(out=Qn[:], in_=pB[:, 384:512]), reads=[bpB], writes=[bQn]); yield
                        c.op('act', lambda e: e.activation(out=Pn[:], in_=pB[:, 256:384], func=AF.Copy), reads=[bpB], writes=[bPn]); yield
                        Pc, bPc = Pn, bPn
                        if lev < 4:
                            Qc, bQc = Qn, bQn
                c.op('pe', lambda e: e.transpose(out=ptb[:, pb0 + 256:pb0 + 384], in_=Wb[:, 128:256], identity=g.identb[:]), reads=[bWb, g.b_identb], writes=[bptb]); yield
                c.op('act', lambda e: e.activation(out=kcT[:], in_=ptb[:, pb0 + 256:pb0 + 384], func=AF.Copy), reads=[bptb], writes=[bkcT]); yield
                for ci in range(2):
                    r0 = 64 * ci
                    c.op('pe', lambda e: e.matmul(pC[r0:r0 + 64, 256:384], lhsT=kcT[:, r0:r0 + 64], rhs=Sb[:, :], start=True, stop=True, skip_group_check=True), reads=[bkcT, bSb], writes=[bpC]); yield
                    c.op('dve', lambda e: e.tensor_tensor(out=vnew[r0:r0 + 64, :], in0=Wb[r0:r0 + 64, 0:128], in1=pC[r0:r0 + 64, 256:384], op=ALU.subtract), reads=[bWb, bpC, bvnew], writes=[bvnew]); yield
                    c.op('pe', lambda e: e.matmul(pA[r0:r0 + 64, 384:512], lhsT=qdec[:, r0:r0 + 64], rhs=Sb[:, :], start=True, stop=False, skip_group_check=True), reads=[bqdec, bSb], writes=[bpA])
                    c.op('pe', lambda e: e.matmul(pA[r0:r0 + 64, 384:512], lhsT=atT[r0:r0 + 64, r0:r0 + 64], rhs=vnew[r0:r0 + 64, :], start=False, stop=True, skip_group_check=True), reads=[batT, bvnew], writes=[bpA]); yield
                    c.op('pe', lambda e: e.matmul(pC[:, 384:512], lhsT=kdec[r0:r0 + 64, :], rhs=vnew[r0:r0 + 64, :], start=True, stop=True, skip_group_check=True), reads=[bkdec, bvnew], writes=[bpC]); yield
                    c.op('dve', lambda e: e.scalar_tensor_tensor(out=Sb[:], in0=S32[:], scalar=elc[:, ci:ci + 1], in1=pC[:, 384:512], op0=ALU.mult, op1=ALU.add), reads=[bS32, belc, bpC], writes=[bSb]); yield
                    c.op('dve', lambda e: e.scalar_tensor_tensor(out=S32[:], in0=S32[:], scalar=elc[:, ci:ci + 1], in1=pC[:, 384:512], op0=ALU.mult, op1=ALU.add), reads=[bS32, belc, bpC], writes=[bS32]); yield
                os_, bos_ = ch.osb[ch.nblk % 2]
                of_, bof_ = ch.osf[ch.nblk % 2]
                ch.nblk += 1
                c.op('act', lambda e: e.activation(out=os_[:], in_=pA[:, 384:512], func=AF.Copy), reads=[bpA], writes=[bos_]); yield
                if d == 0:
                    c.dma('sp', lambda e: e.dma_start(out=OD[blk * 128:(blk + 1) * 128, h * 128:(h + 1) * 128], in_=os_[:]), reads=[bos_], pwrites=[bODs[h]]); yield
                else:
                    pf, bpf = g.ps[6]
                    c.op('pe', lambda e: e.matmul(pf[:, 0:128], lhsT=g.J32[:], rhs=os_[:], start=True, stop=True), reads=[g.b_J32, bos_], writes=[bpf]); yield
                    c.op('act', lambda e: e.activation(out=of_[:], in_=pf[:, 0:128], func=AF.Copy), reads=[bpf], writes=[bof_]); yield
                    c.dma('act', lambda e: e.dma_start(out=OD2[L - (blk + 1) * 128:L - blk * 128, h * 128:(h + 1) * 128], in_=of_[:]), reads=[bof_], pwrites=[bOD2s[h]]); yield

            for h in range(4):
                for ch in chs:
                    S32, bS32 = ch.S32; Sb, bSb = ch.Sb
                    c.op('pool', lambda e: e.memset(S32[:], 0.0), reads=[bS32], writes=[bS32])
                    c.op('pool', lambda e: e.memset(Sb[:], 0.0), reads=[bSb], writes=[bSb])
                for tb in range(L // GTB):
                    curs = []; rcurs = []
                    for ch in chs:
                        d = ch.d
                        cur = []
                        for ai in range(3):
                            a_, ba_ = ch.arr[ai][tb % 2]
                            if d == 0:
                                c.dma('sp' if ai % 2 else 'act', lambda e: e.dma_start(out=a_[:], in_=GQ[ai * 4 + h, :, tb * GTB:(tb + 1) * GTB]), reads=[bGQ], writes=[ba_])
                            else:
                                n_, bn_ = ch.nat[ai]
                                c.dma('sp' if ai % 2 else 'act', lambda e: e.dma_start(out=n_[:], in_=GQ[ai * 4 + h, :, L - (tb + 1) * GTB:L - tb * GTB]), reads=[bGQ], writes=[bn_])
                                c.op('pool', lambda e: e.tensor_copy(out=a_[:], in_=n_[:, ::-1]), reads=[bn_], writes=[ba_])
                            cur.append((a_, ba_))
                        rcur = []
                        for qi in range(3):
                            r_, br_ = ch.rts[qi][tb % 2]
                            c.dma('sp', lambda e: e.dma_start(out=r_[:], in_=GR[d, qi, :, tb * GTB:(tb + 1) * GTB]), reads=[bGR], writes=[br_])
                            rcur.append((r_, br_))
                        curs.append(cur); rcurs.append(rcur)
                    for b in range(GTB // 128):
                        blk = tb * (GTB // 128) + b
                        gens = [block_gen(ch, h, blk, b, curs[i], rcurs[i]) for i, ch in enumerate(chs)]
                        alive = list(gens)
                        while alive:
                            for gnr in list(alive):
                                try:
                                    next(gnr)
                                except StopIteration:
                                    alive.remove(gnr)
                bODs[h].seal(); bOD2s[h].seal()
            barrier(c)
    if stage < 4:
        return
    gated_norm_finalize(c, g, OD, bODs, PV, bPV, 1536, out_gain, MT, bMT, 512, "gf_", OA2=OD2, bOA2s=bOD2s)


N_ACTIVE = 4
DEPTH = 4

PARAM_NAMES = ['mix_norm', 'ffn_norm', 'ev_w_in', 'ev_w_out', 'a_lb_logits', 'a_out_norm', 's5_lambda_re', 's5_lambda_im',
               's5_log_step', 's5_b_re', 's5_b_im', 's5_c_re', 's5_c_im', 's5_d', 's5_glu_w', 's5_glu_b', 'od_w_in', 'od_w_out',
               'c_q_norm', 'c_k_norm', 'c_lambda', 'c_out_norm', 'rel_bias', 'd_conv_w', 'd_a_log', 'd_dt_bias', 'd_out_norm',
               'moe_router', 'moe_w_gate', 'moe_w_up', 'moe_w_down']

PARAM_SHAPES = {
    'mix_norm': (4, 1024), 'ffn_norm': (4, 1024), 'ev_w_in': (2, 1024, 3072), 'ev_w_out': (2, 1024, 1024), 'a_lb_logits': (2, 2, 512),
    'a_out_norm': (2, 128), 's5_lambda_re': (2, 2, 32, 64), 's5_lambda_im': (2, 2, 32, 64), 's5_log_step': (2, 2, 32),
    's5_b_re': (2, 2, 32, 64, 16), 's5_b_im': (2, 2, 32, 64, 16), 's5_c_re': (2, 2, 32, 16, 64), 's5_c_im': (2, 2, 32, 16, 64),
    's5_d': (2, 512), 's5_glu_w': (2, 512, 512), 's5_glu_b': (2, 512), 'od_w_in': (2, 1024, 3600), 'od_w_out': (2, 1024, 1024),
    'c_q_norm': (2, 64), 'c_k_norm': (2, 64), 'c_lambda': (2, 4, 64), 'c_out_norm': (2, 128), 'rel_bias': (32, 4),
    'd_conv_w': (2, 5, 1536), 'd_a_log': (2, 2, 4), 'd_dt_bias': (2, 2, 4), 'd_out_norm': (2, 128), 'moe_router': (4, 1024, 16),
    'moe_w_gate': (4, 16, 1024, 2048), 'moe_w_up': (4, 16, 1024, 2048), 'moe_w_down': (4, 16, 2048, 1024)}


def build_program(layers=range(DEPTH), do_mixer=True, do_moe=True):
    nc = bass.Bass('TRN2', target_bir_lowering=False)
    xin = nc.dram_tensor("x", [L, D], F32, kind="ExternalInput").ap()
    P = {n: nc.dram_tensor(n, list(PARAM_SHAPES[n]), F32, kind="ExternalInput").ap() for n in PARAM_NAMES}
    onehot = nc.dram_tensor("t5_onehot", [32, 512], F32, kind="ExternalInput").ap()
    X = nc.dram_tensor("y", [L, D], F32, kind="ExternalOutput").ap()
    PT = nc.dram_tensor("PT", [2048, L], F32).ap()
    PV = nc.dram_tensor("PV", [L, 2048], BF16).ap()
    QK = nc.dram_tensor("QK", [16, 128, L], BF16).ap()
    QK5 = QK.rearrange("(h r w) p t -> h r w p t", h=4, r=2)
    OA = nc.dram_tensor("OA", [L, 512], F32).ap()
    YT = nc.dram_tensor("YT", [512, L], F32).ap()
    MT = nc.dram_tensor("MT", [1024, L], BF16).ap()
    HB = nc.dram_tensor("HB", [L, D], BF16).ap()
    GQ = nc.dram_tensor("GQ", [12, 128, L], BF16).ap()
    GR = nc.dram_tensor("GR", [2, 3, 4, L], F32).ap()
    OD2 = nc.dram_tensor("OD2", [L, 512], F32).ap()
    FV = nc.dram_tensor("FV", [4, 512], F32).ap()
    c = Ctx(nc); g = G()
    setup_consts(c, g)
    bX = Buf('X'); bPT = Buf(); bPV = Buf(); bQK = Buf(); bOAs = [Buf() for _ in range(4)]; bYT = Buf(); bMT = Buf()
    bHB = Buf(); bGQ = Buf(); bGR = Buf(); bFV = Buf(); bOD2s = [Buf() for _ in range(4)]
    for r in range(0, L, 512):
        c.dma('sp', lambda e: e.dma_start(out=X[r:r + 512, :], in_=xin[r:r + 512, :]), pwrites=[bX])
    bX.seal()
    for layer in layers:
        j = layer // 2
        if do_mixer:
            if layer % 2 == 0:
                spec = [(0, 512, 'F', 0), (512, 512, 'F', 512), (1024, 512, 'F', 1024), (2560, 512, 'F', 1536), (1536, 512, 'T', 0), (2048, 512, 'T', 512)]
                proj_phase(c, g, X, bX, P['mix_norm'][layer], P['ev_w_in'][j], 3072, spec, PT, bPT, PV, bPV)
                hgrn2_phase(c, g, PT, bPT, PV, bPV, P['a_lb_logits'], j, P['a_out_norm'][j], QK5, bQK, OA, bOAs, OD2, bOD2s, MT, bMT)
                s5_phase(c, g, PT, bPT, P['s5_lambda_re'][j], P['s5_lambda_im'][j], P['s5_log_step'][j], P['s5_b_re'][j], P['s5_b_im'][j],
                         P['s5_c_re'][j], P['s5_c_im'][j], P['s5_d'][j], P['s5_glu_w'][j], P['s5_glu_b'][j], YT, bYT, MT, bMT)
                bMT.seal()
                wo = P['ev_w_out'][j]
            else:
                spec = [(0, 512, 'T', 0), (512, 512, 'T', 512), (1024, 512, 'T', 1024), (1536, 1536, 'F', 0), (3072, 16, 'F', 1536), (3088, 512, 'T', 1536)]
                proj_phase(c, g, X, bX, P['mix_norm'][layer], P['od_w_in'][j], 3600, spec, PT, bPT, PV, bPV)
                attn_phase(c, g, PV, bPV, P['c_q_norm'][j], P['c_k_norm'][j], P['c_lambda'][j], P['c_out_norm'][j], P['rel_bias'], onehot, layer,
                           QK, bQK, FV, bFV, MT, bMT)
                gdn_phase(c, g, PT, bPT, PV, bPV, P['d_conv_w'][j], P['d_a_log'][j], P['d_dt_bias'][j], P['d_out_norm'][j], GQ, bGQ, GR, bGR, OA, bOAs, OD2, bOD2s, MT, bMT)
                bMT.seal()
                wo = P['od_w_out'][j]
            bX = outproj_moe(c, g, MT, bMT, wo, X, bX, HB, bHB, P['ffn_norm'][layer], P['moe_router'][layer], P['moe_w_gate'][layer], P['moe_w_up'][layer], P['moe_w_down'][layer])
    barrier(c)
    c.finish([bX])
    return nc, c


def kernel(**inputs):
    x = np.ascontiguousarray(np.asarray(inputs['x'], dtype=np.float32))
    B = x.shape[0]
    assert B == N_ACTIVE and x.shape[1] == L and x.shape[2] == D
    nc, c = build_program()
    params = {n: np.ascontiguousarray(np.asarray(inputs[n], dtype=np.float32)) for n in PARAM_NAMES}
    oh = t5_onehot()
    in_maps = []
    for b in range(N_ACTIVE):
        m = {"x": x[b], "t5_onehot": oh}
        m.update(params)
        in_maps.append(m)
    res = run_bass_kernel_spmd(nc, in_maps, core_ids=list(range(N_ACTIVE)))
    out = np.stack([np.asarray(res.results[b]["y"], dtype=np.float32) for b in range(N_ACTIVE)], axis=0)
    return out
```

```python
import math
from contextlib import ExitStack

import numpy as np
import concourse.bass as bass
import concourse.mybir as mybir
from concourse.bass_utils import run_bass_kernel_spmd

F32 = mybir.dt.float32
BF16 = mybir.dt.bfloat16
U32 = mybir.dt.uint32
I32 = mybir.dt.int32
AF = mybir.ActivationFunctionType
ALU = mybir.AluOpType
AX = mybir.AxisListType


class Buf:
    __slots__ = ("name", "w", "r", "pw", "psum")

    def __init__(self, name="", psum=False):
        self.name = name
        self.psum = psum
        self.w = {}
        self.r = {}
        self.pw = {}

    def seal(self):
        for k, v in self.pw.items():
            if self.w.get(k, 0) < v:
                self.w[k] = v
        self.pw = {}


class Ctx:
    NDMA = 8

    def __init__(self, nc, same_engine_sync=True):
        self.nc = nc
        self.E = dict(pe=nc.tensor, dve=nc.vector, act=nc.scalar, pool=nc.gpsimd, sp=nc.sync)
        self.sem = {}
        self.cnt = {}
        for k in self.E:
            self.sem[k] = nc.alloc_semaphore("c_" + k)
            self.cnt[k] = 0
        self.dslot = {}
        for q in ("sp", "act", "pool"):
            for i in range(self.NDMA):
                key = "d_%s%d" % (q, i)
                self.sem[key] = nc.alloc_semaphore(key)
                self.cnt[key] = 0
            self.dslot[q] = 0
        self.seen = {k: {} for k in self.E}
        self.same = same_engine_sync
        self.ninst = 0

    def _wait(self, eng, tok):
        if tok is None:
            return
        key, val = tok
        if key == eng and (eng == "pe" or not self.same):
            return
        if self.seen[eng].get(key, 0) >= val:
            return
        self.E[eng].wait_ge(self.sem[key], val)
        self.seen[eng][key] = val

    def _deps(self, eng, reads, writes, pwrites=()):
        for b in reads:
            for k, v in b.w.items():
                self._wait(eng, (k, v))
            for k, v in b.pw.items():
                self._wait(eng, (k, v))
            if b.psum:
                for k, v in b.r.items():
                    if k != eng:
                        self._wait(eng, (k, v))
        for b in writes:
            for d in (b.w, b.pw, b.r):
                for k, v in d.items():
                    self._wait(eng, (k, v))
        for b in pwrites:
            for d in (b.w, b.r):
                for k, v in d.items():
                    self._wait(eng, (k, v))

    def _commit(self, tok, reads, writes, pwrites=()):
        k, v = tok
        for b in writes:
            b.w = {k: v}
            b.pw = {}
            b.r = {}
        for b in pwrites:
            if b.pw.get(k, 0) < v:
                b.pw[k] = v
        for b in reads:
            if b.r.get(k, 0) < v:
                b.r[k] = v

    def op(self, eng, fn, reads=(), writes=(), pwrites=()):
        self._deps(eng, reads, writes, pwrites)
        inst = fn(self.E[eng])
        self.cnt[eng] += 1
        tok = (eng, self.cnt[eng])
        inst.then_inc(self.sem[eng], 1)
        self._commit(tok, reads, writes, pwrites)
        self.ninst += 1
        return tok

    def dma(self, q, fn, reads=(), writes=(), pwrites=()):
        self._deps(q, reads, writes, pwrites)
        i = self.dslot[q]
        self.dslot[q] = (i + 1) % self.NDMA
        key = "d_%s%d" % (q, i)
        if self.cnt[key] > 0:
            self._wait(q, (key, self.cnt[key]))
        inst = fn(self.E[q])
        self.cnt[key] += 16
        tok = (key, self.cnt[key])
        inst.then_inc(self.sem[key], 16)
        self._commit(tok, reads, writes, pwrites)
        self.ninst += 1
        return tok

    def finish(self, bufs):
        for b in bufs:
            for d in (b.w, b.pw):
                for k, v in d.items():
                    self._wait("sp", (k, v))


def barrier(c):
    toks = [(k, v) for k, v in c.cnt.items() if v > 0]
    for eng in c.E:
        for tok in toks:
            c._wait(eng, tok)


_UNIQ = [0]


def uniq(name):
    _UNIQ[0] += 1
    return "%s_u%d" % (name, _UNIQ[0])


L = 8192
D = 1024
NE = 16
FF = 2048
CAP = 1024
NT = L // 128


class G:
    pass


def alloc_T(nc, name, shape, dtype, n=1, es=None):
    if es is None:
        return [(nc.alloc_sbuf_tensor("%s_%d" % (name, i), shape, dtype), Buf(name)) for i in range(n)]
    return [(es.enter_context(nc.sbuf_tensor(uniq("%s_%d" % (name, i)), shape, dtype)), Buf(name)) for i in range(n)]


def setup_consts(c, g):
    nc = c.nc
    g.ident32 = nc.alloc_sbuf_tensor("ident32", [128, 128], F32); g.b_ident32 = Buf()
    g.identb = nc.alloc_sbuf_tensor("identb", [128, 128], BF16); g.b_identb = Buf()
    g.ones32 = nc.alloc_sbuf_tensor("ones32", [1, 128], F32); g.b_ones32 = Buf()
    g.neghalf = nc.alloc_sbuf_tensor("neghalf", [128, 1], F32); g.b_neghalf = Buf()
    for t, b in ((g.ident32, g.b_ident32), (g.identb, g.b_identb)):
        c.op('pool', lambda e: e.memset(t[:], 0.0), writes=[b])
        c.op('pool', lambda e: e.affine_select(out=t[:], in_=t[:], pattern=[[-1, 128]], compare_op=ALU.not_equal,
                                               fill=1.0, base=0, channel_multiplier=1), reads=[b], writes=[b])
    g.J32 = nc.alloc_sbuf_tensor("J32", [128, 128], F32); g.b_J32 = Buf()
    g.Jb = nc.alloc_sbuf_tensor("Jb", [128, 128], BF16); g.b_Jb = Buf()
    for t, b in ((g.J32, g.b_J32), (g.Jb, g.b_Jb)):
        c.op('pool', lambda e: e.memset(t[:], 0.0), writes=[b])
        c.op('pool', lambda e: e.affine_select(out=t[:], in_=t[:], pattern=[[1, 128]], compare_op=ALU.not_equal,
                                               fill=1.0, base=-127, channel_multiplier=1), reads=[b], writes=[b])
    c.op('pool', lambda e: e.memset(g.ones32[:], 1.0), writes=[g.b_ones32])
    c.op('pool', lambda e: e.memset(g.neghalf[:], -0.5), writes=[g.b_neghalf])
    g.ps = []
    for i in range(7):
        g.ps.append((nc.alloc_psum_tensor("ps%d" % i, [128, 512], F32), Buf("ps%d" % i, psum=True)))
    g.psb = (nc.alloc_psum_tensor("psb", [128, 1024], BF16), Buf("psb", psum=True))


def bcast_row(c, g, dst, bdst, src_ap, n, tmp, btmp, psi=6):
    c.dma('sp', lambda e: e.dma_start(out=tmp[0:1, 0:n], in_=src_ap.rearrange("(o n) -> o n", o=1)), writes=[btmp])
    ps, bps = g.ps[psi]
    for h in range(0, n, 512):
        w = min(512, n - h)
        c.op('pe', lambda e: e.matmul(ps[:, 0:w], lhsT=g.ones32[0:1, :], rhs=tmp[0:1, h:h + w], start=True, stop=True),
             reads=[g.b_ones32, btmp], writes=[bps])
        c.op('dve', lambda e: e.tensor_copy(out=dst[:, h:h + w], in_=ps[:, 0:w]), reads=[bps], writes=[bdst])


def rmsnorm_tile(c, g, xt, bxt, gB, bgB, h32, bh32, junk, bjunk, ss, bss):
    c.op('dve', lambda e: e.scalar_tensor_tensor(out=junk[:], in0=xt, scalar=1.0, in1=xt, op0=ALU.mult, op1=ALU.mult,
                                                 accum_out=ss[:, 0:1]), reads=[bxt], writes=[bjunk, bss])
    c.op('dve', lambda e: e.tensor_scalar(out=ss[:, 0:1], in0=ss[:, 0:1], scalar1=1.0 / D, scalar2=1e-6, op0=ALU.mult, op1=ALU.add),
         reads=[bss], writes=[bss])
    c.op('act', lambda e: e.activation(out=ss[:, 0:1], in_=ss[:, 0:1], func=AF.Ln), reads=[bss], writes=[bss])
    c.op('act', lambda e: e.activation(out=ss[:, 0:1], in_=ss[:, 0:1], func=AF.Exp, scale=-0.5), reads=[bss], writes=[bss])
    c.op('dve', lambda e: e.scalar_tensor_tensor(out=h32[:], in0=xt, scalar=ss[:, 0:1], in1=gB[:], op0=ALU.mult, op1=ALU.mult),
         reads=[bxt, bss, bgB], writes=[bh32])


def moe_prep(c, g, sb, ffn_g, w_router):
    nc = c.nc
    gB, bgB = sb['gB']
    tmpr, btmpr = sb['tmprow']
    bcast_row(c, g, gB, bgB, ffn_g, D, tmpr, btmpr)
    wr, bwr = sb['wr']
    c.dma('sp', lambda e: e.dma_start(out=wr[:], in_=w_router.rearrange("(k p) e -> p k e", p=128)), writes=[bwr])


def moe_step1_tile(c, g, sb, i, xt, bxt, HB, bHB):
    affT, baffT = sb['affT']
    gB, bgB = sb['gB']
    wr, bwr = sb['wr']
    h32, bh32 = sb['h32'][i % 2]
    hb, bhb = sb['hb'][i % 2]
    junk, bjunk = sb['junk']
    ss, bss = sb['ss'][i % 2]
    rmsnorm_tile(c, g, xt, bxt, gB, bgB, h32, bh32, junk, bjunk, ss, bss)
    c.op('act', lambda e: e.activation(out=hb[:], in_=h32[:], func=AF.Copy), reads=[bh32], writes=[bhb])
    c.dma('act', lambda e: e.dma_start(out=HB[i * 128:(i + 1) * 128, :], in_=hb[:]), reads=[bhb], pwrites=[bHB])
    hT, bhT = sb['hT32'][i % 2]
    for hh in range(2):
        ps, bps = g.ps[4 + hh]
        for k in range(4):
            kk = hh * 4 + k
            c.op('pe', lambda e: e.transpose(out=ps[:, k * 128:(k + 1) * 128], in_=h32[:, kk * 128:(kk + 1) * 128],
                                             identity=g.ident32[:]), reads=[bh32, g.b_ident32], writes=[bps])
        c.op('act', lambda e: e.activation(out=hT[:, hh * 512:(hh + 1) * 512], in_=ps[:, :], func=AF.Copy),
             reads=[bps], writes=[bhT])
    pl, bpl = g.ps[6]
    for k in range(8):
        c.op('pe', lambda e: e.matmul(pl[:, 0:NE], lhsT=hT[:, k * 128:(k + 1) * 128], rhs=wr[:, k, :], start=(k == 0), stop=(k == 7)),
             reads=[bhT, bwr], writes=[bpl])
    sm, bsm = sb['sm'][i % 2]
    ex, bex = sb['ex'][i % 2]
    c.op('dve', lambda e: e.tensor_reduce(out=sm[:, 0:1], in_=pl[:, 0:NE], axis=AX.X, op=ALU.max), reads=[bpl], writes=[bsm])
    c.op('dve', lambda e: e.tensor_scalar(out=sm[:, 0:1], in0=sm[:, 0:1], scalar1=-1.0, scalar2=None, op0=ALU.mult), reads=[bsm], writes=[bsm])
    c.op('act', lambda e: e.activation(out=ex[:], in_=pl[:, 0:NE], func=AF.Exp, bias=sm[:, 0:1], scale=1.0, accum_out=sm[:, 1:2]),
         reads=[bpl, bsm], writes=[bex, bsm])
    c.op('dve', lambda e: e.reciprocal(out=sm[:, 2:3], in_=sm[:, 1:2]), reads=[bsm], writes=[bsm])
    c.op('dve', lambda e: e.tensor_scalar(out=ex[:], in0=ex[:], scalar1=sm[:, 2:3], scalar2=None, op0=ALU.mult), reads=[bex, bsm], writes=[bex])
    pt, bpt = g.ps[6]
    c.op('pe', lambda e: e.transpose(out=pt[0:NE, 0:128], in_=ex[:, 0:NE], identity=g.ident32[:]), reads=[bex, g.b_ident32], writes=[bpt])
    c.op('act', lambda e: e.activation(out=affT[0:NE, i * 128:(i + 1) * 128], in_=pt[0:NE, 0:128], func=AF.Copy), reads=[bpt], writes=[baffT])


def moe_rest(c, g, sb, X, bX, HB, bHB, w_gate, w_up, w_down, stage=3):
    nc = c.nc
    affT, baffT = sb['affT']
    bHB.seal()
    if stage < 2:
        return
    vals, bvals = sb['vals']
    idxu, bidxu = sb['idxu']
    for it in range(CAP // 8):
        sl = slice(it * 8, it * 8 + 8)
        c.op('dve', lambda e: e.max(out=vals[:, sl], in_=affT[:, :]), reads=[baffT], writes=[bvals])
        c.op('dve', lambda e: e.max_index(out=idxu[:, sl], in_max=vals[:, sl], in_values=affT[:, :]), reads=[baffT, bvals], writes=[bidxu])
        c.op('dve', lambda e: e.match_replace(out=affT[:, :], in_to_replace=vals[:, sl], in_values=affT[:, :], imm_value=-1.0),
             reads=[bvals, baffT], writes=[baffT])
    idxf, bidxf = sb['idxf']
    c.op('dve', lambda e: e.tensor_copy(out=idxf[:], in_=idxu[:]), reads=[bidxu], writes=[bidxf])
    idxT, bidxT = sb['idxT']
    gateT, bgateT = sb['gateT']
    for j in range(8):
        pt, bpt = g.ps[4 + (j % 2)]
        c.op('pe', lambda e: e.transpose(out=pt[:, 0:NE], in_=idxf[0:NE, j * 128:(j + 1) * 128], identity=g.ident32[0:NE, 0:NE]),
             reads=[bidxf, g.b_ident32], writes=[bpt])
        c.op('dve', lambda e: e.tensor_copy(out=idxT[:, j, :], in_=pt[:, 0:NE]), reads=[bpt], writes=[bidxT])
        pt2, bpt2 = g.ps[2 + (j % 2)]
        c.op('pe', lambda e: e.transpose(out=pt2[:, 0:NE], in_=vals[0:NE, j * 128:(j + 1) * 128], identity=g.ident32[0:NE, 0:NE]),
             reads=[bvals, g.b_ident32], writes=[bpt2])
        c.op('act', lambda e: e.activation(out=gateT[:, j, :], in_=pt2[:, 0:NE], func=AF.Copy), reads=[bpt2], writes=[bgateT])
    if stage < 3:
        return
    xsT, bxsT = sb['xsT']
    hidT, bhidT = sb['hidT']
    yacc, byacc = sb['yacc']
    ptb, bptb = g.psb
    qi = 0
    for ex_i in range(NE):
        for j in range(8):
            xs, bxs = sb['xs'][j % 2]
            c.dma('pool', lambda e: e.indirect_dma_start(out=xs[:, :], out_offset=None, in_=HB[:, :],
                                                        in_offset=bass.IndirectOffsetOnAxis(ap=idxT[:, j, ex_i:ex_i + 1], axis=0)),
                  reads=[bHB, bidxT], writes=[bxs])
            for k in range(8):
                c.op('pe', lambda e: e.transpose(out=ptb[:, k * 128:(k + 1) * 128], in_=xs[:, k * 128:(k + 1) * 128], identity=g.identb[:]),
                     reads=[bxs, g.b_identb], writes=[bptb])
            c.op('dve', lambda e: e.tensor_copy(out=xsT[:, :, j * 128:(j + 1) * 128], in_=ptb[:, :].rearrange("p (k s) -> p k s", k=8)),
                 reads=[bptb], writes=[bxsT])
        for q in range(4):
            wg, bwg = sb['wg'][qi % 2]
            wu, bwu = sb['wu'][qi % 2]
            wd, bwd = sb['wd'][qi % 2]
            qi += 1
            f0 = q * 512
            c.dma('pool', lambda e: e.dma_start(out=wg[:], in_=w_gate[ex_i, :, f0:f0 + 512].rearrange("(k p) f -> p k f", p=128)), writes=[bwg])
            c.dma('pool', lambda e: e.dma_start(out=wu[:], in_=w_up[ex_i, :, f0:f0 + 512].rearrange("(k p) f -> p k f", p=128)), writes=[bwu])
            c.dma('pool', lambda e: e.dma_start(out=wd[:], in_=w_down[ex_i, f0:f0 + 512, :].rearrange("(k p) d -> p k d", p=128)), writes=[bwd])
            n = 0
            for fc in range(4):
                for sh in range(2):
                    pg, bpg = g.ps[0 + (n % 2)]
                    pu, bpu = g.ps[2 + (n % 2)]
                    sg, bsg = sb['sg'][n % 2]
                    n += 1
                    for k in range(8):
                        c.op('pe', lambda e: e.matmul(pg[:, :], lhsT=wg[:, k, fc * 128:(fc + 1) * 128], rhs=xsT[:, k, sh * 512:(sh + 1) * 512],
                                                      start=(k == 0), stop=(k == 7)), reads=[bwg, bxsT], writes=[bpg])
                    for k in range(8):
                        c.op('pe', lambda e: e.matmul(pu[:, :], lhsT=wu[:, k, fc * 128:(fc + 1) * 128], rhs=xsT[:, k, sh * 512:(sh + 1) * 512],
                                                      start=(k == 0), stop=(k == 7)), reads=[bwu, bxsT], writes=[bpu])
                    c.op('act', lambda e: e.activation(out=sg[:], in_=pg[:, :], func=AF.Silu), reads=[bpg], writes=[bsg])
                    c.op('dve', lambda e: e.tensor_tensor(out=hidT[:, fc, sh * 512:(sh + 1) * 512], in0=sg[:], in1=pu[:, :], op=ALU.mult),
                         reads=[bsg, bpu], writes=[bhidT])
            m = 0
            for j in range(8):
                for dh in range(2):
                    py, bpy = g.ps[4 + (m % 2)]
                    m += 1
                    for fc in range(4):
                        c.op('pe', lambda e: e.matmul(py[:, :], lhsT=hidT[:, fc, j * 128:(j + 1) * 128], rhs=wd[:, fc, dh * 512:(dh + 1) * 512],
                                                      start=(fc == 0), stop=(fc == 3)), reads=[bhidT, bwd], writes=[bpy])
                    ysl = yacc[:, j, dh * 512:(dh + 1) * 512]
                    gsc = gateT[:, j, ex_i:ex_i + 1]
                    if q == 0:
                        c.op('dve', lambda e: e.tensor_scalar(out=ysl, in0=py[:, :], scalar1=gsc, scalar2=None, op0=ALU.mult),
                             reads=[bpy, bgateT], writes=[byacc])
                    else:
                        c.op('dve', lambda e: e.scalar_tensor_tensor(out=ysl, in0=py[:, :], scalar=gsc, in1=ysl, op0=ALU.mult, op1=ALU.add),
                             reads=[bpy, bgateT, byacc], writes=[byacc])
        for j in range(8):
            c.dma('pool', lambda e: e.indirect_dma_start(out=X[:, :], out_offset=bass.IndirectOffsetOnAxis(ap=idxT[:, j, ex_i:ex_i + 1], axis=0),
                                                        in_=yacc[:, j, :], in_offset=None, compute_op=ALU.add),
                  reads=[byacc, bidxT], pwrites=[bX])
        bX.seal()


def moe_phase(c, g, X, bX, HB, bHB, ffn_g, w_router, w_gate, w_up, w_down, sb, stage=3):
    moe_prep(c, g, sb, ffn_g, w_router)
    for i in range(NT):
        xt, bxt = sb['xt'][i % 2]
        c.dma('sp', lambda e: e.dma_start(out=xt[:], in_=X[i * 128:(i + 1) * 128, :]), reads=[bX], writes=[bxt])
        moe_step1_tile(c, g, sb, i, xt[:], bxt, HB, bHB)
    moe_rest(c, g, sb, X, bX, HB, bHB, w_gate, w_up, w_down, stage=stage)


def moe_alloc(nc, es=None, part=0, sb=None):
    sb = {} if sb is None else sb
    if part in (0, 1):
        sb['gB'] = alloc_T(nc, 'gB', [128, D], F32, 1, es=es)[0]
        sb['tmprow'] = alloc_T(nc, 'tmprow', [1, 1024], F32, 1, es=es)[0]
        sb['wr'] = alloc_T(nc, 'wr', [128, 8, NE], F32, 1, es=es)[0]
        big = es.enter_context(nc.sbuf_tensor(uniq('big'), [128, L], F32)) if es is not None else nc.alloc_sbuf_tensor('big', [128, L], F32); bbig = Buf('big')
        sb['affT'] = (big[0:NE, :], bbig)
        sb['yacc'] = (big[:, :].rearrange('p (j d) -> p j d', j=8), bbig)
        if part == 0:
            sb['xt'] = alloc_T(nc, 'xt', [128, D], F32, 2, es=es)
        sb['h32'] = alloc_T(nc, 'h32', [128, D], F32, 2, es=es)
        sb['hb'] = alloc_T(nc, 'hb', [128, D], BF16, 2, es=es)
        sb['junk'] = alloc_T(nc, 'junk', [128, D], F32, 1, es=es)[0]
        sb['ss'] = alloc_T(nc, 'ss', [128, 4], F32, 2, es=es)
        sb['hT32'] = alloc_T(nc, 'hT32', [128, D], F32, 2, es=es)
        sb['sm'] = alloc_T(nc, 'sm', [128, 4], F32, 2, es=es)
        sb['ex'] = alloc_T(nc, 'ex', [128, NE], F32, 2, es=es)
    if part in (0, 3):
        sb['vals'] = alloc_T(nc, 'vals', [NE, CAP], F32, 1, es=es)[0]
        sb['idxu'] = alloc_T(nc, 'idxu', [NE, CAP], U32, 1, es=es)[0]
        sb['idxf'] = alloc_T(nc, 'idxf', [NE, CAP], F32, 1, es=es)[0]
        sb['idxT'] = alloc_T(nc, 'idxT', [128, 8, NE], U32, 1, es=es)[0]
        sb['gateT'] = alloc_T(nc, 'gateT', [128, 8, NE], F32, 1, es=es)[0]
        sb['xsT'] = alloc_T(nc, 'xsT', [128, 8, CAP], BF16, 1, es=es)[0]
        sb['hidT'] = alloc_T(nc, 'hidT', [128, 4, CAP], BF16, 1, es=es)[0]
        sb['xs'] = alloc_T(nc, 'xs', [128, D], BF16, 2, es=es)
        sb['wg'] = alloc_T(nc, 'wg', [128, 8, 512], BF16, 2, es=es)
        sb['wu'] = alloc_T(nc, 'wu', [128, 8, 512], BF16, 2, es=es)
        sb['wd'] = alloc_T(nc, 'wd', [128, 4, D], BF16, 2, es=es)
        sb['sg'] = alloc_T(nc, 'sg', [128, 512], BF16, 2, es=es)
    return sb


def moe_layer(c, g, X, bX, HB, bHB, ffn_g, w_router, w_gate, w_up, w_down):
    with ExitStack() as es:
        sb = moe_alloc(c.nc, es)
        moe_phase(c, g, X, bX, HB, bHB, ffn_g, w_router, w_gate, w_up, w_down, sb)
        barrier(c)


def proj_phase(c, g, X, bX, gain_ap, w_ap, nout, spec, PT, bPT, PV, bPV):
    nc = c.nc
    with ExitStack() as es:
        def T(name, shape, dt):
            return es.enter_context(nc.sbuf_tensor(uniq(name), shape, dt))
        wsb = T("pj_w", [128, 8, nout], BF16); bw = Buf()
        gB = T("pj_gB", [128, D], F32); bgB = Buf()
        tmpr = T("pj_tmpr", [1, D], F32); btmpr = Buf()
        xt = [T("pj_xt%d" % i, [128, 4, D], F32) for i in range(2)]; bxt = [Buf(), Buf()]
        junk = T("pj_junk", [128, D], F32); bjunk = Buf()
        ss = [T("pj_ss%d" % i, [128, 4], F32) for i in range(2)]; bss = [Buf(), Buf()]
        hb = [T("pj_hb%d" % i, [128, D], BF16) for i in range(2)]; bhb = [Buf(), Buf()]
        hT = [T("pj_hT%d" % i, [128, 8, 512], BF16) for i in range(2)]; bhT = [Buf(), Buf()]
        stF = [T("pj_stF%d" % i, [128, 512], F32) for i in range(3)]; bstF = [Buf() for _ in range(3)]
        stT = [T("pj_stT%d" % i, [128, 512], BF16) for i in range(3)]; bstT = [Buf() for _ in range(3)]
        bcast_row(c, g, gB, bgB, gain_ap, D, tmpr, btmpr)
        for c0 in range(0, nout, 512):
            wd = min(512, nout - c0)
            c.dma('pool', lambda e: e.dma_start(out=wsb[:, :, c0:c0 + wd], in_=w_ap[:, c0:c0 + wd].rearrange("(k p) f -> p k f", p=128)), writes=[bw])
        ptb, bptb = g.psb
        nF = 0; nT = 0; npz = 0
        for it in range(L // 512):
            t0 = it * 512
            x_, bx_ = xt[it % 2], bxt[it % 2]
            c.dma('sp', lambda e: e.dma_start(out=x_[:], in_=X[t0:t0 + 512, :].rearrange("(j p) d -> p j d", p=128)), reads=[bX], writes=[bx_])
            hT_, bhT_ = hT[it % 2], bhT[it % 2]
            for j in range(4):
                s_, bs_ = ss[j % 2], bss[j % 2]
                h_, bh_ = hb[j % 2], bhb[j % 2]
                c.op('dve', lambda e: e.scalar_tensor_tensor(out=junk[:], in0=x_[:, j, :], scalar=1.0, in1=x_[:, j, :], op0=ALU.mult, op1=ALU.mult,
                                                             accum_out=s_[:, 0:1]), reads=[bx_], writes=[bjunk, bs_])
                c.op('dve', lambda e: e.tensor_scalar(out=s_[:, 0:1], in0=s_[:, 0:1], scalar1=1.0 / D, scalar2=1e-6, op0=ALU.mult, op1=ALU.add),
                     reads=[bs_], writes=[bs_])
                c.op('act', lambda e: e.activation(out=s_[:, 0:1], in_=s_[:, 0:1], func=AF.Ln), reads=[bs_], writes=[bs_])
                c.op('act', lambda e: e.activation(out=s_[:, 0:1], in_=s_[:, 0:1], func=AF.Exp, scale=-0.5), reads=[bs_], writes=[bs_])
                c.op('dve', lambda e: e.scalar_tensor_tensor(out=h_[:], in0=x_[:, j, :], scalar=s_[:, 0:1], in1=gB[:], op0=ALU.mult, op1=ALU.mult),
                     reads=[bx_, bs_, bgB], writes=[bh_])
                for k in range(8):
                    c.op('pe', lambda e: e.transpose(out=ptb[:, k * 128:(k + 1) * 128], in_=h_[:, k * 128:(k + 1) * 128], identity=g.identb[:]),
                         reads=[bh_, g.b_identb], writes=[bptb])
                c.op('act', lambda e: e.activation(out=hT_[:, :, j * 128:(j + 1) * 128], in_=ptb[:, :].rearrange("p (k s) -> p k s", k=8), func=AF.Copy),
                     reads=[bptb], writes=[bhT_])
            for (col0, ncols, mode, dst0) in spec:
                if mode == 'F':
                    for f0 in range(0, ncols, 128):
                        fw = min(128, ncols - f0)
                        ps, bps = g.ps[npz % 4]; npz += 1
                        for k in range(8):
                            c.op('pe', lambda e: e.matmul(ps[0:fw, :], lhsT=wsb[:, k, col0 + f0:col0 + f0 + fw], rhs=hT_[:, k, :], start=(k == 0), stop=(k == 7)),
                                 reads=[bw, bhT_], writes=[bps])
                        st, bst = stF[nF % 3], bstF[nF % 3]; nF += 1
                        eng = 'act' if nF % 2 else 'dve'
                        if eng == 'act':
                            c.op('act', lambda e: e.activation(out=st[0:fw, :], in_=ps[0:fw, :], func=AF.Copy), reads=[bps], writes=[bst])
                        else:
                            c.op('dve', lambda e: e.tensor_copy(out=st[0:fw, :], in_=ps[0:fw, :]), reads=[bps], writes=[bst])
                        c.dma('sp' if nF % 2 else 'act', lambda e: e.dma_start(out=PT[dst0 + f0:dst0 + f0 + fw, t0:t0 + 512], in_=st[0:fw, :]), reads=[bst], pwrites=[bPT])
                else:
                    for j in range(4):
                        for c0 in range(0, ncols, 512):
                            cw = min(512, ncols - c0)
                            ps, bps = g.ps[npz % 4]; npz += 1
                            for k in range(8):
                                c.op('pe', lambda e: e.matmul(ps[:, 0:cw], lhsT=hT_[:, k, j * 128:(j + 1) * 128], rhs=wsb[:, k, col0 + c0:col0 + c0 + cw], start=(k == 0), stop=(k == 7)),
                                     reads=[bw, bhT_], writes=[bps])
                            st, bst = stT[nT % 3], bstT[nT % 3]; nT += 1
                            eng = 'act' if nT % 2 else 'dve'
                            if eng == 'act':
                                c.op('act', lambda e: e.activation(out=st[:, 0:cw], in_=ps[:, 0:cw], func=AF.Copy), reads=[bps], writes=[bst])
                            else:
                                c.op('dve', lambda e: e.tensor_copy(out=st[:, 0:cw], in_=ps[:, 0:cw]), reads=[bps], writes=[bst])
                            c.dma('sp' if nT % 2 else 'act', lambda e: e.dma_start(out=PV[t0 + j * 128:t0 + (j + 1) * 128, dst0 + c0:dst0 + c0 + cw], in_=st[:, 0:cw]),
                                  reads=[bst], pwrites=[bPV])
        bPT.seal(); bPV.seal()
        barrier(c)


def gated_norm_finalize(c, g, OA, bOAs, PV, bPV, gcol0, gain_ap, MT, bMT, row0, pfx, OA2=None, bOA2s=()):
    nc = c.nc
    with ExitStack() as es:
        def T(name, shape, dt):
            return es.enter_context(nc.sbuf_tensor(uniq(pfx + name), shape, dt))
        gA = T("gA", [128, 128], F32); bgA = Buf()
        oa = [T("oa%d" % i, [128, 512], F32) for i in range(2)]; boa = [Buf(), Buf()]
        oa2 = [T("oa2%d" % i, [128, 512], F32) for i in range(2)]; boa2 = [Buf(), Buf()]
        ga = [T("ga%d" % i, [128, 512], BF16) for i in range(2)]; bga = [Buf(), Buf()]
        sq = T("sq", [128, 512], F32); bsq = Buf()
        ssq = [T("ssq%d" % i, [128, 4], F32) for i in range(2)]; bssq = [Buf(), Buf()]
        sg = T("sg", [128, 512], F32); bsg = Buf()
        t1 = T("t1", [128, 512], F32); bt1 = Buf()
        ob = [T("ob%d" % i, [128, 512], BF16) for i in range(2)]; bob = [Buf(), Buf()]
        mt = [T("mt%d" % i, [128, 4, 512], BF16) for i in range(2)]; bmt = [Buf(), Buf()]
        c.dma('sp', lambda e: e.dma_start(out=gA[:], in_=gain_ap.partition_broadcast(128)), writes=[bgA])
        ptb, bptb = g.psb
        for i in range(NT):
            oa_, boa_ = oa[i % 2], boa[i % 2]
            ga_, bga_ = ga[i % 2], bga[i % 2]
            ss_, bss_ = ssq[i % 2], bssq[i % 2]
            ob_, bob_ = ob[i % 2], bob[i % 2]
            mt_, bmt_ = mt[(i // 4) % 2], bmt[(i // 4) % 2]
            c.dma('sp', lambda e: e.dma_start(out=oa_[:], in_=OA[i * 128:(i + 1) * 128, :]), reads=bOAs, writes=[boa_])
            c.dma('act', lambda e: e.dma_start(out=ga_[:], in_=PV[i * 128:(i + 1) * 128, gcol0:gcol0 + 512]), reads=[bPV], writes=[bga_])
            if OA2 is not None:
                o2_, bo2_ = oa2[i % 2], boa2[i % 2]
                c.dma('act', lambda e: e.dma_start(out=o2_[:], in_=OA2[i * 128:(i + 1) * 128, :]), reads=list(bOA2s), writes=[bo2_])
                c.op('dve', lambda e: e.tensor_tensor(out=oa_[:], in0=oa_[:], in1=o2_[:], op=ALU.add), reads=[boa_, bo2_], writes=[boa_])
            c.op('act', lambda e: e.activation(out=sq[:], in_=oa_[:], func=AF.Square), reads=[boa_], writes=[bsq])
            c.op('dve', lambda e: e.tensor_reduce(out=ss_[:], in_=sq[:].rearrange("p (h d) -> p h d", h=4), axis=AX.X, op=ALU.add), reads=[bsq], writes=[bss_])
            c.op('dve', lambda e: e.tensor_scalar(out=ss_[:], in0=ss_[:], scalar1=1.0 / 128, scalar2=1e-6, op0=ALU.mult, op1=ALU.add), reads=[bss_], writes=[bss_])
            c.op('pool', lambda e: e.tensor_tensor(out=ss_[:], in0=ss_[:], in1=g.neghalf[:, 0:1].broadcast_to([128, 4]), op=ALU.pow), reads=[bss_, g.b_neghalf], writes=[bss_])
            c.op('act', lambda e: e.activation(out=sg[:], in_=ga_[:], func=AF.Silu), reads=[bga_], writes=[bsg])
            c.op('dve', lambda e: e.tensor_tensor(out=t1[:].rearrange("p (h d) -> p h d", h=4), in0=oa_[:].rearrange("p (h d) -> p h d", h=4),
                                                  in1=ss_[:].unsqueeze(2).broadcast_to([128, 4, 128]), op=ALU.mult), reads=[boa_, bss_], writes=[bt1])
            c.op('dve', lambda e: e.tensor_tensor(out=t1[:].rearrange("p (h d) -> p h d", h=4), in0=t1[:].rearrange("p (h d) -> p h d", h=4),
                                                   in1=gA[:].unsqueeze(1).broadcast_to([128, 4, 128]), op=ALU.mult), reads=[bt1, bgA], writes=[bt1])
            c.op('dve', lambda e: e.tensor_tensor(out=ob_[:], in0=t1[:], in1=sg[:], op=ALU.mult), reads=[bt1, bsg], writes=[bob_])
            for k in range(4):
                c.op('pe', lambda e: e.transpose(out=ptb[:, k * 128:(k + 1) * 128], in_=ob_[:, k * 128:(k + 1) * 128], identity=g.identb[:]),
                     reads=[bob_, g.b_identb], writes=[bptb])
            c.op('act', lambda e: e.activation(out=mt_[:, :, (i % 4) * 128:(i % 4 + 1) * 128], in_=ptb[:, 0:512].rearrange("p (k s) -> p k s", k=4), func=AF.Copy),
                 reads=[bptb], writes=[bmt_])
            if i % 4 == 3:
                t0 = (i // 4) * 512
                c.dma('sp', lambda e: e.dma_start(out=MT[row0:row0 + 512, t0:t0 + 512].rearrange("(k p) t -> p k t", p=128), in_=mt_[:]), reads=[bmt_], pwrites=[bMT])
        barrier(c)


TBK = 2048
NTB = L // TBK


def hgrn2_phase(c, g, PT, bPT, PV, bPV, lb_logits, jl, a_out_norm, QK, bQK, OA, bOAs, OA2, bOA2s, MT, bMT, stage=3):
    nc = c.nc
    with ExitStack() as es0:
        def T0(name, shape, dt):
            return es0.enter_context(nc.sbuf_tensor(uniq(name), shape, dt))
        mcols = T0("hg_mcols", [128, 8, 128], F32); bmcols = Buf()
        with ExitStack() as es:
            def T(name, shape, dt):
                return es.enter_context(nc.sbuf_tensor(uniq(name), shape, dt))
            lbt = T("hg_lbt", [128, 2, 2, 4], F32); blbt = Buf()
            lbc = T("hg_lbc", [128, 8], F32); blbc = Buf()
            oml = T("hg_oml", [128, 8], F32); boml = Buf()
            noml = T("hg_noml", [128, 8], F32); bnoml = Buf()
            msk = T("hg_msk", [128, TBK], F32); bmsk = Buf()
            bmid = T("hg_bmid", [128, 128], F32); bbmid = Buf()
            blast = T("hg_blast", [128, 128], F32); bblast = Buf()
            zq = [T("hg_zq%d" % i, [128, TBK], F32) for i in range(2)]; bzq = [Buf(), Buf()]
            zf = [T("hg_zf%d" % i, [128, TBK], F32) for i in range(2)]; bzf = [Buf(), Buf()]
            q_ = T("hg_q", [128, TBK], F32); bq_ = Buf()
            sig = T("hg_sig", [128, TBK], F32); bsig = Buf()
            f_ = T("hg_f", [128, TBK], F32); bf_ = Buf()
            kk = T("hg_kk", [128, TBK], F32); bkk = Buf()
            b_ = T("hg_b", [128, TBK], F32); bb_ = Buf()
            e1 = T("hg_e1", [128, TBK], F32); be1 = Buf()
            eq = T("hg_eq", [128, TBK], F32); beq = Buf()
            ek = T("hg_ek", [128, TBK], F32); bek = Buf()
            qt = [T("hg_qt%d" % i, [128, TBK], BF16) for i in range(2)]; bqt = [Buf(), Buf()]
            kt = [T("hg_kt%d" % i, [128, TBK], BF16) for i in range(2)]; bkt = [Buf(), Buf()]
            if jl == 0:
                c.op('pool', lambda e: e.memset(lbc[:], 0.0), writes=[blbc])
            else:
                with nc.allow_non_contiguous_dma(reason="tiny"):
                    for j_ in range(2):
                        for r_ in range(2):
                            c.dma('sp', lambda e: e.dma_start(out=lbt[:, j_, r_, :], in_=lb_logits[j_, r_, :].rearrange("(h p) -> p h", p=128)), pwrites=[blbt])
                blbt.seal()
                c.op('dve', lambda e: e.tensor_tensor(out=lbc[:].rearrange("p (r h) -> p r h", r=2), in0=lbt[:, 1, :, :], in1=lbt[:, 0, :, :], op=ALU.subtract),
                     reads=[blbt], writes=[blbc])
                c.op('act', lambda e: e.activation(out=lbc[:], in_=lbc[:], func=AF.Sigmoid), reads=[blbc], writes=[blbc])
            c.op('dve', lambda e: e.tensor_scalar(out=oml[:], in0=lbc[:], scalar1=-1.0, scalar2=1.0, op0=ALU.mult, op1=ALU.add), reads=[blbc], writes=[boml])
            c.op('dve', lambda e: e.tensor_scalar(out=noml[:], in0=oml[:], scalar1=-1.0, scalar2=None, op0=ALU.mult), reads=[boml], writes=[bnoml])
            c.op('pool', lambda e: e.memset(msk[:], 1.0), writes=[bmsk])
            c.op('pool', lambda e: e.memset(msk[:].rearrange("p (c j) -> p c j", j=64)[:, :, 0:1], 0.0), writes=[bmsk])
            n = 0
            for h in range(4):
                for r in range(2):
                    hr = r * 4 + h
                    for tb in range(NTB):
                        nb = tb if r == 0 else NTB - 1 - tb
                        zq_, bzq_ = zq[n % 2], bzq[n % 2]
                        zf_, bzf_ = zf[n % 2], bzf[n % 2]
                        qt_, bqt_ = qt[n % 2], bqt[n % 2]
                        kt_, bkt_ = kt[n % 2], bkt[n % 2]
                        n += 1
                        c.dma('sp', lambda e: e.dma_start(out=zq_[:], in_=PT[h * 128:(h + 1) * 128, nb * TBK:(nb + 1) * TBK]), reads=[bPT], writes=[bzq_])
                        fr = 512 + r * 512 + h * 128
                        c.dma('act', lambda e: e.dma_start(out=zf_[:], in_=PT[fr:fr + 128, nb * TBK:(nb + 1) * TBK]), reads=[bPT], writes=[bzf_])
                        zqs = zq_[:, ::-1] if r else zq_[:, :]
                        zfs = zf_[:, ::-1] if r else zf_[:, :]
                        c.op('act', lambda e: e.activation(out=q_[:], in_=zqs, func=AF.Silu), reads=[bzq_], writes=[bq_])
                        c.op('act', lambda e: e.activation(out=sig[:], in_=zfs, func=AF.Sigmoid), reads=[bzf_], writes=[bsig])
                        c.op('dve', lambda e: e.tensor_scalar(out=f_[:], in0=sig[:], scalar1=oml[:, hr:hr + 1], scalar2=lbc[:, hr:hr + 1], op0=ALU.mult, op1=ALU.add),
                             reads=[bsig, boml, blbc], writes=[bf_])
                        c.op('act', lambda e: e.activation(out=f_[:], in_=f_[:], func=AF.Ln), reads=[bf_], writes=[bf_])
                        c.op('dve', lambda e: e.tensor_scalar(out=kk[:], in0=sig[:], scalar1=noml[:, hr:hr + 1], scalar2=oml[:, hr:hr + 1], op0=ALU.mult, op1=ALU.add),
                             reads=[bsig, bnoml, boml], writes=[bkk])
                        c.op('dve', lambda e: e.tensor_tensor_scan(out=b_[:], data0=msk[:], data1=f_[:], initial=0.0, op0=ALU.mult, op1=ALU.add),
                             reads=[bmsk, bf_], writes=[bb_])
                        b3 = b_[:].rearrange("p (c j) -> p c j", j=64)
                        c.op('dve', lambda e: e.tensor_tensor(out=e1[:].rearrange("p (c j) -> p c j", j=64), in0=b3, in1=b3[:, :, 31:32].broadcast_to([128, TBK // 64, 64]), op=ALU.subtract),
                             reads=[bb_], writes=[be1])
                        c.op('act', lambda e: e.activation(out=eq[:], in_=e1[:], func=AF.Exp), reads=[be1], writes=[beq])
                        c.op('act', lambda e: e.activation(out=ek[:], in_=e1[:], func=AF.Exp, scale=-1.0), reads=[be1], writes=[bek])
                        c.op('dve', lambda e: e.tensor_tensor(out=qt_[:], in0=q_[:], in1=eq[:], op=ALU.mult), reads=[bq_, beq], writes=[bqt_])
                        c.op('dve', lambda e: e.tensor_tensor(out=kt_[:], in0=kk[:], in1=ek[:], op=ALU.mult), reads=[bkk, bek], writes=[bkt_])
                        nch = TBK // 64
                        c.op('act', lambda e: e.activation(out=bmid[:, tb * nch:(tb + 1) * nch], in_=b3[:, :, 31], func=AF.Copy), reads=[bb_], writes=[bbmid])
                        c.op('act', lambda e: e.activation(out=blast[:, tb * nch:(tb + 1) * nch], in_=b3[:, :, 63], func=AF.Copy), reads=[bb_], writes=[bblast])
                        c.dma('sp', lambda e: e.dma_start(out=QK[h, r, 0, :, tb * TBK:(tb + 1) * TBK], in_=qt_[:]), reads=[bqt_], pwrites=[bQK])
                        c.dma('act', lambda e: e.dma_start(out=QK[h, r, 1, :, tb * TBK:(tb + 1) * TBK], in_=kt_[:]), reads=[bkt_], pwrites=[bQK])
                    c.op('dve', lambda e: e.tensor_tensor(out=blast[:], in0=blast[:], in1=bmid[:], op=ALU.subtract), reads=[bblast, bbmid], writes=[bblast])
                    c.op('dve', lambda e: e.tensor_tensor(out=blast[:, 0:127], in0=blast[:, 0:127], in1=bmid[:, 1:128], op=ALU.add), reads=[bblast, bbmid], writes=[bblast])
                    c.op('act', lambda e: e.activation(out=mcols[:, hr, :], in_=blast[:], func=AF.Exp), reads=[bblast], writes=[bmcols])
            bQK.seal()
            barrier(c)
        if stage < 2:
            return
        with ExitStack() as es:
            def T(name, shape, dt):
                return es.enter_context(nc.sbuf_tensor(uniq(name), shape, dt))
            mask = T("hr_mask", [128, 128], F32); bmask = Buf()
            c.op('pool', lambda e: e.memset(mask[:], 1.0), writes=[bmask])
            c.op('pool', lambda e: e.affine_select(out=mask[:], in_=mask[:], pattern=[[1, 128]], compare_op=ALU.is_ge, fill=0.0, base=0, channel_multiplier=-1),
                 reads=[bmask], writes=[bmask])
            c.op('pool', lambda e: e.memset(mask[0:64, 64:128], 0.0), reads=[bmask], writes=[bmask])
            ptb, bptb = g.psb

            class CH:
                pass
            chs = []
            for r in range(2):
                ch = CH(); chs.append(ch); ch.r = r

                def TT(name, shape, dt, r=r):
                    return (T("hr%d_%s" % (r, name), shape, dt), Buf())
                ch.qb = [TT("qb%d" % i, [128, TBK], BF16) for i in range(2)]
                ch.kb = [TT("kb%d" % i, [128, TBK], BF16) for i in range(2)]
                ch.vb = [TT("vb%d" % i, [128, TBK // 128, 128], BF16) for i in range(2)]
                ch.vnat = TT("vnat", [128, TBK // 128, 128], BF16)
                ch.attnT = [TT("attn%d" % i, [128, 128], BF16) for i in range(2)]
                ch.ktok = [TT("ktok%d" % i, [128, 128], BF16) for i in range(2)]
                ch.M32 = TT("M32", [128, 128], F32); ch.Mb = TT("Mb", [128, 128], BF16); ch.tmp32 = TT("tmp32", [128, 128], F32)
                ch.osb = [TT("osb%d" % i, [128, 128], F32) for i in range(3)]
                ch.osf = [TT("osf%d" % i, [128, 128], F32) for i in range(3)]
                ch.pa = g.ps[3 * r]; ch.po = g.ps[3 * r + 1]; ch.pk = g.ps[3 * r + 2]
                ch.nblk = 0

            def blk_gen(ch, h, tb, b, qb_, bqb_, kb_, bkb_, vb_, bvb_):
                r = ch.r; hr = r * 4 + h
                blk = tb * (TBK // 128) + b
                at_, bat_ = ch.attnT[ch.nblk % 2]; kt_, bkt_ = ch.ktok[ch.nblk % 2]
                os_, bos_ = ch.osb[ch.nblk % 3]; of_, bof_ = ch.osf[ch.nblk % 3]
                ch.nblk += 1
                pa, bpa = ch.pa; po, bpo = ch.po; pk, bpk = ch.pk
                M32, bM32 = ch.M32; Mb, bMb = ch.Mb; tmp32, btmp32 = ch.tmp32
                pcol = 128 * r
                bs = slice(b * 128, (b + 1) * 128)
                c.op('pe', lambda e: e.matmul(pa[:, 0:128], lhsT=kb_[:, bs], rhs=qb_[:, bs], start=True, stop=True), reads=[bkb_, bqb_], writes=[bpa]); yield
                c.op('dve', lambda e: e.tensor_tensor(out=at_[:], in0=pa[:, 0:128], in1=mask[:], op=ALU.mult), reads=[bpa, bmask], writes=[bat_]); yield
                c.op('pe', lambda e: e.transpose(out=ptb[:, pcol:pcol + 128], in_=kb_[:, bs], identity=g.identb[:]), reads=[bkb_, g.b_identb], writes=[bptb]); yield
                c.op('act', lambda e: e.activation(out=kt_[:], in_=ptb[:, pcol:pcol + 128], func=AF.Copy), reads=[bptb], writes=[bkt_]); yield
                for ci in range(2):
                    r0 = 64 * ci
                    cidx = 2 * blk + ci
                    c.op('pe', lambda e: e.matmul(po[r0:r0 + 64, 0:128], lhsT=at_[r0:r0 + 64, r0:r0 + 64], rhs=vb_[r0:r0 + 64, b, :], start=True, stop=False),
                         reads=[bat_, bvb_], writes=[bpo])
                    c.op('pe', lambda e: e.matmul(po[r0:r0 + 64, 0:128], lhsT=qb_[:, b * 128 + r0:b * 128 + r0 + 64], rhs=Mb[:, :], start=False, stop=True),
                         reads=[bqb_, bMb], writes=[bpo]); yield
                    if cidx < 127:
                        c.op('pe', lambda e: e.matmul(pk[:, 0:128], lhsT=kt_[r0:r0 + 64, :], rhs=vb_[r0:r0 + 64, b, :], start=True, stop=True), reads=[bkt_, bvb_], writes=[bpk]); yield
                        c.op('dve', lambda e: e.tensor_tensor(out=tmp32[:], in0=pk[:, 0:128], in1=M32[:], op=ALU.add), reads=[bpk, bM32], writes=[btmp32]); yield
                        c.op('dve', lambda e: e.tensor_scalar(out=Mb[:], in0=tmp32[:], scalar1=mcols[:, hr, cidx:cidx + 1], scalar2=None, op0=ALU.mult), reads=[btmp32, bmcols], writes=[bMb]); yield
                        c.op('dve', lambda e: e.tensor_scalar(out=M32[:], in0=tmp32[:], scalar1=mcols[:, hr, cidx:cidx + 1], scalar2=None, op0=ALU.mult), reads=[btmp32, bmcols], writes=[bM32]); yield
                c.op('act', lambda e: e.activation(out=os_[:], in_=po[:, 0:128], func=AF.Copy), reads=[bpo], writes=[bos_]); yield
                if r == 0:
                    c.dma('sp', lambda e: e.dma_start(out=OA[blk * 128:(blk + 1) * 128, h * 128:(h + 1) * 128], in_=os_[:]), reads=[bos_], pwrites=[bOAs[h]]); yield
                else:
                    pf, bpf = g.ps[6]
                    c.op('pe', lambda e: e.matmul(pf[:, 0:128], lhsT=g.J32[:], rhs=os_[:], start=True, stop=True), reads=[g.b_J32, bos_], writes=[bpf]); yield
                    c.op('act', lambda e: e.activation(out=of_[:], in_=pf[:, 0:128], func=AF.Copy), reads=[bpf], writes=[bof_]); yield
                    c.dma('act', lambda e: e.dma_start(out=OA2[L - (blk + 1) * 128:L - blk * 128, h * 128:(h + 1) * 128], in_=of_[:]), reads=[bof_], pwrites=[bOA2s[h]]); yield

            for h in range(4):
                for ch in chs:
                    M32, bM32 = ch.M32; Mb, bMb = ch.Mb
                    c.op('pool', lambda e: e.memset(M32[:], 0.0), reads=[bM32], writes=[bM32])
                    c.op('pool', lambda e: e.memset(Mb[:], 0.0), reads=[bMb], writes=[bMb])
                for tb in range(NTB):
                    loaded = []
                    for ch in chs:
                        r = ch.r
                        qb_, bqb_ = ch.qb[tb % 2]; kb_, bkb_ = ch.kb[tb % 2]; vb_, bvb_ = ch.vb[tb % 2]
                        c.dma('sp', lambda e: e.dma_start(out=qb_[:], in_=QK[h, r, 0, :, tb * TBK:(tb + 1) * TBK]), reads=[bQK], writes=[bqb_])
                        c.dma('act', lambda e: e.dma_start(out=kb_[:], in_=QK[h, r, 1, :, tb * TBK:(tb + 1) * TBK]), reads=[bQK], writes=[bkb_])
                        if r == 0:
                            vsrc = PV[tb * TBK:(tb + 1) * TBK, h * 128:(h + 1) * 128].rearrange("(b p) d -> p b d", p=128)
                            c.dma('sp', lambda e: e.dma_start(out=vb_[:], in_=vsrc), reads=[bPV], writes=[bvb_])
                        else:
                            vnat, bvnat = ch.vnat
                            vsrc = PV[L - (tb + 1) * TBK:L - tb * TBK, h * 128:(h + 1) * 128].rearrange("(b p) d -> p b d", p=128)
                            c.dma('sp', lambda e: e.dma_start(out=vnat[:], in_=vsrc), reads=[bPV], writes=[bvnat])
                            nbk = TBK // 128
                            for b4 in range(0, nbk, 4):
                                pf, bpf = g.ps[6]
                                c.op('pe', lambda e: e.matmul(pf[:, :], lhsT=g.Jb[:], rhs=vnat[:, b4:b4 + 4, :], start=True, stop=True), reads=[g.b_Jb, bvnat], writes=[bpf])
                                for bb in range(4):
                                    c.op('act', lambda e: e.activation(out=vb_[:, nbk - 1 - (b4 + bb), :], in_=pf[:, bb * 128:(bb + 1) * 128], func=AF.Copy), reads=[bpf, bvb_], writes=[bvb_])
                        loaded.append((qb_, bqb_, kb_, bkb_, vb_, bvb_))
                    for b in range(TBK // 128):
                        gens = [blk_gen(ch, h, tb, b, *loaded[i]) for i, ch in enumerate(chs)]
                        alive = list(gens)
                        while alive:
                            for gnr in list(alive):
                                try:
                                    next(gnr)
                                except StopIteration:
                                    alive.remove(gnr)
                bOAs[h].seal(); bOA2s[h].seal()
            barrier(c)
        if stage < 3:
            return
    gated_norm_finalize(c, g, OA, bOAs, PV, bPV, 512, a_out_norm, MT, bMT, 0, "hf_", OA2=OA2, bOA2s=bOA2s)


TB5 = 1024
NTB5 = L // TB5
TWO_PI = 2.0 * math.pi


def s5_phase(c, g, PT, bPT, lam_re, lam_im, log_step, b_re, b_im, c_re, c_im, d_skip, glu_w, glu_b, YT, bYT, MT, bMT, stage=3):
    nc = c.nc
    U0 = 1536
    with ExitStack() as es0:
        def T0(name, shape, dt):
            return es0.enter_context(nc.sbuf_tensor(uniq(name), shape, dt))
        WB = [T0("s5_WB%d" % p, [128, 2, 4, 128], BF16) for p in range(2)]; bWB = Buf()
        WC = [T0("s5_WC%d" % p, [128, 2, 4, 128], BF16) for p in range(2)]; bWC = Buf()
        WBx = [T0("s5_WBx%d" % p, [128, 2, 4, 128], BF16) for p in range(2)]
        WCx = [T0("s5_WCx%d" % p, [128, 2, 4, 64], BF16) for p in range(2)]
        mag = T0("s5_mag", [128, 32], F32); bmag = Buf()
        pwc = T0("s5_pwc", [128, 11, 32], F32); bpw = Buf()
        pws = T0("s5_pws", [128, 11, 32], F32)
        with ExitStack() as es:
            def T(name, shape, dt):
                return es.enter_context(nc.sbuf_tensor(uniq(name), shape, dt))
            n_ = [0]

            def S(shape=[128, 32], dt=F32):
                n_[0] += 1
                return T("s5_t%d" % n_[0], shape, dt), Buf()
            lamre, blamre = S(); lamim, blamim = S()
            lsB, blsB = S([128, 64]); ls, bls = S()
            with nc.allow_non_contiguous_dma(reason="small params"):
                for r_ in range(2):
                    for g4 in range(0, 16, 4):
                        c.dma('sp', lambda e: e.dma_start(out=lamre[:, r_ * 16 + g4:r_ * 16 + g4 + 4], in_=lam_re[r_, 2 * g4:2 * g4 + 8, :].rearrange("(gp gl) n -> (gl n) gp", gl=2)), pwrites=[blamre])
                        c.dma('act', lambda e: e.dma_start(out=lamim[:, r_ * 16 + g4:r_ * 16 + g4 + 4], in_=lam_im[r_, 2 * g4:2 * g4 + 8, :].rearrange("(gp gl) n -> (gl n) gp", gl=2)), pwrites=[blamim])
                blamre.seal(); blamim.seal()
                c.dma('sp', lambda e: e.dma_start(out=lsB[:], in_=log_step.rearrange("r g -> (r g)").partition_broadcast(128)), writes=[blsB])
            lsv = lsB[:].rearrange("p (r gp gl) -> p r gp gl", r=2, gl=2)
            c.op('dve', lambda e: e.tensor_copy(out=ls[0:64, :].rearrange("p (r gp) -> p r gp", r=2), in_=lsv[0:64, :, :, 0]), reads=[blsB], writes=[bls])
            c.op('dve', lambda e: e.tensor_copy(out=ls[64:128, :].rearrange("p (r gp) -> p r gp", r=2), in_=lsv[64:128, :, :, 1]), reads=[blsB], writes=[bls])
            step, bstep = S()
            c.op('act', lambda e: e.activation(out=step[:], in_=ls[:], func=AF.Exp), reads=[bls], writes=[bstep])
            lrs, blrs = S(); ang, bang = S()
            c.op('dve', lambda e: e.tensor_tensor(out=lrs[:], in0=lamre[:], in1=step[:], op=ALU.mult), reads=[blamre, bstep], writes=[blrs])
            c.op('act', lambda e: e.activation(out=mag[:], in_=lrs[:], func=AF.Exp), reads=[blrs], writes=[bmag])
            c.op('dve', lambda e: e.tensor_tensor(out=ang[:], in0=lamim[:], in1=step[:], op=ALU.mult), reads=[blamim, bstep], writes=[bang])

            def sin_of(src, bsrc, offset, dst, bdst):
                q, bq = S(); qi, bqi = S(dt=I32); r, br = S(); m, bm = S()
                c.op('dve', lambda e: e.tensor_scalar(out=q[:], in0=src[:], scalar1=offset, scalar2=1.0 / TWO_PI, op0=ALU.add, op1=ALU.mult), reads=[bsrc], writes=[bq])
                c.op('dve', lambda e: e.tensor_copy(out=qi[:], in_=q[:]), reads=[bq], writes=[bqi])
                c.op('dve', lambda e: e.tensor_copy(out=q[:], in_=qi[:]), reads=[bqi], writes=[bq])
                c.op('dve', lambda e: e.scalar_tensor_tensor(out=r[:], in0=q[:], scalar=-TWO_PI, in1=src[:], op0=ALU.mult, op1=ALU.add), reads=[bq, bsrc], writes=[br])
                if offset != 0.0:
                    c.op('dve', lambda e: e.tensor_scalar(out=r[:], in0=r[:], scalar1=offset, scalar2=None, op0=ALU.add), reads=[br], writes=[br])
                c.op('dve', lambda e: e.tensor_scalar(out=m[:], in0=r[:], scalar1=math.pi, scalar2=-TWO_PI, op0=ALU.is_gt, op1=ALU.mult), reads=[br], writes=[bm])
                c.op('dve', lambda e: e.tensor_tensor(out=r[:], in0=r[:], in1=m[:], op=ALU.add), reads=[br, bm], writes=[br])
                c.op('dve', lambda e: e.tensor_scalar(out=m[:], in0=r[:], scalar1=-math.pi, scalar2=TWO_PI, op0=ALU.is_lt, op1=ALU.mult), reads=[br], writes=[bm])
                c.op('dve', lambda e: e.tensor_tensor(out=r[:], in0=r[:], in1=m[:], op=ALU.add), reads=[br, bm], writes=[br])
                c.op('dve', lambda e: e.tensor_scalar(out=r[:], in0=r[:], scalar1=math.pi, scalar2=-math.pi, op0=ALU.min, op1=ALU.max), reads=[br], writes=[br])
                c.op('act', lambda e: e.activation(out=dst, in_=r[:], func=AF.Sin), reads=[br], writes=[bdst])
            sin_of(ang, bang, 0.0, pws[:, 0, :], bpw)
            sin_of(ang, bang, math.pi / 2, pwc[:, 0, :], bpw)
            tq, btq = S(); tq2, btq2 = S()
            for k in range(10):
                c.op('dve', lambda e: e.tensor_tensor(out=tq[:], in0=pwc[:, k, :], in1=pwc[:, k, :], op=ALU.mult), reads=[bpw], writes=[btq])
                c.op('dve', lambda e: e.tensor_tensor(out=tq2[:], in0=pws[:, k, :], in1=pws[:, k, :], op=ALU.mult), reads=[bpw], writes=[btq2])
                c.op('dve', lambda e: e.tensor_tensor(out=pwc[:, k + 1, :], in0=tq[:], in1=tq2[:], op=ALU.subtract), reads=[btq, btq2, bpw], writes=[bpw])
                c.op('dve', lambda e: e.tensor_tensor(out=tq[:], in0=pws[:, k, :], in1=pwc[:, k, :], op=ALU.mult), reads=[bpw], writes=[btq])
                c.op('dve', lambda e: e.tensor_scalar(out=pws[:, k + 1, :], in0=tq[:], scalar1=2.0, scalar2=None, op0=ALU.mult), reads=[btq, bpw], writes=[bpw])
            are, bare = S(); aim, baim = S(); den, bden = S(); am1, bam1 = S(); fr, bfr = S(); fi, bfi = S(); tt, btt = S()
            c.op('dve', lambda e: e.tensor_tensor(out=are[:], in0=mag[:], in1=pwc[:, 0, :], op=ALU.mult), reads=[bmag, bpw], writes=[bare])
            c.op('dve', lambda e: e.tensor_tensor(out=aim[:], in0=mag[:], in1=pws[:, 0, :], op=ALU.mult), reads=[bmag, bpw], writes=[baim])
            c.op('dve', lambda e: e.tensor_tensor(out=den[:], in0=lamre[:], in1=lamre[:], op=ALU.mult), reads=[blamre], writes=[bden])
            c.op('dve', lambda e: e.tensor_tensor(out=tt[:], in0=lamim[:], in1=lamim[:], op=ALU.mult), reads=[blamim], writes=[btt])
            c.op('dve', lambda e: e.tensor_tensor(out=den[:], in0=den[:], in1=tt[:], op=ALU.add), reads=[bden, btt], writes=[bden])
            c.op('dve', lambda e: e.reciprocal(out=den[:], in_=den[:]), reads=[bden], writes=[bden])
            c.op('dve', lambda e: e.tensor_scalar(out=am1[:], in0=are[:], scalar1=-1.0, scalar2=None, op0=ALU.add), reads=[bare], writes=[bam1])
            c.op('dve', lambda e: e.tensor_tensor(out=fr[:], in0=am1[:], in1=lamre[:], op=ALU.mult), reads=[bam1, blamre], writes=[bfr])
            c.op('dve', lambda e: e.tensor_tensor(out=tt[:], in0=aim[:], in1=lamim[:], op=ALU.mult), reads=[baim, blamim], writes=[btt])
            c.op('dve', lambda e: e.tensor_tensor(out=fr[:], in0=fr[:], in1=tt[:], op=ALU.add), reads=[bfr, btt], writes=[bfr])
            c.op('dve', lambda e: e.tensor_tensor(out=fr[:], in0=fr[:], in1=den[:], op=ALU.mult), reads=[bfr, bden], writes=[bfr])
            c.op('dve', lambda e: e.tensor_tensor(out=fi[:], in0=aim[:], in1=lamre[:], op=ALU.mult), reads=[baim, blamre], writes=[bfi])
            c.op('dve', lambda e: e.tensor_tensor(out=tt[:], in0=am1[:], in1=lamim[:], op=ALU.mult), reads=[bam1, blamim], writes=[btt])
            c.op('dve', lambda e: e.tensor_tensor(out=fi[:], in0=fi[:], in1=tt[:], op=ALU.subtract), reads=[bfi, btt], writes=[bfi])
            c.op('dve', lambda e: e.tensor_tensor(out=fi[:], in0=fi[:], in1=den[:], op=ALU.mult), reads=[bfi, bden], writes=[bfi])
            mk, bmk = S([128, 2])
            c.op('pool', lambda e: e.memset(mk[:], 0.0), writes=[bmk])
            c.op('pool', lambda e: e.memset(mk[0:64, 0:1], 1.0), reads=[bmk], writes=[bmk])
            c.op('pool', lambda e: e.memset(mk[64:128, 1:2], 1.0), reads=[bmk], writes=[bmk])
            Bn = [S([128, 2, 16, 16]) for _ in range(2)]
            with nc.allow_non_contiguous_dma(reason="small params"):
                for r_ in range(2):
                    for g4 in range(0, 16, 4):
                        c.dma('sp', lambda e: e.dma_start(out=Bn[0][0][:, r_, g4:g4 + 4, :], in_=b_re[r_, 2 * g4:2 * g4 + 8].rearrange("(gp gl) n p -> (gl n) gp p", gl=2)), pwrites=[Bn[0][1]])
                        c.dma('act', lambda e: e.dma_start(out=Bn[1][0][:, r_, g4:g4 + 4, :], in_=b_im[r_, 2 * g4:2 * g4 + 8].rearrange("(gp gl) n p -> (gl n) gp p", gl=2)), pwrites=[Bn[1][1]])
                Bn[0][1].seal(); Bn[1][1].seal()
            frb = fr[:].rearrange("p (r gp) -> p r gp", r=2).unsqueeze(3).broadcast_to([128, 2, 16, 16])
            fib = fi[:].rearrange("p (r gp) -> p r gp", r=2).unsqueeze(3).broadcast_to([128, 2, 16, 16])
            bbr, bbbr = S([128, 2, 16, 16]); bbi, bbbi = S([128, 2, 16, 16]); t5, bt5 = S([128, 2, 16, 16])
            c.op('dve', lambda e: e.tensor_tensor(out=bbr[:], in0=Bn[0][0][:], in1=frb, op=ALU.mult), reads=[Bn[0][1], bfr], writes=[bbbr])
            c.op('dve', lambda e: e.tensor_tensor(out=t5[:], in0=Bn[1][0][:], in1=fib, op=ALU.mult), reads=[Bn[1][1], bfi], writes=[bt5])
            c.op('dve', lambda e: e.tensor_tensor(out=bbr[:], in0=bbr[:], in1=t5[:], op=ALU.subtract), reads=[bbbr, bt5], writes=[bbbr])
            c.op('dve', lambda e: e.tensor_tensor(out=bbi[:], in0=Bn[1][0][:], in1=frb, op=ALU.mult), reads=[Bn[1][1], bfr], writes=[bbbi])
            c.op('dve', lambda e: e.tensor_tensor(out=t5[:], in0=Bn[0][0][:], in1=fib, op=ALU.mult), reads=[Bn[0][1], bfi], writes=[bt5])
            c.op('dve', lambda e: e.tensor_tensor(out=bbi[:], in0=bbi[:], in1=t5[:], op=ALU.add), reads=[bbbi, bt5], writes=[bbbi])
            BBm, bBBm = S([128, 2, 16, 2, 16], BF16)
            ptb, bptb = g.psb
            for part, (src, bsrc) in enumerate(((bbr, bbbr), (bbi, bbbi))):
                for gl in range(2):
                    c.op('dve', lambda e: e.tensor_scalar(out=BBm[:, :, :, gl, :], in0=src[:], scalar1=mk[:, gl:gl + 1], scalar2=None, op0=ALU.mult), reads=[bsrc, bmk, bBBm], writes=[bBBm])
                for r in range(2):
                    for cb in range(4):
                        c.op('pe', lambda e: e.transpose(out=ptb[:, 0:128], in_=BBm[:, r, 4 * cb:4 * cb + 4, :, :].rearrange("p a b c -> p (a b c)"), identity=g.identb[:]),
                             reads=[bBBm, g.b_identb], writes=[bptb])
                        c.op('act', lambda e: e.activation(out=WB[part][:, r, cb, :], in_=ptb[:, 0:128], func=AF.Copy), reads=[bptb], writes=[bWB])
            Cn = [S([128, 2, 4, 64]) for _ in range(2)]
            c.dma('sp', lambda e: e.dma_start(out=Cn[0][0][:], in_=c_re.rearrange("r (cb g8) p n -> (g8 p) r cb n", g8=8)), writes=[Cn[0][1]])
            c.dma('act', lambda e: e.dma_start(out=Cn[1][0][:], in_=c_im.rearrange("r (cb g8) p n -> (g8 p) r cb n", g8=8)), writes=[Cn[1][1]])
            Cd, bCd = S([128, 2, 4, 2, 64], BF16)
            mkb = mk[:].unsqueeze(1).unsqueeze(3).broadcast_to([128, 4, 2, 16])
            for part in range(2):
                sc = 1.0 if part == 0 else -1.0
                for x in range(2):
                    c.op('dve', lambda e: e.tensor_scalar(out=Cd[:, :, :, x, :], in0=Cn[part][0][:], scalar1=sc, scalar2=None, op0=ALU.mult), reads=[Cn[part][1], bCd], writes=[bCd])
                for r in range(2):
                    for cb in range(4):
                        c.op('pe', lambda e: e.transpose(out=ptb[:, 0:128], in_=Cd[:, r, cb, :, :].rearrange("p a b -> p (a b)"), identity=g.identb[:]),
                             reads=[bCd, g.b_identb], writes=[bptb])
                        c.op('dve', lambda e: e.tensor_tensor(out=WC[part][:, r, cb, :].rearrange("p (k a b) -> p k a b", k=4, a=2), in0=ptb[:, 0:128].rearrange("p (k a b) -> p k a b", k=4, a=2),
                                                              in1=mkb, op=ALU.mult), reads=[bptb, bmk], writes=[bWC])
            for part in range(2):
                c.op('act', lambda e: e.activation(out=WBx[part][64:128], in_=WB[part][64:128], func=AF.Copy), reads=[bWB], writes=[bWB])
                c.op('pool', lambda e: e.memset(WBx[part][64:96], 0.0), reads=[bWB], writes=[bWB])
                c.op('act', lambda e: e.activation(out=WCx[part][:], in_=WC[part][:, :, :, 64:128], func=AF.Copy), reads=[bWC], writes=[bWC])
                c.op('pool', lambda e: e.memset(WCx[part][:, :, :, 0:32], 0.0), reads=[bWC], writes=[bWC])
            barrier(c)
        if stage < 2:
            return
        with ExitStack() as es:
            def T(name, shape, dt):
                return es.enter_context(nc.sbuf_tensor(uniq(name), shape, dt))
            uf = T("s5_uf", [128, TB5 * 2], F32); buf_ = Buf()
            ub = [T("s5_ub%d" % r, [128, L], BF16) for r in range(2)]; bub = [Buf(), Buf()]
            Xa = [[T("s5_X%d%d" % (p, r), [128, L], BF16) for r in range(2)] for p in range(2)]
            bXa = [[Buf(), Buf()], [Buf(), Buf()]]
            tcos = T("s5_cos", [128, TB5], F32); tsin = T("s5_sin", [128, TB5], F32); btab = Buf()
            BUs = [T("s5_BU%d" % p, [128, TB5], F32) for p in range(2)]; bBUs = [Buf(), Buf()]
            t = [T("s5_w%d" % i, [128, TB5], F32) for i in range(4)]; bt = [Buf() for _ in range(4)]
            ini = T("s5_ini", [128, 4], F32); bini = Buf()
            yst = [T("s5_yst%d" % i, [128, 512], F32) for i in range(2)]; byst = [Buf(), Buf()]
            npz = 0
            for cb in range(4):
                for r in range(2):
                    for hh in range(L // (2 * TB5)):
                        nb = hh if r == 0 else L // (2 * TB5) - 1 - hh
                        c.dma('sp', lambda e: e.dma_start(out=uf[:], in_=PT[U0 + cb * 128:U0 + (cb + 1) * 128, nb * 2 * TB5:(nb + 1) * 2 * TB5]), reads=[bPT], writes=[buf_])
                        src = uf[:, ::-1] if r else uf[:, :]
                        c.op('act', lambda e: e.activation(out=ub[r][:, hh * 2 * TB5:(hh + 1) * 2 * TB5], in_=src, func=AF.Copy), reads=[buf_], writes=[bub[r]])
                for k in range(4):
                    gp = cb * 4 + k
                    for r in range(2):
                        col = r * 16 + gp
                        c.op('pool', lambda e: e.memset(tcos[:, 0:1], 1.0), writes=[btab])
                        c.op('pool', lambda e: e.memset(tsin[:, 0:1], 0.0), reads=[btab], writes=[btab])
                        n = 1
                        kk = 0
                        while n < TB5:
                            cr = pwc[:, kk, col:col + 1]; ci = pws[:, kk, col:col + 1]
                            c.op('dve', lambda e: e.tensor_scalar(out=t[0][:, 0:n], in0=tsin[:, 0:n], scalar1=ci, scalar2=None, op0=ALU.mult), reads=[btab, bpw], writes=[bt[0]])
                            c.op('dve', lambda e: e.tensor_scalar(out=t[1][:, 0:n], in0=tsin[:, 0:n], scalar1=cr, scalar2=None, op0=ALU.mult), reads=[btab, bpw], writes=[bt[1]])
                            c.op('dve', lambda e: e.scalar_tensor_tensor(out=tsin[:, n:2 * n], in0=tcos[:, 0:n], scalar=ci, in1=t[1][:, 0:n], op0=ALU.mult, op1=ALU.add),
                                 reads=[btab, bpw, bt[1]], writes=[btab])
                            c.op('dve', lambda e: e.scalar_tensor_tensor(out=tcos[:, n:2 * n], in0=tcos[:, 0:n], scalar=cr, in1=t[0][:, 0:n], op0=ALU.mult, op1=ALU.subtract),
                                 reads=[btab, bpw, bt[0]], writes=[btab])
                            n *= 2; kk += 1
                        cTB = pwc[:, kk, col:col + 1]; sTB = pws[:, kk, col:col + 1]
                        rho = mag[:, col:col + 1]
                        c.op('pool', lambda e: e.memset(ini[:], 0.0), writes=[bini])
                        for tb in range(NTB5):
                            ts0 = tb * TB5
                            for part in range(2):
                                for hf in range(TB5 // 512):
                                    ps, bps = g.ps[npz % 4]; npz += 1
                                    if k < 3:
                                        lh = WB[part][32 * k:32 * k + 32, r, cb, :]; rh = ub[r][32 * k:32 * k + 32, ts0 + hf * 512:ts0 + (hf + 1) * 512]
                                    else:
                                        lh = WBx[part][64:128, r, cb, :]; rh = ub[r][64:128, ts0 + hf * 512:ts0 + (hf + 1) * 512]
                                    c.op('pe', lambda e: e.matmul(ps[:, :], lhsT=lh, rhs=rh, start=True, stop=True),
                                         reads=[bWB, bub[r]], writes=[bps])
                                    c.op('act', lambda e: e.activation(out=BUs[part][:, hf * 512:(hf + 1) * 512], in_=ps[:, :], func=AF.Copy), reads=[bps], writes=[bBUs[part]])
                            c.op('dve', lambda e: e.tensor_tensor(out=t[0][:], in0=BUs[0][:], in1=tcos[:], op=ALU.mult), reads=[bBUs[0], btab], writes=[bt[0]])
                            c.op('dve', lambda e: e.tensor_tensor(out=t[1][:], in0=BUs[1][:], in1=tsin[:], op=ALU.mult), reads=[bBUs[1], btab], writes=[bt[1]])
                            c.op('dve', lambda e: e.tensor_tensor(out=t[0][:], in0=t[0][:], in1=t[1][:], op=ALU.add), reads=[bt[0], bt[1]], writes=[bt[0]])
                            c.op('pool', lambda e: e.tensor_tensor(out=t[2][:], in0=BUs[1][:], in1=tcos[:], op=ALU.mult), reads=[bBUs[1], btab], writes=[bt[2]])
                            c.op('pool', lambda e: e.tensor_tensor(out=t[3][:], in0=BUs[0][:], in1=tsin[:], op=ALU.mult), reads=[bBUs[0], btab], writes=[bt[3]])
                            c.op('dve', lambda e: e.tensor_tensor(out=t[2][:], in0=t[2][:], in1=t[3][:], op=ALU.subtract), reads=[bt[2], bt[3]], writes=[bt[2]])
                            c.op('dve', lambda e: e.tensor_tensor_scan(out=t[1][:], data0=rho.broadcast_to([128, TB5]), data1=t[0][:], initial=ini[:, 0:1], op0=ALU.mult, op1=ALU.add),
                                 reads=[bmag, bt[0], bini], writes=[bt[1]])
                            c.op('dve', lambda e: e.tensor_tensor_scan(out=t[3][:], data0=rho.broadcast_to([128, TB5]), data1=t[2][:], initial=ini[:, 1:2], op0=ALU.mult, op1=ALU.add),
                                 reads=[bmag, bt[2], bini], writes=[bt[3]])
                            if tb < NTB5 - 1:
                                xr = t[1][:, TB5 - 1:TB5]; xi = t[3][:, TB5 - 1:TB5]
                                c.op('dve', lambda e: e.tensor_scalar(out=ini[:, 2:3], in0=xi, scalar1=sTB, scalar2=None, op0=ALU.mult), reads=[bt[3], bpw], writes=[bini])
                                c.op('dve', lambda e: e.scalar_tensor_tensor(out=ini[:, 0:1], in0=xr, scalar=cTB, in1=ini[:, 2:3], op0=ALU.mult, op1=ALU.subtract), reads=[bt[1], bpw, bini], writes=[bini])
                                c.op('dve', lambda e: e.tensor_scalar(out=ini[:, 3:4], in0=xi, scalar1=cTB, scalar2=None, op0=ALU.mult), reads=[bt[3], bpw], writes=[bini])
                                c.op('dve', lambda e: e.scalar_tensor_tensor(out=ini[:, 1:2], in0=xr, scalar=sTB, in1=ini[:, 3:4], op0=ALU.mult, op1=ALU.add), reads=[bt[1], bpw, bini], writes=[bini])
                            if r == 0:
                                oslc = slice(ts0, ts0 + TB5)
                                xo_re = Xa[0][r][:, oslc]; xo_im = Xa[1][r][:, oslc]
                            else:
                                lo = L - ts0 - TB5
                                xo_re = Xa[0][r][:, lo:lo + TB5][:, ::-1]; xo_im = Xa[1][r][:, lo:lo + TB5][:, ::-1]
                            c.op('dve', lambda e: e.tensor_tensor(out=t[0][:], in0=t[1][:], in1=tcos[:], op=ALU.mult), reads=[bt[1], btab], writes=[bt[0]])
                            c.op('pool', lambda e: e.tensor_tensor(out=t[2][:], in0=t[3][:], in1=tsin[:], op=ALU.mult), reads=[bt[3], btab], writes=[bt[2]])
                            c.op('dve', lambda e: e.tensor_tensor(out=xo_re, in0=t[0][:], in1=t[2][:], op=ALU.subtract), reads=[bt[0], bt[2]], pwrites=[bXa[0][r]])
                            c.op('pool', lambda e: e.tensor_tensor(out=t[0][:], in0=t[1][:], in1=tsin[:], op=ALU.mult), reads=[bt[1], btab], writes=[bt[0]])
                            c.op('dve', lambda e: e.tensor_tensor(out=t[2][:], in0=t[3][:], in1=tcos[:], op=ALU.mult), reads=[bt[3], btab], writes=[bt[2]])
                            c.op('dve', lambda e: e.tensor_tensor(out=xo_im, in0=t[0][:], in1=t[2][:], op=ALU.add), reads=[bt[0], bt[2]], pwrites=[bXa[1][r]])
                        bXa[0][r].seal(); bXa[1][r].seal()
                    for it in range(L // 512):
                        ps, bps = g.ps[4 + it % 2]
                        i = 0
                        for r in range(2):
                            for part in range(2):
                                if k < 3:
                                    po = ps[32 * k:32 * k + 32, :]; lh = WC[part][:, r, cb, 32 * k:32 * k + 32]
                                else:
                                    po = ps[64:128, :]; lh = WCx[part][:, r, cb, :]
                                c.op('pe', lambda e: e.matmul(po, lhsT=lh, rhs=Xa[part][r][:, it * 512:(it + 1) * 512], start=(i == 0), stop=(i == 3)),
                                     reads=[bWC, bXa[part][r]], writes=[bps])
                                i += 1
                        ys, bys = yst[it % 2], byst[it % 2]
                        e0 = 32 * k if k < 3 else 64
                        c.op('act', lambda e: e.activation(out=ys[e0:32 * k + 32, :], in_=ps[e0:32 * k + 32, :], func=AF.Copy), reads=[bps], writes=[bys])
                        c.dma('sp', lambda e: e.dma_start(out=YT[cb * 128 + 32 * k:cb * 128 + 32 * k + 32, it * 512:(it + 1) * 512], in_=ys[32 * k:32 * k + 32, :]), reads=[bys], pwrites=[bYT])
            bYT.seal()
            barrier(c)
        if stage < 3:
            return
        with ExitStack() as es:
            def T(name, shape, dt):
                return es.enter_context(nc.sbuf_tensor(uniq(name), shape, dt))
            gw = T("s5_gw", [128, 4, 512], BF16); bgw = Buf()
            dcol = T("s5_dcol", [128, 4], F32); bdcol = Buf()
            gbc = T("s5_gbc", [128, 4], F32); bgbc = Buf()
            yt = [T("s5_yt%d" % i, [128, 4, 512], F32) for i in range(2)]; byt = [Buf(), Buf()]
            ut = [T("s5_ut%d" % i, [128, 4, 512], F32) for i in range(2)]; but = [Buf(), Buf()]
            sq = T("s5_sq", [128, 4, 512], F32); bsq = Buf()
            gy = T("s5_gy", [128, 4, 512], F32); bgy = Buf()
            gyb = T("s5_gyb", [128, 4, 512], BF16); bgyb = Buf()
            sg = [T("s5_sg%d" % i, [128, 512], F32) for i in range(2)]; bsg = [Buf(), Buf()]
            ob = [T("s5_ob%d" % i, [128, 4, 512], BF16) for i in range(2)]; bob = [Buf(), Buf()]
            c.dma('pool', lambda e: e.dma_start(out=gw[:], in_=glu_w.rearrange("(k p) f -> p k f", p=128)), writes=[bgw])
            with nc.allow_non_contiguous_dma(reason="small params"):
                c.dma('sp', lambda e: e.dma_start(out=dcol[:], in_=d_skip.rearrange("(k p) -> p k", p=128)), writes=[bdcol])
                c.dma('sp', lambda e: e.dma_start(out=gbc[:], in_=glu_b.rearrange("(k p) -> p k", p=128)), writes=[bgbc])
            GC = 1.5957691216057308
            for it in range(L // 512):
                yt_, byt_ = yt[it % 2], byt[it % 2]
                ut_, but_ = ut[it % 2], but[it % 2]
                ob_, bob_ = ob[it % 2], bob[it % 2]
                tsl = slice(it * 512, (it + 1) * 512)
                c.dma('sp', lambda e: e.dma_start(out=yt_[:], in_=YT[:, tsl].rearrange("(k p) t -> p k t", p=128)), reads=[bYT], writes=[byt_])
                c.dma('act', lambda e: e.dma_start(out=ut_[:], in_=PT[U0:U0 + 512, tsl].rearrange("(k p) t -> p k t", p=128)), reads=[bPT], writes=[but_])
                for k in range(4):
                    c.op('dve', lambda e: e.scalar_tensor_tensor(out=yt_[:, k, :], in0=ut_[:, k, :], scalar=dcol[:, k:k + 1], in1=yt_[:, k, :], op0=ALU.mult, op1=ALU.add),
                         reads=[but_, bdcol, byt_], writes=[byt_])
                c.op('act', lambda e: e.activation(out=sq[:], in_=yt_[:], func=AF.Square), reads=[byt_], writes=[bsq])
                c.op('dve', lambda e: e.tensor_scalar(out=sq[:], in0=sq[:], scalar1=0.044715, scalar2=1.0, op0=ALU.mult, op1=ALU.add), reads=[bsq], writes=[bsq])
                c.op('dve', lambda e: e.tensor_tensor(out=sq[:], in0=sq[:], in1=yt_[:], op=ALU.mult), reads=[bsq, byt_], writes=[bsq])
                c.op('act', lambda e: e.activation(out=sq[:], in_=sq[:], func=AF.Sigmoid, scale=GC), reads=[bsq], writes=[bsq])
                c.op('dve', lambda e: e.tensor_tensor(out=gy[:], in0=sq[:], in1=yt_[:], op=ALU.mult), reads=[bsq, byt_], writes=[bgy])
                c.op('act', lambda e: e.activation(out=gyb[:], in_=gy[:], func=AF.Copy), reads=[bgy], writes=[bgyb])
                for co in range(4):
                    ps, bps = g.ps[co % 4]
                    for k in range(4):
                        c.op('pe', lambda e: e.matmul(ps[:, :], lhsT=gw[:, k, co * 128:(co + 1) * 128], rhs=gyb[:, k, :], start=(k == 0), stop=(k == 3)),
                             reads=[bgw, bgyb], writes=[bps])
                    sg_, bsg_ = sg[co % 2], bsg[co % 2]
                    c.op('act', lambda e: e.activation(out=sg_[:], in_=ps[:, :], func=AF.Sigmoid, bias=gbc[:, co:co + 1], scale=1.0), reads=[bps, bgbc], writes=[bsg_])
                    c.op('dve', lambda e: e.tensor_tensor(out=ob_[:, co, :], in0=gy[:, co, :], in1=sg_[:], op=ALU.mult), reads=[bgy, bsg_, bob_], writes=[bob_])
                c.dma('sp', lambda e: e.dma_start(out=MT[512:1024, tsl].rearrange("(k p) t -> p k t", p=128), in_=ob_[:]), reads=[bob_], pwrites=[bMT])
            barrier(c)


def outproj_phase(c, g, MT, bMT, w_out, X, bX, tile_cb=None):
    nc = c.nc
    bXn = Buf('Xn')
    with ExitStack() as es:
        def T(name, shape, dt):
            return es.enter_context(nc.sbuf_tensor(uniq(name), shape, dt))
        wsb = T("op_w", [128, 8, D], BF16); bw = Buf()
        mt = [T("op_mt%d" % i, [128, 8, 512], BF16) for i in range(2)]; bmt = [Buf(), Buf()]
        xt = [T("op_xt%d" % i, [128, 4, D], F32) for i in range(2)]; bxt = [Buf(), Buf()]
        xo = [T("op_xo%d" % i, [128, 4, D], F32) for i in range(2)]; bxo = [Buf(), Buf()]
        for c0 in range(0, D, 512):
            c.dma('pool', lambda e: e.dma_start(out=wsb[:, :, c0:c0 + 512], in_=w_out[:, c0:c0 + 512].rearrange("(k p) f -> p k f", p=128)), pwrites=[bw])
        bw.seal()
        n = 0
        for it in range(L // 512):
            t0 = it * 512
            mt_, bmt_ = mt[it % 2], bmt[it % 2]
            xt_, bxt_ = xt[it % 2], bxt[it % 2]
            xo_, bxo_ = xo[it % 2], bxo[it % 2]
            c.dma('sp', lambda e: e.dma_start(out=mt_[:], in_=MT[:, t0:t0 + 512].rearrange("(k p) t -> p k t", p=128)), reads=[bMT], writes=[bmt_])
            c.dma('act', lambda e: e.dma_start(out=xt_[:], in_=X[t0:t0 + 512, :].rearrange("(j p) d -> p j d", p=128)), reads=[bX], writes=[bxt_])
            for j in range(4):
                for dh in range(2):
                    ps, bps = g.ps[n % 4]; n += 1
                    for k in range(8):
                        c.op('pe', lambda e: e.matmul(ps[:, :], lhsT=mt_[:, k, j * 128:(j + 1) * 128], rhs=wsb[:, k, dh * 512:(dh + 1) * 512], start=(k == 0), stop=(k == 7)),
                             reads=[bmt_, bw], writes=[bps])
                    c.op('dve', lambda e: e.tensor_tensor(out=xo_[:, j, dh * 512:(dh + 1) * 512], in0=ps[:, :], in1=xt_[:, j, dh * 512:(dh + 1) * 512], op=ALU.add),
                         reads=[bps, bxt_, bxo_], writes=[bxo_])
            c.dma('sp', lambda e: e.dma_start(out=X[t0:t0 + 512, :].rearrange("(j p) d -> p j d", p=128), in_=xo_[:]), reads=[bxo_], pwrites=[bXn])
            if tile_cb is not None:
                for j in range(4):
                    tile_cb(it * 4 + j, xo_[:, j, :], bxo_)
        bXn.seal()
        barrier(c)
    return bXn


def outproj_moe(c, g, MT, bMT, w_out, X, bX, HB, bHB, ffn_g, w_router, w_gate, w_up, w_down):
    with ExitStack() as es:
        sb = moe_alloc(c.nc, es, part=1)
        moe_prep(c, g, sb, ffn_g, w_router)
        bXn = outproj_phase(c, g, MT, bMT, w_out, X, bX, tile_cb=lambda i, xt, bxt: moe_step1_tile(c, g, sb, i, xt, bxt, HB, bHB))
        moe_alloc(c.nc, es, part=3, sb=sb)
        moe_rest(c, g, sb, X, bXn, HB, bHB, w_gate, w_up, w_down)
        barrier(c)
    return bXn


def t5_onehot():
    half = 16; max_exact = 8
    rel = np.arange(-255, 256)
    n = np.abs(rel)
    nf = np.maximum(n, 1).astype(np.float32)
    large = max_exact + (np.log(nf / np.float32(max_exact)) / np.float32(math.log(128 / max_exact)) * np.float32(half - max_exact)).astype(np.int32)
    large = np.minimum(large, half - 1)
    b = np.where(rel > 0, half, 0) + np.where(n < max_exact, n, large)
    oh = np.zeros((32, 512), np.float32)
    oh[b, np.arange(511)] = 1.0
    return oh


def attn_phase(c, g, PV, bPV, q_gain, k_gain, c_lambda, out_gain, rel_bias, onehot, layer_idx, QKT, bQKT, FV, bFV, MT, bMT, stage=3):
    nc = c.nc
    lam_init = 0.8 - 0.6 * math.exp(-0.3 * layer_idx)
    ptb, bptb = g.psb
    with ExitStack() as es:
        def T(name, shape, dt):
            return es.enter_context(nc.sbuf_tensor(uniq(name), shape, dt))
        g64 = T("at_g64", [128, 2, 64], F32); bg64 = Buf()
        gQK = T("at_gQK", [128, 16, 64], F32); bgQK = Buf()
        xq = [T("at_xq%d" % i, [128, 1024], BF16) for i in range(2)]; bxq = [Buf(), Buf()]
        sq = T("at_sq", [128, 1024], F32); bsq = Buf()
        ss = [T("at_ss%d" % i, [128, 16], F32) for i in range(2)]; bss = [Buf(), Buf()]
        xn = T("at_xn", [128, 1024], F32); bxn = Buf()
        xb = [T("at_xb%d" % i, [128, 1024], BF16) for i in range(2)]; bxb = [Buf(), Buf()]
        st = [T("at_st%d" % i, [128, 8, 512], BF16) for i in range(2)]; bst = [Buf(), Buf()]
        c.dma('sp', lambda e: e.dma_start(out=g64[:, 0, :], in_=q_gain.partition_broadcast(128)), pwrites=[bg64])
        c.dma('sp', lambda e: e.dma_start(out=g64[:, 1, :], in_=k_gain.partition_broadcast(128)), pwrites=[bg64])
        bg64.seal()
        c.op('dve', lambda e: e.tensor_scalar(out=gQK[:, 0:8, :], in0=g64[:, 0:1, :].broadcast_to([128, 8, 64]), scalar1=0.125, scalar2=None, op0=ALU.mult), reads=[bg64], writes=[bgQK])
        c.op('dve', lambda e: e.tensor_copy(out=gQK[:, 8:16, :], in_=g64[:, 1:2, :].broadcast_to([128, 8, 64])), reads=[bg64, bgQK], writes=[bgQK])
        for i in range(NT):
            xq_, bxq_ = xq[i % 2], bxq[i % 2]
            ss_, bss_ = ss[i % 2], bss[i % 2]
            xb_, bxb_ = xb[i % 2], bxb[i % 2]
            st_, bst_ = st[(i // 4) % 2], bst[(i // 4) % 2]
            c.dma('sp', lambda e: e.dma_start(out=xq_[:], in_=PV[i * 128:(i + 1) * 128, 0:1024]), reads=[bPV], writes=[bxq_])
            c.op('act', lambda e: e.activation(out=sq[:], in_=xq_[:], func=AF.Square), reads=[bxq_], writes=[bsq])
            c.op('dve', lambda e: e.tensor_reduce(out=ss_[:], in_=sq[:].rearrange("p (a d) -> p a d", d=64), axis=AX.X, op=ALU.add), reads=[bsq], writes=[bss_])
            c.op('dve', lambda e: e.tensor_scalar(out=ss_[:], in0=ss_[:], scalar1=1.0 / 64, scalar2=1e-6, op0=ALU.mult, op1=ALU.add), reads=[bss_], writes=[bss_])
            c.op('pool', lambda e: e.tensor_tensor(out=ss_[:], in0=ss_[:], in1=g.neghalf[:, 0:1].broadcast_to([128, 16]), op=ALU.pow), reads=[bss_, g.b_neghalf], writes=[bss_])
            c.op('dve', lambda e: e.tensor_tensor(out=xn[:].rearrange("p (a d) -> p a d", d=64), in0=xq_[:].rearrange("p (a d) -> p a d", d=64),
                                                  in1=ss_[:].unsqueeze(2).broadcast_to([128, 16, 64]), op=ALU.mult), reads=[bxq_, bss_], writes=[bxn])
            c.op('dve', lambda e: e.tensor_tensor(out=xb_[:], in0=xn[:], in1=gQK[:].rearrange("p a d -> p (a d)"), op=ALU.mult), reads=[bxn, bgQK], writes=[bxb_])
            for a in range(8):
                c.op('pe', lambda e: e.transpose(out=ptb[:, a * 128:(a + 1) * 128], in_=xb_[:, a * 128:(a + 1) * 128], identity=g.identb[:]), reads=[bxb_, g.b_identb], writes=[bptb])
            c.op('act', lambda e: e.activation(out=st_[:, :, (i % 4) * 128:(i % 4 + 1) * 128], in_=ptb[:, :].rearrange("p (a s) -> p a s", a=8), func=AF.Copy), reads=[bptb], writes=[bst_])
            if i % 4 == 3:
                t0 = (i // 4) * 512
                for a in range(8):
                    c.dma('sp' if a % 2 else 'act', lambda e: e.dma_start(out=QKT[a, :, t0:t0 + 512], in_=st_[:, a, :]), reads=[bst_], pwrites=[bQKT])
        bQKT.seal()
        barrier(c)
    if stage < 2:
        return
    with ExitStack() as es:
        def T(name, shape, dt):
            return es.enter_context(nc.sbuf_tensor(uniq(name), shape, dt))
        KT = T("at_KT", [128, L], BF16); bKT = Buf()
        Va = T("at_Va", [128, 64, 130], BF16); bVa = Buf()
        QT = [T("at_QT%d" % i, [128, 512], BF16) for i in range(2)]; bQT = [Buf(), Buf()]
        Pt = [T("at_P%d" % i, [128, 512], BF16) for i in range(4)]; bPt = [Buf() for _ in range(4)]
        tmp = [T("at_tmp%d" % i, [128, 512], F32) for i in range(2)]; btmp = [Buf(), Buf()]
        biasT = T("at_bias", [128, 4, 3, 128], F32); bbias = Buf()
        hank = T("at_hank", [128, 128], F32); bhank = Buf()
        cfar = T("at_cfar", [128, 4, 2], F32); bcfar = Buf()
        tab = T("at_tab", [32, 4], F32); btab = Buf()
        oh = T("at_oh", [32, 512], F32); boh = Buf()
        fv = T("at_fv", [4, 512], F32); bfv = Buf()
        lamt = T("at_lamt", [128, 4, 64], F32); blamt = Buf()
        lam = T("at_lam", [128, 8], F32); blam = Buf()
        gO = T("at_gO", [128, 128], F32); bgO = Buf()
        rs = [T("at_rs%d" % i, [128, 4], F32) for i in range(2)]; brs = [Buf(), Buf()]
        t1 = T("at_t1", [128, 128], F32); bt1 = Buf()
        w_ = T("at_w", [128, 128], F32); bw_ = Buf()
        junk = T("at_junk", [128, 128], F32); bjunk = Buf()
        wb = [T("at_wb%d" % i, [128, 128], BF16) for i in range(2)]; bwb = [Buf(), Buf()]
        ost = [T("at_ost%d" % i, [128, 512], BF16) for i in range(2)]; bost = [Buf(), Buf()]
        c.dma('sp', lambda e: e.dma_start(out=lamt[:].rearrange("p a d -> p (a d)"), in_=c_lambda.rearrange("a d -> (a d)").partition_broadcast(128)), writes=[blamt])
        c.op('dve', lambda e: e.tensor_tensor(out=lamt[:, 0, :], in0=lamt[:, 0, :], in1=lamt[:, 1, :], op=ALU.mult), reads=[blamt], writes=[blamt])
        c.op('dve', lambda e: e.tensor_tensor(out=lamt[:, 2, :], in0=lamt[:, 2, :], in1=lamt[:, 3, :], op=ALU.mult), reads=[blamt], writes=[blamt])
        c.op('dve', lambda e: e.tensor_reduce(out=lam[:, 0:1], in_=lamt[:, 0, :], axis=AX.X, op=ALU.add), reads=[blamt], writes=[blam])
        c.op('dve', lambda e: e.tensor_reduce(out=lam[:, 1:2], in_=lamt[:, 2, :], axis=AX.X, op=ALU.add), reads=[blamt, blam], writes=[blam])
        c.op('act', lambda e: e.activation(out=lam[:, 2:4], in_=lam[:, 0:2], func=AF.Exp), reads=[blam], writes=[blam])
        c.op('dve', lambda e: e.tensor_tensor(out=lam[:, 4:5], in0=lam[:, 3:4], in1=lam[:, 2:3], op=ALU.subtract), reads=[blam], writes=[blam])
        c.op('dve', lambda e: e.tensor_scalar(out=lam[:, 4:5], in0=lam[:, 4:5], scalar1=-lam_init, scalar2=None, op0=ALU.add), reads=[blam], writes=[blam])
        c.dma('sp', lambda e: e.dma_start(out=gO[:], in_=out_gain.partition_broadcast(128)), writes=[bgO])
        c.op('dve', lambda e: e.tensor_scalar(out=gO[:], in0=gO[:], scalar1=1.0 - lam_init, scalar2=None, op0=ALU.mult), reads=[bgO], writes=[bgO])
        c.dma('sp', lambda e: e.dma_start(out=tab[:], in_=rel_bias), writes=[btab])
        c.dma('act', lambda e: e.dma_start(out=oh[:], in_=onehot), writes=[boh])
        ps6, bps6 = g.ps[6]
        c.op('pe', lambda e: e.matmul(ps6[0:4, :], lhsT=tab[:, :], rhs=oh[:, :], start=True, stop=True), reads=[btab, boh], writes=[bps6])
        c.op('dve', lambda e: e.tensor_copy(out=fv[:], in_=ps6[0:4, :]), reads=[bps6], writes=[bfv])
        c.dma('sp', lambda e: e.dma_start(out=FV, in_=fv[:]), reads=[bfv], writes=[bFV])
        for h in range(4):
            for o in (-1, 0, 1):
                off = h * 512 + 128 * o + 128
                src = bass.AP(FV.tensor, off, [[1, 128], [1, 128]])
                c.dma('sp', lambda e: e.dma_start(out=hank[:], in_=src), reads=[bFV], writes=[bhank])
                c.op('dve', lambda e: e.tensor_copy(out=biasT[:, h, o + 1, :], in_=hank[:, ::-1]), reads=[bhank, bbias], writes=[bbias])
            c.dma('sp', lambda e: e.dma_start(out=cfar[:, h, 0:1], in_=bass.AP(FV.tensor, h * 512 + 0, [[0, 128], [1, 1]])), reads=[bFV], pwrites=[bcfar])
            c.dma('sp', lambda e: e.dma_start(out=cfar[:, h, 1:2], in_=bass.AP(FV.tensor, h * 512 + 510, [[0, 128], [1, 1]])), reads=[bFV], pwrites=[bcfar])
        bcfar.seal()
        ones_col_done = False
        pending = []
        nS = 0; nP = 0; nq = 0; ntmp = 0; nout = 0
        for h in range(4):
            c.dma('sp', lambda e: e.dma_start(out=KT[:], in_=QKT[4 + h, :, :]), reads=[bQKT], writes=[bKT])
            for half in range(2):
                c.dma('act', lambda e: e.dma_start(out=Va[:, half * 32:(half + 1) * 32, 0:128], in_=PV[half * 4096:(half + 1) * 4096, 1024 + h * 128:1024 + (h + 1) * 128].rearrange("(b p) d -> p b d", p=128)),
                      reads=[bPV], writes=[bVa])
            c.op('pool', lambda e: e.memset(Va[:, :, 128:129], 1.0), reads=[bVa], writes=[bVa])
            for qt in range(16):
                QT_, bQT_ = QT[nq % 2], bQT[nq % 2]; nq += 1
                c.dma('sp', lambda e: e.dma_start(out=QT_[:], in_=QKT[h, :, qt * 512:(qt + 1) * 512]), reads=[bQKT], writes=[bQT_])
                steps = [(comp, kb) for kb in range(64) for comp in range(2)]
                Sbank = {}

                SB = [0, 1, 2, 6]

                def emit_S(i):
                    comp, kb = steps[i]
                    S, bS = g.ps[SB[i % 4]]
                    c.op('pe', lambda e: e.matmul(S[:, :], lhsT=KT[64 * comp:64 * comp + 64, kb * 128:(kb + 1) * 128], rhs=QT_[64 * comp:64 * comp + 64, :], start=True, stop=True),
                         reads=[bKT, bQT_], writes=[bS])

                def emit_exp(i):
                    comp, kb = steps[i]
                    S, bS = g.ps[SB[i % 4]]
                    P_, bP_ = Pt[i % 4], bPt[i % 4]
                    near = (4 * qt - 1 <= kb <= 4 * qt + 4)
                    if not near:
                        col = cfar[:, h, 0:1] if kb < 4 * qt else cfar[:, h, 1:2]
                        c.op('act', lambda e: e.activation(out=P_[:], in_=S[:, :], func=AF.Exp, bias=col, scale=1.0), reads=[bS, bcfar], writes=[bP_])
                    else:
                        tm, btm = tmp[i % 2], btmp[i % 2]
                        for qs in range(4):
                            o = kb - (4 * qt + qs)
                            sl = slice(qs * 128, (qs + 1) * 128)
                            if abs(o) <= 1:
                                c.op('dve', lambda e: e.tensor_tensor(out=tm[:, sl], in0=S[:, sl], in1=biasT[:, h, o + 1, :], op=ALU.add), reads=[bS, bbias, btm], writes=[btm])
                            else:
                                col = cfar[:, h, 0:1] if o < 0 else cfar[:, h, 1:2]
                                c.op('dve', lambda e: e.tensor_scalar(out=tm[:, sl], in0=S[:, sl], scalar1=col, scalar2=None, op0=ALU.add), reads=[bS, bcfar, btm], writes=[btm])
                        c.op('act', lambda e: e.activation(out=P_[:], in_=tm[:], func=AF.Exp), reads=[btm], writes=[bP_])

                def emit_PV(i):
                    comp, kb = steps[i]
                    P_, bP_ = Pt[i % 4], bPt[i % 4]
                    for qs in range(4):
                        a = comp * 4 + qs
                        acc, bacc = g.ps[3 + a // 3]
                        c0 = (a % 3) * 130
                        first = (kb == 0) and ((comp == 0 and a in (0, 3)) or (comp == 1 and a == 6))
                        c.op('pe', lambda e: e.matmul(acc[:, c0:c0 + 129], lhsT=P_[:, qs * 128:(qs + 1) * 128], rhs=Va[:, kb, 0:129], start=first, stop=(kb == 63), skip_group_check=True),
                             reads=[bP_, bVa], writes=[bacc])
                npair = len(steps) // 2
                emit_S(0); emit_S(1); emit_S(2); emit_S(3)
                for j in range(npair):
                    emit_exp(2 * j); emit_exp(2 * j + 1)
                    if j + 2 < npair:
                        emit_S(2 * j + 4); emit_S(2 * j + 5)
                    if j == 0:
                        while pending:
                            pending.pop(0)()
                    emit_PV(2 * j); emit_PV(2 * j + 1)
                def make_fin(h=h, qt=qt, slot=nout):
                    def fin():
                        os_, bos_ = ost[slot % 2], bost[slot % 2]
                        for qs in range(4):
                            a0 = qs; a1 = 4 + qs
                            acc0, bacc0 = g.ps[3 + a0 // 3]; o0 = (a0 % 3) * 130
                            acc1, bacc1 = g.ps[3 + a1 // 3]; o1 = (a1 % 3) * 130
                            rs_, brs_ = rs[qs % 2], brs[qs % 2]
                            wb_, bwb_ = wb[qs % 2], bwb[qs % 2]
                            c.op('dve', lambda e: e.reciprocal(out=rs_[:, 0:1], in_=acc0[:, o0 + 128:o0 + 129]), reads=[bacc0], writes=[brs_])
                            c.op('dve', lambda e: e.reciprocal(out=rs_[:, 1:2], in_=acc1[:, o1 + 128:o1 + 129]), reads=[bacc1, brs_], writes=[brs_])
                            c.op('dve', lambda e: e.tensor_tensor(out=rs_[:, 1:2], in0=rs_[:, 1:2], in1=lam[:, 4:5], op=ALU.mult), reads=[brs_, blam], writes=[brs_])
                            c.op('dve', lambda e: e.tensor_scalar(out=t1[:], in0=acc1[:, o1:o1 + 128], scalar1=rs_[:, 1:2], scalar2=None, op0=ALU.mult), reads=[bacc1, brs_], writes=[bt1])
                            c.op('dve', lambda e: e.scalar_tensor_tensor(out=w_[:], in0=acc0[:, o0:o0 + 128], scalar=rs_[:, 0:1], in1=t1[:], op0=ALU.mult, op1=ALU.add), reads=[bacc0, brs_, bt1], writes=[bw_])
                            c.op('dve', lambda e: e.scalar_tensor_tensor(out=junk[:], in0=w_[:], scalar=1.0, in1=w_[:], op0=ALU.mult, op1=ALU.mult, accum_out=rs_[:, 2:3]), reads=[bw_, brs_], writes=[bjunk, brs_])
                            c.op('dve', lambda e: e.tensor_scalar(out=rs_[:, 2:3], in0=rs_[:, 2:3], scalar1=1.0 / 128, scalar2=1e-6, op0=ALU.mult, op1=ALU.add), reads=[brs_], writes=[brs_])
                            c.op('pool', lambda e: e.tensor_tensor(out=rs_[:, 2:3], in0=rs_[:, 2:3], in1=g.neghalf[:, 0:1], op=ALU.pow), reads=[brs_, g.b_neghalf], writes=[brs_])
                            c.op('dve', lambda e: e.scalar_tensor_tensor(out=wb_[:], in0=w_[:], scalar=rs_[:, 2:3], in1=gO[:], op0=ALU.mult, op1=ALU.mult), reads=[bw_, brs_, bgO], writes=[bwb_])
                            c.op('pe', lambda e: e.transpose(out=ptb[:, qs * 128:(qs + 1) * 128], in_=wb_[:], identity=g.identb[:]), reads=[bwb_, g.b_identb], writes=[bptb])
                        c.op('act', lambda e: e.activation(out=os_[:], in_=ptb[:, 0:512], func=AF.Copy), reads=[bptb], writes=[bos_])
                        c.dma('sp', lambda e: e.dma_start(out=MT[h * 128:(h + 1) * 128, qt * 512:(qt + 1) * 512], in_=os_[:]), reads=[bos_], pwrites=[bMT])
                    return fin
                nout += 1
                pending.append(make_fin())
        while pending:
            pending.pop(0)()
        barrier(c)


GTB = 512


def gdn_phase(c, g, PT, bPT, PV, bPV, conv_w, a_log, dt_bias, out_gain, GQ, bGQ, GR, bGR, OD, bODs, OD2, bOD2s, MT, bMT, stage=4):
    nc = c.nc
    ptb, bptb = g.psb
    NB = TBK
    with ExitStack() as es:
        def T(name, shape, dt):
            return es.enter_context(nc.sbuf_tensor(uniq(name), shape, dt))
        cw = T("gd_cw", [128, 12, 5], F32); bcw = Buf()
        onesb = T("gd_onesb", [128, 128], BF16); bonesb = Buf()
        xin = [T("gd_xin%d" % i, [128, NB + 4], F32) for i in range(2)]; bxin = [Buf(), Buf()]
        y = T("gd_y", [128, NB], F32); by = Buf()
        s = T("gd_s", [128, NB], F32); bs = Buf()
        sqb = T("gd_sqb", [128, NB], BF16); bsqb = Buf()
        rst = T("gd_rst", [128, NB], F32); brst = Buf()
        ob = [T("gd_ob%d" % i, [128, NB], BF16) for i in range(2)]; bob = [Buf(), Buf()]
        with nc.allow_non_contiguous_dma(reason="small params"):
            for j in range(5):
                c.dma('sp', lambda e: e.dma_start(out=cw[:, :, j], in_=conv_w[j, :].rearrange("(k p) -> p k", p=128)), pwrites=[bcw])
        bcw.seal()
        c.op('pool', lambda e: e.memset(onesb[:], 1.0), writes=[bonesb])
        n = 0
        for cbk in range(12):
            for tb in range(L // NB):
                x_, bx_ = xin[n % 2], bxin[n % 2]
                o_, bo_ = ob[n % 2], bob[n % 2]
                n += 1
                t0 = tb * NB
                lo = max(t0 - 2, 0); hi = min(t0 + NB + 2, L)
                if tb == 0:
                    c.op('pool', lambda e: e.memset(x_[:, 0:2], 0.0), writes=[bx_])
                if tb == L // NB - 1:
                    c.op('pool', lambda e: e.memset(x_[:, NB + 2:NB + 4], 0.0), writes=[bx_])
                c.dma('sp', lambda e: e.dma_start(out=x_[:, lo - (t0 - 2):hi - (t0 - 2)], in_=PT[cbk * 128:(cbk + 1) * 128, lo:hi]), reads=[bPT, bx_], writes=[bx_])
                c.op('dve', lambda e: e.tensor_scalar(out=y[:], in0=x_[:, 0:NB], scalar1=cw[:, cbk, 0:1], scalar2=None, op0=ALU.mult), reads=[bx_, bcw], writes=[by])
                for j in range(1, 5):
                    c.op('dve', lambda e: e.scalar_tensor_tensor(out=y[:], in0=x_[:, j:j + NB], scalar=cw[:, cbk, j:j + 1], in1=y[:], op0=ALU.mult, op1=ALU.add),
                         reads=[bx_, bcw, by], writes=[by])
                c.op('act', lambda e: e.activation(out=s[:], in_=y[:], func=AF.Silu), reads=[by], writes=[bs])
                if cbk < 8:
                    c.op('act', lambda e: e.activation(out=sqb[:], in_=s[:], func=AF.Square), reads=[bs], writes=[bsqb])
                    for hf in range(NB // 512):
                        ps, bps = g.ps[hf % 4]
                        c.op('pe', lambda e: e.matmul(ps[:, :], lhsT=onesb[:], rhs=sqb[:, hf * 512:(hf + 1) * 512], start=True, stop=True), reads=[bonesb, bsqb], writes=[bps])
                        c.op('dve', lambda e: e.tensor_scalar(out=rst[:, hf * 512:(hf + 1) * 512], in0=ps[:, :], scalar1=1e-6, scalar2=None, op0=ALU.add), reads=[bps, brst], writes=[brst])
                    c.op('act', lambda e: e.activation(out=rst[:], in_=rst[:], func=AF.Ln), reads=[brst], writes=[brst])
                    c.op('act', lambda e: e.activation(out=rst[:], in_=rst[:], func=AF.Exp, scale=-0.5), reads=[brst], writes=[brst])
                    sc = (128.0 ** -0.5) if cbk < 4 else 1.0
                    c.op('dve', lambda e: e.scalar_tensor_tensor(out=o_[:], in0=s[:], scalar=sc, in1=rst[:], op0=ALU.mult, op1=ALU.mult), reads=[bs, brst], writes=[bo_])
                else:
                    c.op('act', lambda e: e.activation(out=o_[:], in_=s[:], func=AF.Copy), reads=[bs], writes=[bo_])
                c.dma('act', lambda e: e.dma_start(out=GQ[cbk, :, t0:t0 + NB], in_=o_[:]), reads=[bo_], pwrites=[bGQ])
        bGQ.seal()
        barrier(c)
    if stage < 2:
        return
    with ExitStack() as es0:
        def T0(name, shape, dt):
            return es0.enter_context(nc.sbuf_tensor(uniq(name), shape, dt))
        NQ = 5
        cols = [T0("gd_cols%d" % d, [128, 64, 4 * NQ], F32) for d in range(2)]; bcols = [Buf(), Buf()]
        sel = T0("gd_sel", [4, 4, 128], F32); bsel = Buf()
        with ExitStack() as es:
            def T(name, shape, dt):
                return es.enter_context(nc.sbuf_tensor(uniq(name), shape, dt))
            GP = 2048
            ar = T("gd_ar", [4, GP], F32); bar_ = Buf()
            br = T("gd_br", [4, GP], F32); bbr = Buf()
            w1 = T("gd_w1", [4, GP], F32); bw1 = Buf()
            w2 = T("gd_w2", [4, GP], F32); bw2 = Buf()
            gam = T("gd_gam", [4, GP], F32); bet = T("gd_bet", [4, GP], F32); egam = T("gd_egam", [4, GP], F32); brw = Buf()
            q3 = T("gd_q3", [4, GP], F32); bq3 = Buf()
            q4 = T("gd_q4", [4, GP], F32); bq4 = Buf()
            q5 = T("gd_q5", [4, GP], F32); bq5 = Buf()
            msk = T("gd_msk", [4, GP], F32); bmsk = Buf()
            pc = T("gd_pc", [4, 4], F32); bpc = Buf()
            c.op('pool', lambda e: e.memset(msk[:], 1.0), writes=[bmsk])
            c.op('pool', lambda e: e.memset(msk[:].rearrange("p (c j) -> p c j", j=64)[:, :, 0:1], 0.0), reads=[bmsk], writes=[bmsk])
            c.op('pool', lambda e: e.memset(sel[:], 0.0), writes=[bsel])
            c.op('pool', lambda e: e.affine_select(out=sel[:], in_=sel[:], pattern=[[-1, 4], [0, 128]], compare_op=ALU.not_equal, fill=1.0, base=0, channel_multiplier=1),
                 reads=[bsel], writes=[bsel])
            for d in range(2):
                with nc.allow_non_contiguous_dma(reason="small params"):
                    c.dma('sp', lambda e: e.dma_start(out=pc[:, 0:1], in_=dt_bias[d, :].rearrange("(h o) -> h o", o=1)), reads=[bpc], writes=[bpc])
                    c.dma('sp', lambda e: e.dma_start(out=pc[:, 1:2], in_=a_log[d, :].rearrange("(h o) -> h o", o=1)), reads=[bpc], writes=[bpc])
                c.op('act', lambda e: e.activation(out=pc[:, 2:3], in_=pc[:, 1:2], func=AF.Exp), reads=[bpc], writes=[bpc])
                c.op('dve', lambda e: e.tensor_scalar(out=pc[:, 2:3], in0=pc[:, 2:3], scalar1=-1.0, scalar2=None, op0=ALU.mult), reads=[bpc], writes=[bpc])
                for tp in range(L // GP):
                    nbp = tp if d == 0 else L // GP - 1 - tp
                    c.dma('sp', lambda e: e.dma_start(out=ar[:], in_=PT[1536 + 4 * d:1540 + 4 * d, nbp * GP:(nbp + 1) * GP]), reads=[bPT, bar_], writes=[bar_])
                    c.dma('act', lambda e: e.dma_start(out=br[:], in_=PT[1544 + 4 * d:1548 + 4 * d, nbp * GP:(nbp + 1) * GP]), reads=[bPT, bbr], writes=[bbr])
                    asrc = ar[:, ::-1] if d else ar[:, :]
                    bsrc = br[:, ::-1] if d else br[:, :]
                    c.op('dve', lambda e: e.tensor_scalar(out=w1[:], in0=asrc, scalar1=pc[:, 0:1], scalar2=None, op0=ALU.add), reads=[bar_, bpc], writes=[bw1])
                    c.op('dve', lambda e: e.tensor_scalar(out=w2[:], in0=w1[:], scalar1=-1.0, scalar2=None, op0=ALU.mult), reads=[bw1], writes=[bw2])
                    c.op('dve', lambda e: e.tensor_tensor(out=w2[:], in0=w2[:], in1=w1[:], op=ALU.min), reads=[bw1, bw2], writes=[bw2])
                    c.op('act', lambda e: e.activation(out=w2[:], in_=w2[:], func=AF.Exp), reads=[bw2], writes=[bw2])
                    c.op('act', lambda e: e.activation(out=w2[:], in_=w2[:], func=AF.Ln, bias=1.0, scale=1.0), reads=[bw2], writes=[bw2])
                    c.op('dve', lambda e: e.scalar_tensor_tensor(out=w1[:], in0=w1[:], scalar=0.0, in1=w2[:], op0=ALU.max, op1=ALU.add), reads=[bw1, bw2], writes=[bw1])
                    c.op('dve', lambda e: e.tensor_scalar(out=w1[:], in0=w1[:], scalar1=pc[:, 2:3], scalar2=None, op0=ALU.mult), reads=[bw1, bpc], writes=[bw1])
                    c.op('dve', lambda e: e.tensor_tensor_scan(out=gam[:], data0=msk[:], data1=w1[:], initial=0.0, op0=ALU.mult, op1=ALU.add), reads=[bmsk, bw1, brw], writes=[brw])
                    c.op('act', lambda e: e.activation(out=bet[:], in_=bsrc, func=AF.Sigmoid), reads=[bbr, brw], writes=[brw])
                    c.op('act', lambda e: e.activation(out=egam[:], in_=gam[:], func=AF.Exp), reads=[brw], writes=[brw])
                    c.op('dve', lambda e: e.tensor_tensor(out=q3[:], in0=bet[:], in1=egam[:], op=ALU.mult), reads=[brw, bq3], writes=[bq3])
                    g3 = gam[:].rearrange("p (c j) -> p c j", j=64)
                    c.op('dve', lambda e: e.tensor_tensor(out=q4[:].rearrange("p (c j) -> p c j", j=64), in0=g3[:, :, 63:64].broadcast_to([4, GP // 64, 64]), in1=g3, op=ALU.subtract),
                         reads=[brw, bq4], writes=[bq4])
                    c.op('act', lambda e: e.activation(out=q4[:], in_=q4[:], func=AF.Exp), reads=[bq4], writes=[bq4])
                    c.op('dve', lambda e: e.tensor_scalar(out=q5[:], in0=gam[:], scalar1=-1.0, scalar2=None, op0=ALU.mult), reads=[brw, bq5], writes=[bq5])
                    quants = [(gam, brw), (bet, brw), (q3, bq3), (q4, bq4), (q5, bq5)]
                    for bl in range(GP // 128):
                        blk = tp * (GP // 128) + bl
                        pc_, bpc_ = g.ps[blk % 2]
                        for qi, (qt_, bq_) in enumerate(quants):
                            c.op('pe', lambda e: e.transpose(out=pc_[:, qi * 4:(qi + 1) * 4], in_=qt_[0:4, bl * 128:(bl + 1) * 128], identity=g.ident32[0:4, 0:4]),
                                 reads=[bq_, g.b_ident32], writes=[bpc_])
                        c.op('act', lambda e: e.activation(out=cols[d][:, blk, :], in_=pc_[:, 0:4 * NQ], func=AF.Copy), reads=[bpc_, bcols[d]], writes=[bcols[d]])
                    for qi, rt in enumerate((gam, bet, egam)):
                        c.dma('sp', lambda e: e.dma_start(out=GR[d, qi, :, tp * GP:(tp + 1) * GP], in_=rt[:]), reads=[brw], pwrites=[bGR])
            bGR.seal()
            barrier(c)
        if stage < 3:
            return
        with ExitStack() as es:
            def T(name, shape, dt):
                return es.enter_context(nc.sbuf_tensor(uniq(name), shape, dt))
            nm_le = T("gm_nmle", [128, 128], F32)
            nm_geT = T("gm_nmgeT", [128, 128], F32)
            m_stT = T("gm_mstT", [128, 128], F32)
            bmk = Buf()
            c.op('pool', lambda e: e.memset(nm_le[:], 0.0), writes=[bmk])
            c.op('pool', lambda e: e.affine_select(out=nm_le[:], in_=nm_le[:], pattern=[[-1, 128]], compare_op=ALU.is_gt, fill=-30000.0, base=0, channel_multiplier=1), reads=[bmk], writes=[bmk])
            c.op('pool', lambda e: e.memset(nm_le[64:128, 0:64], -30000.0), reads=[bmk], writes=[bmk])
            c.op('pool', lambda e: e.memset(nm_geT[:], 0.0), reads=[bmk], writes=[bmk])
            c.op('pool', lambda e: e.affine_select(out=nm_geT[:], in_=nm_geT[:], pattern=[[1, 128]], compare_op=ALU.is_ge, fill=-30000.0, base=0, channel_multiplier=-1), reads=[bmk], writes=[bmk])
            c.op('pool', lambda e: e.memset(nm_geT[0:64, 64:128], -30000.0), reads=[bmk], writes=[bmk])
            c.op('pool', lambda e: e.memset(m_stT[:], 1.0), reads=[bmk], writes=[bmk])
            c.op('pool', lambda e: e.affine_select(out=m_stT[:], in_=m_stT[:], pattern=[[1, 128]], compare_op=ALU.is_gt, fill=0.0, base=0, channel_multiplier=-1), reads=[bmk], writes=[bmk])

            class CH:
                pass
            chs = []
            for d in range(2):
                ch = CH(); chs.append(ch)
                ch.d = d

                def TT(name, shape, dt, d=d):
                    return (T("gm%d_%s" % (d, name), shape, dt), Buf())
                ch.nat = [TT("nat%d" % i, [128, GTB], BF16) for i in range(3)]
                ch.arr = [[TT("arr%d_%d" % (i, j), [128, GTB], BF16) for j in range(2)] for i in range(3)]
                ch.rts = [[TT("rt%d_%d" % (q, j), [4, GTB], F32) for j in range(2)] for q in range(3)]
                ch.S32 = TT("S32", [128, 128], F32); ch.Sb = TT("Sb", [128, 128], BF16)
                ch.tmpD = TT("tmpD", [128, 128], F32); ch.Dst = TT("Dst", [128, 128], F32); ch.DTi = TT("DTi", [128, 128], F32); ch.DTs = TT("DTs", [128, 128], F32)
                ch.A_ = TT("A", [128, 128], BF16); ch.AT_ = TT("AT", [128, 128], BF16); ch.atT = TT("attnT", [128, 128], BF16)
                ch.Pm = [TT("P%d" % i, [128, 128], BF16) for i in range(6)]
                ch.Qm = [TT("Q%d" % i, [128, 128], BF16) for i in range(5)]
                ch.W32 = TT("W32", [128, 256], F32); ch.Wb = TT("Wb", [128, 256], BF16)
                ch.kdec = TT("kdec", [128, 128], BF16); ch.kcT = TT("kcT", [128, 128], BF16); ch.qdec = TT("qdec", [128, 128], BF16); ch.vnew = TT("vnew", [128, 128], BF16)
                ch.elc = TT("elc", [128, 2], F32)
                ch.osb = [TT("osb%d" % i, [128, 128], F32) for i in range(2)]
                ch.osf = [TT("osf%d" % i, [128, 128], F32) for i in range(2)]
                ch.bA = g.ps[3 * d + 0]; ch.bB = g.ps[3 * d + 1]; ch.bC = g.ps[3 * d + 2]
                ch.pb0 = 512 * d
                ch.nblk = 0

            def block_gen(ch, h, blk, b, cur, rcur):
                d = ch.d
                (qA, bqA), (kA, bkA), (vA, bvA) = cur
                bs_ = slice(b * 128, (b + 1) * 128)
                cl = cols[d][:, blk, :]
                gcol = cl[:, 0 + h:0 + h + 1]; bcol = cl[:, 4 + h:4 + h + 1]; begcol = cl[:, 8 + h:8 + h + 1]
                ekdcol = cl[:, 12 + h:12 + h + 1]; ngcol = cl[:, 16 + h:16 + h + 1]
                pA, bpA = ch.bA; pB, bpB = ch.bB; pC, bpC = ch.bC
                pb0 = ch.pb0
                tmpD, btmpD = ch.tmpD; Dst, bDst = ch.Dst; DTi, bDTi = ch.DTi; DTs, bDTs = ch.DTs
                A_, bA_ = ch.A_; AT_, bAT_ = ch.AT_; atT, batT = ch.atT
                W32, bW32 = ch.W32; Wb, bWb = ch.Wb; kdec, bkdec = ch.kdec; kcT, bkcT = ch.kcT; qdec, bqdec = ch.qdec; vnew, bvnew = ch.vnew
                elc, belc = ch.elc; S32, bS32 = ch.S32; Sb, bSb = ch.Sb
                for qi, (rt, brt) in enumerate(rcur):
                    c.op('pe', lambda e: e.matmul(pA[:, qi * 128:(qi + 1) * 128], lhsT=sel[:, h, :], rhs=rt[0:4, bs_], start=True, stop=True, skip_group_check=True),
                         reads=[bsel, brt], writes=[bpA])
                    yield
                c.op('pe', lambda e: e.matmul(pB[:, 0:128], lhsT=kA[:, bs_], rhs=kA[:, bs_], start=True, stop=True, skip_group_check=True), reads=[bkA], writes=[bpB]); yield
                c.op('pe', lambda e: e.matmul(pB[:, 128:256], lhsT=kA[:, bs_], rhs=qA[:, bs_], start=True, stop=True, skip_group_check=True), reads=[bkA, bqA], writes=[bpB]); yield
                c.op('pe', lambda e: e.transpose(out=ptb[:, pb0:pb0 + 128], in_=vA[:, bs_], identity=g.identb[:]), reads=[bvA, g.b_identb], writes=[bptb]); yield
                c.op('pe', lambda e: e.transpose(out=ptb[:, pb0 + 128:pb0 + 256], in_=kA[:, bs_], identity=g.identb[:]), reads=[bkA, g.b_identb], writes=[bptb]); yield
                c.op('dve', lambda e: e.scalar_tensor_tensor(out=tmpD[:], in0=pA[:, 0:128], scalar=-1.0, in1=nm_le[:], op0=ALU.mult, op1=ALU.add), reads=[bpA, bmk], writes=[btmpD]); yield
                c.op('act', lambda e: e.activation(out=Dst[:], in_=tmpD[:], func=AF.Exp, bias=gcol, scale=1.0), reads=[btmpD, bcols[d]], writes=[bDst]); yield
                c.op('dve', lambda e: e.tensor_tensor(out=tmpD[:], in0=pA[:, 0:128], in1=nm_geT[:], op=ALU.add), reads=[bpA, bmk, btmpD], writes=[btmpD]); yield
                c.op('act', lambda e: e.activation(out=DTi[:], in_=tmpD[:], func=AF.Exp, bias=ngcol, scale=1.0), reads=[btmpD, bcols[d]], writes=[bDTi]); yield
                c.op('dve', lambda e: e.tensor_tensor(out=DTs[:], in0=DTi[:], in1=m_stT[:], op=ALU.mult), reads=[bDTi, bmk], writes=[bDTs]); yield
                c.op('dve', lambda e: e.tensor_tensor(out=DTs[:], in0=pA[:, 128:256], in1=DTs[:], op=ALU.mult), reads=[bpA, bDTs], writes=[bDTs]); yield
                c.op('dve', lambda e: e.scalar_tensor_tensor(out=A_[:], in0=pB[:, 0:128], scalar=bcol, in1=Dst[:], op0=ALU.mult, op1=ALU.mult), reads=[bpB, bcols[d], bDst], writes=[bA_]); yield
                c.op('dve', lambda e: e.tensor_tensor(out=AT_[:], in0=pB[:, 0:128], in1=DTs[:], op=ALU.mult), reads=[bpB, bDTs], writes=[bAT_]); yield
                c.op('dve', lambda e: e.tensor_tensor(out=atT[:], in0=pB[:, 128:256], in1=DTi[:], op=ALU.mult), reads=[bpB, bDTi], writes=[batT]); yield
                c.op('act', lambda e: e.activation(out=Wb[:, 0:128], in_=ptb[:, pb0:pb0 + 128], func=AF.Copy, scale=bcol), reads=[bptb, bcols[d], bWb], writes=[bWb]); yield
                c.op('act', lambda e: e.activation(out=Wb[:, 128:256], in_=ptb[:, pb0 + 128:pb0 + 256], func=AF.Copy, scale=begcol), reads=[bptb, bcols[d], bWb], writes=[bWb]); yield
                c.op('act', lambda e: e.activation(out=kdec[:], in_=ptb[:, pb0 + 128:pb0 + 256], func=AF.Copy, scale=ekdcol), reads=[bptb, bcols[d]], writes=[bkdec]); yield
                c.op('dve', lambda e: e.tensor_tensor(out=qdec[:], in0=pA[:, 256:384], in1=qA[:, bs_], op=ALU.mult), reads=[bpA, bqA], writes=[bqdec]); yield
                c.op('act', lambda e: e.activation(out=elc[:], in_=pA[:, 256:384].rearrange("p (c j) -> p c j", j=64)[:, :, 63], func=AF.Copy), reads=[bpA], writes=[belc]); yield
                Pc, bPc = AT_, bAT_
                Qc, bQc = A_, bA_
                for lev in range(6):
                    c.op('pe', lambda e: e.matmul(pC[:, 0:256], lhsT=Pc[:], rhs=Wb[:], start=True, stop=True, skip_group_check=True), reads=[bPc, bWb], writes=[bpC]); yield
                    c.op('dve', lambda e: e.tensor_tensor(out=Wb[:], in0=Wb[:], in1=pC[:, 0:256], op=(ALU.subtract if lev == 0 else ALU.add)), reads=[bWb, bpC], writes=[bWb]); yield
                    if lev < 5:
                        Pn, bPn = ch.Pm[lev + 1]
                        c.op('pe', lambda e: e.matmul(pB[:, 256:384], lhsT=Qc[:], rhs=Pc[:], start=True, stop=True, skip_group_check=True), reads=[bQc, bPc], writes=[bpB]); yield
                        if lev < 4:
                            Qn, bQn = ch.Qm[lev + 1]
                            c.op('pe', lambda e: e.matmul(pB[:, 384:512], lhsT=Pc[:], rhs=Qc[:], start=True, stop=True, skip_group_check=True), reads=[bQc, bPc], writes=[bpB]); yield
                            c.op('act', lambda e: e.activation(out=Qn[:], in_=pB[:, 384:512], func=AF.Copy), reads=[bpB], writes=[bQn]); yield
                        c.op('act', lambda e: e.activation(out=Pn[:], in_=pB[:, 256:384], func=AF.Copy), reads=[bpB], writes=[bPn]); yield
                        Pc, bPc = Pn, bPn
                        if lev < 4:
                            Qc, bQc = Qn, bQn
                c.op('pe', lambda e: e.transpose(out=ptb[:, pb0 + 256:pb0 + 384], in_=Wb[:, 128:256], identity=g.identb[:]), reads=[bWb, g.b_identb], writes=[bptb]); yield
                c.op('act', lambda e: e.activation(out=kcT[:], in_=ptb[:, pb0 + 256:pb0 + 384], func=AF.Copy), reads=[bptb], writes=[bkcT]); yield
                for ci in range(2):
                    r0 = 64 * ci
                    c.op('pe', lambda e: e.matmul(pC[r0:r0 + 64, 256:384], lhsT=kcT[:, r0:r0 + 64], rhs=Sb[:, :], start=True, stop=True, skip_group_check=True), reads=[bkcT, bSb], writes=[bpC]); yield
                    c.op('dve', lambda e: e.tensor_tensor(out=vnew[r0:r0 + 64, :], in0=Wb[r0:r0 + 64, 0:128], in1=pC[r0:r0 + 64, 256:384], op=ALU.subtract), reads=[bWb, bpC, bvnew], writes=[bvnew]); yield
                    c.op('pe', lambda e: e.matmul(pA[r0:r0 + 64, 384:512], lhsT=qdec[:, r0:r0 + 64], rhs=Sb[:, :], start=True, stop=False, skip_group_check=True), reads=[bqdec, bSb], writes=[bpA])
                    c.op('pe', lambda e: e.matmul(pA[r0:r0 + 64, 384:512], lhsT=atT[r0:r0 + 64, r0:r0 + 64], rhs=vnew[r0:r0 + 64, :], start=False, stop=True, skip_group_check=True), reads=[batT, bvnew], writes=[bpA]); yield
                    c.op('pe', lambda e: e.matmul(pC[:, 384:512], lhsT=kdec[r0:r0 + 64, :], rhs=vnew[r0:r0 + 64, :], start=True, stop=True, skip_group_check=True), reads=[bkdec, bvnew], writes=[bpC]); yield
                    c.op('dve', lambda e: e.scalar_tensor_tensor(out=Sb[:], in0=S32[:], scalar=elc[:, ci:ci + 1], in1=pC[:, 384:512], op0=ALU.mult, op1=ALU.add), reads=[bS32, belc, bpC], writes=[bSb]); yield
                    c.op('dve', lambda e: e.scalar_tensor_tensor(out=S32[:], in0=S32[:], scalar=elc[:, ci:ci + 1], in1=pC[:, 384:512], op0=ALU.mult, op1=ALU.add), reads=[bS32, belc, bpC], writes=[bS32]); yield
                os_, bos_ = ch.osb[ch.nblk % 2]
                of_, bof_ = ch.osf[ch.nblk % 2]
                ch.nblk += 1
                c.op('act', lambda e: e.activation(out=os_[:], in_=pA[:, 384:512], func=AF.Copy), reads=[bpA], writes=[bos_]); yield
                if d == 0:
                    c.dma('sp', lambda e: e.dma_start(out=OD[blk * 128:(blk + 1) * 128, h * 128:(h + 1) * 128], in_=os_[:]), reads=[bos_], pwrites=[bODs[h]]); yield
                else:
                    pf, bpf = g.ps[6]
                    c.op('pe', lambda e: e.matmul(pf[:, 0:128], lhsT=g.J32[:], rhs=os_[:], start=True, stop=True), reads=[g.b_J32, bos_], writes=[bpf]); yield
                    c.op('act', lambda e: e.activation(out=of_[:], in_=pf[:, 0:128], func=AF.Copy), reads=[bpf], writes=[bof_]); yield
                    c.dma('act', lambda e: e.dma_start(out=OD2[L - (blk + 1) * 128:L - blk * 128, h * 128:(h + 1) * 128], in_=of_[:]), reads=[bof_], pwrites=[bOD2s[h]]); yield

            for h in range(4):
                for ch in chs:
                    S32, bS32 = ch.S32; Sb, bSb = ch.Sb
                    c.op('pool', lambda e: e.memset(S32[:], 0.0), reads=[bS32], writes=[bS32])
                    c.op('pool', lambda e: e.memset(Sb[:], 0.0), reads=[bSb], writes=[bSb])
                for tb in range(L // GTB):
                    curs = []; rcurs = []
                    for ch in chs:
                        d = ch.d
                        cur = []
                        for ai in range(3):
                            a_, ba_ = ch.arr[ai][tb % 2]
                            if d == 0:
                                c.dma('sp' if ai % 2 else 'act', lambda e: e.dma_start(out=a_[:], in_=GQ[ai * 4 + h, :, tb * GTB:(tb + 1) * GTB]), reads=[bGQ], writes=[ba_])
                            else:
                                n_, bn_ = ch.nat[ai]
                                c.dma('sp' if ai % 2 else 'act', lambda e: e.dma_start(out=n_[:], in_=GQ[ai * 4 + h, :, L - (tb + 1) * GTB:L - tb * GTB]), reads=[bGQ], writes=[bn_])
                                c.op('pool', lambda e: e.tensor_copy(out=a_[:], in_=n_[:, ::-1]), reads=[bn_], writes=[ba_])
                            cur.append((a_, ba_))
                        rcur = []
                        for qi in range(3):
                            r_, br_ = ch.rts[qi][tb % 2]
                            c.dma('sp', lambda e: e.dma_start(out=r_[:], in_=GR[d, qi, :, tb * GTB:(tb + 1) * GTB]), reads=[bGR], writes=[br_])
                            rcur.append((r_, br_))
                        curs.append(cur); rcurs.append(rcur)
                    for b in range(GTB // 128):
                        blk = tb * (GTB // 128) + b
                        gens = [block_gen(ch, h, blk, b, curs[i], rcurs[i]) for i, ch in enumerate(chs)]
                        alive = list(gens)
                        while alive:
                            for gnr in list(alive):
                                try:
                                    next(gnr)
                                except StopIteration:
                                    alive.remove(gnr)
                bODs[h].seal(); bOD2s[h].seal()
            barrier(c)
    if stage < 4:
        return
    gated_norm_finalize(c, g, OD, bODs, PV, bPV, 1536, out_gain, MT, bMT, 512, "gf_", OA2=OD2, bOA2s=bOD2s)


N_ACTIVE = 4
DEPTH = 4

PARAM_NAMES = ['mix_norm', 'ffn_norm', 'ev_w_in', 'ev_w_out', 'a_lb_logits', 'a_out_norm', 's5_lambda_re', 's5_lambda_im',
               's5_log_step', 's5_b_re', 's5_b_im', 's5_c_re', 's5_c_im', 's5_d', 's5_glu_w', 's5_glu_b', 'od_w_in', 'od_w_out',
               'c_q_norm', 'c_k_norm', 'c_lambda', 'c_out_norm', 'rel_bias', 'd_conv_w', 'd_a_log', 'd_dt_bias', 'd_out_norm',
               'moe_router', 'moe_w_gate', 'moe_w_up', 'moe_w_down']

PARAM_SHAPES = {
    'mix_norm': (4, 1024), 'ffn_norm': (4, 1024), 'ev_w_in': (2, 1024, 3072), 'ev_w_out': (2, 1024, 1024), 'a_lb_logits': (2, 2, 512),
    'a_out_norm': (2, 128), 's5_lambda_re': (2, 2, 32, 64), 's5_lambda_im': (2, 2, 32, 64), 's5_log_step': (2, 2, 32),
    's5_b_re': (2, 2, 32, 64, 16), 's5_b_im': (2, 2, 32, 64, 16), 's5_c_re': (2, 2, 32, 16, 64), 's5_c_im': (2, 2, 32, 16, 64),
    's5_d': (2, 512), 's5_glu_w': (2, 512, 512), 's5_glu_b': (2, 512), 'od_w_in': (2, 1024, 3600), 'od_w_out': (2, 1024, 1024),
    'c_q_norm': (2, 64), 'c_k_norm': (2, 64), 'c_lambda': (2, 4, 64), 'c_out_norm': (2, 128), 'rel_bias': (32, 4),
    'd_conv_w': (2, 5, 1536), 'd_a_log': (2, 2, 4), 'd_dt_bias': (2, 2, 4), 'd_out_norm': (2, 128), 'moe_router': (4, 1024, 16),
    'moe_w_gate': (4, 16, 1024, 2048), 'moe_w_up': (4, 16, 1024, 2048), 'moe_w_down': (4, 16, 2048, 1024)}


def build_program(layers=range(DEPTH), do_mixer=True, do_moe=True):
    nc = bass.Bass('TRN2', target_bir_lowering=False)
    xin = nc.dram_tensor("x", [L, D], F32, kind="ExternalInput").ap()
    P = {n: nc.dram_tensor(n, list(PARAM_SHAPES[n]), F32, kind="ExternalInput").ap() for n in PARAM_NAMES}
    onehot = nc.dram_tensor("t5_onehot", [32, 512], F32, kind="ExternalInput").ap()
    X = nc.dram_tensor("y", [L, D], F32, kind="ExternalOutput").ap()
    PT = nc.dram_tensor("PT", [2048, L], F32).ap()
    PV = nc.dram_tensor("PV", [L, 2048], BF16).ap()
    QK = nc.dram_tensor("QK", [16, 128, L], BF16).ap()
    QK5 = QK.rearrange("(h r w) p t -> h r w p t", h=4, r=2)
    OA = nc.dram_tensor("OA", [L, 512], F32).ap()
    YT = nc.dram_tensor("YT", [512, L], F32).ap()
    MT = nc.dram_tensor("MT", [1024, L], BF16).ap()
    HB = nc.dram_tensor("HB", [L, D], BF16).ap()
    GQ = nc.dram_tensor("GQ", [12, 128, L], BF16).ap()
    GR = nc.dram_tensor("GR", [2, 3, 4, L], F32).ap()
    OD2 = nc.dram_tensor("OD2", [L, 512], F32).ap()
    FV = nc.dram_tensor("FV", [4, 512], F32).ap()
    c = Ctx(nc); g = G()
    setup_consts(c, g)
    bX = Buf('X'); bPT = Buf(); bPV = Buf(); bQK = Buf(); bOAs = [Buf() for _ in range(4)]; bYT = Buf(); bMT = Buf()
    bHB = Buf(); bGQ = Buf(); bGR = Buf(); bFV = Buf(); bOD2s = [Buf() for _ in range(4)]
    for r in range(0, L, 512):
        c.dma('sp', lambda e: e.dma_start(out=X[r:r + 512, :], in_=xin[r:r + 512, :]), pwrites=[bX])
    bX.seal()
    for layer in layers:
        j = layer // 2
        if do_mixer:
            if layer % 2 == 0:
                spec = [(0, 512, 'F', 0), (512, 512, 'F', 512), (1024, 512, 'F', 1024), (2560, 512, 'F', 1536), (1536, 512, 'T', 0), (2048, 512, 'T', 512)]
                proj_phase(c, g, X, bX, P['mix_norm'][layer], P['ev_w_in'][j], 3072, spec, PT, bPT, PV, bPV)
                hgrn2_phase(c, g, PT, bPT, PV, bPV, P['a_lb_logits'], j, P['a_out_norm'][j], QK5, bQK, OA, bOAs, OD2, bOD2s, MT, bMT)
                s5_phase(c, g, PT, bPT, P['s5_lambda_re'][j], P['s5_lambda_im'][j], P['s5_log_step'][j], P['s5_b_re'][j], P['s5_b_im'][j],
                         P['s5_c_re'][j], P['s5_c_im'][j], P['s5_d'][j], P['s5_glu_w'][j], P['s5_glu_b'][j], YT, bYT, MT, bMT)
                bMT.seal()
                wo = P['ev_w_out'][j]
            else:
                spec = [(0, 512, 'T', 0), (512, 512, 'T', 512), (1024, 512, 'T', 1024), (1536, 1536, 'F', 0), (3072, 16, 'F', 1536), (3088, 512, 'T', 1536)]
                proj_phase(c, g, X, bX, P['mix_norm'][layer], P['od_w_in'][j], 3600, spec, PT, bPT, PV, bPV)
                attn_phase(c, g, PV, bPV, P['c_q_norm'][j], P['c_k_norm'][j], P['c_lambda'][j], P['c_out_norm'][j], P['rel_bias'], onehot, layer,
                           QK, bQK, FV, bFV, MT, bMT)
                gdn_phase(c, g, PT, bPT, PV, bPV, P['d_conv_w'][j], P['d_a_log'][j], P['d_dt_bias'][j], P['d_out_norm'][j], GQ, bGQ, GR, bGR, OA, bOAs, OD2, bOD2s, MT, bMT)
                bMT.seal()
                wo = P['od_w_out'][j]
            bX = outproj_moe(c, g, MT, bMT, wo, X, bX, HB, bHB, P['ffn_norm'][layer], P['moe_router'][layer], P['moe_w_gate'][layer], P['moe_w_up'][layer], P['moe_w_down'][layer])
    barrier(c)
    c.finish([bX])
    return nc, c


def kernel(**inputs):
    x = np.ascontiguousarray(np.asarray(inputs['x'], dtype=np.float32))
    B = x.shape[0]
    assert B == N_ACTIVE and x.shape[1] == L and x.shape[2] == D
    nc, c = build_program()
    params = {n: np.ascontiguousarray(np.asarray(inputs[n], dtype=np.float32)) for n in PARAM_NAMES}
    oh = t5_onehot()
    in_maps = []
    for b in range(N_ACTIVE):
        m = {"x": x[b], "t5_onehot": oh}
        m.update(params)
        in_maps.append(m)
    res = run_bass_kernel_spmd(nc, in_maps, core_ids=list(range(N_ACTIVE)))
    out = np.stack([np.asarray(res.results[b]["y"], dtype=np.float32) for b in range(N_ACTIVE)], axis=0)
    return out
```

```python
import math
from contextlib import ExitStack

import numpy as np
import concourse.bass as bass
import concourse.mybir as mybir
from concourse.bass_utils import run_bass_kernel_spmd

F32 = mybir.dt.float32
BF16 = mybir.dt.bfloat16
U32 = mybir.dt.uint32
I32 = mybir.dt.int32
AF = mybir.ActivationFunctionType
ALU = mybir.AluOpType
AX = mybir.AxisListType


class Buf:
    __slots__ = ("name", "w", "r", "pw", "psum")

    def __init__(self, name="", psum=False):
        self.name = name
        self.psum = psum
        self.w = {}
        self.r = {}
        self.pw = {}

    def seal(self):
        for k, v in self.pw.items():
            if self.w.get(k, 0) < v:
                self.w[k] = v
        self.pw = {}


class Ctx:
    NDMA = 8

    def __init__(self, nc, same_engine_sync=True):
        self.nc = nc
        self.E = dict(pe=nc.tensor, dve=nc.vector, act=nc.scalar, pool=nc.gpsimd, sp=nc.sync)
        self.sem = {}
        self.cnt = {}
        for k in self.E:
            self.sem[k] = nc.alloc_semaphore("c_" + k)
            self.cnt[k] = 0
        self.dslot = {}
        for q in ("sp", "act", "pool"):
            for i in range(self.NDMA):
                key = "d_%s%d" % (q, i)
                self.sem[key] = nc.alloc_semaphore(key)
                self.cnt[key] = 0
            self.dslot[q] = 0
        self.seen = {k: {} for k in self.E}
        self.same = same_engine_sync
        self.ninst = 0

    def _wait(self, eng, tok):
        if tok is None:
            return
        key, val = tok
        if key == eng and (eng == "pe" or not self.same):
            return
        if self.seen[eng].get(key, 0) >= val:
            return
        self.E[eng].wait_ge(self.sem[key], val)
        self.seen[eng][key] = val

    def _deps(self, eng, reads, writes, pwrites=()):
        for b in reads:
            for k, v in b.w.items():
                self._wait(eng, (k, v))
            for k, v in b.pw.items():
                self._wait(eng, (k, v))
            if b.psum:
                for k, v in b.r.items():
                    if k != eng:
                        self._wait(eng, (k, v))
        for b in writes:
            for d in (b.w, b.pw, b.r):
                for k, v in d.items():
                    self._wait(eng, (k, v))
        for b in pwrites:
            for d in (b.w, b.r):
                for k, v in d.items():
                    self._wait(eng, (k, v))

    def _commit(self, tok, reads, writes, pwrites=()):
        k, v = tok
        for b in writes:
            b.w = {k: v}
            b.pw = {}
            b.r = {}
        for b in pwrites:
            if b.pw.get(k, 0) < v:
                b.pw[k] = v
        for b in reads:
            if b.r.get(k, 0) < v:
                b.r[k] = v

    def op(self, eng, fn, reads=(), writes=(), pwrites=()):
        self._deps(eng, reads, writes, pwrites)
        inst = fn(self.E[eng])
        self.cnt[eng] += 1
        tok = (eng, self.cnt[eng])
        inst.then_inc(self.sem[eng], 1)
        self._commit(tok, reads, writes, pwrites)
        self.ninst += 1
        return tok

    def dma(self, q, fn, reads=(), writes=(), pwrites=()):
        self._deps(q, reads, writes, pwrites)
        i = self.dslot[q]
        self.dslot[q] = (i + 1) % self.NDMA
        key = "d_%s%d" % (q, i)
        if self.cnt[key] > 0:
            self._wait(q, (key, self.cnt[key]))
        inst = fn(self.E[q])
        self.cnt[key] += 16
        tok = (key, self.cnt[key])
        inst.then_inc(self.sem[key], 16)
        self._commit(tok, reads, writes, pwrites)
        self.ninst += 1
        return tok

    def finish(self, bufs):
        for b in bufs:
            for d in (b.w, b.pw):
                for k, v in d.items():
                    self._wait("sp", (k, v))


def barrier(c):
    toks = [(k, v) for k, v in c.cnt.items() if v > 0]
    for eng in c.E:
        for tok in toks:
            c._wait(eng, tok)


_UNIQ = [0]


def uniq(name):
    _UNIQ[0] += 1
    return "%s_u%d" % (name, _UNIQ[0])


L = 8192
D = 1024
NE = 16
FF = 2048
CAP = 1024
NT = L // 128


class G:
    pass


def alloc_T(nc, name, shape, dtype, n=1, es=None):
    if es is None:
        return [(nc.alloc_sbuf_tensor("%s_%d" % (name, i), shape, dtype), Buf(name)) for i in range(n)]
    return [(es.enter_context(nc.sbuf_tensor(uniq("%s_%d" % (name, i)), shape, dtype)), Buf(name)) for i in range(n)]


def setup_consts(c, g):
    nc = c.nc
    g.ident32 = nc.alloc_sbuf_tensor("ident32", [128, 128], F32); g.b_ident32 = Buf()
    g.identb = nc.alloc_sbuf_tensor("identb", [128, 128], BF16); g.b_identb = Buf()
    g.ones32 = nc.alloc_sbuf_tensor("ones32", [1, 128], F32); g.b_ones32 = Buf()
    g.neghalf = nc.alloc_sbuf_tensor("neghalf", [128, 1], F32); g.b_neghalf = Buf()
    for t, b in ((g.ident32, g.b_ident32), (g.identb, g.b_identb)):
        c.op('pool', lambda e: e.memset(t[:], 0.0), writes=[b])
        c.op('pool', lambda e: e.affine_select(out=t[:], in_=t[:], pattern=[[-1, 128]], compare_op=ALU.not_equal,
                                               fill=1.0, base=0, channel_multiplier=1), reads=[b], writes=[b])
    g.J32 = nc.alloc_sbuf_tensor("J32", [128, 128], F32); g.b_J32 = Buf()
    g.Jb = nc.alloc_sbuf_tensor("Jb", [128, 128], BF16); g.b_Jb = Buf()
    for t, b in ((g.J32, g.b_J32), (g.Jb, g.b_Jb)):
        c.op('pool', lambda e: e.memset(t[:], 0.0), writes=[b])
        c.op('pool', lambda e: e.affine_select(out=t[:], in_=t[:], pattern=[[1, 128]], compare_op=ALU.not_equal,
                                               fill=1.0, base=-127, channel_multiplier=1), reads=[b], writes=[b])
    c.op('pool', lambda e: e.memset(g.ones32[:], 1.0), writes=[g.b_ones32])
    c.op('pool', lambda e: e.memset(g.neghalf[:], -0.5), writes=[g.b_neghalf])
    g.ps = []
    for i in range(7):
        g.ps.append((nc.alloc_psum_tensor("ps%d" % i, [128, 512], F32), Buf("ps%d" % i, psum=True)))
    g.psb = (nc.alloc_psum_tensor("psb", [128, 1024], BF16), Buf("psb", psum=True))


def bcast_row(c, g, dst, bdst, src_ap, n, tmp, btmp, psi=6):
    c.dma('sp', lambda e: e.dma_start(out=tmp[0:1, 0:n], in_=src_ap.rearrange("(o n) -> o n", o=1)), writes=[btmp])
    ps, bps = g.ps[psi]
    for h in range(0, n, 512):
        w = min(512, n - h)
        c.op('pe', lambda e: e.matmul(ps[:, 0:w], lhsT=g.ones32[0:1, :], rhs=tmp[0:1, h:h + w], start=True, stop=True),
             reads=[g.b_ones32, btmp], writes=[bps])
        c.op('dve', lambda e: e.tensor_copy(out=dst[:, h:h + w], in_=ps[:, 0:w]), reads=[bps], writes=[bdst])


def rmsnorm_tile(c, g, xt, bxt, gB, bgB, h32, bh32, junk, bjunk, ss, bss):
    c.op('dve', lambda e: e.scalar_tensor_tensor(out=junk[:], in0=xt, scalar=1.0, in1=xt, op0=ALU.mult, op1=ALU.mult,
                                                 accum_out=ss[:, 0:1]), reads=[bxt], writes=[bjunk, bss])
    c.op('dve', lambda e: e.tensor_scalar(out=ss[:, 0:1], in0=ss[:, 0:1], scalar1=1.0 / D, scalar2=1e-6, op0=ALU.mult, op1=ALU.add),
         reads=[bss], writes=[bss])
    c.op('act', lambda e: e.activation(out=ss[:, 0:1], in_=ss[:, 0:1], func=AF.Ln), reads=[bss], writes=[bss])
    c.op('act', lambda e: e.activation(out=ss[:, 0:1], in_=ss[:, 0:1], func=AF.Exp, scale=-0.5), reads=[bss], writes=[bss])
    c.op('dve', lambda e: e.scalar_tensor_tensor(out=h32[:], in0=xt, scalar=ss[:, 0:1], in1=gB[:], op0=ALU.mult, op1=ALU.mult),
         reads=[bxt, bss, bgB], writes=[bh32])


def moe_prep(c, g, sb, ffn_g, w_router):
    nc = c.nc
    gB, bgB = sb['gB']
    tmpr, btmpr = sb['tmprow']
    bcast_row(c, g, gB, bgB, ffn_g, D, tmpr, btmpr)
    wr, bwr = sb['wr']
    c.dma('sp', lambda e: e.dma_start(out=wr[:], in_=w_router.rearrange("(k p) e -> p k e", p=128)), writes=[bwr])


def moe_step1_tile(c, g, sb, i, xt, bxt, HB, bHB):
    affT, baffT = sb['affT']
    gB, bgB = sb['gB']
    wr, bwr = sb['wr']
    h32, bh32 = sb['h32'][i % 2]
    hb, bhb = sb['hb'][i % 2]
    junk, bjunk = sb['junk']
    ss, bss = sb['ss'][i % 2]
    rmsnorm_tile(c, g, xt, bxt, gB, bgB, h32, bh32, junk, bjunk, ss, bss)
    c.op('act', lambda e: e.activation(out=hb[:], in_=h32[:], func=AF.Copy), reads=[bh32], writes=[bhb])
    c.dma('act', lambda e: e.dma_start(out=HB[i * 128:(i + 1) * 128, :], in_=hb[:]), reads=[bhb], pwrites=[bHB])
    hT, bhT = sb['hT32'][i % 2]
    for hh in range(2):
        ps, bps = g.ps[4 + hh]
        for k in range(4):
            kk = hh * 4 + k
            c.op('pe', lambda e: e.transpose(out=ps[:, k * 128:(k + 1) * 128], in_=h32[:, kk * 128:(kk + 1) * 128],
                                             identity=g.ident32[:]), reads=[bh32, g.b_ident32], writes=[bps])
        c.op('act', lambda e: e.activation(out=hT[:, hh * 512:(hh + 1) * 512], in_=ps[:, :], func=AF.Copy),
             reads=[bps], writes=[bhT])
    pl, bpl = g.ps[6]
    for k in range(8):
        c.op('pe', lambda e: e.matmul(pl[:, 0:NE], lhsT=hT[:, k * 128:(k + 1) * 128], rhs=wr[:, k, :], start=(k == 0), stop=(k == 7)),
             reads=[bhT, bwr], writes=[bpl])
    sm, bsm = sb['sm'][i % 2]
    ex, bex = sb['ex'][i % 2]
    c.op('dve', lambda e: e.tensor_reduce(out=sm[:, 0:1], in_=pl[:, 0:NE], axis=AX.X, op=ALU.max), reads=[bpl], writes=[bsm])
    c.op('dve', lambda e: e.tensor_scalar(out=sm[:, 0:1], in0=sm[:, 0:1], scalar1=-1.0, scalar2=None, op0=ALU.mult), reads=[bsm], writes=[bsm])
    c.op('act', lambda e: e.activation(out=ex[:], in_=pl[:, 0:NE], func=AF.Exp, bias=sm[:, 0:1], scale=1.0, accum_out=sm[:, 1:2]),
         reads=[bpl, bsm], writes=[bex, bsm])
    c.op('dve', lambda e: e.reciprocal(out=sm[:, 2:3], in_=sm[:, 1:2]), reads=[bsm], writes=[bsm])
    c.op('dve', lambda e: e.tensor_scalar(out=ex[:], in0=ex[:], scalar1=sm[:, 2:3], scalar2=None, op0=ALU.mult), reads=[bex, bsm], writes=[bex])
    pt, bpt = g.ps[6]
    c.op('pe', lambda e: e.transpose(out=pt[0:NE, 0:128], in_=ex[:, 0:NE], identity=g.ident32[:]), reads=[bex, g.b_ident32], writes=[bpt])
    c.op('act', lambda e: e.activation(out=affT[0:NE, i * 128:(i + 1) * 128], in_=pt[0:NE, 0:128], func=AF.Copy), reads=[bpt], writes=[baffT])


def moe_rest(c, g, sb, X, bX, HB, bHB, w_gate, w_up, w_down, stage=3):
    nc = c.nc
    affT, baffT = sb['affT']
    bHB.seal()
    if stage < 2:
        return
    vals, bvals = sb['vals']
    idxu, bidxu = sb['idxu']
    for it in range(CAP // 8):
        sl = slice(it * 8, it * 8 + 8)
        c.op('dve', lambda e: e.max(out=vals[:, sl], in_=affT[:, :]), reads=[baffT], writes=[bvals])
        c.op('dve', lambda e: e.max_index(out=idxu[:, sl], in_max=vals[:, sl], in_values=affT[:, :]), reads=[baffT, bvals], writes=[bidxu])
        c.op('dve', lambda e: e.match_replace(out=affT[:, :], in_to_replace=vals[:, sl], in_values=affT[:, :], imm_value=-1.0),
             reads=[bvals, baffT], writes=[baffT])
    idxf, bidxf = sb['idxf']
    c.op('dve', lambda e: e.tensor_copy(out=idxf[:], in_=idxu[:]), reads=[bidxu], writes=[bidxf])
    idxT, bidxT = sb['idxT']
    gateT, bgateT = sb['gateT']
    for j in range(8):
        pt, bpt = g.ps[4 + (j % 2)]
        c.op('pe', lambda e: e.transpose(out=pt[:, 0:NE], in_=idxf[0:NE, j * 128:(j + 1) * 128], identity=g.ident32[0:NE, 0:NE]),
             reads=[bidxf, g.b_ident32], writes=[bpt])
        c.op('dve', lambda e: e.tensor_copy(out=idxT[:, j, :], in_=pt[:, 0:NE]), reads=[bpt], writes=[bidxT])
        pt2, bpt2 = g.ps[2 + (j % 2)]
        c.op('pe', lambda e: e.transpose(out=pt2[:, 0:NE], in_=vals[0:NE, j * 128:(j + 1) * 128], identity=g.ident32[0:NE, 0:NE]),
             reads=[bvals, g.b_ident32], writes=[bpt2])
        c.op('act', lambda e: e.activation(out=gateT[:, j, :], in_=pt2[:, 0:NE], func=AF.Copy), reads=[bpt2], writes=[bgateT])
    if stage < 3:
        return
    xsT, bxsT = sb['xsT']
    hidT, bhidT = sb['hidT']
    yacc, byacc = sb['yacc']
    ptb, bptb = g.psb
    qi = 0
    for ex_i in range(NE):
        for j in range(8):
            xs, bxs = sb['xs'][j % 2]
            c.dma('pool', lambda e: e.indirect_dma_start(out=xs[:, :], out_offset=None, in_=HB[:, :],
                                                        in_offset=bass.IndirectOffsetOnAxis(ap=idxT[:, j, ex_i:ex_i + 1], axis=0)),
                  reads=[bHB, bidxT], writes=[bxs])
            for k in range(8):
                c.op('pe', lambda e: e.transpose(out=ptb[:, k * 128:(k + 1) * 128], in_=xs[:, k * 128:(k + 1) * 128], identity=g.identb[:]),
                     reads=[bxs, g.b_identb], writes=[bptb])
            c.op('dve', lambda e: e.tensor_copy(out=xsT[:, :, j * 128:(j + 1) * 128], in_=ptb[:, :].rearrange("p (k s) -> p k s", k=8)),
                 reads=[bptb], writes=[bxsT])
        for q in range(4):
            wg, bwg = sb['wg'][qi % 2]
            wu, bwu = sb['wu'][qi % 2]
            wd, bwd = sb['wd'][qi % 2]
            qi += 1
            f0 = q * 512
            c.dma('pool', lambda e: e.dma_start(out=wg[:], in_=w_gate[ex_i, :, f0:f0 + 512].rearrange("(k p) f -> p k f", p=128)), writes=[bwg])
            c.dma('pool', lambda e: e.dma_start(out=wu[:], in_=w_up[ex_i, :, f0:f0 + 512].rearrange("(k p) f -> p k f", p=128)), writes=[bwu])
            c.dma('pool', lambda e: e.dma_start(out=wd[:], in_=w_down[ex_i, f0:f0 + 512, :].rearrange("(k p) d -> p k d", p=128)), writes=[bwd])
            n = 0
            for fc in range(4):
                for sh in range(2):
                    pg, bpg = g.ps[0 + (n % 2)]
                    pu, bpu = g.ps[2 + (n % 2)]
                    sg, bsg = sb['sg'][n % 2]
                    n += 1
                    for k in range(8):
                        c.op('pe', lambda e: e.matmul(pg[:, :], lhsT=wg[:, k, fc * 128:(fc + 1) * 128], rhs=xsT[:, k, sh * 512:(sh + 1) * 512],
                                                      start=(k == 0), stop=(k == 7)), reads=[bwg, bxsT], writes=[bpg])
                    for k in range(8):
                        c.op('pe', lambda e: e.matmul(pu[:, :], lhsT=wu[:, k, fc * 128:(fc + 1) * 128], rhs=xsT[:, k, sh * 512:(sh + 1) * 512],
                                                      start=(k == 0), stop=(k == 7)), reads=[bwu, bxsT], writes=[bpu])
                    c.op('act', lambda e: e.activation(out=sg[:], in_=pg[:, :], func=AF.Silu), reads=[bpg], writes=[bsg])
                    c.op('dve', lambda e: e.tensor_tensor(out=hidT[:, fc, sh * 512:(sh + 1) * 512], in0=sg[:], in1=pu[:, :], op=ALU.mult),
                         reads=[bsg, bpu], writes=[bhidT])
            m = 0
            for j in range(8):
                for dh in range(2):
                    py, bpy = g.ps[4 + (m % 2)]
                    m += 1
                    for fc in range(4):
                        c.op('pe', lambda e: e.matmul(py[:, :], lhsT=hidT[:, fc, j * 128:(j + 1) * 128], rhs=wd[:, fc, dh * 512:(dh + 1) * 512],
                                                      start=(fc == 0), stop=(fc == 3)), reads=[bhidT, bwd], writes=[bpy])
                    ysl = yacc[:, j, dh * 512:(dh + 1) * 512]
                    gsc = gateT[:, j, ex_i:ex_i + 1]
                    if q == 0:
                        c.op('dve', lambda e: e.tensor_scalar(out=ysl, in0=py[:, :], scalar1=gsc, scalar2=None, op0=ALU.mult),
                             reads=[bpy, bgateT], writes=[byacc])
                    else:
                        c.op('dve', lambda e: e.scalar_tensor_tensor(out=ysl, in0=py[:, :], scalar=gsc, in1=ysl, op0=ALU.mult, op1=ALU.add),
                             reads=[bpy, bgateT, byacc], writes=[byacc])
        for j in range(8):
            c.dma('pool', lambda e: e.indirect_dma_start(out=X[:, :], out_offset=bass.IndirectOffsetOnAxis(ap=idxT[:, j, ex_i:ex_i + 1], axis=0),
                                                        in_=yacc[:, j, :], in_offset=None, compute_op=ALU.add),
                  reads=[byacc, bidxT], pwrites=[bX])
        bX.seal()


def moe_phase(c, g, X, bX, HB, bHB, ffn_g, w_router, w_gate, w_up, w_down, sb, stage=3):
    moe_prep(c, g, sb, ffn_g, w_router)
    for i in range(NT):
        xt, bxt = sb['xt'][i % 2]
        c.dma('sp', lambda e: e.dma_start(out=xt[:], in_=X[i * 128:(i + 1) * 128, :]), reads=[bX], writes=[bxt])
        moe_step1_tile(c, g, sb, i, xt[:], bxt, HB, bHB)
    moe_rest(c, g, sb, X, bX, HB, bHB, w_gate, w_up, w_down, stage=stage)


def moe_alloc(nc, es=None, part=0, sb=None):
    sb = {} if sb is None else sb
    if part in (0, 1):
        sb['gB'] = alloc_T(nc, 'gB', [128, D], F32, 1, es=es)[0]
        sb['tmprow'] = alloc_T(nc, 'tmprow', [1, 1024], F32, 1, es=es)[0]
        sb['wr'] = alloc_T(nc, 'wr', [128, 8, NE], F32, 1, es=es)[0]
        big = es.enter_context(nc.sbuf_tensor(uniq('big'), [128, L], F32)) if es is not None else nc.alloc_sbuf_tensor('big', [128, L], F32); bbig = Buf('big')
        sb['affT'] = (big[0:NE, :], bbig)
        sb['yacc'] = (big[:, :].rearrange('p (j d) -> p j d', j=8), bbig)
        if part == 0:
            sb['xt'] = alloc_T(nc, 'xt', [128, D], F32, 2, es=es)
        sb['h32'] = alloc_T(nc, 'h32', [128, D], F32, 2, es=es)
        sb['hb'] = alloc_T(nc, 'hb', [128, D], BF16, 2, es=es)
        sb['junk'] = alloc_T(nc, 'junk', [128, D], F32, 1, es=es)[0]
        sb['ss'] = alloc_T(nc, 'ss', [128, 4], F32, 2, es=es)
        sb['hT32'] = alloc_T(nc, 'hT32', [128, D], F32, 2, es=es)
        sb['sm'] = alloc_T(nc, 'sm', [128, 4], F32, 2, es=es)
        sb['ex'] = alloc_T(nc, 'ex', [128, NE], F32, 2, es=es)
    if part in (0, 3):
        sb['vals'] = alloc_T(nc, 'vals', [NE, CAP], F32, 1, es=es)[0]
        sb['idxu'] = alloc_T(nc, 'idxu', [NE, CAP], U32, 1, es=es)[0]
        sb['idxf'] = alloc_T(nc, 'idxf', [NE, CAP], F32, 1, es=es)[0]
        sb['idxT'] = alloc_T(nc, 'idxT', [128, 8, NE], U32, 1, es=es)[0]
        sb['gateT'] = alloc_T(nc, 'gateT', [128, 8, NE], F32, 1, es=es)[0]
        sb['xsT'] = alloc_T(nc, 'xsT', [128, 8, CAP], BF16, 1, es=es)[0]
        sb['hidT'] = alloc_T(nc, 'hidT', [128, 4, CAP], BF16, 1, es=es)[0]
        sb['xs'] = alloc_T(nc, 'xs', [128, D], BF16, 2, es=es)
        sb['wg'] = alloc_T(nc, 'wg', [128, 8, 512], BF16, 2, es=es)
        sb['wu'] = alloc_T(nc, 'wu', [128, 8, 512], BF16, 2, es=es)
        sb['wd'] = alloc_T(nc, 'wd', [128, 4, D], BF16, 2, es=es)
        sb['sg'] = alloc_T(nc, 'sg', [128, 512], BF16, 2, es=es)
    return sb


def moe_layer(c, g, X, bX, HB, bHB, ffn_g, w_router, w_gate, w_up, w_down):
    with ExitStack() as es:
        sb = moe_alloc(c.nc, es)
        moe_phase(c, g, X, bX, HB, bHB, ffn_g, w_router, w_gate, w_up, w_down, sb)
        barrier(c)


def proj_phase(c, g, X, bX, gain_ap, w_ap, nout, spec, PT, bPT, PV, bPV):
    nc = c.nc
    with ExitStack() as es:
        def T(name, shape, dt):
            return es.enter_context(nc.sbuf_tensor(uniq(name), shape, dt))
        wsb = T("pj_w", [128, 8, nout], BF16); bw = Buf()
        gB = T("pj_gB", [128, D], F32); bgB = Buf()
        tmpr = T("pj_tmpr", [1, D], F32); btmpr = Buf()
        xt = [T("pj_xt%d" % i, [128, 4, D], F32) for i in range(2)]; bxt = [Buf(), Buf()]
        junk = T("pj_junk", [128, D], F32); bjunk = Buf()
        ss = [T("pj_ss%d" % i, [128, 4], F32) for i in range(2)]; bss = [Buf(), Buf()]
        hb = [T("pj_hb%d" % i, [128, D], BF16) for i in range(2)]; bhb = [Buf(), Buf()]
        hT = [T("pj_hT%d" % i, [128, 8, 512], BF16) for i in range(2)]; bhT = [Buf(), Buf()]
        stF = [T("pj_stF%d" % i, [128, 512], F32) for i in range(3)]; bstF = [Buf() for _ in range(3)]
        stT = [T("pj_stT%d" % i, [128, 512], BF16) for i in range(3)]; bstT = [Buf() for _ in range(3)]
        bcast_row(c, g, gB, bgB, gain_ap, D, tmpr, btmpr)
        for c0 in range(0, nout, 512):
            wd = min(512, nout - c0)
            c.dma('pool', lambda e: e.dma_start(out=wsb[:, :, c0:c0 + wd], in_=w_ap[:, c0:c0 + wd].rearrange("(k p) f -> p k f", p=128)), writes=[bw])
        ptb, bptb = g.psb
        nF = 0; nT = 0; npz = 0
        for it in range(L // 512):
            t0 = it * 512
            x_, bx_ = xt[it % 2], bxt[it % 2]
            c.dma('sp', lambda e: e.dma_start(out=x_[:], in_=X[t0:t0 + 512, :].rearrange("(j p) d -> p j d", p=128)), reads=[bX], writes=[bx_])
            hT_, bhT_ = hT[it % 2], bhT[it % 2]
            for j in range(4):
                s_, bs_ = ss[j % 2], bss[j % 2]
                h_, bh_ = hb[j % 2], bhb[j % 2]
                c.op('dve', lambda e: e.scalar_tensor_tensor(out=junk[:], in0=x_[:, j, :], scalar=1.0, in1=x_[:, j, :], op0=ALU.mult, op1=ALU.mult,
                                                             accum_out=s_[:, 0:1]), reads=[bx_], writes=[bjunk, bs_])
                c.op('dve', lambda e: e.tensor_scalar(out=s_[:, 0:1], in0=s_[:, 0:1], scalar1=1.0 / D, scalar2=1e-6, op0=ALU.mult, op1=ALU.add),
                     reads=[bs_], writes=[bs_])
                c.op('act', lambda e: e.activation(out=s_[:, 0:1], in_=s_[:, 0:1], func=AF.Ln), reads=[bs_], writes=[bs_])
                c.op('act', lambda e: e.activation(out=s_[:, 0:1], in_=s_[:, 0:1], func=AF.Exp, scale=-0.5), reads=[bs_], writes=[bs_])
                c.op('dve', lambda e: e.scalar_tensor_tensor(out=h_[:], in0=x_[:, j, :], scalar=s_[:, 0:1], in1=gB[:], op0=ALU.mult, op1=ALU.mult),
                     reads=[bx_, bs_, bgB], writes=[bh_])
                for k in range(8):
                    c.op('pe', lambda e: e.transpose(out=ptb[:, k * 128:(k + 1) * 128], in_=h_[:, k * 128:(k + 1) * 128], identity=g.identb[:]),
                         reads=[bh_, g.b_identb], writes=[bptb])
                c.op('act', lambda e: e.activation(out=hT_[:, :, j * 128:(j + 1) * 128], in_=ptb[:, :].rearrange("p (k s) -> p k s", k=8), func=AF.Copy),
                     reads=[bptb], writes=[bhT_])
            for (col0, ncols, mode, dst0) in spec:
                if mode == 'F':
                    for f0 in range(0, ncols, 128):
                        fw = min(128, ncols - f0)
                        ps, bps = g.ps[npz % 4]; npz += 1
                        for k in range(8):
                            c.op('pe', lambda e: e.matmul(ps[0:fw, :], lhsT=wsb[:, k, col0 + f0:col0 + f0 + fw], rhs=hT_[:, k, :], start=(k == 0), stop=(k == 7)),
                                 reads=[bw, bhT_], writes=[bps])
                        st, bst = stF[nF % 3], bstF[nF % 3]; nF += 1
                        eng = 'act' if nF % 2 else 'dve'
                        if eng == 'act':
                            c.op('act', lambda e: e.activation(out=st[0:fw, :], in_=ps[0:fw, :], func=AF.Copy), reads=[bps], writes=[bst])
                        else:
                            c.op('dve', lambda e: e.tensor_copy(out=st[0:fw, :], in_=ps[0:fw, :]), reads=[bps], writes=[bst])
                        c.dma('sp' if nF % 2 else 'act', lambda e: e.dma_start(out=PT[dst0 + f0:dst0 + f0 + fw, t0:t0 + 512], in_=st[0:fw, :]), reads=[bst], pwrites=[bPT])
                else:
                    for j in range(4):
                        for c0 in range(0, ncols, 512):
                            cw = min(512, ncols - c0)
                            ps, bps = g.ps[npz % 4]; npz += 1
                            for k in range(8):
                                c.op('pe', lambda e: e.matmul(ps[:, 0:cw], lhsT=hT_[:, k, j * 128:(j + 1) * 128], rhs=wsb[:, k, col0 + c0:col0 + c0 + cw], start=(k == 0), stop=(k == 7)),
                                     reads=[bw, bhT_], writes=[bps])
                            st, bst = stT[nT % 3], bstT[nT % 3]; nT += 1
                            eng = 'act' if nT % 2 else 'dve'
                            if eng == 'act':
                                c.op('act', lambda e: e.activation(out=st[:, 0:cw], in_=ps[:, 0:cw], func=AF.Copy), reads=[bps], writes=[bst])
                            else:
                                c.op('dve', lambda e: e.tensor_copy(out=st[:, 0:cw], in_=ps[:, 0:cw]), reads=[bps], writes=[bst])
                            c.dma('sp' if nT % 2 else 'act', lambda e: e.dma_start(out=PV[t0 + j * 128:t0 + (j + 1) * 128, dst0 + c0:dst0 + c0 + cw], in_=st[:, 0:cw]),
                                  reads=[bst], pwrites=[bPV])
        bPT.seal(); bPV.seal()
        barrier(c)


def gated_norm_finalize(c, g, OA, bOAs, PV, bPV, gcol0, gain_ap, MT, bMT, row0, pfx, OA2=None, bOA2s=()):
    nc = c.nc
    with ExitStack() as es:
        def T(name, shape, dt):
            return es.enter_context(nc.sbuf_tensor(uniq(pfx + name), shape, dt))
        gA = T("gA", [128, 128], F32); bgA = Buf()
        oa = [T("oa%d" % i, [128, 512], F32) for i in range(2)]; boa = [Buf(), Buf()]
        oa2 = [T("oa2%d" % i, [128, 512], F32) for i in range(2)]; boa2 = [Buf(), Buf()]
        ga = [T("ga%d" % i, [128, 512], BF16) for i in range(2)]; bga = [Buf(), Buf()]
        sq = T("sq", [128, 512], F32); bsq = Buf()
        ssq = [T("ssq%d" % i, [128, 4], F32) for i in range(2)]; bssq = [Buf(), Buf()]
        sg = T("sg", [128, 512], F32); bsg = Buf()
        t1 = T("t1", [128, 512], F32); bt1 = Buf()
        ob = [T("ob%d" % i, [128, 512], BF16) for i in range(2)]; bob = [Buf(), Buf()]
        mt = [T("mt%d" % i, [128, 4, 512], BF16) for i in range(2)]; bmt = [Buf(), Buf()]
        c.dma('sp', lambda e: e.dma_start(out=gA[:], in_=gain_ap.partition_broadcast(128)), writes=[bgA])
        ptb, bptb = g.psb
        for i in range(NT):
            oa_, boa_ = oa[i % 2], boa[i % 2]
            ga_, bga_ = ga[i % 2], bga[i % 2]
            ss_, bss_ = ssq[i % 2], bssq[i % 2]
            ob_, bob_ = ob[i % 2], bob[i % 2]
            mt_, bmt_ = mt[(i // 4) % 2], bmt[(i // 4) % 2]
            c.dma('sp', lambda e: e.dma_start(out=oa_[:], in_=OA[i * 128:(i + 1) * 128, :]), reads=bOAs, writes=[boa_])
            c.dma('act', lambda e: e.dma_start(out=ga_[:], in_=PV[i * 128:(i + 1) * 128, gcol0:gcol0 + 512]), reads=[bPV], writes=[bga_])
            if OA2 is not None:
                o2_, bo2_ = oa2[i % 2], boa2[i % 2]
                c.dma('act', lambda e: e.dma_start(out=o2_[:], in_=OA2[i * 128:(i + 1) * 128, :]), reads=list(bOA2s), writes=[bo2_])
                c.op('dve', lambda e: e.tensor_tensor(out=oa_[:], in0=oa_[:], in1=o2_[:], op=ALU.add), reads=[boa_, bo2_], writes=[boa_])
            c.op('act', lambda e: e.activation(out=sq[:], in_=oa_[:], func=AF.Square), reads=[boa_], writes=[bsq])
            c.op('dve', lambda e: e.tensor_reduce(out=ss_[:], in_=sq[:].rearrange("p (h d) -> p h d", h=4), axis=AX.X, op=ALU.add), reads=[bsq], writes=[bss_])
            c.op('dve', lambda e: e.tensor_scalar(out=ss_[:], in0=ss_[:], scalar1=1.0 / 128, scalar2=1e-6, op0=ALU.mult, op1=ALU.add), reads=[bss_], writes=[bss_])
            c.op('pool', lambda e: e.tensor_tensor(out=ss_[:], in0=ss_[:], in1=g.neghalf[:, 0:1].broadcast_to([128, 4]), op=ALU.pow), reads=[bss_, g.b_neghalf], writes=[bss_])
            c.op('act', lambda e: e.activation(out=sg[:], in_=ga_[:], func=AF.Silu), reads=[bga_], writes=[bsg])
            c.op('dve', lambda e: e.tensor_tensor(out=t1[:].rearrange("p (h d) -> p h d", h=4), in0=oa_[:].rearrange("p (h d) -> p h d", h=4),
                                                  in1=ss_[:].unsqueeze(2).broadcast_to([128, 4, 128]), op=ALU.mult), reads=[boa_, bss_], writes=[bt1])
            c.op('dve', lambda e: e.tensor_tensor(out=t1[:].rearrange("p (h d) -> p h d", h=4), in0=t1[:].rearrange("p (h d) -> p h d", h=4),
                                                   in1=gA[:].unsqueeze(1).broadcast_to([128, 4, 128]), op=ALU.mult), reads=[bt1, bgA], writes=[bt1])
            c.op('dve', lambda e: e.tensor_tensor(out=ob_[:], in0=t1[:], in1=sg[:], op=ALU.mult), reads=[bt1, bsg], writes=[bob_])
            for k in range(4):
                c.op('pe', lambda e: e.transpose(out=ptb[:, k * 128:(k + 1) * 128], in_=ob_[:, k * 128:(k + 1) * 128], identity=g.identb[:]),
                     reads=[bob_, g.b_identb], writes=[bptb])
            c.op('act', lambda e: e.activation(out=mt_[:, :, (i % 4) * 128:(i % 4 + 1) * 128], in_=ptb[:, 0:512].rearrange("p (k s) -> p k s", k=4), func=AF.Copy),
                 reads=[bptb], writes=[bmt_])
            if i % 4 == 3:
                t0 = (i // 4) * 512
                c.dma('sp', lambda e: e.dma_start(out=MT[row0:row0 + 512, t0:t0 + 512].rearrange("(k p) t -> p k t", p=128), in_=mt_[:]), reads=[bmt_], pwrites=[bMT])
        barrier(c)


TBK = 2048
NTB = L // TBK


def hgrn2_phase(c, g, PT, bPT, PV, bPV, lb_logits, jl, a_out_norm, QK, bQK, OA, bOAs, OA2, bOA2s, MT, bMT, stage=3):
    nc = c.nc
    with ExitStack() as es0:
        def T0(name, shape, dt):
            return es0.enter_context(nc.sbuf_tensor(uniq(name), shape, dt))
        mcols = T0("hg_mcols", [128, 8, 128], F32); bmcols = Buf()
        with ExitStack() as es:
            def T(name, shape, dt):
                return es.enter_context(nc.sbuf_tensor(uniq(name), shape, dt))
            lbt = T("hg_lbt", [128, 2, 2, 4], F32); blbt = Buf()
            lbc = T("hg_lbc", [128, 8], F32); blbc = Buf()
            oml = T("hg_oml", [128, 8], F32); boml = Buf()
            noml = T("hg_noml", [128, 8], F32); bnoml = Buf()
            msk = T("hg_msk", [128, TBK], F32); bmsk = Buf()
            bmid = T("hg_bmid", [128, 128], F32); bbmid = Buf()
            blast = T("hg_blast", [128, 128], F32); bblast = Buf()
            zq = [T("hg_zq%d" % i, [128, TBK], F32) for i in range(2)]; bzq = [Buf(), Buf()]
            zf = [T("hg_zf%d" % i, [128, TBK], F32) for i in range(2)]; bzf = [Buf(), Buf()]
            q_ = T("hg_q", [128, TBK], F32); bq_ = Buf()
            sig = T("hg_sig", [128, TBK], F32); bsig = Buf()
            f_ = T("hg_f", [128, TBK], F32); bf_ = Buf()
            kk = T("hg_kk", [128, TBK], F32); bkk = Buf()
            b_ = T("hg_b", [128, TBK], F32); bb_ = Buf()
            e1 = T("hg_e1", [128, TBK], F32); be1 = Buf()
            eq = T("hg_eq", [128, TBK], F32); beq = Buf()
            ek = T("hg_ek", [128, TBK], F32); bek = Buf()
            qt = [T("hg_qt%d" % i, [128, TBK], BF16) for i in range(2)]; bqt = [Buf(), Buf()]
            kt = [T("hg_kt%d" % i, [128, TBK], BF16) for i in range(2)]; bkt = [Buf(), Buf()]
            if jl == 0:
                c.op('pool', lambda e: e.memset(lbc[:], 0.0), writes=[blbc])
            else:
                with nc.allow_non_contiguous_dma(reason="tiny"):
                    for j_ in range(2):
                        for r_ in range(2):
                            c.dma('sp', lambda e: e.dma_start(out=lbt[:, j_, r_, :], in_=lb_logits[j_, r_, :].rearrange("(h p) -> p h", p=128)), pwrites=[blbt])
                blbt.seal()
                c.op('dve', lambda e: e.tensor_tensor(out=lbc[:].rearrange("p (r h) -> p r h", r=2), in0=lbt[:, 1, :, :], in1=lbt[:, 0, :, :], op=ALU.subtract),
                     reads=[blbt], writes=[blbc])
                c.op('act', lambda e: e.activation(out=lbc[:], in_=lbc[:], func=AF.Sigmoid), reads=[blbc], writes=[blbc])
            c.op('dve', lambda e: e.tensor_scalar(out=oml[:], in0=lbc[:], scalar1=-1.0, scalar2=1.0, op0=ALU.mult, op1=ALU.add), reads=[blbc], writes=[boml])
            c.op('dve', lambda e: e.tensor_scalar(out=noml[:], in0=oml[:], scalar1=-1.0, scalar2=None, op0=ALU.mult), reads=[boml], writes=[bnoml])
            c.op('pool', lambda e: e.memset(msk[:], 1.0), writes=[bmsk])
            c.op('pool', lambda e: e.memset(msk[:].rearrange("p (c j) -> p c j", j=64)[:, :, 0:1], 0.0), writes=[bmsk])
            n = 0
            for h in range(4):
                for r in range(2):
                    hr = r * 4 + h
                    for tb in range(NTB):
                        nb = tb if r == 0 else NTB - 1 - tb
                        zq_, bzq_ = zq[n % 2], bzq[n % 2]
                        zf_, bzf_ = zf[n % 2], bzf[n % 2]
                        qt_, bqt_ = qt[n % 2], bqt[n % 2]
                        kt_, bkt_ = kt[n % 2], bkt[n % 2]
                        n += 1
                        c.dma('sp', lambda e: e.dma_start(out=zq_[:], in_=PT[h * 128:(h + 1) * 128, nb * TBK:(nb + 1) * TBK]), reads=[bPT], writes=[bzq_])
                        fr = 512 + r * 512 + h * 128
                        c.dma('act', lambda e: e.dma_start(out=zf_[:], in_=PT[fr:fr + 128, nb * TBK:(nb + 1) * TBK]), reads=[bPT], writes=[bzf_])
                        zqs = zq_[:, ::-1] if r else zq_[:, :]
                        zfs = zf_[:, ::-1] if r else zf_[:, :]
                        c.op('act', lambda e: e.activation(out=q_[:], in_=zqs, func=AF.Silu), reads=[bzq_], writes=[bq_])
                        c.op('act', lambda e: e.activation(out=sig[:], in_=zfs, func=AF.Sigmoid), reads=[bzf_], writes=[bsig])
                        c.op('dve', lambda e: e.tensor_scalar(out=f_[:], in0=sig[:], scalar1=oml[:, hr:hr + 1], scalar2=lbc[:, hr:hr + 1], op0=ALU.mult, op1=ALU.add),
                             reads=[bsig, boml, blbc], writes=[bf_])
                        c.op('act', lambda e: e.activation(out=f_[:], in_=f_[:], func=AF.Ln), reads=[bf_], writes=[bf_])
                        c.op('dve', lambda e: e.tensor_scalar(out=kk[:], in0=sig[:], scalar1=noml[:, hr:hr + 1], scalar2=oml[:, hr:hr + 1], op0=ALU.mult, op1=ALU.add),
                             reads=[bsig, bnoml, boml], writes=[bkk])
                        c.op('dve', lambda e: e.tensor_tensor_scan(out=b_[:], data0=msk[:], data1=f_[:], initial=0.0, op0=ALU.mult, op1=ALU.add),
                             reads=[bmsk, bf_], writes=[bb_])
                        b3 = b_[:].rearrange("p (c j) -> p c j", j=64)
                        c.op('dve', lambda e: e.tensor_tensor(out=e1[:].rearrange("p (c j) -> p c j", j=64), in0=b3, in1=b3[:, :, 31:32].broadcast_to([128, TBK // 64, 64]), op=ALU.subtract),
                             reads=[bb_], writes=[be1])
                        c.op('act', lambda e: e.activation(out=eq[:], in_=e1[:], func=AF.Exp), reads=[be1], writes=[beq])
                        c.op('act', lambda e: e.activation(out=ek[:], in_=e1[:], func=AF.Exp, scale=-1.0), reads=[be1], writes=[bek])
                        c.op('dve', lambda e: e.tensor_tensor(out=qt_[:], in0=q_[:], in1=eq[:], op=ALU.mult), reads=[bq_, beq], writes=[bqt_])
                        c.op('dve', lambda e: e.tensor_tensor(out=kt_[:], in0=kk[:], in1=ek[:], op=ALU.mult), reads=[bkk, bek], writes=[bkt_])
                        nch = TBK // 64
                        c.op('act', lambda e: e.activation(out=bmid[:, tb * nch:(tb + 1) * nch], in_=b3[:, :, 31], func=AF.Copy), reads=[bb_], writes=[bbmid])
                        c.op('act', lambda e: e.activation(out=blast[:, tb * nch:(tb + 1) * nch], in_=b3[:, :, 63], func=AF.Copy), reads=[bb_], writes=[bblast])
                        c.dma('sp', lambda e: e.dma_start(out=QK[h, r, 0, :, tb * TBK:(tb + 1) * TBK], in_=qt_[:]), reads=[bqt_], pwrites=[bQK])
                        c.dma('act', lambda e: e.dma_start(out=QK[h, r, 1, :, tb * TBK:(tb + 1) * TBK], in_=kt_[:]), reads=[bkt_], pwrites=[bQK])
                    c.op('dve', lambda e: e.tensor_tensor(out=blast[:], in0=blast[:], in1=bmid[:], op=ALU.subtract), reads=[bblast, bbmid], writes=[bblast])
                    c.op('dve', lambda e: e.tensor_tensor(out=blast[:, 0:127], in0=blast[:, 0:127], in1=bmid[:, 1:128], op=ALU.add), reads=[bblast, bbmid], writes=[bblast])
                    c.op('act', lambda e: e.activation(out=mcols[:, hr, :], in_=blast[:], func=AF.Exp), reads=[bblast], writes=[bmcols])
            bQK.seal()
            barrier(c)
        if stage < 2:
            return
        with ExitStack() as es:
            def T(name, shape, dt):
                return es.enter_context(nc.sbuf_tensor(uniq(name), shape, dt))
            mask = T("hr_mask", [128, 128], F32); bmask = Buf()
            c.op('pool', lambda e: e.memset(mask[:], 1.0), writes=[bmask])
            c.op('pool', lambda e: e.affine_select(out=mask[:], in_=mask[:], pattern=[[1, 128]], compare_op=ALU.is_ge, fill=0.0, base=0, channel_multiplier=-1),
                 reads=[bmask], writes=[bmask])
            c.op('pool', lambda e: e.memset(mask[0:64, 64:128], 0.0), reads=[bmask], writes=[bmask])
            ptb, bptb = g.psb

            class CH:
                pass
            chs = []
            for r in range(2):
                ch = CH(); chs.append(ch); ch.r = r

                def TT(name, shape, dt, r=r):
                    return (T("hr%d_%s" % (r, name), shape, dt), Buf())
                ch.qb = [TT("qb%d" % i, [128, TBK], BF16) for i in range(2)]
                ch.kb = [TT("kb%d" % i, [128, TBK], BF16) for i in range(2)]
                ch.vb = [TT("vb%d" % i, [128, TBK // 128, 128], BF16) for i in range(2)]
                ch.vnat = TT("vnat", [128, TBK // 128, 128], BF16)
                ch.attnT = [TT("attn%d" % i, [128, 128], BF16) for i in range(2)]
                ch.ktok = [TT("ktok%d" % i, [128, 128], BF16) for i in range(2)]
                ch.M32 = TT("M32", [128, 128], F32); ch.Mb = TT("Mb", [128, 128], BF16); ch.tmp32 = TT("tmp32", [128, 128], F32)
                ch.osb = [TT("osb%d" % i, [128, 128], F32) for i in range(3)]
                ch.osf = [TT("osf%d" % i, [128, 128], F32) for i in range(3)]
                ch.pa = g.ps[3 * r]; ch.po = g.ps[3 * r + 1]; ch.pk = g.ps[3 * r + 2]
                ch.nblk = 0

            def blk_gen(ch, h, tb, b, qb_, bqb_, kb_, bkb_, vb_, bvb_):
                r = ch.r; hr = r * 4 + h
                blk = tb * (TBK // 128) + b
                at_, bat_ = ch.attnT[ch.nblk % 2]; kt_, bkt_ = ch.ktok[ch.nblk % 2]
                os_, bos_ = ch.osb[ch.nblk % 3]; of_, bof_ = ch.osf[ch.nblk % 3]
                ch.nblk += 1
                pa, bpa = ch.pa; po, bpo = ch.po; pk, bpk = ch.pk
                M32, bM32 = ch.M32; Mb, bMb = ch.Mb; tmp32, btmp32 = ch.tmp32
                pcol = 128 * r
                bs = slice(b * 128, (b + 1) * 128)
                c.op('pe', lambda e: e.matmul(pa[:, 0:128], lhsT=kb_[:, bs], rhs=qb_[:, bs], start=True, stop=True), reads=[bkb_, bqb_], writes=[bpa]); yield
                c.op('dve', lambda e: e.tensor_tensor(out=at_[:], in0=pa[:, 0:128], in1=mask[:], op=ALU.mult), reads=[bpa, bmask], writes=[bat_]); yield
                c.op('pe', lambda e: e.transpose(out=ptb[:, pcol:pcol + 128], in_=kb_[:, bs], identity=g.identb[:]), reads=[bkb_, g.b_identb], writes=[bptb]); yield
                c.op('act', lambda e: e.activation(out=kt_[:], in_=ptb[:, pcol:pcol + 128], func=AF.Copy), reads=[bptb], writes=[bkt_]); yield
                for ci in range(2):
                    r0 = 64 * ci
                    cidx = 2 * blk + ci
                    c.op('pe', lambda e: e.matmul(po[r0:r0 + 64, 0:128], lhsT=at_[r0:r0 + 64, r0:r0 + 64], rhs=vb_[r0:r0 + 64, b, :], start=True, stop=False),
                         reads=[bat_, bvb_], writes=[bpo])
                    c.op('pe', lambda e: e.matmul(po[r0:r0 + 64, 0:128], lhsT=qb_[:, b * 128 + r0:b * 128 + r0 + 64], rhs=Mb[:, :], start=False, stop=True),
                         reads=[bqb_, bMb], writes=[bpo]); yield
                    if cidx < 127:
                        c.op('pe', lambda e: e.matmul(pk[:, 0:128], lhsT=kt_[r0:r0 + 64, :], rhs=vb_[r0:r0 + 64, b, :], start=True, stop=True), reads=[bkt_, bvb_], writes=[bpk]); yield
                        c.op('dve', lambda e: e.tensor_tensor(out=tmp32[:], in0=pk[:, 0:128], in1=M32[:], op=ALU.add), reads=[bpk, bM32], writes=[btmp32]); yield
                        c.op('dve', lambda e: e.tensor_scalar(out=Mb[:], in0=tmp32[:], scalar1=mcols[:, hr, cidx:cidx + 1], scalar2=None, op0=ALU.mult), reads=[btmp32, bmcols], writes=[bMb]); yield
                        c.op('dve', lambda e: e.tensor_scalar(out=M32[:], in0=tmp32[:], scalar1=mcols[:, hr, cidx:cidx + 1], scalar2=None, op0=ALU.mult), reads=[btmp32, bmcols], writes=[bM32]); yield
                c.op('act', lambda e: e.activation(out=os_[:], in_=po[:, 0:128], func=AF.Copy), reads=[bpo], writes=[bos_]); yield
                if r == 0:
                    c.dma('sp', lambda e: e.dma_start(out=OA[blk * 128:(blk + 1) * 128, h * 128:(h + 1) * 128], in_=os_[:]), reads=[bos_], pwrites=[bOAs[h]]); yield
                else:
                    pf, bpf = g.ps[6]
                    c.op('pe', lambda e: e.matmul(pf[:, 0:128], lhsT=g.J32[:], rhs=os_[:], start=True, stop=True), reads=[g.b_J32, bos_], writes=[bpf]); yield
                    c.op('act', lambda e: e.activation(out=of_[:], in_=pf[:, 0:128], func=AF.Copy), reads=[bpf], writes=[bof_]); yield
                    c.dma('act', lambda e: e.dma_start(out=OA2[L - (blk + 1) * 128:L - blk * 128, h * 128:(h + 1) * 128], in_=of_[:]), reads=[bof_], pwrites=[bOA2s[h]]); yield

            for h in range(4):
                for ch in chs:
                    M32, bM32 = ch.M32; Mb, bMb = ch.Mb
                    c.op('pool', lambda e: e.memset(M32[:], 0.0), reads=[bM32], writes=[bM32])
                    c.op('pool', lambda e: e.memset(Mb[:], 0.0), reads=[bMb], writes=[bMb])
                for tb in range(NTB):
                    loaded = []
                    for ch in chs:
                        r = ch.r
                        qb_, bqb_ = ch.qb[tb % 2]; kb_, bkb_ = ch.kb[tb % 2]; vb_, bvb_ = ch.vb[tb % 2]
                        c.dma('sp', lambda e: e.dma_start(out=qb_[:], in_=QK[h, r, 0, :, tb * TBK:(tb + 1) * TBK]), reads=[bQK], writes=[bqb_])
                        c.dma('act', lambda e: e.dma_start(out=kb_[:], in_=QK[h, r, 1, :, tb * TBK:(tb + 1) * TBK]), reads=[bQK], writes=[bkb_])
                        if r == 0:
                            vsrc = PV[tb * TBK:(tb + 1) * TBK, h * 128:(h + 1) * 128].rearrange("(b p) d -> p b d", p=128)
                            c.dma('sp', lambda e: e.dma_start(out=vb_[:], in_=vsrc), reads=[bPV], writes=[bvb_])
                        else:
                            vnat, bvnat = ch.vnat
                            vsrc = PV[L - (tb + 1) * TBK:L - tb * TBK, h * 128:(h + 1) * 128].rearrange("(b p) d -> p b d", p=128)
                            c.dma('sp', lambda e: e.dma_start(out=vnat[:], in_=vsrc), reads=[bPV], writes=[bvnat])
                            nbk = TBK // 128
                            for b4 in range(0, nbk, 4):
                                pf, bpf = g.ps[6]
                                c.op('pe', lambda e: e.matmul(pf[:, :], lhsT=g.Jb[:], rhs=vnat[:, b4:b4 + 4, :], start=True, stop=True), reads=[g.b_Jb, bvnat], writes=[bpf])
                                for bb in range(4):
                                    c.op('act', lambda e: e.activation(out=vb_[:, nbk - 1 - (b4 + bb), :], in_=pf[:, bb * 128:(bb + 1) * 128], func=AF.Copy), reads=[bpf, bvb_], writes=[bvb_])
                        loaded.append((qb_, bqb_, kb_, bkb_, vb_, bvb_))
                    for b in range(TBK // 128):
                        gens = [blk_gen(ch, h, tb, b, *loaded[i]) for i, ch in enumerate(chs)]
                        alive = list(gens)
                        while alive:
                            for gnr in list(alive):
                                try:
                                    next(gnr)
                                except StopIteration:
                                    alive.remove(gnr)
                bOAs[h].seal(); bOA2s[h].seal()
            barrier(c)
        if stage < 3:
            return
    gated_norm_finalize(c, g, OA, bOAs, PV, bPV, 512, a_out_norm, MT, bMT, 0, "hf_", OA2=OA2, bOA2s=bOA2s)


TB5 = 1024
NTB5 = L // TB5
TWO_PI = 2.0 * math.pi


def s5_phase(c, g, PT, bPT, lam_re, lam_im, log_step, b_re, b_im, c_re, c_im, d_skip, glu_w, glu_b, YT, bYT, MT, bMT, stage=3):
    nc = c.nc
    U0 = 1536
    with ExitStack() as es0:
        def T0(name, shape, dt):
            return es0.enter_context(nc.sbuf_tensor(uniq(name), shape, dt))
        WB = [T0("s5_WB%d" % p, [128, 2, 4, 128], BF16) for p in range(2)]; bWB = Buf()
        WC = [T0("s5_WC%d" % p, [128, 2, 4, 128], BF16) for p in range(2)]; bWC = Buf()
        WBx = [T0("s5_WBx%d" % p, [128, 2, 4, 128], BF16) for p in range(2)]
        WCx = [T0("s5_WCx%d" % p, [128, 2, 4, 64], BF16) for p in range(2)]
        mag = T0("s5_mag", [128, 32], F32); bmag = Buf()
        pwc = T0("s5_pwc", [128, 11, 32], F32); bpw = Buf()
        pws = T0("s5_pws", [128, 11, 32], F32)
        with ExitStack() as es:
            def T(name, shape, dt):
                return es.enter_context(nc.sbuf_tensor(uniq(name), shape, dt))
            n_ = [0]

            def S(shape=[128, 32], dt=F32):
                n_[0] += 1
                return T("s5_t%d" % n_[0], shape, dt), Buf()
            lamre, blamre = S(); lamim, blamim = S()
            lsB, blsB = S([128, 64]); ls, bls = S()
            with nc.allow_non_contiguous_dma(reason="small params"):
                for r_ in range(2):
                    for g4 in range(0, 16, 4):
                        c.dma('sp', lambda e: e.dma_start(out=lamre[:, r_ * 16 + g4:r_ * 16 + g4 + 4], in_=lam_re[r_, 2 * g4:2 * g4 + 8, :].rearrange("(gp gl) n -> (gl n) gp", gl=2)), pwrites=[blamre])
                        c.dma('act', lambda e: e.dma_start(out=lamim[:, r_ * 16 + g4:r_ * 16 + g4 + 4], in_=lam_im[r_, 2 * g4:2 * g4 + 8, :].rearrange("(gp gl) n -> (gl n) gp", gl=2)), pwrites=[blamim])
                blamre.seal(); blamim.seal()
                c.dma('sp', lambda e: e.dma_start(out=lsB[:], in_=log_step.rearrange("r g -> (r g)").partition_broadcast(128)), writes=[blsB])
            lsv = lsB[:].rearrange("p (r gp gl) -> p r gp gl", r=2, gl=2)
            c.op('dve', lambda e: e.tensor_copy(out=ls[0:64, :].rearrange("p (r gp) -> p r gp", r=2), in_=lsv[0:64, :, :, 0]), reads=[blsB], writes=[bls])
            c.op('dve', lambda e: e.tensor_copy(out=ls[64:128, :].rearrange("p (r gp) -> p r gp", r=2), in_=lsv[64:128, :, :, 1]), reads=[blsB], writes=[bls])
            step, bstep = S()
            c.op('act', lambda e: e.activation(out=step[:], in_=ls[:], func=AF.Exp), reads=[bls], writes=[bstep])
            lrs, blrs = S(); ang, bang = S()
            c.op('dve', lambda e: e.tensor_tensor(out=lrs[:], in0=lamre[:], in1=step[:], op=ALU.mult), reads=[blamre, bstep], writes=[blrs])
            c.op('act', lambda e: e.activation(out=mag[:], in_=lrs[:], func=AF.Exp), reads=[blrs], writes=[bmag])
            c.op('dve', lambda e: e.tensor_tensor(out=ang[:], in0=lamim[:], in1=step[:], op=ALU.mult), reads=[blamim, bstep], writes=[bang])

            def sin_of(src, bsrc, offset, dst, bdst):
                q, bq = S(); qi, bqi = S(dt=I32); r, br = S(); m, bm = S()
                c.op('dve', lambda e: e.tensor_scalar(out=q[:], in0=src[:], scalar1=offset, scalar2=1.0 / TWO_PI, op0=ALU.add, op1=ALU.mult), reads=[bsrc], writes=[bq])
                c.op('dve', lambda e: e.tensor_copy(out=qi[:], in_=q[:]), reads=[bq], writes=[bqi])
                c.op('dve', lambda e: e.tensor_copy(out=q[:], in_=qi[:]), reads=[bqi], writes=[bq])
                c.op('dve', lambda e: e.scalar_tensor_tensor(out=r[:], in0=q[:], scalar=-TWO_PI, in1=src[:], op0=ALU.mult, op1=ALU.add), reads=[bq, bsrc], writes=[br])
                if offset != 0.0:
                    c.op('dve', lambda e: e.tensor_scalar(out=r[:], in0=r[:], scalar1=offset, scalar2=None, op0=ALU.add), reads=[br], writes=[br])
                c.op('dve', lambda e: e.tensor_scalar(out=m[:], in0=r[:], scalar1=math.pi, scalar2=-TWO_PI, op0=ALU.is_gt, op1=ALU.mult), reads=[br], writes=[bm])
                c.op('dve', lambda e: e.tensor_tensor(out=r[:], in0=r[:], in1=m[:], op=ALU.add), reads=[br, bm], writes=[br])
                c.op('dve', lambda e: e.tensor_scalar(out=m[:], in0=r[:], scalar1=-math.pi, scalar2=TWO_PI, op0=ALU.is_lt, op1=ALU.mult), reads=[br], writes=[bm])
                c.op('dve', lambda e: e.tensor_tensor(out=r[:], in0=r[:], in1=m[:], op=ALU.add), reads=[br, bm], writes=[br])
                c.op('dve', lambda e: e.tensor_scalar(out=r[:], in0=r[:], scalar1=math.pi, scalar2=-math.pi, op0=ALU.min, op1=ALU.max), reads=[br], writes=[br])
                c.op('act', lambda e: e.activation(out=dst, in_=r[:], func=AF.Sin), reads=[br], writes=[bdst])
            sin_of(ang, bang, 0.0, pws[:, 0, :], bpw)
            sin_of(ang, bang, math.pi / 2, pwc[:, 0, :], bpw)
            tq, btq = S(); tq2, btq2 = S()
            for k in range(10):
                c.op('dve', lambda e: e.tensor_tensor(out=tq[:], in0=pwc[:, k, :], in1=pwc[:, k, :], op=ALU.mult), reads=[bpw], writes=[btq])
                c.op('dve', lambda e: e.tensor_tensor(out=tq2[:], in0=pws[:, k, :], in1=pws[:, k, :], op=ALU.mult), reads=[bpw], writes=[btq2])
                c.op('dve', lambda e: e.tensor_tensor(out=pwc[:, k + 1, :], in0=tq[:], in1=tq2[:], op=ALU.subtract), reads=[btq, btq2, bpw], writes=[bpw])
                c.op('dve', lambda e: e.tensor_tensor(out=tq[:], in0=pws[:, k, :], in1=pwc[:, k, :], op=ALU.mult), reads=[bpw], writes=[btq])
                c.op('dve', lambda e: e.tensor_scalar(out=pws[:, k + 1, :], in0=tq[:], scalar1=2.0, scalar2=None, op0=ALU.mult), reads=[btq, bpw], writes=[bpw])
            are, bare = S(); aim, baim = S(); den, bden = S(); am1, bam1 = S(); fr, bfr = S(); fi, bfi = S(); tt, btt = S()
            c.op('dve', lambda e: e.tensor_tensor(out=are[:], in0=mag[:], in1=pwc[:, 0, :], op=ALU.mult), reads=[bmag, bpw], writes=[bare])
            c.op('dve', lambda e: e.tensor_tensor(out=aim[:], in0=mag[:], in1=pws[:, 0, :], op=ALU.mult), reads=[bmag, bpw], writes=[baim])
            c.op('dve', lambda e: e.tensor_tensor(out=den[:], in0=lamre[:], in1=lamre[:], op=ALU.mult), reads=[blamre], writes=[bden])
            c.op('dve', lambda e: e.tensor_tensor(out=tt[:], in0=lamim[:], in1=lamim[:], op=ALU.mult), reads=[blamim], writes=[btt])
            c.op('dve', lambda e: e.tensor_tensor(out=den[:], in0=den[:], in1=tt[:], op=ALU.add), reads=[bden, btt], writes=[bden])
            c.op('dve', lambda e: e.reciprocal(out=den[:], in_=den[:]), reads=[bden], writes=[bden])
            c.op('dve', lambda e: e.tensor_scalar(out=am1[:], in0=are[:], scalar1=-1.0, scalar2=None, op0=ALU.add), reads=[bare], writes=[bam1])
            c.op('dve', lambda e: e.tensor_tensor(out=fr[:], in0=am1[:], in1=lamre[:], op=ALU.mult), reads=[bam1, blamre], writes=[bfr])
            c.op('dve', lambda e: e.tensor_tensor(out=tt[:], in0=aim[:], in1=lamim[:], op=ALU.mult), reads=[baim, blamim], writes=[btt])
            c.op('dve', lambda e: e.tensor_tensor(out=fr[:], in0=fr[:], in1=tt[:], op=ALU.add), reads=[bfr, btt], writes=[bfr])
            c.op('dve', lambda e: e.tensor_tensor(out=fr[:], in0=fr[:], in1=den[:], op=ALU.mult), reads=[bfr, bden], writes=[bfr])
            c.op('dve', lambda e: e.tensor_tensor(out=fi[:], in0=aim[:], in1=lamre[:], op=ALU.mult), reads=[baim, blamre], writes=[bfi])
            c.op('dve', lambda e: e.tensor_tensor(out=tt[:], in0=am1[:], in1=lamim[:], op=ALU.mult), reads=[bam1, blamim], writes=[btt])
            c.op('dve', lambda e: e.tensor_tensor(out=fi[:], in0=fi[:], in1=tt[:], op=ALU.subtract), reads=[bfi, btt], writes=[bfi])
            c.op('dve', lambda e: e.tensor_tensor(out=fi[:], in0=fi[:], in1=den[:], op=ALU.mult), reads=[bfi, bden], writes=[bfi])
            mk, bmk = S([128, 2])
            c.op('pool', lambda e: e.memset(mk[:], 0.0), writes=[bmk])
            c.op('pool', lambda e: e.memset(mk[0:64, 0:1], 1.0), reads=[bmk], writes=[bmk])
            c.op('pool', lambda e: e.memset(mk[64:128, 1:2], 1.0), reads=[bmk], writes=[bmk])
            Bn = [S([128, 2, 16, 16]) for _ in range(2)]
            with nc.allow_non_contiguous_dma(reason="small params"):
                for r_ in range(2):
                    for g4 in range(0, 16, 4):
                        c.dma('sp', lambda e: e.dma_start(out=Bn[0][0][:, r_, g4:g4 + 4, :], in_=b_re[r_, 2 * g4:2 * g4 + 8].rearrange("(gp gl) n p -> (gl n) gp p", gl=2)), pwrites=[Bn[0][1]])
                        c.dma('act', lambda e: e.dma_start(out=Bn[1][0][:, r_, g4:g4 + 4, :], in_=b_im[r_, 2 * g4:2 * g4 + 8].rearrange("(gp gl) n p -> (gl n) gp p", gl=2)), pwrites=[Bn[1][1]])
                Bn[0][1].seal(); Bn[1][1].seal()
            frb = fr[:].rearrange("p (r gp) -> p r gp", r=2).unsqueeze(3).broadcast_to([128, 2, 16, 16])
            fib = fi[:].rearrange("p (r gp) -> p r gp", r=2).unsqueeze(3).broadcast_to([128, 2, 16, 16])
            bbr, bbbr = S([128, 2, 16, 16]); bbi, bbbi = S([128, 2, 16, 16]); t5, bt5 = S([128, 2, 16, 16])
            c.op('dve', lambda e: e.tensor_tensor(out=bbr[:], in0=Bn[0][0][:], in1=frb, op=ALU.mult), reads=[Bn[0][1], bfr], writes=[bbbr])
            c.op('dve', lambda e: e.tensor_tensor(out=t5[:], in0=Bn[1][0][:], in1=fib, op=ALU.mult), reads=[Bn[1][1], bfi], writes=[bt5])
            c.op('dve', lambda e: e.tensor_tensor(out=bbr[:], in0=bbr[:], in1=t5[:], op=ALU.subtract), reads=[bbbr, bt5], writes=[bbbr])
            c.op('dve', lambda e: e.tensor_tensor(out=bbi[:], in0=Bn[1][0][:], in1=frb, op=ALU.mult), reads=[Bn[1][1], bfr], writes=[bbbi])
            c.op('dve', lambda e: e.tensor_tensor(out=t5[:], in0=Bn[0][0][:], in1=fib, op=ALU.mult), reads=[Bn[0][1], bfi], writes=[bt5])
            c.op('dve', lambda e: e.tensor_tensor(out=bbi[:], in0=bbi[:], in1=t5[:], op=ALU.add), reads=[bbbi, bt5], writes=[bbbi])
            BBm, bBBm = S([128, 2, 16, 2, 16], BF16)
            ptb, bptb = g.psb
            for part, (src, bsrc) in enumerate(((bbr, bbbr), (bbi, bbbi))):
                for gl in range(2):
                    c.op('dve', lambda e: e.tensor_scalar(out=BBm[:, :, :, gl, :], in0=src[:], scalar1=mk[:, gl:gl + 1], scalar2=None, op0=ALU.mult), reads=[bsrc, bmk, bBBm], writes=[bBBm])
                for r in range(2):
                    for cb in range(4):
                        c.op('pe', lambda e: e.transpose(out=ptb[:, 0:128], in_=BBm[:, r, 4 * cb:4 * cb + 4, :, :].rearrange("p a b c -> p (a b c)"), identity=g.identb[:]),
                             reads=[bBBm, g.b_identb], writes=[bptb])
                        c.op('act', lambda e: e.activation(out=WB[part][:, r, cb, :], in_=ptb[:, 0:128], func=AF.Copy), reads=[bptb], writes=[bWB])
            Cn = [S([128, 2, 4, 64]) for _ in range(2)]
            c.dma('sp', lambda e: e.dma_start(out=Cn[0][0][:], in_=c_re.rearrange("r (cb g8) p n -> (g8 p) r cb n", g8=8)), writes=[Cn[0][1]])
            c.dma('act', lambda e: e.dma_start(out=Cn[1][0][:], in_=c_im.rearrange("r (cb g8) p n -> (g8 p) r cb n", g8=8)), writes=[Cn[1][1]])
            Cd, bCd = S([128, 2, 4, 2, 64], BF16)
            mkb = mk[:].unsqueeze(1).unsqueeze(3).broadcast_to([128, 4, 2, 16])
            for part in range(2):
                sc = 1.0 if part == 0 else -1.0
                for x in range(2):
                    c.op('dve', lambda e: e.tensor_scalar(out=Cd[:, :, :, x, :], in0=Cn[part][0][:], scalar1=sc, scalar2=None, op0=ALU.mult), reads=[Cn[part][1], bCd], writes=[bCd])
                for r in range(2):
                    for cb in range(4):
                        c.op('pe', lambda e: e.transpose(out=ptb[:, 0:128], in_=Cd[:, r, cb, :, :].rearrange("p a b -> p (a b)"), identity=g.identb[:]),
                             reads=[bCd, g.b_identb], writes=[bptb])
                        c.op('dve', lambda e: e.tensor_tensor(out=WC[part][:, r, cb, :].rearrange("p (k a b) -> p k a b", k=4, a=2), in0=ptb[:, 0:128].rearrange("p (k a b) -> p k a b", k=4, a=2),
                                                              in1=mkb, op=ALU.mult), reads=[bptb, bmk], writes=[bWC])
            for part in range(2):
                c.op('act', lambda e: e.activation(out=WBx[part][64:128], in_=WB[part][64:128], func=AF.Copy), reads=[bWB], writes=[bWB])
                c.op('pool', lambda e: e.memset(WBx[part][64:96], 0.0), reads=[bWB], writes=[bWB])
                c.op('act', lambda e: e.activation(out=WCx[part][:], in_=WC[part][:, :, :, 64:128], func=AF.Copy), reads=[bWC], writes=[bWC])
                c.op('pool', lambda e: e.memset(WCx[part][:, :, :, 0:32], 0.0), reads=[bWC], writes=[bWC])
            barrier(c)
        if stage < 2:
            return
        with ExitStack() as es:
            def T(name, shape, dt):
                return es.enter_context(nc.sbuf_tensor(uniq(name), shape, dt))
            uf = T("s5_uf", [128, TB5 * 2], F32); buf_ = Buf()
            ub = [T("s5_ub%d" % r, [128, L], BF16) for r in range(2)]; bub = [Buf(), Buf()]
            Xa = [[T("s5_X%d%d" % (p, r), [128, L], BF16) for r in range(2)] for p in range(2)]
            bXa = [[Buf(), Buf()], [Buf(), Buf()]]
            tcos = T("s5_cos", [128, TB5], F32); tsin = T("s5_sin", [128, TB5], F32); btab = Buf()
            BUs = [T("s5_BU%d" % p, [128, TB5], F32) for p in range(2)]; bBUs = [Buf(), Buf()]
            t = [T("s5_w%d" % i, [128, TB5], F32) for i in range(4)]; bt = [Buf() for _ in range(4)]
            ini = T("s5_ini", [128, 4], F32); bini = Buf()
            yst = [T("s5_yst%d" % i, [128, 512], F32) for i in range(2)]; byst = [Buf(), Buf()]
            npz = 0
            for cb in range(4):
                for r in range(2):
                    for hh in range(L // (2 * TB5)):
                        nb = hh if r == 0 else L // (2 * TB5) - 1 - hh
                        c.dma('sp', lambda e: e.dma_start(out=uf[:], in_=PT[U0 + cb * 128:U0 + (cb + 1) * 128, nb * 2 * TB5:(nb + 1) * 2 * TB5]), reads=[bPT], writes=[buf_])
                        src = uf[:, ::-1] if r else uf[:, :]
                        c.op('act', lambda e: e.activation(out=ub[r][:, hh * 2 * TB5:(hh + 1) * 2 * TB5], in_=src, func=AF.Copy), reads=[buf_], writes=[bub[r]])
                for k in range(4):
                    gp = cb * 4 + k
                    for r in range(2):
                        col = r * 16 + gp
                        c.op('pool', lambda e: e.memset(tcos[:, 0:1], 1.0), writes=[btab])
                        c.op('pool', lambda e: e.memset(tsin[:, 0:1], 0.0), reads=[btab], writes=[btab])
                        n = 1
                        kk = 0
                        while n < TB5:
                            cr = pwc[:, kk, col:col + 1]; ci = pws[:, kk, col:col + 1]
                            c.op('dve', lambda e: e.tensor_scalar(out=t[0][:, 0:n], in0=tsin[:, 0:n], scalar1=ci, scalar2=None, op0=ALU.mult), reads=[btab, bpw], writes=[bt[0]])
                            c.op('dve', lambda e: e.tensor_scalar(out=t[1][:, 0:n], in0=tsin[:, 0:n], scalar1=cr, scalar2=None, op0=ALU.mult), reads=[btab, bpw], writes=[bt[1]])
                            c.op('dve', lambda e: e.scalar_tensor_tensor(out=tsin[:, n:2 * n], in0=tcos[:, 0:n], scalar=ci, in1=t[1][:, 0:n], op0=ALU.mult, op1=ALU.add),
                                 reads=[btab, bpw, bt[1]], writes=[btab])
                            c.op('dve', lambda e: e.scalar_tensor_tensor(out=tcos[:, n:2 * n], in0=tcos[:, 0:n], scalar=cr, in1=t[0][:, 0:n], op0=ALU.mult, op1=ALU.subtract),
                                 reads=[btab, bpw, bt[0]], writes=[btab])
                            n *= 2; kk += 1
                        cTB = pwc[:, kk, col:col + 1]; sTB = pws[:, kk, col:col + 1]
                        rho = mag[:, col:col + 1]
                        c.op('pool', lambda e: e.memset(ini[:], 0.0), writes=[bini])
                        for tb in range(NTB5):
                            ts0 = tb * TB5
                            for part in range(2):
                                for hf in range(TB5 // 512):
                                    ps, bps = g.ps[npz % 4]; npz += 1
                                    if k < 3:
                                        lh = WB[part][32 * k:32 * k + 32, r, cb, :]; rh = ub[r][32 * k:32 * k + 32, ts0 + hf * 512:ts0 + (hf + 1) * 512]
                                    else:
                                        lh = WBx[part][64:128, r, cb, :]; rh = ub[r][64:128, ts0 + hf * 512:ts0 + (hf + 1) * 512]
                                    c.op('pe', lambda e: e.matmul(ps[:, :], lhsT=lh, rhs=rh, start=True, stop=True),
                                         reads=[bWB, bub[r]], writes=[bps])
                                    c.op('act', lambda e: e.activation(out=BUs[part][:, hf * 512:(hf + 1) * 512], in_=ps[:, :], func=AF.Copy), reads=[bps], writes=[bBUs[part]])
                            c.op('dve', lambda e: e.tensor_tensor(out=t[0][:], in0=BUs[0][:], in1=tcos[:], op=ALU.mult), reads=[bBUs[0], btab], writes=[bt[0]])
                            c.op('dve', lambda e: e.tensor_tensor(out=t[1][:], in0=BUs[1][:], in1=tsin[:], op=ALU.mult), reads=[bBUs[1], btab], writes=[bt[1]])
                            c.op('dve', lambda e: e.tensor_tensor(out=t[0][:], in0=t[0][:], in1=t[1][:], op=ALU.add), reads=[bt[0], bt[1]], writes=[bt[0]])
                            c.op('pool', lambda e: e.tensor_tensor(out=t[2][:], in0=BUs[1][:], in1=tcos[:], op=ALU.mult), reads=[bBUs[1], btab], writes=[bt[2]])
                            c.op('pool', lambda e: e.tensor_tensor(out=t[3][:], in0=BUs[0][:], in1=tsin[:], op=ALU.mult), reads=[bBUs[0], btab], writes=[bt[3]])
                            c.op('dve', lambda e: e.tensor_tensor(out=t[2][:], in0=t[2][:], in1=t[3][:], op=ALU.subtract), reads=[bt[2], bt[3]], writes=[bt[2]])
                            c.op('dve', lambda e: e.tensor_tensor_scan(out=t[1][:], data0=rho.broadcast_to([128, TB5]), data1=t[0][:], initial=ini[:, 0:1], op0=ALU.mult, op1=ALU.add),
                                 reads=[bmag, bt[0], bini], writes=[bt[1]])
                            c.op('dve', lambda e: e.tensor_tensor_scan(out=t[3][:], data0=rho.broadcast_to([128, TB5]), data1=t[2][:], initial=ini[:, 1:2], op0=ALU.mult, op1=ALU.add),
                                 reads=[bmag, bt[2], bini], writes=[bt[3]])
                            if tb < NTB5 - 1:
                                xr = t[1][:, TB5 - 1:TB5]; xi = t[3][:, TB5 - 1:TB5]
                                c.op('dve', lambda e: e.tensor_scalar(out=ini[:, 2:3], in0=xi, scalar1=sTB, scalar2=None, op0=ALU.mult), reads=[bt[3], bpw], writes=[bini])
                                c.op('dve', lambda e: e.scalar_tensor_tensor(out=ini[:, 0:1], in0=xr, scalar=cTB, in1=ini[:, 2:3], op0=ALU.mult, op1=ALU.subtract), reads=[bt[1], bpw, bini], writes=[bini])
                                c.op('dve', lambda e: e.tensor_scalar(out=ini[:, 3:4], in0=xi, scalar1=cTB, scalar2=None, op0=ALU.mult), reads=[bt[3], bpw], writes=[bini])
                                c.op('dve', lambda e: e.scalar_tensor_tensor(out=ini[:, 1:2], in0=xr, scalar=sTB, in1=ini[:, 3:4], op0=ALU.mult, op1=ALU.add), reads=[bt[1], bpw, bini], writes=[bini])
                            if r == 0:
                                oslc = slice(ts0, ts0 + TB5)
                                xo_re = Xa[0][r][:, oslc]; xo_im = Xa[1][r][:, oslc]
                            else:
                                lo = L - ts0 - TB5
                                xo_re = Xa[0][r][:, lo:lo + TB5][:, ::-1]; xo_im = Xa[1][r][:, lo:lo + TB5][:, ::-1]
                            c.op('dve', lambda e: e.tensor_tensor(out=t[0][:], in0=t[1][:], in1=tcos[:], op=ALU.mult), reads=[bt[1], btab], writes=[bt[0]])
                            c.op('pool', lambda e: e.tensor_tensor(out=t[2][:], in0=t[3][:], in1=tsin[:], op=ALU.mult), reads=[bt[3], btab], writes=[bt[2]])
                            c.op('dve', lambda e: e.tensor_tensor(out=xo_re, in0=t[0][:], in1=t[2][:], op=ALU.subtract), reads=[bt[0], bt[2]], pwrites=[bXa[0][r]])
                            c.op('pool', lambda e: e.tensor_tensor(out=t[0][:], in0=t[1][:], in1=tsin[:], op=ALU.mult), reads=[bt[1], btab], writes=[bt[0]])
                            c.op('dve', lambda e: e.tensor_tensor(out=t[2][:], in0=t[3][:], in1=tcos[:], op=ALU.mult), reads=[bt[3], btab], writes=[bt[2]])
                            c.op('dve', lambda e: e.tensor_tensor(out=xo_im, in0=t[0][:], in1=t[2][:], op=ALU.add), reads=[bt[0], bt[2]], pwrites=[bXa[1][r]])
                        bXa[0][r].seal(); bXa[1][r].seal()
                    for it in range(L // 512):
                        ps, bps = g.ps[4 + it % 2]
                        i = 0
                        for r in range(2):
                            for part in range(2):
                                if k < 3:
                                    po = ps[32 * k:32 * k + 32, :]; lh = WC[part][:, r, cb, 32 * k:32 * k + 32]
                                else:
                                    po = ps[64:128, :]; lh = WCx[part][:, r, cb, :]
                                c.op('pe', lambda e: e.matmul(po, lhsT=lh, rhs=Xa[part][r][:, it * 512:(it + 1) * 512], start=(i == 0), stop=(i == 3)),
                                     reads=[bWC, bXa[part][r]], writes=[bps])
                                i += 1
                        ys, bys = yst[it % 2], byst[it % 2]
                        e0 = 32 * k if k < 3 else 64
                        c.op('act', lambda e: e.activation(out=ys[e0:32 * k + 32, :], in_=ps[e0:32 * k + 32, :], func=AF.Copy), reads=[bps], writes=[bys])
                        c.dma('sp', lambda e: e.dma_start(out=YT[cb * 128 + 32 * k:cb * 128 + 32 * k + 32, it * 512:(it + 1) * 512], in_=ys[32 * k:32 * k + 32, :]), reads=[bys], pwrites=[bYT])
            bYT.seal()
            barrier(c)
        if stage < 3:
            return
        with ExitStack() as es:
            def T(name, shape, dt):
                return es.enter_context(nc.sbuf_tensor(uniq(name), shape, dt))
            gw = T("s5_gw", [128, 4, 512], BF16); bgw = Buf()
            dcol = T("s5_dcol", [128, 4], F32); bdcol = Buf()
            gbc = T("s5_gbc", [128, 4], F32); bgbc = Buf()
            yt = [T("s5_yt%d" % i, [128, 4, 512], F32) for i in range(2)]; byt = [Buf(), Buf()]
            ut = [T("s5_ut%d" % i, [128, 4, 512], F32) for i in range(2)]; but = [Buf(), Buf()]
            sq = T("s5_sq", [128, 4, 512], F32); bsq = Buf()
            gy = T("s5_gy", [128, 4, 512], F32); bgy = Buf()
            gyb = T("s5_gyb", [128, 4, 512], BF16); bgyb = Buf()
            sg = [T("s5_sg%d" % i, [128, 512], F32) for i in range(2)]; bsg = [Buf(), Buf()]
            ob = [T("s5_ob%d" % i, [128, 4, 512], BF16) for i in range(2)]; bob = [Buf(), Buf()]
            c.dma('pool', lambda e: e.dma_start(out=gw[:], in_=glu_w.rearrange("(k p) f -> p k f", p=128)), writes=[bgw])
            with nc.allow_non_contiguous_dma(reason="small params"):
                c.dma('sp', lambda e: e.dma_start(out=dcol[:], in_=d_skip.rearrange("(k p) -> p k", p=128)), writes=[bdcol])
                c.dma('sp', lambda e: e.dma_start(out=gbc[:], in_=glu_b.rearrange("(k p) -> p k", p=128)), writes=[bgbc])
            GC = 1.5957691216057308
            for it in range(L // 512):
                yt_, byt_ = yt[it % 2], byt[it % 2]
                ut_, but_ = ut[it % 2], but[it % 2]
                ob_, bob_ = ob[it % 2], bob[it % 2]
                tsl = slice(it * 512, (it + 1) * 512)
                c.dma('sp', lambda e: e.dma_start(out=yt_[:], in_=YT[:, tsl].rearrange("(k p) t -> p k t", p=128)), reads=[bYT], writes=[byt_])
                c.dma('act', lambda e: e.dma_start(out=ut_[:], in_=PT[U0:U0 + 512, tsl].rearrange("(k p) t -> p k t", p=128)), reads=[bPT], writes=[but_])
                for k in range(4):
                    c.op('dve', lambda e: e.scalar_tensor_tensor(out=yt_[:, k, :], in0=ut_[:, k, :], scalar=dcol[:, k:k + 1], in1=yt_[:, k, :], op0=ALU.mult, op1=ALU.add),
                         reads=[but_, bdcol, byt_], writes=[byt_])
                c.op('act', lambda e: e.activation(out=sq[:], in_=yt_[:], func=AF.Square), reads=[byt_], writes=[bsq])
                c.op('dve', lambda e: e.tensor_scalar(out=sq[:], in0=sq[:], scalar1=0.044715, scalar2=1.0, op0=ALU.mult, op1=ALU.add), reads=[bsq], writes=[bsq])
                c.op('dve', lambda e: e.tensor_tensor(out=sq[:], in0=sq[:], in1=yt_[:], op=ALU.mult), reads=[bsq, byt_], writes=[bsq])
                c.op('act', lambda e: e.activation(out=sq[:], in_=sq[:], func=AF.Sigmoid, scale=GC), reads=[bsq], writes=[bsq])
                c.op('dve', lambda e: e.tensor_tensor(out=gy[:], in0=sq[:], in1=yt_[:], op=ALU.mult), reads=[bsq, byt_], writes=[bgy])
                c.op('act', lambda e: e.activation(out=gyb[:], in_=gy[:], func=AF.Copy), reads=[bgy], writes=[bgyb])
                for co in range(4):
                    ps, bps = g.ps[co % 4]
                    for k in range(4):
                        c.op('pe', lambda e: e.matmul(ps[:, :], lhsT=gw[:, k, co * 128:(co + 1) * 128], rhs=gyb[:, k, :], start=(k == 0), stop=(k == 3)),
                             reads=[bgw, bgyb], writes=[bps])
                    sg_, bsg_ = sg[co % 2], bsg[co % 2]
                    c.op('act', lambda e: e.activation(out=sg_[:], in_=ps[:, :], func=AF.Sigmoid, bias=gbc[:, co:co + 1], scale=1.0), reads=[bps, bgbc], writes=[bsg_])
                    c.op('dve', lambda e: e.tensor_tensor(out=ob_[:, co, :], in0=gy[:, co, :], in1=sg_[:], op=ALU.mult), reads=[bgy, bsg_, bob_], writes=[bob_])
                c.dma('sp', lambda e: e.dma_start(out=MT[512:1024, tsl].rearrange("(k p) t -> p k t", p=128), in_=ob_[:]), reads=[bob_], pwrites=[bMT])
            barrier(c)


def outproj_phase(c, g, MT, bMT, w_out, X, bX, tile_cb=None):
    nc = c.nc
    bXn = Buf('Xn')
    with ExitStack() as es:
        def T(name, shape, dt):
            return es.enter_context(nc.sbuf_tensor(uniq(name), shape, dt))
        wsb = T("op_w", [128, 8, D], BF16); bw = Buf()
        mt = [T("op_mt%d" % i, [128, 8, 512], BF16) for i in range(2)]; bmt = [Buf(), Buf()]
        xt = [T("op_xt%d" % i, [128, 4, D], F32) for i in range(2)]; bxt = [Buf(), Buf()]
        xo = [T("op_xo%d" % i, [128, 4, D], F32) for i in range(2)]; bxo = [Buf(), Buf()]
        for c0 in range(0, D, 512):
            c.dma('pool', lambda e: e.dma_start(out=wsb[:, :, c0:c0 + 512], in_=w_out[:, c0:c0 + 512].rearrange("(k p) f -> p k f", p=128)), pwrites=[bw])
        bw.seal()
        n = 0
        for it in range(L // 512):
            t0 = it * 512
            mt_, bmt_ = mt[it % 2], bmt[it % 2]
            xt_, bxt_ = xt[it % 2], bxt[it % 2]
            xo_, bxo_ = xo[it % 2], bxo[it % 2]
            c.dma('sp', lambda e: e.dma_start(out=mt_[:], in_=MT[:, t0:t0 + 512].rearrange("(k p) t -> p k t", p=128)), reads=[bMT], writes=[bmt_])
            c.dma('act', lambda e: e.dma_start(out=xt_[:], in_=X[t0:t0 + 512, :].rearrange("(j p) d -> p j d", p=128)), reads=[bX], writes=[bxt_])
            for j in range(4):
                for dh in range(2):
                    ps, bps = g.ps[n % 4]; n += 1
                    for k in range(8):
                        c.op('pe', lambda e: e.matmul(ps[:, :], lhsT=mt_[:, k, j * 128:(j + 1) * 128], rhs=wsb[:, k, dh * 512:(dh + 1) * 512], start=(k == 0), stop=(k == 7)),
                             reads=[bmt_, bw], writes=[bps])
                    c.op('dve', lambda e: e.tensor_tensor(out=xo_[:, j, dh * 512:(dh + 1) * 512], in0=ps[:, :], in1=xt_[:, j, dh * 512:(dh + 1) * 512], op=ALU.add),
                         reads=[bps, bxt_, bxo_], writes=[bxo_])
            c.dma('sp', lambda e: e.dma_start(out=X[t0:t0 + 512, :].rearrange("(j p) d -> p j d", p=128), in_=xo_[:]), reads=[bxo_], pwrites=[bXn])
            if tile_cb is not None:
                for j in range(4):
                    tile_cb(it * 4 + j, xo_[:, j, :], bxo_)
        bXn.seal()
        barrier(c)
    return bXn


def outproj_moe(c, g, MT, bMT, w_out, X, bX, HB, bHB, ffn_g, w_router, w_gate, w_up, w_down):
    with ExitStack() as es:
        sb = moe_alloc(c.nc, es, part=1)
        moe_prep(c, g, sb, ffn_g, w_router)
        bXn = outproj_phase(c, g, MT, bMT, w_out, X, bX, tile_cb=lambda i, xt, bxt: moe_step1_tile(c, g, sb, i, xt, bxt, HB, bHB))
        moe_alloc(c.nc, es, part=3, sb=sb)
        moe_rest(c, g, sb, X, bXn, HB, bHB, w_gate, w_up, w_down)
        barrier(c)
    return bXn


def t5_onehot():
    half = 16; max_exact = 8
    rel = np.arange(-255, 256)
    n = np.abs(rel)
    nf = np.maximum(n, 1).astype(np.float32)
    large = max_exact + (np.log(nf / np.float32(max_exact)) / np.float32(math.log(128 / max_exact)) * np.float32(half - max_exact)).astype(np.int32)
    large = np.minimum(large, half - 1)
    b = np.where(rel > 0, half, 0) + np.where(n < max_exact, n, large)
    oh = np.zeros((32, 512), np.float32)
    oh[b, np.arange(511)] = 1.0
    return oh


def attn_phase(c, g, PV, bPV, q_gain, k_gain, c_lambda, out_gain, rel_bias, onehot, layer_idx, QKT, bQKT, FV, bFV, MT, bMT, stage=3):
    nc = c.nc
    lam_init = 0.8 - 0.6 * math.exp(-0.3 * layer_idx)
    ptb, bptb = g.psb
    with ExitStack() as es:
        def T(name, shape, dt):
            return es.enter_context(nc.sbuf_tensor(uniq(name), shape, dt))
        g64 = T("at_g64", [128, 2, 64], F32); bg64 = Buf()
        gQK = T("at_gQK", [128, 16, 64], F32); bgQK = Buf()
        xq = [T("at_xq%d" % i, [128, 1024], BF16) for i in range(2)]; bxq = [Buf(), Buf()]
        sq = T("at_sq", [128, 1024], F32); bsq = Buf()
        ss = [T("at_ss%d" % i, [128, 16], F32) for i in range(2)]; bss = [Buf(), Buf()]
        xn = T("at_xn", [128, 1024], F32); bxn = Buf()
        xb = [T("at_xb%d" % i, [128, 1024], BF16) for i in range(2)]; bxb = [Buf(), Buf()]
        st = [T("at_st%d" % i, [128, 8, 512], BF16) for i in range(2)]; bst = [Buf(), Buf()]
        c.dma('sp', lambda e: e.dma_start(out=g64[:, 0, :], in_=q_gain.partition_broadcast(128)), pwrites=[bg64])
        c.dma('sp', lambda e: e.dma_start(out=g64[:, 1, :], in_=k_gain.partition_broadcast(128)), pwrites=[bg64])
        bg64.seal()
        c.op('dve', lambda e: e.tensor_scalar(out=gQK[:, 0:8, :], in0=g64[:, 0:1, :].broadcast_to([128, 8, 64]), scalar1=0.125, scalar2=None, op0=ALU.mult), reads=[bg64], writes=[bgQK])
        c.op('dve', lambda e: e.tensor_copy(out=gQK[:, 8:16, :], in_=g64[:, 1:2, :].broadcast_to([128, 8, 64])), reads=[bg64, bgQK], writes=[bgQK])
        for i in range(NT):
            xq_, bxq_ = xq[i % 2], bxq[i % 2]
            ss_, bss_ = ss[i % 2], bss[i % 2]
            xb_, bxb_ = xb[i % 2], bxb[i % 2]
            st_, bst_ = st[(i // 4) % 2], bst[(i // 4) % 2]
            c.dma('sp', lambda e: e.dma_start(out=xq_[:], in_=PV[i * 128:(i + 1) * 128, 0:1024]), reads=[bPV], writes=[bxq_])
            c.op('act', lambda e: e.activation(out=sq[:], in_=xq_[:], func=AF.Square), reads=[bxq_], writes=[bsq])
            c.op('dve', lambda e: e.tensor_reduce(out=ss_[:], in_=sq[:].rearrange("p (a d) -> p a d", d=64), axis=AX.X, op=ALU.add), reads=[bsq], writes=[bss_])
            c.op('dve', lambda e: e.tensor_scalar(out=ss_[:], in0=ss_[:], scalar1=1.0 / 64, scalar2=1e-6, op0=ALU.mult, op1=ALU.add), reads=[bss_], writes=[bss_])
            c.op('pool', lambda e: e.tensor_tensor(out=ss_[:], in0=ss_[:], in1=g.neghalf[:, 0:1].broadcast_to([128, 16]), op=ALU.pow), reads=[bss_, g.b_neghalf], writes=[bss_])
            c.op('dve', lambda e: e.tensor_tensor(out=xn[:].rearrange("p (a d) -> p a d", d=64), in0=xq_[:].rearrange("p (a d) -> p a d", d=64),
                                                  in1=ss_[:].unsqueeze(2).broadcast_to([128, 16, 64]), op=ALU.mult), reads=[bxq_, bss_], writes=[bxn])
            c.op('dve', lambda e: e.tensor_tensor(out=xb_[:], in0=xn[:], in1=gQK[:].rearrange("p a d -> p (a d)"), op=ALU.mult), reads=[bxn, bgQK], writes=[bxb_])
            for a in range(8):
                c.op('pe', lambda e: e.transpose(out=ptb[:, a * 128:(a + 1) * 128], in_=xb_[:, a * 128:(a + 1) * 128], identity=g.identb[:]), reads=[bxb_, g.b_identb], writes=[bptb])
            c.op('act', lambda e: e.activation(out=st_[:, :, (i % 4) * 128:(i % 4 + 1) * 128], in_=ptb[:, :].rearrange("p (a s) -> p a s", a=8), func=AF.Copy), reads=[bptb], writes=[bst_])
            if i % 4 == 3:
                t0 = (i // 4) * 512
                for a in range(8):
                    c.dma('sp' if a % 2 else 'act', lambda e: e.dma_start(out=QKT[a, :, t0:t0 + 512], in_=st_[:, a, :]), reads=[bst_], pwrites=[bQKT])
        bQKT.seal()
        barrier(c)
    if stage < 2:
        return
    with ExitStack() as es:
        def T(name, shape, dt):
            return es.enter_context(nc.sbuf_tensor(uniq(name), shape, dt))
        KT = T("at_KT", [128, L], BF16); bKT = Buf()
        Va = T("at_Va", [128, 64, 130], BF16); bVa = Buf()
        Va_lo = T("at_Va_lo", [128, 64, 130], BF16); Va_hi = T("at_Va_hi", [128, 64, 130], BF16)
        ecf = T("at_ecf", [128, 4, 2], F32); becf = Buf()
        QT = [T("at_QT%d" % i, [128, 512], BF16) for i in range(2)]; bQT = [Buf(), Buf()]
        Pt = [T("at_P%d" % i, [128, 512], BF16) for i in range(4)]; bPt = [Buf() for _ in range(4)]
        tmp = [T("at_tmp%d" % i, [128, 512], F32) for i in range(2)]; btmp = [Buf(), Buf()]
        biasT = T("at_bias", [128, 4, 3, 128], F32); bbias = Buf()
        hank = T("at_hank", [128, 128], F32); bhank = Buf()
        cfar = T("at_cfar", [128, 4, 2], F32); bcfar = Buf()
        tab = T("at_tab", [32, 4], F32); btab = Buf()
        oh = T("at_oh", [32, 512], F32); boh = Buf()
        fv = T("at_fv", [4, 512], F32); bfv = Buf()
        lamt = T("at_lamt", [128, 4, 64], F32); blamt = Buf()
        lam = T("at_lam", [128, 8], F32); blam = Buf()
        gO = T("at_gO", [128, 128], F32); bgO = Buf()
        rs = [T("at_rs%d" % i, [128, 4], F32) for i in range(2)]; brs = [Buf(), Buf()]
        t1 = T("at_t1", [128, 128], F32); bt1 = Buf()
        w_ = T("at_w", [128, 128], F32); bw_ = Buf()
        junk = T("at_junk", [128, 128], F32); bjunk = Buf()
        wb = [T("at_wb%d" % i, [128, 128], BF16) for i in range(2)]; bwb = [Buf(), Buf()]
        ost = [T("at_ost%d" % i, [128, 512], BF16) for i in range(2)]; bost = [Buf(), Buf()]
        c.dma('sp', lambda e: e.dma_start(out=lamt[:].rearrange("p a d -> p (a d)"), in_=c_lambda.rearrange("a d -> (a d)").partition_broadcast(128)), writes=[blamt])
        c.op('dve', lambda e: e.tensor_tensor(out=lamt[:, 0, :], in0=lamt[:, 0, :], in1=lamt[:, 1, :], op=ALU.mult), reads=[blamt], writes=[blamt])
        c.op('dve', lambda e: e.tensor_tensor(out=lamt[:, 2, :], in0=lamt[:, 2, :], in1=lamt[:, 3, :], op=ALU.mult), reads=[blamt], writes=[blamt])
        c.op('dve', lambda e: e.tensor_reduce(out=lam[:, 0:1], in_=lamt[:, 0, :], axis=AX.X, op=ALU.add), reads=[blamt], writes=[blam])
        c.op('dve', lambda e: e.tensor_reduce(out=lam[:, 1:2], in_=lamt[:, 2, :], axis=AX.X, op=ALU.add), reads=[blamt, blam], writes=[blam])
        c.op('act', lambda e: e.activation(out=lam[:, 2:4], in_=lam[:, 0:2], func=AF.Exp), reads=[blam], writes=[blam])
        c.op('dve', lambda e: e.tensor_tensor(out=lam[:, 4:5], in0=lam[:, 3:4], in1=lam[:, 2:3], op=ALU.subtract), reads=[blam], writes=[blam])
        c.op('dve', lambda e: e.tensor_scalar(out=lam[:, 4:5], in0=lam[:, 4:5], scalar1=-lam_init, scalar2=None, op0=ALU.add), reads=[blam], writes=[blam])
        c.dma('sp', lambda e: e.dma_start(out=gO[:], in_=out_gain.partition_broadcast(128)), writes=[bgO])
        c.op('dve', lambda e: e.tensor_scalar(out=gO[:], in0=gO[:], scalar1=1.0 - lam_init, scalar2=None, op0=ALU.mult), reads=[bgO], writes=[bgO])
        c.dma('sp', lambda e: e.dma_start(out=tab[:], in_=rel_bias), writes=[btab])
        c.dma('act', lambda e: e.dma_start(out=oh[:], in_=onehot), writes=[boh])
        ps6, bps6 = g.ps[6]
        c.op('pe', lambda e: e.matmul(ps6[0:4, :], lhsT=tab[:, :], rhs=oh[:, :], start=True, stop=True), reads=[btab, boh], writes=[bps6])
        c.op('dve', lambda e: e.tensor_copy(out=fv[:], in_=ps6[0:4, :]), reads=[bps6], writes=[bfv])
        c.dma('sp', lambda e: e.dma_start(out=FV, in_=fv[:]), reads=[bfv], writes=[bFV])
        for h in range(4):
            for o in (-1, 0, 1):
                off = h * 512 + 128 * o + 128
                src = bass.AP(FV.tensor, off, [[1, 128], [1, 128]])
                c.dma('sp', lambda e: e.dma_start(out=hank[:], in_=src), reads=[bFV], writes=[bhank])
                c.op('dve', lambda e: e.tensor_copy(out=biasT[:, h, o + 1, :], in_=hank[:, ::-1]), reads=[bhank, bbias], writes=[bbias])
            c.dma('sp', lambda e: e.dma_start(out=cfar[:, h, 0:1], in_=bass.AP(FV.tensor, h * 512 + 0, [[0, 128], [1, 1]])), reads=[bFV], pwrites=[bcfar])
            c.dma('sp', lambda e: e.dma_start(out=cfar[:, h, 1:2], in_=bass.AP(FV.tensor, h * 512 + 510, [[0, 128], [1, 1]])), reads=[bFV], pwrites=[bcfar])
        bcfar.seal()
        c.op('act', lambda e: e.activation(out=ecf[:], in_=cfar[:], func=AF.Exp), reads=[bcfar], writes=[becf])
        ones_col_done = False
        pending = []
        nS = 0; nP = 0; nq = 0; ntmp = 0; nout = 0
        for h in range(4):
            c.dma('sp', lambda e: e.dma_start(out=KT[:], in_=QKT[4 + h, :, :]), reads=[bQKT], writes=[bKT])
            for half in range(2):
                c.dma('act', lambda e: e.dma_start(out=Va[:, half * 32:(half + 1) * 32, 0:128], in_=PV[half * 4096:(half + 1) * 4096, 1024 + h * 128:1024 + (h + 1) * 128].rearrange("(b p) d -> p b d", p=128)),
                      reads=[bPV], writes=[bVa])
            c.op('pool', lambda e: e.memset(Va[:, :, 128:129], 1.0), reads=[bVa], writes=[bVa])
            c.op('act', lambda e: e.activation(out=Va_lo[:, :, 0:129], in_=Va[:, :, 0:129], func=AF.Copy, scale=ecf[:, h, 0:1]), reads=[bVa, becf], writes=[bVa])
            c.op('act', lambda e: e.activation(out=Va_hi[:, :, 0:129], in_=Va[:, :, 0:129], func=AF.Copy, scale=ecf[:, h, 1:2]), reads=[bVa, becf], writes=[bVa])
            for qt in range(16):
                QT_, bQT_ = QT[nq % 2], bQT[nq % 2]; nq += 1
                c.dma('sp', lambda e: e.dma_start(out=QT_[:], in_=QKT[h, :, qt * 512:(qt + 1) * 512]), reads=[bQKT], writes=[bQT_])
                steps = [(comp, kb) for kb in range(64) for comp in range(2)]
                Sbank = {}

                SB = [0, 1, 2, 6]

                def emit_S(i):
                    comp, kb = steps[i]
                    S, bS = g.ps[SB[i % 4]]
                    c.op('pe', lambda e: e.matmul(S[:, :], lhsT=KT[64 * comp:64 * comp + 64, kb * 128:(kb + 1) * 128], rhs=QT_[64 * comp:64 * comp + 64, :], start=True, stop=True),
                         reads=[bKT, bQT_], writes=[bS])

                def emit_exp(i):
                    comp, kb = steps[i]
                    S, bS = g.ps[SB[i % 4]]
                    P_, bP_ = Pt[i % 4], bPt[i % 4]
                    near = (4 * qt - 1 <= kb <= 4 * qt + 4)
                    if not near:
                        c.op('act', lambda e: e.activation(out=P_[:], in_=S[:, :], func=AF.Exp), reads=[bS], writes=[bP_])
                    else:
                        tm, btm = tmp[i % 2], btmp[i % 2]
                        for qs in range(4):
                            o = kb - (4 * qt + qs)
                            sl = slice(qs * 128, (qs + 1) * 128)
                            if abs(o) <= 1:
                                c.op('dve', lambda e: e.tensor_tensor(out=tm[:, sl], in0=S[:, sl], in1=biasT[:, h, o + 1, :], op=ALU.add), reads=[bS, bbias, btm], writes=[btm])
                            else:
                                col = cfar[:, h, 0:1] if o < 0 else cfar[:, h, 1:2]
                                c.op('dve', lambda e: e.tensor_scalar(out=tm[:, sl], in0=S[:, sl], scalar1=col, scalar2=None, op0=ALU.add), reads=[bS, bcfar, btm], writes=[btm])
                        c.op('act', lambda e: e.activation(out=P_[:], in_=tm[:], func=AF.Exp), reads=[btm], writes=[bP_])

                def emit_PV(i):
                    comp, kb = steps[i]
                    P_, bP_ = Pt[i % 4], bPt[i % 4]
                    near = (4 * qt - 1 <= kb <= 4 * qt + 4)
                    Vs = Va if near else (Va_lo if kb < 4 * qt else Va_hi)
                    for qs in range(4):
                        a = comp * 4 + qs
                        acc, bacc = g.ps[3 + a // 3]
                        c0 = (a % 3) * 130
                        first = (kb == 0) and ((comp == 0 and a in (0, 3)) or (comp == 1 and a == 6))
                        c.op('pe', lambda e: e.matmul(acc[:, c0:c0 + 129], lhsT=P_[:, qs * 128:(qs + 1) * 128], rhs=Vs[:, kb, 0:129], start=first, stop=(kb == 63), skip_group_check=True),
                             reads=[bP_, bVa], writes=[bacc])
                npair = len(steps) // 2
                emit_S(0); emit_S(1); emit_S(2); emit_S(3)
                for j in range(npair):
                    emit_exp(2 * j); emit_exp(2 * j + 1)
                    if j + 2 < npair:
                        emit_S(2 * j + 4); emit_S(2 * j + 5)
                    if j == 0:
                        while pending:
                            pending.pop(0)()
                    emit_PV(2 * j); emit_PV(2 * j + 1)
                def make_fin(h=h, qt=qt, slot=nout):
                    def fin():
                        os_, bos_ = ost[slot % 2], bost[slot % 2]
                        for qs in range(4):
                            a0 = qs; a1 = 4 + qs
                            acc0, bacc0 = g.ps[3 + a0 // 3]; o0 = (a0 % 3) * 130
                            acc1, bacc1 = g.ps[3 + a1 // 3]; o1 = (a1 % 3) * 130
                            rs_, brs_ = rs[qs % 2], brs[qs % 2]
                            wb_, bwb_ = wb[qs % 2], bwb[qs % 2]
                            c.op('dve', lambda e: e.reciprocal(out=rs_[:, 0:1], in_=acc0[:, o0 + 128:o0 + 129]), reads=[bacc0], writes=[brs_])
                            c.op('dve', lambda e: e.reciprocal(out=rs_[:, 1:2], in_=acc1[:, o1 + 128:o1 + 129]), reads=[bacc1, brs_], writes=[brs_])
                            c.op('dve', lambda e: e.tensor_tensor(out=rs_[:, 1:2], in0=rs_[:, 1:2], in1=lam[:, 4:5], op=ALU.mult), reads=[brs_, blam], writes=[brs_])
                            c.op('dve', lambda e: e.tensor_scalar(out=t1[:], in0=acc1[:, o1:o1 + 128], scalar1=rs_[:, 1:2], scalar2=None, op0=ALU.mult), reads=[bacc1, brs_], writes=[bt1])
                            c.op('dve', lambda e: e.scalar_tensor_tensor(out=w_[:], in0=acc0[:, o0:o0 + 128], scalar=rs_[:, 0:1], in1=t1[:], op0=ALU.mult, op1=ALU.add), reads=[bacc0, brs_, bt1], writes=[bw_])
                            c.op('dve', lambda e: e.scalar_tensor_tensor(out=junk[:], in0=w_[:], scalar=1.0, in1=w_[:], op0=ALU.mult, op1=ALU.mult, accum_out=rs_[:, 2:3]), reads=[bw_, brs_], writes=[bjunk, brs_])
                            c.op('dve', lambda e: e.tensor_scalar(out=rs_[:, 2:3], in0=rs_[:, 2:3], scalar1=1.0 / 128, scalar2=1e-6, op0=ALU.mult, op1=ALU.add), reads=[brs_], writes=[brs_])
                            c.op('pool', lambda e: e.tensor_tensor(out=rs_[:, 2:3], in0=rs_[:, 2:3], in1=g.neghalf[:, 0:1], op=ALU.pow), reads=[brs_, g.b_neghalf], writes=[brs_])
                            c.op('dve', lambda e: e.scalar_tensor_tensor(out=wb_[:], in0=w_[:], scalar=rs_[:, 2:3], in1=gO[:], op0=ALU.mult, op1=ALU.mult), reads=[bw_, brs_, bgO], writes=[bwb_])
                            c.op('pe', lambda e: e.transpose(out=ptb[:, qs * 128:(qs + 1) * 128], in_=wb_[:], identity=g.identb[:]), reads=[bwb_, g.b_identb], writes=[bptb])
                        c.op('act', lambda e: e.activation(out=os_[:], in_=ptb[:, 0:512], func=AF.Copy), reads=[bptb], writes=[bos_])
                        c.dma('sp', lambda e: e.dma_start(out=MT[h * 128:(h + 1) * 128, qt * 512:(qt + 1) * 512], in_=os_[:]), reads=[bos_], pwrites=[bMT])
                    return fin
                nout += 1
                pending.append(make_fin())
        while pending:
            pending.pop(0)()
        barrier(c)


GTB = 512


def gdn_phase(c, g, PT, bPT, PV, bPV, conv_w, a_log, dt_bias, out_gain, GQ, bGQ, GR, bGR, OD, bODs, OD2, bOD2s, MT, bMT, stage=4):
    nc = c.nc
    ptb, bptb = g.psb
    NB = TBK
    with ExitStack() as es:
        def T(name, shape, dt):
            return es.enter_context(nc.sbuf_tensor(uniq(name), shape, dt))
        cw = T("gd_cw", [128, 12, 5], F32); bcw = Buf()
        onesb = T("gd_onesb", [128, 128], BF16); bonesb = Buf()
        xin = [T("gd_xin%d" % i, [128, NB + 4], F32) for i in range(2)]; bxin = [Buf(), Buf()]
        y = T("gd_y", [128, NB], F32); by = Buf()
        s = T("gd_s", [128, NB], F32); bs = Buf()
        sqb = T("gd_sqb", [128, NB], BF16); bsqb = Buf()
        rst = T("gd_rst", [128, NB], F32); brst = Buf()
        ob = [T("gd_ob%d" % i, [128, NB], BF16) for i in range(2)]; bob = [Buf(), Buf()]
        with nc.allow_non_contiguous_dma(reason="small params"):
            for j in range(5):
                c.dma('sp', lambda e: e.dma_start(out=cw[:, :, j], in_=conv_w[j, :].rearrange("(k p) -> p k", p=128)), pwrites=[bcw])
        bcw.seal()
        c.op('pool', lambda e: e.memset(onesb[:], 1.0), writes=[bonesb])
        n = 0
        for cbk in range(12):
            for tb in range(L // NB):
                x_, bx_ = xin[n % 2], bxin[n % 2]
                o_, bo_ = ob[n % 2], bob[n % 2]
                n += 1
                t0 = tb * NB
                lo = max(t0 - 2, 0); hi = min(t0 + NB + 2, L)
                if tb == 0:
                    c.op('pool', lambda e: e.memset(x_[:, 0:2], 0.0), writes=[bx_])
                if tb == L // NB - 1:
                    c.op('pool', lambda e: e.memset(x_[:, NB + 2:NB + 4], 0.0), writes=[bx_])
                c.dma('sp', lambda e: e.dma_start(out=x_[:, lo - (t0 - 2):hi - (t0 - 2)], in_=PT[cbk * 128:(cbk + 1) * 128, lo:hi]), reads=[bPT, bx_], writes=[bx_])
                c.op('dve', lambda e: e.tensor_scalar(out=y[:], in0=x_[:, 0:NB], scalar1=cw[:, cbk, 0:1], scalar2=None, op0=ALU.mult), reads=[bx_, bcw], writes=[by])
                for j in range(1, 5):
                    c.op('dve', lambda e: e.scalar_tensor_tensor(out=y[:], in0=x_[:, j:j + NB], scalar=cw[:, cbk, j:j + 1], in1=y[:], op0=ALU.mult, op1=ALU.add),
                         reads=[bx_, bcw, by], writes=[by])
                c.op('act', lambda e: e.activation(out=s[:], in_=y[:], func=AF.Silu), reads=[by], writes=[bs])
                if cbk < 8:
                    c.op('act', lambda e: e.activation(out=sqb[:], in_=s[:], func=AF.Square), reads=[bs], writes=[bsqb])
                    for hf in range(NB // 512):
                        ps, bps = g.ps[hf % 4]
                        c.op('pe', lambda e: e.matmul(ps[:, :], lhsT=onesb[:], rhs=sqb[:, hf * 512:(hf + 1) * 512], start=True, stop=True), reads=[bonesb, bsqb], writes=[bps])
                        c.op('dve', lambda e: e.tensor_scalar(out=rst[:, hf * 512:(hf + 1) * 512], in0=ps[:, :], scalar1=1e-6, scalar2=None, op0=ALU.add), reads=[bps, brst], writes=[brst])
                    c.op('act', lambda e: e.activation(out=rst[:], in_=rst[:], func=AF.Ln), reads=[brst], writes=[brst])
                    c.op('act', lambda e: e.activation(out=rst[:], in_=rst[:], func=AF.Exp, scale=-0.5), reads=[brst], writes=[brst])
                    sc = (128.0 ** -0.5) if cbk < 4 else 1.0
                    c.op('dve', lambda e: e.scalar_tensor_tensor(out=o_[:], in0=s[:], scalar=sc, in1=rst[:], op0=ALU.mult, op1=ALU.mult), reads=[bs, brst], writes=[bo_])
                else:
                    c.op('act', lambda e: e.activation(out=o_[:], in_=s[:], func=AF.Copy), reads=[bs], writes=[bo_])
                c.dma('act', lambda e: e.dma_start(out=GQ[cbk, :, t0:t0 + NB], in_=o_[:]), reads=[bo_], pwrites=[bGQ])
        bGQ.seal()
        barrier(c)
    if stage < 2:
        return
    with ExitStack() as es0:
        def T0(name, shape, dt):
            return es0.enter_context(nc.sbuf_tensor(uniq(name), shape, dt))
        NQ = 5
        cols = [T0("gd_cols%d" % d, [128, 64, 4 * NQ], F32) for d in range(2)]; bcols = [Buf(), Buf()]
        sel = T0("gd_sel", [4, 4, 128], F32); bsel = Buf()
        with ExitStack() as es:
            def T(name, shape, dt):
                return es.enter_context(nc.sbuf_tensor(uniq(name), shape, dt))
            GP = 2048
            ar = T("gd_ar", [4, GP], F32); bar_ = Buf()
            br = T("gd_br", [4, GP], F32); bbr = Buf()
            w1 = T("gd_w1", [4, GP], F32); bw1 = Buf()
            w2 = T("gd_w2", [4, GP], F32); bw2 = Buf()
            gam = T("gd_gam", [4, GP], F32); bet = T("gd_bet", [4, GP], F32); egam = T("gd_egam", [4, GP], F32); brw = Buf()
            q3 = T("gd_q3", [4, GP], F32); bq3 = Buf()
            q4 = T("gd_q4", [4, GP], F32); bq4 = Buf()
            q5 = T("gd_q5", [4, GP], F32); bq5 = Buf()
            msk = T("gd_msk", [4, GP], F32); bmsk = Buf()
            pc = T("gd_pc", [4, 4], F32); bpc = Buf()
            c.op('pool', lambda e: e.memset(msk[:], 1.0), writes=[bmsk])
            c.op('pool', lambda e: e.memset(msk[:].rearrange("p (c j) -> p c j", j=64)[:, :, 0:1], 0.0), reads=[bmsk], writes=[bmsk])
            c.op('pool', lambda e: e.memset(sel[:], 0.0), writes=[bsel])
            c.op('pool', lambda e: e.affine_select(out=sel[:], in_=sel[:], pattern=[[-1, 4], [0, 128]], compare_op=ALU.not_equal, fill=1.0, base=0, channel_multiplier=1),
                 reads=[bsel], writes=[bsel])
            for d in range(2):
                with nc.allow_non_contiguous_dma(reason="small params"):
                    c.dma('sp', lambda e: e.dma_start(out=pc[:, 0:1], in_=dt_bias[d, :].rearrange("(h o) -> h o", o=1)), reads=[bpc], writes=[bpc])
                    c.dma('sp', lambda e: e.dma_start(out=pc[:, 1:2], in_=a_log[d, :].rearrange("(h o) -> h o", o=1)), reads=[bpc], writes=[bpc])
                c.op('act', lambda e: e.activation(out=pc[:, 2:3], in_=pc[:, 1:2], func=AF.Exp), reads=[bpc], writes=[bpc])
                c.op('dve', lambda e: e.tensor_scalar(out=pc[:, 2:3], in0=pc[:, 2:3], scalar1=-1.0, scalar2=None, op0=ALU.mult), reads=[bpc], writes=[bpc])
                for tp in range(L // GP):
                    nbp = tp if d == 0 else L // GP - 1 - tp
                    c.dma('sp', lambda e: e.dma_start(out=ar[:], in_=PT[1536 + 4 * d:1540 + 4 * d, nbp * GP:(nbp + 1) * GP]), reads=[bPT, bar_], writes=[bar_])
                    c.dma('act', lambda e: e.dma_start(out=br[:], in_=PT[1544 + 4 * d:1548 + 4 * d, nbp * GP:(nbp + 1) * GP]), reads=[bPT, bbr], writes=[bbr])
                    asrc = ar[:, ::-1] if d else ar[:, :]
                    bsrc = br[:, ::-1] if d else br[:, :]
                    c.op('dve', lambda e: e.tensor_scalar(out=w1[:], in0=asrc, scalar1=pc[:, 0:1], scalar2=None, op0=ALU.add), reads=[bar_, bpc], writes=[bw1])
                    c.op('dve', lambda e: e.tensor_scalar(out=w2[:], in0=w1[:], scalar1=-1.0, scalar2=None, op0=ALU.mult), reads=[bw1], writes=[bw2])
                    c.op('dve', lambda e: e.tensor_tensor(out=w2[:], in0=w2[:], in1=w1[:], op=ALU.min), reads=[bw1, bw2], writes=[bw2])
                    c.op('act', lambda e: e.activation(out=w2[:], in_=w2[:], func=AF.Exp), reads=[bw2], writes=[bw2])
                    c.op('act', lambda e: e.activation(out=w2[:], in_=w2[:], func=AF.Ln, bias=1.0, scale=1.0), reads=[bw2], writes=[bw2])
                    c.op('dve', lambda e: e.scalar_tensor_tensor(out=w1[:], in0=w1[:], scalar=0.0, in1=w2[:], op0=ALU.max, op1=ALU.add), reads=[bw1, bw2], writes=[bw1])
                    c.op('dve', lambda e: e.tensor_scalar(out=w1[:], in0=w1[:], scalar1=pc[:, 2:3], scalar2=None, op0=ALU.mult), reads=[bw1, bpc], writes=[bw1])
                    c.op('dve', lambda e: e.tensor_tensor_scan(out=gam[:], data0=msk[:], data1=w1[:], initial=0.0, op0=ALU.mult, op1=ALU.add), reads=[bmsk, bw1, brw], writes=[brw])
                    c.op('act', lambda e: e.activation(out=bet[:], in_=bsrc, func=AF.Sigmoid), reads=[bbr, brw], writes=[brw])
                    c.op('act', lambda e: e.activation(out=egam[:], in_=gam[:], func=AF.Exp), reads=[brw], writes=[brw])
                    c.op('dve', lambda e: e.tensor_tensor(out=q3[:], in0=bet[:], in1=egam[:], op=ALU.mult), reads=[brw, bq3], writes=[bq3])
                    g3 = gam[:].rearrange("p (c j) -> p c j", j=64)
                    c.op('dve', lambda e: e.tensor_tensor(out=q4[:].rearrange("p (c j) -> p c j", j=64), in0=g3[:, :, 63:64].broadcast_to([4, GP // 64, 64]), in1=g3, op=ALU.subtract),
                         reads=[brw, bq4], writes=[bq4])
                    c.op('act', lambda e: e.activation(out=q4[:], in_=q4[:], func=AF.Exp), reads=[bq4], writes=[bq4])
                    c.op('dve', lambda e: e.tensor_scalar(out=q5[:], in0=gam[:], scalar1=-1.0, scalar2=None, op0=ALU.mult), reads=[brw, bq5], writes=[bq5])
                    quants = [(gam, brw), (bet, brw), (q3, bq3), (q4, bq4), (q5, bq5)]
                    for bl in range(GP // 128):
                        blk = tp * (GP // 128) + bl
                        pc_, bpc_ = g.ps[blk % 2]
                        for qi, (qt_, bq_) in enumerate(quants):
                            c.op('pe', lambda e: e.transpose(out=pc_[:, qi * 4:(qi + 1) * 4], in_=qt_[0:4, bl * 128:(bl + 1) * 128], identity=g.ident32[0:4, 0:4]),
                                 reads=[bq_, g.b_ident32], writes=[bpc_])
                        c.op('act', lambda e: e.activation(out=cols[d][:, blk, :], in_=pc_[:, 0:4 * NQ], func=AF.Copy), reads=[bpc_, bcols[d]], writes=[bcols[d]])
                    for qi, rt in enumerate((gam, bet, egam)):
                        c.dma('sp', lambda e: e.dma_start(out=GR[d, qi, :, tp * GP:(tp + 1) * GP], in_=rt[:]), reads=[brw], pwrites=[bGR])
            bGR.seal()
            barrier(c)
        if stage < 3:
            return
        with ExitStack() as es:
            def T(name, shape, dt):
                return es.enter_context(nc.sbuf_tensor(uniq(name), shape, dt))
            nm_le = T("gm_nmle", [128, 128], F32)
            nm_geT = T("gm_nmgeT", [128, 128], F32)
            m_stT = T("gm_mstT", [128, 128], F32)
            bmk = Buf()
            c.op('pool', lambda e: e.memset(nm_le[:], 0.0), writes=[bmk])
            c.op('pool', lambda e: e.affine_select(out=nm_le[:], in_=nm_le[:], pattern=[[-1, 128]], compare_op=ALU.is_gt, fill=-30000.0, base=0, channel_multiplier=1), reads=[bmk], writes=[bmk])
            c.op('pool', lambda e: e.memset(nm_le[64:128, 0:64], -30000.0), reads=[bmk], writes=[bmk])
            c.op('pool', lambda e: e.memset(nm_geT[:], 0.0), reads=[bmk], writes=[bmk])
            c.op('pool', lambda e: e.affine_select(out=nm_geT[:], in_=nm_geT[:], pattern=[[1, 128]], compare_op=ALU.is_ge, fill=-30000.0, base=0, channel_multiplier=-1), reads=[bmk], writes=[bmk])
            c.op('pool', lambda e: e.memset(nm_geT[0:64, 64:128], -30000.0), reads=[bmk], writes=[bmk])
            c.op('pool', lambda e: e.memset(m_stT[:], 1.0), reads=[bmk], writes=[bmk])
            c.op('pool', lambda e: e.affine_select(out=m_stT[:], in_=m_stT[:], pattern=[[1, 128]], compare_op=ALU.is_gt, fill=0.0, base=0, channel_multiplier=-1), reads=[bmk], writes=[bmk])

            class CH:
                pass
            chs = []
            for d in range(2):
                ch = CH(); chs.append(ch)
                ch.d = d

                def TT(name, shape, dt, d=d):
                    return (T("gm%d_%s" % (d, name), shape, dt), Buf())
                ch.nat = [TT("nat%d" % i, [128, GTB], BF16) for i in range(3)]
                ch.arr = [[TT("arr%d_%d" % (i, j), [128, GTB], BF16) for j in range(2)] for i in range(3)]
                ch.rts = [[TT("rt%d_%d" % (q, j), [4, GTB], F32) for j in range(2)] for q in range(3)]
                ch.S32 = TT("S32", [128, 128], F32); ch.Sb = TT("Sb", [128, 128], BF16)
                ch.tmpD = TT("tmpD", [128, 128], F32); ch.Dst = TT("Dst", [128, 128], F32); ch.DTi = TT("DTi", [128, 128], F32); ch.DTs = TT("DTs", [128, 128], F32)
                ch.A_ = TT("A", [128, 128], BF16); ch.AT_ = TT("AT", [128, 128], BF16); ch.atT = TT("attnT", [128, 128], BF16)
                ch.Pm = [TT("P%d" % i, [128, 128], BF16) for i in range(6)]
                ch.Qm = [TT("Q%d" % i, [128, 128], BF16) for i in range(5)]
                ch.W32 = TT("W32", [128, 256], F32); ch.Wb = TT("Wb", [128, 256], BF16)
                ch.kdec = TT("kdec", [128, 128], BF16); ch.kcT = TT("kcT", [128, 128], BF16); ch.qdec = TT("qdec", [128, 128], BF16); ch.vnew = TT("vnew", [128, 128], BF16)
                ch.elc = TT("elc", [128, 2], F32)
                ch.osb = [TT("osb%d" % i, [128, 128], F32) for i in range(2)]
                ch.osf = [TT("osf%d" % i, [128, 128], F32) for i in range(2)]
                ch.bA = g.ps[3 * d + 0]; ch.bB = g.ps[3 * d + 1]; ch.bC = g.ps[3 * d + 2]
                ch.pb0 = 512 * d
                ch.nblk = 0

            def block_gen(ch, h, blk, b, cur, rcur):
                d = ch.d
                (qA, bqA), (kA, bkA), (vA, bvA) = cur
                bs_ = slice(b * 128, (b + 1) * 128)
                cl = cols[d][:, blk, :]
                gcol = cl[:, 0 + h:0 + h + 1]; bcol = cl[:, 4 + h:4 + h + 1]; begcol = cl[:, 8 + h:8 + h + 1]
                ekdcol = cl[:, 12 + h:12 + h + 1]; ngcol = cl[:, 16 + h:16 + h + 1]
                pA, bpA = ch.bA; pB, bpB = ch.bB; pC, bpC = ch.bC
                pb0 = ch.pb0
                tmpD, btmpD = ch.tmpD; Dst, bDst = ch.Dst; DTi, bDTi = ch.DTi; DTs, bDTs = ch.DTs
                A_, bA_ = ch.A_; AT_, bAT_ = ch.AT_; atT, batT = ch.atT
                W32, bW32 = ch.W32; Wb, bWb = ch.Wb; kdec, bkdec = ch.kdec; kcT, bkcT = ch.kcT; qdec, bqdec = ch.qdec; vnew, bvnew = ch.vnew
                elc, belc = ch.elc; S32, bS32 = ch.S32; Sb, bSb = ch.Sb
                for qi, (rt, brt) in enumerate(rcur):
                    c.op('pe', lambda e: e.matmul(pA[:, qi * 128:(qi + 1) * 128], lhsT=sel[:, h, :], rhs=rt[0:4, bs_], start=True, stop=True, skip_group_check=True),
                         reads=[bsel, brt], writes=[bpA])
                    yield
                c.op('pe', lambda e: e.matmul(pB[:, 0:128], lhsT=kA[:, bs_], rhs=kA[:, bs_], start=True, stop=True, skip_group_check=True), reads=[bkA], writes=[bpB]); yield
                c.op('pe', lambda e: e.matmul(pB[:, 128:256], lhsT=kA[:, bs_], rhs=qA[:, bs_], start=True, stop=True, skip_group_check=True), reads=[bkA, bqA], writes=[bpB]); yield
                c.op('pe', lambda e: e.transpose(out=ptb[:, pb0:pb0 + 128], in_=vA[:, bs_], identity=g.identb[:]), reads=[bvA, g.b_identb], writes=[bptb]); yield
                c.op('pe', lambda e: e.transpose(out=ptb[:, pb0 + 128:pb0 + 256], in_=kA[:, bs_], identity=g.identb[:]), reads=[bkA, g.b_identb], writes=[bptb]); yield
                c.op('dve', lambda e: e.scalar_tensor_tensor(out=tmpD[:], in0=pA[:, 0:128], scalar=-1.0, in1=nm_le[:], op0=ALU.mult, op1=ALU.add), reads=[bpA, bmk], writes=[btmpD]); yield
                c.op('act', lambda e: e.activation(out=Dst[:], in_=tmpD[:], func=AF.Exp, bias=gcol, scale=1.0), reads=[btmpD, bcols[d]], writes=[bDst]); yield
                c.op('dve', lambda e: e.tensor_tensor(out=tmpD[:], in0=pA[:, 0:128], in1=nm_geT[:], op=ALU.add), reads=[bpA, bmk, btmpD], writes=[btmpD]); yield
                c.op('act', lambda e: e.activation(out=DTi[:], in_=tmpD[:], func=AF.Exp, bias=ngcol, scale=1.0), reads=[btmpD, bcols[d]], writes=[bDTi]); yield
                c.op('dve', lambda e: e.tensor_tensor(out=DTs[:], in0=DTi[:], in1=m_stT[:], op=ALU.mult), reads=[bDTi, bmk], writes=[bDTs]); yield
                c.op('dve', lambda e: e.tensor_tensor(out=DTs[:], in0=pA[:, 128:256], in1=DTs[:], op=ALU.mult), reads=[bpA, bDTs], writes=[bDTs]); yield
                c.op('dve', lambda e: e.scalar_tensor_tensor(out=A_[:], in0=pB[:, 0:128], scalar=bcol, in1=Dst[:], op0=ALU.mult, op1=ALU.mult), reads=[bpB, bcols[d], bDst], writes=[bA_]); yield
                c.op('dve', lambda e: e.tensor_tensor(out=AT_[:], in0=pB[:, 0:128], in1=DTs[:], op=ALU.mult), reads=[bpB, bDTs], writes=[bAT_]); yield
                c.op('dve', lambda e: e.tensor_tensor(out=atT[:], in0=pB[:, 128:256], in1=DTi[:], op=ALU.mult), reads=[bpB, bDTi], writes=[batT]); yield
                c.op('act', lambda e: e.activation(out=Wb[:, 0:128], in_=ptb[:, pb0:pb0 + 128], func=AF.Copy, scale=bcol), reads=[bptb, bcols[d], bWb], writes=[bWb]); yield
                c.op('act', lambda e: e.activation(out=Wb[:, 128:256], in_=ptb[:, pb0 + 128:pb0 + 256], func=AF.Copy, scale=begcol), reads=[bptb, bcols[d], bWb], writes=[bWb]); yield
                c.op('act', lambda e: e.activation(out=kdec[:], in_=ptb[:, pb0 + 128:pb0 + 256], func=AF.Copy, scale=ekdcol), reads=[bptb, bcols[d]], writes=[bkdec]); yield
                c.op('dve', lambda e: e.tensor_tensor(out=qdec[:], in0=pA[:, 256:384], in1=qA[:, bs_], op=ALU.mult), reads=[bpA, bqA], writes=[bqdec]); yield
                c.op('act', lambda e: e.activation(out=elc[:], in_=pA[:, 256:384].rearrange("p (c j) -> p c j", j=64)[:, :, 63], func=AF.Copy), reads=[bpA], writes=[belc]); yield
                Pc, bPc = AT_, bAT_
                Qc, bQc = A_, bA_
                for lev in range(6):
                    c.op('pe', lambda e: e.matmul(pC[:, 0:256], lhsT=Pc[:], rhs=Wb[:], start=True, stop=True, skip_group_check=True), reads=[bPc, bWb], writes=[bpC]); yield
                    c.op('dve', lambda e: e.tensor_tensor(out=Wb[:], in0=Wb[:], in1=pC[:, 0:256], op=(ALU.subtract if lev == 0 else ALU.add)), reads=[bWb, bpC], writes=[bWb]); yield
                    if lev < 5:
                        Pn, bPn = ch.Pm[lev + 1]
                        c.op('pe', lambda e: e.matmul(pB[:, 256:384], lhsT=Qc[:], rhs=Pc[:], start=True, stop=True, skip_group_check=True), reads=[bQc, bPc], writes=[bpB]); yield
                        if lev < 4:
                            Qn, bQn = ch.Qm[lev + 1]
                            c.op('pe', lambda e: e.matmul(pB[:, 384:512], lhsT=Pc[:], rhs=Qc[:], start=True, stop=True, skip_group_check=True), reads=[bQc, bPc], writes=[bpB]); yield
                            c.op('act', lambda e: e.activation(out=Qn[:], in_=pB[:, 384:512], func=AF.Copy), reads=[bpB], writes=[bQn]); yield
                        c.op('act', lambda e: e.activation(out=Pn[:], in_=pB[:, 256:384], func=AF.Copy), reads=[bpB], writes=[bPn]); yield
                        Pc, bPc = Pn, bPn
                        if lev < 4:
                            Qc, bQc = Qn, bQn
                c.op('pe', lambda e: e.transpose(out=ptb[:, pb0 + 256:pb0 + 384], in_=Wb[:, 128:256], identity=g.identb[:]), reads=[bWb, g.b_identb], writes=[bptb]); yield
                c.op('act', lambda e: e.activation(out=kcT[:], in_=ptb[:, pb0 + 256:pb0 + 384], func=AF.Copy), reads=[bptb], writes=[bkcT]); yield
                for ci in range(2):
                    r0 = 64 * ci
                    c.op('pe', lambda e: e.matmul(pC[r0:r0 + 64, 256:384], lhsT=kcT[:, r0:r0 + 64], rhs=Sb[:, :], start=True, stop=True, skip_group_check=True), reads=[bkcT, bSb], writes=[bpC]); yield
                    c.op('dve', lambda e: e.tensor_tensor(out=vnew[r0:r0 + 64, :], in0=Wb[r0:r0 + 64, 0:128], in1=pC[r0:r0 + 64, 256:384], op=ALU.subtract), reads=[bWb, bpC, bvnew], writes=[bvnew]); yield
                    c.op('pe', lambda e: e.matmul(pA[r0:r0 + 64, 384:512], lhsT=qdec[:, r0:r0 + 64], rhs=Sb[:, :], start=True, stop=False, skip_group_check=True), reads=[bqdec, bSb], writes=[bpA])
                    c.op('pe', lambda e: e.matmul(pA[r0:r0 + 64, 384:512], lhsT=atT[r0:r0 + 64, r0:r0 + 64], rhs=vnew[r0:r0 + 64, :], start=False, stop=True, skip_group_check=True), reads=[batT, bvnew], writes=[bpA]); yield
                    c.op('pe', lambda e: e.matmul(pC[:, 384:512], lhsT=kdec[r0:r0 + 64, :], rhs=vnew[r0:r0 + 64, :], start=True, stop=True, skip_group_check=True), reads=[bkdec, bvnew], writes=[bpC]); yield
                    c.op('dve', lambda e: e.scalar_tensor_tensor(out=Sb[:], in0=S32[:], scalar=elc[:, ci:ci + 1], in1=pC[:, 384:512], op0=ALU.mult, op1=ALU.add), reads=[bS32, belc, bpC], writes=[bSb]); yield
                    c.op('dve', lambda e: e.scalar_tensor_tensor(out=S32[:], in0=S32[:], scalar=elc[:, ci:ci + 1], in1=pC[:, 384:512], op0=ALU.mult, op1=ALU.add), reads=[bS32, belc, bpC], writes=[bS32]); yield
                os_, bos_ = ch.osb[ch.nblk % 2]
                of_, bof_ = ch.osf[ch.nblk % 2]
                ch.nblk += 1
                c.op('act', lambda e: e.activation(out=os_[:], in_=pA[:, 384:512], func=AF.Copy), reads=[bpA], writes=[bos_]); yield
                if d == 0:
                    c.dma('sp', lambda e: e.dma_start(out=OD[blk * 128:(blk + 1) * 128, h * 128:(h + 1) * 128], in_=os_[:]), reads=[bos_], pwrites=[bODs[h]]); yield
                else:
                    pf, bpf = g.ps[6]
                    c.op('pe', lambda e: e.matmul(pf[:, 0:128], lhsT=g.J32[:], rhs=os_[:], start=True, stop=True), reads=[g.b_J32, bos_], writes=[bpf]); yield
                    c.op('act', lambda e: e.activation(out=of_[:], in_=pf[:, 0:128], func=AF.Copy), reads=[bpf], writes=[bof_]); yield
                    c.dma('act', lambda e: e.dma_start(out=OD2[L - (blk + 1) * 128:L - blk * 128, h * 128:(h + 1) * 128], in_=of_[:]), reads=[bof_], pwrites=[bOD2s[h]]); yield

            for h in range(4):
                for ch in chs:
                    S32, bS32 = ch.S32; Sb, bSb = ch.Sb
                    c.op('pool', lambda e: e.memset(S32[:], 0.0), reads=[bS32], writes=[bS32])
                    c.op('pool', lambda e: e.memset(Sb[:], 0.0), reads=[bSb], writes=[bSb])
                for tb in range(L // GTB):
                    curs = []; rcurs = []
                    for ch in chs:
                        d = ch.d
                        cur = []
                        for ai in range(3):
                            a_, ba_ = ch.arr[ai][tb % 2]
                            if d == 0:
                                c.dma('sp' if ai % 2 else 'act', lambda e: e.dma_start(out=a_[:], in_=GQ[ai * 4 + h, :, tb * GTB:(tb + 1) * GTB]), reads=[bGQ], writes=[ba_])
                            else:
                                n_, bn_ = ch.nat[ai]
                                c.dma('sp' if ai % 2 else 'act', lambda e: e.dma_start(out=n_[:], in_=GQ[ai * 4 + h, :, L - (tb + 1) * GTB:L - tb * GTB]), reads=[bGQ], writes=[bn_])
                                c.op('pool', lambda e: e.tensor_copy(out=a_[:], in_=n_[:, ::-1]), reads=[bn_], writes=[ba_])
                            cur.append((a_, ba_))
                        rcur = []
                        for qi in range(3):
                            r_, br_ = ch.rts[qi][tb % 2]
                            c.dma('sp', lambda e: e.dma_start(out=r_[:], in_=GR[d, qi, :, tb * GTB:(tb + 1) * GTB]), reads=[bGR], writes=[br_])
                            rcur.append((r_, br_))
                        curs.append(cur); rcurs.append(rcur)
                    for b in range(GTB // 128):
                        blk = tb * (GTB // 128) + b
                        gens = [block_gen(ch, h, blk, b, curs[i], rcurs[i]) for i, ch in enumerate(chs)]
                        alive = list(gens)
                        while alive:
                            for gnr in list(alive):
                                try:
                                    next(gnr)
                                except StopIteration:
                                    alive.remove(gnr)
                bODs[h].seal(); bOD2s[h].seal()
            barrier(c)
    if stage < 4:
        return
    gated_norm_finalize(c, g, OD, bODs, PV, bPV, 1536, out_gain, MT, bMT, 512, "gf_", OA2=OD2, bOA2s=bOD2s)


N_ACTIVE = 4
DEPTH = 4

PARAM_NAMES = ['mix_norm', 'ffn_norm', 'ev_w_in', 'ev_w_out', 'a_lb_logits', 'a_out_norm', 's5_lambda_re', 's5_lambda_im',
               's5_log_step', 's5_b_re', 's5_b_im', 's5_c_re', 's5_c_im', 's5_d', 's5_glu_w', 's5_glu_b', 'od_w_in', 'od_w_out',
               'c_q_norm', 'c_k_norm', 'c_lambda', 'c_out_norm', 'rel_bias', 'd_conv_w', 'd_a_log', 'd_dt_bias', 'd_out_norm',
               'moe_router', 'moe_w_gate', 'moe_w_up', 'moe_w_down']

PARAM_SHAPES = {
    'mix_norm': (4, 1024), 'ffn_norm': (4, 1024), 'ev_w_in': (2, 1024, 3072), 'ev_w_out': (2, 1024, 1024), 'a_lb_logits': (2, 2, 512),
    'a_out_norm': (2, 128), 's5_lambda_re': (2, 2, 32, 64), 's5_lambda_im': (2, 2, 32, 64), 's5_log_step': (2, 2, 32),
    's5_b_re': (2, 2, 32, 64, 16), 's5_b_im': (2, 2, 32, 64, 16), 's5_c_re': (2, 2, 32, 16, 64), 's5_c_im': (2, 2, 32, 16, 64),
    's5_d': (2, 512), 's5_glu_w': (2, 512, 512), 's5_glu_b': (2, 512), 'od_w_in': (2, 1024, 3600), 'od_w_out': (2, 1024, 1024),
    'c_q_norm': (2, 64), 'c_k_norm': (2, 64), 'c_lambda': (2, 4, 64), 'c_out_norm': (2, 128), 'rel_bias': (32, 4),
    'd_conv_w': (2, 5, 1536), 'd_a_log': (2, 2, 4), 'd_dt_bias': (2, 2, 4), 'd_out_norm': (2, 128), 'moe_router': (4, 1024, 16),
    'moe_w_gate': (4, 16, 1024, 2048), 'moe_w_up': (4, 16, 1024, 2048), 'moe_w_down': (4, 16, 2048, 1024)}


def build_program(layers=range(DEPTH), do_mixer=True, do_moe=True):
    nc = bass.Bass('TRN2', target_bir_lowering=False)
    xin = nc.dram_tensor("x", [L, D], F32, kind="ExternalInput").ap()
    P = {n: nc.dram_tensor(n, list(PARAM_SHAPES[n]), F32, kind="ExternalInput").ap() for n in PARAM_NAMES}
    onehot = nc.dram_tensor("t5_onehot", [32, 512], F32, kind="ExternalInput").ap()
    X = nc.dram_tensor("y", [L, D], F32, kind="ExternalOutput").ap()
    PT = nc.dram_tensor("PT", [2048, L], F32).ap()
    PV = nc.dram_tensor("PV", [L, 2048], BF16).ap()
    QK = nc.dram_tensor("QK", [16, 128, L], BF16).ap()
    QK5 = QK.rearrange("(h r w) p t -> h r w p t", h=4, r=2)
    OA = nc.dram_tensor("OA", [L, 512], F32).ap()
    YT = nc.dram_tensor("YT", [512, L], F32).ap()
    MT = nc.dram_tensor("MT", [1024, L], BF16).ap()
    HB = nc.dram_tensor("HB", [L, D], BF16).ap()
    GQ = nc.dram_tensor("GQ", [12, 128, L], BF16).ap()
    GR = nc.dram_tensor("GR", [2, 3, 4, L], F32).ap()
    OD2 = nc.dram_tensor("OD2", [L, 512], F32).ap()
    FV = nc.dram_tensor("FV", [4, 512], F32).ap()
    c = Ctx(nc); g = G()
    setup_consts(c, g)
    bX = Buf('X'); bPT = Buf(); bPV = Buf(); bQK = Buf(); bOAs = [Buf() for _ in range(4)]; bYT = Buf(); bMT = Buf()
    bHB = Buf(); bGQ = Buf(); bGR = Buf(); bFV = Buf(); bOD2s = [Buf() for _ in range(4)]
    for r in range(0, L, 512):
        c.dma('sp', lambda e: e.dma_start(out=X[r:r + 512, :], in_=xin[r:r + 512, :]), pwrites=[bX])
    bX.seal()
    for layer in layers:
        j = layer // 2
        if do_mixer:
            if layer % 2 == 0:
                spec = [(0, 512, 'F', 0), (512, 512, 'F', 512), (1024, 512, 'F', 1024), (2560, 512, 'F', 1536), (1536, 512, 'T', 0), (2048, 512, 'T', 512)]
                proj_phase(c, g, X, bX, P['mix_norm'][layer], P['ev_w_in'][j], 3072, spec, PT, bPT, PV, bPV)
                hgrn2_phase(c, g, PT, bPT, PV, bPV, P['a_lb_logits'], j, P['a_out_norm'][j], QK5, bQK, OA, bOAs, OD2, bOD2s, MT, bMT)
                s5_phase(c, g, PT, bPT, P['s5_lambda_re'][j], P['s5_lambda_im'][j], P['s5_log_step'][j], P['s5_b_re'][j], P['s5_b_im'][j],
                         P['s5_c_re'][j], P['s5_c_im'][j], P['s5_d'][j], P['s5_glu_w'][j], P['s5_glu_b'][j], YT, bYT, MT, bMT)
                bMT.seal()
                wo = P['ev_w_out'][j]
            else:
                spec = [(0, 512, 'T', 0), (512, 512, 'T', 512), (1024, 512, 'T', 1024), (1536, 1536, 'F', 0), (3072, 16, 'F', 1536), (3088, 512, 'T', 1536)]
                proj_phase(c, g, X, bX, P['mix_norm'][layer], P['od_w_in'][j], 3600, spec, PT, bPT, PV, bPV)
                attn_phase(c, g, PV, bPV, P['c_q_norm'][j], P['c_k_norm'][j], P['c_lambda'][j], P['c_out_norm'][j], P['rel_bias'], onehot, layer,
                           QK, bQK, FV, bFV, MT, bMT)
                gdn_phase(c, g, PT, bPT, PV, bPV, P['d_conv_w'][j], P['d_a_log'][j], P['d_dt_bias'][j], P['d_out_norm'][j], GQ, bGQ, GR, bGR, OA, bOAs, OD2, bOD2s, MT, bMT)
                bMT.seal()
                wo = P['od_w_out'][j]
            bX = outproj_moe(c, g, MT, bMT, wo, X, bX, HB, bHB, P['ffn_norm'][layer], P['moe_router'][layer], P['moe_w_gate'][layer], P['moe_w_up'][layer], P['moe_w_down'][layer])
    barrier(c)
    c.finish([bX])
    return nc, c


def kernel(**inputs):
    x = np.ascontiguousarray(np.asarray(inputs['x'], dtype=np.float32))
    B = x.shape[0]
    assert B == N_ACTIVE and x.shape[1] == L and x.shape[2] == D
    nc, c = build_program()
    params = {n: np.ascontiguousarray(np.asarray(inputs[n], dtype=np.float32)) for n in PARAM_NAMES}
    oh = t5_onehot()
    in_maps = []
    for b in range(N_ACTIVE):
        m = {"x": x[b], "t5_onehot": oh}
        m.update(params)
        in_maps.append(m)
    res = run_bass_kernel_spmd(nc, in_maps, core_ids=list(range(N_ACTIVE)))
    out = np.stack([np.asarray(res.results[b]["y"], dtype=np.float32) for b in range(N_ACTIVE)], axis=0)
    return out
```
